# Optimizing a Trainium2 kernel written in Bass

```python
import jax
import jax.numpy as jnp
from jax import lax
import numpy as np

D_MODEL = 1024
BATCH = 8
SEQ = 2048
DEPTH = 2
DEC_BATCH = 128
DEC_SEQ = 1
PAST_LEN = 16384
PAGE_SIZE = 128

D_MIX = D_MODEL
SSD_W = D_MIX // 2
SSD_HEAD_DIM = 64
SSD_H = SSD_W // SSD_HEAD_DIM
SSD_N = 64
SSD_G = 2
SSD_CONV_W = 4
SSD_CONV_DIM = SSD_W + 2 * SSD_G * SSD_N
CHUNK = 128
RWKV_W = D_MIX // 4
RWKV_N = 64
RWKV_H = RWKV_W // RWKV_N
W_LORA = 32
A_LORA = 32
G_LORA = 64
RWKV_FEAT = 3 * RWKV_W + W_LORA + A_LORA + G_LORA
RWKV_GN_EPS = RWKV_N * 1e-5
GLA_W = D_MIX - SSD_W - RWKV_W
GLA_H = 4
GLA_DV = GLA_W // GLA_H
GLA_DK = GLA_DV // 2
GK_LORA = 16
GATE_NORMALIZER = 16.0
IN_SIZES = (SSD_W, SSD_CONV_DIM, SSD_H, RWKV_FEAT, GLA_H * GLA_DK, GLA_H * GLA_DK, GLA_W, GK_LORA, GLA_W)
IN_DIM = SSD_W + SSD_CONV_DIM + SSD_H + RWKV_FEAT + 2 * GLA_H * GLA_DK + GLA_W + GK_LORA + GLA_W
F_DENSE = ((8 * D_MODEL // 3 + 127) // 128) * 128
N_EXPERTS = 8
TOP_K = 2
F_EXPERT = D_MODEL
N_DENSE = (DEPTH + 1) // 2
N_MOE = DEPTH // 2
ALPHA = (2.0 * DEPTH) ** 0.25
BETA = (8.0 * DEPTH) ** -0.25
LN_EPS = 1e-5
RMS_EPS = 1e-6

kernel_name = 'hybrid_ssd_rwkv7_gla_decoder_step'


def _layernorm(x, g, b):
    xf = x.astype(jnp.float32)
    mu = jnp.mean(xf, axis=-1, keepdims=True)
    var = jnp.mean(jnp.square(xf - mu), axis=-1, keepdims=True)
    return ((xf - mu) * lax.rsqrt(var + LN_EPS)).astype(x.dtype) * g + b


def _rmsnorm(x, g):
    xf = x.astype(jnp.float32)
    return (xf * lax.rsqrt(jnp.mean(jnp.square(xf), axis=-1, keepdims=True) + RMS_EPS)).astype(x.dtype) * g


def _split(t, sizes):
    parts, off = [], 0
    for s in sizes:
        parts.append(t[..., off:off + s])
        off += s
    return parts


def _to_chunks(t, chunk):
    pad = (-t.shape[1]) % chunk
    t = jnp.pad(t, [(0, 0), (0, pad)] + [(0, 0)] * (t.ndim - 2))
    t = t.reshape((t.shape[0], t.shape[1] // chunk, chunk) + t.shape[2:])
    return jnp.moveaxis(t, 1, 0)


def _from_chunks(t, length):
    t = jnp.moveaxis(t, 0, 1)
    return t.reshape((t.shape[0], t.shape[1] * t.shape[2]) + t.shape[3:])[:, :length]


def _ssd_scan(x, dt, a, bm, cm, h0):
    length = x.shape[1]
    chunk = min(CHUNK, length)
    rep = SSD_H // SSD_G
    bm = jnp.repeat(bm, rep, axis=2)
    cm = jnp.repeat(cm, rep, axis=2)
    causal = jnp.tril(jnp.ones((chunk, chunk), dtype=bool))[None, :, :, None]

    def step(h, inp):
        xc, dtc, bc, cc = inp
        cum = jnp.cumsum(dtc.astype(jnp.float32) * a, axis=1)
        seg = cum[:, :, None, :] - cum[:, None, :, :]
        decay = jnp.exp(jnp.where(causal, seg, -jnp.inf))
        scores = jnp.einsum('bihn,bjhn->bijh', cc, bc) * decay * dtc[:, None, :, :]
        y = jnp.einsum('bijh,bjhp->bihp', scores, xc)
        y = y + jnp.einsum('bihn,bhpn->bihp', cc, h) * jnp.exp(cum)[..., None]
        tail = jnp.exp(cum[:, -1:, :] - cum) * dtc
        h_new = h * jnp.exp(cum[:, -1])[:, :, None, None] + jnp.einsum('bjhn,bjhp->bhpn', bc * tail[..., None], xc)
        return h_new.astype(h.dtype), y.astype(xc.dtype)

    xs = tuple(_to_chunks(t, chunk) for t in (x, dt, bm, cm))
    h, y = lax.scan(step, h0, xs)
    return _from_chunks(y, length), h


def _gla_scan(q, k, v, g, s0):
    length = q.shape[1]
    chunk = min(CHUNK, length)
    causal = jnp.tril(jnp.ones((chunk, chunk), dtype=bool))[None, :, :, None, None]

    def step(s, inp):
        qc, kc, vc, gc = inp
        cum = jnp.cumsum(gc.astype(jnp.float32), axis=1)
        seg = cum[:, :, None] - cum[:, None]
        decay = jnp.exp(jnp.where(causal, seg, -jnp.inf))
        att = jnp.einsum('bihd,bjhd,bijhd->bijh', qc, kc, decay)
        o = jnp.einsum('bijh,bjhe->bihe', att, vc)
        o = o + jnp.einsum('bihd,bhde->bihe', qc * jnp.exp(cum), s)
        tail = jnp.exp(cum[:, -1:] - cum)
        s_new = s * jnp.exp(cum[:, -1])[..., None] + jnp.einsum('bjhd,bjhe->bhde', kc * tail, vc)
        return s_new.astype(s.dtype), o.astype(vc.dtype)

    xs = tuple(_to_chunks(t, chunk) for t in (q, k, v, g))
    s, o = lax.scan(step, s0, xs)
    return _from_chunks(o, length), s


def _rwkv_scan(r, w, k, v, kk, a, s0):
    def step(s, inp):
        rt, wt, kt, vt, kkt, at = inp
        sa = jnp.einsum('bhvk,bhk->bhv', s, -kkt)
        s_new = s * wt[:, :, None, :] + sa[..., None] * (kkt * at)[:, :, None, :] + vt[..., None] * kt[:, :, None, :]
        o = jnp.einsum('bhvk,bhk->bhv', s_new, rt)
        return s_new.astype(s.dtype), o.astype(rt.dtype)

    xs = tuple(jnp.moveaxis(t, 1, 0) for t in (r, w, k, v, kk, a))
    s, o = lax.scan(step, s0, xs)
    return jnp.moveaxis(o, 0, 1), s


def _mixer(h, l, p, st):
    ssd_h0, conv0, rwkv_s0, shift0, gla_s0 = st
    bsz, length, _ = h.shape
    proj = h @ p['w_in'][l]
    z, xbc, dt_raw, rw, gq, gk, gv, glo, gg = _split(proj, IN_SIZES)

    conv_w = p['ssd_conv_w'][l]
    xpad = jnp.concatenate([conv0.astype(xbc.dtype), xbc], axis=1)
    conv = p['ssd_conv_b'][l] + xpad[:, 0:length] * conv_w[0]
    for i in range(1, SSD_CONV_W):
        conv = conv + xpad[:, i:i + length] * conv_w[i]
    new_conv = xpad[:, -(SSD_CONV_W - 1):]
    xs, bs, cs = _split(jax.nn.silu(conv), (SSD_W, SSD_G * SSD_N, SSD_G * SSD_N))
    xs = xs.reshape(bsz, length, SSD_H, SSD_HEAD_DIM)
    bs = bs.reshape(bsz, length, SSD_G, SSD_N)
    cs = cs.reshape(bsz, length, SSD_G, SSD_N)
    dt = jax.nn.softplus(dt_raw + p['ssd_dt_bias'][l])
    a = -jnp.exp(p['ssd_a_log'][l])
    y, ssd_h = _ssd_scan(xs, dt, a, bs, cs, ssd_h0)
    y = y + xs * p['ssd_d'][l][:, None]
    y_ssd = _rmsnorm(y.reshape(bsz, length, SSD_W) * jax.nn.silu(z), p['ssd_norm_g'][l])

    prev = jnp.concatenate([shift0[:, None].astype(rw.dtype), rw[:, :-1]], axis=1)
    rw_mix = rw + p['rwkv_mu'][l] * (prev - rw)
    new_shift = rw[:, -1]
    r, k, v, wl, al, gl = _split(rw_mix, (RWKV_W, RWKV_W, RWKV_W, W_LORA, A_LORA, G_LORA))
    w_log = -jax.nn.softplus(-(p['rwkv_w0'][l] + jnp.tanh(wl) @ p['rwkv_w2'][l])) - 0.5
    decay = jnp.exp(-jnp.exp(w_log))
    aic = jax.nn.sigmoid(p['rwkv_a0'][l] + al @ p['rwkv_a2'][l])
    gate = jax.nn.sigmoid(gl) @ p['rwkv_g2'][l]

    def hd(t):
        return t.reshape(bsz, length, RWKV_H, RWKV_N)

    kkf = hd(k * p['rwkv_k_k'][l]).astype(jnp.float32)
    kk = (kkf * lax.rsqrt(jnp.sum(jnp.square(kkf), axis=-1, keepdims=True) + 1e-12)).astype(k.dtype)
    k = k * (1.0 + (aic - 1.0) * p['rwkv_k_a'][l])
    rh, kh, vh = hd(r), hd(k), hd(v)
    o, rwkv_s = _rwkv_scan(rh, hd(decay), kh, vh, kk, hd(aic), rwkv_s0)
    of = o.astype(jnp.float32)
    mu = jnp.mean(of, axis=-1, keepdims=True)
    var = jnp.mean(jnp.square(of - mu), axis=-1, keepdims=True)
    o = ((of - mu) * lax.rsqrt(var + RWKV_GN_EPS)).astype(o.dtype).reshape(bsz, length, RWKV_W)
    o = o * p['rwkv_ln_g'][l] + p['rwkv_ln_b'][l]
    bonus = jnp.sum(rh * kh * p['rwkv_r_k'][l], axis=-1, keepdims=True) * vh
    y_rwkv = (o + bonus.reshape(bsz, length, RWKV_W)) * gate

    q = gq.reshape(bsz, length, GLA_H, GLA_DK) * (GLA_DK ** -0.5)
    kg = gk.reshape(bsz, length, GLA_H, GLA_DK)
    vg = gv.reshape(bsz, length, GLA_H, GLA_DV)
    lg = jax.nn.log_sigmoid(glo @ p['gla_w_gk2'][l] + p['gla_b_gk'][l]) / GATE_NORMALIZER
    og, gla_s = _gla_scan(q, kg, vg, lg.reshape(bsz, length, GLA_H, GLA_DK), gla_s0)
    y_gla = _rmsnorm(og, p['gla_norm_g'][l]).reshape(bsz, length, GLA_W) * jax.nn.silu(gg)

    y = jnp.concatenate([y_ssd, y_rwkv, y_gla], axis=-1) @ p['w_out'][l]
    return y, (ssd_h, new_conv, rwkv_s, new_shift, gla_s)


def _swiglu(h, wg, wu, wd):
    return (jax.nn.silu(h @ wg) * (h @ wu)) @ wd


def _moe(h, router, wg, wu, wd):
    logits = (h @ router).astype(jnp.float32)
    vals, idx = lax.top_k(logits, TOP_K)
    wts = jax.nn.softmax(vals, axis=-1)
    comb = jnp.sum(jax.nn.one_hot(idx, N_EXPERTS, dtype=jnp.float32) * wts[..., None], axis=-2).astype(h.dtype)
    out = jnp.zeros_like(h)
    for e in range(N_EXPERTS):
        out = out + comb[..., e:e + 1] * _swiglu(h, wg[e], wu[e], wd[e])
    return out


def _layer(x, c, l, p, st):
    mod = jax.nn.silu(c) @ p['w_ada'][l] + p['b_ada'][l]
    sh1, sc1, gt1, sh2, sc2, gt2 = [m[:, None, :] for m in jnp.split(mod, 6, axis=-1)]
    h = x * (1.0 + sc1) + sh1
    m, new_st = _mixer(h, l, p, st)
    x = _layernorm(ALPHA * x + (1.0 + gt1) * m, p['ln_mix_g'][l], p['ln_mix_b'][l])
    h = x * (1.0 + sc2) + sh2
    i = l // 2
    if l % 2 == 0:
        f = _swiglu(h, p['ffn_w_gate'][i], p['ffn_w_up'][i], p['ffn_w_down'][i])
    else:
        f = _moe(h, p['moe_router'][i], p['moe_w_gate'][i], p['moe_w_up'][i], p['moe_w_down'][i])
    x = _layernorm(ALPHA * x + (1.0 + gt2) * f, p['ln_ffn_g'][l], p['ln_ffn_b'][l])
    return x, new_st


def _run_group(x, c, states, p):
    new = ([], [], [], [], [])
    for l in range(DEPTH):
        x, st = _layer(x, c, l, p, tuple(s[l] for s in states))
        for acc, s in zip(new, st):
            acc.append(s)
    return x, tuple(jnp.stack(acc, axis=0) for acc in new)


def setup_inputs(seed: int = 0) -> dict:
    key = jax.random.key(seed)
    keys = iter(jax.random.split(key, 64))

    def nrm(shape, scale):
        return jax.random.normal(next(keys), shape, jnp.float32) * scale

    def gain(shape):
        return 1.0 + nrm(shape, 0.02)

    dt0 = jnp.exp(jax.random.uniform(next(keys), (DEPTH, SSD_H), jnp.float32, float(np.log(1e-3)), float(np.log(1e-1))))
    return {
        'x_prompt': nrm((BATCH, SEQ, D_MODEL), 1.0),
        'x_sample': nrm((DEC_BATCH, DEC_SEQ, D_MODEL), 1.0),
        'c_prompt': nrm((BATCH, D_MODEL), 1.0),
        'c_sample': nrm((DEC_BATCH, D_MODEL), 1.0),
        'state_ssd': nrm((DEPTH, DEC_BATCH, SSD_H, SSD_HEAD_DIM, SSD_N), 0.1),
        'state_ssd_conv': nrm((DEPTH, DEC_BATCH, SSD_CONV_W - 1, SSD_CONV_DIM), 0.5),
        'state_rwkv': nrm((DEPTH, DEC_BATCH, RWKV_H, RWKV_N, RWKV_N), 0.1),
        'state_rwkv_shift': nrm((DEPTH, DEC_BATCH, RWKV_FEAT), 0.5),
        'state_gla': nrm((DEPTH, DEC_BATCH, GLA_H, GLA_DK, GLA_DV), 0.1),
        'w_ada': nrm((DEPTH, D_MODEL, 6 * D_MODEL), 0.1 * D_MODEL ** -0.5),
        'b_ada': nrm((DEPTH, 6 * D_MODEL), 0.01),
        'w_in': nrm((DEPTH, D_MODEL, IN_DIM), D_MODEL ** -0.5),
        'w_out': nrm((DEPTH, D_MIX, D_MODEL), BETA * D_MIX ** -0.5),
        'ssd_conv_w': nrm((DEPTH, SSD_CONV_W, SSD_CONV_DIM), SSD_CONV_W ** -0.5),
        'ssd_conv_b': nrm((DEPTH, SSD_CONV_DIM), 0.02),
        'ssd_dt_bias': dt0 + jnp.log(-jnp.expm1(-dt0)),
        'ssd_a_log': jnp.log(jax.random.uniform(next(keys), (DEPTH, SSD_H), jnp.float32, 1.0, 16.0)),
        'ssd_d': 1.0 + nrm((DEPTH, SSD_H), 0.1),
        'ssd_norm_g': gain((DEPTH, SSD_W)),
        'rwkv_mu': jax.random.uniform(next(keys), (DEPTH, RWKV_FEAT), jnp.float32, 0.0, 1.0),
        'rwkv_w0': jax.random.uniform(next(keys), (DEPTH, RWKV_W), jnp.float32, -4.0, 0.0),
        'rwkv_w2': nrm((DEPTH, W_LORA, RWKV_W), 0.1 * W_LORA ** -0.5),
        'rwkv_a0': nrm((DEPTH, RWKV_W), 0.1),
        'rwkv_a2': nrm((DEPTH, A_LORA, RWKV_W), 0.1 * A_LORA ** -0.5),
        'rwkv_g2': nrm((DEPTH, G_LORA, RWKV_W), G_LORA ** -0.5),
        'rwkv_k_k': 0.85 + nrm((DEPTH, RWKV_W), 0.05),
        'rwkv_k_a': 1.0 + nrm((DEPTH, RWKV_W), 0.05),
        'rwkv_r_k': nrm((DEPTH, RWKV_H, RWKV_N), 0.1),
        'rwkv_ln_g': gain((DEPTH, RWKV_W)),
        'rwkv_ln_b': nrm((DEPTH, RWKV_W), 0.02),
        'gla_w_gk2': nrm((DEPTH, GK_LORA, GLA_H * GLA_DK), GK_LORA ** -0.5),
        'gla_b_gk': nrm((DEPTH, GLA_H * GLA_DK), 0.1),
        'gla_norm_g': gain((DEPTH, GLA_DV)),
        'ln_mix_g': gain((DEPTH, D_MODEL)),
        'ln_mix_b': nrm((DEPTH, D_MODEL), 0.02),
        'ln_ffn_g': gain((DEPTH, D_MODEL)),
        'ln_ffn_b': nrm((DEPTH, D_MODEL), 0.02),
        'ffn_w_gate': nrm((N_DENSE, D_MODEL, F_DENSE), D_MODEL ** -0.5),
        'ffn_w_up': nrm((N_DENSE, D_MODEL, F_DENSE), D_MODEL ** -0.5),
        'ffn_w_down': nrm((N_DENSE, F_DENSE, D_MODEL), BETA * F_DENSE ** -0.5),
        'moe_router': nrm((N_MOE, D_MODEL, N_EXPERTS), D_MODEL ** -0.5),
        'moe_w_gate': nrm((N_MOE, N_EXPERTS, D_MODEL, F_EXPERT), D_MODEL ** -0.5),
        'moe_w_up': nrm((N_MOE, N_EXPERTS, D_MODEL, F_EXPERT), D_MODEL ** -0.5),
        'moe_w_down': nrm((N_MOE, N_EXPERTS, F_EXPERT, D_MODEL), BETA * F_EXPERT ** -0.5),
    }


def reference(x_prompt, x_sample, c_prompt, c_sample, state_ssd, state_ssd_conv, state_rwkv, state_rwkv_shift, state_gla, w_ada, b_ada, w_in, w_out, ssd_conv_w, ssd_conv_b, ssd_dt_bias, ssd_a_log, ssd_d, ssd_norm_g, rwkv_mu, rwkv_w0, rwkv_w2, rwkv_a0, rwkv_a2, rwkv_g2, rwkv_k_k, rwkv_k_a, rwkv_r_k, rwkv_ln_g, rwkv_ln_b, gla_w_gk2, gla_b_gk, gla_norm_g, ln_mix_g, ln_mix_b, ln_ffn_g, ln_ffn_b, ffn_w_gate, ffn_w_up, ffn_w_down, moe_router, moe_w_gate, moe_w_up, moe_w_down):
    p = dict(w_ada=w_ada, b_ada=b_ada, w_in=w_in, w_out=w_out, ssd_conv_w=ssd_conv_w, ssd_conv_b=ssd_conv_b,
             ssd_dt_bias=ssd_dt_bias, ssd_a_log=ssd_a_log, ssd_d=ssd_d, ssd_norm_g=ssd_norm_g,
             rwkv_mu=rwkv_mu, rwkv_w0=rwkv_w0, rwkv_w2=rwkv_w2, rwkv_a0=rwkv_a0, rwkv_a2=rwkv_a2,
             rwkv_g2=rwkv_g2, rwkv_k_k=rwkv_k_k, rwkv_k_a=rwkv_k_a, rwkv_r_k=rwkv_r_k,
             rwkv_ln_g=rwkv_ln_g, rwkv_ln_b=rwkv_ln_b, gla_w_gk2=gla_w_gk2, gla_b_gk=gla_b_gk,
             gla_norm_g=gla_norm_g, ln_mix_g=ln_mix_g, ln_mix_b=ln_mix_b, ln_ffn_g=ln_ffn_g,
             ln_ffn_b=ln_ffn_b, ffn_w_gate=ffn_w_gate, ffn_w_up=ffn_w_up, ffn_w_down=ffn_w_down,
             moe_router=moe_router, moe_w_gate=moe_w_gate, moe_w_up=moe_w_up, moe_w_down=moe_w_down)
    bp = x_prompt.shape[0]
    zero_states = (jnp.zeros((DEPTH, bp) + state_ssd.shape[2:], state_ssd.dtype),
                   jnp.zeros((DEPTH, bp) + state_ssd_conv.shape[2:], state_ssd_conv.dtype),
                   jnp.zeros((DEPTH, bp) + state_rwkv.shape[2:], state_rwkv.dtype),
                   jnp.zeros((DEPTH, bp) + state_rwkv_shift.shape[2:], state_rwkv_shift.dtype),
                   jnp.zeros((DEPTH, bp) + state_gla.shape[2:], state_gla.dtype))
    y_prompt, (ps_ssd, ps_conv, ps_rwkv, ps_shift, ps_gla) = _run_group(x_prompt, c_prompt, zero_states, p)
    sample_states = (state_ssd, state_ssd_conv, state_rwkv, state_rwkv_shift, state_gla)
    y_sample, (ss_ssd, ss_conv, ss_rwkv, ss_shift, ss_gla) = _run_group(x_sample, c_sample, sample_states, p)
    return (y_prompt, y_sample, ps_ssd, ps_conv, ps_rwkv, ps_shift, ps_gla, ss_ssd, ss_conv, ss_rwkv, ss_shift, ss_gla)
```

```python
import contextlib
import numpy as np
import concourse.bass as bass
import concourse.mybir as mybir
from concourse.bass_utils import run_bass_kernel_spmd

F32 = mybir.dt.float32
BF16 = mybir.dt.bfloat16
AF = mybir.ActivationFunctionType
ALU = mybir.AluOpType
AX = mybir.AxisListType

NCORES = 8
D = 1024
KT = 8
T = 2048
NS = 16
NT = T + NS
NCH = T // 128
DEPTH = 2
IN_DIM = 2968
OFF = dict(z=0, xbc=512, dt=1280, rw=1288, gq=2184, gk=2312, gv=2440, glo=2696, gg=2712)
F_DENSE = 2816
ALPHA = (2.0 * DEPTH) ** 0.25
LN_EPS = 1e-5
RMS_EPS = 1e-6
RWKV_GN_EPS = 64 * 1e-5
BLOCKS = [(0, 512), (512, 512), (1024, 512), (1536, 512), (2048, 16)]

ENGS = ["pe", "dve", "act", "pool", "sp"]


class Res:
    __slots__ = ("name", "w", "r", "excl")

    def __init__(self, name="", excl=False):
        self.name = name
        self.w = None
        self.r = []
        self.excl = excl


class Prog:
    NDMA = 8

    def __init__(self, nc):
        self.nc = nc
        self.q = {e: [] for e in ENGS}
        self.cnt = {e: 0 for e in ENGS}
        self.seen = {e: {} for e in ENGS}
        self.dma_i = {e: 0 for e in ENGS}
        self.dma_last = {}
        self.sems = {}

    def sem(self, key):
        if key not in self.sems:
            self.sems[key] = self.nc.alloc_semaphore(name="s_" + "_".join(str(k) for k in key))
        return self.sems[key]

    def _collect(self, eng, reads, writes):
        waits = {}

        def add(tok):
            if tok is None:
                return
            key, val = tok
            if self.seen[eng].get(key, 0) >= val:
                return
            if waits.get(key, 0) < val:
                waits[key] = val

        for r in reads:
            add(r.w)
        for w in writes:
            add(w.w)
            for t in w.r:
                add(t)
        if eng == "pe":
            waits.pop(("e", "pe"), None)
        for k, v in waits.items():
            self.seen[eng][k] = v
        return list(waits.items())

    def _commit(self, tok, reads, writes):
        for r in reads:
            r.r.append(tok)
            if len(r.r) > 64:
                r.r = _prune(r.r)
        for w in writes:
            w.w = tok
            w.r = []

    def op(self, eng, fn, reads=(), writes=(), inc=True, self_wait=False):
        assert inc or eng == "pe"
        if any(r.excl for r in reads):
            writes = list(writes) + [r for r in reads if r.excl]
            reads = [r for r in reads if not r.excl]
        waits = self._collect(eng, reads, writes)
        if self_wait and self.cnt[eng] > 0:
            waits.append((("e", eng), self.cnt[eng]))
        tok = (("e", eng), self.cnt[eng] + 1)
        if inc:
            self.cnt[eng] += 1
        self._commit(tok, reads, writes)

        def emit(e, fn=fn, waits=waits, inc=inc, eng=eng):
            for k, v in waits:
                e.wait_ge(self.sem(k), v)
            ins = fn(e)
            if inc:
                ins.then_inc(self.sem(("e", eng)), 1)
        self.q[eng].append(emit)
        return tok

    def dma(self, queue, out, in_, reads=(), writes=(), **kw):
        i = self.dma_i[queue]
        self.dma_i[queue] += 1
        slot = i % self.NDMA
        key = ("d", queue, slot)
        val = 16 * (i // self.NDMA + 1)
        waits = self._collect(queue, reads, writes)
        prev = val - 16
        if prev > 0 and self.seen[queue].get(key, 0) < prev:
            self.seen[queue][key] = prev
            waits.append((key, prev))
        tok = (key, val)
        self.dma_last[key] = val
        self._commit(tok, reads, writes)

        def emit(e, waits=waits, key=key):
            for k, v in waits:
                e.wait_ge(self.sem(k), v)
            e.dma_start(out=out, in_=in_, **kw).then_inc(self.sem(key), 16)
        self.q[queue].append(emit)
        return tok

    def barrier(self, engines=ENGS):
        toks = [(("e", e), self.cnt[e]) for e in ENGS if self.cnt[e] > 0]
        toks += list(self.dma_last.items())
        for eng in engines:
            waits = []
            for k, v in toks:
                if k == ("e", eng) and eng == "pe":
                    continue
                if self.seen[eng].get(k, 0) < v:
                    self.seen[eng][k] = v
                    waits.append((k, v))

            def emit(e, waits=waits):
                for k, v in waits:
                    e.wait_ge(self.sem(k), v)
            self.q[eng].append(emit)

    def emit(self):
        with self.nc.Block() as block:
            @block.tensor
            def _(e):
                for f in self.q["pe"]:
                    f(e)

            @block.vector
            def _(e):
                for f in self.q["dve"]:
                    f(e)

            @block.scalar
            def _(e):
                for f in self.q["act"]:
                    f(e)

            @block.gpsimd
            def _(e):
                for f in self.q["pool"]:
                    f(e)

            @block.sync
            def _(e):
                for f in self.q["sp"]:
                    f(e)


def _prune(toks):
    best = {}
    for k, v in toks:
        if best.get(k, 0) < v:
            best[k] = v
    return list(best.items())


class Tn:
    def __init__(self, h, name, excl=False):
        self.h = h
        self.name = name
        self._res = {}
        self.excl = excl

    def ap(self):
        return self.h.ap()

    def r(self, key=0):
        if key not in self._res:
            self._res[key] = Res(f"{self.name}:{key}", self.excl)
        return self._res[key]


class KB:
    def __init__(self, nc):
        self.nc = nc
        self.P = Prog(nc)
        self.uid = 0

    def sb(self, stack, name, shape, dt=F32):
        self.uid += 1
        h = stack.enter_context(self.nc.sbuf_tensor(f"{name}_{self.uid}", list(shape), dt))
        return Tn(h, name)

    def ps(self, stack, name, shape, dt=F32):
        self.uid += 1
        h = stack.enter_context(self.nc.psum_tensor(f"{name}_{self.uid}", list(shape), dt))
        return Tn(h, name, excl=True)

    def mm(self, out, lhsT, rhs, r, w, start=True, stop=True, inc=None, self_wait=False, sgc=False):
        inc = stop if inc is None else inc
        kw = {"skip_group_check": True} if sgc else {}
        self.P.op("pe", lambda e: e.matmul(out, lhsT=lhsT, rhs=rhs, start=start, stop=stop, **kw),
                  reads=r, writes=w, inc=inc, self_wait=self_wait)

    def tr(self, out, in_, ident, r, w, inc=True):
        self.P.op("pe", lambda e: e.transpose(out, in_, ident), reads=r, writes=w, inc=inc)

    def act(self, out, in_, func, r, w, scale=None, bias=None, accum_out=None):
        kw = {}
        if scale is not None:
            kw["scale"] = scale
        if bias is not None:
            kw["bias"] = bias
        if accum_out is not None:
            kw["accum_out"] = accum_out
        self.P.op("act", lambda e: e.activation(out=out, in_=in_, func=func, **kw), reads=r, writes=w)

    def tt(self, out, in0, in1, op, r, w, eng="dve"):
        self.P.op(eng, lambda e: e.tensor_tensor(out=out, in0=in0, in1=in1, op=op), reads=r, writes=w)

    def ts(self, out, in0, s1, op0, r, w, s2=None, op1=None, eng="dve", accum_out=None):
        kw = {}
        if op1 is not None:
            kw["op1"] = op1
        if accum_out is not None:
            kw["accum_out"] = accum_out
        self.P.op(eng, lambda e: e.tensor_scalar(out=out, in0=in0, scalar1=s1, scalar2=s2, op0=op0, **kw),
                  reads=r, writes=w)

    def stt(self, out, in0, scalar, in1, op0, op1, r, w, eng="dve"):
        self.P.op(eng, lambda e: e.scalar_tensor_tensor(out=out, in0=in0, scalar=scalar, in1=in1, op0=op0, op1=op1),
                  reads=r, writes=w)

    def cp(self, out, in_, r, w, eng="dve"):
        if eng == "act":
            self.P.op("act", lambda e: e.activation(out=out, in_=in_, func=AF.Copy), reads=r, writes=w)
        else:
            self.P.op(eng, lambda e: e.tensor_copy(out=out, in_=in_), reads=r, writes=w)

    def recip(self, out, in_, r, w):
        self.P.op("dve", lambda e: e.reciprocal(out=out, in_=in_), reads=r, writes=w)

    def red(self, out, in_, r, w, op=ALU.add, axis=AX.X, eng="dve"):
        self.P.op(eng, lambda e: e.tensor_reduce(out=out, in_=in_, axis=axis, op=op), reads=r, writes=w)

    def memset(self, ap, val, w, eng="dve"):
        self.P.op(eng, lambda e: e.memset(ap, val), writes=w)

    def dma(self, out, in_, r=(), w=(), q="sp", **kw):
        return self.P.dma(q, out, in_, reads=r, writes=w, **kw)


W_SHAPES = dict(
    w_ada=[DEPTH, D, 6 * D], b_ada=[DEPTH, 6 * D], w_in=[DEPTH, D, IN_DIM], w_out=[DEPTH, D, D],
    ssd_conv_w=[DEPTH, 4, 768], ssd_conv_b=[DEPTH, 768], ssd_dt_bias=[DEPTH, 8], ssd_a_log=[DEPTH, 8],
    ssd_d=[DEPTH, 8], ssd_norm_g=[DEPTH, 512], rwkv_mu=[DEPTH, 896], rwkv_w0=[DEPTH, 256],
    rwkv_w2=[DEPTH, 32, 256], rwkv_a0=[DEPTH, 256], rwkv_a2=[DEPTH, 32, 256], rwkv_g2=[DEPTH, 64, 256],
    rwkv_k_k=[DEPTH, 256], rwkv_k_a=[DEPTH, 256], rwkv_r_k=[DEPTH, 4, 64], rwkv_ln_g=[DEPTH, 256],
    rwkv_ln_b=[DEPTH, 256], gla_w_gk2=[DEPTH, 16, 128], gla_b_gk=[DEPTH, 128], gla_norm_g=[DEPTH, 64],
    ln_mix_g=[DEPTH, D], ln_mix_b=[DEPTH, D], ln_ffn_g=[DEPTH, D], ln_ffn_b=[DEPTH, D],
    ffn_w_gate=[1, D, F_DENSE], ffn_w_up=[1, D, F_DENSE], ffn_w_down=[1, F_DENSE, D],
    moe_router=[1, D, 8], moe_w_gate=[1, 8, D, D], moe_w_up=[1, 8, D, D], moe_w_down=[1, 8, D, D],
)
IN_SHAPES = dict(
    xp=[T, D], xs=[NS, D], cc=[1 + NS, D],
    st_ssd=[DEPTH, NS, 8, 64, 64], st_conv=[DEPTH, NS, 3, 768], st_rwkv=[DEPTH, NS, 4, 64, 64],
    st_shift=[DEPTH, NS, 896], st_gla=[DEPTH, NS, 4, 32, 64],
)
OUT_SHAPES = dict(
    y_p=[T, D], y_s=[NS, D],
    p_ssd=[DEPTH, 8, 64, 64], p_conv=[DEPTH, 3, 768], p_rwkv=[DEPTH, 4, 64, 64], p_shift=[DEPTH, 896],
    p_gla=[DEPTH, 4, 32, 64],
    s_ssd=[DEPTH, NS, 8, 64, 64], s_conv=[DEPTH, NS, 3, 768], s_rwkv=[DEPTH, NS, 4, 64, 64],
    s_shift=[DEPTH, NS, 896], s_gla=[DEPTH, NS, 4, 32, 64],
)

def xr(t, b, tiles=range(KT)):
    return [t.r((d, b)) for d in tiles]


SH1, SC1, GT1, SH2, SC2, GT2 = 0, 8, 16, 24, 32, 40


def build(stub_mixer=False, dbg=None, n_layers=DEPTH):
    nc = bass.Bass("TRN2", target_bir_lowering=False)
    K = KB(nc)
    P = K.P
    dr = {}
    for n, s in IN_SHAPES.items():
        dr[n] = nc.dram_tensor(n, s, F32, kind="ExternalInput").ap()
    for n, s in W_SHAPES.items():
        dr[n] = nc.dram_tensor(n, s, F32, kind="ExternalInput").ap()
    for n, s in OUT_SHAPES.items():
        dr[n] = nc.dram_tensor(n, s, F32, kind="ExternalOutput").ap()
    dbg_out = {}
    if dbg:
        for n, s in dbg.items():
            dbg_out[n] = nc.dram_tensor("dbg_" + n, s, F32, kind="ExternalOutput").ap()
    out_res = Res("outputs")

    with contextlib.ExitStack() as perm, nc.allow_non_contiguous_dma(reason="small param loads"):
        xT = K.sb(perm, "xT", [128, KT, NT], F32)
        modT = [K.sb(perm, f"modT{l}", [128, 48, 1 + NS], F32) for l in range(DEPTH)]
        identf = K.sb(perm, "identf", [128, 128], F32)
        identb = K.sb(perm, "identb", [128, 128], BF16)
        onesM = K.sb(perm, "onesM", [128, 128], F32)
        ones1 = K.sb(perm, "ones1", [128, 128], F32)
        maskU = K.sb(perm, "maskU", [128, 128], F32)
        maskSU = K.sb(perm, "maskSU", [128, 128], F32)
        maskSL = K.sb(perm, "maskSL", [128, 128], F32)
        blk64 = K.sb(perm, "blk64", [128, 128], F32)
        C = dict(xT=xT, modT=modT, identf=identf, identb=identb, onesM=onesM, ones1=ones1,
                 maskU=maskU, maskSU=maskSU, maskSL=maskSL, blk64=blk64)

        def sel(t, val_keep, cmp, fill, base=0, cm=1, pat=None, ap=None):
            ap = t.ap() if ap is None else ap
            pat = [[-1, ap.shape[-1]]] if pat is None else pat
            P.op("pool", lambda e: e.affine_select(out=ap, in_=ap, pattern=pat, compare_op=cmp, fill=fill,
                                                   base=base, channel_multiplier=cm),
                 reads=[t.r()], writes=[t.r()])

        K.memset(identf.ap(), 0.0, [identf.r()], eng="pool")
        sel(identf, 0, ALU.not_equal, 1.0)
        K.cp(identb.ap(), identf.ap(), [identf.r()], [identb.r()], eng="pool")
        K.memset(onesM.ap(), 1.0 / D, [onesM.r()], eng="pool")
        K.memset(ones1.ap(), 1.0, [ones1.r()], eng="pool")
        K.memset(maskU.ap(), 1.0, [maskU.r()], eng="pool")
        sel(maskU, 1, ALU.is_ge, 0.0, cm=-1, pat=[[1, 128]])
        K.memset(maskSU.ap(), 1.0, [maskSU.r()], eng="pool")
        sel(maskSU, 1, ALU.is_gt, 0.0, cm=-1, pat=[[1, 128]])
        K.memset(maskSL.ap(), 1.0, [maskSL.r()], eng="pool")
        sel(maskSL, 1, ALU.is_gt, 0.0)
        K.memset(blk64.ap(), 0.0, [blk64.r()], eng="pool")
        K.memset(blk64.ap()[0:64, 0:64], 1.0, [blk64.r()], eng="pool")
        K.memset(blk64.ap()[64:128, 64:128], 1.0, [blk64.r()], eng="pool")

        with contextlib.ExitStack() as ph:
            stage = [K.sb(ph, f"stg{i}", [128, D], F32) for i in range(2)]
            ctm = K.sb(ph, "ctm", [1 + NS, D], F32)
            scT = K.sb(ph, "scT", [128, KT, 1 + NS], BF16)
            wada = [K.sb(ph, f"wada{i}", [128, KT, 512], BF16) for i in range(2)]
            bB = [K.sb(ph, f"bB{i}", [1 + NS, 512], F32) for i in range(2)]
            modsb = [K.sb(ph, f"modsb{i}", [1 + NS, 512], F32) for i in range(2)]
            pst = [K.ps(ph, f"pst{i}", [128, 1024], F32) for i in range(2)]
            psm = [K.ps(ph, f"psm{i}", [128, 512], F32) for i in range(2)]
            pss = K.ps(ph, "pss", [128, 512], F32)
            for tt in range(NCH + 1):
                st = stage[tt % 2]
                pt = pst[tt % 2]
                n = 128 if tt < NCH else NS
                src = dr["xp"][tt * 128:(tt + 1) * 128, :] if tt < NCH else dr["xs"]
                K.dma(st.ap()[0:n, :], src, w=[st.r()])
                for d in range(KT):
                    K.tr(pt.ap()[:, d * n:(d + 1) * n], st.ap()[0:n, d * 128:(d + 1) * 128],
                         identf.ap()[0:n, 0:n], [st.r(), identf.r()], [pt.r()], inc=(d == KT - 1))
                dst = xT.ap()[:, :, tt * 128:tt * 128 + n]
                srcp = pt.ap()[:, 0:KT * n].rearrange("p (d n) -> p d n", d=KT)
                K.cp(dst, srcp, [pt.r()], xr(xT, min(tt // 4, 4)), eng=("act" if tt % 2 == 0 else "dve"))
            K.dma(ctm.ap(), dr["cc"], w=[ctm.r()])
            for d in range(KT):
                K.tr(pss.ap()[:, d * 17:(d + 1) * 17], ctm.ap()[:, d * 128:(d + 1) * 128],
                     identf.ap()[0:17, 0:17], [ctm.r(), identf.r()], [pss.r()], inc=(d == KT - 1))
            K.act(scT.ap(), pss.ap()[:, 0:KT * 17].rearrange("p (d n) -> p d n", d=KT), AF.Silu,
                  [pss.r()], [scT.r()])
            it = 0
            for l in range(DEPTH):
                for j in range(12):
                    wb = wada[it % 2]
                    bb = bB[it % 2]
                    ms = modsb[it % 2]
                    pm = psm[it % 2]
                    K.dma(wb.ap(), dr["w_ada"][l, :, j * 512:(j + 1) * 512].rearrange("(k p) n -> p k n", p=128),
                          w=[wb.r()], q="pool")
                    K.dma(bb.ap(), dr["b_ada"][l:l + 1, j * 512:(j + 1) * 512].to_broadcast([1 + NS, 512]),
                          w=[bb.r()])
                    for k in range(KT):
                        K.mm(pm.ap()[0:17, :], scT.ap()[:, k, :], wb.ap()[:, k, :], [scT.r(), wb.r()], [pm.r()],
                             start=(k == 0), stop=(k == KT - 1))
                    K.tt(ms.ap(), pm.ap()[0:17, :], bb.ap(), ALU.add, [pm.r(), bb.r()], [ms.r()])
                    for q4 in range(4):
                        K.tr(pss.ap()[:, q4 * 17:(q4 + 1) * 17], ms.ap()[:, q4 * 128:(q4 + 1) * 128],
                             identf.ap()[0:17, 0:17], [ms.r(), identf.r()], [pss.r()], inc=(q4 == 3))
                    K.cp(modT[l].ap()[:, j * 4:(j + 1) * 4, :],
                         pss.ap()[:, 0:4 * 17].rearrange("p (d n) -> p d n", d=4), [pss.r()], [modT[l].r()],
                         eng="act")
                    it += 1
                for seg in (SC1, GT1, SC2, GT2):
                    K.ts(modT[l].ap()[:, seg:seg + 8, :], modT[l].ap()[:, seg:seg + 8, :], 1.0, ALU.add,
                         [modT[l].r()], [modT[l].r()])
            P.barrier()

        for l in range(n_layers):
            layer(K, dr, C, l, stub_mixer, dbg_out)

        with contextlib.ExitStack() as ph:
            stage = [K.sb(ph, f"ostg{i}", [128, D], F32) for i in range(2)]
            pst = [K.ps(ph, f"opst{i}", [128, 1024], F32) for i in range(2)]
            for tt in range(NCH + 1):
                st = stage[tt % 2]
                pt = pst[tt % 2]
                n = 128 if tt < NCH else NS
                for d in range(KT):
                    K.tr(pt.ap()[0:n, d * 128:(d + 1) * 128], xT.ap()[:, d, tt * 128:tt * 128 + n],
                         identf.ap(), [xT.r((d, min(tt // 4, 4))), identf.r()], [pt.r()], inc=(d == KT - 1))
                K.cp(st.ap()[0:n, :], pt.ap()[0:n, :], [pt.r()], [st.r()], eng=("act" if tt % 2 == 0 else "dve"))
                dst = dr["y_p"][tt * 128:(tt + 1) * 128, :] if tt < NCH else dr["y_s"]
                K.dma(dst, st.ap()[0:n, :], r=[st.r()])
            P.barrier()
    with nc.allow_non_contiguous_dma(reason="small param loads"):
        P.emit()
    return nc


def modulate(K, C, l, src, dst, b, sh, sc, dres):
    mod = C["modT"][l]
    c0, n = BLOCKS[b]
    if b < 4:
        for d in range(KT):
            K.act(dst.ap()[:, d, c0:c0 + n], src.ap()[:, d, c0:c0 + n], AF.Identity,
                  [src.r((d, b)), mod.r()], [dres(d)],
                  scale=mod.ap()[:, sc + d, 0:1], bias=mod.ap()[:, sh + d, 0:1])
    else:
        tmp = C["tmp_s"]
        K.tt(tmp.ap(), src.ap()[:, :, c0:c0 + n], mod.ap()[:, sc:sc + 8, 1:1 + NS], ALU.mult,
             xr(src, b) + [mod.r()], [tmp.r()])
        K.tt(dst.ap()[:, :, c0:c0 + n], tmp.ap(), mod.ap()[:, sh:sh + 8, 1:1 + NS], ALU.add,
             [tmp.r(), mod.r()], [dres(d) for d in range(KT)])


def layernorm(K, C, l, gi, psA, psB, scr):
    xT, onesM, lncol = C["xT"], C["onesM"], C["lncol"]
    sq, mean_sb, var, tt_ = scr["sq"], scr["mean"], scr["var"], scr["t"]
    for b, (c0, n) in enumerate(BLOCKS):
        for d in range(KT):
            s = sq[d % 2]
            xs = xT.ap()[:, d, c0:c0 + n]
            K.act(s.ap()[:, :n], xs, AF.Square, [xT.r((d, b))], [s.r()])
            K.mm(psA.ap()[:, :n], onesM.ap(), xs, [onesM.r(), xT.r((d, b))], [psA.r()],
                 start=(d == 0), stop=(d == KT - 1), inc=True)
            K.mm(psB.ap()[:, :n], onesM.ap(), s.ap()[:, :n], [onesM.r(), s.r()], [psB.r()],
                 start=(d == 0), stop=(d == KT - 1), inc=True)
        K.cp(mean_sb.ap()[:, :n], psA.ap()[:, :n], [psA.r()], [mean_sb.r()], eng="act")
        K.tt(var.ap()[:, :n], mean_sb.ap()[:, :n], mean_sb.ap()[:, :n], ALU.mult, [mean_sb.r()], [var.r()])
        K.tt(var.ap()[:, :n], psB.ap()[:, :n], var.ap()[:, :n], ALU.subtract, [psB.r(), var.r()], [var.r()])
        K.act(var.ap()[:, :n], var.ap()[:, :n], AF.Sqrt, [var.r()], [var.r()], bias=LN_EPS)
        K.recip(var.ap()[:, :n], var.ap()[:, :n], [var.r()], [var.r()])
        for d in range(KT):
            t = tt_[d % 2]
            xs = xT.ap()[:, d, c0:c0 + n]
            K.tt(t.ap()[:, :n], xs, mean_sb.ap()[:, :n], ALU.subtract, [xT.r((d, b)), mean_sb.r()], [t.r()])
            K.tt(t.ap()[:, :n], t.ap()[:, :n], var.ap()[:, :n], ALU.mult, [t.r(), var.r()], [t.r()])
            K.act(xs, t.ap()[:, :n], AF.Identity, [t.r(), lncol.r()], [xT.r((d, b))],
                  scale=lncol.ap()[:, gi, d:d + 1], bias=lncol.ap()[:, gi + 1, d:d + 1])


def residual_add(K, C, l, ps, n, dout, b, gt, comb=None):
    xT, mod = C["xT"], C["modT"][l]
    c0, _ = BLOCKS[b]
    xs = xT.ap()[:, dout, c0:c0 + n]
    src = ps.ap()[:, :n]
    rd = [ps.r(), mod.r(), xT.r((dout, b))]
    if comb is not None:
        tmp = C["tmp_c"][dout % 2]
        K.tt(tmp.ap()[:, :n], src, comb.ap()[:, c0:c0 + n], ALU.mult, [ps.r(), comb.r(b)], [tmp.r()])
        src = tmp.ap()[:, :n]
        rd = [tmp.r(), mod.r(), xT.r((dout, b))]
    if b < 4:
        K.stt(xs, src, mod.ap()[:, gt + dout, 0:1], xs, ALU.mult, ALU.add, rd, [xT.r((dout, b))])
    else:
        tmp2 = C["tmp_s2"]
        K.tt(tmp2.ap(), src, mod.ap()[:, gt + dout, 1:1 + NS], ALU.mult, rd[:2], [tmp2.r()])
        K.tt(xs, tmp2.ap(), xs, ALU.add, [tmp2.r(), xT.r((dout, b))], [xT.r((dout, b))])


def scale_x(K, C):
    xT = C["xT"]
    for b, (c0, n) in enumerate(BLOCKS):
        for d in range(KT):
            xs = xT.ap()[:, d, c0:c0 + n]
            K.P.op("act", lambda e, xs=xs: e.mul(out=xs, in_=xs, mul=ALPHA), reads=[xT.r((d, b))],
                   writes=[xT.r((d, b))])


def ffn_group(K, C, l, hT, actb, wpool, srcs, nf, ps, comb=None):
    wg_src, wu_src, wd_src = srcs
    sg = C["sg"]

    def unit():
        u = wpool["bufs"][wpool["i"] % len(wpool["bufs"])]
        wpool["i"] += 1
        return u

    i = 0
    for f0 in range(0, nf, 4):
        nfu = min(4, nf - f0)
        WG, WU = unit(), unit()
        K.dma(WG.ap()[:, :, 0:nfu * 128], wg_src[:, f0 * 128:(f0 + nfu) * 128].rearrange("(k p) n -> p k n", p=128),
              w=[WG.r()], q="pool")
        K.dma(WU.ap()[:, :, 0:nfu * 128], wu_src[:, f0 * 128:(f0 + nfu) * 128].rearrange("(k p) n -> p k n", p=128),
              w=[WU.r()], q="pool")
        for fu in range(nfu):
            f = f0 + fu
            for b, (c0, n) in enumerate(BLOCKS):
                pg, pu = ps["g"][i % 2], ps["u"][i % 2]
                for k in range(KT):
                    K.mm(pg.ap()[:, :n], WG.ap()[:, k, fu * 128:(fu + 1) * 128], hT.ap()[:, k, c0:c0 + n],
                         [WG.r(), hT.r((k, b))], [pg.r()], start=(k == 0), stop=(k == KT - 1))
                for k in range(KT):
                    K.mm(pu.ap()[:, :n], WU.ap()[:, k, fu * 128:(fu + 1) * 128], hT.ap()[:, k, c0:c0 + n],
                         [WU.r(), hT.r((k, b))], [pu.r()], start=(k == 0), stop=(k == KT - 1))
                s_ = sg[i % 2]
                K.act(s_.ap()[:, :n], pg.ap()[:, :n], AF.Silu, [pg.r()], [s_.r()])
                K.tt(actb.ap()[:, f, c0:c0 + n], s_.ap()[:, :n], pu.ap()[:, :n], ALU.mult, [s_.r(), pu.r()],
                     [actb.r((f, b))])
                i += 1
    i = 0
    for dh in range(2):
        WD = unit()
        K.dma(WD.ap()[:, 0:nf, :], wd_src[:, dh * 512:(dh + 1) * 512].rearrange("(f p) n -> p f n", p=128),
              w=[WD.r()], q="pool")
        for dd in range(4):
            dout = dh * 4 + dd
            for b, (c0, n) in enumerate(BLOCKS):
                pd = ps["d"][i % 2]
                for f in range(nf):
                    K.mm(pd.ap()[:, :n], WD.ap()[:, f, dd * 128:(dd + 1) * 128], actb.ap()[:, f, c0:c0 + n],
                         [WD.r(), actb.r((f, b))], [pd.r()], start=(f == 0), stop=(f == nf - 1))
                residual_add(K, C, l, pd, n, dout, b, GT2, comb=comb)
                i += 1


def moe_routing(K, C, dr, l, ph, ps):
    xT, mod, identf = C["xT"], C["modT"][l], C["identf"]
    router = K.sb(ph, "router", [128, KT, 8], F32)
    K.dma(router.ap(), dr["moe_router"][0].rearrange("(k p) e -> p k e", p=128), w=[router.r()])
    combT = K.sb(ph, "combT", [8, NT], F32)
    tp = C["tpair"]
    sm = {n: K.sb(ph, "rt_" + n, [128, 8], F32) for n in ["lg", "eq1", "l2", "eq2", "cb"]}
    sc1 = {n: K.sb(ph, "rs_" + n, [128, 1], F32) for n in ["m1", "m2", "e", "w1", "w2"]}
    pl, pt = ps["g"][0], ps["u"][0]
    for tt in range(NCH + 1):
        n = 128 if tt < NCH else NS
        c0 = tt * 128
        b = min(tt // 4, 4)
        hap = tp.ap().rearrange("p a (b c) -> p (a b) c", c=128)
        hres = [tp.r(0), tp.r(1)]
        if tt < NCH:
            for d in range(KT):
                K.act(hap[:, d, :], xT.ap()[:, d, c0:c0 + n], AF.Identity, [xT.r((d, b)), mod.r()], hres,
                      scale=mod.ap()[:, SC2 + d, 0:1], bias=mod.ap()[:, SH2 + d, 0:1])
        else:
            K.tt(hap[:, :, 0:n], xT.ap()[:, :, c0:c0 + n], mod.ap()[:, SC2:SC2 + 8, 1:1 + NS], ALU.mult,
                 xr(xT, b) + [mod.r()], hres)
            K.tt(hap[:, :, 0:n], hap[:, :, 0:n], mod.ap()[:, SH2:SH2 + 8, 1:1 + NS], ALU.add,
                 hres + [mod.r()], hres)
        for d in range(KT):
            K.mm(pl.ap()[0:n, 0:8], hap[:, d, 0:n], router.ap()[:, d, :], hres + [router.r()], [pl.r()],
                 start=(d == 0), stop=(d == KT - 1), inc=True)
        lg, eq1, l2, eq2, cb = (sm[k].ap()[0:n, :] for k in ["lg", "eq1", "l2", "eq2", "cb"])
        m1, m2, ee, w1, w2 = (sc1[k].ap()[0:n, :] for k in ["m1", "m2", "e", "w1", "w2"])
        R = lambda *ks: [(sm[k] if k in sm else sc1[k]).r() for k in ks]
        K.cp(lg, pl.ap()[0:n, 0:8], [pl.r()], R("lg"))
        K.red(m1, lg, R("lg"), R("m1"), op=ALU.max)
        K.ts(eq1, lg, m1, ALU.is_equal, R("lg", "m1"), R("eq1"))
        K.stt(l2, eq1, -1e30, lg, ALU.mult, ALU.add, R("eq1", "lg"), R("l2"))
        K.red(m2, l2, R("l2"), R("m2"), op=ALU.max)
        K.ts(eq2, l2, m2, ALU.is_equal, R("l2", "m2"), R("eq2"))
        K.tt(ee, m2, m1, ALU.subtract, R("m1", "m2"), R("e"))
        K.act(ee, ee, AF.Exp, R("e"), R("e"))
        K.ts(w1, ee, 1.0, ALU.add, R("e"), R("w1"))
        K.recip(w1, w1, R("w1"), R("w1"))
        K.tt(w2, ee, w1, ALU.mult, R("e", "w1"), R("w2"))
        K.ts(cb, eq1, w1, ALU.mult, R("eq1", "w1"), R("cb"))
        K.stt(cb, eq2, w2, cb, ALU.mult, ALU.add, R("eq2", "w2", "cb"), R("cb"))
        K.tr(pt.ap()[0:8, 0:n], cb, identf.ap()[0:n, 0:n], R("cb") + [identf.r()], [pt.r()])
        K.cp(combT.ap()[:, c0:c0 + n], pt.ap()[0:8, 0:n], [pt.r()], [combT.r()], eng="act")
    return combT


def layer(K, dr, C, l, stub_mixer, dbg_out):
    nc, P = K.nc, K.P
    xT, mod = C["xT"], C["modT"][l]
    with contextlib.ExitStack() as lay:
        bufA = K.sb(lay, "bufA", [128, KT, NT], BF16)
        lncol = K.sb(lay, "lncol", [128, 4, KT], F32)
        C["lncol"] = lncol
        C["tmp_s"] = K.sb(lay, "tmp_s", [128, KT, NS], F32)
        C["tmp_s2"] = K.sb(lay, "tmp_s2", [128, NS], F32)
        for i, nme in enumerate(["ln_mix_g", "ln_mix_b", "ln_ffn_g", "ln_ffn_b"]):
            K.dma(lncol.ap()[:, i, :], dr[nme][l].rearrange("(d p) -> p d", p=128), w=[lncol.r()])

        with contextlib.ExitStack() as ph:
            wout = K.sb(ph, "wout", [128, KT, D], BF16)
            K.dma(wout.ap(), dr["w_out"][l].rearrange("(k p) n -> p k n", p=128), w=[wout.r()], q="pool")
            if stub_mixer:
                for b in range(5):
                    modulate(K, C, l, xT, bufA, b, SH1, SC1, lambda d, b=b: bufA.r((d, b)))
            else:
                mixers(K, dr, C, l, bufA, dbg_out)
            P.barrier()
            scale_x(K, C)
            with contextlib.ExitStack() as ph2:
                pso = [K.ps(ph2, f"pso{i}", [128, 512], F32) for i in range(4)]
                i = 0
                for dout in range(KT):
                    for b, (c0, n) in enumerate(BLOCKS):
                        pd = pso[i % 4]
                        for k in range(KT):
                            K.mm(pd.ap()[:, :n], wout.ap()[:, k, dout * 128:(dout + 1) * 128],
                                 bufA.ap()[:, k, c0:c0 + n], [wout.r(), bufA.r((k, b))], [pd.r()],
                                 start=(k == 0), stop=(k == KT - 1))
                        residual_add(K, C, l, pd, n, dout, b, GT1)
                        i += 1
                P.barrier()

        with contextlib.ExitStack() as ph:
            hT = K.sb(ph, "hT", [128, KT, NT], BF16)
            tpair = K.sb(ph, "tpair", [128, 2, 512], F32)
            C["tpair"] = tpair

            class _V:
                def __init__(self, i):
                    self.i = i

                def ap(self):
                    return tpair.ap()[:, self.i, :]

                def r(self, key=0):
                    return tpair.r(self.i)
            scr = dict(sq=[K.sb(ph, f"sq{i}", [128, 512], F32) for i in range(2)],
                       mean=K.sb(ph, "mean", [128, 512], F32), var=K.sb(ph, "var", [128, 512], F32),
                       t=[_V(0), _V(1)])
            C["sg"] = scr["sq"]
            C["tmp_c"] = scr["t"]
            W = dict(bufs=[K.sb(ph, f"WP{i}", [128, KT, 512], BF16) for i in range(4)], i=0)
            ps = dict(g=[K.ps(ph, f"pg{i}", [128, 512], F32) for i in range(2)],
                      u=[K.ps(ph, f"pu{i}", [128, 512], F32) for i in range(2)],
                      d=[K.ps(ph, f"pd{i}", [128, 512], F32) for i in range(2)])
            psA = K.ps(ph, "psA", [128, 512], F32)
            psB = K.ps(ph, "psB", [128, 512], F32)
            layernorm(K, C, l, 0, psA, psB, scr)
            for b in range(5):
                modulate(K, C, l, xT, hT, b, SH2, SC2, lambda d, b=b: hT.r((d, b)))
            if l % 2 == 0:
                scale_x(K, C)
                i = l // 2
                for f0 in range(0, F_DENSE // 128, 8):
                    nf = min(8, F_DENSE // 128 - f0)
                    srcs = (dr["ffn_w_gate"][i, :, f0 * 128:(f0 + nf) * 128],
                            dr["ffn_w_up"][i, :, f0 * 128:(f0 + nf) * 128],
                            dr["ffn_w_down"][i, f0 * 128:(f0 + nf) * 128, :])
                    ffn_group(K, C, l, hT, bufA, W, srcs, nf, ps)
            else:
                combT = moe_routing(K, C, dr, l, ph, ps)
                scale_x(K, C)
                combB = K.sb(ph, "combB", [128, NT], F32)
                sele = K.sb(ph, "sele", [8, 128], F32)
                i = l // 2
                for e_ in range(8):
                    K.memset(sele.ap(), 0.0, [sele.r()])
                    K.P.op("dve", lambda e, e_=e_: e.memset(sele.ap()[e_:e_ + 1, :], 1.0), reads=[sele.r()],
                           writes=[sele.r()]) if False else K.ts(
                        sele.ap(), C["identf"].ap()[0:8, e_:e_ + 1].to_broadcast([8, 128]), 1.0, ALU.mult,
                        [C["identf"].r()], [sele.r()])
                    for b, (c0, n) in enumerate(BLOCKS):
                        pb = ps["d"][b % 2]
                        K.mm(pb.ap()[:, :n], sele.ap(), combT.ap()[:, c0:c0 + n], [sele.r(), combT.r()],
                             [pb.r()])
                        K.cp(combB.ap()[:, c0:c0 + n], pb.ap()[:, :n], [pb.r()], [combB.r(b)], eng="act")
                    srcs = (dr["moe_w_gate"][i, e_], dr["moe_w_up"][i, e_], dr["moe_w_down"][i, e_])
                    ffn_group(K, C, l, hT, bufA, W, srcs, 8, ps, comb=combB)
            layernorm(K, C, l, 2, psA, psB, scr)
            P.barrier()


def make_in_maps(inp):
    g = lambda k: np.ascontiguousarray(np.asarray(inp[k], dtype=np.float32))
    xp, xs, cp, cs = g("x_prompt"), g("x_sample"), g("c_prompt"), g("c_sample")
    st = {k: g(k) for k in ["state_ssd", "state_ssd_conv", "state_rwkv", "state_rwkv_shift", "state_gla"]}
    wts = {k: g(k) for k in W_SHAPES}
    maps = []
    for c in range(NCORES):
        sl = slice(c * NS, (c + 1) * NS)
        m = dict(wts)
        m["xp"] = xp[c]
        m["xs"] = np.ascontiguousarray(xs[sl, 0, :])
        m["cc"] = np.ascontiguousarray(np.concatenate([cp[c:c + 1], cs[sl]], axis=0))
        m["st_ssd"] = np.ascontiguousarray(st["state_ssd"][:, sl])
        m["st_conv"] = np.ascontiguousarray(st["state_ssd_conv"][:, sl])
        m["st_rwkv"] = np.ascontiguousarray(st["state_rwkv"][:, sl])
        m["st_shift"] = np.ascontiguousarray(st["state_rwkv_shift"][:, sl])
        m["st_gla"] = np.ascontiguousarray(st["state_gla"][:, sl])
        maps.append(m)
    return maps


_NC_CACHE = {}


def gather(results):
    R = lambda k: [np.asarray(r[k], dtype=np.float32) for r in results]
    y_p = np.stack(R("y_p"), axis=0)
    y_s = np.concatenate(R("y_s"), axis=0)[:, None, :]
    outs = [y_p, y_s]
    for k in ["p_ssd", "p_conv", "p_rwkv", "p_shift", "p_gla"]:
        outs.append(np.stack(R(k), axis=1))
    for k in ["s_ssd", "s_conv", "s_rwkv", "s_shift", "s_gla"]:
        outs.append(np.concatenate(R(k), axis=1))
    return tuple(np.ascontiguousarray(o) for o in outs)


def kernel(**inputs):
    if "nc" not in _NC_CACHE:
        _NC_CACHE["nc"] = build()
    res = run_bass_kernel_spmd(_NC_CACHE["nc"], make_in_maps(inputs), core_ids=list(range(NCORES)))
    return gather(res.results)


def bc(ap, axis, shape):
    return ap.unsqueeze(axis).to_broadcast(list(shape))


def softplus_(K, x, tmp, r, n):
    xa, ta = x[0], tmp[0]
    K.act(ta, xa, AF.Abs, [x[1]], [tmp[1]])
    K.act(ta, ta, AF.Exp, [tmp[1]], [tmp[1]], scale=-1.0)
    K.act(ta, ta, AF.Ln, [tmp[1]], [tmp[1]], bias=1.0)
    K.ts(xa, xa, 0.0, ALU.max, [x[1]], [x[1]])
    K.tt(xa, xa, ta, ALU.add, [x[1], tmp[1]], [x[1]])


def make_hc(K, C, l, hc, c):
    xT, mod = C["xT"], C["modT"][l]
    for d in range(KT):
        K.act(hc.ap()[:, d, :], xT.ap()[:, d, c * 128:(c + 1) * 128], AF.Identity,
              [xT.r((d, c // 4)), mod.r()], [hc.r()],
              scale=mod.ap()[:, SC1 + d, 0:1], bias=mod.ap()[:, SH1 + d, 0:1])


def mixers(K, dr, C, l, yT, dbg_out):
    P = K.P
    xT, mod = C["xT"], C["modT"][l]
    en = C.get("enable", ("ssd", "rwkv", "gla"))
    with contextlib.ExitStack() as mx:
        hc = [K.sb(mx, f"hc{i}", [128, KT, 128], BF16) for i in range(2)]
        hs = K.sb(mx, "hs", [128, KT, NS], BF16)
        C["hc"], C["hs"] = hc, hs
        modulate(K, C, l, xT, _Shift(hs, 2048), 4, SH1, SC1, lambda d: hs.r())
        for name, tiles in (("ssd", range(0, 4)), ("rwkv", range(4, 6)), ("gla", range(6, 8))):
            if name not in en:
                for d in tiles:
                    for b, (c0, n) in enumerate(BLOCKS):
                        K.memset(yT.ap()[:, d, c0:c0 + n], 0.0, [yT.r((d, b))])
        if "ssd" in en:
            ssd_phase(K, dr, C, l, yT, dbg_out)
            P.barrier()
        if "rwkv" in en:
            rwkv_phase(K, dr, C, l, yT, dbg_out)
            P.barrier()
        if "gla" in en:
            gla_phase(K, dr, C, l, yT, dbg_out)
            P.barrier()


class _Shift:
    def __init__(self, t, off):
        self.t, self.off = t, off

    def ap(self):
        return _ShiftAP(self.t.ap(), self.off)

    def r(self, key=0):
        return self.t.r()


class _ShiftAP:
    def __init__(self, ap, off):
        self._ap, self.off = ap, off

    def __getitem__(self, key):
        p, d, s = key
        return self._ap[p, d, slice(s.start - self.off, s.stop - self.off)]


def ssd_phase(K, dr, C, l, yT, dbg_out):
    P = K.P
    nc = K.nc
    identb, identf, maskU, maskSL, ones1 = C["identb"], C["identf"], C["maskU"], C["maskSL"], C["ones1"]
    hc, hs = C["hc"], C["hs"]
    with contextlib.ExitStack() as ph:
        win = K.sb(ph, "win_ssd", [128, KT, 1288], BF16)
        K.dma(win.ap(), dr["w_in"][l, :, 0:1288].rearrange("(k p) n -> p k n", p=128), w=[win.r()], q="pool")
        convw = K.sb(ph, "convw", [128, 6, 4], F32)
        convb = K.sb(ph, "convb", [128, 6], F32)
        for i in range(4):
            K.dma(convw.ap()[:, :, i], dr["ssd_conv_w"][l, i].rearrange("(t p) -> p t", p=128), w=[convw.r()])
        K.dma(convb.ap(), dr["ssd_conv_b"][l].rearrange("(t p) -> p t", p=128), w=[convb.r()])
        normg = K.sb(ph, "normg", [128, 4], F32)
        K.dma(normg.ap(), dr["ssd_norm_g"][l].rearrange("(t p) -> p t", p=128), w=[normg.r()])
        dtbB = K.sb(ph, "dtbB", [128, 8], F32)
        aB = K.sb(ph, "aB", [128, 8], F32)
        dB = K.sb(ph, "dB", [128, 8], F32)
        K.dma(dtbB.ap(), dr["ssd_dt_bias"][l:l + 1, :].to_broadcast([128, 8]), w=[dtbB.r()])
        K.dma(aB.ap(), dr["ssd_a_log"][l:l + 1, :].to_broadcast([128, 8]), w=[aB.r()])
        K.dma(dB.ap(), dr["ssd_d"][l:l + 1, :].to_broadcast([128, 8]), w=[dB.r()])
        K.act(aB.ap(), aB.ap(), AF.Exp, [aB.r()], [aB.r()])
        K.ts(aB.ap(), aB.ap(), -1.0, ALU.mult, [aB.r()], [aB.r()])
        import os
        if os.environ.get("SKIP_SSD_PROMPT") != "1":
            ssd_prompt(K, dr, C, l, yT, win, convw, convb, normg, dtbB, aB, dB, dbg_out)
        P.barrier()
        if os.environ.get("SKIP_SSD_SAMPLE") != "1":
            ssd_sample(K, dr, C, l, yT, win, aB, dB, dtbB, dbg_out)


def ssd_prompt(K, dr, C, l, yT, win, convw, convb, normg, dtbB, aB, dB, dbg_out):
    P = K.P
    identb, identf, maskU, maskSL, ones1 = C["identb"], C["identf"], C["maskU"], C["maskSL"], C["ones1"]
    hc, hs = C["hc"], C["hs"]
    with contextlib.ExitStack() as ph:
        XB = [K.sb(ph, f"XB{i}", [128, 6, 131], F32) for i in range(2)]
        XC = [K.sb(ph, f"XC{i}", [128, 6, 128], BF16) for i in range(2)]
        cacc = [K.sb(ph, f"cacc{i}", [128, 128], F32) for i in range(2)]
        sz = K.sb(ph, "sz", [128, 512], F32)
        dtt = K.sb(ph, "dtt", [128, 8], F32)
        dtmp = K.sb(ph, "dtmp", [128, 8], F32)
        dtA = K.sb(ph, "dtA", [128, 8], F32)
        csb = K.sb(ph, "csb", [128, 16], F32)
        e1 = K.sb(ph, "e1", [128, 8], F32)
        el = K.sb(ph, "el", [128, 8], F32)
        tail = K.sb(ph, "tail", [128, 8], F32)
        Rt = K.sb(ph, "Rt", [128, 8, 128], F32)
        dec = K.sb(ph, "dec", [128, 8, 128], F32)
        Gs = K.sb(ph, "Gs", [128, 2, 128], F32)
        Mb = K.sb(ph, "Mb", [128, 8, 128], BF16)
        XT = K.sb(ph, "XT", [128, 640], BF16)
        xD = K.sb(ph, "xD", [128, 512], BF16)
        xw = K.sb(ph, "xw", [128, 512], BF16)
        t1 = K.sb(ph, "t1", [128, 512], F32)
        yn = K.sb(ph, "yn", [128, 512], BF16)
        ss = K.sb(ph, "ss", [128, 1], F32)
        HS32 = K.sb(ph, "HS32", [128, 4, 64], F32)
        HSb = K.sb(ph, "HSb", [128, 4, 64], BF16)
        hsT = K.sb(ph, "hsT", [128, 2, 128], F32)

        ps_x = K.ps(ph, "ps_x", [128, 1024], F32)
        ps_z = K.ps(ph, "ps_z", [128, 512], F32)
        ps_c = K.ps(ph, "ps_c", [128, 512], F32)
        ps_t = K.ps(ph, "ps_t", [128, 1024], BF16)
        ps_y = K.ps(ph, "ps_y", [128, 512], F32)
        ps_i = K.ps(ph, "ps_i", [128, 512], F32)
        ps_h = K.ps(ph, "ps_h", [128, 512], F32)

        K.memset(HS32.ap(), 0.0, [HS32.r()])
        K.memset(HSb.ap(), 0.0, [HSb.r()])
        K.memset(XB[0].ap()[:, :, 0:3], 0.0, [XB[0].r()])

        for c in range(NCH):
            h = hc[c % 2]
            make_hc(K, C, l, h, c)
            xb, xc = XB[c % 2], XC[c % 2]
            for ct in range(6):
                for k in range(KT):
                    K.mm(ps_x.ap()[:, ct * 128:(ct + 1) * 128], win.ap()[:, k, 512 + ct * 128:512 + (ct + 1) * 128],
                         h.ap()[:, k, :], [win.r(), h.r()], [ps_x.r()], start=(k == 0), stop=(k == KT - 1),
                         inc=(k == KT - 1 and ct == 5))
            K.cp(xb.ap()[:, :, 3:131], ps_x.ap()[:, 0:768].rearrange("p (t n) -> p t n", t=6), [ps_x.r()], [xb.r()],
                 eng="act")
            if c + 1 < NCH:
                K.cp(XB[(c + 1) % 2].ap()[:, :, 0:3], xb.ap()[:, :, 128:131], [xb.r()], [XB[(c + 1) % 2].r()])
            for ct in range(6):
                ca = cacc[ct % 2]
                K.ts(ca.ap(), xb.ap()[:, ct, 0:128], convw.ap()[:, ct, 0:1], ALU.mult, [xb.r(), convw.r(), convb.r()],
                     [ca.r()], s2=convb.ap()[:, ct:ct + 1], op1=ALU.add)
                for i in range(1, 4):
                    K.stt(ca.ap(), xb.ap()[:, ct, i:i + 128], convw.ap()[:, ct, i:i + 1], ca.ap(), ALU.mult, ALU.add,
                          [xb.r(), convw.r(), ca.r()], [ca.r()])
                K.act(xc.ap()[:, ct, :], ca.ap(), AF.Silu, [ca.r()], [xc.r()])
            for k in range(KT):
                K.mm(ps_z.ap(), h.ap()[:, k, :], win.ap()[:, k, 0:512], [h.r(), win.r()], [ps_z.r()],
                     start=(k == 0), stop=(k == KT - 1))
            for k in range(KT):
                K.mm(ps_c.ap()[:, 0:8], h.ap()[:, k, :], win.ap()[:, k, 1280:1288], [h.r(), win.r()], [ps_c.r()],
                     start=(k == 0), stop=(k == KT - 1))
            K.act(sz.ap(), ps_z.ap(), AF.Silu, [ps_z.r()], [sz.r()])
            K.tt(dtt.ap(), ps_c.ap()[:, 0:8], dtbB.ap(), ALU.add, [ps_c.r(), dtbB.r()], [dtt.r()])
            softplus_(K, (dtt.ap(), dtt.r()), (dtmp.ap(), dtmp.r()), None, None)
            K.tt(dtA.ap(), dtt.ap(), aB.ap(), ALU.mult, [dtt.r(), aB.r()], [dtA.r()])
            K.mm(ps_c.ap()[:, 8:16], maskU.ap(), dtA.ap(), [maskU.r(), dtA.r()], [ps_c.r()])
            K.mm(ps_c.ap()[:, 16:24], ones1.ap(), dtA.ap(), [ones1.r(), dtA.r()], [ps_c.r()])
            K.tt(Rt.ap(), bc(maskU.ap(), 1, [128, 8, 128]), bc(dtA.ap(), 2, [128, 8, 128]), ALU.mult,
                 [maskU.r(), dtA.r()], [Rt.r()])
            for hf in range(2):
                K.mm(ps_x.ap()[:, hf * 512:(hf + 1) * 512], maskSL.ap(),
                     Rt.ap()[:, hf * 4:(hf + 1) * 4, :].rearrange("p h i -> p (h i)"), [maskSL.r(), Rt.r()],
                     [ps_x.r()])
            K.act(dec.ap().rearrange("p h i -> p (h i)"), ps_x.ap(), AF.Exp, [ps_x.r()], [dec.r()])
            K.cp(csb.ap(), ps_c.ap()[:, 8:24], [ps_c.r()], [csb.r()], eng="act")
            for g in range(2):
                K.mm(ps_c.ap()[:, 256 + g * 128:256 + (g + 1) * 128], xc.ap()[64 * g:64 * g + 64, 4, :],
                     xc.ap()[64 * g:64 * g + 64, 5, :], [xc.r()], [ps_c.r()], self_wait=(g == 1))
            K.tt(Gs.ap(), ps_c.ap()[:, 256:512].rearrange("p (g i) -> p g i", g=2), bc(maskU.ap(), 1, [128, 2, 128]),
                 ALU.mult, [ps_c.r(), maskU.r()], [Gs.r()])
            K.tt(dec.ap().rearrange("p (g r) i -> p g r i", g=2), dec.ap().rearrange("p (g r) i -> p g r i", g=2),
                 bc(Gs.ap(), 2, [128, 2, 4, 128]), ALU.mult, [dec.r(), Gs.r()], [dec.r()])
            K.tt(Mb.ap(), dec.ap(), bc(dtt.ap(), 2, [128, 8, 128]), ALU.mult, [dec.r(), dtt.r()], [Mb.r()])
            for ct in range(5):
                K.tr(ps_t.ap()[:, ct * 128:(ct + 1) * 128], xc.ap()[:, ct, :], identb.ap(), [xc.r(), identb.r()],
                     [ps_t.r()], inc=(ct == 4))
            K.cp(XT.ap(), ps_t.ap()[:, 0:640], [ps_t.r()], [XT.r()], eng="act")
            K.tt(xD.ap().rearrange("p (h q) -> p h q", h=8), XT.ap()[:, 0:512].rearrange("p (h q) -> p h q", h=8),
                 bc(dB.ap(), 2, [128, 8, 64]), ALU.mult, [XT.r(), dB.r()], [xD.r()])
            K.mm(ps_y.ap(), identb.ap(), xD.ap(), [identb.r(), xD.r()], [ps_y.r()], start=True, stop=False)
            for hh in range(8):
                K.mm(ps_y.ap()[:, hh * 64:(hh + 1) * 64], Mb.ap()[:, hh, :], XT.ap()[:, hh * 64:(hh + 1) * 64],
                     [Mb.r(), XT.r()], [ps_y.r()], start=False, stop=(hh == 7))
            for g in range(2):
                K.mm(ps_i.ap()[:, g * 256:(g + 1) * 256], xc.ap()[64 * g:64 * g + 64, 5, :],
                     HSb.ap()[64 * g:64 * g + 64, :, :].rearrange("p h q -> p (h q)"), [xc.r(), HSb.r()], [ps_i.r()],
                     self_wait=(g == 1))
            K.act(e1.ap(), csb.ap()[:, 0:8], AF.Exp, [csb.r()], [e1.r()])
            K.tt(t1.ap().rearrange("p (h q) -> p h q", h=8), ps_i.ap().rearrange("p (h q) -> p h q", h=8),
                 bc(e1.ap(), 2, [128, 8, 64]), ALU.mult, [ps_i.r(), e1.r()], [t1.r()])
            K.tt(t1.ap(), t1.ap(), ps_y.ap(), ALU.add, [t1.r(), ps_y.r()], [t1.r()])
            ssd_epilogue(K, C, t1, sz, ss, yn, 128)
            for q in range(4):
                K.tr(ps_t.ap()[:, q * 128:(q + 1) * 128], yn.ap()[:, q * 128:(q + 1) * 128], identb.ap(),
                     [yn.r(), identb.r()], [ps_t.r()], inc=(q == 3))
            K.tt(yT.ap()[:, 0:4, c * 128:(c + 1) * 128], ps_t.ap()[:, 0:512].rearrange("p (t n) -> p t n", t=4),
                 bc(normg.ap(), 2, [128, 4, 128]), ALU.mult, [ps_t.r(), normg.r()], xr(yT, c // 4, range(4)))
            K.act(el.ap(), csb.ap()[:, 8:16], AF.Exp, [csb.r()], [el.r()])
            K.tt(tail.ap(), csb.ap()[:, 8:16], csb.ap()[:, 0:8], ALU.subtract, [csb.r()], [tail.r()])
            K.act(tail.ap(), tail.ap(), AF.Exp, [tail.r()], [tail.r()])
            K.tt(tail.ap(), tail.ap(), dtt.ap(), ALU.mult, [tail.r(), dtt.r()], [tail.r()])
            K.tt(xw.ap().rearrange("p (h q) -> p h q", h=8), XT.ap()[:, 0:512].rearrange("p (h q) -> p h q", h=8),
                 bc(tail.ap(), 2, [128, 8, 64]), ALU.mult, [XT.r(), tail.r()], [xw.r()])
            K.mm(ps_h.ap(), XT.ap()[:, 512:640], xw.ap(), [XT.r(), xw.r()], [ps_h.r()])
            for g in range(2):
                sl = slice(64 * g, 64 * g + 64)
                K.tt(HS32.ap()[sl], HS32.ap()[sl], bc(el.ap()[sl, 4 * g:4 * g + 4], 2, [64, 4, 64]), ALU.mult,
                     [HS32.r(), el.r()], [HS32.r()])
                K.tt(HS32.ap()[sl], HS32.ap()[sl],
                     ps_h.ap()[sl, 256 * g:256 * g + 256].rearrange("p (h q) -> p h q", h=4), ALU.add,
                     [HS32.r(), ps_h.r()], [HS32.r()])
            K.cp(HSb.ap(), HS32.ap(), [HS32.r()], [HSb.r()], eng="act")

        xb = XB[(NCH - 1) % 2]
        for i in range(3):
            K.dma(dr["p_conv"][l, i].rearrange("(t p) -> p t", p=128), xb.ap()[:, :, 128 + i], r=[xb.r()])
        for q in range(2):
            K.tr(ps_y.ap()[:, q * 128:(q + 1) * 128], HS32.ap().rearrange("p h q -> p (h q)")[:, q * 128:(q + 1) * 128],
                 identf.ap(), [HS32.r(), identf.r()], [ps_y.r()], inc=(q == 1))
        K.cp(hsT.ap(), ps_y.ap()[:, 0:256].rearrange("p (q n) -> p q n", q=2), [ps_y.r()], [hsT.r()])
        for g in range(2):
            for q in range(2):
                K.dma(dr["p_ssd"][l, 4 * g + 2 * q:4 * g + 2 * q + 2].rearrange("h p n -> (h p) n"),
                      hsT.ap()[:, q, 64 * g:64 * g + 64], r=[hsT.r()])
        P.barrier()


def ssd_epilogue(K, C, y, sz, ss, yn, n):
    K.tt(y.ap()[0:n], y.ap()[0:n], sz.ap()[0:n], ALU.mult, [y.r(), sz.r()], [y.r()])
    K.act(yn.ap()[0:n], y.ap()[0:n], AF.Square, [y.r()], [yn.r(), ss.r()], accum_out=ss.ap()[0:n])
    K.act(ss.ap()[0:n], ss.ap()[0:n], AF.Sqrt, [ss.r()], [ss.r()], scale=1.0 / 512, bias=RMS_EPS)
    K.recip(ss.ap()[0:n], ss.ap()[0:n], [ss.r()], [ss.r()])
    K.ts(yn.ap()[0:n], y.ap()[0:n], ss.ap()[0:n], ALU.mult, [y.r(), ss.r()], [yn.r()])


def dram_scratch(K, name, shape):
    K.uid += 1
    h = K.nc.dram_tensor(f"scr_{name}_{K.uid}", list(shape), F32)
    return Tn(h, name)


def ssd_sample(K, dr, C, l, yT, win, aB, dB, dtbB, dbg_out):
    P = K.P
    hs, identb = C["hs"], C["identb"]
    with contextlib.ExitStack() as ph:
        cs = K.sb(ph, "cs", [NS, 768], F32)
        wB = K.sb(ph, "wB", [NS, 768], F32)
        gB = K.sb(ph, "gB", [NS, 512], F32)
        xbcs = K.sb(ph, "xbcs", [NS, 768], F32)
        acc = K.sb(ph, "acc", [NS, 768], F32)
        tmpc = K.sb(ph, "tmpc", [NS, 768], F32)
        szs = K.sb(ph, "szs", [NS, 512], F32)
        dts = K.sb(ph, "dts", [NS, 8], F32)
        dtm = K.sb(ph, "dtm", [NS, 8], F32)
        rep = K.sb(ph, "rep", [NS, 2, 8, 64], F32)
        pk = K.sb(ph, "pk", [NS, 8, 3], F32)
        Hs = K.sb(ph, "Hs", [128, 64, 64], F32)
        tmpH = K.sb(ph, "tmpH", [128, 32, 64], F32)
        xh = K.sb(ph, "xh", [128, 64], F32)
        BCh = K.sb(ph, "BCh", [128, 2, 64], F32)
        pkh = K.sb(ph, "pkh", [128, 3], F32)
        dA = K.sb(ph, "dA", [128, 1], F32)
        xdt = K.sb(ph, "xdt", [128, 64], F32)
        yh = K.sb(ph, "yh", [128, 64], F32)
        ysm = K.sb(ph, "ysm", [NS, 512], F32)
        yns = K.sb(ph, "yns", [NS, 512], BF16)
        sss = K.sb(ph, "sss", [NS, 1], F32)
        ps_a = K.ps(ph, "pss_a", [128, 512], F32)
        ps_b = K.ps(ph, "pss_b", [128, 512], F32)
        ps_d = K.ps(ph, "pss_d", [128, 512], F32)
        ps_t = K.ps(ph, "pss_t", [128, 1024], BF16)
        sx = dram_scratch(K, "sx", [NS, 512])
        sbc = dram_scratch(K, "sbc", [2, NS, 512])
        spk = dram_scratch(K, "spk", [NS, 24])
        sy = dram_scratch(K, "sy", [NS, 512])

        K.dma(gB.ap(), dr["ssd_norm_g"][l:l + 1, :].to_broadcast([NS, 512]), w=[gB.r()])
        K.dma(Hs.ap().rearrange("p a b -> p (a b)"), dr["st_ssd"][l].rearrange("b h p n -> (b h) (p n)"), w=[Hs.r()])
        for k in range(KT):
            K.mm(ps_a.ap()[0:NS, :], hs.ap()[:, k, :], win.ap()[:, k, 0:512], [hs.r(), win.r()], [ps_a.r()],
                 start=(k == 0), stop=(k == KT - 1))
        for k in range(KT):
            K.mm(ps_b.ap()[0:NS, :], hs.ap()[:, k, :], win.ap()[:, k, 512:1024], [hs.r(), win.r()], [ps_b.r()],
                 start=(k == 0), stop=(k == KT - 1))
        for k in range(KT):
            K.mm(ps_d.ap()[0:NS, 0:264], hs.ap()[:, k, :], win.ap()[:, k, 1024:1288], [hs.r(), win.r()], [ps_d.r()],
                 start=(k == 0), stop=(k == KT - 1))
        K.act(szs.ap(), ps_a.ap()[0:NS, :], AF.Silu, [ps_a.r()], [szs.r()])
        K.cp(xbcs.ap()[:, 0:512], ps_b.ap()[0:NS, :], [ps_b.r()], [xbcs.r()], eng="act")
        K.cp(xbcs.ap()[:, 512:768], ps_d.ap()[0:NS, 0:256], [ps_d.r()], [xbcs.r()], eng="act")
        K.tt(dts.ap(), ps_d.ap()[0:NS, 256:264], dtbB.ap()[0:NS, :], ALU.add, [ps_d.r(), dtbB.r()], [dts.r()])
        softplus_(K, (dts.ap(), dts.r()), (dtm.ap(), dtm.r()), None, None)
        K.dma(wB.ap(), dr["ssd_conv_w"][l, 3:4, :].to_broadcast([NS, 768]), w=[wB.r()])
        K.tt(acc.ap(), xbcs.ap(), wB.ap(), ALU.mult, [xbcs.r(), wB.r()], [acc.r()])
        for i in range(3):
            K.dma(wB.ap(), dr["ssd_conv_w"][l, i:i + 1, :].to_broadcast([NS, 768]), w=[wB.r()])
            K.dma(cs.ap(), dr["st_conv"][l][:, i, :], w=[cs.r()])
            K.tt(tmpc.ap(), cs.ap(), wB.ap(), ALU.mult, [cs.r(), wB.r()], [tmpc.r()])
            K.tt(acc.ap(), acc.ap(), tmpc.ap(), ALU.add, [acc.r(), tmpc.r()], [acc.r()])
        K.dma(wB.ap(), dr["ssd_conv_b"][l:l + 1, :].to_broadcast([NS, 768]), w=[wB.r()])
        K.tt(acc.ap(), acc.ap(), wB.ap(), ALU.add, [acc.r(), wB.r()], [acc.r()])
        K.act(acc.ap(), acc.ap(), AF.Silu, [acc.r()], [acc.r()])
        K.dma(dr["s_conv"][l][:, 0:2, :], dr["st_conv"][l][:, 1:3, :])
        K.dma(dr["s_conv"][l][:, 2, :], xbcs.ap(), r=[xbcs.r()])
        K.dma(sx.ap(), acc.ap()[:, 0:512], r=[acc.r()], w=[sx.r()])
        K.cp(rep.ap().rearrange("p t (g r) n -> p t g r n", g=2),
             bc(acc.ap()[:, 512:768].rearrange("p (t g n) -> p t g n", t=2, g=2), 3, [NS, 2, 2, 4, 64]),
             [acc.r()], [rep.r()])
        K.cp(pk.ap()[:, :, 0], dts.ap(), [dts.r()], [pk.r()])
        K.cp(pk.ap()[:, :, 1], aB.ap()[0:NS, :], [aB.r()], [pk.r()])
        K.cp(pk.ap()[:, :, 2], dB.ap()[0:NS, :], [dB.r()], [pk.r()])
        K.dma(sbc.ap().rearrange("t b x -> b t x"), rep.ap().rearrange("p t h n -> p t (h n)"), r=[rep.r()],
              w=[sbc.r()])
        K.dma(spk.ap(), pk.ap().rearrange("p h q -> p (h q)"), r=[pk.r()], w=[spk.r()])
        K.dma(xh.ap(), sx.ap().rearrange("b (h p) -> (b h) p", h=8), r=[sx.r()], w=[xh.r()])
        for t in range(2):
            K.dma(BCh.ap()[:, t, :], sbc.ap()[t].rearrange("b (h n) -> (b h) n", h=8), r=[sbc.r()],
                  w=[BCh.r()])
        K.dma(pkh.ap(), spk.ap().rearrange("b (h q) -> (b h) q", h=8), r=[spk.r()], w=[pkh.r()])
        K.act(dA.ap(), pkh.ap()[:, 0:1], AF.Exp, [pkh.r()], [dA.r()], scale=pkh.ap()[:, 1:2])
        K.ts(xdt.ap(), xh.ap(), pkh.ap()[:, 0:1], ALU.mult, [xh.r(), pkh.r()], [xdt.r()])
        K.ts(Hs.ap(), Hs.ap(), dA.ap(), ALU.mult, [Hs.r(), dA.r()], [Hs.r()])
        for hf in range(2):
            sl = slice(32 * hf, 32 * hf + 32)
            K.tt(tmpH.ap(), bc(xdt.ap()[:, sl], 2, [128, 32, 64]), bc(BCh.ap()[:, 0, :], 1, [128, 32, 64]), ALU.mult,
                 [xdt.r(), BCh.r()], [tmpH.r()])
            K.tt(Hs.ap()[:, sl, :], Hs.ap()[:, sl, :], tmpH.ap(), ALU.add, [Hs.r(), tmpH.r()], [Hs.r()])
        K.dma(dr["s_ssd"][l].rearrange("b h p n -> (b h) (p n)"), Hs.ap().rearrange("p a b -> p (a b)"), r=[Hs.r()])
        for hf in range(2):
            sl = slice(32 * hf, 32 * hf + 32)
            K.tt(tmpH.ap(), Hs.ap()[:, sl, :], bc(BCh.ap()[:, 1, :], 1, [128, 32, 64]), ALU.mult, [Hs.r(), BCh.r()],
                 [tmpH.r()])
            K.red(yh.ap()[:, sl], tmpH.ap(), [tmpH.r()], [yh.r()])
        K.stt(yh.ap(), xh.ap(), pkh.ap()[:, 2:3], yh.ap(), ALU.mult, ALU.add, [xh.r(), pkh.r(), yh.r()], [yh.r()])
        K.dma(sy.ap().rearrange("b (h p) -> (b h) p", h=8), yh.ap(), r=[yh.r()], w=[sy.r()])
        K.dma(ysm.ap(), sy.ap(), r=[sy.r()], w=[ysm.r()])
        ssd_epilogue(K, C, ysm, szs, sss, yns, NS)
        K.tt(ysm.ap(), ysm.ap(), gB.ap(), ALU.mult, [ysm.r(), gB.r()], [ysm.r()])
        K.ts(yns.ap(), ysm.ap(), sss.ap(), ALU.mult, [ysm.r(), sss.r()], [yns.r()])
        for q in range(4):
            K.tr(ps_t.ap()[:, q * NS:(q + 1) * NS], yns.ap()[:, q * 128:(q + 1) * 128], identb.ap()[0:NS, 0:NS],
                 [yns.r(), identb.r()], [ps_t.r()], inc=(q == 3))
        K.cp(yT.ap()[:, 0:4, T:T + NS], ps_t.ap()[:, 0:4 * NS].rearrange("p (t n) -> p t n", t=4), [ps_t.r()],
             xr(yT, 4, range(4)))
        P.barrier()


def gla_phase(K, dr, C, l, yT, dbg_out):
    P = K.P
    identb, maskU = C["identb"], C["maskU"]
    hc, hs = C["hc"], C["hs"]
    G0 = OFF["gq"]
    with contextlib.ExitStack() as ph:
        win = K.sb(ph, "win_gla", [128, KT, 784], BF16)
        K.dma(win.ap(), dr["w_in"][l, :, G0:G0 + 784].rearrange("(k p) n -> p k n", p=128), w=[win.r()], q="pool")
        wgk2 = K.sb(ph, "wgk2", [16, 128], BF16)
        K.dma(wgk2.ap(), dr["gla_w_gk2"][l], w=[wgk2.r()], q="pool")
        bgkB = K.sb(ph, "bgkB", [128, 128], F32)
        K.dma(bgkB.ap(), dr["gla_b_gk"][l:l + 1, :].to_broadcast([128, 128]), w=[bgkB.r()])
        gcol = K.sb(ph, "gcol", [128, 1], F32)
        for t in range(2):
            K.dma(gcol.ap()[64 * t:64 * t + 64, :], dr["gla_norm_g"][l].rearrange("(e o) -> e o", o=1), w=[gcol.r()])
        BM = K.sb(ph, "BM", [128, 256], F32)
        hm = K.sb(ph, "hm", [128, 4], F32)
        K.memset(BM.ap(), 1.0, [BM.r()], eng="pool")
        K.memset(hm.ap(), 1.0, [hm.r()], eng="pool")
        for hh in range(4):
            for (t, sl, n) in ((BM, slice(64 * hh, 64 * hh + 64), 64), (hm, slice(hh, hh + 1), 1)):
                ap = t.ap()[:, sl]
                K.P.op("pool", lambda e, ap=ap, n=n, hh=hh: e.affine_select(
                    out=ap, in_=ap, pattern=[[0, n]], compare_op=ALU.is_ge, fill=0.0, base=-32 * hh,
                    channel_multiplier=1), reads=[t.r()], writes=[t.r()])
                K.P.op("pool", lambda e, ap=ap, n=n, hh=hh: e.affine_select(
                    out=ap, in_=ap, pattern=[[0, n]], compare_op=ALU.is_gt, fill=0.0, base=32 * hh + 32,
                    channel_multiplier=-1), reads=[t.r()], writes=[t.r()])
        gla_prompt(K, dr, C, l, yT, win, wgk2, bgkB, gcol, BM, hm)
        P.barrier()
        gla_sample(K, dr, C, l, yT, win, wgk2, bgkB)


def gla_prompt(K, dr, C, l, yT, win, wgk2, bgkB, gcol, BM, hm):
    P = K.P
    identb, maskU = C["identb"], C["maskU"]
    hc = C["hc"]
    with contextlib.ExitStack() as ph:
        glo = K.sb(ph, "glo", [16, 128], BF16)
        lg = K.sb(ph, "lg", [128, 128], F32)
        lgt = K.sb(ph, "lgt", [128, 128], F32)
        Eq = K.sb(ph, "Eq", [128, 128], F32)
        Ek = K.sb(ph, "Ek", [128, 128], F32)
        Ekt = K.sb(ph, "Ekt", [128, 128], F32)
        qt = K.sb(ph, "qt", [128, 128], BF16)
        kf = K.sb(ph, "kf", [128, 128], F32)
        km = K.sb(ph, "km", [128, 4, 128], BF16)
        ktm = K.sb(ph, "ktm", [128, 128], BF16)
        vtm = K.sb(ph, "vtm", [128, 256], BF16)
        sgg = K.sb(ph, "sgg", [128, 256], F32)
        A = K.sb(ph, "A", [128, 4, 128], BF16)
        osq = K.sb(ph, "osq", [128, 256], F32)
        ms = K.sb(ph, "ms", [128, 4], F32)
        on = K.sb(ph, "on", [128, 256], F32)
        onb = K.sb(ph, "onb", [128, 256], BF16)
        tmpS = K.sb(ph, "tmpS", [128, 256], F32)
        S32 = K.sb(ph, "S32", [128, 256], F32)
        Sb = K.sb(ph, "Sb", [128, 256], BF16)
        ps_f = K.ps(ph, "psg_f", [128, 512], F32)
        ps_m = K.ps(ph, "psg_m", [128, 512], F32)
        ps_g = K.ps(ph, "psg_g", [128, 512], F32)
        ps_l = K.ps(ph, "psg_l", [128, 512], F32)
        ps_a = K.ps(ph, "psg_a", [128, 512], F32)
        ps_o = K.ps(ph, "psg_o", [128, 512], F32)
        ps_t = K.ps(ph, "psg_t", [128, 1024], BF16)
        K.memset(S32.ap(), 0.0, [S32.r()])
        K.memset(Sb.ap(), 0.0, [Sb.r()])
        for c in range(NCH):
            h = hc[c % 2]
            make_hc(K, C, l, h, c)
            for (dst, cols, M) in ((ps_f.ap()[:, 0:128], slice(0, 128), 128), (ps_f.ap()[:, 128:256], slice(128, 256), 128),
                                   (ps_f.ap()[0:16, 256:384], slice(512, 528), 16)):
                for k in range(KT):
                    K.mm(dst, win.ap()[:, k, cols], h.ap()[:, k, :], [win.r(), h.r()], [ps_f.r()],
                         start=(k == 0), stop=(k == KT - 1))
            for (dst, cols, pst) in ((ps_m.ap()[:, 0:256], slice(256, 512), ps_m), (ps_m.ap()[:, 256:384], slice(128, 256), ps_m),
                                     (ps_g.ap()[:, 0:256], slice(528, 784), ps_g)):
                for k in range(KT):
                    K.mm(dst, h.ap()[:, k, :], win.ap()[:, k, cols], [win.r(), h.r()], [pst.r()],
                         start=(k == 0), stop=(k == KT - 1))
            K.cp(glo.ap(), ps_f.ap()[0:16, 256:384], [ps_f.r()], [glo.r()], eng="act")
            K.mm(ps_l.ap()[:, 0:128], glo.ap(), wgk2.ap(), [glo.r(), wgk2.r()], [ps_l.r()])
            K.stt(lg.ap(), ps_l.ap()[:, 0:128], -1.0, bgkB.ap(), ALU.mult, ALU.subtract, [ps_l.r(), bgkB.r()], [lg.r()])
            softplus_(K, (lg.ap(), lg.r()), (lgt.ap(), lgt.r()), None, None)
            K.ts(lg.ap(), lg.ap(), -1.0 / 16.0, ALU.mult, [lg.r()], [lg.r()])
            K.mm(ps_l.ap()[:, 128:256], lg.ap(), maskU.ap(), [lg.r(), maskU.r()], [ps_l.r()])
            K.mm(ps_l.ap()[:, 256:384], maskU.ap(), lg.ap(), [lg.r(), maskU.r()], [ps_l.r()])
            K.act(Eq.ap(), ps_l.ap()[:, 128:256], AF.Exp, [ps_l.r()], [Eq.r()])
            K.act(Ek.ap(), ps_l.ap()[:, 128:256], AF.Exp, [ps_l.r()], [Ek.r()], scale=-1.0)
            K.act(Ekt.ap(), ps_l.ap()[:, 256:384], AF.Exp, [ps_l.r()], [Ekt.r()], scale=-1.0)
            K.stt(qt.ap(), ps_f.ap()[:, 0:128], 32.0 ** -0.5, Eq.ap(), ALU.mult, ALU.mult, [ps_f.r(), Eq.r()], [qt.r()])
            K.tt(kf.ap(), ps_f.ap()[:, 128:256], Ek.ap(), ALU.mult, [ps_f.r(), Ek.r()], [kf.r()])
            K.tt(km.ap(), bc(kf.ap(), 1, [128, 4, 128]), bc(hm.ap(), 2, [128, 4, 128]), ALU.mult, [kf.r(), hm.r()],
                 [km.r()])
            K.tt(ktm.ap(), ps_m.ap()[:, 256:384], Ekt.ap(), ALU.mult, [ps_m.r(), Ekt.r()], [ktm.r()])
            K.cp(vtm.ap(), ps_m.ap()[:, 0:256], [ps_m.r()], [vtm.r()], eng="act")
            K.act(sgg.ap(), ps_g.ap()[:, 0:256], AF.Silu, [ps_g.r()], [sgg.r()])
            for hh in range(4):
                K.mm(ps_a.ap()[:, hh * 128:(hh + 1) * 128], km.ap()[:, hh, :], qt.ap(), [km.r(), qt.r()], [ps_a.r()],
                     inc=(hh == 3))
            K.tt(A.ap(), ps_a.ap().rearrange("p (h i) -> p h i", h=4), bc(maskU.ap(), 1, [128, 4, 128]), ALU.mult,
                 [ps_a.r(), maskU.r()], [A.r()])
            K.mm(ps_o.ap()[:, 0:256], qt.ap(), Sb.ap(), [qt.r(), Sb.r()], [ps_o.r()], start=True, stop=False)
            for hh in range(4):
                K.mm(ps_o.ap()[:, hh * 64:(hh + 1) * 64], A.ap()[:, hh, :], vtm.ap()[:, hh * 64:(hh + 1) * 64],
                     [A.r(), vtm.r()], [ps_o.r()], start=False, stop=(hh == 3))
            K.act(osq.ap(), ps_o.ap()[:, 0:256], AF.Square, [ps_o.r()], [osq.r()])
            K.red(ms.ap(), osq.ap().rearrange("p (h e) -> p h e", h=4), [osq.r()], [ms.r()])
            K.act(ms.ap(), ms.ap(), AF.Sqrt, [ms.r()], [ms.r()], scale=1.0 / 64, bias=RMS_EPS)
            K.recip(ms.ap(), ms.ap(), [ms.r()], [ms.r()])
            K.tt(on.ap().rearrange("p (h e) -> p h e", h=4), ps_o.ap()[:, 0:256].rearrange("p (h e) -> p h e", h=4),
                 bc(ms.ap(), 2, [128, 4, 64]), ALU.mult, [ps_o.r(), ms.r()], [on.r()])
            K.tt(onb.ap(), on.ap(), sgg.ap(), ALU.mult, [on.r(), sgg.r()], [onb.r()])
            for q in range(2):
                K.tr(ps_t.ap()[:, q * 128:(q + 1) * 128], onb.ap()[:, q * 128:(q + 1) * 128], identb.ap(),
                     [onb.r(), identb.r()], [ps_t.r()], inc=(q == 1))
            K.ts(yT.ap()[:, 6:8, c * 128:(c + 1) * 128], ps_t.ap()[:, 0:256].rearrange("p (t n) -> p t n", t=2),
                 gcol.ap(), ALU.mult, [ps_t.r(), gcol.r()], xr(yT, c // 4, range(6, 8)))
            K.mm(ps_o.ap()[:, 256:512], ktm.ap(), vtm.ap(), [ktm.r(), vtm.r()], [ps_o.r()])
            K.tt(tmpS.ap(), ps_o.ap()[:, 256:512], BM.ap(), ALU.mult, [ps_o.r(), BM.r()], [tmpS.r()])
            K.tt(S32.ap(), S32.ap(), tmpS.ap(), ALU.add, [S32.r(), tmpS.r()], [S32.r()])
            K.ts(S32.ap(), S32.ap(), Eq.ap()[:, 127:128], ALU.mult, [S32.r(), Eq.r()], [S32.r()])
            K.cp(Sb.ap(), S32.ap(), [S32.r()], [Sb.r()], eng="act")
        for hh in range(4):
            K.dma(dr["p_gla"][l, hh], S32.ap()[32 * hh:32 * hh + 32, 64 * hh:64 * hh + 64], r=[S32.r()])
        P.barrier()


def gla_sample(K, dr, C, l, yT, win, wgk2, bgkB):
    P = K.P
    hs, identb = C["hs"], C["identb"]
    with contextlib.ExitStack() as ph:
        glo = K.sb(ph, "glos", [16, NS], BF16)
        lg = K.sb(ph, "lgs", [NS, 128], F32)
        lgt = K.sb(ph, "lgts", [NS, 128], F32)
        pk = K.sb(ph, "pkg", [NS, 4, 160], F32)
        sgg = K.sb(ph, "sggs", [NS, 256], F32)
        gB = K.sb(ph, "gBg", [64, 64], F32)
        S = K.sb(ph, "Sg", [64, 32, 64], F32)
        tmp = K.sb(ph, "tmpg", [64, 32, 64], F32)
        pkh = K.sb(ph, "pkhg", [64, 160], F32)
        o = K.sb(ph, "og", [64, 64], F32)
        junk = K.sb(ph, "junkg", [64, 64], F32)
        ss = K.sb(ph, "ssg", [64, 1], F32)
        otm = K.sb(ph, "otm", [NS, 256], F32)
        otb = K.sb(ph, "otb", [NS, 256], BF16)
        ps_a = K.ps(ph, "psgs_a", [128, 512], F32)
        ps_b = K.ps(ph, "psgs_b", [128, 512], F32)
        ps_c = K.ps(ph, "psgs_c", [128, 512], F32)
        ps_t = K.ps(ph, "psgs_t", [128, 1024], BF16)
        spk = dram_scratch(K, "gpk", [NS, 640])
        so = dram_scratch(K, "go", [NS, 256])
        K.dma(gB.ap(), dr["gla_norm_g"][l:l + 1, :].to_broadcast([64, 64]), w=[gB.r()])
        K.dma(S.ap().rearrange("p d e -> p (d e)"), dr["st_gla"][l].rearrange("b h d e -> (b h) (d e)"), w=[S.r()])
        for k in range(KT):
            K.mm(ps_a.ap()[0:NS, :], hs.ap()[:, k, :], win.ap()[:, k, 0:512], [hs.r(), win.r()], [ps_a.r()],
                 start=(k == 0), stop=(k == KT - 1))
        for k in range(KT):
            K.mm(ps_b.ap()[0:NS, 0:256], hs.ap()[:, k, :], win.ap()[:, k, 528:784], [hs.r(), win.r()], [ps_b.r()],
                 start=(k == 0), stop=(k == KT - 1))
        for k in range(KT):
            K.mm(ps_c.ap()[0:16, 0:NS], win.ap()[:, k, 512:528], hs.ap()[:, k, :], [hs.r(), win.r()], [ps_c.r()],
                 start=(k == 0), stop=(k == KT - 1))
        K.cp(glo.ap(), ps_c.ap()[0:16, 0:NS], [ps_c.r()], [glo.r()], eng="act")
        K.mm(ps_c.ap()[0:NS, 128:256], glo.ap(), wgk2.ap(), [glo.r(), wgk2.r()], [ps_c.r()])
        K.stt(lg.ap(), ps_c.ap()[0:NS, 128:256], -1.0, bgkB.ap()[0:NS, :], ALU.mult, ALU.subtract,
              [ps_c.r(), bgkB.r()], [lg.r()])
        softplus_(K, (lg.ap(), lg.r()), (lgt.ap(), lgt.r()), None, None)
        K.act(lg.ap(), lg.ap(), AF.Exp, [lg.r()], [lg.r()], scale=-1.0 / 16.0)
        K.act(sgg.ap(), ps_b.ap()[0:NS, 0:256], AF.Silu, [ps_b.r()], [sgg.r()])
        K.ts(pk.ap()[:, :, 0:32], ps_a.ap()[0:NS, 0:128].rearrange("p (h d) -> p h d", h=4), 32.0 ** -0.5, ALU.mult,
             [ps_a.r()], [pk.r()])
        K.cp(pk.ap()[:, :, 32:64], ps_a.ap()[0:NS, 128:256].rearrange("p (h d) -> p h d", h=4), [ps_a.r()], [pk.r()])
        K.cp(pk.ap()[:, :, 64:96], lg.ap().rearrange("p (h d) -> p h d", h=4), [lg.r()], [pk.r()])
        K.cp(pk.ap()[:, :, 96:160], ps_a.ap()[0:NS, 256:512].rearrange("p (h e) -> p h e", h=4), [ps_a.r()], [pk.r()])
        K.dma(spk.ap(), pk.ap().rearrange("p h x -> p (h x)"), r=[pk.r()], w=[spk.r()])
        K.dma(pkh.ap(), spk.ap().rearrange("b (h x) -> (b h) x", h=4), r=[spk.r()], w=[pkh.r()])
        qh, kh, eh, vh = pkh.ap()[:, 0:32], pkh.ap()[:, 32:64], pkh.ap()[:, 64:96], pkh.ap()[:, 96:160]
        K.tt(S.ap(), S.ap(), bc(eh, 2, [64, 32, 64]), ALU.mult, [S.r(), pkh.r()], [S.r()])
        K.tt(tmp.ap(), bc(kh, 2, [64, 32, 64]), bc(vh, 1, [64, 32, 64]), ALU.mult, [pkh.r()], [tmp.r()])
        K.tt(S.ap(), S.ap(), tmp.ap(), ALU.add, [S.r(), tmp.r()], [S.r()])
        K.dma(dr["s_gla"][l].rearrange("b h d e -> (b h) (d e)"), S.ap().rearrange("p d e -> p (d e)"), r=[S.r()])
        K.tt(tmp.ap(), S.ap(), bc(qh, 2, [64, 32, 64]), ALU.mult, [S.r(), pkh.r()], [tmp.r()])
        K.red(o.ap(), tmp.ap().rearrange("p d e -> p e d"), [tmp.r()], [o.r()])
        K.act(junk.ap(), o.ap(), AF.Square, [o.r()], [junk.r(), ss.r()], accum_out=ss.ap())
        K.act(ss.ap(), ss.ap(), AF.Sqrt, [ss.r()], [ss.r()], scale=1.0 / 64, bias=RMS_EPS)
        K.recip(ss.ap(), ss.ap(), [ss.r()], [ss.r()])
        K.stt(o.ap(), o.ap(), ss.ap(), gB.ap(), ALU.mult, ALU.mult, [o.r(), ss.r(), gB.r()], [o.r()])
        K.dma(so.ap().rearrange("b (h e) -> (b h) e", h=4), o.ap(), r=[o.r()], w=[so.r()])
        K.dma(otm.ap(), so.ap(), r=[so.r()], w=[otm.r()])
        K.tt(otb.ap(), otm.ap(), sgg.ap(), ALU.mult, [otm.r(), sgg.r()], [otb.r()])
        for q in range(2):
            K.tr(ps_t.ap()[:, q * NS:(q + 1) * NS], otb.ap()[:, q * 128:(q + 1) * 128], identb.ap()[0:NS, 0:NS],
                 [otb.r(), identb.r()], [ps_t.r()], inc=(q == 1))
        K.cp(yT.ap()[:, 6:8, T:T + NS], ps_t.ap()[:, 0:2 * NS].rearrange("p (t n) -> p t n", t=2), [ps_t.r()],
             xr(yT, 4, range(6, 8)))
        P.barrier()


C0 = float(np.exp(-0.5))


def rwkv_prep(K, C, pc, LW, N, rw, prev, B, pl, pg, pn):
    blk64 = C["blk64"]
    MX, LI = B["MX"], B["LI"]
    mxa = MX.ap()[:, :, 0:N]
    K.tt(mxa, prev, rw, ALU.subtract, B["_rw_res"], [MX.r()])
    yield
    K.tt(mxa, mxa, bc(pc["mu"].ap(), 2, [128, 7, N]), ALU.mult, [MX.r(), pc["mu"].r()], [MX.r()])
    yield
    K.tt(mxa, mxa, rw, ALU.add, [MX.r()] + B["_rw_res"], [MX.r()])
    yield
    r, k, v = (MX.ap()[:, 0:2, 0:N], MX.ap()[:, 2:4, 0:N], MX.ap()[:, 4:6, 0:N])
    lia = LI.ap()[:, 0:N]
    K.act(lia[0:32], MX.ap()[0:32, 6, 0:N], AF.Tanh, [MX.r()], [LI.r()])
    yield
    K.cp(lia[32:64], MX.ap()[32:64, 6, 0:N], [MX.r()], [LI.r()], eng="act")
    yield
    K.act(lia[64:128], MX.ap()[64:128, 6, 0:N], AF.Sigmoid, [MX.r()], [LI.r()])
    yield
    for t in range(2):
        cs = slice(t * 128, (t + 1) * 128)
        K.mm(pl.ap()[:, t * N:(t + 1) * N], LW.ap()[0:32, cs], lia[0:32], [LW.r(), LI.r()], [pl.r()], self_wait=True)
        K.mm(pl.ap()[:, (2 + t) * N:(3 + t) * N], LW.ap()[32:64, cs], lia[32:64], [LW.r(), LI.r()], [pl.r()],
             self_wait=True)
        K.mm(pg.ap()[:, t * N:(t + 1) * N], LW.ap()[64:128, cs], lia[64:128], [LW.r(), LI.r()], [pg.r()],
             self_wait=True)
    g = lambda n: B[n].ap()[:, :, 0:N]
    for t in range(2):
        K.act(B["sig"].ap()[:, t, 0:N], pl.ap()[:, t * N:(t + 1) * N], AF.Sigmoid, [pl.r(), pc["w0"].r()],
              [B["sig"].r()], bias=pc["w0"].ap()[:, t:t + 1])
        K.act(B["aic"].ap()[:, t, 0:N], pl.ap()[:, (2 + t) * N:(3 + t) * N], AF.Sigmoid, [pl.r(), pc["a0"].r()],
              [B["aic"].r()], bias=pc["a0"].ap()[:, t:t + 1])
    K.cp(g("gate"), pg.ap()[:, 0:2 * N].rearrange("p (t n) -> p t n", t=2), [pg.r()], [B["gate"].r()], eng="act")
    yield
    K.tt(g("kk"), k, bc(pc["k_k"].ap(), 2, [128, 2, N]), ALU.mult, [MX.r(), pc["k_k"].r()], [B["kk"].r()])
    yield
    K.tt(g("t1"), g("kk"), g("kk"), ALU.mult, [B["kk"].r()], [B["t1"].r()])
    yield
    for t in range(2):
        K.mm(pn.ap()[:, t * N:(t + 1) * N], blk64.ap(), B["t1"].ap()[:, t, 0:N], [blk64.r(), B["t1"].r()], [pn.r()])
    K.act(g("t1"), pn.ap()[:, 0:2 * N].rearrange("p (t n) -> p t n", t=2), AF.Sqrt, [pn.r()], [B["t1"].r()],
          bias=1e-12)
    K.recip(g("t1"), g("t1"), [B["t1"].r()], [B["t1"].r()])
    yield
    K.tt(g("kk"), g("kk"), g("t1"), ALU.mult, [B["kk"].r(), B["t1"].r()], [B["kk"].r()])
    yield
    yield
    K.tt(g("t1"), g("aic"), bc(pc["k_a"].ap(), 2, [128, 2, N]), ALU.mult, [B["aic"].r(), pc["k_a"].r()], [B["t1"].r()])
    yield
    K.tt(g("t1"), g("t1"), bc(pc["omka"].ap(), 2, [128, 2, N]), ALU.add, [B["t1"].r(), pc["omka"].r()], [B["t1"].r()])
    yield
    K.tt(g("kp"), k, g("t1"), ALU.mult, [MX.r(), B["t1"].r()], [B["kp"].r()])
    yield
    K.tt(g("t1"), r, g("kp"), ALU.mult, [MX.r(), B["kp"].r()], [B["t1"].r()])
    yield
    K.tt(g("t1"), g("t1"), bc(pc["r_k"].ap(), 2, [128, 2, N]), ALU.mult, [B["t1"].r(), pc["r_k"].r()], [B["t1"].r()])
    yield
    for t in range(2):
        K.mm(pn.ap()[:, t * N:(t + 1) * N], blk64.ap(), B["t1"].ap()[:, t, 0:N], [blk64.r(), B["t1"].r()], [pn.r()])
    K.tt(g("bonus"), pn.ap()[:, 0:2 * N].rearrange("p (t n) -> p t n", t=2), v, ALU.mult, [pn.r(), MX.r()],
         [B["bonus"].r()])
    B['_rkv'] = (r, k, v)
    yield


def rwkv_params(K, dr, l, ph):
    pc = {}
    mu = K.sb(ph, "mu", [128, 7], F32)
    K.dma(mu.ap(), dr["rwkv_mu"][l].rearrange("(t p) -> p t", p=128), w=[mu.r()])
    pc["mu"] = mu
    for n, src in (("w0", dr["rwkv_w0"][l]), ("a0", dr["rwkv_a0"][l]), ("k_k", dr["rwkv_k_k"][l]),
                   ("k_a", dr["rwkv_k_a"][l]), ("r_k", dr["rwkv_r_k"][l].rearrange("h n -> (h n)")),
                   ("ln_g", dr["rwkv_ln_g"][l]), ("ln_b", dr["rwkv_ln_b"][l])):
        t = K.sb(ph, "pc_" + n, [128, 2], F32)
        K.dma(t.ap(), src.rearrange("(t p) -> p t", p=128), w=[t.r()])
        pc[n] = t
    omka = K.sb(ph, "omka", [128, 2], F32)
    K.ts(omka.ap(), pc["k_a"].ap(), -1.0, ALU.mult, [pc["k_a"].r()], [omka.r()], s2=1.0, op1=ALU.add)
    pc["omka"] = omka
    LW = K.sb(ph, "LW", [128, 256], BF16)
    K.dma(LW.ap()[0:32, :], dr["rwkv_w2"][l], w=[LW.r()], q="pool")
    K.dma(LW.ap()[32:64, :], dr["rwkv_a2"][l], w=[LW.r()], q="pool")
    K.dma(LW.ap()[64:128, :], dr["rwkv_g2"][l], w=[LW.r()], q="pool")
    return pc, LW


def rwkv_epilogue(K, C, pc, B, N, pT, ydst, yres):
    for t in range(2):
        K.act(B["t1"].ap()[:, t, 0:N], pT.ap()[:, t * N:(t + 1) * N], AF.Identity, [pT.r(), pc["ln_g"].r(), pc["ln_b"].r()],
              [B["t1"].r()], scale=pc["ln_g"].ap()[:, t:t + 1], bias=pc["ln_b"].ap()[:, t:t + 1])
    g = lambda n: B[n].ap()[:, :, 0:N]
    K.tt(g("t1"), g("t1"), g("bonus"), ALU.add, [B["t1"].r(), B["bonus"].r()], [B["t1"].r()])
    K.tt(ydst, g("t1"), g("gate"), ALU.mult, [B["t1"].r(), B["gate"].r()], yres)


def groupnorm64(K, o_ap, n, G, scr, res_in, out_ap, out_res):
    mean, xc, sq, var = scr
    K.red(mean.ap()[0:n, 0:G], o_ap, res_in, [mean.r()])
    K.ts(mean.ap()[0:n, 0:G], mean.ap()[0:n, 0:G], 1.0 / 64, ALU.mult, [mean.r()], [mean.r()])
    xca = xc.ap()[0:n, 0:G * 64].rearrange("p (g e) -> p g e", g=G)
    K.tt(xca, o_ap, bc(mean.ap()[0:n, 0:G], 2, [n, G, 64]), ALU.subtract, res_in + [mean.r()], [xc.r()])
    sqa = sq.ap()[0:n, 0:G * 64].rearrange("p (g e) -> p g e", g=G)
    K.tt(sqa, xca, xca, ALU.mult, [xc.r()], [sq.r()])
    K.red(var.ap()[0:n, 0:G], sqa, [sq.r()], [var.r()])
    K.act(var.ap()[0:n, 0:G], var.ap()[0:n, 0:G], AF.Sqrt, [var.r()], [var.r()], scale=1.0 / 64, bias=RWKV_GN_EPS)
    K.recip(var.ap()[0:n, 0:G], var.ap()[0:n, 0:G], [var.r()], [var.r()])
    K.tt(out_ap, xca, bc(var.ap()[0:n, 0:G], 2, [n, G, 64]), ALU.mult, [xc.r(), var.r()], out_res)


def rwkv_phase(K, dr, C, l, yT, dbg_out):
    P = K.P
    R0 = OFF["rw"]
    with contextlib.ExitStack() as ph:
        win = K.sb(ph, "win_rwkv", [128, KT, 896], BF16)
        K.dma(win.ap(), dr["w_in"][l, :, R0:R0 + 896].rearrange("(k p) n -> p k n", p=128), w=[win.r()], q="pool")
        pc, LW = rwkv_params(K, dr, l, ph)
        import os
        if os.environ.get("SKIP_RWKV_PROMPT") != "1":
            rwkv_prompt(K, dr, C, l, yT, win, pc, LW, dbg_out)
        P.barrier()
        if os.environ.get("SKIP_RWKV_SAMPLE") != "1":
            rwkv_sample(K, dr, C, l, yT, win, pc, LW, dbg_out)


def interleave(gens, ratio=None):
    gens = [g for g in gens if g is not None]
    ratio = ratio or [1] * len(gens)
    live = list(zip(gens, ratio))
    while live:
        for item in list(live):
            g, n = item
            for _ in range(n):
                try:
                    next(g)
                except StopIteration:
                    live.remove(item)
                    break


def rwkv_prompt(K, dr, C, l, yT, win, pc, LW, dbg_out):
    P = K.P
    identb, identf, maskU, maskSU, maskSL, blk64 = (C[k] for k in ["identb", "identf", "maskU", "maskSU", "maskSL", "blk64"])
    hc = C["hc"]
    N = 128
    with contextlib.ExitStack() as ph:
        f3 = lambda n: K.sb(ph, n, [128, 2, N], F32)
        Bs = []
        for i in range(2):
            B = {n: f3(f"rb{i}_" + n) for n in ["sig", "aic", "gate", "kk", "t1", "kp", "bonus", "cs", "e1", "e2", "bb"]}
            Bs.append(B)
        MX = K.sb(ph, "MX", [128, 7, N], F32)
        LI = K.sb(ph, "LI", [128, N], BF16)
        for B in Bs:
            B["MX"], B["LI"] = MX, LI
        RW = [K.sb(ph, f"RW{i}", [128, 7, N + 1], F32) for i in range(2)]
        ones_r = K.sb(ph, "ones_r", [128, N], F32)
        bcol = K.sb(ph, "bcol", [128, 2], F32)
        MK2 = K.sb(ph, "MK2", [128, 2, N], F32)
        ARs = [K.sb(ph, f"AR{i}", [128, 2, 2, N], BF16) for i in range(2)]
        BKs = [K.sb(ph, f"BK{i}", [128, 2, 2, N], BF16) for i in range(2)]
        FH = K.sb(ph, "FH", [128, 3, 2, N], BF16)
        TMs = [K.sb(ph, f"TM{i}", [128, 4, 2, N], BF16) for i in range(2)]
        t2 = f3("rb_t2")
        A1 = K.sb(ph, "A1", [128, 4, 2, N], BF16)
        A2 = K.sb(ph, "A2", [128, 4, 2, N], BF16)
        Lb = [K.sb(ph, f"Lb{i}", [128, 4, N], BF16) for i in range(2)]
        Nb = [K.sb(ph, f"Nb{i}", [128, 4, N], BF16) for i in range(2)]
        X32 = K.sb(ph, "X32", [128, 4, 2, 64], F32)
        Xb = K.sb(ph, "Xb", [128, 4, 2, 64], BF16)
        Apf = K.sb(ph, "Apf", [128, 2, N], BF16)
        XAc = K.sb(ph, "XAc", [128, 256], BF16)
        Utm = K.sb(ph, "Utm", [128, 4, 64], BF16)
        ST32 = K.sb(ph, "ST32", [128, 2, N], F32)
        STb = K.sb(ph, "STb", [128, 2, N], BF16)
        tmpS = K.sb(ph, "tmpSr", [128, 2, N], F32)
        gn = (K.sb(ph, "gn_mean", [128, 4], F32), K.sb(ph, "gn_xc", [128, 256], F32),
              K.sb(ph, "gn_sq", [128, 256], F32), K.sb(ph, "gn_var", [128, 4], F32))
        onb = K.sb(ph, "onbr", [128, 256], BF16)
        stT = K.sb(ph, "stT", [128, 2, N], F32)
        pI = K.ps(ph, "pr_I", [128, 512], F32)
        pM = K.ps(ph, "pr_M", [128, 512], F32)
        pT = K.ps(ph, "pr_T", [128, 1024], BF16)
        pT2 = pT
        pAT = K.ps(ph, "pr_AT", [128, 1024], F32)
        pL = K.ps(ph, "pr_L", [128, 512], F32)
        pL2 = K.ps(ph, "pr_L2", [128, 512], F32)
        pX = K.ps(ph, "pr_X", [128, 512], F32)
        pO = pX

        K.memset(ones_r.ap(), 1.0, [ones_r.r()])
        K.cp(MK2.ap()[:, 0, :], maskSU.ap(), [maskSU.r()], [MK2.r()])
        K.cp(MK2.ap()[:, 1, :], maskU.ap(), [maskU.r()], [MK2.r()])
        K.memset(ST32.ap(), 0.0, [ST32.r()])
        K.memset(STb.ap(), 0.0, [STb.r()])
        K.memset(RW[0].ap()[:, :, 0:1], 0.0, [RW[0].r()])

        def s1(c):
            B, AR, BK, TM = Bs[c % 2], ARs[c % 2], BKs[c % 2], TMs[c % 2]
            g = lambda n: B[n].ap()
            h = hc[c % 2]
            make_hc(K, C, l, h, c)
            yield
            rw = RW[c % 2]
            for (t0, t1) in ((0, 4), (4, 7)):
                for t in range(t0, t1):
                    for k in range(KT):
                        K.mm(pI.ap()[:, (t - t0) * N:(t - t0 + 1) * N], win.ap()[:, k, t * N:(t + 1) * N], h.ap()[:, k, :],
                             [win.r(), h.r()], [pI.r()], start=(k == 0), stop=(k == KT - 1))
                    yield
                K.cp(rw.ap()[:, t0:t1, 1:N + 1], pI.ap()[:, 0:(t1 - t0) * N].rearrange("p (t n) -> p t n", t=t1 - t0),
                     [pI.r()], [rw.r()], eng="act")
                yield
            if c + 1 < NCH:
                K.cp(RW[(c + 1) % 2].ap()[:, :, 0:1], rw.ap()[:, :, N:N + 1], [rw.r()], [RW[(c + 1) % 2].r()])
            B["_rw_res"] = [rw.r()]
            yield from rwkv_prep(K, C, pc, LW, N, rw.ap()[:, :, 1:N + 1], rw.ap()[:, :, 0:N], B, pM, pI, pI)
            r, k_, v = B["_rkv"]
            MXr = MX.r()
            for t in range(2):
                K.P.op("dve", lambda e, t=t, B=B: e.tensor_tensor_scan(out=B["cs"].ap()[:, t, :], data0=ones_r.ap(),
                                                                        data1=B["sig"].ap()[:, t, :], initial=0.0,
                                                                        op0=ALU.mult, op1=ALU.add),
                       reads=[ones_r.r(), B["sig"].r()], writes=[B["cs"].r()])
            yield
            K.act(g("e1"), g("cs"), AF.Exp, [B["cs"].r()], [B["e1"].r()], scale=-C0)
            yield
            K.act(g("e2"), g("cs"), AF.Exp, [B["cs"].r()], [B["e2"].r()], scale=C0)
            yield
            K.tt(AR.ap()[:, :, 1, :], r, g("e1"), ALU.mult, [MXr, B["e1"].r()], [AR.r()])
            yield
            K.tt(g("bb"), g("kk"), g("aic"), ALU.mult, [B["kk"].r(), B["aic"].r()], [B["bb"].r()])
            yield
            K.tt(BK.ap()[:, :, 0, :], g("bb"), g("e2"), ALU.mult, [B["bb"].r(), B["e2"].r()], [BK.r()])
            yield
            K.tt(BK.ap()[:, :, 1, :], g("kp"), g("e2"), ALU.mult, [B["kp"].r(), B["e2"].r()], [BK.r()])
            yield
            K.tt(g("t1"), g("cs"), g("sig"), ALU.subtract, [B["cs"].r(), B["sig"].r()], [B["t1"].r()])
            yield
            K.act(g("e2"), g("t1"), AF.Exp, [B["t1"].r()], [B["e2"].r()], scale=-C0)
            yield
            K.stt(AR.ap()[:, :, 0, :], g("kk"), -1.0, g("e2"), ALU.mult, ALU.mult, [B["kk"].r(), B["e2"].r()], [AR.r()])
            yield
            K.ts(bcol.ap(), B["cs"].ap()[:, :, N - 1], -C0, ALU.mult, [B["cs"].r()], [bcol.r()])
            yield
            for t in range(2):
                K.act(B["e2"].ap()[:, t, :], B["cs"].ap()[:, t, :], AF.Exp, [B["cs"].r(), bcol.r()], [B["e2"].r()],
                      scale=C0, bias=bcol.ap()[:, t:t + 1])
            yield
            K.tt(FH.ap()[:, 0], g("bb"), g("e2"), ALU.mult, [B["bb"].r(), B["e2"].r()], [FH.r()])
            yield
            K.tt(FH.ap()[:, 1], g("kp"), g("e2"), ALU.mult, [B["kp"].r(), B["e2"].r()], [FH.r()])
            yield
            K.cp(FH.ap()[:, 2], v, [MXr], [FH.r()], eng="act")
            yield
            for q in range(4):
                for t in range(2):
                    src = FH.ap()[:, q, t, :] if q < 3 else AR.ap()[:, t, 0, :]
                    K.tr(pT.ap()[:, (q * 2 + t) * N:(q * 2 + t + 1) * N], src, identb.ap(),
                         [FH.r(), AR.r(), identb.r()], [pT.r()], inc=(t == 1))
                yield
            K.cp(TM.ap().rearrange("p q t n -> p (q t n)"), pT.ap(), [pT.r()], [TM.r()], eng="act")
            yield

        def s2(c):
            B, AR, BK, TM = Bs[c % 2], ARs[c % 2], BKs[c % 2], TMs[c % 2]
            mk = bc(MK2.ap(), 1, [128, 4, 2, N])
            for which, Adst in ((0, A1), (1, A2)):
                for hd in range(4):
                    t, o = hd // 2, 64 * (hd % 2)
                    sl = slice(o, o + 64)
                    arf = AR.ap()[sl, t].rearrange("p a n -> p (a n)")
                    K.mm(pAT.ap()[:, hd * 256:(hd + 1) * 256], BK.ap()[sl, t, which, :], arf, [BK.r(), AR.r()], [pAT.r()],
                         self_wait=True)
                yield
                K.tt(Adst.ap(), pAT.ap().rearrange("p (h a n) -> p h a n", h=4, a=2), mk, ALU.mult, [pAT.r(), MK2.r()],
                     [Adst.r()])
                yield
            for hd in range(4):
                t, o = hd // 2, 64 * (hd % 2)
                sl = slice(o, o + 64)
                K.mm(pL.ap()[:, hd * N:(hd + 1) * N], AR.ap()[sl, t, 0, :], BK.ap()[sl, t, 0, :], [BK.r(), AR.r()],
                     [pL.r()], self_wait=True)
            yield
            K.tt(Lb[0].ap(), pL.ap().rearrange("p (h n) -> p h n", h=4), bc(maskSL.ap(), 1, [128, 4, N]), ALU.mult,
                 [pL.r(), maskSL.r()], [Lb[0].r()])
            yield
            vtm = TM.ap()[:, 2].rearrange("p t n -> p (t n)")
            for hd in range(4):
                K.mm(pX.ap()[:, hd * 64:(hd + 1) * 64], A2.ap()[:, hd, 0, :], vtm[:, hd * 64:(hd + 1) * 64],
                     [A2.r(), TM.r()], [pX.r()], inc=(hd == 3))
            yield
            K.cp(X32.ap()[:, :, 0, :], TM.ap()[:, 3].rearrange("p t (hh k) -> p (t hh) k", hh=2), [TM.r()], [X32.r()])
            yield
            K.cp(X32.ap()[:, :, 1, :], pX.ap()[:, 0:256].rearrange("p (h v) -> p h v", h=4), [pX.r()], [X32.r()],
                 eng="act")
            yield
            K.cp(Xb.ap(), X32.ap(), [X32.r()], [Xb.r()], eng="act")
            yield
            for i in range(7):
                if i == 0:
                    nref = lambda hd: A1.ap()[:, hd, 0, :]
                    nres = A1.r()
                    lcur = Lb[0]
                else:
                    nprev_ref, nprev_res, lprev = nref, nres, lcur
                    nnew, lnew = Nb[i % 2], Lb[i % 2]
                    for hd in range(4):
                        K.mm(pL.ap()[:, hd * N:(hd + 1) * N], lprev.ap()[:, hd, :], nprev_ref(hd), [lprev.r(), nprev_res],
                             [pL.r()], inc=(hd == 3))
                    yield
                    if i < 6:
                        for hd in range(4):
                            K.mm(pL2.ap()[:, hd * N:(hd + 1) * N], nprev_ref(hd), lprev.ap()[:, hd, :],
                                 [lprev.r(), nprev_res], [pL2.r()], inc=(hd == 3))
                        yield
                    K.cp(nnew.ap(), pL.ap().rearrange("p (h n) -> p h n", h=4), [pL.r()], [nnew.r()], eng="act")
                    yield
                    if i < 6:
                        K.cp(lnew.ap(), pL2.ap().rearrange("p (h n) -> p h n", h=4), [pL2.r()], [lnew.r()])
                        yield
                    nref = lambda hd, nnew=nnew: nnew.ap()[:, hd, :]
                    nres = nnew.r()
                    lcur = lnew
                for hd in range(4):
                    K.mm(pX.ap()[:, hd * N:(hd + 1) * N], nref(hd), Xb.ap()[:, hd].rearrange("p a k -> p (a k)"),
                         [nres, Xb.r()], [pX.r()], inc=(hd == 3))
                yield
                K.tt(X32.ap(), X32.ap(), pX.ap().rearrange("p (h a k) -> p h a k", h=4, a=2), ALU.add,
                     [X32.r(), pX.r()], [X32.r()])
                yield
                K.cp(Xb.ap(), X32.ap(), [X32.r()], [Xb.r()], eng="act")
                yield
            K.cp(XAc.ap().rearrange("p (h k) -> p h k", h=4), X32.ap()[:, :, 0, :], [X32.r()], [XAc.r()])
            yield
            for t in range(2):
                K.tr(pT2.ap()[:, t * N:(t + 1) * N], XAc.ap()[:, t * N:(t + 1) * N], identb.ap(), [XAc.r(), identb.r()],
                     [pT2.r()], inc=(t == 1))
            yield
            K.cp(Apf.ap(), pT2.ap()[:, 0:2 * N].rearrange("p (t n) -> p t n", t=2), [pT2.r()], [Apf.r()], eng="act")
            yield
            for t in range(2):
                K.mm(pX.ap()[:, t * N:(t + 1) * N], Apf.ap()[:, t, :], STb.ap()[:, t, :], [Apf.r(), STb.r()], [pX.r()],
                     inc=(t == 1))
            yield
            K.tt(Utm.ap(), pX.ap()[:, 0:256].rearrange("p (h v) -> p h v", h=4), X32.ap()[:, :, 1, :], ALU.add,
                 [pX.r(), X32.r()], [Utm.r()])
            yield
            for t in range(2):
                K.mm(pO.ap()[:, t * N:(t + 1) * N], AR.ap()[:, t, 1, :], STb.ap()[:, t, :], [AR.r(), STb.r()], [pO.r()],
                     start=(t == 0), stop=False, sgc=True)
            for hd in range(4):
                K.mm(pO.ap()[:, hd * 64:(hd + 1) * 64], A1.ap()[:, hd, 1, :], Utm.ap()[:, hd, :], [A1.r(), Utm.r()],
                     [pO.r()], start=False, stop=False, sgc=True)
                K.mm(pO.ap()[:, hd * 64:(hd + 1) * 64], A2.ap()[:, hd, 1, :], vtm[:, hd * 64:(hd + 1) * 64],
                     [A2.r(), TM.r()], [pO.r()], start=False, stop=False, sgc=True)
            for t in range(2):
                K.mm(pO.ap()[:, 256 + t * N:256 + (t + 1) * N], TM.ap()[:, 0, t, :],
                     Utm.ap()[:, 2 * t:2 * t + 2, :].rearrange("p h v -> p (h v)"), [TM.r(), Utm.r()], [pO.r()],
                     start=False, stop=False, sgc=True)
                K.mm(pO.ap()[:, 256 + t * N:256 + (t + 1) * N], TM.ap()[:, 1, t, :], vtm[:, t * N:(t + 1) * N],
                     [TM.r()], [pO.r()], start=False, stop=(t == 1), inc=(t == 1), sgc=True)
            yield
            K.tt(tmpS.ap(), pO.ap()[:, 256:512].rearrange("p (t n) -> p t n", t=2), bc(blk64.ap(), 1, [128, 2, N]),
                 ALU.mult, [pO.r(), blk64.r()], [tmpS.r()])
            yield
            K.tt(ST32.ap(), ST32.ap(), bc(B["e1"].ap()[:, :, N - 1], 2, [128, 2, N]), ALU.mult, [ST32.r(), B["e1"].r()],
                 [ST32.r()])
            yield
            K.tt(ST32.ap(), ST32.ap(), tmpS.ap(), ALU.add, [ST32.r(), tmpS.r()], [ST32.r()])
            yield
            K.cp(STb.ap(), ST32.ap(), [ST32.r()], [STb.r()], eng="act")
            yield
            groupnorm64(K, pO.ap()[:, 0:256].rearrange("p (h v) -> p h v", h=4), 128, 4, gn, [pO.r()],
                        onb.ap().rearrange("p (h v) -> p h v", h=4), [onb.r()])
            yield
            for t in range(2):
                K.tr(pT2.ap()[:, t * N:(t + 1) * N], onb.ap()[:, t * N:(t + 1) * N], identb.ap(), [onb.r(), identb.r()],
                     [pT2.r()], inc=(t == 1))
            yield
            B2 = dict(B)
            B2["t1"] = t2
            rwkv_epilogue(K, C, pc, B2, N, pT2, yT.ap()[:, 4:6, c * N:(c + 1) * N], xr(yT, c // 4, range(4, 6)))
            yield

        for _ in s1(0):
            pass
        for c in range(NCH):
            interleave([s2(c), s1(c + 1) if c + 1 < NCH else None], ratio=[3, 2])
        rw = RW[(NCH - 1) % 2]
        K.dma(dr["p_shift"][l].rearrange("(t p) -> p t", p=128), rw.ap()[:, :, N], r=[rw.r()])
        for t in range(2):
            K.tr(pL.ap()[:, t * N:(t + 1) * N], ST32.ap()[:, t, :], identf.ap(), [ST32.r(), identf.r()], [pL.r()],
                 inc=(t == 1))
        K.cp(stT.ap(), pL.ap()[:, 0:2 * N].rearrange("p (t n) -> p t n", t=2), [pL.r()], [stT.r()])
        for hd in range(4):
            t, o = hd // 2, 64 * (hd % 2)
            K.dma(dr["p_rwkv"][l, hd], stT.ap()[o:o + 64, t, o:o + 64], r=[stT.r()])
        P.barrier()


def rwkv_sample(K, dr, C, l, yT, win, pc, LW, dbg_out):
    P = K.P
    hs, identb, identf = C["hs"], C["identb"], C["identf"]
    N = NS
    with contextlib.ExitStack() as ph:
        f3 = lambda n: K.sb(ph, n, [128, 2, N], F32)
        B = {n: f3("rs_" + n) for n in ["sig", "aic", "gate", "kk", "t1", "kp", "bonus", "e1", "e2", "bb"]}
        B["MX"] = K.sb(ph, "MXs", [128, 7, N], F32)
        B["LI"] = K.sb(ph, "LIs", [128, N], BF16)
        rws = K.sb(ph, "rws", [128, 7, N], F32)
        prevs = K.sb(ph, "prevs", [128, 7, N], F32)
        shs = K.sb(ph, "shs", [NS, 896], F32)
        rwtm = K.sb(ph, "rwtm", [NS, 896], F32)
        pkT = K.sb(ph, "pkT", [NS, 6, 256], F32)
        pkh = K.sb(ph, "pkhr", [64, 6, 64], F32)
        S = K.sb(ph, "Sr", [64, 64, 64], F32)
        tmp = K.sb(ph, "tmpr", [64, 64, 64], F32)
        sa = K.sb(ph, "sa", [64, 64], F32)
        o = K.sb(ph, "orr", [64, 64], F32)
        on = K.sb(ph, "onr", [64, 64], F32)
        gn = (K.sb(ph, "gns_mean", [64, 1], F32), K.sb(ph, "gns_xc", [64, 64], F32),
              K.sb(ph, "gns_sq", [64, 64], F32), K.sb(ph, "gns_var", [64, 1], F32))
        otm = K.sb(ph, "otmr", [NS, 256], F32)
        otb = K.sb(ph, "otbr", [NS, 256], BF16)
        pA = K.ps(ph, "prs_A", [128, 1024], F32)
        pB = K.ps(ph, "prs_B", [128, 1024], F32)
        pL = K.ps(ph, "prs_L", [128, 512], F32)
        pM = K.ps(ph, "prs_M", [128, 512], F32)
        pT = K.ps(ph, "prs_T", [128, 1024], BF16)
        scr = dram_scratch(K, "rpk", [NS, 4, 6, 64])
        so = dram_scratch(K, "ro", [NS, 256])

        K.dma(shs.ap(), dr["st_shift"][l], w=[shs.r()])
        K.dma(S.ap().rearrange("p v k -> p (v k)"), dr["st_rwkv"][l].rearrange("b h v k -> (b h) (v k)"), w=[S.r()])
        for t in range(7):
            for k in range(KT):
                K.mm(pM.ap()[:, t * N:(t + 1) * N], win.ap()[:, k, t * 128:(t + 1) * 128], hs.ap()[:, k, :],
                     [win.r(), hs.r()], [pM.r()], start=(k == 0), stop=(k == KT - 1), inc=(k == KT - 1 and t == 6))
        K.cp(rws.ap(), pM.ap()[:, 0:7 * N].rearrange("p (t n) -> p t n", t=7), [pM.r()], [rws.r()], eng="act")
        for (c0, c1) in ((0, 512), (512, 896)):
            for k in range(KT):
                K.mm(pA.ap()[0:NS, c0:c1], hs.ap()[:, k, :], win.ap()[:, k, c0:c1], [win.r(), hs.r()], [pA.r()],
                     start=(k == 0), stop=(k == KT - 1))
        K.cp(rwtm.ap(), pA.ap()[0:NS, 0:896], [pA.r()], [rwtm.r()], eng="act")
        K.dma(dr["s_shift"][l], rwtm.ap(), r=[rwtm.r()])
        for t in range(7):
            K.tr(pL.ap()[:, t * N:(t + 1) * N], shs.ap()[:, t * 128:(t + 1) * 128], identf.ap()[0:NS, 0:NS],
                 [shs.r(), identf.r()], [pL.r()], inc=(t == 6))
        K.cp(prevs.ap(), pL.ap()[:, 0:7 * N].rearrange("p (t n) -> p t n", t=7), [pL.r()], [prevs.r()])
        B["_rw_res"] = [rws.r(), prevs.r()]
        for _ in rwkv_prep(K, C, pc, LW, N, rws.ap(), prevs.ap(), B, pB, pL, pM):
            pass
        r, k_, v = B["_rkv"]
        g = lambda n: B[n].ap()
        K.act(g("e1"), g("sig"), AF.Exp, [B["sig"].r()], [B["e1"].r()], scale=-C0)
        K.ts(g("e2"), g("kk"), -1.0, ALU.mult, [B["kk"].r()], [B["e2"].r()])
        K.tt(g("bb"), g("kk"), g("aic"), ALU.mult, [B["kk"].r(), B["aic"].r()], [B["bb"].r()])
        srcs = [(r, B["MX"].r()), (g("e1"), B["e1"].r()), (g("kp"), B["kp"].r()), (v, B["MX"].r()),
                (g("e2"), B["e2"].r()), (g("bb"), B["bb"].r())]
        for q, (ap, res) in enumerate(srcs):
            pp = pA if q < 4 else pB
            for t in range(2):
                col = ((q % 4) * 2 + t) * 128
                K.tr(pp.ap()[0:NS, col:col + 128], ap[:, t, :], identf.ap(), [res, identf.r()], [pp.r()])
        K.cp(pkT.ap()[:, 0:4, :], pA.ap()[0:NS, :].rearrange("p (q n) -> p q n", q=4), [pA.r()], [pkT.r()], eng="act")
        K.cp(pkT.ap()[:, 4:6, :], pB.ap()[0:NS, 0:512].rearrange("p (q n) -> p q n", q=2), [pB.r()], [pkT.r()])
        for q in range(6):
            K.dma(scr.ap()[:, :, q, :], pkT.ap()[:, q, :].rearrange("p (h k) -> p h k", h=4), r=[pkT.r()], w=[scr.r()])
        K.dma(pkh.ap(), scr.ap().rearrange("b h q k -> (b h) q k"), r=[scr.r()], w=[pkh.r()])
        rq, wq, kq, vq, aq, bq = (pkh.ap()[:, i, :] for i in range(6))
        K.tt(tmp.ap(), S.ap(), bc(aq, 1, [64, 64, 64]), ALU.mult, [S.r(), pkh.r()], [tmp.r()])
        K.red(sa.ap(), tmp.ap(), [tmp.r()], [sa.r()])
        K.tt(S.ap(), S.ap(), bc(wq, 1, [64, 64, 64]), ALU.mult, [S.r(), pkh.r()], [S.r()])
        K.tt(tmp.ap(), bc(sa.ap(), 2, [64, 64, 64]), bc(bq, 1, [64, 64, 64]), ALU.mult, [sa.r(), pkh.r()], [tmp.r()])
        K.tt(S.ap(), S.ap(), tmp.ap(), ALU.add, [S.r(), tmp.r()], [S.r()])
        K.tt(tmp.ap(), bc(vq, 2, [64, 64, 64]), bc(kq, 1, [64, 64, 64]), ALU.mult, [pkh.r()], [tmp.r()])
        K.tt(S.ap(), S.ap(), tmp.ap(), ALU.add, [S.r(), tmp.r()], [S.r()])
        K.dma(dr["s_rwkv"][l].rearrange("b h v k -> (b h) (v k)"), S.ap().rearrange("p v k -> p (v k)"), r=[S.r()])
        K.tt(tmp.ap(), S.ap(), bc(rq, 1, [64, 64, 64]), ALU.mult, [S.r(), pkh.r()], [tmp.r()])
        K.red(o.ap(), tmp.ap(), [tmp.r()], [o.r()])
        groupnorm64(K, o.ap().rearrange("p (g e) -> p g e", g=1), 64, 1, gn, [o.r()],
                    on.ap().rearrange("p (g e) -> p g e", g=1), [on.r()])
        K.dma(so.ap().rearrange("b (h v) -> (b h) v", h=4), on.ap(), r=[on.r()], w=[so.r()])
        K.dma(otm.ap(), so.ap(), r=[so.r()], w=[otm.r()])
        K.cp(otb.ap(), otm.ap(), [otm.r()], [otb.r()])
        for t in range(2):
            K.tr(pT.ap()[:, t * N:(t + 1) * N], otb.ap()[:, t * 128:(t + 1) * 128], identb.ap()[0:NS, 0:NS],
                 [otb.r(), identb.r()], [pT.r()], inc=(t == 1))
        rwkv_epilogue(K, C, pc, B, N, pT, yT.ap()[:, 4:6, T:T + NS], xr(yT, 4, range(4, 6)))
        P.barrier()
```

```python
import contextlib
import numpy as np
import concourse.bass as bass
import concourse.mybir as mybir
from concourse.bass_utils import run_bass_kernel_spmd

F32 = mybir.dt.float32
BF16 = mybir.dt.bfloat16
AF = mybir.ActivationFunctionType
ALU = mybir.AluOpType
AX = mybir.AxisListType

NCORES = 8
D = 1024
KT = 8
T = 2048
NS = 16
NT = T + NS
NCH = T // 128
DEPTH = 2
IN_DIM = 2968
OFF = dict(z=0, xbc=512, dt=1280, rw=1288, gq=2184, gk=2312, gv=2440, glo=2696, gg=2712)
F_DENSE = 2816
ALPHA = (2.0 * DEPTH) ** 0.25
LN_EPS = 1e-5
RMS_EPS = 1e-6
RWKV_GN_EPS = 64 * 1e-5
BLOCKS = [(0, 512), (512, 512), (1024, 512), (1536, 512), (2048, 16)]

ENGS = ["pe", "dve", "act", "pool", "sp"]


class Res:
    __slots__ = ("name", "w", "r", "excl")

    def __init__(self, name="", excl=False):
        self.name = name
        self.w = None
        self.r = []
        self.excl = excl


class Prog:
    NDMA = 8

    def __init__(self, nc):
        self.nc = nc
        self.q = {e: [] for e in ENGS}
        self.cnt = {e: 0 for e in ENGS}
        self.seen = {e: {} for e in ENGS}
        self.dma_i = {e: 0 for e in ENGS}
        self.dma_last = {}
        self.sems = {}

    def sem(self, key):
        if key not in self.sems:
            self.sems[key] = self.nc.alloc_semaphore(name="s_" + "_".join(str(k) for k in key))
        return self.sems[key]

    def _collect(self, eng, reads, writes):
        waits = {}

        def add(tok):
            if tok is None:
                return
            key, val = tok
            if self.seen[eng].get(key, 0) >= val:
                return
            if waits.get(key, 0) < val:
                waits[key] = val

        for r in reads:
            add(r.w)
        for w in writes:
            add(w.w)
            for t in w.r:
                add(t)
        if eng == "pe":
            waits.pop(("e", "pe"), None)
        for k, v in waits.items():
            self.seen[eng][k] = v
        return list(waits.items())

    def _commit(self, tok, reads, writes):
        for r in reads:
            r.r.append(tok)
            if len(r.r) > 64:
                r.r = _prune(r.r)
        for w in writes:
            w.w = tok
            w.r = []

    def op(self, eng, fn, reads=(), writes=(), inc=True, self_wait=False):
        assert inc or eng == "pe"
        if any(r.excl for r in reads):
            writes = list(writes) + [r for r in reads if r.excl]
            reads = [r for r in reads if not r.excl]
        waits = self._collect(eng, reads, writes)
        if self_wait and self.cnt[eng] > 0:
            waits.append((("e", eng), self.cnt[eng]))
        tok = (("e", eng), self.cnt[eng] + 1)
        if inc:
            self.cnt[eng] += 1
        self._commit(tok, reads, writes)

        def emit(e, fn=fn, waits=waits, inc=inc, eng=eng):
            for k, v in waits:
                e.wait_ge(self.sem(k), v)
            ins = fn(e)
            if inc:
                ins.then_inc(self.sem(("e", eng)), 1)
        self.q[eng].append(emit)
        return tok

    def dma(self, queue, out, in_, reads=(), writes=(), **kw):
        i = self.dma_i[queue]
        self.dma_i[queue] += 1
        slot = i % self.NDMA
        key = ("d", queue, slot)
        val = 16 * (i // self.NDMA + 1)
        waits = self._collect(queue, reads, writes)
        prev = val - 16
        if prev > 0 and self.seen[queue].get(key, 0) < prev:
            self.seen[queue][key] = prev
            waits.append((key, prev))
        tok = (key, val)
        self.dma_last[key] = val
        self._commit(tok, reads, writes)

        def emit(e, waits=waits, key=key):
            for k, v in waits:
                e.wait_ge(self.sem(k), v)
            e.dma_start(out=out, in_=in_, **kw).then_inc(self.sem(key), 16)
        self.q[queue].append(emit)
        return tok

    def barrier(self, engines=ENGS):
        toks = [(("e", e), self.cnt[e]) for e in ENGS if self.cnt[e] > 0]
        toks += list(self.dma_last.items())
        for eng in engines:
            waits = []
            for k, v in toks:
                if k == ("e", eng) and eng == "pe":
                    continue
                if self.seen[eng].get(k, 0) < v:
                    self.seen[eng][k] = v
                    waits.append((k, v))

            def emit(e, waits=waits):
                for k, v in waits:
                    e.wait_ge(self.sem(k), v)
            self.q[eng].append(emit)

    def emit(self):
        with self.nc.Block() as block:
            @block.tensor
            def _(e):
                for f in self.q["pe"]:
                    f(e)

            @block.vector
            def _(e):
                for f in self.q["dve"]:
                    f(e)

            @block.scalar
            def _(e):
                for f in self.q["act"]:
                    f(e)

            @block.gpsimd
            def _(e):
                for f in self.q["pool"]:
                    f(e)

            @block.sync
            def _(e):
                for f in self.q["sp"]:
                    f(e)


def _prune(toks):
    best = {}
    for k, v in toks:
        if best.get(k, 0) < v:
            best[k] = v
    return list(best.items())


class Tn:
    def __init__(self, h, name, excl=False):
        self.h = h
        self.name = name
        self._res = {}
        self.excl = excl

    def ap(self):
        return self.h.ap()

    def r(self, key=0):
        if key not in self._res:
            self._res[key] = Res(f"{self.name}:{key}", self.excl)
        return self._res[key]


class KB:
    def __init__(self, nc):
        self.nc = nc
        self.P = Prog(nc)
        self.uid = 0

    def sb(self, stack, name, shape, dt=F32):
        self.uid += 1
        h = stack.enter_context(self.nc.sbuf_tensor(f"{name}_{self.uid}", list(shape), dt))
        return Tn(h, name)

    def ps(self, stack, name, shape, dt=F32):
        self.uid += 1
        h = stack.enter_context(self.nc.psum_tensor(f"{name}_{self.uid}", list(shape), dt))
        return Tn(h, name, excl=True)

    def mm(self, out, lhsT, rhs, r, w, start=True, stop=True, inc=None, self_wait=False, sgc=False):
        inc = stop if inc is None else inc
        kw = {"skip_group_check": True} if sgc else {}
        self.P.op("pe", lambda e: e.matmul(out, lhsT=lhsT, rhs=rhs, start=start, stop=stop, **kw),
                  reads=r, writes=w, inc=inc, self_wait=self_wait)

    def tr(self, out, in_, ident, r, w, inc=True):
        self.P.op("pe", lambda e: e.transpose(out, in_, ident), reads=r, writes=w, inc=inc)

    def act(self, out, in_, func, r, w, scale=None, bias=None, accum_out=None):
        kw = {}
        if scale is not None:
            kw["scale"] = scale
        if bias is not None:
            kw["bias"] = bias
        if accum_out is not None:
            kw["accum_out"] = accum_out
        self.P.op("act", lambda e: e.activation(out=out, in_=in_, func=func, **kw), reads=r, writes=w)

    def tt(self, out, in0, in1, op, r, w, eng="dve"):
        self.P.op(eng, lambda e: e.tensor_tensor(out=out, in0=in0, in1=in1, op=op), reads=r, writes=w)

    def ts(self, out, in0, s1, op0, r, w, s2=None, op1=None, eng="dve", accum_out=None):
        kw = {}
        if op1 is not None:
            kw["op1"] = op1
        if accum_out is not None:
            kw["accum_out"] = accum_out
        self.P.op(eng, lambda e: e.tensor_scalar(out=out, in0=in0, scalar1=s1, scalar2=s2, op0=op0, **kw),
                  reads=r, writes=w)

    def stt(self, out, in0, scalar, in1, op0, op1, r, w, eng="dve"):
        self.P.op(eng, lambda e: e.scalar_tensor_tensor(out=out, in0=in0, scalar=scalar, in1=in1, op0=op0, op1=op1),
                  reads=r, writes=w)

    def cp(self, out, in_, r, w, eng="dve"):
        if eng == "act":
            self.P.op("act", lambda e: e.activation(out=out, in_=in_, func=AF.Copy), reads=r, writes=w)
        else:
            self.P.op(eng, lambda e: e.tensor_copy(out=out, in_=in_), reads=r, writes=w)

    def recip(self, out, in_, r, w):
        self.P.op("dve", lambda e: e.reciprocal(out=out, in_=in_), reads=r, writes=w)

    def red(self, out, in_, r, w, op=ALU.add, axis=AX.X, eng="dve"):
        self.P.op(eng, lambda e: e.tensor_reduce(out=out, in_=in_, axis=axis, op=op), reads=r, writes=w)

    def memset(self, ap, val, w, eng="dve"):
        self.P.op(eng, lambda e: e.memset(ap, val), writes=w)

    def dma(self, out, in_, r=(), w=(), q="sp", **kw):
        return self.P.dma(q, out, in_, reads=r, writes=w, **kw)


W_SHAPES = dict(
    w_ada=[DEPTH, D, 6 * D], b_ada=[DEPTH, 6 * D], w_in=[DEPTH, D, IN_DIM], w_out=[DEPTH, D, D],
    ssd_conv_w=[DEPTH, 4, 768], ssd_conv_b=[DEPTH, 768], ssd_dt_bias=[DEPTH, 8], ssd_a_log=[DEPTH, 8],
    ssd_d=[DEPTH, 8], ssd_norm_g=[DEPTH, 512], rwkv_mu=[DEPTH, 896], rwkv_w0=[DEPTH, 256],
    rwkv_w2=[DEPTH, 32, 256], rwkv_a0=[DEPTH, 256], rwkv_a2=[DEPTH, 32, 256], rwkv_g2=[DEPTH, 64, 256],
    rwkv_k_k=[DEPTH, 256], rwkv_k_a=[DEPTH, 256], rwkv_r_k=[DEPTH, 4, 64], rwkv_ln_g=[DEPTH, 256],
    rwkv_ln_b=[DEPTH, 256], gla_w_gk2=[DEPTH, 16, 128], gla_b_gk=[DEPTH, 128], gla_norm_g=[DEPTH, 64],
    ln_mix_g=[DEPTH, D], ln_mix_b=[DEPTH, D], ln_ffn_g=[DEPTH, D], ln_ffn_b=[DEPTH, D],
    ffn_w_gate=[1, D, F_DENSE], ffn_w_up=[1, D, F_DENSE], ffn_w_down=[1, F_DENSE, D],
    moe_router=[1, D, 8], moe_w_gate=[1, 8, D, D], moe_w_up=[1, 8, D, D], moe_w_down=[1, 8, D, D],
)
IN_SHAPES = dict(
    xp=[T, D], xs=[NS, D], cc=[1 + NS, D],
    st_ssd=[DEPTH, NS, 8, 64, 64], st_conv=[DEPTH, NS, 3, 768], st_rwkv=[DEPTH, NS, 4, 64, 64],
    st_shift=[DEPTH, NS, 896], st_gla=[DEPTH, NS, 4, 32, 64],
)
OUT_SHAPES = dict(
    y_p=[T, D], y_s=[NS, D],
    p_ssd=[DEPTH, 8, 64, 64], p_conv=[DEPTH, 3, 768], p_rwkv=[DEPTH, 4, 64, 64], p_shift=[DEPTH, 896],
    p_gla=[DEPTH, 4, 32, 64],
    s_ssd=[DEPTH, NS, 8, 64, 64], s_conv=[DEPTH, NS, 3, 768], s_rwkv=[DEPTH, NS, 4, 64, 64],
    s_shift=[DEPTH, NS, 896], s_gla=[DEPTH, NS, 4, 32, 64],
)

def xr(t, b, tiles=range(KT)):
    return [t.r((d, b)) for d in tiles]


SH1, SC1, GT1, SH2, SC2, GT2 = 0, 8, 16, 24, 32, 40


def build(stub_mixer=False, dbg=None, n_layers=DEPTH):
    nc = bass.Bass("TRN2", target_bir_lowering=False)
    K = KB(nc)
    P = K.P
    dr = {}
    for n, s in IN_SHAPES.items():
        dr[n] = nc.dram_tensor(n, s, F32, kind="ExternalInput").ap()
    for n, s in W_SHAPES.items():
        dr[n] = nc.dram_tensor(n, s, F32, kind="ExternalInput").ap()
    for n, s in OUT_SHAPES.items():
        dr[n] = nc.dram_tensor(n, s, F32, kind="ExternalOutput").ap()
    dbg_out = {}
    if dbg:
        for n, s in dbg.items():
            dbg_out[n] = nc.dram_tensor("dbg_" + n, s, F32, kind="ExternalOutput").ap()
    out_res = Res("outputs")

    with contextlib.ExitStack() as perm, nc.allow_non_contiguous_dma(reason="small param loads"):
        xT = K.sb(perm, "xT", [128, KT, NT], F32)
        modT = [K.sb(perm, f"modT{l}", [128, 48, 1 + NS], F32) for l in range(DEPTH)]
        identf = K.sb(perm, "identf", [128, 128], F32)
        identb = K.sb(perm, "identb", [128, 128], BF16)
        onesM = K.sb(perm, "onesM", [128, 128], F32)
        ones1 = K.sb(perm, "ones1", [128, 128], F32)
        maskU = K.sb(perm, "maskU", [128, 128], F32)
        maskSU = K.sb(perm, "maskSU", [128, 128], F32)
        maskSL = K.sb(perm, "maskSL", [128, 128], F32)
        blk64 = K.sb(perm, "blk64", [128, 128], F32)
        C = dict(xT=xT, modT=modT, identf=identf, identb=identb, onesM=onesM, ones1=ones1,
                 maskU=maskU, maskSU=maskSU, maskSL=maskSL, blk64=blk64)

        def sel(t, val_keep, cmp, fill, base=0, cm=1, pat=None, ap=None):
            ap = t.ap() if ap is None else ap
            pat = [[-1, ap.shape[-1]]] if pat is None else pat
            P.op("pool", lambda e: e.affine_select(out=ap, in_=ap, pattern=pat, compare_op=cmp, fill=fill,
                                                   base=base, channel_multiplier=cm),
                 reads=[t.r()], writes=[t.r()])

        K.memset(identf.ap(), 0.0, [identf.r()], eng="pool")
        sel(identf, 0, ALU.not_equal, 1.0)
        K.cp(identb.ap(), identf.ap(), [identf.r()], [identb.r()], eng="pool")
        K.memset(onesM.ap(), 1.0 / D, [onesM.r()], eng="pool")
        K.memset(ones1.ap(), 1.0, [ones1.r()], eng="pool")
        K.memset(maskU.ap(), 1.0, [maskU.r()], eng="pool")
        sel(maskU, 1, ALU.is_ge, 0.0, cm=-1, pat=[[1, 128]])
        K.memset(maskSU.ap(), 1.0, [maskSU.r()], eng="pool")
        sel(maskSU, 1, ALU.is_gt, 0.0, cm=-1, pat=[[1, 128]])
        K.memset(maskSL.ap(), 1.0, [maskSL.r()], eng="pool")
        sel(maskSL, 1, ALU.is_gt, 0.0)
        K.memset(blk64.ap(), 0.0, [blk64.r()], eng="pool")
        K.memset(blk64.ap()[0:64, 0:64], 1.0, [blk64.r()], eng="pool")
        K.memset(blk64.ap()[64:128, 64:128], 1.0, [blk64.r()], eng="pool")

        with contextlib.ExitStack() as ph:
            stage = [K.sb(ph, f"stg{i}", [128, D], F32) for i in range(2)]
            ctm = K.sb(ph, "ctm", [1 + NS, D], F32)
            scT = K.sb(ph, "scT", [128, KT, 1 + NS], BF16)
            wada = [K.sb(ph, f"wada{i}", [128, KT, 512], BF16) for i in range(2)]
            bB = [K.sb(ph, f"bB{i}", [1 + NS, 512], F32) for i in range(2)]
            modsb = [K.sb(ph, f"modsb{i}", [1 + NS, 512], F32) for i in range(2)]
            pst = [K.ps(ph, f"pst{i}", [128, 1024], F32) for i in range(2)]
            psm = [K.ps(ph, f"psm{i}", [128, 512], F32) for i in range(2)]
            pss = K.ps(ph, "pss", [128, 512], F32)
            for tt in range(NCH + 1):
                st = stage[tt % 2]
                pt = pst[tt % 2]
                n = 128 if tt < NCH else NS
                src = dr["xp"][tt * 128:(tt + 1) * 128, :] if tt < NCH else dr["xs"]
                K.dma(st.ap()[0:n, :], src, w=[st.r()])
                for d in range(KT):
                    K.tr(pt.ap()[:, d * n:(d + 1) * n], st.ap()[0:n, d * 128:(d + 1) * 128],
                         identf.ap()[0:n, 0:n], [st.r(), identf.r()], [pt.r()], inc=(d == KT - 1))
                dst = xT.ap()[:, :, tt * 128:tt * 128 + n]
                srcp = pt.ap()[:, 0:KT * n].rearrange("p (d n) -> p d n", d=KT)
                K.cp(dst, srcp, [pt.r()], xr(xT, min(tt // 4, 4)), eng=("act" if tt % 2 == 0 else "dve"))
            K.dma(ctm.ap(), dr["cc"], w=[ctm.r()])
            for d in range(KT):
                K.tr(pss.ap()[:, d * 17:(d + 1) * 17], ctm.ap()[:, d * 128:(d + 1) * 128],
                     identf.ap()[0:17, 0:17], [ctm.r(), identf.r()], [pss.r()], inc=(d == KT - 1))
            K.act(scT.ap(), pss.ap()[:, 0:KT * 17].rearrange("p (d n) -> p d n", d=KT), AF.Silu,
                  [pss.r()], [scT.r()])
            it = 0
            for l in range(DEPTH):
                for j in range(12):
                    wb = wada[it % 2]
                    bb = bB[it % 2]
                    ms = modsb[it % 2]
                    pm = psm[it % 2]
                    K.dma(wb.ap(), dr["w_ada"][l, :, j * 512:(j + 1) * 512].rearrange("(k p) n -> p k n", p=128),
                          w=[wb.r()], q="pool")
                    K.dma(bb.ap(), dr["b_ada"][l:l + 1, j * 512:(j + 1) * 512].to_broadcast([1 + NS, 512]),
                          w=[bb.r()])
                    for k in range(KT):
                        K.mm(pm.ap()[0:17, :], scT.ap()[:, k, :], wb.ap()[:, k, :], [scT.r(), wb.r()], [pm.r()],
                             start=(k == 0), stop=(k == KT - 1))
                    K.tt(ms.ap(), pm.ap()[0:17, :], bb.ap(), ALU.add, [pm.r(), bb.r()], [ms.r()])
                    for q4 in range(4):
                        K.tr(pss.ap()[:, q4 * 17:(q4 + 1) * 17], ms.ap()[:, q4 * 128:(q4 + 1) * 128],
                             identf.ap()[0:17, 0:17], [ms.r(), identf.r()], [pss.r()], inc=(q4 == 3))
                    K.cp(modT[l].ap()[:, j * 4:(j + 1) * 4, :],
                         pss.ap()[:, 0:4 * 17].rearrange("p (d n) -> p d n", d=4), [pss.r()], [modT[l].r()],
                         eng="act")
                    it += 1
                for seg in (SC1, GT1, SC2, GT2):
                    K.ts(modT[l].ap()[:, seg:seg + 8, :], modT[l].ap()[:, seg:seg + 8, :], 1.0, ALU.add,
                         [modT[l].r()], [modT[l].r()])
            P.barrier()

        for l in range(n_layers):
            layer(K, dr, C, l, stub_mixer, dbg_out)

        with contextlib.ExitStack() as ph:
            stage = [K.sb(ph, f"ostg{i}", [128, D], F32) for i in range(2)]
            pst = [K.ps(ph, f"opst{i}", [128, 1024], F32) for i in range(2)]
            for tt in range(NCH + 1):
                st = stage[tt % 2]
                pt = pst[tt % 2]
                n = 128 if tt < NCH else NS
                for d in range(KT):
                    K.tr(pt.ap()[0:n, d * 128:(d + 1) * 128], xT.ap()[:, d, tt * 128:tt * 128 + n],
                         identf.ap(), [xT.r((d, min(tt // 4, 4))), identf.r()], [pt.r()], inc=(d == KT - 1))
                K.cp(st.ap()[0:n, :], pt.ap()[0:n, :], [pt.r()], [st.r()], eng=("act" if tt % 2 == 0 else "dve"))
                dst = dr["y_p"][tt * 128:(tt + 1) * 128, :] if tt < NCH else dr["y_s"]
                K.dma(dst, st.ap()[0:n, :], r=[st.r()])
            P.barrier()
    with nc.allow_non_contiguous_dma(reason="small param loads"):
        P.emit()
    return nc


def modulate(K, C, l, src, dst, b, sh, sc, dres):
    mod = C["modT"][l]
    c0, n = BLOCKS[b]
    if b < 4:
        for d in range(KT):
            K.act(dst.ap()[:, d, c0:c0 + n], src.ap()[:, d, c0:c0 + n], AF.Identity,
                  [src.r((d, b)), mod.r()], [dres(d)],
                  scale=mod.ap()[:, sc + d, 0:1], bias=mod.ap()[:, sh + d, 0:1])
    else:
        tmp = C["tmp_s"]
        K.tt(tmp.ap(), src.ap()[:, :, c0:c0 + n], mod.ap()[:, sc:sc + 8, 1:1 + NS], ALU.mult,
             xr(src, b) + [mod.r()], [tmp.r()])
        K.tt(dst.ap()[:, :, c0:c0 + n], tmp.ap(), mod.ap()[:, sh:sh + 8, 1:1 + NS], ALU.add,
             [tmp.r(), mod.r()], [dres(d) for d in range(KT)])


def layernorm(K, C, l, gi, psA, psB, scr):
    xT, onesM, lncol = C["xT"], C["onesM"], C["lncol"]
    sq, mean_sb, var, tt_ = scr["sq"], scr["mean"], scr["var"], scr["t"]
    for b, (c0, n) in enumerate(BLOCKS):
        for d in range(KT):
            s = sq[d % 2]
            xs = xT.ap()[:, d, c0:c0 + n]
            K.act(s.ap()[:, :n], xs, AF.Square, [xT.r((d, b))], [s.r()])
            K.mm(psA.ap()[:, :n], onesM.ap(), xs, [onesM.r(), xT.r((d, b))], [psA.r()],
                 start=(d == 0), stop=(d == KT - 1), inc=True)
            K.mm(psB.ap()[:, :n], onesM.ap(), s.ap()[:, :n], [onesM.r(), s.r()], [psB.r()],
                 start=(d == 0), stop=(d == KT - 1), inc=True)
        K.cp(mean_sb.ap()[:, :n], psA.ap()[:, :n], [psA.r()], [mean_sb.r()], eng="act")
        K.tt(var.ap()[:, :n], mean_sb.ap()[:, :n], mean_sb.ap()[:, :n], ALU.mult, [mean_sb.r()], [var.r()])
        K.tt(var.ap()[:, :n], psB.ap()[:, :n], var.ap()[:, :n], ALU.subtract, [psB.r(), var.r()], [var.r()])
        K.act(var.ap()[:, :n], var.ap()[:, :n], AF.Sqrt, [var.r()], [var.r()], bias=LN_EPS)
        K.recip(var.ap()[:, :n], var.ap()[:, :n], [var.r()], [var.r()])
        for d in range(KT):
            t = tt_[d % 2]
            xs = xT.ap()[:, d, c0:c0 + n]
            K.tt(t.ap()[:, :n], xs, mean_sb.ap()[:, :n], ALU.subtract, [xT.r((d, b)), mean_sb.r()], [t.r()])
            K.tt(t.ap()[:, :n], t.ap()[:, :n], var.ap()[:, :n], ALU.mult, [t.r(), var.r()], [t.r()])
            K.act(xs, t.ap()[:, :n], AF.Identity, [t.r(), lncol.r()], [xT.r((d, b))],
                  scale=lncol.ap()[:, gi, d:d + 1], bias=lncol.ap()[:, gi + 1, d:d + 1])


def residual_add(K, C, l, ps, n, dout, b, gt, comb=None):
    xT, mod = C["xT"], C["modT"][l]
    c0, _ = BLOCKS[b]
    xs = xT.ap()[:, dout, c0:c0 + n]
    src = ps.ap()[:, :n]
    rd = [ps.r(), mod.r(), xT.r((dout, b))]
    if comb is not None:
        tmp = C["tmp_c"][dout % 2]
        K.tt(tmp.ap()[:, :n], src, comb.ap()[:, c0:c0 + n], ALU.mult, [ps.r(), comb.r(b)], [tmp.r()])
        src = tmp.ap()[:, :n]
        rd = [tmp.r(), mod.r(), xT.r((dout, b))]
    if b < 4:
        K.stt(xs, src, mod.ap()[:, gt + dout, 0:1], xs, ALU.mult, ALU.add, rd, [xT.r((dout, b))])
    else:
        tmp2 = C["tmp_s2"]
        K.tt(tmp2.ap(), src, mod.ap()[:, gt + dout, 1:1 + NS], ALU.mult, rd[:2], [tmp2.r()])
        K.tt(xs, tmp2.ap(), xs, ALU.add, [tmp2.r(), xT.r((dout, b))], [xT.r((dout, b))])


def scale_x(K, C):
    xT = C["xT"]
    for b, (c0, n) in enumerate(BLOCKS):
        for d in range(KT):
            xs = xT.ap()[:, d, c0:c0 + n]
            K.P.op("act", lambda e, xs=xs: e.mul(out=xs, in_=xs, mul=ALPHA), reads=[xT.r((d, b))],
                   writes=[xT.r((d, b))])


def ffn_group(K, C, l, hT, actb, wpool, srcs, nf, ps, comb=None):
    wg_src, wu_src, wd_src = srcs
    sg = C["sg"]

    def unit():
        u = wpool["bufs"][wpool["i"] % len(wpool["bufs"])]
        wpool["i"] += 1
        return u

    i = 0
    for f0 in range(0, nf, 4):
        nfu = min(4, nf - f0)
        WG, WU = unit(), unit()
        K.dma(WG.ap()[:, :, 0:nfu * 128], wg_src[:, f0 * 128:(f0 + nfu) * 128].rearrange("(k p) n -> p k n", p=128),
              w=[WG.r()], q="pool")
        K.dma(WU.ap()[:, :, 0:nfu * 128], wu_src[:, f0 * 128:(f0 + nfu) * 128].rearrange("(k p) n -> p k n", p=128),
              w=[WU.r()], q="pool")
        for fu in range(nfu):
            f = f0 + fu
            for b, (c0, n) in enumerate(BLOCKS):
                pg, pu = ps["g"][i % 2], ps["u"][i % 2]
                for k in range(KT):
                    K.mm(pg.ap()[:, :n], WG.ap()[:, k, fu * 128:(fu + 1) * 128], hT.ap()[:, k, c0:c0 + n],
                         [WG.r(), hT.r((k, b))], [pg.r()], start=(k == 0), stop=(k == KT - 1))
                for k in range(KT):
                    K.mm(pu.ap()[:, :n], WU.ap()[:, k, fu * 128:(fu + 1) * 128], hT.ap()[:, k, c0:c0 + n],
                         [WU.r(), hT.r((k, b))], [pu.r()], start=(k == 0), stop=(k == KT - 1))
                s_ = sg[i % 2]
                K.act(s_.ap()[:, :n], pg.ap()[:, :n], AF.Silu, [pg.r()], [s_.r()])
                K.tt(actb.ap()[:, f, c0:c0 + n], s_.ap()[:, :n], pu.ap()[:, :n], ALU.mult, [s_.r(), pu.r()],
                     [actb.r((f, b))])
                i += 1
    i = 0
    for dh in range(2):
        WD = unit()
        K.dma(WD.ap()[:, 0:nf, :], wd_src[:, dh * 512:(dh + 1) * 512].rearrange("(f p) n -> p f n", p=128),
              w=[WD.r()], q="pool")
        for dd in range(4):
            dout = dh * 4 + dd
            for b, (c0, n) in enumerate(BLOCKS):
                pd = ps["d"][i % 2]
                for f in range(nf):
                    K.mm(pd.ap()[:, :n], WD.ap()[:, f, dd * 128:(dd + 1) * 128], actb.ap()[:, f, c0:c0 + n],
                         [WD.r(), actb.r((f, b))], [pd.r()], start=(f == 0), stop=(f == nf - 1))
                residual_add(K, C, l, pd, n, dout, b, GT2, comb=comb)
                i += 1


def moe_routing(K, C, dr, l, ph, ps):
    xT, mod, identf = C["xT"], C["modT"][l], C["identf"]
    router = K.sb(ph, "router", [128, KT, 8], F32)
    K.dma(router.ap(), dr["moe_router"][0].rearrange("(k p) e -> p k e", p=128), w=[router.r()])
    combT = K.sb(ph, "combT", [8, NT], F32)
    tp = C["tpair"]
    sm = {n: K.sb(ph, "rt_" + n, [128, 8], F32) for n in ["lg", "eq1", "l2", "eq2", "cb"]}
    sc1 = {n: K.sb(ph, "rs_" + n, [128, 1], F32) for n in ["m1", "m2", "e", "w1", "w2"]}
    pl, pt = ps["g"][0], ps["u"][0]
    for tt in range(NCH + 1):
        n = 128 if tt < NCH else NS
        c0 = tt * 128
        b = min(tt // 4, 4)
        hap = tp.ap().rearrange("p a (b c) -> p (a b) c", c=128)
        hres = [tp.r(0), tp.r(1)]
        if tt < NCH:
            for d in range(KT):
                K.act(hap[:, d, :], xT.ap()[:, d, c0:c0 + n], AF.Identity, [xT.r((d, b)), mod.r()], hres,
                      scale=mod.ap()[:, SC2 + d, 0:1], bias=mod.ap()[:, SH2 + d, 0:1])
        else:
            K.tt(hap[:, :, 0:n], xT.ap()[:, :, c0:c0 + n], mod.ap()[:, SC2:SC2 + 8, 1:1 + NS], ALU.mult,
                 xr(xT, b) + [mod.r()], hres)
            K.tt(hap[:, :, 0:n], hap[:, :, 0:n], mod.ap()[:, SH2:SH2 + 8, 1:1 + NS], ALU.add,
                 hres + [mod.r()], hres)
        for d in range(KT):
            K.mm(pl.ap()[0:n, 0:8], hap[:, d, 0:n], router.ap()[:, d, :], hres + [router.r()], [pl.r()],
                 start=(d == 0), stop=(d == KT - 1), inc=True)
        lg, eq1, l2, eq2, cb = (sm[k].ap()[0:n, :] for k in ["lg", "eq1", "l2", "eq2", "cb"])
        m1, m2, ee, w1, w2 = (sc1[k].ap()[0:n, :] for k in ["m1", "m2", "e", "w1", "w2"])
        R = lambda *ks: [(sm[k] if k in sm else sc1[k]).r() for k in ks]
        K.cp(lg, pl.ap()[0:n, 0:8], [pl.r()], R("lg"))
        K.red(m1, lg, R("lg"), R("m1"), op=ALU.max)
        K.ts(eq1, lg, m1, ALU.is_equal, R("lg", "m1"), R("eq1"))
        K.stt(l2, eq1, -1e30, lg, ALU.mult, ALU.add, R("eq1", "lg"), R("l2"))
        K.red(m2, l2, R("l2"), R("m2"), op=ALU.max)
        K.ts(eq2, l2, m2, ALU.is_equal, R("l2", "m2"), R("eq2"))
        K.tt(ee, m2, m1, ALU.subtract, R("m1", "m2"), R("e"))
        K.act(ee, ee, AF.Exp, R("e"), R("e"))
        K.ts(w1, ee, 1.0, ALU.add, R("e"), R("w1"))
        K.recip(w1, w1, R("w1"), R("w1"))
        K.tt(w2, ee, w1, ALU.mult, R("e", "w1"), R("w2"))
        K.ts(cb, eq1, w1, ALU.mult, R("eq1", "w1"), R("cb"))
        K.stt(cb, eq2, w2, cb, ALU.mult, ALU.add, R("eq2", "w2", "cb"), R("cb"))
        K.tr(pt.ap()[0:8, 0:n], cb, identf.ap()[0:n, 0:n], R("cb") + [identf.r()], [pt.r()])
        K.cp(combT.ap()[:, c0:c0 + n], pt.ap()[0:8, 0:n], [pt.r()], [combT.r()], eng="act")
    return combT


def layer(K, dr, C, l, stub_mixer, dbg_out):
    nc, P = K.nc, K.P
    xT, mod = C["xT"], C["modT"][l]
    with contextlib.ExitStack() as lay:
        bufA = K.sb(lay, "bufA", [128, KT, NT], BF16)
        lncol = K.sb(lay, "lncol", [128, 4, KT], F32)
        C["lncol"] = lncol
        C["tmp_s"] = K.sb(lay, "tmp_s", [128, KT, NS], F32)
        C["tmp_s2"] = K.sb(lay, "tmp_s2", [128, NS], F32)
        for i, nme in enumerate(["ln_mix_g", "ln_mix_b", "ln_ffn_g", "ln_ffn_b"]):
            K.dma(lncol.ap()[:, i, :], dr[nme][l].rearrange("(d p) -> p d", p=128), w=[lncol.r()])

        with contextlib.ExitStack() as ph:
            if stub_mixer:
                for b in range(5):
                    modulate(K, C, l, xT, bufA, b, SH1, SC1, lambda d, b=b: bufA.r((d, b)))
            else:
                mixers(K, dr, C, l, bufA, dbg_out)
            P.barrier()
            wout = K.sb(ph, "wout", [128, KT, D], BF16)
            K.dma(wout.ap(), dr["w_out"][l].rearrange("(k p) n -> p k n", p=128), w=[wout.r()], q="pool")
            scale_x(K, C)
            with contextlib.ExitStack() as ph2:
                pso = [K.ps(ph2, f"pso{i}", [128, 512], F32) for i in range(4)]
                i = 0
                for dout in range(KT):
                    for b, (c0, n) in enumerate(BLOCKS):
                        pd = pso[i % 4]
                        for k in range(KT):
                            K.mm(pd.ap()[:, :n], wout.ap()[:, k, dout * 128:(dout + 1) * 128],
                                 bufA.ap()[:, k, c0:c0 + n], [wout.r(), bufA.r((k, b))], [pd.r()],
                                 start=(k == 0), stop=(k == KT - 1))
                        residual_add(K, C, l, pd, n, dout, b, GT1)
                        i += 1
                P.barrier()

        with contextlib.ExitStack() as ph:
            hT = K.sb(ph, "hT", [128, KT, NT], BF16)
            tpair = K.sb(ph, "tpair", [128, 2, 512], F32)
            C["tpair"] = tpair

            class _V:
                def __init__(self, i):
                    self.i = i

                def ap(self):
                    return tpair.ap()[:, self.i, :]

                def r(self, key=0):
                    return tpair.r(self.i)
            scr = dict(sq=[K.sb(ph, f"sq{i}", [128, 512], F32) for i in range(2)],
                       mean=K.sb(ph, "mean", [128, 512], F32), var=K.sb(ph, "var", [128, 512], F32),
                       t=[_V(0), _V(1)])
            C["sg"] = scr["sq"]
            C["tmp_c"] = scr["t"]
            W = dict(bufs=[K.sb(ph, f"WP{i}", [128, KT, 512], BF16) for i in range(4)], i=0)
            ps = dict(g=[K.ps(ph, f"pg{i}", [128, 512], F32) for i in range(2)],
                      u=[K.ps(ph, f"pu{i}", [128, 512], F32) for i in range(2)],
                      d=[K.ps(ph, f"pd{i}", [128, 512], F32) for i in range(2)])
            psA = K.ps(ph, "psA", [128, 512], F32)
            psB = K.ps(ph, "psB", [128, 512], F32)
            layernorm(K, C, l, 0, psA, psB, scr)
            for b in range(5):
                modulate(K, C, l, xT, hT, b, SH2, SC2, lambda d, b=b: hT.r((d, b)))
            if l % 2 == 0:
                scale_x(K, C)
                i = l // 2
                for f0 in range(0, F_DENSE // 128, 8):
                    nf = min(8, F_DENSE // 128 - f0)
                    srcs = (dr["ffn_w_gate"][i, :, f0 * 128:(f0 + nf) * 128],
                            dr["ffn_w_up"][i, :, f0 * 128:(f0 + nf) * 128],
                            dr["ffn_w_down"][i, f0 * 128:(f0 + nf) * 128, :])
                    ffn_group(K, C, l, hT, bufA, W, srcs, nf, ps)
            else:
                combT = moe_routing(K, C, dr, l, ph, ps)
                scale_x(K, C)
                combB = K.sb(ph, "combB", [128, NT], F32)
                sele = K.sb(ph, "sele", [8, 128], F32)
                i = l // 2
                for e_ in range(8):
                    K.memset(sele.ap(), 0.0, [sele.r()])
                    K.P.op("dve", lambda e, e_=e_: e.memset(sele.ap()[e_:e_ + 1, :], 1.0), reads=[sele.r()],
                           writes=[sele.r()]) if False else K.ts(
                        sele.ap(), C["identf"].ap()[0:8, e_:e_ + 1].to_broadcast([8, 128]), 1.0, ALU.mult,
                        [C["identf"].r()], [sele.r()])
                    for b, (c0, n) in enumerate(BLOCKS):
                        pb = ps["d"][b % 2]
                        K.mm(pb.ap()[:, :n], sele.ap(), combT.ap()[:, c0:c0 + n], [sele.r(), combT.r()],
                             [pb.r()])
                        K.cp(combB.ap()[:, c0:c0 + n], pb.ap()[:, :n], [pb.r()], [combB.r(b)], eng="act")
                    srcs = (dr["moe_w_gate"][i, e_], dr["moe_w_up"][i, e_], dr["moe_w_down"][i, e_])
                    ffn_group(K, C, l, hT, bufA, W, srcs, 8, ps, comb=combB)
            layernorm(K, C, l, 2, psA, psB, scr)
            P.barrier()


def make_in_maps(inp):
    g = lambda k: np.ascontiguousarray(np.asarray(inp[k], dtype=np.float32))
    xp, xs, cp, cs = g("x_prompt"), g("x_sample"), g("c_prompt"), g("c_sample")
    st = {k: g(k) for k in ["state_ssd", "state_ssd_conv", "state_rwkv", "state_rwkv_shift", "state_gla"]}
    wts = {k: g(k) for k in W_SHAPES}
    maps = []
    for c in range(NCORES):
        sl = slice(c * NS, (c + 1) * NS)
        m = dict(wts)
        m["xp"] = xp[c]
        m["xs"] = np.ascontiguousarray(xs[sl, 0, :])
        m["cc"] = np.ascontiguousarray(np.concatenate([cp[c:c + 1], cs[sl]], axis=0))
        m["st_ssd"] = np.ascontiguousarray(st["state_ssd"][:, sl])
        m["st_conv"] = np.ascontiguousarray(st["state_ssd_conv"][:, sl])
        m["st_rwkv"] = np.ascontiguousarray(st["state_rwkv"][:, sl])
        m["st_shift"] = np.ascontiguousarray(st["state_rwkv_shift"][:, sl])
        m["st_gla"] = np.ascontiguousarray(st["state_gla"][:, sl])
        maps.append(m)
    return maps


_NC_CACHE = {}


def gather(results):
    R = lambda k: [np.asarray(r[k], dtype=np.float32) for r in results]
    y_p = np.stack(R("y_p"), axis=0)
    y_s = np.concatenate(R("y_s"), axis=0)[:, None, :]
    outs = [y_p, y_s]
    for k in ["p_ssd", "p_conv", "p_rwkv", "p_shift", "p_gla"]:
        outs.append(np.stack(R(k), axis=1))
    for k in ["s_ssd", "s_conv", "s_rwkv", "s_shift", "s_gla"]:
        outs.append(np.concatenate(R(k), axis=1))
    return tuple(np.ascontiguousarray(o) for o in outs)


def kernel(**inputs):
    if "nc" not in _NC_CACHE:
        _NC_CACHE["nc"] = build()
    res = run_bass_kernel_spmd(_NC_CACHE["nc"], make_in_maps(inputs), core_ids=list(range(NCORES)))
    return gather(res.results)


def bc(ap, axis, shape):
    return ap.unsqueeze(axis).to_broadcast(list(shape))


def softplus_(K, x, tmp, r, n):
    xa, ta = x[0], tmp[0]
    K.act(ta, xa, AF.Abs, [x[1]], [tmp[1]])
    K.act(ta, ta, AF.Exp, [tmp[1]], [tmp[1]], scale=-1.0)
    K.act(ta, ta, AF.Ln, [tmp[1]], [tmp[1]], bias=1.0)
    K.ts(xa, xa, 0.0, ALU.max, [x[1]], [x[1]])
    K.tt(xa, xa, ta, ALU.add, [x[1], tmp[1]], [x[1]])


def make_hc(K, C, l, hc, c):
    xT, mod = C["xT"], C["modT"][l]
    for d in range(KT):
        K.act(hc.ap()[:, d, :], xT.ap()[:, d, c * 128:(c + 1) * 128], AF.Identity,
              [xT.r((d, c // 4)), mod.r()], [hc.r()],
              scale=mod.ap()[:, SC1 + d, 0:1], bias=mod.ap()[:, SH1 + d, 0:1])


def mixers(K, dr, C, l, yT, dbg_out):
    P = K.P
    xT, mod = C["xT"], C["modT"][l]
    en = C.get("enable", ("ssd", "rwkv", "gla"))
    with contextlib.ExitStack() as mx:
        hc = [K.sb(mx, f"hc{i}", [128, KT, 128], BF16) for i in range(2)]
        hs = K.sb(mx, "hs", [128, KT, NS], BF16)
        C["hc"], C["hs"] = hc, hs
        modulate(K, C, l, xT, _Shift(hs, 2048), 4, SH1, SC1, lambda d: hs.r())
        for name, tiles in (("ssd", range(0, 4)), ("rwkv", range(4, 6)), ("gla", range(6, 8))):
            if name not in en:
                for d in tiles:
                    for b, (c0, n) in enumerate(BLOCKS):
                        K.memset(yT.ap()[:, d, c0:c0 + n], 0.0, [yT.r((d, b))])
        if "ssd" in en and "gla" in en:
            ssd_gla_phase(K, dr, C, l, yT, dbg_out)
            P.barrier()
        else:
            if "ssd" in en:
                ssd_phase(K, dr, C, l, yT, dbg_out)
                P.barrier()
            if "gla" in en:
                gla_phase(K, dr, C, l, yT, dbg_out)
                P.barrier()
        if "rwkv" in en:
            rwkv_phase(K, dr, C, l, yT, dbg_out)
            P.barrier()


class _Shift:
    def __init__(self, t, off):
        self.t, self.off = t, off

    def ap(self):
        return _ShiftAP(self.t.ap(), self.off)

    def r(self, key=0):
        return self.t.r()


class _ShiftAP:
    def __init__(self, ap, off):
        self._ap, self.off = ap, off

    def __getitem__(self, key):
        p, d, s = key
        return self._ap[p, d, slice(s.start - self.off, s.stop - self.off)]


def ssd_phase(K, dr, C, l, yT, dbg_out):
    P = K.P
    nc = K.nc
    identb, identf, maskU, maskSL, ones1 = C["identb"], C["identf"], C["maskU"], C["maskSL"], C["ones1"]
    hc, hs = C["hc"], C["hs"]
    with contextlib.ExitStack() as ph:
        win = K.sb(ph, "win_ssd", [128, KT, 1288], BF16)
        K.dma(win.ap(), dr["w_in"][l, :, 0:1288].rearrange("(k p) n -> p k n", p=128), w=[win.r()], q="pool")
        convw = K.sb(ph, "convw", [128, 6, 4], F32)
        convb = K.sb(ph, "convb", [128, 6], F32)
        for i in range(4):
            K.dma(convw.ap()[:, :, i], dr["ssd_conv_w"][l, i].rearrange("(t p) -> p t", p=128), w=[convw.r()])
        K.dma(convb.ap(), dr["ssd_conv_b"][l].rearrange("(t p) -> p t", p=128), w=[convb.r()])
        normg = K.sb(ph, "normg", [128, 4], F32)
        K.dma(normg.ap(), dr["ssd_norm_g"][l].rearrange("(t p) -> p t", p=128), w=[normg.r()])
        dtbB = K.sb(ph, "dtbB", [128, 8], F32)
        aB = K.sb(ph, "aB", [128, 8], F32)
        dB = K.sb(ph, "dB", [128, 8], F32)
        K.dma(dtbB.ap(), dr["ssd_dt_bias"][l:l + 1, :].to_broadcast([128, 8]), w=[dtbB.r()])
        K.dma(aB.ap(), dr["ssd_a_log"][l:l + 1, :].to_broadcast([128, 8]), w=[aB.r()])
        K.dma(dB.ap(), dr["ssd_d"][l:l + 1, :].to_broadcast([128, 8]), w=[dB.r()])
        K.act(aB.ap(), aB.ap(), AF.Exp, [aB.r()], [aB.r()])
        K.ts(aB.ap(), aB.ap(), -1.0, ALU.mult, [aB.r()], [aB.r()])
        import os
        if os.environ.get("SKIP_SSD_PROMPT") != "1":
            ssd_prompt(K, dr, C, l, yT, win, convw, convb, normg, dtbB, aB, dB, dbg_out)
        P.barrier()
        if os.environ.get("SKIP_SSD_SAMPLE") != "1":
            ssd_sample(K, dr, C, l, yT, win, aB, dB, dtbB, dbg_out)


def ssd_prompt(K, dr, C, l, yT, win, convw, convb, normg, dtbB, aB, dB, dbg_out):
    P = K.P
    identb, identf, maskU, maskSL, ones1 = C["identb"], C["identf"], C["maskU"], C["maskSL"], C["ones1"]
    hc, hs = C["hc"], C["hs"]
    with contextlib.ExitStack() as ph:
        XB = [K.sb(ph, f"XB{i}", [128, 6, 131], F32) for i in range(2)]
        XC = [K.sb(ph, f"XC{i}", [128, 6, 128], BF16) for i in range(2)]
        cacc = [K.sb(ph, f"cacc{i}", [128, 128], F32) for i in range(2)]
        sz = K.sb(ph, "sz", [128, 512], F32)
        dtt = K.sb(ph, "dtt", [128, 8], F32)
        dtmp = K.sb(ph, "dtmp", [128, 8], F32)
        dtA = K.sb(ph, "dtA", [128, 8], F32)
        csb = K.sb(ph, "csb", [128, 16], F32)
        e1 = K.sb(ph, "e1", [128, 8], F32)
        el = K.sb(ph, "el", [128, 8], F32)
        tail = K.sb(ph, "tail", [128, 8], F32)
        Rt = K.sb(ph, "Rt", [128, 8, 128], F32)
        dec = K.sb(ph, "dec", [128, 8, 128], F32)
        Gs = K.sb(ph, "Gs", [128, 2, 128], F32)
        Mb = K.sb(ph, "Mb", [128, 8, 128], BF16)
        XT = K.sb(ph, "XT", [128, 640], BF16)
        xD = K.sb(ph, "xD", [128, 512], BF16)
        xw = K.sb(ph, "xw", [128, 512], BF16)
        t1 = K.sb(ph, "t1", [128, 512], F32)
        yn = K.sb(ph, "yn", [128, 512], BF16)
        ss = K.sb(ph, "ss", [128, 1], F32)
        HS32 = K.sb(ph, "HS32", [128, 4, 64], F32)
        HSb = K.sb(ph, "HSb", [128, 4, 64], BF16)
        hsT = K.sb(ph, "hsT", [128, 2, 128], F32)

        ps_x = K.ps(ph, "ps_x", [128, 1024], F32)
        ps_z = K.ps(ph, "ps_z", [128, 512], F32)
        ps_c = K.ps(ph, "ps_c", [128, 512], F32)
        ps_t = K.ps(ph, "ps_t", [128, 1024], BF16)
        ps_y = K.ps(ph, "ps_y", [128, 512], F32)
        ps_i = K.ps(ph, "ps_i", [128, 512], F32)
        ps_h = K.ps(ph, "ps_h", [128, 512], F32)

        K.memset(HS32.ap(), 0.0, [HS32.r()])
        K.memset(HSb.ap(), 0.0, [HSb.r()])
        K.memset(XB[0].ap()[:, :, 0:3], 0.0, [XB[0].r()])

        for c in range(NCH):
            h = hc[c % 2]
            make_hc(K, C, l, h, c)
            xb, xc = XB[c % 2], XC[c % 2]
            for ct in range(6):
                for k in range(KT):
                    K.mm(ps_x.ap()[:, ct * 128:(ct + 1) * 128], win.ap()[:, k, 512 + ct * 128:512 + (ct + 1) * 128],
                         h.ap()[:, k, :], [win.r(), h.r()], [ps_x.r()], start=(k == 0), stop=(k == KT - 1),
                         inc=(k == KT - 1 and ct == 5))
            K.cp(xb.ap()[:, :, 3:131], ps_x.ap()[:, 0:768].rearrange("p (t n) -> p t n", t=6), [ps_x.r()], [xb.r()],
                 eng="act")
            if c + 1 < NCH:
                K.cp(XB[(c + 1) % 2].ap()[:, :, 0:3], xb.ap()[:, :, 128:131], [xb.r()], [XB[(c + 1) % 2].r()])
            for ct in range(6):
                ca = cacc[ct % 2]
                K.ts(ca.ap(), xb.ap()[:, ct, 0:128], convw.ap()[:, ct, 0:1], ALU.mult, [xb.r(), convw.r(), convb.r()],
                     [ca.r()], s2=convb.ap()[:, ct:ct + 1], op1=ALU.add)
                for i in range(1, 4):
                    K.stt(ca.ap(), xb.ap()[:, ct, i:i + 128], convw.ap()[:, ct, i:i + 1], ca.ap(), ALU.mult, ALU.add,
                          [xb.r(), convw.r(), ca.r()], [ca.r()])
                K.act(xc.ap()[:, ct, :], ca.ap(), AF.Silu, [ca.r()], [xc.r()])
            for k in range(KT):
                K.mm(ps_z.ap(), h.ap()[:, k, :], win.ap()[:, k, 0:512], [h.r(), win.r()], [ps_z.r()],
                     start=(k == 0), stop=(k == KT - 1))
            for k in range(KT):
                K.mm(ps_c.ap()[:, 0:8], h.ap()[:, k, :], win.ap()[:, k, 1280:1288], [h.r(), win.r()], [ps_c.r()],
                     start=(k == 0), stop=(k == KT - 1))
            K.act(sz.ap(), ps_z.ap(), AF.Silu, [ps_z.r()], [sz.r()])
            K.tt(dtt.ap(), ps_c.ap()[:, 0:8], dtbB.ap(), ALU.add, [ps_c.r(), dtbB.r()], [dtt.r()])
            softplus_(K, (dtt.ap(), dtt.r()), (dtmp.ap(), dtmp.r()), None, None)
            K.tt(dtA.ap(), dtt.ap(), aB.ap(), ALU.mult, [dtt.r(), aB.r()], [dtA.r()])
            K.mm(ps_c.ap()[:, 8:16], maskU.ap(), dtA.ap(), [maskU.r(), dtA.r()], [ps_c.r()])
            K.mm(ps_c.ap()[:, 16:24], ones1.ap(), dtA.ap(), [ones1.r(), dtA.r()], [ps_c.r()])
            K.tt(Rt.ap(), bc(maskU.ap(), 1, [128, 8, 128]), bc(dtA.ap(), 2, [128, 8, 128]), ALU.mult,
                 [maskU.r(), dtA.r()], [Rt.r()])
            for hf in range(2):
                K.mm(ps_x.ap()[:, hf * 512:(hf + 1) * 512], maskSL.ap(),
                     Rt.ap()[:, hf * 4:(hf + 1) * 4, :].rearrange("p h i -> p (h i)"), [maskSL.r(), Rt.r()],
                     [ps_x.r()])
            K.act(dec.ap().rearrange("p h i -> p (h i)"), ps_x.ap(), AF.Exp, [ps_x.r()], [dec.r()])
            K.cp(csb.ap(), ps_c.ap()[:, 8:24], [ps_c.r()], [csb.r()], eng="act")
            for g in range(2):
                K.mm(ps_c.ap()[:, 256 + g * 128:256 + (g + 1) * 128], xc.ap()[64 * g:64 * g + 64, 4, :],
                     xc.ap()[64 * g:64 * g + 64, 5, :], [xc.r()], [ps_c.r()], self_wait=(g == 1))
            K.tt(Gs.ap(), ps_c.ap()[:, 256:512].rearrange("p (g i) -> p g i", g=2), bc(maskU.ap(), 1, [128, 2, 128]),
                 ALU.mult, [ps_c.r(), maskU.r()], [Gs.r()])
            K.tt(dec.ap().rearrange("p (g r) i -> p g r i", g=2), dec.ap().rearrange("p (g r) i -> p g r i", g=2),
                 bc(Gs.ap(), 2, [128, 2, 4, 128]), ALU.mult, [dec.r(), Gs.r()], [dec.r()])
            K.tt(Mb.ap(), dec.ap(), bc(dtt.ap(), 2, [128, 8, 128]), ALU.mult, [dec.r(), dtt.r()], [Mb.r()])
            for ct in range(5):
                K.tr(ps_t.ap()[:, ct * 128:(ct + 1) * 128], xc.ap()[:, ct, :], identb.ap(), [xc.r(), identb.r()],
                     [ps_t.r()], inc=(ct == 4))
            K.cp(XT.ap(), ps_t.ap()[:, 0:640], [ps_t.r()], [XT.r()], eng="act")
            K.tt(xD.ap().rearrange("p (h q) -> p h q", h=8), XT.ap()[:, 0:512].rearrange("p (h q) -> p h q", h=8),
                 bc(dB.ap(), 2, [128, 8, 64]), ALU.mult, [XT.r(), dB.r()], [xD.r()])
            K.mm(ps_y.ap(), identb.ap(), xD.ap(), [identb.r(), xD.r()], [ps_y.r()], start=True, stop=False)
            for hh in range(8):
                K.mm(ps_y.ap()[:, hh * 64:(hh + 1) * 64], Mb.ap()[:, hh, :], XT.ap()[:, hh * 64:(hh + 1) * 64],
                     [Mb.r(), XT.r()], [ps_y.r()], start=False, stop=(hh == 7))
            for g in range(2):
                K.mm(ps_i.ap()[:, g * 256:(g + 1) * 256], xc.ap()[64 * g:64 * g + 64, 5, :],
                     HSb.ap()[64 * g:64 * g + 64, :, :].rearrange("p h q -> p (h q)"), [xc.r(), HSb.r()], [ps_i.r()],
                     self_wait=(g == 1))
            K.act(e1.ap(), csb.ap()[:, 0:8], AF.Exp, [csb.r()], [e1.r()])
            K.tt(t1.ap().rearrange("p (h q) -> p h q", h=8), ps_i.ap().rearrange("p (h q) -> p h q", h=8),
                 bc(e1.ap(), 2, [128, 8, 64]), ALU.mult, [ps_i.r(), e1.r()], [t1.r()])
            K.tt(t1.ap(), t1.ap(), ps_y.ap(), ALU.add, [t1.r(), ps_y.r()], [t1.r()])
            ssd_epilogue(K, C, t1, sz, ss, yn, 128)
            for q in range(4):
                K.tr(ps_t.ap()[:, q * 128:(q + 1) * 128], yn.ap()[:, q * 128:(q + 1) * 128], identb.ap(),
                     [yn.r(), identb.r()], [ps_t.r()], inc=(q == 3))
            K.tt(yT.ap()[:, 0:4, c * 128:(c + 1) * 128], ps_t.ap()[:, 0:512].rearrange("p (t n) -> p t n", t=4),
                 bc(normg.ap(), 2, [128, 4, 128]), ALU.mult, [ps_t.r(), normg.r()], xr(yT, c // 4, range(4)))
            K.act(el.ap(), csb.ap()[:, 8:16], AF.Exp, [csb.r()], [el.r()])
            K.tt(tail.ap(), csb.ap()[:, 8:16], csb.ap()[:, 0:8], ALU.subtract, [csb.r()], [tail.r()])
            K.act(tail.ap(), tail.ap(), AF.Exp, [tail.r()], [tail.r()])
            K.tt(tail.ap(), tail.ap(), dtt.ap(), ALU.mult, [tail.r(), dtt.r()], [tail.r()])
            K.tt(xw.ap().rearrange("p (h q) -> p h q", h=8), XT.ap()[:, 0:512].rearrange("p (h q) -> p h q", h=8),
                 bc(tail.ap(), 2, [128, 8, 64]), ALU.mult, [XT.r(), tail.r()], [xw.r()])
            K.mm(ps_h.ap(), XT.ap()[:, 512:640], xw.ap(), [XT.r(), xw.r()], [ps_h.r()])
            for g in range(2):
                sl = slice(64 * g, 64 * g + 64)
                K.tt(HS32.ap()[sl], HS32.ap()[sl], bc(el.ap()[sl, 4 * g:4 * g + 4], 2, [64, 4, 64]), ALU.mult,
                     [HS32.r(), el.r()], [HS32.r()])
                K.tt(HS32.ap()[sl], HS32.ap()[sl],
                     ps_h.ap()[sl, 256 * g:256 * g + 256].rearrange("p (h q) -> p h q", h=4), ALU.add,
                     [HS32.r(), ps_h.r()], [HS32.r()])
            K.cp(HSb.ap(), HS32.ap(), [HS32.r()], [HSb.r()], eng="act")

        xb = XB[(NCH - 1) % 2]
        for i in range(3):
            K.dma(dr["p_conv"][l, i].rearrange("(t p) -> p t", p=128), xb.ap()[:, :, 128 + i], r=[xb.r()])
        for q in range(2):
            K.tr(ps_y.ap()[:, q * 128:(q + 1) * 128], HS32.ap().rearrange("p h q -> p (h q)")[:, q * 128:(q + 1) * 128],
                 identf.ap(), [HS32.r(), identf.r()], [ps_y.r()], inc=(q == 1))
        K.cp(hsT.ap(), ps_y.ap()[:, 0:256].rearrange("p (q n) -> p q n", q=2), [ps_y.r()], [hsT.r()])
        for g in range(2):
            for q in range(2):
                K.dma(dr["p_ssd"][l, 4 * g + 2 * q:4 * g + 2 * q + 2].rearrange("h p n -> (h p) n"),
                      hsT.ap()[:, q, 64 * g:64 * g + 64], r=[hsT.r()])
        P.barrier()


def ssd_epilogue(K, C, y, sz, ss, yn, n):
    K.tt(y.ap()[0:n], y.ap()[0:n], sz.ap()[0:n], ALU.mult, [y.r(), sz.r()], [y.r()])
    K.act(yn.ap()[0:n], y.ap()[0:n], AF.Square, [y.r()], [yn.r(), ss.r()], accum_out=ss.ap()[0:n])
    K.act(ss.ap()[0:n], ss.ap()[0:n], AF.Sqrt, [ss.r()], [ss.r()], scale=1.0 / 512, bias=RMS_EPS)
    K.recip(ss.ap()[0:n], ss.ap()[0:n], [ss.r()], [ss.r()])
    K.ts(yn.ap()[0:n], y.ap()[0:n], ss.ap()[0:n], ALU.mult, [y.r(), ss.r()], [yn.r()])


def ssd_gla_phase(K, dr, C, l, yT, dbg_out):
    P = K.P
    identb, identf, maskU, maskSL, ones1 = C["identb"], C["identf"], C["maskU"], C["maskSL"], C["ones1"]
    hc, hs = C["hc"], C["hs"]
    G0 = OFF["gq"]
    with contextlib.ExitStack() as ph0:
        win = K.sb(ph0, "win_ssd", [128, KT, 1288], BF16)
        K.dma(win.ap(), dr["w_in"][l, :, 0:1288].rearrange("(k p) n -> p k n", p=128), w=[win.r()], q="pool")
        convw = K.sb(ph0, "convw", [128, 6, 4], F32)
        convb = K.sb(ph0, "convb", [128, 6], F32)
        for i in range(4):
            K.dma(convw.ap()[:, :, i], dr["ssd_conv_w"][l, i].rearrange("(t p) -> p t", p=128), w=[convw.r()])
        K.dma(convb.ap(), dr["ssd_conv_b"][l].rearrange("(t p) -> p t", p=128), w=[convb.r()])
        normg = K.sb(ph0, "normg", [128, 4], F32)
        K.dma(normg.ap(), dr["ssd_norm_g"][l].rearrange("(t p) -> p t", p=128), w=[normg.r()])
        dtbB = K.sb(ph0, "dtbB", [128, 8], F32)
        aB = K.sb(ph0, "aB", [128, 8], F32)
        dB = K.sb(ph0, "dB", [128, 8], F32)
        K.dma(dtbB.ap(), dr["ssd_dt_bias"][l:l + 1, :].to_broadcast([128, 8]), w=[dtbB.r()])
        K.dma(aB.ap(), dr["ssd_a_log"][l:l + 1, :].to_broadcast([128, 8]), w=[aB.r()])
        K.dma(dB.ap(), dr["ssd_d"][l:l + 1, :].to_broadcast([128, 8]), w=[dB.r()])
        K.act(aB.ap(), aB.ap(), AF.Exp, [aB.r()], [aB.r()])
        K.ts(aB.ap(), aB.ap(), -1.0, ALU.mult, [aB.r()], [aB.r()])
        wing = K.sb(ph0, "win_gla", [128, KT, 784], BF16)
        K.dma(wing.ap(), dr["w_in"][l, :, G0:G0 + 784].rearrange("(k p) n -> p k n", p=128), w=[wing.r()], q="pool")
        wgk2 = K.sb(ph0, "wgk2", [16, 128], BF16)
        K.dma(wgk2.ap(), dr["gla_w_gk2"][l], w=[wgk2.r()], q="pool")
        bgkB = K.sb(ph0, "bgkB", [128, 128], F32)
        K.dma(bgkB.ap(), dr["gla_b_gk"][l:l + 1, :].to_broadcast([128, 128]), w=[bgkB.r()])
        gcol = K.sb(ph0, "gcol", [128, 1], F32)
        for t in range(2):
            K.dma(gcol.ap()[64 * t:64 * t + 64, :], dr["gla_norm_g"][l].rearrange("(e o) -> e o", o=1), w=[gcol.r()])
        with contextlib.ExitStack() as ph:
            BM = K.sb(ph, "BM", [128, 256], F32)
            hm = K.sb(ph, "hm", [128, 4], F32)
            K.memset(BM.ap(), 1.0, [BM.r()], eng="pool")
            K.memset(hm.ap(), 1.0, [hm.r()], eng="pool")
            for hh in range(4):
                for (t, sl, n) in ((BM, slice(64 * hh, 64 * hh + 64), 64), (hm, slice(hh, hh + 1), 1)):
                    ap = t.ap()[:, sl]
                    K.P.op("pool", lambda e, ap=ap, n=n, hh=hh: e.affine_select(
                        out=ap, in_=ap, pattern=[[0, n]], compare_op=ALU.is_ge, fill=0.0, base=-32 * hh,
                        channel_multiplier=1), reads=[t.r()], writes=[t.r()])
                    K.P.op("pool", lambda e, ap=ap, n=n, hh=hh: e.affine_select(
                        out=ap, in_=ap, pattern=[[0, n]], compare_op=ALU.is_gt, fill=0.0, base=32 * hh + 32,
                        channel_multiplier=-1), reads=[t.r()], writes=[t.r()])
            PX = [K.ps(ph, f"PX{i}", [128, 512], F32) for i in range(2)]
            PZ = K.ps(ph, "PZ", [128, 512], F32)
            PC = K.ps(ph, "PC", [128, 512], F32)
            PF = K.ps(ph, "PF", [128, 512], F32)
            PV = K.ps(ph, "PV", [128, 512], F32)
            PL = K.ps(ph, "PL", [128, 512], F32)
            PT = K.ps(ph, "PT", [128, 1024], BF16)
            XB = [K.sb(ph, f"XB{i}", [128, 6, 131], F32) for i in range(2)]
            XC = [K.sb(ph, f"XC{i}", [128, 6, 128], BF16) for i in range(2)]
            cacc = [K.sb(ph, f"cacc{i}", [128, 128], F32) for i in range(2)]
            sz = K.sb(ph, "sz", [128, 512], F32)
            dtt = K.sb(ph, "dtt", [128, 8], F32)
            dtmp = K.sb(ph, "dtmp", [128, 8], F32)
            dtA = K.sb(ph, "dtA", [128, 8], F32)
            csb = K.sb(ph, "csb", [128, 16], F32)
            e1 = K.sb(ph, "e1", [128, 8], F32)
            el = K.sb(ph, "el", [128, 8], F32)
            tail = K.sb(ph, "tail", [128, 8], F32)
            Rt = K.sb(ph, "Rt", [128, 8, 128], F32)
            dec = K.sb(ph, "dec", [128, 8, 128], F32)
            Gs = K.sb(ph, "Gs", [128, 2, 128], F32)
            Mb = K.sb(ph, "Mb", [128, 8, 128], BF16)
            XT = K.sb(ph, "XT", [128, 640], BF16)
            xD = K.sb(ph, "xD", [128, 512], BF16)
            xw = K.sb(ph, "xw", [128, 512], BF16)
            t1 = K.sb(ph, "t1", [128, 512], F32)
            yn = K.sb(ph, "yn", [128, 512], BF16)
            ss = K.sb(ph, "ss", [128, 1], F32)
            HS32 = K.sb(ph, "HS32", [128, 4, 64], F32)
            HSb = K.sb(ph, "HSb", [128, 4, 64], BF16)
            glo = K.sb(ph, "glo", [16, 128], BF16)
            lg = K.sb(ph, "lg", [128, 128], F32)
            lgt = K.sb(ph, "lgt", [128, 128], F32)
            Eq = K.sb(ph, "Eq", [128, 128], F32)
            Ek = K.sb(ph, "Ek", [128, 128], F32)
            Ekt = K.sb(ph, "Ekt", [128, 128], F32)
            qt = K.sb(ph, "qt", [128, 128], BF16)
            kf = K.sb(ph, "kf", [128, 128], F32)
            km = K.sb(ph, "km", [128, 4, 128], BF16)
            ktm = K.sb(ph, "ktm", [128, 128], BF16)
            vtm = K.sb(ph, "vtm", [128, 256], BF16)
            sgg = K.sb(ph, "sgg", [128, 256], F32)
            A = K.sb(ph, "A", [128, 4, 128], BF16)
            osq = K.sb(ph, "osq", [128, 256], F32)
            ms = K.sb(ph, "ms", [128, 4], F32)
            on = K.sb(ph, "on", [128, 256], F32)
            onb = K.sb(ph, "onb", [128, 256], BF16)
            tmpS = K.sb(ph, "tmpS", [128, 256], F32)
            S32 = K.sb(ph, "S32", [128, 256], F32)
            Sb = K.sb(ph, "Sb", [128, 256], BF16)

            K.memset(HS32.ap(), 0.0, [HS32.r()])
            K.memset(HSb.ap(), 0.0, [HSb.r()])
            K.memset(XB[0].ap()[:, :, 0:3], 0.0, [XB[0].r()])
            K.memset(S32.ap(), 0.0, [S32.r()])
            K.memset(Sb.ap(), 0.0, [Sb.r()])

            def ssd_body(c):
                h = hc[c % 2]
                xb, xc = XB[c % 2], XC[c % 2]
                for ct in range(6):
                    px = PX[0] if ct < 4 else PX[1]
                    cc = ct if ct < 4 else ct - 4
                    for k in range(KT):
                        K.mm(px.ap()[:, cc * 128:(cc + 1) * 128], win.ap()[:, k, 512 + ct * 128:512 + (ct + 1) * 128],
                             h.ap()[:, k, :], [win.r(), h.r()], [px.r()], start=(k == 0), stop=(k == KT - 1))
                    yield
                K.cp(xb.ap()[:, 0:4, 3:131], PX[0].ap().rearrange("p (t n) -> p t n", t=4), [PX[0].r()], [xb.r()],
                     eng="act")
                yield
                K.cp(xb.ap()[:, 4:6, 3:131], PX[1].ap()[:, 0:256].rearrange("p (t n) -> p t n", t=2), [PX[1].r()],
                     [xb.r()], eng="act")
                yield
                if c + 1 < NCH:
                    K.cp(XB[(c + 1) % 2].ap()[:, :, 0:3], xb.ap()[:, :, 128:131], [xb.r()], [XB[(c + 1) % 2].r()])
                for ct in range(6):
                    ca = cacc[ct % 2]
                    K.ts(ca.ap(), xb.ap()[:, ct, 0:128], convw.ap()[:, ct, 0:1], ALU.mult,
                         [xb.r(), convw.r(), convb.r()], [ca.r()], s2=convb.ap()[:, ct:ct + 1], op1=ALU.add)
                    for i in range(1, 4):
                        K.stt(ca.ap(), xb.ap()[:, ct, i:i + 128], convw.ap()[:, ct, i:i + 1], ca.ap(), ALU.mult, ALU.add,
                              [xb.r(), convw.r(), ca.r()], [ca.r()])
                    yield
                    K.act(xc.ap()[:, ct, :], ca.ap(), AF.Silu, [ca.r()], [xc.r()])
                    yield
                for k in range(KT):
                    K.mm(PZ.ap(), h.ap()[:, k, :], win.ap()[:, k, 0:512], [h.r(), win.r()], [PZ.r()],
                         start=(k == 0), stop=(k == KT - 1))
                for k in range(KT):
                    K.mm(PC.ap()[:, 0:8], h.ap()[:, k, :], win.ap()[:, k, 1280:1288], [h.r(), win.r()], [PC.r()],
                         start=(k == 0), stop=(k == KT - 1))
                yield
                K.act(sz.ap(), PZ.ap(), AF.Silu, [PZ.r()], [sz.r()])
                yield
                K.tt(dtt.ap(), PC.ap()[:, 0:8], dtbB.ap(), ALU.add, [PC.r(), dtbB.r()], [dtt.r()])
                yield
                softplus_(K, (dtt.ap(), dtt.r()), (dtmp.ap(), dtmp.r()), None, None)
                yield
                K.tt(dtA.ap(), dtt.ap(), aB.ap(), ALU.mult, [dtt.r(), aB.r()], [dtA.r()])
                yield
                K.mm(PC.ap()[:, 8:16], maskU.ap(), dtA.ap(), [maskU.r(), dtA.r()], [PC.r()])
                K.mm(PC.ap()[:, 16:24], ones1.ap(), dtA.ap(), [ones1.r(), dtA.r()], [PC.r()])
                yield
                K.tt(Rt.ap(), bc(maskU.ap(), 1, [128, 8, 128]), bc(dtA.ap(), 2, [128, 8, 128]), ALU.mult,
                     [maskU.r(), dtA.r()], [Rt.r()])
                yield
                for hf in range(2):
                    K.mm(PX[hf].ap(), maskSL.ap(), Rt.ap()[:, hf * 4:(hf + 1) * 4, :].rearrange("p h i -> p (h i)"),
                         [maskSL.r(), Rt.r()], [PX[hf].r()])
                    yield
                for hf in range(2):
                    K.act(dec.ap()[:, hf * 4:(hf + 1) * 4, :].rearrange("p h i -> p (h i)"), PX[hf].ap(), AF.Exp,
                          [PX[hf].r()], [dec.r()])
                    yield
                K.cp(csb.ap(), PC.ap()[:, 8:24], [PC.r()], [csb.r()], eng="act")
                yield
                for g in range(2):
                    K.mm(PC.ap()[:, 256 + g * 128:256 + (g + 1) * 128], xc.ap()[64 * g:64 * g + 64, 4, :],
                         xc.ap()[64 * g:64 * g + 64, 5, :], [xc.r()], [PC.r()], self_wait=(g == 1))
                yield
                K.tt(Gs.ap(), PC.ap()[:, 256:512].rearrange("p (g i) -> p g i", g=2), bc(maskU.ap(), 1, [128, 2, 128]),
                     ALU.mult, [PC.r(), maskU.r()], [Gs.r()])
                yield
                K.tt(dec.ap().rearrange("p (g r) i -> p g r i", g=2), dec.ap().rearrange("p (g r) i -> p g r i", g=2),
                     bc(Gs.ap(), 2, [128, 2, 4, 128]), ALU.mult, [dec.r(), Gs.r()], [dec.r()])
                yield
                K.tt(Mb.ap(), dec.ap(), bc(dtt.ap(), 2, [128, 8, 128]), ALU.mult, [dec.r(), dtt.r()], [Mb.r()])
                yield
                for ct in range(5):
                    K.tr(PT.ap()[:, ct * 128:(ct + 1) * 128], xc.ap()[:, ct, :], identb.ap(), [xc.r(), identb.r()],
                         [PT.r()], inc=(ct == 4))
                yield
                K.cp(XT.ap(), PT.ap()[:, 0:640], [PT.r()], [XT.r()], eng="act")
                yield
                K.tt(xD.ap().rearrange("p (h q) -> p h q", h=8), XT.ap()[:, 0:512].rearrange("p (h q) -> p h q", h=8),
                     bc(dB.ap(), 2, [128, 8, 64]), ALU.mult, [XT.r(), dB.r()], [xD.r()])
                yield
                K.mm(PZ.ap(), identb.ap(), xD.ap(), [identb.r(), xD.r()], [PZ.r()], start=True, stop=False)
                for hh in range(8):
                    K.mm(PZ.ap()[:, hh * 64:(hh + 1) * 64], Mb.ap()[:, hh, :], XT.ap()[:, hh * 64:(hh + 1) * 64],
                         [Mb.r(), XT.r()], [PZ.r()], start=False, stop=(hh == 7))
                for g in range(2):
                    K.mm(PC.ap()[:, g * 256:(g + 1) * 256], xc.ap()[64 * g:64 * g + 64, 5, :],
                         HSb.ap()[64 * g:64 * g + 64, :, :].rearrange("p h q -> p (h q)"), [xc.r(), HSb.r()], [PC.r()],
                         self_wait=(g == 1))
                yield
                K.act(e1.ap(), csb.ap()[:, 0:8], AF.Exp, [csb.r()], [e1.r()])
                yield
                K.tt(t1.ap().rearrange("p (h q) -> p h q", h=8), PC.ap().rearrange("p (h q) -> p h q", h=8),
                     bc(e1.ap(), 2, [128, 8, 64]), ALU.mult, [PC.r(), e1.r()], [t1.r()])
                yield
                K.tt(t1.ap(), t1.ap(), PZ.ap(), ALU.add, [t1.r(), PZ.r()], [t1.r()])
                yield
                ssd_epilogue(K, C, t1, sz, ss, yn, 128)
                yield
                for q in range(4):
                    K.tr(PT.ap()[:, q * 128:(q + 1) * 128], yn.ap()[:, q * 128:(q + 1) * 128], identb.ap(),
                         [yn.r(), identb.r()], [PT.r()], inc=(q == 3))
                yield
                K.tt(yT.ap()[:, 0:4, c * 128:(c + 1) * 128], PT.ap()[:, 0:512].rearrange("p (t n) -> p t n", t=4),
                     bc(normg.ap(), 2, [128, 4, 128]), ALU.mult, [PT.r(), normg.r()], xr(yT, c // 4, range(4)))
                yield
                K.act(el.ap(), csb.ap()[:, 8:16], AF.Exp, [csb.r()], [el.r()])
                yield
                K.tt(tail.ap(), csb.ap()[:, 8:16], csb.ap()[:, 0:8], ALU.subtract, [csb.r()], [tail.r()])
                yield
                K.act(tail.ap(), tail.ap(), AF.Exp, [tail.r()], [tail.r()])
                yield
                K.tt(tail.ap(), tail.ap(), dtt.ap(), ALU.mult, [tail.r(), dtt.r()], [tail.r()])
                yield
                K.tt(xw.ap().rearrange("p (h q) -> p h q", h=8), XT.ap()[:, 0:512].rearrange("p (h q) -> p h q", h=8),
                     bc(tail.ap(), 2, [128, 8, 64]), ALU.mult, [XT.r(), tail.r()], [xw.r()])
                yield
                K.mm(PC.ap(), XT.ap()[:, 512:640], xw.ap(), [XT.r(), xw.r()], [PC.r()])
                yield
                for g in range(2):
                    sl = slice(64 * g, 64 * g + 64)
                    K.tt(HS32.ap()[sl], HS32.ap()[sl], bc(el.ap()[sl, 4 * g:4 * g + 4], 2, [64, 4, 64]), ALU.mult,
                         [HS32.r(), el.r()], [HS32.r()])
                    K.tt(HS32.ap()[sl], HS32.ap()[sl],
                         PC.ap()[sl, 256 * g:256 * g + 256].rearrange("p (h q) -> p h q", h=4), ALU.add,
                         [HS32.r(), PC.r()], [HS32.r()])
                    yield
                K.cp(HSb.ap(), HS32.ap(), [HS32.r()], [HSb.r()], eng="act")
                yield

            def gla_body(c):
                h = hc[c % 2]
                for (dst, cols) in ((PF.ap()[:, 0:128], slice(0, 128)), (PF.ap()[:, 128:256], slice(128, 256)),
                                    (PF.ap()[0:16, 256:384], slice(512, 528))):
                    for k in range(KT):
                        K.mm(dst, wing.ap()[:, k, cols], h.ap()[:, k, :], [wing.r(), h.r()], [PF.r()],
                             start=(k == 0), stop=(k == KT - 1))
                    yield
                for (dst, cols, pst) in ((PV.ap()[:, 0:256], slice(256, 512), PV), (PF.ap()[:, 384:512], slice(128, 256), PF),
                                         (PV.ap()[:, 256:512], slice(528, 784), PV)):
                    for k in range(KT):
                        K.mm(dst, h.ap()[:, k, :], wing.ap()[:, k, cols], [wing.r(), h.r()], [pst.r()],
                             start=(k == 0), stop=(k == KT - 1))
                    yield
                K.cp(glo.ap(), PF.ap()[0:16, 256:384], [PF.r()], [glo.r()], eng="act")
                yield
                K.mm(PL.ap()[:, 0:128], glo.ap(), wgk2.ap(), [glo.r(), wgk2.r()], [PL.r()])
                yield
                K.stt(lg.ap(), PL.ap()[:, 0:128], -1.0, bgkB.ap(), ALU.mult, ALU.subtract, [PL.r(), bgkB.r()], [lg.r()])
                yield
                softplus_(K, (lg.ap(), lg.r()), (lgt.ap(), lgt.r()), None, None)
                yield
                K.ts(lg.ap(), lg.ap(), -1.0 / 16.0, ALU.mult, [lg.r()], [lg.r()])
                yield
                K.mm(PL.ap()[:, 128:256], lg.ap(), maskU.ap(), [lg.r(), maskU.r()], [PL.r()])
                K.mm(PL.ap()[:, 256:384], maskU.ap(), lg.ap(), [lg.r(), maskU.r()], [PL.r()])
                yield
                K.act(Eq.ap(), PL.ap()[:, 128:256], AF.Exp, [PL.r()], [Eq.r()])
                yield
                K.act(Ek.ap(), PL.ap()[:, 128:256], AF.Exp, [PL.r()], [Ek.r()], scale=-1.0)
                yield
                K.act(Ekt.ap(), PL.ap()[:, 256:384], AF.Exp, [PL.r()], [Ekt.r()], scale=-1.0)
                yield
                K.stt(qt.ap(), PF.ap()[:, 0:128], 32.0 ** -0.5, Eq.ap(), ALU.mult, ALU.mult, [PF.r(), Eq.r()], [qt.r()])
                yield
                K.tt(kf.ap(), PF.ap()[:, 128:256], Ek.ap(), ALU.mult, [PF.r(), Ek.r()], [kf.r()])
                yield
                K.tt(km.ap(), bc(kf.ap(), 1, [128, 4, 128]), bc(hm.ap(), 2, [128, 4, 128]), ALU.mult, [kf.r(), hm.r()],
                     [km.r()])
                yield
                K.tt(ktm.ap(), PF.ap()[:, 384:512], Ekt.ap(), ALU.mult, [PF.r(), Ekt.r()], [ktm.r()])
                yield
                K.cp(vtm.ap(), PV.ap()[:, 0:256], [PV.r()], [vtm.r()], eng="act")
                yield
                K.act(sgg.ap(), PV.ap()[:, 256:512], AF.Silu, [PV.r()], [sgg.r()])
                yield
                for hh in range(4):
                    K.mm(PL.ap()[:, hh * 128:(hh + 1) * 128], km.ap()[:, hh, :], qt.ap(), [km.r(), qt.r()], [PL.r()],
                         inc=(hh == 3))
                yield
                K.tt(A.ap(), PL.ap().rearrange("p (h i) -> p h i", h=4), bc(maskU.ap(), 1, [128, 4, 128]), ALU.mult,
                     [PL.r(), maskU.r()], [A.r()])
                yield
                K.mm(PL.ap()[:, 0:256], qt.ap(), Sb.ap(), [qt.r(), Sb.r()], [PL.r()], start=True, stop=False)
                for hh in range(4):
                    K.mm(PL.ap()[:, hh * 64:(hh + 1) * 64], A.ap()[:, hh, :], vtm.ap()[:, hh * 64:(hh + 1) * 64],
                         [A.r(), vtm.r()], [PL.r()], start=False, stop=(hh == 3))
                yield
                K.act(osq.ap(), PL.ap()[:, 0:256], AF.Square, [PL.r()], [osq.r()])
                yield
                K.red(ms.ap(), osq.ap().rearrange("p (h e) -> p h e", h=4), [osq.r()], [ms.r()])
                yield
                K.act(ms.ap(), ms.ap(), AF.Sqrt, [ms.r()], [ms.r()], scale=1.0 / 64, bias=RMS_EPS)
                yield
                K.recip(ms.ap(), ms.ap(), [ms.r()], [ms.r()])
                yield
                K.tt(on.ap().rearrange("p (h e) -> p h e", h=4), PL.ap()[:, 0:256].rearrange("p (h e) -> p h e", h=4),
                     bc(ms.ap(), 2, [128, 4, 64]), ALU.mult, [PL.r(), ms.r()], [on.r()])
                yield
                K.tt(onb.ap(), on.ap(), sgg.ap(), ALU.mult, [on.r(), sgg.r()], [onb.r()])
                yield
                for q in range(2):
                    K.tr(PT.ap()[:, 768 + q * 128:768 + (q + 1) * 128], onb.ap()[:, q * 128:(q + 1) * 128], identb.ap(),
                         [onb.r(), identb.r()], [PT.r()], inc=(q == 1))
                yield
                K.ts(yT.ap()[:, 6:8, c * 128:(c + 1) * 128], PT.ap()[:, 768:1024].rearrange("p (t n) -> p t n", t=2),
                     gcol.ap(), ALU.mult, [PT.r(), gcol.r()], xr(yT, c // 4, range(6, 8)))
                yield
                K.mm(PL.ap()[:, 256:512], ktm.ap(), vtm.ap(), [ktm.r(), vtm.r()], [PL.r()])
                yield
                K.tt(tmpS.ap(), PL.ap()[:, 256:512], BM.ap(), ALU.mult, [PL.r(), BM.r()], [tmpS.r()])
                yield
                K.tt(S32.ap(), S32.ap(), tmpS.ap(), ALU.add, [S32.r(), tmpS.r()], [S32.r()])
                yield
                K.ts(S32.ap(), S32.ap(), Eq.ap()[:, 127:128], ALU.mult, [S32.r(), Eq.r()], [S32.r()])
                yield
                K.cp(Sb.ap(), S32.ap(), [S32.r()], [Sb.r()], eng="act")
                yield

            for c in range(NCH):
                make_hc(K, C, l, hc[c % 2], c)
                interleave([ssd_body(c), gla_body(c)], ratio=[3, 2])

            xb = XB[(NCH - 1) % 2]
            for i in range(3):
                K.dma(dr["p_conv"][l, i].rearrange("(t p) -> p t", p=128), xb.ap()[:, :, 128 + i], r=[xb.r()])
            hsT = t1
            for q in range(2):
                K.tr(PZ.ap()[:, q * 128:(q + 1) * 128], HS32.ap().rearrange("p h q -> p (h q)")[:, q * 128:(q + 1) * 128],
                     identf.ap(), [HS32.r(), identf.r()], [PZ.r()], inc=(q == 1))
            K.cp(hsT.ap()[:, 0:256], PZ.ap()[:, 0:256], [PZ.r()], [hsT.r()])
            for g in range(2):
                for q in range(2):
                    K.dma(dr["p_ssd"][l, 4 * g + 2 * q:4 * g + 2 * q + 2].rearrange("h p n -> (h p) n"),
                          hsT.ap()[:, q * 128 + 64 * g:q * 128 + 64 * g + 64], r=[hsT.r()])
            for hh in range(4):
                K.dma(dr["p_gla"][l, hh], S32.ap()[32 * hh:32 * hh + 32, 64 * hh:64 * hh + 64], r=[S32.r()])
            P.barrier()
        ssd_sample(K, dr, C, l, yT, win, aB, dB, dtbB, dbg_out)
        gla_sample(K, dr, C, l, yT, wing, wgk2, bgkB)


def dram_scratch(K, name, shape):
    K.uid += 1
    h = K.nc.dram_tensor(f"scr_{name}_{K.uid}", list(shape), F32)
    return Tn(h, name)


def ssd_sample(K, dr, C, l, yT, win, aB, dB, dtbB, dbg_out):
    P = K.P
    hs, identb = C["hs"], C["identb"]
    with contextlib.ExitStack() as ph:
        cs = K.sb(ph, "cs", [NS, 768], F32)
        wB = K.sb(ph, "wB", [NS, 768], F32)
        gB = K.sb(ph, "gB", [NS, 512], F32)
        xbcs = K.sb(ph, "xbcs", [NS, 768], F32)
        acc = K.sb(ph, "acc", [NS, 768], F32)
        tmpc = K.sb(ph, "tmpc", [NS, 768], F32)
        szs = K.sb(ph, "szs", [NS, 512], F32)
        dts = K.sb(ph, "dts", [NS, 8], F32)
        dtm = K.sb(ph, "dtm", [NS, 8], F32)
        rep = K.sb(ph, "rep", [NS, 2, 8, 64], F32)
        pk = K.sb(ph, "pk", [NS, 8, 3], F32)
        Hs = K.sb(ph, "Hs", [128, 64, 64], F32)
        tmpH = K.sb(ph, "tmpH", [128, 32, 64], F32)
        xh = K.sb(ph, "xh", [128, 64], F32)
        BCh = K.sb(ph, "BCh", [128, 2, 64], F32)
        pkh = K.sb(ph, "pkh", [128, 3], F32)
        dA = K.sb(ph, "dA", [128, 1], F32)
        xdt = K.sb(ph, "xdt", [128, 64], F32)
        yh = K.sb(ph, "yh", [128, 64], F32)
        ysm = K.sb(ph, "ysm", [NS, 512], F32)
        yns = K.sb(ph, "yns", [NS, 512], BF16)
        sss = K.sb(ph, "sss", [NS, 1], F32)
        ps_a = K.ps(ph, "pss_a", [128, 512], F32)
        ps_b = K.ps(ph, "pss_b", [128, 512], F32)
        ps_d = K.ps(ph, "pss_d", [128, 512], F32)
        ps_t = K.ps(ph, "pss_t", [128, 1024], BF16)
        sx = dram_scratch(K, "sx", [NS, 512])
        sbc = dram_scratch(K, "sbc", [2, NS, 512])
        spk = dram_scratch(K, "spk", [NS, 24])
        sy = dram_scratch(K, "sy", [NS, 512])

        K.dma(gB.ap(), dr["ssd_norm_g"][l:l + 1, :].to_broadcast([NS, 512]), w=[gB.r()])
        K.dma(Hs.ap().rearrange("p a b -> p (a b)"), dr["st_ssd"][l].rearrange("b h p n -> (b h) (p n)"), w=[Hs.r()])
        for k in range(KT):
            K.mm(ps_a.ap()[0:NS, :], hs.ap()[:, k, :], win.ap()[:, k, 0:512], [hs.r(), win.r()], [ps_a.r()],
                 start=(k == 0), stop=(k == KT - 1))
        for k in range(KT):
            K.mm(ps_b.ap()[0:NS, :], hs.ap()[:, k, :], win.ap()[:, k, 512:1024], [hs.r(), win.r()], [ps_b.r()],
                 start=(k == 0), stop=(k == KT - 1))
        for k in range(KT):
            K.mm(ps_d.ap()[0:NS, 0:264], hs.ap()[:, k, :], win.ap()[:, k, 1024:1288], [hs.r(), win.r()], [ps_d.r()],
                 start=(k == 0), stop=(k == KT - 1))
        K.act(szs.ap(), ps_a.ap()[0:NS, :], AF.Silu, [ps_a.r()], [szs.r()])
        K.cp(xbcs.ap()[:, 0:512], ps_b.ap()[0:NS, :], [ps_b.r()], [xbcs.r()], eng="act")
        K.cp(xbcs.ap()[:, 512:768], ps_d.ap()[0:NS, 0:256], [ps_d.r()], [xbcs.r()], eng="act")
        K.tt(dts.ap(), ps_d.ap()[0:NS, 256:264], dtbB.ap()[0:NS, :], ALU.add, [ps_d.r(), dtbB.r()], [dts.r()])
        softplus_(K, (dts.ap(), dts.r()), (dtm.ap(), dtm.r()), None, None)
        K.dma(wB.ap(), dr["ssd_conv_w"][l, 3:4, :].to_broadcast([NS, 768]), w=[wB.r()])
        K.tt(acc.ap(), xbcs.ap(), wB.ap(), ALU.mult, [xbcs.r(), wB.r()], [acc.r()])
        for i in range(3):
            K.dma(wB.ap(), dr["ssd_conv_w"][l, i:i + 1, :].to_broadcast([NS, 768]), w=[wB.r()])
            K.dma(cs.ap(), dr["st_conv"][l][:, i, :], w=[cs.r()])
            K.tt(tmpc.ap(), cs.ap(), wB.ap(), ALU.mult, [cs.r(), wB.r()], [tmpc.r()])
            K.tt(acc.ap(), acc.ap(), tmpc.ap(), ALU.add, [acc.r(), tmpc.r()], [acc.r()])
        K.dma(wB.ap(), dr["ssd_conv_b"][l:l + 1, :].to_broadcast([NS, 768]), w=[wB.r()])
        K.tt(acc.ap(), acc.ap(), wB.ap(), ALU.add, [acc.r(), wB.r()], [acc.r()])
        K.act(acc.ap(), acc.ap(), AF.Silu, [acc.r()], [acc.r()])
        K.dma(dr["s_conv"][l][:, 0:2, :], dr["st_conv"][l][:, 1:3, :])
        K.dma(dr["s_conv"][l][:, 2, :], xbcs.ap(), r=[xbcs.r()])
        K.dma(sx.ap(), acc.ap()[:, 0:512], r=[acc.r()], w=[sx.r()])
        K.cp(rep.ap().rearrange("p t (g r) n -> p t g r n", g=2),
             bc(acc.ap()[:, 512:768].rearrange("p (t g n) -> p t g n", t=2, g=2), 3, [NS, 2, 2, 4, 64]),
             [acc.r()], [rep.r()])
        K.cp(pk.ap()[:, :, 0], dts.ap(), [dts.r()], [pk.r()])
        K.cp(pk.ap()[:, :, 1], aB.ap()[0:NS, :], [aB.r()], [pk.r()])
        K.cp(pk.ap()[:, :, 2], dB.ap()[0:NS, :], [dB.r()], [pk.r()])
        K.dma(sbc.ap().rearrange("t b x -> b t x"), rep.ap().rearrange("p t h n -> p t (h n)"), r=[rep.r()],
              w=[sbc.r()])
        K.dma(spk.ap(), pk.ap().rearrange("p h q -> p (h q)"), r=[pk.r()], w=[spk.r()])
        K.dma(xh.ap(), sx.ap().rearrange("b (h p) -> (b h) p", h=8), r=[sx.r()], w=[xh.r()])
        for t in range(2):
            K.dma(BCh.ap()[:, t, :], sbc.ap()[t].rearrange("b (h n) -> (b h) n", h=8), r=[sbc.r()],
                  w=[BCh.r()])
        K.dma(pkh.ap(), spk.ap().rearrange("b (h q) -> (b h) q", h=8), r=[spk.r()], w=[pkh.r()])
        K.act(dA.ap(), pkh.ap()[:, 0:1], AF.Exp, [pkh.r()], [dA.r()], scale=pkh.ap()[:, 1:2])
        K.ts(xdt.ap(), xh.ap(), pkh.ap()[:, 0:1], ALU.mult, [xh.r(), pkh.r()], [xdt.r()])
        K.ts(Hs.ap(), Hs.ap(), dA.ap(), ALU.mult, [Hs.r(), dA.r()], [Hs.r()])
        for hf in range(2):
            sl = slice(32 * hf, 32 * hf + 32)
            K.tt(tmpH.ap(), bc(xdt.ap()[:, sl], 2, [128, 32, 64]), bc(BCh.ap()[:, 0, :], 1, [128, 32, 64]), ALU.mult,
                 [xdt.r(), BCh.r()], [tmpH.r()])
            K.tt(Hs.ap()[:, sl, :], Hs.ap()[:, sl, :], tmpH.ap(), ALU.add, [Hs.r(), tmpH.r()], [Hs.r()])
        K.dma(dr["s_ssd"][l].rearrange("b h p n -> (b h) (p n)"), Hs.ap().rearrange("p a b -> p (a b)"), r=[Hs.r()])
        for hf in range(2):
            sl = slice(32 * hf, 32 * hf + 32)
            K.tt(tmpH.ap(), Hs.ap()[:, sl, :], bc(BCh.ap()[:, 1, :], 1, [128, 32, 64]), ALU.mult, [Hs.r(), BCh.r()],
                 [tmpH.r()])
            K.red(yh.ap()[:, sl], tmpH.ap(), [tmpH.r()], [yh.r()])
        K.stt(yh.ap(), xh.ap(), pkh.ap()[:, 2:3], yh.ap(), ALU.mult, ALU.add, [xh.r(), pkh.r(), yh.r()], [yh.r()])
        K.dma(sy.ap().rearrange("b (h p) -> (b h) p", h=8), yh.ap(), r=[yh.r()], w=[sy.r()])
        K.dma(ysm.ap(), sy.ap(), r=[sy.r()], w=[ysm.r()])
        ssd_epilogue(K, C, ysm, szs, sss, yns, NS)
        K.tt(ysm.ap(), ysm.ap(), gB.ap(), ALU.mult, [ysm.r(), gB.r()], [ysm.r()])
        K.ts(yns.ap(), ysm.ap(), sss.ap(), ALU.mult, [ysm.r(), sss.r()], [yns.r()])
        for q in range(4):
            K.tr(ps_t.ap()[:, q * NS:(q + 1) * NS], yns.ap()[:, q * 128:(q + 1) * 128], identb.ap()[0:NS, 0:NS],
                 [yns.r(), identb.r()], [ps_t.r()], inc=(q == 3))
        K.cp(yT.ap()[:, 0:4, T:T + NS], ps_t.ap()[:, 0:4 * NS].rearrange("p (t n) -> p t n", t=4), [ps_t.r()],
             xr(yT, 4, range(4)))
        P.barrier()


def gla_phase(K, dr, C, l, yT, dbg_out):
    P = K.P
    identb, maskU = C["identb"], C["maskU"]
    hc, hs = C["hc"], C["hs"]
    G0 = OFF["gq"]
    with contextlib.ExitStack() as ph:
        win = K.sb(ph, "win_gla", [128, KT, 784], BF16)
        K.dma(win.ap(), dr["w_in"][l, :, G0:G0 + 784].rearrange("(k p) n -> p k n", p=128), w=[win.r()], q="pool")
        wgk2 = K.sb(ph, "wgk2", [16, 128], BF16)
        K.dma(wgk2.ap(), dr["gla_w_gk2"][l], w=[wgk2.r()], q="pool")
        bgkB = K.sb(ph, "bgkB", [128, 128], F32)
        K.dma(bgkB.ap(), dr["gla_b_gk"][l:l + 1, :].to_broadcast([128, 128]), w=[bgkB.r()])
        gcol = K.sb(ph, "gcol", [128, 1], F32)
        for t in range(2):
            K.dma(gcol.ap()[64 * t:64 * t + 64, :], dr["gla_norm_g"][l].rearrange("(e o) -> e o", o=1), w=[gcol.r()])
        BM = K.sb(ph, "BM", [128, 256], F32)
        hm = K.sb(ph, "hm", [128, 4], F32)
        K.memset(BM.ap(), 1.0, [BM.r()], eng="pool")
        K.memset(hm.ap(), 1.0, [hm.r()], eng="pool")
        for hh in range(4):
            for (t, sl, n) in ((BM, slice(64 * hh, 64 * hh + 64), 64), (hm, slice(hh, hh + 1), 1)):
                ap = t.ap()[:, sl]
                K.P.op("pool", lambda e, ap=ap, n=n, hh=hh: e.affine_select(
                    out=ap, in_=ap, pattern=[[0, n]], compare_op=ALU.is_ge, fill=0.0, base=-32 * hh,
                    channel_multiplier=1), reads=[t.r()], writes=[t.r()])
                K.P.op("pool", lambda e, ap=ap, n=n, hh=hh: e.affine_select(
                    out=ap, in_=ap, pattern=[[0, n]], compare_op=ALU.is_gt, fill=0.0, base=32 * hh + 32,
                    channel_multiplier=-1), reads=[t.r()], writes=[t.r()])
        gla_prompt(K, dr, C, l, yT, win, wgk2, bgkB, gcol, BM, hm)
        P.barrier()
        gla_sample(K, dr, C, l, yT, win, wgk2, bgkB)


def gla_prompt(K, dr, C, l, yT, win, wgk2, bgkB, gcol, BM, hm):
    P = K.P
    identb, maskU = C["identb"], C["maskU"]
    hc = C["hc"]
    with contextlib.ExitStack() as ph:
        glo = K.sb(ph, "glo", [16, 128], BF16)
        lg = K.sb(ph, "lg", [128, 128], F32)
        lgt = K.sb(ph, "lgt", [128, 128], F32)
        Eq = K.sb(ph, "Eq", [128, 128], F32)
        Ek = K.sb(ph, "Ek", [128, 128], F32)
        Ekt = K.sb(ph, "Ekt", [128, 128], F32)
        qt = K.sb(ph, "qt", [128, 128], BF16)
        kf = K.sb(ph, "kf", [128, 128], F32)
        km = K.sb(ph, "km", [128, 4, 128], BF16)
        ktm = K.sb(ph, "ktm", [128, 128], BF16)
        vtm = K.sb(ph, "vtm", [128, 256], BF16)
        sgg = K.sb(ph, "sgg", [128, 256], F32)
        A = K.sb(ph, "A", [128, 4, 128], BF16)
        osq = K.sb(ph, "osq", [128, 256], F32)
        ms = K.sb(ph, "ms", [128, 4], F32)
        on = K.sb(ph, "on", [128, 256], F32)
        onb = K.sb(ph, "onb", [128, 256], BF16)
        tmpS = K.sb(ph, "tmpS", [128, 256], F32)
        S32 = K.sb(ph, "S32", [128, 256], F32)
        Sb = K.sb(ph, "Sb", [128, 256], BF16)
        ps_f = K.ps(ph, "psg_f", [128, 512], F32)
        ps_m = K.ps(ph, "psg_m", [128, 512], F32)
        ps_g = K.ps(ph, "psg_g", [128, 512], F32)
        ps_l = K.ps(ph, "psg_l", [128, 512], F32)
        ps_a = K.ps(ph, "psg_a", [128, 512], F32)
        ps_o = K.ps(ph, "psg_o", [128, 512], F32)
        ps_t = K.ps(ph, "psg_t", [128, 1024], BF16)
        K.memset(S32.ap(), 0.0, [S32.r()])
        K.memset(Sb.ap(), 0.0, [Sb.r()])
        for c in range(NCH):
            h = hc[c % 2]
            make_hc(K, C, l, h, c)
            for (dst, cols, M) in ((ps_f.ap()[:, 0:128], slice(0, 128), 128), (ps_f.ap()[:, 128:256], slice(128, 256), 128),
                                   (ps_f.ap()[0:16, 256:384], slice(512, 528), 16)):
                for k in range(KT):
                    K.mm(dst, win.ap()[:, k, cols], h.ap()[:, k, :], [win.r(), h.r()], [ps_f.r()],
                         start=(k == 0), stop=(k == KT - 1))
            for (dst, cols, pst) in ((ps_m.ap()[:, 0:256], slice(256, 512), ps_m), (ps_m.ap()[:, 256:384], slice(128, 256), ps_m),
                                     (ps_g.ap()[:, 0:256], slice(528, 784), ps_g)):
                for k in range(KT):
                    K.mm(dst, h.ap()[:, k, :], win.ap()[:, k, cols], [win.r(), h.r()], [pst.r()],
                         start=(k == 0), stop=(k == KT - 1))
            K.cp(glo.ap(), ps_f.ap()[0:16, 256:384], [ps_f.r()], [glo.r()], eng="act")
            K.mm(ps_l.ap()[:, 0:128], glo.ap(), wgk2.ap(), [glo.r(), wgk2.r()], [ps_l.r()])
            K.stt(lg.ap(), ps_l.ap()[:, 0:128], -1.0, bgkB.ap(), ALU.mult, ALU.subtract, [ps_l.r(), bgkB.r()], [lg.r()])
            softplus_(K, (lg.ap(), lg.r()), (lgt.ap(), lgt.r()), None, None)
            K.ts(lg.ap(), lg.ap(), -1.0 / 16.0, ALU.mult, [lg.r()], [lg.r()])
            K.mm(ps_l.ap()[:, 128:256], lg.ap(), maskU.ap(), [lg.r(), maskU.r()], [ps_l.r()])
            K.mm(ps_l.ap()[:, 256:384], maskU.ap(), lg.ap(), [lg.r(), maskU.r()], [ps_l.r()])
            K.act(Eq.ap(), ps_l.ap()[:, 128:256], AF.Exp, [ps_l.r()], [Eq.r()])
            K.act(Ek.ap(), ps_l.ap()[:, 128:256], AF.Exp, [ps_l.r()], [Ek.r()], scale=-1.0)
            K.act(Ekt.ap(), ps_l.ap()[:, 256:384], AF.Exp, [ps_l.r()], [Ekt.r()], scale=-1.0)
            K.stt(qt.ap(), ps_f.ap()[:, 0:128], 32.0 ** -0.5, Eq.ap(), ALU.mult, ALU.mult, [ps_f.r(), Eq.r()], [qt.r()])
            K.tt(kf.ap(), ps_f.ap()[:, 128:256], Ek.ap(), ALU.mult, [ps_f.r(), Ek.r()], [kf.r()])
            K.tt(km.ap(), bc(kf.ap(), 1, [128, 4, 128]), bc(hm.ap(), 2, [128, 4, 128]), ALU.mult, [kf.r(), hm.r()],
                 [km.r()])
            K.tt(ktm.ap(), ps_m.ap()[:, 256:384], Ekt.ap(), ALU.mult, [ps_m.r(), Ekt.r()], [ktm.r()])
            K.cp(vtm.ap(), ps_m.ap()[:, 0:256], [ps_m.r()], [vtm.r()], eng="act")
            K.act(sgg.ap(), ps_g.ap()[:, 0:256], AF.Silu, [ps_g.r()], [sgg.r()])
            for hh in range(4):
                K.mm(ps_a.ap()[:, hh * 128:(hh + 1) * 128], km.ap()[:, hh, :], qt.ap(), [km.r(), qt.r()], [ps_a.r()],
                     inc=(hh == 3))
            K.tt(A.ap(), ps_a.ap().rearrange("p (h i) -> p h i", h=4), bc(maskU.ap(), 1, [128, 4, 128]), ALU.mult,
                 [ps_a.r(), maskU.r()], [A.r()])
            K.mm(ps_o.ap()[:, 0:256], qt.ap(), Sb.ap(), [qt.r(), Sb.r()], [ps_o.r()], start=True, stop=False)
            for hh in range(4):
                K.mm(ps_o.ap()[:, hh * 64:(hh + 1) * 64], A.ap()[:, hh, :], vtm.ap()[:, hh * 64:(hh + 1) * 64],
                     [A.r(), vtm.r()], [ps_o.r()], start=False, stop=(hh == 3))
            K.act(osq.ap(), ps_o.ap()[:, 0:256], AF.Square, [ps_o.r()], [osq.r()])
            K.red(ms.ap(), osq.ap().rearrange("p (h e) -> p h e", h=4), [osq.r()], [ms.r()])
            K.act(ms.ap(), ms.ap(), AF.Sqrt, [ms.r()], [ms.r()], scale=1.0 / 64, bias=RMS_EPS)
            K.recip(ms.ap(), ms.ap(), [ms.r()], [ms.r()])
            K.tt(on.ap().rearrange("p (h e) -> p h e", h=4), ps_o.ap()[:, 0:256].rearrange("p (h e) -> p h e", h=4),
                 bc(ms.ap(), 2, [128, 4, 64]), ALU.mult, [ps_o.r(), ms.r()], [on.r()])
            K.tt(onb.ap(), on.ap(), sgg.ap(), ALU.mult, [on.r(), sgg.r()], [onb.r()])
            for q in range(2):
                K.tr(ps_t.ap()[:, q * 128:(q + 1) * 128], onb.ap()[:, q * 128:(q + 1) * 128], identb.ap(),
                     [onb.r(), identb.r()], [ps_t.r()], inc=(q == 1))
            K.ts(yT.ap()[:, 6:8, c * 128:(c + 1) * 128], ps_t.ap()[:, 0:256].rearrange("p (t n) -> p t n", t=2),
                 gcol.ap(), ALU.mult, [ps_t.r(), gcol.r()], xr(yT, c // 4, range(6, 8)))
            K.mm(ps_o.ap()[:, 256:512], ktm.ap(), vtm.ap(), [ktm.r(), vtm.r()], [ps_o.r()])
            K.tt(tmpS.ap(), ps_o.ap()[:, 256:512], BM.ap(), ALU.mult, [ps_o.r(), BM.r()], [tmpS.r()])
            K.tt(S32.ap(), S32.ap(), tmpS.ap(), ALU.add, [S32.r(), tmpS.r()], [S32.r()])
            K.ts(S32.ap(), S32.ap(), Eq.ap()[:, 127:128], ALU.mult, [S32.r(), Eq.r()], [S32.r()])
            K.cp(Sb.ap(), S32.ap(), [S32.r()], [Sb.r()], eng="act")
        for hh in range(4):
            K.dma(dr["p_gla"][l, hh], S32.ap()[32 * hh:32 * hh + 32, 64 * hh:64 * hh + 64], r=[S32.r()])
        P.barrier()


def gla_sample(K, dr, C, l, yT, win, wgk2, bgkB):
    P = K.P
    hs, identb = C["hs"], C["identb"]
    with contextlib.ExitStack() as ph:
        glo = K.sb(ph, "glos", [16, NS], BF16)
        lg = K.sb(ph, "lgs", [NS, 128], F32)
        lgt = K.sb(ph, "lgts", [NS, 128], F32)
        pk = K.sb(ph, "pkg", [NS, 4, 160], F32)
        sgg = K.sb(ph, "sggs", [NS, 256], F32)
        gB = K.sb(ph, "gBg", [64, 64], F32)
        S = K.sb(ph, "Sg", [64, 32, 64], F32)
        tmp = K.sb(ph, "tmpg", [64, 32, 64], F32)
        pkh = K.sb(ph, "pkhg", [64, 160], F32)
        o = K.sb(ph, "og", [64, 64], F32)
        junk = K.sb(ph, "junkg", [64, 64], F32)
        ss = K.sb(ph, "ssg", [64, 1], F32)
        otm = K.sb(ph, "otm", [NS, 256], F32)
        otb = K.sb(ph, "otb", [NS, 256], BF16)
        ps_a = K.ps(ph, "psgs_a", [128, 512], F32)
        ps_b = K.ps(ph, "psgs_b", [128, 512], F32)
        ps_c = K.ps(ph, "psgs_c", [128, 512], F32)
        ps_t = K.ps(ph, "psgs_t", [128, 1024], BF16)
        spk = dram_scratch(K, "gpk", [NS, 640])
        so = dram_scratch(K, "go", [NS, 256])
        K.dma(gB.ap(), dr["gla_norm_g"][l:l + 1, :].to_broadcast([64, 64]), w=[gB.r()])
        K.dma(S.ap().rearrange("p d e -> p (d e)"), dr["st_gla"][l].rearrange("b h d e -> (b h) (d e)"), w=[S.r()])
        for k in range(KT):
            K.mm(ps_a.ap()[0:NS, :], hs.ap()[:, k, :], win.ap()[:, k, 0:512], [hs.r(), win.r()], [ps_a.r()],
                 start=(k == 0), stop=(k == KT - 1))
        for k in range(KT):
            K.mm(ps_b.ap()[0:NS, 0:256], hs.ap()[:, k, :], win.ap()[:, k, 528:784], [hs.r(), win.r()], [ps_b.r()],
                 start=(k == 0), stop=(k == KT - 1))
        for k in range(KT):
            K.mm(ps_c.ap()[0:16, 0:NS], win.ap()[:, k, 512:528], hs.ap()[:, k, :], [hs.r(), win.r()], [ps_c.r()],
                 start=(k == 0), stop=(k == KT - 1))
        K.cp(glo.ap(), ps_c.ap()[0:16, 0:NS], [ps_c.r()], [glo.r()], eng="act")
        K.mm(ps_c.ap()[0:NS, 128:256], glo.ap(), wgk2.ap(), [glo.r(), wgk2.r()], [ps_c.r()])
        K.stt(lg.ap(), ps_c.ap()[0:NS, 128:256], -1.0, bgkB.ap()[0:NS, :], ALU.mult, ALU.subtract,
              [ps_c.r(), bgkB.r()], [lg.r()])
        softplus_(K, (lg.ap(), lg.r()), (lgt.ap(), lgt.r()), None, None)
        K.act(lg.ap(), lg.ap(), AF.Exp, [lg.r()], [lg.r()], scale=-1.0 / 16.0)
        K.act(sgg.ap(), ps_b.ap()[0:NS, 0:256], AF.Silu, [ps_b.r()], [sgg.r()])
        K.ts(pk.ap()[:, :, 0:32], ps_a.ap()[0:NS, 0:128].rearrange("p (h d) -> p h d", h=4), 32.0 ** -0.5, ALU.mult,
             [ps_a.r()], [pk.r()])
        K.cp(pk.ap()[:, :, 32:64], ps_a.ap()[0:NS, 128:256].rearrange("p (h d) -> p h d", h=4), [ps_a.r()], [pk.r()])
        K.cp(pk.ap()[:, :, 64:96], lg.ap().rearrange("p (h d) -> p h d", h=4), [lg.r()], [pk.r()])
        K.cp(pk.ap()[:, :, 96:160], ps_a.ap()[0:NS, 256:512].rearrange("p (h e) -> p h e", h=4), [ps_a.r()], [pk.r()])
        K.dma(spk.ap(), pk.ap().rearrange("p h x -> p (h x)"), r=[pk.r()], w=[spk.r()])
        K.dma(pkh.ap(), spk.ap().rearrange("b (h x) -> (b h) x", h=4), r=[spk.r()], w=[pkh.r()])
        qh, kh, eh, vh = pkh.ap()[:, 0:32], pkh.ap()[:, 32:64], pkh.ap()[:, 64:96], pkh.ap()[:, 96:160]
        K.tt(S.ap(), S.ap(), bc(eh, 2, [64, 32, 64]), ALU.mult, [S.r(), pkh.r()], [S.r()])
        K.tt(tmp.ap(), bc(kh, 2, [64, 32, 64]), bc(vh, 1, [64, 32, 64]), ALU.mult, [pkh.r()], [tmp.r()])
        K.tt(S.ap(), S.ap(), tmp.ap(), ALU.add, [S.r(), tmp.r()], [S.r()])
        K.dma(dr["s_gla"][l].rearrange("b h d e -> (b h) (d e)"), S.ap().rearrange("p d e -> p (d e)"), r=[S.r()])
        K.tt(tmp.ap(), S.ap(), bc(qh, 2, [64, 32, 64]), ALU.mult, [S.r(), pkh.r()], [tmp.r()])
        K.red(o.ap(), tmp.ap().rearrange("p d e -> p e d"), [tmp.r()], [o.r()])
        K.act(junk.ap(), o.ap(), AF.Square, [o.r()], [junk.r(), ss.r()], accum_out=ss.ap())
        K.act(ss.ap(), ss.ap(), AF.Sqrt, [ss.r()], [ss.r()], scale=1.0 / 64, bias=RMS_EPS)
        K.recip(ss.ap(), ss.ap(), [ss.r()], [ss.r()])
        K.stt(o.ap(), o.ap(), ss.ap(), gB.ap(), ALU.mult, ALU.mult, [o.r(), ss.r(), gB.r()], [o.r()])
        K.dma(so.ap().rearrange("b (h e) -> (b h) e", h=4), o.ap(), r=[o.r()], w=[so.r()])
        K.dma(otm.ap(), so.ap(), r=[so.r()], w=[otm.r()])
        K.tt(otb.ap(), otm.ap(), sgg.ap(), ALU.mult, [otm.r(), sgg.r()], [otb.r()])
        for q in range(2):
            K.tr(ps_t.ap()[:, q * NS:(q + 1) * NS], otb.ap()[:, q * 128:(q + 1) * 128], identb.ap()[0:NS, 0:NS],
                 [otb.r(), identb.r()], [ps_t.r()], inc=(q == 1))
        K.cp(yT.ap()[:, 6:8, T:T + NS], ps_t.ap()[:, 0:2 * NS].rearrange("p (t n) -> p t n", t=2), [ps_t.r()],
             xr(yT, 4, range(6, 8)))
        P.barrier()


C0 = float(np.exp(-0.5))


def rwkv_prep(K, C, pc, LW, N, rw, prev, B, pl, pg, pn):
    blk64 = C["blk64"]
    MX, LI = B["MX"], B["LI"]
    mxa = MX.ap()[:, :, 0:N]
    K.tt(mxa, prev, rw, ALU.subtract, B["_rw_res"], [MX.r()])
    yield
    K.tt(mxa, mxa, bc(pc["mu"].ap(), 2, [128, 7, N]), ALU.mult, [MX.r(), pc["mu"].r()], [MX.r()])
    yield
    K.tt(mxa, mxa, rw, ALU.add, [MX.r()] + B["_rw_res"], [MX.r()])
    yield
    r, k, v = (MX.ap()[:, 0:2, 0:N], MX.ap()[:, 2:4, 0:N], MX.ap()[:, 4:6, 0:N])
    lia = LI.ap()[:, 0:N]
    K.act(lia[0:32], MX.ap()[0:32, 6, 0:N], AF.Tanh, [MX.r()], [LI.r()])
    yield
    K.cp(lia[32:64], MX.ap()[32:64, 6, 0:N], [MX.r()], [LI.r()], eng="act")
    yield
    K.act(lia[64:128], MX.ap()[64:128, 6, 0:N], AF.Sigmoid, [MX.r()], [LI.r()])
    yield
    for t in range(2):
        cs = slice(t * 128, (t + 1) * 128)
        K.mm(pl.ap()[:, t * N:(t + 1) * N], LW.ap()[0:32, cs], lia[0:32], [LW.r(), LI.r()], [pl.r()], self_wait=True)
        K.mm(pl.ap()[:, (2 + t) * N:(3 + t) * N], LW.ap()[32:64, cs], lia[32:64], [LW.r(), LI.r()], [pl.r()],
             self_wait=True)
        K.mm(pg.ap()[:, t * N:(t + 1) * N], LW.ap()[64:128, cs], lia[64:128], [LW.r(), LI.r()], [pg.r()],
             self_wait=True)
    g = lambda n: B[n].ap()[:, :, 0:N]
    for t in range(2):
        K.act(B["sig"].ap()[:, t, 0:N], pl.ap()[:, t * N:(t + 1) * N], AF.Sigmoid, [pl.r(), pc["w0"].r()],
              [B["sig"].r()], bias=pc["w0"].ap()[:, t:t + 1])
        K.act(B["aic"].ap()[:, t, 0:N], pl.ap()[:, (2 + t) * N:(3 + t) * N], AF.Sigmoid, [pl.r(), pc["a0"].r()],
              [B["aic"].r()], bias=pc["a0"].ap()[:, t:t + 1])
    K.cp(g("gate"), pg.ap()[:, 0:2 * N].rearrange("p (t n) -> p t n", t=2), [pg.r()], [B["gate"].r()], eng="act")
    yield
    K.tt(g("kk"), k, bc(pc["k_k"].ap(), 2, [128, 2, N]), ALU.mult, [MX.r(), pc["k_k"].r()], [B["kk"].r()])
    yield
    K.tt(g("t1"), g("kk"), g("kk"), ALU.mult, [B["kk"].r()], [B["t1"].r()])
    yield
    for t in range(2):
        K.mm(pn.ap()[:, t * N:(t + 1) * N], blk64.ap(), B["t1"].ap()[:, t, 0:N], [blk64.r(), B["t1"].r()], [pn.r()])
    K.act(g("t1"), pn.ap()[:, 0:2 * N].rearrange("p (t n) -> p t n", t=2), AF.Sqrt, [pn.r()], [B["t1"].r()],
          bias=1e-12)
    K.recip(g("t1"), g("t1"), [B["t1"].r()], [B["t1"].r()])
    yield
    K.tt(g("kk"), g("kk"), g("t1"), ALU.mult, [B["kk"].r(), B["t1"].r()], [B["kk"].r()])
    yield
    yield
    K.tt(g("t1"), g("aic"), bc(pc["k_a"].ap(), 2, [128, 2, N]), ALU.mult, [B["aic"].r(), pc["k_a"].r()], [B["t1"].r()])
    yield
    K.tt(g("t1"), g("t1"), bc(pc["omka"].ap(), 2, [128, 2, N]), ALU.add, [B["t1"].r(), pc["omka"].r()], [B["t1"].r()])
    yield
    K.tt(g("kp"), k, g("t1"), ALU.mult, [MX.r(), B["t1"].r()], [B["kp"].r()])
    yield
    K.tt(g("t1"), r, g("kp"), ALU.mult, [MX.r(), B["kp"].r()], [B["t1"].r()])
    yield
    K.tt(g("t1"), g("t1"), bc(pc["r_k"].ap(), 2, [128, 2, N]), ALU.mult, [B["t1"].r(), pc["r_k"].r()], [B["t1"].r()])
    yield
    for t in range(2):
        K.mm(pn.ap()[:, t * N:(t + 1) * N], blk64.ap(), B["t1"].ap()[:, t, 0:N], [blk64.r(), B["t1"].r()], [pn.r()])
    K.tt(g("bonus"), pn.ap()[:, 0:2 * N].rearrange("p (t n) -> p t n", t=2), v, ALU.mult, [pn.r(), MX.r()],
         [B["bonus"].r()])
    B['_rkv'] = (r, k, v)
    yield


def rwkv_params(K, dr, l, ph):
    pc = {}
    mu = K.sb(ph, "mu", [128, 7], F32)
    K.dma(mu.ap(), dr["rwkv_mu"][l].rearrange("(t p) -> p t", p=128), w=[mu.r()])
    pc["mu"] = mu
    for n, src in (("w0", dr["rwkv_w0"][l]), ("a0", dr["rwkv_a0"][l]), ("k_k", dr["rwkv_k_k"][l]),
                   ("k_a", dr["rwkv_k_a"][l]), ("r_k", dr["rwkv_r_k"][l].rearrange("h n -> (h n)")),
                   ("ln_g", dr["rwkv_ln_g"][l]), ("ln_b", dr["rwkv_ln_b"][l])):
        t = K.sb(ph, "pc_" + n, [128, 2], F32)
        K.dma(t.ap(), src.rearrange("(t p) -> p t", p=128), w=[t.r()])
        pc[n] = t
    omka = K.sb(ph, "omka", [128, 2], F32)
    K.ts(omka.ap(), pc["k_a"].ap(), -1.0, ALU.mult, [pc["k_a"].r()], [omka.r()], s2=1.0, op1=ALU.add)
    pc["omka"] = omka
    LW = K.sb(ph, "LW", [128, 256], BF16)
    K.dma(LW.ap()[0:32, :], dr["rwkv_w2"][l], w=[LW.r()], q="pool")
    K.dma(LW.ap()[32:64, :], dr["rwkv_a2"][l], w=[LW.r()], q="pool")
    K.dma(LW.ap()[64:128, :], dr["rwkv_g2"][l], w=[LW.r()], q="pool")
    return pc, LW


def rwkv_epilogue(K, C, pc, B, N, pT, ydst, yres):
    for t in range(2):
        K.act(B["t1"].ap()[:, t, 0:N], pT.ap()[:, t * N:(t + 1) * N], AF.Identity, [pT.r(), pc["ln_g"].r(), pc["ln_b"].r()],
              [B["t1"].r()], scale=pc["ln_g"].ap()[:, t:t + 1], bias=pc["ln_b"].ap()[:, t:t + 1])
    g = lambda n: B[n].ap()[:, :, 0:N]
    K.tt(g("t1"), g("t1"), g("bonus"), ALU.add, [B["t1"].r(), B["bonus"].r()], [B["t1"].r()])
    K.tt(ydst, g("t1"), g("gate"), ALU.mult, [B["t1"].r(), B["gate"].r()], yres)


def groupnorm64(K, o_ap, n, G, scr, res_in, out_ap, out_res):
    mean, xc, sq, var = scr
    K.red(mean.ap()[0:n, 0:G], o_ap, res_in, [mean.r()])
    K.ts(mean.ap()[0:n, 0:G], mean.ap()[0:n, 0:G], 1.0 / 64, ALU.mult, [mean.r()], [mean.r()])
    xca = xc.ap()[0:n, 0:G * 64].rearrange("p (g e) -> p g e", g=G)
    K.tt(xca, o_ap, bc(mean.ap()[0:n, 0:G], 2, [n, G, 64]), ALU.subtract, res_in + [mean.r()], [xc.r()])
    sqa = sq.ap()[0:n, 0:G * 64].rearrange("p (g e) -> p g e", g=G)
    K.tt(sqa, xca, xca, ALU.mult, [xc.r()], [sq.r()])
    K.red(var.ap()[0:n, 0:G], sqa, [sq.r()], [var.r()])
    K.act(var.ap()[0:n, 0:G], var.ap()[0:n, 0:G], AF.Sqrt, [var.r()], [var.r()], scale=1.0 / 64, bias=RWKV_GN_EPS)
    K.recip(var.ap()[0:n, 0:G], var.ap()[0:n, 0:G], [var.r()], [var.r()])
    K.tt(out_ap, xca, bc(var.ap()[0:n, 0:G], 2, [n, G, 64]), ALU.mult, [xc.r(), var.r()], out_res)


def rwkv_phase(K, dr, C, l, yT, dbg_out):
    P = K.P
    R0 = OFF["rw"]
    with contextlib.ExitStack() as ph:
        win = K.sb(ph, "win_rwkv", [128, KT, 896], BF16)
        K.dma(win.ap(), dr["w_in"][l, :, R0:R0 + 896].rearrange("(k p) n -> p k n", p=128), w=[win.r()], q="pool")
        pc, LW = rwkv_params(K, dr, l, ph)
        import os
        if os.environ.get("SKIP_RWKV_PROMPT") != "1":
            rwkv_prompt(K, dr, C, l, yT, win, pc, LW, dbg_out)
        P.barrier()
        if os.environ.get("SKIP_RWKV_SAMPLE") != "1":
            rwkv_sample(K, dr, C, l, yT, win, pc, LW, dbg_out)


def interleave(gens, ratio=None):
    gens = [g for g in gens if g is not None]
    ratio = ratio or [1] * len(gens)
    live = list(zip(gens, ratio))
    while live:
        for item in list(live):
            g, n = item
            for _ in range(n):
                try:
                    next(g)
                except StopIteration:
                    live.remove(item)
                    break


def rwkv_prompt(K, dr, C, l, yT, win, pc, LW, dbg_out):
    P = K.P
    identb, identf, maskU, maskSU, maskSL, blk64 = (C[k] for k in ["identb", "identf", "maskU", "maskSU", "maskSL", "blk64"])
    hc = C["hc"]
    N = 128
    with contextlib.ExitStack() as ph:
        f3 = lambda n: K.sb(ph, n, [128, 2, N], F32)
        Bs = []
        for i in range(2):
            B = {n: f3(f"rb{i}_" + n) for n in ["sig", "aic", "gate", "kk", "t1", "kp", "bonus", "cs", "e1", "e2", "bb"]}
            Bs.append(B)
        MX = K.sb(ph, "MX", [128, 7, N], F32)
        LI = K.sb(ph, "LI", [128, N], BF16)
        for B in Bs:
            B["MX"], B["LI"] = MX, LI
        RW = [K.sb(ph, f"RW{i}", [128, 7, N + 1], F32) for i in range(2)]
        ones_r = K.sb(ph, "ones_r", [128, N], F32)
        bcol = K.sb(ph, "bcol", [128, 2], F32)
        MK2 = K.sb(ph, "MK2", [128, 2, N], F32)
        ARs = [K.sb(ph, f"AR{i}", [128, 2, 2, N], BF16) for i in range(2)]
        BKs = [K.sb(ph, f"BK{i}", [128, 2, 2, N], BF16) for i in range(2)]
        FH = K.sb(ph, "FH", [128, 3, 2, N], BF16)
        TMs = [K.sb(ph, f"TM{i}", [128, 4, 2, N], BF16) for i in range(2)]
        t2 = f3("rb_t2")
        A1 = K.sb(ph, "A1", [128, 4, 2, N], BF16)
        A2 = K.sb(ph, "A2", [128, 4, 2, N], BF16)
        Lb = [K.sb(ph, f"Lb{i}", [128, 4, N], BF16) for i in range(2)]
        Nb = [K.sb(ph, f"Nb{i}", [128, 4, N], BF16) for i in range(2)]
        X32 = K.sb(ph, "X32", [128, 4, 2, 64], F32)
        Xb = K.sb(ph, "Xb", [128, 4, 2, 64], BF16)
        Apf = K.sb(ph, "Apf", [128, 2, N], BF16)
        XAc = K.sb(ph, "XAc", [128, 256], BF16)
        Utm = K.sb(ph, "Utm", [128, 4, 64], BF16)
        ST32 = K.sb(ph, "ST32", [128, 2, N], F32)
        STb = K.sb(ph, "STb", [128, 2, N], BF16)
        tmpS = K.sb(ph, "tmpSr", [128, 2, N], F32)
        gn = (K.sb(ph, "gn_mean", [128, 4], F32), K.sb(ph, "gn_xc", [128, 256], F32),
              K.sb(ph, "gn_sq", [128, 256], F32), K.sb(ph, "gn_var", [128, 4], F32))
        onb = K.sb(ph, "onbr", [128, 256], BF16)
        stT = K.sb(ph, "stT", [128, 2, N], F32)
        pI = K.ps(ph, "pr_I", [128, 512], F32)
        pM = K.ps(ph, "pr_M", [128, 512], F32)
        pT = K.ps(ph, "pr_T", [128, 1024], BF16)
        pT2 = pT
        pAT = K.ps(ph, "pr_AT", [128, 1024], F32)
        pL = K.ps(ph, "pr_L", [128, 512], F32)
        pL2 = K.ps(ph, "pr_L2", [128, 512], F32)
        pX = K.ps(ph, "pr_X", [128, 512], F32)
        pO = pX

        K.memset(ones_r.ap(), 1.0, [ones_r.r()])
        K.cp(MK2.ap()[:, 0, :], maskSU.ap(), [maskSU.r()], [MK2.r()])
        K.cp(MK2.ap()[:, 1, :], maskU.ap(), [maskU.r()], [MK2.r()])
        K.memset(ST32.ap(), 0.0, [ST32.r()])
        K.memset(STb.ap(), 0.0, [STb.r()])
        K.memset(RW[0].ap()[:, :, 0:1], 0.0, [RW[0].r()])

        def s1(c):
            B, AR, BK, TM = Bs[c % 2], ARs[c % 2], BKs[c % 2], TMs[c % 2]
            g = lambda n: B[n].ap()
            h = hc[c % 2]
            make_hc(K, C, l, h, c)
            yield
            rw = RW[c % 2]
            for (t0, t1) in ((0, 4), (4, 7)):
                for t in range(t0, t1):
                    for k in range(KT):
                        K.mm(pI.ap()[:, (t - t0) * N:(t - t0 + 1) * N], win.ap()[:, k, t * N:(t + 1) * N], h.ap()[:, k, :],
                             [win.r(), h.r()], [pI.r()], start=(k == 0), stop=(k == KT - 1))
                    yield
                K.cp(rw.ap()[:, t0:t1, 1:N + 1], pI.ap()[:, 0:(t1 - t0) * N].rearrange("p (t n) -> p t n", t=t1 - t0),
                     [pI.r()], [rw.r()], eng="act")
                yield
            if c + 1 < NCH:
                K.cp(RW[(c + 1) % 2].ap()[:, :, 0:1], rw.ap()[:, :, N:N + 1], [rw.r()], [RW[(c + 1) % 2].r()])
            B["_rw_res"] = [rw.r()]
            yield from rwkv_prep(K, C, pc, LW, N, rw.ap()[:, :, 1:N + 1], rw.ap()[:, :, 0:N], B, pM, pI, pI)
            r, k_, v = B["_rkv"]
            MXr = MX.r()
            for t in range(2):
                K.P.op("dve", lambda e, t=t, B=B: e.tensor_tensor_scan(out=B["cs"].ap()[:, t, :], data0=ones_r.ap(),
                                                                        data1=B["sig"].ap()[:, t, :], initial=0.0,
                                                                        op0=ALU.mult, op1=ALU.add),
                       reads=[ones_r.r(), B["sig"].r()], writes=[B["cs"].r()])
            yield
            K.act(g("e1"), g("cs"), AF.Exp, [B["cs"].r()], [B["e1"].r()], scale=-C0)
            yield
            K.act(g("e2"), g("cs"), AF.Exp, [B["cs"].r()], [B["e2"].r()], scale=C0)
            yield
            K.tt(AR.ap()[:, :, 1, :], r, g("e1"), ALU.mult, [MXr, B["e1"].r()], [AR.r()])
            yield
            K.tt(g("bb"), g("kk"), g("aic"), ALU.mult, [B["kk"].r(), B["aic"].r()], [B["bb"].r()])
            yield
            K.tt(BK.ap()[:, :, 0, :], g("bb"), g("e2"), ALU.mult, [B["bb"].r(), B["e2"].r()], [BK.r()])
            yield
            K.tt(BK.ap()[:, :, 1, :], g("kp"), g("e2"), ALU.mult, [B["kp"].r(), B["e2"].r()], [BK.r()])
            yield
            K.tt(g("t1"), g("cs"), g("sig"), ALU.subtract, [B["cs"].r(), B["sig"].r()], [B["t1"].r()])
            yield
            K.act(g("e2"), g("t1"), AF.Exp, [B["t1"].r()], [B["e2"].r()], scale=-C0)
            yield
            K.stt(AR.ap()[:, :, 0, :], g("kk"), -1.0, g("e2"), ALU.mult, ALU.mult, [B["kk"].r(), B["e2"].r()], [AR.r()])
            yield
            K.ts(bcol.ap(), B["cs"].ap()[:, :, N - 1], -C0, ALU.mult, [B["cs"].r()], [bcol.r()])
            yield
            for t in range(2):
                K.act(B["e2"].ap()[:, t, :], B["cs"].ap()[:, t, :], AF.Exp, [B["cs"].r(), bcol.r()], [B["e2"].r()],
                      scale=C0, bias=bcol.ap()[:, t:t + 1])
            yield
            K.tt(FH.ap()[:, 0], g("bb"), g("e2"), ALU.mult, [B["bb"].r(), B["e2"].r()], [FH.r()])
            yield
            K.tt(FH.ap()[:, 1], g("kp"), g("e2"), ALU.mult, [B["kp"].r(), B["e2"].r()], [FH.r()])
            yield
            K.cp(FH.ap()[:, 2], v, [MXr], [FH.r()], eng="act")
            yield
            for q in range(4):
                for t in range(2):
                    src = FH.ap()[:, q, t, :] if q < 3 else AR.ap()[:, t, 0, :]
                    K.tr(pT.ap()[:, (q * 2 + t) * N:(q * 2 + t + 1) * N], src, identb.ap(),
                         [FH.r(), AR.r(), identb.r()], [pT.r()], inc=(t == 1))
                yield
            K.cp(TM.ap().rearrange("p q t n -> p (q t n)"), pT.ap(), [pT.r()], [TM.r()], eng="act")
            yield

        def s2(c):
            B, AR, BK, TM = Bs[c % 2], ARs[c % 2], BKs[c % 2], TMs[c % 2]
            mk = bc(MK2.ap(), 1, [128, 4, 2, N])
            for which, Adst in ((0, A1), (1, A2)):
                for hd in range(4):
                    t, o = hd // 2, 64 * (hd % 2)
                    sl = slice(o, o + 64)
                    arf = AR.ap()[sl, t].rearrange("p a n -> p (a n)")
                    K.mm(pAT.ap()[:, hd * 256:(hd + 1) * 256], BK.ap()[sl, t, which, :], arf, [BK.r(), AR.r()], [pAT.r()],
                         self_wait=True)
                yield
                K.tt(Adst.ap(), pAT.ap().rearrange("p (h a n) -> p h a n", h=4, a=2), mk, ALU.mult, [pAT.r(), MK2.r()],
                     [Adst.r()])
                yield
            for hd in range(4):
                t, o = hd // 2, 64 * (hd % 2)
                sl = slice(o, o + 64)
                K.mm(pL.ap()[:, hd * N:(hd + 1) * N], AR.ap()[sl, t, 0, :], BK.ap()[sl, t, 0, :], [BK.r(), AR.r()],
                     [pL.r()], self_wait=True)
            yield
            K.tt(Lb[0].ap(), pL.ap().rearrange("p (h n) -> p h n", h=4), bc(maskSL.ap(), 1, [128, 4, N]), ALU.mult,
                 [pL.r(), maskSL.r()], [Lb[0].r()])
            yield
            vtm = TM.ap()[:, 2].rearrange("p t n -> p (t n)")
            for hd in range(4):
                K.mm(pX.ap()[:, hd * 64:(hd + 1) * 64], A2.ap()[:, hd, 0, :], vtm[:, hd * 64:(hd + 1) * 64],
                     [A2.r(), TM.r()], [pX.r()], inc=(hd == 3))
            yield
            K.cp(X32.ap()[:, :, 0, :], TM.ap()[:, 3].rearrange("p t (hh k) -> p (t hh) k", hh=2), [TM.r()], [X32.r()])
            yield
            K.cp(X32.ap()[:, :, 1, :], pX.ap()[:, 0:256].rearrange("p (h v) -> p h v", h=4), [pX.r()], [X32.r()],
                 eng="act")
            yield
            K.cp(Xb.ap(), X32.ap(), [X32.r()], [Xb.r()], eng="act")
            yield
            for i in range(7):
                if i == 0:
                    nref = lambda hd: A1.ap()[:, hd, 0, :]
                    nres = A1.r()
                    lcur = Lb[0]
                else:
                    nprev_ref, nprev_res, lprev = nref, nres, lcur
                    nnew, lnew = Nb[i % 2], Lb[i % 2]
                    for hd in range(4):
                        K.mm(pL.ap()[:, hd * N:(hd + 1) * N], lprev.ap()[:, hd, :], nprev_ref(hd), [lprev.r(), nprev_res],
                             [pL.r()], inc=(hd == 3))
                    yield
                    if i < 6:
                        for hd in range(4):
                            K.mm(pL2.ap()[:, hd * N:(hd + 1) * N], nprev_ref(hd), lprev.ap()[:, hd, :],
                                 [lprev.r(), nprev_res], [pL2.r()], inc=(hd == 3))
                        yield
                    K.cp(nnew.ap(), pL.ap().rearrange("p (h n) -> p h n", h=4), [pL.r()], [nnew.r()], eng="act")
                    yield
                    if i < 6:
                        K.cp(lnew.ap(), pL2.ap().rearrange("p (h n) -> p h n", h=4), [pL2.r()], [lnew.r()])
                        yield
                    nref = lambda hd, nnew=nnew: nnew.ap()[:, hd, :]
                    nres = nnew.r()
                    lcur = lnew
                for hd in range(4):
                    K.mm(pX.ap()[:, hd * N:(hd + 1) * N], nref(hd), Xb.ap()[:, hd].rearrange("p a k -> p (a k)"),
                         [nres, Xb.r()], [pX.r()], inc=(hd == 3))
                yield
                K.tt(X32.ap(), X32.ap(), pX.ap().rearrange("p (h a k) -> p h a k", h=4, a=2), ALU.add,
                     [X32.r(), pX.r()], [X32.r()])
                yield
                K.cp(Xb.ap(), X32.ap(), [X32.r()], [Xb.r()], eng="act")
                yield
            K.cp(XAc.ap().rearrange("p (h k) -> p h k", h=4), X32.ap()[:, :, 0, :], [X32.r()], [XAc.r()])
            yield
            for t in range(2):
                K.tr(pT2.ap()[:, t * N:(t + 1) * N], XAc.ap()[:, t * N:(t + 1) * N], identb.ap(), [XAc.r(), identb.r()],
                     [pT2.r()], inc=(t == 1))
            yield
            K.cp(Apf.ap(), pT2.ap()[:, 0:2 * N].rearrange("p (t n) -> p t n", t=2), [pT2.r()], [Apf.r()], eng="act")
            yield
            for t in range(2):
                K.mm(pX.ap()[:, t * N:(t + 1) * N], Apf.ap()[:, t, :], STb.ap()[:, t, :], [Apf.r(), STb.r()], [pX.r()],
                     inc=(t == 1))
            yield
            K.tt(Utm.ap(), pX.ap()[:, 0:256].rearrange("p (h v) -> p h v", h=4), X32.ap()[:, :, 1, :], ALU.add,
                 [pX.r(), X32.r()], [Utm.r()])
            yield
            for t in range(2):
                K.mm(pO.ap()[:, t * N:(t + 1) * N], AR.ap()[:, t, 1, :], STb.ap()[:, t, :], [AR.r(), STb.r()], [pO.r()],
                     start=(t == 0), stop=False, sgc=True)
            for hd in range(4):
                K.mm(pO.ap()[:, hd * 64:(hd + 1) * 64], A1.ap()[:, hd, 1, :], Utm.ap()[:, hd, :], [A1.r(), Utm.r()],
                     [pO.r()], start=False, stop=False, sgc=True)
                K.mm(pO.ap()[:, hd * 64:(hd + 1) * 64], A2.ap()[:, hd, 1, :], vtm[:, hd * 64:(hd + 1) * 64],
                     [A2.r(), TM.r()], [pO.r()], start=False, stop=False, sgc=True)
            for t in range(2):
                K.mm(pO.ap()[:, 256 + t * N:256 + (t + 1) * N], TM.ap()[:, 0, t, :],
                     Utm.ap()[:, 2 * t:2 * t + 2, :].rearrange("p h v -> p (h v)"), [TM.r(), Utm.r()], [pO.r()],
                     start=False, stop=False, sgc=True)
                K.mm(pO.ap()[:, 256 + t * N:256 + (t + 1) * N], TM.ap()[:, 1, t, :], vtm[:, t * N:(t + 1) * N],
                     [TM.r()], [pO.r()], start=False, stop=(t == 1), inc=(t == 1), sgc=True)
            yield
            K.tt(tmpS.ap(), pO.ap()[:, 256:512].rearrange("p (t n) -> p t n", t=2), bc(blk64.ap(), 1, [128, 2, N]),
                 ALU.mult, [pO.r(), blk64.r()], [tmpS.r()])
            yield
            K.tt(ST32.ap(), ST32.ap(), bc(B["e1"].ap()[:, :, N - 1], 2, [128, 2, N]), ALU.mult, [ST32.r(), B["e1"].r()],
                 [ST32.r()])
            yield
            K.tt(ST32.ap(), ST32.ap(), tmpS.ap(), ALU.add, [ST32.r(), tmpS.r()], [ST32.r()])
            yield
            K.cp(STb.ap(), ST32.ap(), [ST32.r()], [STb.r()], eng="act")
            yield
            groupnorm64(K, pO.ap()[:, 0:256].rearrange("p (h v) -> p h v", h=4), 128, 4, gn, [pO.r()],
                        onb.ap().rearrange("p (h v) -> p h v", h=4), [onb.r()])
            yield
            for t in range(2):
                K.tr(pT2.ap()[:, t * N:(t + 1) * N], onb.ap()[:, t * N:(t + 1) * N], identb.ap(), [onb.r(), identb.r()],
                     [pT2.r()], inc=(t == 1))
            yield
            B2 = dict(B)
            B2["t1"] = t2
            rwkv_epilogue(K, C, pc, B2, N, pT2, yT.ap()[:, 4:6, c * N:(c + 1) * N], xr(yT, c // 4, range(4, 6)))
            yield

        for _ in s1(0):
            pass
        for c in range(NCH):
            interleave([s2(c), s1(c + 1) if c + 1 < NCH else None], ratio=[3, 2])
        rw = RW[(NCH - 1) % 2]
        K.dma(dr["p_shift"][l].rearrange("(t p) -> p t", p=128), rw.ap()[:, :, N], r=[rw.r()])
        for t in range(2):
            K.tr(pL.ap()[:, t * N:(t + 1) * N], ST32.ap()[:, t, :], identf.ap(), [ST32.r(), identf.r()], [pL.r()],
                 inc=(t == 1))
        K.cp(stT.ap(), pL.ap()[:, 0:2 * N].rearrange("p (t n) -> p t n", t=2), [pL.r()], [stT.r()])
        for hd in range(4):
            t, o = hd // 2, 64 * (hd % 2)
            K.dma(dr["p_rwkv"][l, hd], stT.ap()[o:o + 64, t, o:o + 64], r=[stT.r()])
        P.barrier()


def rwkv_sample(K, dr, C, l, yT, win, pc, LW, dbg_out):
    P = K.P
    hs, identb, identf = C["hs"], C["identb"], C["identf"]
    N = NS
    with contextlib.ExitStack() as ph:
        f3 = lambda n: K.sb(ph, n, [128, 2, N], F32)
        B = {n: f3("rs_" + n) for n in ["sig", "aic", "gate", "kk", "t1", "kp", "bonus", "e1", "e2", "bb"]}
        B["MX"] = K.sb(ph, "MXs", [128, 7, N], F32)
        B["LI"] = K.sb(ph, "LIs", [128, N], BF16)
        rws = K.sb(ph, "rws", [128, 7, N], F32)
        prevs = K.sb(ph, "prevs", [128, 7, N], F32)
        shs = K.sb(ph, "shs", [NS, 896], F32)
        rwtm = K.sb(ph, "rwtm", [NS, 896], F32)
        pkT = K.sb(ph, "pkT", [NS, 6, 256], F32)
        pkh = K.sb(ph, "pkhr", [64, 6, 64], F32)
        S = K.sb(ph, "Sr", [64, 64, 64], F32)
        tmp = K.sb(ph, "tmpr", [64, 64, 64], F32)
        sa = K.sb(ph, "sa", [64, 64], F32)
        o = K.sb(ph, "orr", [64, 64], F32)
        on = K.sb(ph, "onr", [64, 64], F32)
        gn = (K.sb(ph, "gns_mean", [64, 1], F32), K.sb(ph, "gns_xc", [64, 64], F32),
              K.sb(ph, "gns_sq", [64, 64], F32), K.sb(ph, "gns_var", [64, 1], F32))
        otm = K.sb(ph, "otmr", [NS, 256], F32)
        otb = K.sb(ph, "otbr", [NS, 256], BF16)
        pA = K.ps(ph, "prs_A", [128, 1024], F32)
        pB = K.ps(ph, "prs_B", [128, 1024], F32)
        pL = K.ps(ph, "prs_L", [128, 512], F32)
        pM = K.ps(ph, "prs_M", [128, 512], F32)
        pT = K.ps(ph, "prs_T", [128, 1024], BF16)
        scr = dram_scratch(K, "rpk", [NS, 4, 6, 64])
        so = dram_scratch(K, "ro", [NS, 256])

        K.dma(shs.ap(), dr["st_shift"][l], w=[shs.r()])
        K.dma(S.ap().rearrange("p v k -> p (v k)"), dr["st_rwkv"][l].rearrange("b h v k -> (b h) (v k)"), w=[S.r()])
        for t in range(7):
            for k in range(KT):
                K.mm(pM.ap()[:, t * N:(t + 1) * N], win.ap()[:, k, t * 128:(t + 1) * 128], hs.ap()[:, k, :],
                     [win.r(), hs.r()], [pM.r()], start=(k == 0), stop=(k == KT - 1), inc=(k == KT - 1 and t == 6))
        K.cp(rws.ap(), pM.ap()[:, 0:7 * N].rearrange("p (t n) -> p t n", t=7), [pM.r()], [rws.r()], eng="act")
        for (c0, c1) in ((0, 512), (512, 896)):
            for k in range(KT):
                K.mm(pA.ap()[0:NS, c0:c1], hs.ap()[:, k, :], win.ap()[:, k, c0:c1], [win.r(), hs.r()], [pA.r()],
                     start=(k == 0), stop=(k == KT - 1))
        K.cp(rwtm.ap(), pA.ap()[0:NS, 0:896], [pA.r()], [rwtm.r()], eng="act")
        K.dma(dr["s_shift"][l], rwtm.ap(), r=[rwtm.r()])
        for t in range(7):
            K.tr(pL.ap()[:, t * N:(t + 1) * N], shs.ap()[:, t * 128:(t + 1) * 128], identf.ap()[0:NS, 0:NS],
                 [shs.r(), identf.r()], [pL.r()], inc=(t == 6))
        K.cp(prevs.ap(), pL.ap()[:, 0:7 * N].rearrange("p (t n) -> p t n", t=7), [pL.r()], [prevs.r()])
        B["_rw_res"] = [rws.r(), prevs.r()]
        for _ in rwkv_prep(K, C, pc, LW, N, rws.ap(), prevs.ap(), B, pB, pL, pM):
            pass
        r, k_, v = B["_rkv"]
        g = lambda n: B[n].ap()
        K.act(g("e1"), g("sig"), AF.Exp, [B["sig"].r()], [B["e1"].r()], scale=-C0)
        K.ts(g("e2"), g("kk"), -1.0, ALU.mult, [B["kk"].r()], [B["e2"].r()])
        K.tt(g("bb"), g("kk"), g("aic"), ALU.mult, [B["kk"].r(), B["aic"].r()], [B["bb"].r()])
        srcs = [(r, B["MX"].r()), (g("e1"), B["e1"].r()), (g("kp"), B["kp"].r()), (v, B["MX"].r()),
                (g("e2"), B["e2"].r()), (g("bb"), B["bb"].r())]
        for q, (ap, res) in enumerate(srcs):
            pp = pA if q < 4 else pB
            for t in range(2):
                col = ((q % 4) * 2 + t) * 128
                K.tr(pp.ap()[0:NS, col:col + 128], ap[:, t, :], identf.ap(), [res, identf.r()], [pp.r()])
        K.cp(pkT.ap()[:, 0:4, :], pA.ap()[0:NS, :].rearrange("p (q n) -> p q n", q=4), [pA.r()], [pkT.r()], eng="act")
        K.cp(pkT.ap()[:, 4:6, :], pB.ap()[0:NS, 0:512].rearrange("p (q n) -> p q n", q=2), [pB.r()], [pkT.r()])
        for q in range(6):
            K.dma(scr.ap()[:, :, q, :], pkT.ap()[:, q, :].rearrange("p (h k) -> p h k", h=4), r=[pkT.r()], w=[scr.r()])
        K.dma(pkh.ap(), scr.ap().rearrange("b h q k -> (b h) q k"), r=[scr.r()], w=[pkh.r()])
        rq, wq, kq, vq, aq, bq = (pkh.ap()[:, i, :] for i in range(6))
        K.tt(tmp.ap(), S.ap(), bc(aq, 1, [64, 64, 64]), ALU.mult, [S.r(), pkh.r()], [tmp.r()])
        K.red(sa.ap(), tmp.ap(), [tmp.r()], [sa.r()])
        K.tt(S.ap(), S.ap(), bc(wq, 1, [64, 64, 64]), ALU.mult, [S.r(), pkh.r()], [S.r()])
        K.tt(tmp.ap(), bc(sa.ap(), 2, [64, 64, 64]), bc(bq, 1, [64, 64, 64]), ALU.mult, [sa.r(), pkh.r()], [tmp.r()])
        K.tt(S.ap(), S.ap(), tmp.ap(), ALU.add, [S.r(), tmp.r()], [S.r()])
        K.tt(tmp.ap(), bc(vq, 2, [64, 64, 64]), bc(kq, 1, [64, 64, 64]), ALU.mult, [pkh.r()], [tmp.r()])
        K.tt(S.ap(), S.ap(), tmp.ap(), ALU.add, [S.r(), tmp.r()], [S.r()])
        K.dma(dr["s_rwkv"][l].rearrange("b h v k -> (b h) (v k)"), S.ap().rearrange("p v k -> p (v k)"), r=[S.r()])
        K.tt(tmp.ap(), S.ap(), bc(rq, 1, [64, 64, 64]), ALU.mult, [S.r(), pkh.r()], [tmp.r()])
        K.red(o.ap(), tmp.ap(), [tmp.r()], [o.r()])
        groupnorm64(K, o.ap().rearrange("p (g e) -> p g e", g=1), 64, 1, gn, [o.r()],
                    on.ap().rearrange("p (g e) -> p g e", g=1), [on.r()])
        K.dma(so.ap().rearrange("b (h v) -> (b h) v", h=4), on.ap(), r=[on.r()], w=[so.r()])
        K.dma(otm.ap(), so.ap(), r=[so.r()], w=[otm.r()])
        K.cp(otb.ap(), otm.ap(), [otm.r()], [otb.r()])
        for t in range(2):
            K.tr(pT.ap()[:, t * N:(t + 1) * N], otb.ap()[:, t * 128:(t + 1) * 128], identb.ap()[0:NS, 0:NS],
                 [otb.r(), identb.r()], [pT.r()], inc=(t == 1))
        rwkv_epilogue(K, C, pc, B, N, pT, yT.ap()[:, 4:6, T:T + NS], xr(yT, 4, range(4, 6)))
        P.barrier()
```

```python
import contextlib
import numpy as np
import concourse.bass as bass
import concourse.mybir as mybir
from concourse.bass_utils import run_bass_kernel_spmd

F32 = mybir.dt.float32
BF16 = mybir.dt.bfloat16
AF = mybir.ActivationFunctionType
ALU = mybir.AluOpType
AX = mybir.AxisListType

NCORES = 8
D = 1024
KT = 8
T = 2048
NS = 16
NT = T + NS
NCH = T // 128
DEPTH = 2
IN_DIM = 2968
OFF = dict(z=0, xbc=512, dt=1280, rw=1288, gq=2184, gk=2312, gv=2440, glo=2696, gg=2712)
F_DENSE = 2816
ALPHA = (2.0 * DEPTH) ** 0.25
LN_EPS = 1e-5
RMS_EPS = 1e-6
RWKV_GN_EPS = 64 * 1e-5
BLOCKS = [(0, 512), (512, 512), (1024, 512), (1536, 512), (2048, 16)]

ENGS = ["pe", "dve", "act", "pool", "sp"]


class Res:
    __slots__ = ("name", "w", "r", "excl")

    def __init__(self, name="", excl=False):
        self.name = name
        self.w = None
        self.r = []
        self.excl = excl


class Prog:
    NDMA = 8

    def __init__(self, nc):
        self.nc = nc
        self.q = {e: [] for e in ENGS}
        self.cnt = {e: 0 for e in ENGS}
        self.seen = {e: {} for e in ENGS}
        self.dma_i = {e: 0 for e in ENGS}
        self.dma_last = {}
        self.sems = {}

    def sem(self, key):
        if key not in self.sems:
            self.sems[key] = self.nc.alloc_semaphore(name="s_" + "_".join(str(k) for k in key))
        return self.sems[key]

    def _collect(self, eng, reads, writes):
        waits = {}

        def add(tok):
            if tok is None:
                return
            key, val = tok
            if self.seen[eng].get(key, 0) >= val:
                return
            if waits.get(key, 0) < val:
                waits[key] = val

        for r in reads:
            add(r.w)
        for w in writes:
            add(w.w)
            for t in w.r:
                add(t)
        if eng == "pe":
            waits.pop(("e", "pe"), None)
        for k, v in waits.items():
            self.seen[eng][k] = v
        return list(waits.items())

    def _commit(self, tok, reads, writes):
        for r in reads:
            r.r.append(tok)
            if len(r.r) > 64:
                r.r = _prune(r.r)
        for w in writes:
            w.w = tok
            w.r = []

    def op(self, eng, fn, reads=(), writes=(), inc=True, self_wait=False):
        assert inc or eng == "pe"
        if any(r.excl for r in reads):
            writes = list(writes) + [r for r in reads if r.excl]
            reads = [r for r in reads if not r.excl]
        waits = self._collect(eng, reads, writes)
        if self_wait and self.cnt[eng] > 0:
            waits.append((("e", eng), self.cnt[eng]))
        tok = (("e", eng), self.cnt[eng] + 1)
        if inc:
            self.cnt[eng] += 1
        self._commit(tok, reads, writes)

        def emit(e, fn=fn, waits=waits, inc=inc, eng=eng):
            for k, v in waits:
                e.wait_ge(self.sem(k), v)
            ins = fn(e)
            if inc:
                ins.then_inc(self.sem(("e", eng)), 1)
        self.q[eng].append(emit)
        return tok

    def dma(self, queue, out, in_, reads=(), writes=(), **kw):
        i = self.dma_i[queue]
        self.dma_i[queue] += 1
        slot = i % self.NDMA
        key = ("d", queue, slot)
        val = 16 * (i // self.NDMA + 1)
        waits = self._collect(queue, reads, writes)
        prev = val - 16
        if prev > 0 and self.seen[queue].get(key, 0) < prev:
            self.seen[queue][key] = prev
            waits.append((key, prev))
        tok = (key, val)
        self.dma_last[key] = val
        self._commit(tok, reads, writes)

        def emit(e, waits=waits, key=key):
            for k, v in waits:
                e.wait_ge(self.sem(k), v)
            e.dma_start(out=out, in_=in_, **kw).then_inc(self.sem(key), 16)
        self.q[queue].append(emit)
        return tok

    def barrier(self, engines=ENGS):
        toks = [(("e", e), self.cnt[e]) for e in ENGS if self.cnt[e] > 0]
        toks += list(self.dma_last.items())
        for eng in engines:
            waits = []
            for k, v in toks:
                if k == ("e", eng) and eng == "pe":
                    continue
                if self.seen[eng].get(k, 0) < v:
                    self.seen[eng][k] = v
                    waits.append((k, v))

            def emit(e, waits=waits):
                for k, v in waits:
                    e.wait_ge(self.sem(k), v)
            self.q[eng].append(emit)

    def emit(self):
        with self.nc.Block() as block:
            @block.tensor
            def _(e):
                for f in self.q["pe"]:
                    f(e)

            @block.vector
            def _(e):
                for f in self.q["dve"]:
                    f(e)

            @block.scalar
            def _(e):
                for f in self.q["act"]:
                    f(e)

            @block.gpsimd
            def _(e):
                for f in self.q["pool"]:
                    f(e)

            @block.sync
            def _(e):
                for f in self.q["sp"]:
                    f(e)


def _prune(toks):
    best = {}
    for k, v in toks:
        if best.get(k, 0) < v:
            best[k] = v
    return list(best.items())


class Tn:
    def __init__(self, h, name, excl=False):
        self.h = h
        self.name = name
        self._res = {}
        self.excl = excl

    def ap(self):
        return self.h.ap()

    def r(self, key=0):
        if key not in self._res:
            self._res[key] = Res(f"{self.name}:{key}", self.excl)
        return self._res[key]


class KB:
    def __init__(self, nc):
        self.nc = nc
        self.P = Prog(nc)
        self.uid = 0

    def sb(self, stack, name, shape, dt=F32):
        self.uid += 1
        h = stack.enter_context(self.nc.sbuf_tensor(f"{name}_{self.uid}", list(shape), dt))
        return Tn(h, name)

    def ps(self, stack, name, shape, dt=F32):
        self.uid += 1
        h = stack.enter_context(self.nc.psum_tensor(f"{name}_{self.uid}", list(shape), dt))
        return Tn(h, name, excl=True)

    def mm(self, out, lhsT, rhs, r, w, start=True, stop=True, inc=None, self_wait=False, sgc=False):
        inc = stop if inc is None else inc
        kw = {"skip_group_check": True} if sgc else {}
        self.P.op("pe", lambda e: e.matmul(out, lhsT=lhsT, rhs=rhs, start=start, stop=stop, **kw),
                  reads=r, writes=w, inc=inc, self_wait=self_wait)

    def tr(self, out, in_, ident, r, w, inc=True):
        self.P.op("pe", lambda e: e.transpose(out, in_, ident), reads=r, writes=w, inc=inc)

    def act(self, out, in_, func, r, w, scale=None, bias=None, accum_out=None):
        kw = {}
        if scale is not None:
            kw["scale"] = scale
        if bias is not None:
            kw["bias"] = bias
        if accum_out is not None:
            kw["accum_out"] = accum_out
        self.P.op("act", lambda e: e.activation(out=out, in_=in_, func=func, **kw), reads=r, writes=w)

    def tt(self, out, in0, in1, op, r, w, eng="dve"):
        self.P.op(eng, lambda e: e.tensor_tensor(out=out, in0=in0, in1=in1, op=op), reads=r, writes=w)

    def ts(self, out, in0, s1, op0, r, w, s2=None, op1=None, eng="dve", accum_out=None):
        kw = {}
        if op1 is not None:
            kw["op1"] = op1
        if accum_out is not None:
            kw["accum_out"] = accum_out
        self.P.op(eng, lambda e: e.tensor_scalar(out=out, in0=in0, scalar1=s1, scalar2=s2, op0=op0, **kw),
                  reads=r, writes=w)

    def stt(self, out, in0, scalar, in1, op0, op1, r, w, eng="dve"):
        self.P.op(eng, lambda e: e.scalar_tensor_tensor(out=out, in0=in0, scalar=scalar, in1=in1, op0=op0, op1=op1),
                  reads=r, writes=w)

    def cp(self, out, in_, r, w, eng="dve"):
        if eng == "act":
            self.P.op("act", lambda e: e.activation(out=out, in_=in_, func=AF.Copy), reads=r, writes=w)
        else:
            self.P.op(eng, lambda e: e.tensor_copy(out=out, in_=in_), reads=r, writes=w)

    def recip(self, out, in_, r, w):
        self.P.op("dve", lambda e: e.reciprocal(out=out, in_=in_), reads=r, writes=w)

    def red(self, out, in_, r, w, op=ALU.add, axis=AX.X, eng="dve"):
        self.P.op(eng, lambda e: e.tensor_reduce(out=out, in_=in_, axis=axis, op=op), reads=r, writes=w)

    def memset(self, ap, val, w, eng="dve"):
        self.P.op(eng, lambda e: e.memset(ap, val), writes=w)

    def dma(self, out, in_, r=(), w=(), q="sp", **kw):
        return self.P.dma(q, out, in_, reads=r, writes=w, **kw)


W_SHAPES = dict(
    w_ada=[DEPTH, D, 6 * D], b_ada=[DEPTH, 6 * D], w_in=[DEPTH, D, IN_DIM], w_out=[DEPTH, D, D],
    ssd_conv_w=[DEPTH, 4, 768], ssd_conv_b=[DEPTH, 768], ssd_dt_bias=[DEPTH, 8], ssd_a_log=[DEPTH, 8],
    ssd_d=[DEPTH, 8], ssd_norm_g=[DEPTH, 512], rwkv_mu=[DEPTH, 896], rwkv_w0=[DEPTH, 256],
    rwkv_w2=[DEPTH, 32, 256], rwkv_a0=[DEPTH, 256], rwkv_a2=[DEPTH, 32, 256], rwkv_g2=[DEPTH, 64, 256],
    rwkv_k_k=[DEPTH, 256], rwkv_k_a=[DEPTH, 256], rwkv_r_k=[DEPTH, 4, 64], rwkv_ln_g=[DEPTH, 256],
    rwkv_ln_b=[DEPTH, 256], gla_w_gk2=[DEPTH, 16, 128], gla_b_gk=[DEPTH, 128], gla_norm_g=[DEPTH, 64],
    ln_mix_g=[DEPTH, D], ln_mix_b=[DEPTH, D], ln_ffn_g=[DEPTH, D], ln_ffn_b=[DEPTH, D],
    ffn_w_gate=[1, D, F_DENSE], ffn_w_up=[1, D, F_DENSE], ffn_w_down=[1, F_DENSE, D],
    moe_router=[1, D, 8], moe_w_gate=[1, 8, D, D], moe_w_up=[1, 8, D, D], moe_w_down=[1, 8, D, D],
)
IN_SHAPES = dict(
    xp=[T, D], xs=[NS, D], cc=[1 + NS, D],
    st_ssd=[DEPTH, NS, 8, 64, 64], st_conv=[DEPTH, NS, 3, 768], st_rwkv=[DEPTH, NS, 4, 64, 64],
    st_shift=[DEPTH, NS, 896], st_gla=[DEPTH, NS, 4, 32, 64],
)
OUT_SHAPES = dict(
    y_p=[T, D], y_s=[NS, D],
    p_ssd=[DEPTH, 8, 64, 64], p_conv=[DEPTH, 3, 768], p_rwkv=[DEPTH, 4, 64, 64], p_shift=[DEPTH, 896],
    p_gla=[DEPTH, 4, 32, 64],
    s_ssd=[DEPTH, NS, 8, 64, 64], s_conv=[DEPTH, NS, 3, 768], s_rwkv=[DEPTH, NS, 4, 64, 64],
    s_shift=[DEPTH, NS, 896], s_gla=[DEPTH, NS, 4, 32, 64],
)

def xr(t, b, tiles=range(KT)):
    return [t.r((d, b)) for d in tiles]


SH1, SC1, GT1, SH2, SC2, GT2 = 0, 8, 16, 24, 32, 40


def build(stub_mixer=False, dbg=None, n_layers=DEPTH):
    nc = bass.Bass("TRN2", target_bir_lowering=False)
    K = KB(nc)
    P = K.P
    dr = {}
    for n, s in IN_SHAPES.items():
        dr[n] = nc.dram_tensor(n, s, F32, kind="ExternalInput").ap()
    for n, s in W_SHAPES.items():
        dr[n] = nc.dram_tensor(n, s, F32, kind="ExternalInput").ap()
    for n, s in OUT_SHAPES.items():
        dr[n] = nc.dram_tensor(n, s, F32, kind="ExternalOutput").ap()
    dbg_out = {}
    if dbg:
        for n, s in dbg.items():
            dbg_out[n] = nc.dram_tensor("dbg_" + n, s, F32, kind="ExternalOutput").ap()
    out_res = Res("outputs")

    with contextlib.ExitStack() as perm, nc.allow_non_contiguous_dma(reason="small param loads"):
        xT = K.sb(perm, "xT", [128, KT, NT], F32)
        modT = [K.sb(perm, f"modT{l}", [128, 48, 1 + NS], F32) for l in range(DEPTH)]
        identf = K.sb(perm, "identf", [128, 128], F32)
        identb = K.sb(perm, "identb", [128, 128], BF16)
        onesM = K.sb(perm, "onesM", [128, 128], F32)
        ones1 = K.sb(perm, "ones1", [128, 128], F32)
        maskU = K.sb(perm, "maskU", [128, 128], F32)
        maskSU = K.sb(perm, "maskSU", [128, 128], F32)
        maskSL = K.sb(perm, "maskSL", [128, 128], F32)
        blk64 = K.sb(perm, "blk64", [128, 128], F32)
        C = dict(xT=xT, modT=modT, identf=identf, identb=identb, onesM=onesM, ones1=ones1,
                 maskU=maskU, maskSU=maskSU, maskSL=maskSL, blk64=blk64)

        def sel(t, val_keep, cmp, fill, base=0, cm=1, pat=None, ap=None):
            ap = t.ap() if ap is None else ap
            pat = [[-1, ap.shape[-1]]] if pat is None else pat
            P.op("pool", lambda e: e.affine_select(out=ap, in_=ap, pattern=pat, compare_op=cmp, fill=fill,
                                                   base=base, channel_multiplier=cm),
                 reads=[t.r()], writes=[t.r()])

        K.memset(identf.ap(), 0.0, [identf.r()], eng="pool")
        sel(identf, 0, ALU.not_equal, 1.0)
        K.cp(identb.ap(), identf.ap(), [identf.r()], [identb.r()], eng="pool")
        K.memset(onesM.ap(), 1.0 / D, [onesM.r()], eng="pool")
        K.memset(ones1.ap(), 1.0, [ones1.r()], eng="pool")
        K.memset(maskU.ap(), 1.0, [maskU.r()], eng="pool")
        sel(maskU, 1, ALU.is_ge, 0.0, cm=-1, pat=[[1, 128]])
        K.memset(maskSU.ap(), 1.0, [maskSU.r()], eng="pool")
        sel(maskSU, 1, ALU.is_gt, 0.0, cm=-1, pat=[[1, 128]])
        K.memset(maskSL.ap(), 1.0, [maskSL.r()], eng="pool")
        sel(maskSL, 1, ALU.is_gt, 0.0)
        K.memset(blk64.ap(), 0.0, [blk64.r()], eng="pool")
        K.memset(blk64.ap()[0:64, 0:64], 1.0, [blk64.r()], eng="pool")
        K.memset(blk64.ap()[64:128, 64:128], 1.0, [blk64.r()], eng="pool")

        with contextlib.ExitStack() as ph:
            stage = [K.sb(ph, f"stg{i}", [128, D], F32) for i in range(2)]
            ctm = K.sb(ph, "ctm", [1 + NS, D], F32)
            scT = K.sb(ph, "scT", [128, KT, 1 + NS], BF16)
            wada = [K.sb(ph, f"wada{i}", [128, KT, 512], BF16) for i in range(2)]
            bB = [K.sb(ph, f"bB{i}", [1 + NS, 512], F32) for i in range(2)]
            modsb = [K.sb(ph, f"modsb{i}", [1 + NS, 512], F32) for i in range(2)]
            pst = [K.ps(ph, f"pst{i}", [128, 1024], F32) for i in range(2)]
            psm = [K.ps(ph, f"psm{i}", [128, 512], F32) for i in range(2)]
            pss = K.ps(ph, "pss", [128, 512], F32)
            for tt in range(NCH + 1):
                st = stage[tt % 2]
                pt = pst[tt % 2]
                n = 128 if tt < NCH else NS
                src = dr["xp"][tt * 128:(tt + 1) * 128, :] if tt < NCH else dr["xs"]
                K.dma(st.ap()[0:n, :], src, w=[st.r()])
                for d in range(KT):
                    K.tr(pt.ap()[:, d * n:(d + 1) * n], st.ap()[0:n, d * 128:(d + 1) * 128],
                         identf.ap()[0:n, 0:n], [st.r(), identf.r()], [pt.r()], inc=(d == KT - 1))
                dst = xT.ap()[:, :, tt * 128:tt * 128 + n]
                srcp = pt.ap()[:, 0:KT * n].rearrange("p (d n) -> p d n", d=KT)
                K.cp(dst, srcp, [pt.r()], xr(xT, min(tt // 4, 4)), eng=("act" if tt % 2 == 0 else "dve"))
            K.dma(ctm.ap(), dr["cc"], w=[ctm.r()])
            for d in range(KT):
                K.tr(pss.ap()[:, d * 17:(d + 1) * 17], ctm.ap()[:, d * 128:(d + 1) * 128],
                     identf.ap()[0:17, 0:17], [ctm.r(), identf.r()], [pss.r()], inc=(d == KT - 1))
            K.act(scT.ap(), pss.ap()[:, 0:KT * 17].rearrange("p (d n) -> p d n", d=KT), AF.Silu,
                  [pss.r()], [scT.r()])
            it = 0
            for l in range(DEPTH):
                for j in range(12):
                    wb = wada[it % 2]
                    bb = bB[it % 2]
                    ms = modsb[it % 2]
                    pm = psm[it % 2]
                    K.dma(wb.ap(), dr["w_ada"][l, :, j * 512:(j + 1) * 512].rearrange("(k p) n -> p k n", p=128),
                          w=[wb.r()], q="pool")
                    K.dma(bb.ap(), dr["b_ada"][l:l + 1, j * 512:(j + 1) * 512].to_broadcast([1 + NS, 512]),
                          w=[bb.r()])
                    for k in range(KT):
                        K.mm(pm.ap()[0:17, :], scT.ap()[:, k, :], wb.ap()[:, k, :], [scT.r(), wb.r()], [pm.r()],
                             start=(k == 0), stop=(k == KT - 1))
                    K.tt(ms.ap(), pm.ap()[0:17, :], bb.ap(), ALU.add, [pm.r(), bb.r()], [ms.r()])
                    for q4 in range(4):
                        K.tr(pss.ap()[:, q4 * 17:(q4 + 1) * 17], ms.ap()[:, q4 * 128:(q4 + 1) * 128],
                             identf.ap()[0:17, 0:17], [ms.r(), identf.r()], [pss.r()], inc=(q4 == 3))
                    K.cp(modT[l].ap()[:, j * 4:(j + 1) * 4, :],
                         pss.ap()[:, 0:4 * 17].rearrange("p (d n) -> p d n", d=4), [pss.r()], [modT[l].r()],
                         eng="act")
                    it += 1
                for seg in (SC1, GT1, SC2, GT2):
                    K.ts(modT[l].ap()[:, seg:seg + 8, :], modT[l].ap()[:, seg:seg + 8, :], 1.0, ALU.add,
                         [modT[l].r()], [modT[l].r()])
            P.barrier()

        for l in range(n_layers):
            layer(K, dr, C, l, stub_mixer, dbg_out)

        with contextlib.ExitStack() as ph:
            stage = [K.sb(ph, f"ostg{i}", [128, D], F32) for i in range(2)]
            pst = [K.ps(ph, f"opst{i}", [128, 1024], F32) for i in range(2)]
            for tt in range(NCH + 1):
                st = stage[tt % 2]
                pt = pst[tt % 2]
                n = 128 if tt < NCH else NS
                for d in range(KT):
                    K.tr(pt.ap()[0:n, d * 128:(d + 1) * 128], xT.ap()[:, d, tt * 128:tt * 128 + n],
                         identf.ap(), [xT.r((d, min(tt // 4, 4))), identf.r()], [pt.r()], inc=(d == KT - 1))
                K.cp(st.ap()[0:n, :], pt.ap()[0:n, :], [pt.r()], [st.r()], eng=("act" if tt % 2 == 0 else "dve"))
                dst = dr["y_p"][tt * 128:(tt + 1) * 128, :] if tt < NCH else dr["y_s"]
                K.dma(dst, st.ap()[0:n, :], r=[st.r()])
            P.barrier()
    with nc.allow_non_contiguous_dma(reason="small param loads"):
        P.emit()
    return nc


def modulate(K, C, l, src, dst, b, sh, sc, dres):
    mod = C["modT"][l]
    c0, n = BLOCKS[b]
    if b < 4:
        for d in range(KT):
            K.act(dst.ap()[:, d, c0:c0 + n], src.ap()[:, d, c0:c0 + n], AF.Identity,
                  [src.r((d, b)), mod.r()], [dres(d)],
                  scale=mod.ap()[:, sc + d, 0:1], bias=mod.ap()[:, sh + d, 0:1])
    else:
        tmp = C["tmp_s"]
        K.tt(tmp.ap(), src.ap()[:, :, c0:c0 + n], mod.ap()[:, sc:sc + 8, 1:1 + NS], ALU.mult,
             xr(src, b) + [mod.r()], [tmp.r()])
        K.tt(dst.ap()[:, :, c0:c0 + n], tmp.ap(), mod.ap()[:, sh:sh + 8, 1:1 + NS], ALU.add,
             [tmp.r(), mod.r()], [dres(d) for d in range(KT)])


def layernorm(K, C, l, gi, psA, psB, scr):
    xT, onesM, lncol = C["xT"], C["onesM"], C["lncol"]
    sq, mean_sb, var, tt_ = scr["sq"], scr["mean"], scr["var"], scr["t"]
    for b, (c0, n) in enumerate(BLOCKS):
        for d in range(KT):
            s = sq[d % 2]
            xs = xT.ap()[:, d, c0:c0 + n]
            K.act(s.ap()[:, :n], xs, AF.Square, [xT.r((d, b))], [s.r()])
            K.mm(psA.ap()[:, :n], onesM.ap(), xs, [onesM.r(), xT.r((d, b))], [psA.r()],
                 start=(d == 0), stop=(d == KT - 1), inc=True)
            K.mm(psB.ap()[:, :n], onesM.ap(), s.ap()[:, :n], [onesM.r(), s.r()], [psB.r()],
                 start=(d == 0), stop=(d == KT - 1), inc=True)
        K.cp(mean_sb.ap()[:, :n], psA.ap()[:, :n], [psA.r()], [mean_sb.r()], eng="act")
        K.tt(var.ap()[:, :n], mean_sb.ap()[:, :n], mean_sb.ap()[:, :n], ALU.mult, [mean_sb.r()], [var.r()])
        K.tt(var.ap()[:, :n], psB.ap()[:, :n], var.ap()[:, :n], ALU.subtract, [psB.r(), var.r()], [var.r()])
        K.act(var.ap()[:, :n], var.ap()[:, :n], AF.Sqrt, [var.r()], [var.r()], bias=LN_EPS)
        K.recip(var.ap()[:, :n], var.ap()[:, :n], [var.r()], [var.r()])
        for d in range(KT):
            t = tt_[d % 2]
            xs = xT.ap()[:, d, c0:c0 + n]
            K.tt(t.ap()[:, :n], xs, mean_sb.ap()[:, :n], ALU.subtract, [xT.r((d, b)), mean_sb.r()], [t.r()])
            K.tt(t.ap()[:, :n], t.ap()[:, :n], var.ap()[:, :n], ALU.mult, [t.r(), var.r()], [t.r()])
            K.act(xs, t.ap()[:, :n], AF.Identity, [t.r(), lncol.r()], [xT.r((d, b))],
                  scale=lncol.ap()[:, gi, d:d + 1], bias=lncol.ap()[:, gi + 1, d:d + 1])


def residual_add(K, C, l, ps, n, dout, b, gt, comb=None):
    xT, mod = C["xT"], C["modT"][l]
    c0, _ = BLOCKS[b]
    xs = xT.ap()[:, dout, c0:c0 + n]
    src = ps.ap()[:, :n]
    rd = [ps.r(), mod.r(), xT.r((dout, b))]
    if comb is not None:
        tmp = C["tmp_c"][dout % 2]
        K.tt(tmp.ap()[:, :n], src, comb.ap()[:, c0:c0 + n], ALU.mult, [ps.r(), comb.r(b)], [tmp.r()])
        src = tmp.ap()[:, :n]
        rd = [tmp.r(), mod.r(), xT.r((dout, b))]
    if b < 4:
        K.stt(xs, src, mod.ap()[:, gt + dout, 0:1], xs, ALU.mult, ALU.add, rd, [xT.r((dout, b))])
    else:
        tmp2 = C["tmp_s2"]
        K.tt(tmp2.ap(), src, mod.ap()[:, gt + dout, 1:1 + NS], ALU.mult, rd[:2], [tmp2.r()])
        K.tt(xs, tmp2.ap(), xs, ALU.add, [tmp2.r(), xT.r((dout, b))], [xT.r((dout, b))])


def scale_x(K, C):
    xT = C["xT"]
    for b, (c0, n) in enumerate(BLOCKS):
        for d in range(KT):
            xs = xT.ap()[:, d, c0:c0 + n]
            K.P.op("act", lambda e, xs=xs: e.mul(out=xs, in_=xs, mul=ALPHA), reads=[xT.r((d, b))],
                   writes=[xT.r((d, b))])


def ffn_gateup(K, C, l, hT, actb, wpool, srcs, nf, ps):
    wg_src, wu_src, wd_src = srcs
    sg = C["sg"]

    def unit():
        u = wpool["bufs"][wpool["i"] % len(wpool["bufs"])]
        wpool["i"] += 1
        return u

    i = 0
    for f0 in range(0, nf, 4):
        nfu = min(4, nf - f0)
        WG, WU = unit(), unit()
        K.dma(WG.ap()[:, :, 0:nfu * 128], wg_src[:, f0 * 128:(f0 + nfu) * 128].rearrange("(k p) n -> p k n", p=128),
              w=[WG.r()], q="pool")
        K.dma(WU.ap()[:, :, 0:nfu * 128], wu_src[:, f0 * 128:(f0 + nfu) * 128].rearrange("(k p) n -> p k n", p=128),
              w=[WU.r()], q="pool")
        for fu in range(nfu):
            f = f0 + fu
            for b, (c0, n) in enumerate(BLOCKS):
                pg, pu = ps["g"][i % 2], ps["u"][i % 2]
                for k in range(KT):
                    K.mm(pg.ap()[:, :n], WG.ap()[:, k, fu * 128:(fu + 1) * 128], hT.ap()[:, k, c0:c0 + n],
                         [WG.r(), hT.r((k, b))], [pg.r()], start=(k == 0), stop=(k == KT - 1))
                for k in range(KT):
                    K.mm(pu.ap()[:, :n], WU.ap()[:, k, fu * 128:(fu + 1) * 128], hT.ap()[:, k, c0:c0 + n],
                         [WU.r(), hT.r((k, b))], [pu.r()], start=(k == 0), stop=(k == KT - 1))
                s_ = sg[i % 2]
                K.act(s_.ap()[:, :n], pg.ap()[:, :n], AF.Silu, [pg.r()], [s_.r()])
                K.tt(actb.ap()[:, f, c0:c0 + n], s_.ap()[:, :n], pu.ap()[:, :n], ALU.mult, [s_.r(), pu.r()],
                     [actb.r((f, b))])
                i += 1
                yield


def ffn_down(K, C, l, actb, wpool, srcs, nf, ps, comb=None):
    wg_src, wu_src, wd_src = srcs

    def unit():
        u = wpool["bufs"][wpool["i"] % len(wpool["bufs"])]
        wpool["i"] += 1
        return u

    i = 0
    for dh in range(2):
        WD = unit()
        K.dma(WD.ap()[:, 0:nf, :], wd_src[:, dh * 512:(dh + 1) * 512].rearrange("(f p) n -> p f n", p=128),
              w=[WD.r()], q="pool")
        for dd in range(4):
            dout = dh * 4 + dd
            for b, (c0, n) in enumerate(BLOCKS):
                pd = ps["d"][i % 2]
                for f in range(nf):
                    K.mm(pd.ap()[:, :n], WD.ap()[:, f, dd * 128:(dd + 1) * 128], actb.ap()[:, f, c0:c0 + n],
                         [WD.r(), actb.r((f, b))], [pd.r()], start=(f == 0), stop=(f == nf - 1))
                residual_add(K, C, l, pd, n, dout, b, GT2, comb=comb)
                i += 1


def ffn_group(K, C, l, hT, actb, wpool, srcs, nf, ps, comb=None):
    for _ in ffn_gateup(K, C, l, hT, actb, wpool, srcs, nf, ps):
        pass
    ffn_down(K, C, l, actb, wpool, srcs, nf, ps, comb=comb)


def moe_routing(K, C, dr, l, ph, ps):
    xT, mod, identf = C["xT"], C["modT"][l], C["identf"]
    router = K.sb(ph, "router", [128, KT, 8], F32)
    K.dma(router.ap(), dr["moe_router"][0].rearrange("(k p) e -> p k e", p=128), w=[router.r()])
    combT = K.sb(ph, "combT", [8, NT], F32)
    tp = C["tpair"]
    sm = {n: K.sb(ph, "rt_" + n, [128, 8], F32) for n in ["lg", "eq1", "l2", "eq2", "cb"]}
    sc1 = {n: K.sb(ph, "rs_" + n, [128, 1], F32) for n in ["m1", "m2", "e", "w1", "w2"]}
    pl, pt = ps["rA"], ps["rB"]
    for tt in range(NCH + 1):
        n = 128 if tt < NCH else NS
        c0 = tt * 128
        b = min(tt // 4, 4)
        hap = tp.ap().rearrange("p a (b c) -> p (a b) c", c=128)
        hres = [tp.r(0), tp.r(1)]
        if tt < NCH:
            for d in range(KT):
                K.act(hap[:, d, :], xT.ap()[:, d, c0:c0 + n], AF.Identity, [xT.r((d, b)), mod.r()], hres,
                      scale=mod.ap()[:, SC2 + d, 0:1], bias=mod.ap()[:, SH2 + d, 0:1])
        else:
            K.tt(hap[:, :, 0:n], xT.ap()[:, :, c0:c0 + n], mod.ap()[:, SC2:SC2 + 8, 1:1 + NS], ALU.mult,
                 xr(xT, b) + [mod.r()], hres)
            K.tt(hap[:, :, 0:n], hap[:, :, 0:n], mod.ap()[:, SH2:SH2 + 8, 1:1 + NS], ALU.add,
                 hres + [mod.r()], hres)
        for d in range(KT):
            K.mm(pl.ap()[0:n, 0:8], hap[:, d, 0:n], router.ap()[:, d, :], hres + [router.r()], [pl.r()],
                 start=(d == 0), stop=(d == KT - 1), inc=True)
        lg, eq1, l2, eq2, cb = (sm[k].ap()[0:n, :] for k in ["lg", "eq1", "l2", "eq2", "cb"])
        m1, m2, ee, w1, w2 = (sc1[k].ap()[0:n, :] for k in ["m1", "m2", "e", "w1", "w2"])
        R = lambda *ks: [(sm[k] if k in sm else sc1[k]).r() for k in ks]
        K.cp(lg, pl.ap()[0:n, 0:8], [pl.r()], R("lg"))
        yield
        K.red(m1, lg, R("lg"), R("m1"), op=ALU.max)
        yield
        K.ts(eq1, lg, m1, ALU.is_equal, R("lg", "m1"), R("eq1"))
        yield
        K.stt(l2, eq1, -1e30, lg, ALU.mult, ALU.add, R("eq1", "lg"), R("l2"))
        yield
        K.red(m2, l2, R("l2"), R("m2"), op=ALU.max)
        yield
        K.ts(eq2, l2, m2, ALU.is_equal, R("l2", "m2"), R("eq2"))
        yield
        K.tt(ee, m2, m1, ALU.subtract, R("m1", "m2"), R("e"))
        yield
        K.act(ee, ee, AF.Exp, R("e"), R("e"))
        yield
        K.ts(w1, ee, 1.0, ALU.add, R("e"), R("w1"))
        yield
        K.recip(w1, w1, R("w1"), R("w1"))
        yield
        K.tt(w2, ee, w1, ALU.mult, R("e", "w1"), R("w2"))
        yield
        K.ts(cb, eq1, w1, ALU.mult, R("eq1", "w1"), R("cb"))
        yield
        K.stt(cb, eq2, w2, cb, ALU.mult, ALU.add, R("eq2", "w2", "cb"), R("cb"))
        yield
        K.tr(pt.ap()[0:8, 0:n], cb, identf.ap()[0:n, 0:n], R("cb") + [identf.r()], [pt.r()])
        yield
        K.cp(combT.ap()[:, c0:c0 + n], pt.ap()[0:8, 0:n], [pt.r()], [combT.r()], eng="act")
        yield
    C["_combT"] = combT
    yield


def layer(K, dr, C, l, stub_mixer, dbg_out):
    nc, P = K.nc, K.P
    xT, mod = C["xT"], C["modT"][l]
    with contextlib.ExitStack() as lay:
        bufA = K.sb(lay, "bufA", [128, KT, NT], BF16)
        lncol = K.sb(lay, "lncol", [128, 4, KT], F32)
        C["lncol"] = lncol
        C["tmp_s"] = K.sb(lay, "tmp_s", [128, KT, NS], F32)
        C["tmp_s2"] = K.sb(lay, "tmp_s2", [128, NS], F32)
        for i, nme in enumerate(["ln_mix_g", "ln_mix_b", "ln_ffn_g", "ln_ffn_b"]):
            K.dma(lncol.ap()[:, i, :], dr[nme][l].rearrange("(d p) -> p d", p=128), w=[lncol.r()])

        with contextlib.ExitStack() as ph:
            if stub_mixer:
                for b in range(5):
                    modulate(K, C, l, xT, bufA, b, SH1, SC1, lambda d, b=b: bufA.r((d, b)))
            else:
                mixers(K, dr, C, l, bufA, dbg_out)
            P.barrier()
            wout = K.sb(ph, "wout", [128, KT, D], BF16)
            K.dma(wout.ap(), dr["w_out"][l].rearrange("(k p) n -> p k n", p=128), w=[wout.r()], q="pool")
            scale_x(K, C)
            with contextlib.ExitStack() as ph2:
                pso = [K.ps(ph2, f"pso{i}", [128, 512], F32) for i in range(4)]
                i = 0
                for dout in range(KT):
                    for b, (c0, n) in enumerate(BLOCKS):
                        pd = pso[i % 4]
                        for k in range(KT):
                            K.mm(pd.ap()[:, :n], wout.ap()[:, k, dout * 128:(dout + 1) * 128],
                                 bufA.ap()[:, k, c0:c0 + n], [wout.r(), bufA.r((k, b))], [pd.r()],
                                 start=(k == 0), stop=(k == KT - 1))
                        residual_add(K, C, l, pd, n, dout, b, GT1)
                        i += 1
                P.barrier()

        with contextlib.ExitStack() as ph:
            hT = K.sb(ph, "hT", [128, KT, NT], BF16)
            tpair = K.sb(ph, "tpair", [128, 2, 512], F32)
            C["tpair"] = tpair

            class _V:
                def __init__(self, i):
                    self.i = i

                def ap(self):
                    return tpair.ap()[:, self.i, :]

                def r(self, key=0):
                    return tpair.r(self.i)
            scr = dict(sq=[K.sb(ph, f"sq{i}", [128, 512], F32) for i in range(2)],
                       mean=K.sb(ph, "mean", [128, 512], F32), var=K.sb(ph, "var", [128, 512], F32),
                       t=[_V(0), _V(1)])
            C["sg"] = scr["sq"]
            C["tmp_c"] = scr["t"]
            W = dict(bufs=[K.sb(ph, f"WP{i}", [128, KT, 512], BF16) for i in range(4)], i=0)
            ps = dict(g=[K.ps(ph, f"pg{i}", [128, 512], F32) for i in range(2)],
                      u=[K.ps(ph, f"pu{i}", [128, 512], F32) for i in range(2)],
                      d=[K.ps(ph, f"pd{i}", [128, 512], F32) for i in range(2)])
            psA = K.ps(ph, "psA", [128, 512], F32)
            psB = K.ps(ph, "psB", [128, 512], F32)
            layernorm(K, C, l, 0, psA, psB, scr)
            for b in range(5):
                modulate(K, C, l, xT, hT, b, SH2, SC2, lambda d, b=b: hT.r((d, b)))
            if l % 2 == 0:
                scale_x(K, C)
                i = l // 2
                for f0 in range(0, F_DENSE // 128, 8):
                    nf = min(8, F_DENSE // 128 - f0)
                    srcs = (dr["ffn_w_gate"][i, :, f0 * 128:(f0 + nf) * 128],
                            dr["ffn_w_up"][i, :, f0 * 128:(f0 + nf) * 128],
                            dr["ffn_w_down"][i, f0 * 128:(f0 + nf) * 128, :])
                    ffn_group(K, C, l, hT, bufA, W, srcs, nf, ps)
            else:
                ps["rA"], ps["rB"] = psA, psB
                i = l // 2
                srcs0 = (dr["moe_w_gate"][i, 0], dr["moe_w_up"][i, 0], dr["moe_w_down"][i, 0])
                interleave([moe_routing(K, C, dr, l, ph, ps), ffn_gateup(K, C, l, hT, bufA, W, srcs0, 8, ps)],
                           ratio=[6, 1])
                combT = C["_combT"]
                scale_x(K, C)
                combB = K.sb(ph, "combB", [128, NT], F32)
                sele = K.sb(ph, "sele", [8, 128], F32)
                i = l // 2
                for e_ in range(8):
                    K.memset(sele.ap(), 0.0, [sele.r()])
                    K.P.op("dve", lambda e, e_=e_: e.memset(sele.ap()[e_:e_ + 1, :], 1.0), reads=[sele.r()],
                           writes=[sele.r()]) if False else K.ts(
                        sele.ap(), C["identf"].ap()[0:8, e_:e_ + 1].to_broadcast([8, 128]), 1.0, ALU.mult,
                        [C["identf"].r()], [sele.r()])
                    for b, (c0, n) in enumerate(BLOCKS):
                        pb = ps["d"][b % 2]
                        K.mm(pb.ap()[:, :n], sele.ap(), combT.ap()[:, c0:c0 + n], [sele.r(), combT.r()],
                             [pb.r()])
                        K.cp(combB.ap()[:, c0:c0 + n], pb.ap()[:, :n], [pb.r()], [combB.r(b)], eng="act")
                    srcs = (dr["moe_w_gate"][i, e_], dr["moe_w_up"][i, e_], dr["moe_w_down"][i, e_])
                    if e_ > 0:
                        for _ in ffn_gateup(K, C, l, hT, bufA, W, srcs, 8, ps):
                            pass
                    ffn_down(K, C, l, bufA, W, srcs, 8, ps, comb=combB)
            layernorm(K, C, l, 2, psA, psB, scr)
            P.barrier()


def make_in_maps(inp):
    g = lambda k: np.ascontiguousarray(np.asarray(inp[k], dtype=np.float32))
    xp, xs, cp, cs = g("x_prompt"), g("x_sample"), g("c_prompt"), g("c_sample")
    st = {k: g(k) for k in ["state_ssd", "state_ssd_conv", "state_rwkv", "state_rwkv_shift", "state_gla"]}
    wts = {k: g(k) for k in W_SHAPES}
    maps = []
    for c in range(NCORES):
        sl = slice(c * NS, (c + 1) * NS)
        m = dict(wts)
        m["xp"] = xp[c]
        m["xs"] = np.ascontiguousarray(xs[sl, 0, :])
        m["cc"] = np.ascontiguousarray(np.concatenate([cp[c:c + 1], cs[sl]], axis=0))
        m["st_ssd"] = np.ascontiguousarray(st["state_ssd"][:, sl])
        m["st_conv"] = np.ascontiguousarray(st["state_ssd_conv"][:, sl])
        m["st_rwkv"] = np.ascontiguousarray(st["state_rwkv"][:, sl])
        m["st_shift"] = np.ascontiguousarray(st["state_rwkv_shift"][:, sl])
        m["st_gla"] = np.ascontiguousarray(st["state_gla"][:, sl])
        maps.append(m)
    return maps


_NC_CACHE = {}


def gather(results):
    R = lambda k: [np.asarray(r[k], dtype=np.float32) for r in results]
    y_p = np.stack(R("y_p"), axis=0)
    y_s = np.concatenate(R("y_s"), axis=0)[:, None, :]
    outs = [y_p, y_s]
    for k in ["p_ssd", "p_conv", "p_rwkv", "p_shift", "p_gla"]:
        outs.append(np.stack(R(k), axis=1))
    for k in ["s_ssd", "s_conv", "s_rwkv", "s_shift", "s_gla"]:
        outs.append(np.concatenate(R(k), axis=1))
    return tuple(np.ascontiguousarray(o) for o in outs)


def kernel(**inputs):
    if "nc" not in _NC_CACHE:
        _NC_CACHE["nc"] = build()
    res = run_bass_kernel_spmd(_NC_CACHE["nc"], make_in_maps(inputs), core_ids=list(range(NCORES)))
    return gather(res.results)


def bc(ap, axis, shape):
    return ap.unsqueeze(axis).to_broadcast(list(shape))


def softplus_(K, x, tmp, r, n):
    xa, ta = x[0], tmp[0]
    K.act(ta, xa, AF.Abs, [x[1]], [tmp[1]])
    K.act(ta, ta, AF.Exp, [tmp[1]], [tmp[1]], scale=-1.0)
    K.act(ta, ta, AF.Ln, [tmp[1]], [tmp[1]], bias=1.0)
    K.ts(xa, xa, 0.0, ALU.max, [x[1]], [x[1]])
    K.tt(xa, xa, ta, ALU.add, [x[1], tmp[1]], [x[1]])


def make_hc(K, C, l, hc, c):
    xT, mod = C["xT"], C["modT"][l]
    for d in range(KT):
        K.act(hc.ap()[:, d, :], xT.ap()[:, d, c * 128:(c + 1) * 128], AF.Identity,
              [xT.r((d, c // 4)), mod.r()], [hc.r()],
              scale=mod.ap()[:, SC1 + d, 0:1], bias=mod.ap()[:, SH1 + d, 0:1])


def mixers(K, dr, C, l, yT, dbg_out):
    P = K.P
    xT, mod = C["xT"], C["modT"][l]
    en = C.get("enable", ("ssd", "rwkv", "gla"))
    with contextlib.ExitStack() as mx:
        hc = [K.sb(mx, f"hc{i}", [128, KT, 128], BF16) for i in range(2)]
        hs = K.sb(mx, "hs", [128, KT, NS], BF16)
        C["hc"], C["hs"] = hc, hs
        modulate(K, C, l, xT, _Shift(hs, 2048), 4, SH1, SC1, lambda d: hs.r())
        for name, tiles in (("ssd", range(0, 4)), ("rwkv", range(4, 6)), ("gla", range(6, 8))):
            if name not in en:
                for d in tiles:
                    for b, (c0, n) in enumerate(BLOCKS):
                        K.memset(yT.ap()[:, d, c0:c0 + n], 0.0, [yT.r((d, b))])
        if "ssd" in en and "gla" in en:
            ssd_gla_phase(K, dr, C, l, yT, dbg_out)
            P.barrier()
        else:
            if "ssd" in en:
                ssd_phase(K, dr, C, l, yT, dbg_out)
                P.barrier()
            if "gla" in en:
                gla_phase(K, dr, C, l, yT, dbg_out)
                P.barrier()
        if "rwkv" in en:
            rwkv_phase(K, dr, C, l, yT, dbg_out)
            P.barrier()


class _Shift:
    def __init__(self, t, off):
        self.t, self.off = t, off

    def ap(self):
        return _ShiftAP(self.t.ap(), self.off)

    def r(self, key=0):
        return self.t.r()


class _ShiftAP:
    def __init__(self, ap, off):
        self._ap, self.off = ap, off

    def __getitem__(self, key):
        p, d, s = key
        return self._ap[p, d, slice(s.start - self.off, s.stop - self.off)]


def ssd_phase(K, dr, C, l, yT, dbg_out):
    P = K.P
    nc = K.nc
    identb, identf, maskU, maskSL, ones1 = C["identb"], C["identf"], C["maskU"], C["maskSL"], C["ones1"]
    hc, hs = C["hc"], C["hs"]
    with contextlib.ExitStack() as ph:
        win = K.sb(ph, "win_ssd", [128, KT, 1288], BF16)
        K.dma(win.ap(), dr["w_in"][l, :, 0:1288].rearrange("(k p) n -> p k n", p=128), w=[win.r()], q="pool")
        convw = K.sb(ph, "convw", [128, 6, 4], F32)
        convb = K.sb(ph, "convb", [128, 6], F32)
        for i in range(4):
            K.dma(convw.ap()[:, :, i], dr["ssd_conv_w"][l, i].rearrange("(t p) -> p t", p=128), w=[convw.r()])
        K.dma(convb.ap(), dr["ssd_conv_b"][l].rearrange("(t p) -> p t", p=128), w=[convb.r()])
        normg = K.sb(ph, "normg", [128, 4], F32)
        K.dma(normg.ap(), dr["ssd_norm_g"][l].rearrange("(t p) -> p t", p=128), w=[normg.r()])
        dtbB = K.sb(ph, "dtbB", [128, 8], F32)
        aB = K.sb(ph, "aB", [128, 8], F32)
        dB = K.sb(ph, "dB", [128, 8], F32)
        K.dma(dtbB.ap(), dr["ssd_dt_bias"][l:l + 1, :].to_broadcast([128, 8]), w=[dtbB.r()])
        K.dma(aB.ap(), dr["ssd_a_log"][l:l + 1, :].to_broadcast([128, 8]), w=[aB.r()])
        K.dma(dB.ap(), dr["ssd_d"][l:l + 1, :].to_broadcast([128, 8]), w=[dB.r()])
        K.act(aB.ap(), aB.ap(), AF.Exp, [aB.r()], [aB.r()])
        K.ts(aB.ap(), aB.ap(), -1.0, ALU.mult, [aB.r()], [aB.r()])
        import os
        if os.environ.get("SKIP_SSD_PROMPT") != "1":
            ssd_prompt(K, dr, C, l, yT, win, convw, convb, normg, dtbB, aB, dB, dbg_out)
        P.barrier()
        if os.environ.get("SKIP_SSD_SAMPLE") != "1":
            ssd_sample(K, dr, C, l, yT, win, aB, dB, dtbB, dbg_out)


def ssd_prompt(K, dr, C, l, yT, win, convw, convb, normg, dtbB, aB, dB, dbg_out):
    P = K.P
    identb, identf, maskU, maskSL, ones1 = C["identb"], C["identf"], C["maskU"], C["maskSL"], C["ones1"]
    hc, hs = C["hc"], C["hs"]
    with contextlib.ExitStack() as ph:
        XB = [K.sb(ph, f"XB{i}", [128, 6, 131], F32) for i in range(2)]
        XC = [K.sb(ph, f"XC{i}", [128, 6, 128], BF16) for i in range(2)]
        cacc = [K.sb(ph, f"cacc{i}", [128, 128], F32) for i in range(2)]
        sz = K.sb(ph, "sz", [128, 512], F32)
        dtt = K.sb(ph, "dtt", [128, 8], F32)
        dtmp = K.sb(ph, "dtmp", [128, 8], F32)
        dtA = K.sb(ph, "dtA", [128, 8], F32)
        csb = K.sb(ph, "csb", [128, 16], F32)
        e1 = K.sb(ph, "e1", [128, 8], F32)
        el = K.sb(ph, "el", [128, 8], F32)
        tail = K.sb(ph, "tail", [128, 8], F32)
        Rt = K.sb(ph, "Rt", [128, 8, 128], F32)
        dec = K.sb(ph, "dec", [128, 8, 128], F32)
        Gs = K.sb(ph, "Gs", [128, 2, 128], F32)
        Mb = K.sb(ph, "Mb", [128, 8, 128], BF16)
        XT = K.sb(ph, "XT", [128, 640], BF16)
        xD = K.sb(ph, "xD", [128, 512], BF16)
        xw = K.sb(ph, "xw", [128, 512], BF16)
        t1 = K.sb(ph, "t1", [128, 512], F32)
        yn = K.sb(ph, "yn", [128, 512], BF16)
        ss = K.sb(ph, "ss", [128, 1], F32)
        HS32 = K.sb(ph, "HS32", [128, 4, 64], F32)
        HSb = K.sb(ph, "HSb", [128, 4, 64], BF16)
        hsT = K.sb(ph, "hsT", [128, 2, 128], F32)

        ps_x = K.ps(ph, "ps_x", [128, 1024], F32)
        ps_z = K.ps(ph, "ps_z", [128, 512], F32)
        ps_c = K.ps(ph, "ps_c", [128, 512], F32)
        ps_t = K.ps(ph, "ps_t", [128, 1024], BF16)
        ps_y = K.ps(ph, "ps_y", [128, 512], F32)
        ps_i = K.ps(ph, "ps_i", [128, 512], F32)
        ps_h = K.ps(ph, "ps_h", [128, 512], F32)

        K.memset(HS32.ap(), 0.0, [HS32.r()])
        K.memset(HSb.ap(), 0.0, [HSb.r()])
        K.memset(XB[0].ap()[:, :, 0:3], 0.0, [XB[0].r()])

        for c in range(NCH):
            h = hc[c % 2]
            make_hc(K, C, l, h, c)
            xb, xc = XB[c % 2], XC[c % 2]
            for ct in range(6):
                for k in range(KT):
                    K.mm(ps_x.ap()[:, ct * 128:(ct + 1) * 128], win.ap()[:, k, 512 + ct * 128:512 + (ct + 1) * 128],
                         h.ap()[:, k, :], [win.r(), h.r()], [ps_x.r()], start=(k == 0), stop=(k == KT - 1),
                         inc=(k == KT - 1 and ct == 5))
            K.cp(xb.ap()[:, :, 3:131], ps_x.ap()[:, 0:768].rearrange("p (t n) -> p t n", t=6), [ps_x.r()], [xb.r()],
                 eng="act")
            if c + 1 < NCH:
                K.cp(XB[(c + 1) % 2].ap()[:, :, 0:3], xb.ap()[:, :, 128:131], [xb.r()], [XB[(c + 1) % 2].r()])
            for ct in range(6):
                ca = cacc[ct % 2]
                K.ts(ca.ap(), xb.ap()[:, ct, 0:128], convw.ap()[:, ct, 0:1], ALU.mult, [xb.r(), convw.r(), convb.r()],
                     [ca.r()], s2=convb.ap()[:, ct:ct + 1], op1=ALU.add)
                for i in range(1, 4):
                    K.stt(ca.ap(), xb.ap()[:, ct, i:i + 128], convw.ap()[:, ct, i:i + 1], ca.ap(), ALU.mult, ALU.add,
                          [xb.r(), convw.r(), ca.r()], [ca.r()])
                K.act(xc.ap()[:, ct, :], ca.ap(), AF.Silu, [ca.r()], [xc.r()])
            for k in range(KT):
                K.mm(ps_z.ap(), h.ap()[:, k, :], win.ap()[:, k, 0:512], [h.r(), win.r()], [ps_z.r()],
                     start=(k == 0), stop=(k == KT - 1))
            for k in range(KT):
                K.mm(ps_c.ap()[:, 0:8], h.ap()[:, k, :], win.ap()[:, k, 1280:1288], [h.r(), win.r()], [ps_c.r()],
                     start=(k == 0), stop=(k == KT - 1))
            K.act(sz.ap(), ps_z.ap(), AF.Silu, [ps_z.r()], [sz.r()])
            K.tt(dtt.ap(), ps_c.ap()[:, 0:8], dtbB.ap(), ALU.add, [ps_c.r(), dtbB.r()], [dtt.r()])
            softplus_(K, (dtt.ap(), dtt.r()), (dtmp.ap(), dtmp.r()), None, None)
            K.tt(dtA.ap(), dtt.ap(), aB.ap(), ALU.mult, [dtt.r(), aB.r()], [dtA.r()])
            K.mm(ps_c.ap()[:, 8:16], maskU.ap(), dtA.ap(), [maskU.r(), dtA.r()], [ps_c.r()])
            K.mm(ps_c.ap()[:, 16:24], ones1.ap(), dtA.ap(), [ones1.r(), dtA.r()], [ps_c.r()])
            K.tt(Rt.ap(), bc(maskU.ap(), 1, [128, 8, 128]), bc(dtA.ap(), 2, [128, 8, 128]), ALU.mult,
                 [maskU.r(), dtA.r()], [Rt.r()])
            for hf in range(2):
                K.mm(ps_x.ap()[:, hf * 512:(hf + 1) * 512], maskSL.ap(),
                     Rt.ap()[:, hf * 4:(hf + 1) * 4, :].rearrange("p h i -> p (h i)"), [maskSL.r(), Rt.r()],
                     [ps_x.r()])
            K.act(dec.ap().rearrange("p h i -> p (h i)"), ps_x.ap(), AF.Exp, [ps_x.r()], [dec.r()])
            K.cp(csb.ap(), ps_c.ap()[:, 8:24], [ps_c.r()], [csb.r()], eng="act")
            for g in range(2):
                K.mm(ps_c.ap()[:, 256 + g * 128:256 + (g + 1) * 128], xc.ap()[64 * g:64 * g + 64, 4, :],
                     xc.ap()[64 * g:64 * g + 64, 5, :], [xc.r()], [ps_c.r()], self_wait=(g == 1))
            K.tt(Gs.ap(), ps_c.ap()[:, 256:512].rearrange("p (g i) -> p g i", g=2), bc(maskU.ap(), 1, [128, 2, 128]),
                 ALU.mult, [ps_c.r(), maskU.r()], [Gs.r()])
            K.tt(dec.ap().rearrange("p (g r) i -> p g r i", g=2), dec.ap().rearrange("p (g r) i -> p g r i", g=2),
                 bc(Gs.ap(), 2, [128, 2, 4, 128]), ALU.mult, [dec.r(), Gs.r()], [dec.r()])
            K.tt(Mb.ap(), dec.ap(), bc(dtt.ap(), 2, [128, 8, 128]), ALU.mult, [dec.r(), dtt.r()], [Mb.r()])
            for ct in range(5):
                K.tr(ps_t.ap()[:, ct * 128:(ct + 1) * 128], xc.ap()[:, ct, :], identb.ap(), [xc.r(), identb.r()],
                     [ps_t.r()], inc=(ct == 4))
            K.cp(XT.ap(), ps_t.ap()[:, 0:640], [ps_t.r()], [XT.r()], eng="act")
            K.tt(xD.ap().rearrange("p (h q) -> p h q", h=8), XT.ap()[:, 0:512].rearrange("p (h q) -> p h q", h=8),
                 bc(dB.ap(), 2, [128, 8, 64]), ALU.mult, [XT.r(), dB.r()], [xD.r()])
            K.mm(ps_y.ap(), identb.ap(), xD.ap(), [identb.r(), xD.r()], [ps_y.r()], start=True, stop=False)
            for hh in range(8):
                K.mm(ps_y.ap()[:, hh * 64:(hh + 1) * 64], Mb.ap()[:, hh, :], XT.ap()[:, hh * 64:(hh + 1) * 64],
                     [Mb.r(), XT.r()], [ps_y.r()], start=False, stop=(hh == 7))
            for g in range(2):
                K.mm(ps_i.ap()[:, g * 256:(g + 1) * 256], xc.ap()[64 * g:64 * g + 64, 5, :],
                     HSb.ap()[64 * g:64 * g + 64, :, :].rearrange("p h q -> p (h q)"), [xc.r(), HSb.r()], [ps_i.r()],
                     self_wait=(g == 1))
            K.act(e1.ap(), csb.ap()[:, 0:8], AF.Exp, [csb.r()], [e1.r()])
            K.tt(t1.ap().rearrange("p (h q) -> p h q", h=8), ps_i.ap().rearrange("p (h q) -> p h q", h=8),
                 bc(e1.ap(), 2, [128, 8, 64]), ALU.mult, [ps_i.r(), e1.r()], [t1.r()])
            K.tt(t1.ap(), t1.ap(), ps_y.ap(), ALU.add, [t1.r(), ps_y.r()], [t1.r()])
            ssd_epilogue(K, C, t1, sz, ss, yn, 128)
            for q in range(4):
                K.tr(ps_t.ap()[:, q * 128:(q + 1) * 128], yn.ap()[:, q * 128:(q + 1) * 128], identb.ap(),
                     [yn.r(), identb.r()], [ps_t.r()], inc=(q == 3))
            K.tt(yT.ap()[:, 0:4, c * 128:(c + 1) * 128], ps_t.ap()[:, 0:512].rearrange("p (t n) -> p t n", t=4),
                 bc(normg.ap(), 2, [128, 4, 128]), ALU.mult, [ps_t.r(), normg.r()], xr(yT, c // 4, range(4)))
            K.act(el.ap(), csb.ap()[:, 8:16], AF.Exp, [csb.r()], [el.r()])
            K.tt(tail.ap(), csb.ap()[:, 8:16], csb.ap()[:, 0:8], ALU.subtract, [csb.r()], [tail.r()])
            K.act(tail.ap(), tail.ap(), AF.Exp, [tail.r()], [tail.r()])
            K.tt(tail.ap(), tail.ap(), dtt.ap(), ALU.mult, [tail.r(), dtt.r()], [tail.r()])
            K.tt(xw.ap().rearrange("p (h q) -> p h q", h=8), XT.ap()[:, 0:512].rearrange("p (h q) -> p h q", h=8),
                 bc(tail.ap(), 2, [128, 8, 64]), ALU.mult, [XT.r(), tail.r()], [xw.r()])
            K.mm(ps_h.ap(), XT.ap()[:, 512:640], xw.ap(), [XT.r(), xw.r()], [ps_h.r()])
            for g in range(2):
                sl = slice(64 * g, 64 * g + 64)
                K.tt(HS32.ap()[sl], HS32.ap()[sl], bc(el.ap()[sl, 4 * g:4 * g + 4], 2, [64, 4, 64]), ALU.mult,
                     [HS32.r(), el.r()], [HS32.r()])
                K.tt(HS32.ap()[sl], HS32.ap()[sl],
                     ps_h.ap()[sl, 256 * g:256 * g + 256].rearrange("p (h q) -> p h q", h=4), ALU.add,
                     [HS32.r(), ps_h.r()], [HS32.r()])
            K.cp(HSb.ap(), HS32.ap(), [HS32.r()], [HSb.r()], eng="act")

        xb = XB[(NCH - 1) % 2]
        for i in range(3):
            K.dma(dr["p_conv"][l, i].rearrange("(t p) -> p t", p=128), xb.ap()[:, :, 128 + i], r=[xb.r()])
        for q in range(2):
            K.tr(ps_y.ap()[:, q * 128:(q + 1) * 128], HS32.ap().rearrange("p h q -> p (h q)")[:, q * 128:(q + 1) * 128],
                 identf.ap(), [HS32.r(), identf.r()], [ps_y.r()], inc=(q == 1))
        K.cp(hsT.ap(), ps_y.ap()[:, 0:256].rearrange("p (q n) -> p q n", q=2), [ps_y.r()], [hsT.r()])
        for g in range(2):
            for q in range(2):
                K.dma(dr["p_ssd"][l, 4 * g + 2 * q:4 * g + 2 * q + 2].rearrange("h p n -> (h p) n"),
                      hsT.ap()[:, q, 64 * g:64 * g + 64], r=[hsT.r()])
        P.barrier()


def ssd_epilogue(K, C, y, sz, ss, yn, n):
    K.tt(y.ap()[0:n], y.ap()[0:n], sz.ap()[0:n], ALU.mult, [y.r(), sz.r()], [y.r()])
    K.act(yn.ap()[0:n], y.ap()[0:n], AF.Square, [y.r()], [yn.r(), ss.r()], accum_out=ss.ap()[0:n])
    K.act(ss.ap()[0:n], ss.ap()[0:n], AF.Sqrt, [ss.r()], [ss.r()], scale=1.0 / 512, bias=RMS_EPS)
    K.recip(ss.ap()[0:n], ss.ap()[0:n], [ss.r()], [ss.r()])
    K.ts(yn.ap()[0:n], y.ap()[0:n], ss.ap()[0:n], ALU.mult, [y.r(), ss.r()], [yn.r()])


def ssd_gla_phase(K, dr, C, l, yT, dbg_out):
    P = K.P
    identb, identf, maskU, maskSL, ones1 = C["identb"], C["identf"], C["maskU"], C["maskSL"], C["ones1"]
    hc, hs = C["hc"], C["hs"]
    G0 = OFF["gq"]
    with contextlib.ExitStack() as ph0:
        win = K.sb(ph0, "win_ssd", [128, KT, 1288], BF16)
        K.dma(win.ap(), dr["w_in"][l, :, 0:1288].rearrange("(k p) n -> p k n", p=128), w=[win.r()], q="pool")
        convw = K.sb(ph0, "convw", [128, 6, 4], F32)
        convb = K.sb(ph0, "convb", [128, 6], F32)
        for i in range(4):
            K.dma(convw.ap()[:, :, i], dr["ssd_conv_w"][l, i].rearrange("(t p) -> p t", p=128), w=[convw.r()])
        K.dma(convb.ap(), dr["ssd_conv_b"][l].rearrange("(t p) -> p t", p=128), w=[convb.r()])
        normg = K.sb(ph0, "normg", [128, 4], F32)
        K.dma(normg.ap(), dr["ssd_norm_g"][l].rearrange("(t p) -> p t", p=128), w=[normg.r()])
        dtbB = K.sb(ph0, "dtbB", [128, 8], F32)
        aB = K.sb(ph0, "aB", [128, 8], F32)
        dB = K.sb(ph0, "dB", [128, 8], F32)
        K.dma(dtbB.ap(), dr["ssd_dt_bias"][l:l + 1, :].to_broadcast([128, 8]), w=[dtbB.r()])
        K.dma(aB.ap(), dr["ssd_a_log"][l:l + 1, :].to_broadcast([128, 8]), w=[aB.r()])
        K.dma(dB.ap(), dr["ssd_d"][l:l + 1, :].to_broadcast([128, 8]), w=[dB.r()])
        K.act(aB.ap(), aB.ap(), AF.Exp, [aB.r()], [aB.r()])
        K.ts(aB.ap(), aB.ap(), -1.0, ALU.mult, [aB.r()], [aB.r()])
        wing = K.sb(ph0, "win_gla", [128, KT, 784], BF16)
        K.dma(wing.ap(), dr["w_in"][l, :, G0:G0 + 784].rearrange("(k p) n -> p k n", p=128), w=[wing.r()], q="pool")
        wgk2 = K.sb(ph0, "wgk2", [16, 128], BF16)
        K.dma(wgk2.ap(), dr["gla_w_gk2"][l], w=[wgk2.r()], q="pool")
        bgkB = K.sb(ph0, "bgkB", [128, 128], F32)
        K.dma(bgkB.ap(), dr["gla_b_gk"][l:l + 1, :].to_broadcast([128, 128]), w=[bgkB.r()])
        gcol = K.sb(ph0, "gcol", [128, 1], F32)
        for t in range(2):
            K.dma(gcol.ap()[64 * t:64 * t + 64, :], dr["gla_norm_g"][l].rearrange("(e o) -> e o", o=1), w=[gcol.r()])
        with contextlib.ExitStack() as ph:
            BM = K.sb(ph, "BM", [128, 256], F32)
            hm = K.sb(ph, "hm", [128, 4], F32)
            K.memset(BM.ap(), 1.0, [BM.r()], eng="pool")
            K.memset(hm.ap(), 1.0, [hm.r()], eng="pool")
            for hh in range(4):
                for (t, sl, n) in ((BM, slice(64 * hh, 64 * hh + 64), 64), (hm, slice(hh, hh + 1), 1)):
                    ap = t.ap()[:, sl]
                    K.P.op("pool", lambda e, ap=ap, n=n, hh=hh: e.affine_select(
                        out=ap, in_=ap, pattern=[[0, n]], compare_op=ALU.is_ge, fill=0.0, base=-32 * hh,
                        channel_multiplier=1), reads=[t.r()], writes=[t.r()])
                    K.P.op("pool", lambda e, ap=ap, n=n, hh=hh: e.affine_select(
                        out=ap, in_=ap, pattern=[[0, n]], compare_op=ALU.is_gt, fill=0.0, base=32 * hh + 32,
                        channel_multiplier=-1), reads=[t.r()], writes=[t.r()])
            PX = [K.ps(ph, f"PX{i}", [128, 512], F32) for i in range(2)]
            PZ = K.ps(ph, "PZ", [128, 512], F32)
            PC = K.ps(ph, "PC", [128, 512], F32)
            PF = K.ps(ph, "PF", [128, 512], F32)
            PV = K.ps(ph, "PV", [128, 512], F32)
            PL = K.ps(ph, "PL", [128, 512], F32)
            PT = K.ps(ph, "PT", [128, 1024], BF16)
            XB = [K.sb(ph, f"XB{i}", [128, 6, 131], F32) for i in range(2)]
            XC = [K.sb(ph, f"XC{i}", [128, 6, 128], BF16) for i in range(2)]
            cacc = [K.sb(ph, f"cacc{i}", [128, 128], F32) for i in range(2)]
            sz = K.sb(ph, "sz", [128, 512], F32)
            dtt = K.sb(ph, "dtt", [128, 8], F32)
            dtmp = K.sb(ph, "dtmp", [128, 8], F32)
            dtA = K.sb(ph, "dtA", [128, 8], F32)
            csb = K.sb(ph, "csb", [128, 16], F32)
            e1 = K.sb(ph, "e1", [128, 8], F32)
            el = K.sb(ph, "el", [128, 8], F32)
            tail = K.sb(ph, "tail", [128, 8], F32)
            Rt = K.sb(ph, "Rt", [128, 8, 128], F32)
            dec = K.sb(ph, "dec", [128, 8, 128], F32)
            Gs = K.sb(ph, "Gs", [128, 2, 128], F32)
            Mb = K.sb(ph, "Mb", [128, 8, 128], BF16)
            XT = K.sb(ph, "XT", [128, 640], BF16)
            xD = K.sb(ph, "xD", [128, 512], BF16)
            xw = K.sb(ph, "xw", [128, 512], BF16)
            t1 = K.sb(ph, "t1", [128, 512], F32)
            yn = K.sb(ph, "yn", [128, 512], BF16)
            ss = K.sb(ph, "ss", [128, 1], F32)
            HS32 = K.sb(ph, "HS32", [128, 4, 64], F32)
            HSb = K.sb(ph, "HSb", [128, 4, 64], BF16)
            glo = K.sb(ph, "glo", [16, 128], BF16)
            lg = K.sb(ph, "lg", [128, 128], F32)
            lgt = K.sb(ph, "lgt", [128, 128], F32)
            Eq = K.sb(ph, "Eq", [128, 128], F32)
            Ek = K.sb(ph, "Ek", [128, 128], F32)
            Ekt = K.sb(ph, "Ekt", [128, 128], F32)
            qt = K.sb(ph, "qt", [128, 128], BF16)
            kf = K.sb(ph, "kf", [128, 128], F32)
            km = K.sb(ph, "km", [128, 4, 128], BF16)
            ktm = K.sb(ph, "ktm", [128, 128], BF16)
            vtm = K.sb(ph, "vtm", [128, 256], BF16)
            sgg = K.sb(ph, "sgg", [128, 256], F32)
            A = K.sb(ph, "A", [128, 4, 128], BF16)
            osq = K.sb(ph, "osq", [128, 256], F32)
            ms = K.sb(ph, "ms", [128, 4], F32)
            on = K.sb(ph, "on", [128, 256], F32)
            onb = K.sb(ph, "onb", [128, 256], BF16)
            tmpS = K.sb(ph, "tmpS", [128, 256], F32)
            S32 = K.sb(ph, "S32", [128, 256], F32)
            Sb = K.sb(ph, "Sb", [128, 256], BF16)

            K.memset(HS32.ap(), 0.0, [HS32.r()])
            K.memset(HSb.ap(), 0.0, [HSb.r()])
            K.memset(XB[0].ap()[:, :, 0:3], 0.0, [XB[0].r()])
            K.memset(S32.ap(), 0.0, [S32.r()])
            K.memset(Sb.ap(), 0.0, [Sb.r()])

            def ssd_body(c):
                h = hc[c % 2]
                xb, xc = XB[c % 2], XC[c % 2]
                for ct in range(6):
                    px = PX[0] if ct < 4 else PX[1]
                    cc = ct if ct < 4 else ct - 4
                    for k in range(KT):
                        K.mm(px.ap()[:, cc * 128:(cc + 1) * 128], win.ap()[:, k, 512 + ct * 128:512 + (ct + 1) * 128],
                             h.ap()[:, k, :], [win.r(), h.r()], [px.r()], start=(k == 0), stop=(k == KT - 1))
                    yield
                K.cp(xb.ap()[:, 0:4, 3:131], PX[0].ap().rearrange("p (t n) -> p t n", t=4), [PX[0].r()], [xb.r()],
                     eng="act")
                yield
                K.cp(xb.ap()[:, 4:6, 3:131], PX[1].ap()[:, 0:256].rearrange("p (t n) -> p t n", t=2), [PX[1].r()],
                     [xb.r()], eng="act")
                yield
                if c + 1 < NCH:
                    K.cp(XB[(c + 1) % 2].ap()[:, :, 0:3], xb.ap()[:, :, 128:131], [xb.r()], [XB[(c + 1) % 2].r()])
                for ct in range(6):
                    ca = cacc[ct % 2]
                    K.ts(ca.ap(), xb.ap()[:, ct, 0:128], convw.ap()[:, ct, 0:1], ALU.mult,
                         [xb.r(), convw.r(), convb.r()], [ca.r()], s2=convb.ap()[:, ct:ct + 1], op1=ALU.add)
                    for i in range(1, 4):
                        K.stt(ca.ap(), xb.ap()[:, ct, i:i + 128], convw.ap()[:, ct, i:i + 1], ca.ap(), ALU.mult, ALU.add,
                              [xb.r(), convw.r(), ca.r()], [ca.r()])
                    yield
                    K.act(xc.ap()[:, ct, :], ca.ap(), AF.Silu, [ca.r()], [xc.r()])
                    yield
                for k in range(KT):
                    K.mm(PZ.ap(), h.ap()[:, k, :], win.ap()[:, k, 0:512], [h.r(), win.r()], [PZ.r()],
                         start=(k == 0), stop=(k == KT - 1))
                for k in range(KT):
                    K.mm(PC.ap()[:, 0:8], h.ap()[:, k, :], win.ap()[:, k, 1280:1288], [h.r(), win.r()], [PC.r()],
                         start=(k == 0), stop=(k == KT - 1))
                yield
                K.act(sz.ap(), PZ.ap(), AF.Silu, [PZ.r()], [sz.r()])
                yield
                K.tt(dtt.ap(), PC.ap()[:, 0:8], dtbB.ap(), ALU.add, [PC.r(), dtbB.r()], [dtt.r()])
                yield
                softplus_(K, (dtt.ap(), dtt.r()), (dtmp.ap(), dtmp.r()), None, None)
                yield
                K.tt(dtA.ap(), dtt.ap(), aB.ap(), ALU.mult, [dtt.r(), aB.r()], [dtA.r()])
                yield
                K.mm(PC.ap()[:, 8:16], maskU.ap(), dtA.ap(), [maskU.r(), dtA.r()], [PC.r()])
                K.mm(PC.ap()[:, 16:24], ones1.ap(), dtA.ap(), [ones1.r(), dtA.r()], [PC.r()])
                yield
                K.tt(Rt.ap(), bc(maskU.ap(), 1, [128, 8, 128]), bc(dtA.ap(), 2, [128, 8, 128]), ALU.mult,
                     [maskU.r(), dtA.r()], [Rt.r()])
                yield
                for hf in range(2):
                    K.mm(PX[hf].ap(), maskSL.ap(), Rt.ap()[:, hf * 4:(hf + 1) * 4, :].rearrange("p h i -> p (h i)"),
                         [maskSL.r(), Rt.r()], [PX[hf].r()])
                    yield
                for hf in range(2):
                    K.act(dec.ap()[:, hf * 4:(hf + 1) * 4, :].rearrange("p h i -> p (h i)"), PX[hf].ap(), AF.Exp,
                          [PX[hf].r()], [dec.r()])
                    yield
                K.cp(csb.ap(), PC.ap()[:, 8:24], [PC.r()], [csb.r()], eng="act")
                yield
                for g in range(2):
                    K.mm(PC.ap()[:, 256 + g * 128:256 + (g + 1) * 128], xc.ap()[64 * g:64 * g + 64, 4, :],
                         xc.ap()[64 * g:64 * g + 64, 5, :], [xc.r()], [PC.r()], self_wait=(g == 1))
                yield
                K.tt(Gs.ap(), PC.ap()[:, 256:512].rearrange("p (g i) -> p g i", g=2), bc(maskU.ap(), 1, [128, 2, 128]),
                     ALU.mult, [PC.r(), maskU.r()], [Gs.r()])
                yield
                K.tt(dec.ap().rearrange("p (g r) i -> p g r i", g=2), dec.ap().rearrange("p (g r) i -> p g r i", g=2),
                     bc(Gs.ap(), 2, [128, 2, 4, 128]), ALU.mult, [dec.r(), Gs.r()], [dec.r()])
                yield
                K.tt(Mb.ap(), dec.ap(), bc(dtt.ap(), 2, [128, 8, 128]), ALU.mult, [dec.r(), dtt.r()], [Mb.r()])
                yield
                for ct in range(5):
                    K.tr(PT.ap()[:, ct * 128:(ct + 1) * 128], xc.ap()[:, ct, :], identb.ap(), [xc.r(), identb.r()],
                         [PT.r()], inc=(ct == 4))
                yield
                K.cp(XT.ap(), PT.ap()[:, 0:640], [PT.r()], [XT.r()], eng="act")
                yield
                K.tt(xD.ap().rearrange("p (h q) -> p h q", h=8), XT.ap()[:, 0:512].rearrange("p (h q) -> p h q", h=8),
                     bc(dB.ap(), 2, [128, 8, 64]), ALU.mult, [XT.r(), dB.r()], [xD.r()])
                yield
                K.mm(PZ.ap(), identb.ap(), xD.ap(), [identb.r(), xD.r()], [PZ.r()], start=True, stop=False)
                for hh in range(8):
                    K.mm(PZ.ap()[:, hh * 64:(hh + 1) * 64], Mb.ap()[:, hh, :], XT.ap()[:, hh * 64:(hh + 1) * 64],
                         [Mb.r(), XT.r()], [PZ.r()], start=False, stop=(hh == 7))
                for g in range(2):
                    K.mm(PC.ap()[:, g * 256:(g + 1) * 256], xc.ap()[64 * g:64 * g + 64, 5, :],
                         HSb.ap()[64 * g:64 * g + 64, :, :].rearrange("p h q -> p (h q)"), [xc.r(), HSb.r()], [PC.r()],
                         self_wait=(g == 1))
                yield
                K.act(e1.ap(), csb.ap()[:, 0:8], AF.Exp, [csb.r()], [e1.r()])
                yield
                K.tt(t1.ap().rearrange("p (h q) -> p h q", h=8), PC.ap().rearrange("p (h q) -> p h q", h=8),
                     bc(e1.ap(), 2, [128, 8, 64]), ALU.mult, [PC.r(), e1.r()], [t1.r()])
                yield
                K.tt(t1.ap(), t1.ap(), PZ.ap(), ALU.add, [t1.r(), PZ.r()], [t1.r()])
                yield
                ssd_epilogue(K, C, t1, sz, ss, yn, 128)
                yield
                for q in range(4):
                    K.tr(PT.ap()[:, q * 128:(q + 1) * 128], yn.ap()[:, q * 128:(q + 1) * 128], identb.ap(),
                         [yn.r(), identb.r()], [PT.r()], inc=(q == 3))
                yield
                K.tt(yT.ap()[:, 0:4, c * 128:(c + 1) * 128], PT.ap()[:, 0:512].rearrange("p (t n) -> p t n", t=4),
                     bc(normg.ap(), 2, [128, 4, 128]), ALU.mult, [PT.r(), normg.r()], xr(yT, c // 4, range(4)))
                yield
                K.act(el.ap(), csb.ap()[:, 8:16], AF.Exp, [csb.r()], [el.r()])
                yield
                K.tt(tail.ap(), csb.ap()[:, 8:16], csb.ap()[:, 0:8], ALU.subtract, [csb.r()], [tail.r()])
                yield
                K.act(tail.ap(), tail.ap(), AF.Exp, [tail.r()], [tail.r()])
                yield
                K.tt(tail.ap(), tail.ap(), dtt.ap(), ALU.mult, [tail.r(), dtt.r()], [tail.r()])
                yield
                K.tt(xw.ap().rearrange("p (h q) -> p h q", h=8), XT.ap()[:, 0:512].rearrange("p (h q) -> p h q", h=8),
                     bc(tail.ap(), 2, [128, 8, 64]), ALU.mult, [XT.r(), tail.r()], [xw.r()])
                yield
                K.mm(PC.ap(), XT.ap()[:, 512:640], xw.ap(), [XT.r(), xw.r()], [PC.r()])
                yield
                for g in range(2):
                    sl = slice(64 * g, 64 * g + 64)
                    K.tt(HS32.ap()[sl], HS32.ap()[sl], bc(el.ap()[sl, 4 * g:4 * g + 4], 2, [64, 4, 64]), ALU.mult,
                         [HS32.r(), el.r()], [HS32.r()])
                    K.tt(HS32.ap()[sl], HS32.ap()[sl],
                         PC.ap()[sl, 256 * g:256 * g + 256].rearrange("p (h q) -> p h q", h=4), ALU.add,
                         [HS32.r(), PC.r()], [HS32.r()])
                    yield
                K.cp(HSb.ap(), HS32.ap(), [HS32.r()], [HSb.r()], eng="act")
                yield

            def gla_body(c):
                h = hc[c % 2]
                for (dst, cols) in ((PF.ap()[:, 0:128], slice(0, 128)), (PF.ap()[:, 128:256], slice(128, 256)),
                                    (PF.ap()[0:16, 256:384], slice(512, 528))):
                    for k in range(KT):
                        K.mm(dst, wing.ap()[:, k, cols], h.ap()[:, k, :], [wing.r(), h.r()], [PF.r()],
                             start=(k == 0), stop=(k == KT - 1))
                    yield
                for (dst, cols, pst) in ((PV.ap()[:, 0:256], slice(256, 512), PV), (PF.ap()[:, 384:512], slice(128, 256), PF),
                                         (PV.ap()[:, 256:512], slice(528, 784), PV)):
                    for k in range(KT):
                        K.mm(dst, h.ap()[:, k, :], wing.ap()[:, k, cols], [wing.r(), h.r()], [pst.r()],
                             start=(k == 0), stop=(k == KT - 1))
                    yield
                K.cp(glo.ap(), PF.ap()[0:16, 256:384], [PF.r()], [glo.r()], eng="act")
                yield
                K.mm(PL.ap()[:, 0:128], glo.ap(), wgk2.ap(), [glo.r(), wgk2.r()], [PL.r()])
                yield
                K.stt(lg.ap(), PL.ap()[:, 0:128], -1.0, bgkB.ap(), ALU.mult, ALU.subtract, [PL.r(), bgkB.r()], [lg.r()])
                yield
                softplus_(K, (lg.ap(), lg.r()), (lgt.ap(), lgt.r()), None, None)
                yield
                K.ts(lg.ap(), lg.ap(), -1.0 / 16.0, ALU.mult, [lg.r()], [lg.r()])
                yield
                K.mm(PL.ap()[:, 128:256], lg.ap(), maskU.ap(), [lg.r(), maskU.r()], [PL.r()])
                K.mm(PL.ap()[:, 256:384], maskU.ap(), lg.ap(), [lg.r(), maskU.r()], [PL.r()])
                yield
                K.act(Eq.ap(), PL.ap()[:, 128:256], AF.Exp, [PL.r()], [Eq.r()])
                yield
                K.act(Ek.ap(), PL.ap()[:, 128:256], AF.Exp, [PL.r()], [Ek.r()], scale=-1.0)
                yield
                K.act(Ekt.ap(), PL.ap()[:, 256:384], AF.Exp, [PL.r()], [Ekt.r()], scale=-1.0)
                yield
                K.stt(qt.ap(), PF.ap()[:, 0:128], 32.0 ** -0.5, Eq.ap(), ALU.mult, ALU.mult, [PF.r(), Eq.r()], [qt.r()])
                yield
                K.tt(kf.ap(), PF.ap()[:, 128:256], Ek.ap(), ALU.mult, [PF.r(), Ek.r()], [kf.r()])
                yield
                K.tt(km.ap(), bc(kf.ap(), 1, [128, 4, 128]), bc(hm.ap(), 2, [128, 4, 128]), ALU.mult, [kf.r(), hm.r()],
                     [km.r()])
                yield
                K.tt(ktm.ap(), PF.ap()[:, 384:512], Ekt.ap(), ALU.mult, [PF.r(), Ekt.r()], [ktm.r()])
                yield
                K.cp(vtm.ap(), PV.ap()[:, 0:256], [PV.r()], [vtm.r()], eng="act")
                yield
                K.act(sgg.ap(), PV.ap()[:, 256:512], AF.Silu, [PV.r()], [sgg.r()])
                yield
                for hh in range(4):
                    K.mm(PL.ap()[:, hh * 128:(hh + 1) * 128], km.ap()[:, hh, :], qt.ap(), [km.r(), qt.r()], [PL.r()],
                         inc=(hh == 3))
                yield
                K.tt(A.ap(), PL.ap().rearrange("p (h i) -> p h i", h=4), bc(maskU.ap(), 1, [128, 4, 128]), ALU.mult,
                     [PL.r(), maskU.r()], [A.r()])
                yield
                K.mm(PL.ap()[:, 0:256], qt.ap(), Sb.ap(), [qt.r(), Sb.r()], [PL.r()], start=True, stop=False)
                for hh in range(4):
                    K.mm(PL.ap()[:, hh * 64:(hh + 1) * 64], A.ap()[:, hh, :], vtm.ap()[:, hh * 64:(hh + 1) * 64],
                         [A.r(), vtm.r()], [PL.r()], start=False, stop=(hh == 3))
                yield
                K.act(osq.ap(), PL.ap()[:, 0:256], AF.Square, [PL.r()], [osq.r()])
                yield
                K.red(ms.ap(), osq.ap().rearrange("p (h e) -> p h e", h=4), [osq.r()], [ms.r()])
                yield
                K.act(ms.ap(), ms.ap(), AF.Sqrt, [ms.r()], [ms.r()], scale=1.0 / 64, bias=RMS_EPS)
                yield
                K.recip(ms.ap(), ms.ap(), [ms.r()], [ms.r()])
                yield
                K.tt(on.ap().rearrange("p (h e) -> p h e", h=4), PL.ap()[:, 0:256].rearrange("p (h e) -> p h e", h=4),
                     bc(ms.ap(), 2, [128, 4, 64]), ALU.mult, [PL.r(), ms.r()], [on.r()])
                yield
                K.tt(onb.ap(), on.ap(), sgg.ap(), ALU.mult, [on.r(), sgg.r()], [onb.r()])
                yield
                for q in range(2):
                    K.tr(PT.ap()[:, 768 + q * 128:768 + (q + 1) * 128], onb.ap()[:, q * 128:(q + 1) * 128], identb.ap(),
                         [onb.r(), identb.r()], [PT.r()], inc=(q == 1))
                yield
                K.ts(yT.ap()[:, 6:8, c * 128:(c + 1) * 128], PT.ap()[:, 768:1024].rearrange("p (t n) -> p t n", t=2),
                     gcol.ap(), ALU.mult, [PT.r(), gcol.r()], xr(yT, c // 4, range(6, 8)))
                yield
                K.mm(PL.ap()[:, 256:512], ktm.ap(), vtm.ap(), [ktm.r(), vtm.r()], [PL.r()])
                yield
                K.tt(tmpS.ap(), PL.ap()[:, 256:512], BM.ap(), ALU.mult, [PL.r(), BM.r()], [tmpS.r()])
                yield
                K.tt(S32.ap(), S32.ap(), tmpS.ap(), ALU.add, [S32.r(), tmpS.r()], [S32.r()])
                yield
                K.ts(S32.ap(), S32.ap(), Eq.ap()[:, 127:128], ALU.mult, [S32.r(), Eq.r()], [S32.r()])
                yield
                K.cp(Sb.ap(), S32.ap(), [S32.r()], [Sb.r()], eng="act")
                yield

            for c in range(NCH):
                make_hc(K, C, l, hc[c % 2], c)
                interleave([ssd_body(c), gla_body(c)], ratio=[3, 2])

            xb = XB[(NCH - 1) % 2]
            for i in range(3):
                K.dma(dr["p_conv"][l, i].rearrange("(t p) -> p t", p=128), xb.ap()[:, :, 128 + i], r=[xb.r()])
            hsT = t1
            for q in range(2):
                K.tr(PZ.ap()[:, q * 128:(q + 1) * 128], HS32.ap().rearrange("p h q -> p (h q)")[:, q * 128:(q + 1) * 128],
                     identf.ap(), [HS32.r(), identf.r()], [PZ.r()], inc=(q == 1))
            K.cp(hsT.ap()[:, 0:256], PZ.ap()[:, 0:256], [PZ.r()], [hsT.r()])
            for g in range(2):
                for q in range(2):
                    K.dma(dr["p_ssd"][l, 4 * g + 2 * q:4 * g + 2 * q + 2].rearrange("h p n -> (h p) n"),
                          hsT.ap()[:, q * 128 + 64 * g:q * 128 + 64 * g + 64], r=[hsT.r()])
            for hh in range(4):
                K.dma(dr["p_gla"][l, hh], S32.ap()[32 * hh:32 * hh + 32, 64 * hh:64 * hh + 64], r=[S32.r()])
            P.barrier()
        ssd_sample(K, dr, C, l, yT, win, aB, dB, dtbB, dbg_out)
        gla_sample(K, dr, C, l, yT, wing, wgk2, bgkB)


def dram_scratch(K, name, shape):
    K.uid += 1
    h = K.nc.dram_tensor(f"scr_{name}_{K.uid}", list(shape), F32)
    return Tn(h, name)


def ssd_sample(K, dr, C, l, yT, win, aB, dB, dtbB, dbg_out):
    P = K.P
    hs, identb = C["hs"], C["identb"]
    with contextlib.ExitStack() as ph:
        cs = K.sb(ph, "cs", [NS, 768], F32)
        wB = K.sb(ph, "wB", [NS, 768], F32)
        gB = K.sb(ph, "gB", [NS, 512], F32)
        xbcs = K.sb(ph, "xbcs", [NS, 768], F32)
        acc = K.sb(ph, "acc", [NS, 768], F32)
        tmpc = K.sb(ph, "tmpc", [NS, 768], F32)
        szs = K.sb(ph, "szs", [NS, 512], F32)
        dts = K.sb(ph, "dts", [NS, 8], F32)
        dtm = K.sb(ph, "dtm", [NS, 8], F32)
        rep = K.sb(ph, "rep", [NS, 2, 8, 64], F32)
        pk = K.sb(ph, "pk", [NS, 8, 3], F32)
        Hs = K.sb(ph, "Hs", [128, 64, 64], F32)
        tmpH = K.sb(ph, "tmpH", [128, 32, 64], F32)
        xh = K.sb(ph, "xh", [128, 64], F32)
        BCh = K.sb(ph, "BCh", [128, 2, 64], F32)
        pkh = K.sb(ph, "pkh", [128, 3], F32)
        dA = K.sb(ph, "dA", [128, 1], F32)
        xdt = K.sb(ph, "xdt", [128, 64], F32)
        yh = K.sb(ph, "yh", [128, 64], F32)
        ysm = K.sb(ph, "ysm", [NS, 512], F32)
        yns = K.sb(ph, "yns", [NS, 512], BF16)
        sss = K.sb(ph, "sss", [NS, 1], F32)
        ps_a = K.ps(ph, "pss_a", [128, 512], F32)
        ps_b = K.ps(ph, "pss_b", [128, 512], F32)
        ps_d = K.ps(ph, "pss_d", [128, 512], F32)
        ps_t = K.ps(ph, "pss_t", [128, 1024], BF16)
        sx = dram_scratch(K, "sx", [NS, 512])
        sbc = dram_scratch(K, "sbc", [2, NS, 512])
        spk = dram_scratch(K, "spk", [NS, 24])
        sy = dram_scratch(K, "sy", [NS, 512])

        K.dma(gB.ap(), dr["ssd_norm_g"][l:l + 1, :].to_broadcast([NS, 512]), w=[gB.r()])
        K.dma(Hs.ap().rearrange("p a b -> p (a b)"), dr["st_ssd"][l].rearrange("b h p n -> (b h) (p n)"), w=[Hs.r()])
        for k in range(KT):
            K.mm(ps_a.ap()[0:NS, :], hs.ap()[:, k, :], win.ap()[:, k, 0:512], [hs.r(), win.r()], [ps_a.r()],
                 start=(k == 0), stop=(k == KT - 1))
        for k in range(KT):
            K.mm(ps_b.ap()[0:NS, :], hs.ap()[:, k, :], win.ap()[:, k, 512:1024], [hs.r(), win.r()], [ps_b.r()],
                 start=(k == 0), stop=(k == KT - 1))
        for k in range(KT):
            K.mm(ps_d.ap()[0:NS, 0:264], hs.ap()[:, k, :], win.ap()[:, k, 1024:1288], [hs.r(), win.r()], [ps_d.r()],
                 start=(k == 0), stop=(k == KT - 1))
        K.act(szs.ap(), ps_a.ap()[0:NS, :], AF.Silu, [ps_a.r()], [szs.r()])
        K.cp(xbcs.ap()[:, 0:512], ps_b.ap()[0:NS, :], [ps_b.r()], [xbcs.r()], eng="act")
        K.cp(xbcs.ap()[:, 512:768], ps_d.ap()[0:NS, 0:256], [ps_d.r()], [xbcs.r()], eng="act")
        K.tt(dts.ap(), ps_d.ap()[0:NS, 256:264], dtbB.ap()[0:NS, :], ALU.add, [ps_d.r(), dtbB.r()], [dts.r()])
        softplus_(K, (dts.ap(), dts.r()), (dtm.ap(), dtm.r()), None, None)
        K.dma(wB.ap(), dr["ssd_conv_w"][l, 3:4, :].to_broadcast([NS, 768]), w=[wB.r()])
        K.tt(acc.ap(), xbcs.ap(), wB.ap(), ALU.mult, [xbcs.r(), wB.r()], [acc.r()])
        for i in range(3):
            K.dma(wB.ap(), dr["ssd_conv_w"][l, i:i + 1, :].to_broadcast([NS, 768]), w=[wB.r()])
            K.dma(cs.ap(), dr["st_conv"][l][:, i, :], w=[cs.r()])
            K.tt(tmpc.ap(), cs.ap(), wB.ap(), ALU.mult, [cs.r(), wB.r()], [tmpc.r()])
            K.tt(acc.ap(), acc.ap(), tmpc.ap(), ALU.add, [acc.r(), tmpc.r()], [acc.r()])
        K.dma(wB.ap(), dr["ssd_conv_b"][l:l + 1, :].to_broadcast([NS, 768]), w=[wB.r()])
        K.tt(acc.ap(), acc.ap(), wB.ap(), ALU.add, [acc.r(), wB.r()], [acc.r()])
        K.act(acc.ap(), acc.ap(), AF.Silu, [acc.r()], [acc.r()])
        K.dma(dr["s_conv"][l][:, 0:2, :], dr["st_conv"][l][:, 1:3, :])
        K.dma(dr["s_conv"][l][:, 2, :], xbcs.ap(), r=[xbcs.r()])
        K.dma(sx.ap(), acc.ap()[:, 0:512], r=[acc.r()], w=[sx.r()])
        K.cp(rep.ap().rearrange("p t (g r) n -> p t g r n", g=2),
             bc(acc.ap()[:, 512:768].rearrange("p (t g n) -> p t g n", t=2, g=2), 3, [NS, 2, 2, 4, 64]),
             [acc.r()], [rep.r()])
        K.cp(pk.ap()[:, :, 0], dts.ap(), [dts.r()], [pk.r()])
        K.cp(pk.ap()[:, :, 1], aB.ap()[0:NS, :], [aB.r()], [pk.r()])
        K.cp(pk.ap()[:, :, 2], dB.ap()[0:NS, :], [dB.r()], [pk.r()])
        K.dma(sbc.ap().rearrange("t b x -> b t x"), rep.ap().rearrange("p t h n -> p t (h n)"), r=[rep.r()],
              w=[sbc.r()])
        K.dma(spk.ap(), pk.ap().rearrange("p h q -> p (h q)"), r=[pk.r()], w=[spk.r()])
        K.dma(xh.ap(), sx.ap().rearrange("b (h p) -> (b h) p", h=8), r=[sx.r()], w=[xh.r()])
        for t in range(2):
            K.dma(BCh.ap()[:, t, :], sbc.ap()[t].rearrange("b (h n) -> (b h) n", h=8), r=[sbc.r()],
                  w=[BCh.r()])
        K.dma(pkh.ap(), spk.ap().rearrange("b (h q) -> (b h) q", h=8), r=[spk.r()], w=[pkh.r()])
        K.act(dA.ap(), pkh.ap()[:, 0:1], AF.Exp, [pkh.r()], [dA.r()], scale=pkh.ap()[:, 1:2])
        K.ts(xdt.ap(), xh.ap(), pkh.ap()[:, 0:1], ALU.mult, [xh.r(), pkh.r()], [xdt.r()])
        K.ts(Hs.ap(), Hs.ap(), dA.ap(), ALU.mult, [Hs.r(), dA.r()], [Hs.r()])
        for hf in range(2):
            sl = slice(32 * hf, 32 * hf + 32)
            K.tt(tmpH.ap(), bc(xdt.ap()[:, sl], 2, [128, 32, 64]), bc(BCh.ap()[:, 0, :], 1, [128, 32, 64]), ALU.mult,
                 [xdt.r(), BCh.r()], [tmpH.r()])
            K.tt(Hs.ap()[:, sl, :], Hs.ap()[:, sl, :], tmpH.ap(), ALU.add, [Hs.r(), tmpH.r()], [Hs.r()])
        K.dma(dr["s_ssd"][l].rearrange("b h p n -> (b h) (p n)"), Hs.ap().rearrange("p a b -> p (a b)"), r=[Hs.r()])
        for hf in range(2):
            sl = slice(32 * hf, 32 * hf + 32)
            K.tt(tmpH.ap(), Hs.ap()[:, sl, :], bc(BCh.ap()[:, 1, :], 1, [128, 32, 64]), ALU.mult, [Hs.r(), BCh.r()],
                 [tmpH.r()])
            K.red(yh.ap()[:, sl], tmpH.ap(), [tmpH.r()], [yh.r()])
        K.stt(yh.ap(), xh.ap(), pkh.ap()[:, 2:3], yh.ap(), ALU.mult, ALU.add, [xh.r(), pkh.r(), yh.r()], [yh.r()])
        K.dma(sy.ap().rearrange("b (h p) -> (b h) p", h=8), yh.ap(), r=[yh.r()], w=[sy.r()])
        K.dma(ysm.ap(), sy.ap(), r=[sy.r()], w=[ysm.r()])
        ssd_epilogue(K, C, ysm, szs, sss, yns, NS)
        K.tt(ysm.ap(), ysm.ap(), gB.ap(), ALU.mult, [ysm.r(), gB.r()], [ysm.r()])
        K.ts(yns.ap(), ysm.ap(), sss.ap(), ALU.mult, [ysm.r(), sss.r()], [yns.r()])
        for q in range(4):
            K.tr(ps_t.ap()[:, q * NS:(q + 1) * NS], yns.ap()[:, q * 128:(q + 1) * 128], identb.ap()[0:NS, 0:NS],
                 [yns.r(), identb.r()], [ps_t.r()], inc=(q == 3))
        K.cp(yT.ap()[:, 0:4, T:T + NS], ps_t.ap()[:, 0:4 * NS].rearrange("p (t n) -> p t n", t=4), [ps_t.r()],
             xr(yT, 4, range(4)))
        P.barrier()


def gla_phase(K, dr, C, l, yT, dbg_out):
    P = K.P
    identb, maskU = C["identb"], C["maskU"]
    hc, hs = C["hc"], C["hs"]
    G0 = OFF["gq"]
    with contextlib.ExitStack() as ph:
        win = K.sb(ph, "win_gla", [128, KT, 784], BF16)
        K.dma(win.ap(), dr["w_in"][l, :, G0:G0 + 784].rearrange("(k p) n -> p k n", p=128), w=[win.r()], q="pool")
        wgk2 = K.sb(ph, "wgk2", [16, 128], BF16)
        K.dma(wgk2.ap(), dr["gla_w_gk2"][l], w=[wgk2.r()], q="pool")
        bgkB = K.sb(ph, "bgkB", [128, 128], F32)
        K.dma(bgkB.ap(), dr["gla_b_gk"][l:l + 1, :].to_broadcast([128, 128]), w=[bgkB.r()])
        gcol = K.sb(ph, "gcol", [128, 1], F32)
        for t in range(2):
            K.dma(gcol.ap()[64 * t:64 * t + 64, :], dr["gla_norm_g"][l].rearrange("(e o) -> e o", o=1), w=[gcol.r()])
        BM = K.sb(ph, "BM", [128, 256], F32)
        hm = K.sb(ph, "hm", [128, 4], F32)
        K.memset(BM.ap(), 1.0, [BM.r()], eng="pool")
        K.memset(hm.ap(), 1.0, [hm.r()], eng="pool")
        for hh in range(4):
            for (t, sl, n) in ((BM, slice(64 * hh, 64 * hh + 64), 64), (hm, slice(hh, hh + 1), 1)):
                ap = t.ap()[:, sl]
                K.P.op("pool", lambda e, ap=ap, n=n, hh=hh: e.affine_select(
                    out=ap, in_=ap, pattern=[[0, n]], compare_op=ALU.is_ge, fill=0.0, base=-32 * hh,
                    channel_multiplier=1), reads=[t.r()], writes=[t.r()])
                K.P.op("pool", lambda e, ap=ap, n=n, hh=hh: e.affine_select(
                    out=ap, in_=ap, pattern=[[0, n]], compare_op=ALU.is_gt, fill=0.0, base=32 * hh + 32,
                    channel_multiplier=-1), reads=[t.r()], writes=[t.r()])
        gla_prompt(K, dr, C, l, yT, win, wgk2, bgkB, gcol, BM, hm)
        P.barrier()
        gla_sample(K, dr, C, l, yT, win, wgk2, bgkB)


def gla_prompt(K, dr, C, l, yT, win, wgk2, bgkB, gcol, BM, hm):
    P = K.P
    identb, maskU = C["identb"], C["maskU"]
    hc = C["hc"]
    with contextlib.ExitStack() as ph:
        glo = K.sb(ph, "glo", [16, 128], BF16)
        lg = K.sb(ph, "lg", [128, 128], F32)
        lgt = K.sb(ph, "lgt", [128, 128], F32)
        Eq = K.sb(ph, "Eq", [128, 128], F32)
        Ek = K.sb(ph, "Ek", [128, 128], F32)
        Ekt = K.sb(ph, "Ekt", [128, 128], F32)
        qt = K.sb(ph, "qt", [128, 128], BF16)
        kf = K.sb(ph, "kf", [128, 128], F32)
        km = K.sb(ph, "km", [128, 4, 128], BF16)
        ktm = K.sb(ph, "ktm", [128, 128], BF16)
        vtm = K.sb(ph, "vtm", [128, 256], BF16)
        sgg = K.sb(ph, "sgg", [128, 256], F32)
        A = K.sb(ph, "A", [128, 4, 128], BF16)
        osq = K.sb(ph, "osq", [128, 256], F32)
        ms = K.sb(ph, "ms", [128, 4], F32)
        on = K.sb(ph, "on", [128, 256], F32)
        onb = K.sb(ph, "onb", [128, 256], BF16)
        tmpS = K.sb(ph, "tmpS", [128, 256], F32)
        S32 = K.sb(ph, "S32", [128, 256], F32)
        Sb = K.sb(ph, "Sb", [128, 256], BF16)
        ps_f = K.ps(ph, "psg_f", [128, 512], F32)
        ps_m = K.ps(ph, "psg_m", [128, 512], F32)
        ps_g = K.ps(ph, "psg_g", [128, 512], F32)
        ps_l = K.ps(ph, "psg_l", [128, 512], F32)
        ps_a = K.ps(ph, "psg_a", [128, 512], F32)
        ps_o = K.ps(ph, "psg_o", [128, 512], F32)
        ps_t = K.ps(ph, "psg_t", [128, 1024], BF16)
        K.memset(S32.ap(), 0.0, [S32.r()])
        K.memset(Sb.ap(), 0.0, [Sb.r()])
        for c in range(NCH):
            h = hc[c % 2]
            make_hc(K, C, l, h, c)
            for (dst, cols, M) in ((ps_f.ap()[:, 0:128], slice(0, 128), 128), (ps_f.ap()[:, 128:256], slice(128, 256), 128),
                                   (ps_f.ap()[0:16, 256:384], slice(512, 528), 16)):
                for k in range(KT):
                    K.mm(dst, win.ap()[:, k, cols], h.ap()[:, k, :], [win.r(), h.r()], [ps_f.r()],
                         start=(k == 0), stop=(k == KT - 1))
            for (dst, cols, pst) in ((ps_m.ap()[:, 0:256], slice(256, 512), ps_m), (ps_m.ap()[:, 256:384], slice(128, 256), ps_m),
                                     (ps_g.ap()[:, 0:256], slice(528, 784), ps_g)):
                for k in range(KT):
                    K.mm(dst, h.ap()[:, k, :], win.ap()[:, k, cols], [win.r(), h.r()], [pst.r()],
                         start=(k == 0), stop=(k == KT - 1))
            K.cp(glo.ap(), ps_f.ap()[0:16, 256:384], [ps_f.r()], [glo.r()], eng="act")
            K.mm(ps_l.ap()[:, 0:128], glo.ap(), wgk2.ap(), [glo.r(), wgk2.r()], [ps_l.r()])
            K.stt(lg.ap(), ps_l.ap()[:, 0:128], -1.0, bgkB.ap(), ALU.mult, ALU.subtract, [ps_l.r(), bgkB.r()], [lg.r()])
            softplus_(K, (lg.ap(), lg.r()), (lgt.ap(), lgt.r()), None, None)
            K.ts(lg.ap(), lg.ap(), -1.0 / 16.0, ALU.mult, [lg.r()], [lg.r()])
            K.mm(ps_l.ap()[:, 128:256], lg.ap(), maskU.ap(), [lg.r(), maskU.r()], [ps_l.r()])
            K.mm(ps_l.ap()[:, 256:384], maskU.ap(), lg.ap(), [lg.r(), maskU.r()], [ps_l.r()])
            K.act(Eq.ap(), ps_l.ap()[:, 128:256], AF.Exp, [ps_l.r()], [Eq.r()])
            K.act(Ek.ap(), ps_l.ap()[:, 128:256], AF.Exp, [ps_l.r()], [Ek.r()], scale=-1.0)
            K.act(Ekt.ap(), ps_l.ap()[:, 256:384], AF.Exp, [ps_l.r()], [Ekt.r()], scale=-1.0)
            K.stt(qt.ap(), ps_f.ap()[:, 0:128], 32.0 ** -0.5, Eq.ap(), ALU.mult, ALU.mult, [ps_f.r(), Eq.r()], [qt.r()])
            K.tt(kf.ap(), ps_f.ap()[:, 128:256], Ek.ap(), ALU.mult, [ps_f.r(), Ek.r()], [kf.r()])
            K.tt(km.ap(), bc(kf.ap(), 1, [128, 4, 128]), bc(hm.ap(), 2, [128, 4, 128]), ALU.mult, [kf.r(), hm.r()],
                 [km.r()])
            K.tt(ktm.ap(), ps_m.ap()[:, 256:384], Ekt.ap(), ALU.mult, [ps_m.r(), Ekt.r()], [ktm.r()])
            K.cp(vtm.ap(), ps_m.ap()[:, 0:256], [ps_m.r()], [vtm.r()], eng="act")
            K.act(sgg.ap(), ps_g.ap()[:, 0:256], AF.Silu, [ps_g.r()], [sgg.r()])
            for hh in range(4):
                K.mm(ps_a.ap()[:, hh * 128:(hh + 1) * 128], km.ap()[:, hh, :], qt.ap(), [km.r(), qt.r()], [ps_a.r()],
                     inc=(hh == 3))
            K.tt(A.ap(), ps_a.ap().rearrange("p (h i) -> p h i", h=4), bc(maskU.ap(), 1, [128, 4, 128]), ALU.mult,
                 [ps_a.r(), maskU.r()], [A.r()])
            K.mm(ps_o.ap()[:, 0:256], qt.ap(), Sb.ap(), [qt.r(), Sb.r()], [ps_o.r()], start=True, stop=False)
            for hh in range(4):
                K.mm(ps_o.ap()[:, hh * 64:(hh + 1) * 64], A.ap()[:, hh, :], vtm.ap()[:, hh * 64:(hh + 1) * 64],
                     [A.r(), vtm.r()], [ps_o.r()], start=False, stop=(hh == 3))
            K.act(osq.ap(), ps_o.ap()[:, 0:256], AF.Square, [ps_o.r()], [osq.r()])
            K.red(ms.ap(), osq.ap().rearrange("p (h e) -> p h e", h=4), [osq.r()], [ms.r()])
            K.act(ms.ap(), ms.ap(), AF.Sqrt, [ms.r()], [ms.r()], scale=1.0 / 64, bias=RMS_EPS)
            K.recip(ms.ap(), ms.ap(), [ms.r()], [ms.r()])
            K.tt(on.ap().rearrange("p (h e) -> p h e", h=4), ps_o.ap()[:, 0:256].rearrange("p (h e) -> p h e", h=4),
                 bc(ms.ap(), 2, [128, 4, 64]), ALU.mult, [ps_o.r(), ms.r()], [on.r()])
            K.tt(onb.ap(), on.ap(), sgg.ap(), ALU.mult, [on.r(), sgg.r()], [onb.r()])
            for q in range(2):
                K.tr(ps_t.ap()[:, q * 128:(q + 1) * 128], onb.ap()[:, q * 128:(q + 1) * 128], identb.ap(),
                     [onb.r(), identb.r()], [ps_t.r()], inc=(q == 1))
            K.ts(yT.ap()[:, 6:8, c * 128:(c + 1) * 128], ps_t.ap()[:, 0:256].rearrange("p (t n) -> p t n", t=2),
                 gcol.ap(), ALU.mult, [ps_t.r(), gcol.r()], xr(yT, c // 4, range(6, 8)))
            K.mm(ps_o.ap()[:, 256:512], ktm.ap(), vtm.ap(), [ktm.r(), vtm.r()], [ps_o.r()])
            K.tt(tmpS.ap(), ps_o.ap()[:, 256:512], BM.ap(), ALU.mult, [ps_o.r(), BM.r()], [tmpS.r()])
            K.tt(S32.ap(), S32.ap(), tmpS.ap(), ALU.add, [S32.r(), tmpS.r()], [S32.r()])
            K.ts(S32.ap(), S32.ap(), Eq.ap()[:, 127:128], ALU.mult, [S32.r(), Eq.r()], [S32.r()])
            K.cp(Sb.ap(), S32.ap(), [S32.r()], [Sb.r()], eng="act")
        for hh in range(4):
            K.dma(dr["p_gla"][l, hh], S32.ap()[32 * hh:32 * hh + 32, 64 * hh:64 * hh + 64], r=[S32.r()])
        P.barrier()


def gla_sample(K, dr, C, l, yT, win, wgk2, bgkB):
    P = K.P
    hs, identb = C["hs"], C["identb"]
    with contextlib.ExitStack() as ph:
        glo = K.sb(ph, "glos", [16, NS], BF16)
        lg = K.sb(ph, "lgs", [NS, 128], F32)
        lgt = K.sb(ph, "lgts", [NS, 128], F32)
        pk = K.sb(ph, "pkg", [NS, 4, 160], F32)
        sgg = K.sb(ph, "sggs", [NS, 256], F32)
        gB = K.sb(ph, "gBg", [64, 64], F32)
        S = K.sb(ph, "Sg", [64, 32, 64], F32)
        tmp = K.sb(ph, "tmpg", [64, 32, 64], F32)
        pkh = K.sb(ph, "pkhg", [64, 160], F32)
        o = K.sb(ph, "og", [64, 64], F32)
        junk = K.sb(ph, "junkg", [64, 64], F32)
        ss = K.sb(ph, "ssg", [64, 1], F32)
        otm = K.sb(ph, "otm", [NS, 256], F32)
        otb = K.sb(ph, "otb", [NS, 256], BF16)
        ps_a = K.ps(ph, "psgs_a", [128, 512], F32)
        ps_b = K.ps(ph, "psgs_b", [128, 512], F32)
        ps_c = K.ps(ph, "psgs_c", [128, 512], F32)
        ps_t = K.ps(ph, "psgs_t", [128, 1024], BF16)
        spk = dram_scratch(K, "gpk", [NS, 640])
        so = dram_scratch(K, "go", [NS, 256])
        K.dma(gB.ap(), dr["gla_norm_g"][l:l + 1, :].to_broadcast([64, 64]), w=[gB.r()])
        K.dma(S.ap().rearrange("p d e -> p (d e)"), dr["st_gla"][l].rearrange("b h d e -> (b h) (d e)"), w=[S.r()])
        for k in range(KT):
            K.mm(ps_a.ap()[0:NS, :], hs.ap()[:, k, :], win.ap()[:, k, 0:512], [hs.r(), win.r()], [ps_a.r()],
                 start=(k == 0), stop=(k == KT - 1))
        for k in range(KT):
            K.mm(ps_b.ap()[0:NS, 0:256], hs.ap()[:, k, :], win.ap()[:, k, 528:784], [hs.r(), win.r()], [ps_b.r()],
                 start=(k == 0), stop=(k == KT - 1))
        for k in range(KT):
            K.mm(ps_c.ap()[0:16, 0:NS], win.ap()[:, k, 512:528], hs.ap()[:, k, :], [hs.r(), win.r()], [ps_c.r()],
                 start=(k == 0), stop=(k == KT - 1))
        K.cp(glo.ap(), ps_c.ap()[0:16, 0:NS], [ps_c.r()], [glo.r()], eng="act")
        K.mm(ps_c.ap()[0:NS, 128:256], glo.ap(), wgk2.ap(), [glo.r(), wgk2.r()], [ps_c.r()])
        K.stt(lg.ap(), ps_c.ap()[0:NS, 128:256], -1.0, bgkB.ap()[0:NS, :], ALU.mult, ALU.subtract,
              [ps_c.r(), bgkB.r()], [lg.r()])
        softplus_(K, (lg.ap(), lg.r()), (lgt.ap(), lgt.r()), None, None)
        K.act(lg.ap(), lg.ap(), AF.Exp, [lg.r()], [lg.r()], scale=-1.0 / 16.0)
        K.act(sgg.ap(), ps_b.ap()[0:NS, 0:256], AF.Silu, [ps_b.r()], [sgg.r()])
        K.ts(pk.ap()[:, :, 0:32], ps_a.ap()[0:NS, 0:128].rearrange("p (h d) -> p h d", h=4), 32.0 ** -0.5, ALU.mult,
             [ps_a.r()], [pk.r()])
        K.cp(pk.ap()[:, :, 32:64], ps_a.ap()[0:NS, 128:256].rearrange("p (h d) -> p h d", h=4), [ps_a.r()], [pk.r()])
        K.cp(pk.ap()[:, :, 64:96], lg.ap().rearrange("p (h d) -> p h d", h=4), [lg.r()], [pk.r()])
        K.cp(pk.ap()[:, :, 96:160], ps_a.ap()[0:NS, 256:512].rearrange("p (h e) -> p h e", h=4), [ps_a.r()], [pk.r()])
        K.dma(spk.ap(), pk.ap().rearrange("p h x -> p (h x)"), r=[pk.r()], w=[spk.r()])
        K.dma(pkh.ap(), spk.ap().rearrange("b (h x) -> (b h) x", h=4), r=[spk.r()], w=[pkh.r()])
        qh, kh, eh, vh = pkh.ap()[:, 0:32], pkh.ap()[:, 32:64], pkh.ap()[:, 64:96], pkh.ap()[:, 96:160]
        K.tt(S.ap(), S.ap(), bc(eh, 2, [64, 32, 64]), ALU.mult, [S.r(), pkh.r()], [S.r()])
        K.tt(tmp.ap(), bc(kh, 2, [64, 32, 64]), bc(vh, 1, [64, 32, 64]), ALU.mult, [pkh.r()], [tmp.r()])
        K.tt(S.ap(), S.ap(), tmp.ap(), ALU.add, [S.r(), tmp.r()], [S.r()])
        K.dma(dr["s_gla"][l].rearrange("b h d e -> (b h) (d e)"), S.ap().rearrange("p d e -> p (d e)"), r=[S.r()])
        K.tt(tmp.ap(), S.ap(), bc(qh, 2, [64, 32, 64]), ALU.mult, [S.r(), pkh.r()], [tmp.r()])
        K.red(o.ap(), tmp.ap().rearrange("p d e -> p e d"), [tmp.r()], [o.r()])
        K.act(junk.ap(), o.ap(), AF.Square, [o.r()], [junk.r(), ss.r()], accum_out=ss.ap())
        K.act(ss.ap(), ss.ap(), AF.Sqrt, [ss.r()], [ss.r()], scale=1.0 / 64, bias=RMS_EPS)
        K.recip(ss.ap(), ss.ap(), [ss.r()], [ss.r()])
        K.stt(o.ap(), o.ap(), ss.ap(), gB.ap(), ALU.mult, ALU.mult, [o.r(), ss.r(), gB.r()], [o.r()])
        K.dma(so.ap().rearrange("b (h e) -> (b h) e", h=4), o.ap(), r=[o.r()], w=[so.r()])
        K.dma(otm.ap(), so.ap(), r=[so.r()], w=[otm.r()])
        K.tt(otb.ap(), otm.ap(), sgg.ap(), ALU.mult, [otm.r(), sgg.r()], [otb.r()])
        for q in range(2):
            K.tr(ps_t.ap()[:, q * NS:(q + 1) * NS], otb.ap()[:, q * 128:(q + 1) * 128], identb.ap()[0:NS, 0:NS],
                 [otb.r(), identb.r()], [ps_t.r()], inc=(q == 1))
        K.cp(yT.ap()[:, 6:8, T:T + NS], ps_t.ap()[:, 0:2 * NS].rearrange("p (t n) -> p t n", t=2), [ps_t.r()],
             xr(yT, 4, range(6, 8)))
        P.barrier()


C0 = float(np.exp(-0.5))


def rwkv_prep(K, C, pc, LW, N, rw, prev, B, pl, pg, pn):
    blk64 = C["blk64"]
    MX, LI = B["MX"], B["LI"]
    mxa = MX.ap()[:, :, 0:N]
    K.tt(mxa, prev, rw, ALU.subtract, B["_rw_res"], [MX.r()])
    yield
    K.tt(mxa, mxa, bc(pc["mu"].ap(), 2, [128, 7, N]), ALU.mult, [MX.r(), pc["mu"].r()], [MX.r()])
    yield
    K.tt(mxa, mxa, rw, ALU.add, [MX.r()] + B["_rw_res"], [MX.r()])
    yield
    r, k, v = (MX.ap()[:, 0:2, 0:N], MX.ap()[:, 2:4, 0:N], MX.ap()[:, 4:6, 0:N])
    lia = LI.ap()[:, 0:N]
    K.act(lia[0:32], MX.ap()[0:32, 6, 0:N], AF.Tanh, [MX.r()], [LI.r()])
    yield
    K.cp(lia[32:64], MX.ap()[32:64, 6, 0:N], [MX.r()], [LI.r()], eng="act")
    yield
    K.act(lia[64:128], MX.ap()[64:128, 6, 0:N], AF.Sigmoid, [MX.r()], [LI.r()])
    yield
    for t in range(2):
        cs = slice(t * 128, (t + 1) * 128)
        K.mm(pl.ap()[:, t * N:(t + 1) * N], LW.ap()[0:32, cs], lia[0:32], [LW.r(), LI.r()], [pl.r()], self_wait=True)
        K.mm(pl.ap()[:, (2 + t) * N:(3 + t) * N], LW.ap()[32:64, cs], lia[32:64], [LW.r(), LI.r()], [pl.r()],
             self_wait=True)
        K.mm(pg.ap()[:, t * N:(t + 1) * N], LW.ap()[64:128, cs], lia[64:128], [LW.r(), LI.r()], [pg.r()],
             self_wait=True)
    g = lambda n: B[n].ap()[:, :, 0:N]
    for t in range(2):
        K.act(B["sig"].ap()[:, t, 0:N], pl.ap()[:, t * N:(t + 1) * N], AF.Sigmoid, [pl.r(), pc["w0"].r()],
              [B["sig"].r()], bias=pc["w0"].ap()[:, t:t + 1])
        K.act(B["aic"].ap()[:, t, 0:N], pl.ap()[:, (2 + t) * N:(3 + t) * N], AF.Sigmoid, [pl.r(), pc["a0"].r()],
              [B["aic"].r()], bias=pc["a0"].ap()[:, t:t + 1])
    K.cp(g("gate"), pg.ap()[:, 0:2 * N].rearrange("p (t n) -> p t n", t=2), [pg.r()], [B["gate"].r()], eng="act")
    yield
    K.tt(g("kk"), k, bc(pc["k_k"].ap(), 2, [128, 2, N]), ALU.mult, [MX.r(), pc["k_k"].r()], [B["kk"].r()])
    yield
    K.tt(g("t1"), g("kk"), g("kk"), ALU.mult, [B["kk"].r()], [B["t1"].r()])
    yield
    for t in range(2):
        K.mm(pn.ap()[:, t * N:(t + 1) * N], blk64.ap(), B["t1"].ap()[:, t, 0:N], [blk64.r(), B["t1"].r()], [pn.r()])
    K.act(g("t1"), pn.ap()[:, 0:2 * N].rearrange("p (t n) -> p t n", t=2), AF.Sqrt, [pn.r()], [B["t1"].r()],
          bias=1e-12)
    K.recip(g("t1"), g("t1"), [B["t1"].r()], [B["t1"].r()])
    yield
    K.tt(g("kk"), g("kk"), g("t1"), ALU.mult, [B["kk"].r(), B["t1"].r()], [B["kk"].r()])
    yield
    yield
    K.tt(g("t1"), g("aic"), bc(pc["k_a"].ap(), 2, [128, 2, N]), ALU.mult, [B["aic"].r(), pc["k_a"].r()], [B["t1"].r()])
    yield
    K.tt(g("t1"), g("t1"), bc(pc["omka"].ap(), 2, [128, 2, N]), ALU.add, [B["t1"].r(), pc["omka"].r()], [B["t1"].r()])
    yield
    K.tt(g("kp"), k, g("t1"), ALU.mult, [MX.r(), B["t1"].r()], [B["kp"].r()])
    yield
    K.tt(g("t1"), r, g("kp"), ALU.mult, [MX.r(), B["kp"].r()], [B["t1"].r()])
    yield
    K.tt(g("t1"), g("t1"), bc(pc["r_k"].ap(), 2, [128, 2, N]), ALU.mult, [B["t1"].r(), pc["r_k"].r()], [B["t1"].r()])
    yield
    for t in range(2):
        K.mm(pn.ap()[:, t * N:(t + 1) * N], blk64.ap(), B["t1"].ap()[:, t, 0:N], [blk64.r(), B["t1"].r()], [pn.r()])
    K.tt(g("bonus"), pn.ap()[:, 0:2 * N].rearrange("p (t n) -> p t n", t=2), v, ALU.mult, [pn.r(), MX.r()],
         [B["bonus"].r()])
    B['_rkv'] = (r, k, v)
    yield


def rwkv_params(K, dr, l, ph):
    pc = {}
    mu = K.sb(ph, "mu", [128, 7], F32)
    K.dma(mu.ap(), dr["rwkv_mu"][l].rearrange("(t p) -> p t", p=128), w=[mu.r()])
    pc["mu"] = mu
    for n, src in (("w0", dr["rwkv_w0"][l]), ("a0", dr["rwkv_a0"][l]), ("k_k", dr["rwkv_k_k"][l]),
                   ("k_a", dr["rwkv_k_a"][l]), ("r_k", dr["rwkv_r_k"][l].rearrange("h n -> (h n)")),
                   ("ln_g", dr["rwkv_ln_g"][l]), ("ln_b", dr["rwkv_ln_b"][l])):
        t = K.sb(ph, "pc_" + n, [128, 2], F32)
        K.dma(t.ap(), src.rearrange("(t p) -> p t", p=128), w=[t.r()])
        pc[n] = t
    omka = K.sb(ph, "omka", [128, 2], F32)
    K.ts(omka.ap(), pc["k_a"].ap(), -1.0, ALU.mult, [pc["k_a"].r()], [omka.r()], s2=1.0, op1=ALU.add)
    pc["omka"] = omka
    LW = K.sb(ph, "LW", [128, 256], BF16)
    K.dma(LW.ap()[0:32, :], dr["rwkv_w2"][l], w=[LW.r()], q="pool")
    K.dma(LW.ap()[32:64, :], dr["rwkv_a2"][l], w=[LW.r()], q="pool")
    K.dma(LW.ap()[64:128, :], dr["rwkv_g2"][l], w=[LW.r()], q="pool")
    return pc, LW


def rwkv_epilogue(K, C, pc, B, N, pT, ydst, yres):
    for t in range(2):
        K.act(B["t1"].ap()[:, t, 0:N], pT.ap()[:, t * N:(t + 1) * N], AF.Identity, [pT.r(), pc["ln_g"].r(), pc["ln_b"].r()],
              [B["t1"].r()], scale=pc["ln_g"].ap()[:, t:t + 1], bias=pc["ln_b"].ap()[:, t:t + 1])
    g = lambda n: B[n].ap()[:, :, 0:N]
    K.tt(g("t1"), g("t1"), g("bonus"), ALU.add, [B["t1"].r(), B["bonus"].r()], [B["t1"].r()])
    K.tt(ydst, g("t1"), g("gate"), ALU.mult, [B["t1"].r(), B["gate"].r()], yres)


def groupnorm64(K, o_ap, n, G, scr, res_in, out_ap, out_res):
    mean, xc, sq, var = scr
    K.red(mean.ap()[0:n, 0:G], o_ap, res_in, [mean.r()])
    K.ts(mean.ap()[0:n, 0:G], mean.ap()[0:n, 0:G], 1.0 / 64, ALU.mult, [mean.r()], [mean.r()])
    xca = xc.ap()[0:n, 0:G * 64].rearrange("p (g e) -> p g e", g=G)
    K.tt(xca, o_ap, bc(mean.ap()[0:n, 0:G], 2, [n, G, 64]), ALU.subtract, res_in + [mean.r()], [xc.r()])
    sqa = sq.ap()[0:n, 0:G * 64].rearrange("p (g e) -> p g e", g=G)
    K.tt(sqa, xca, xca, ALU.mult, [xc.r()], [sq.r()])
    K.red(var.ap()[0:n, 0:G], sqa, [sq.r()], [var.r()])
    K.act(var.ap()[0:n, 0:G], var.ap()[0:n, 0:G], AF.Sqrt, [var.r()], [var.r()], scale=1.0 / 64, bias=RWKV_GN_EPS)
    K.recip(var.ap()[0:n, 0:G], var.ap()[0:n, 0:G], [var.r()], [var.r()])
    K.tt(out_ap, xca, bc(var.ap()[0:n, 0:G], 2, [n, G, 64]), ALU.mult, [xc.r(), var.r()], out_res)


def rwkv_phase(K, dr, C, l, yT, dbg_out):
    P = K.P
    R0 = OFF["rw"]
    with contextlib.ExitStack() as ph:
        win = K.sb(ph, "win_rwkv", [128, KT, 896], BF16)
        K.dma(win.ap(), dr["w_in"][l, :, R0:R0 + 896].rearrange("(k p) n -> p k n", p=128), w=[win.r()], q="pool")
        pc, LW = rwkv_params(K, dr, l, ph)
        import os
        if os.environ.get("SKIP_RWKV_PROMPT") != "1":
            rwkv_prompt(K, dr, C, l, yT, win, pc, LW, dbg_out)
        P.barrier()
        if os.environ.get("SKIP_RWKV_SAMPLE") != "1":
            rwkv_sample(K, dr, C, l, yT, win, pc, LW, dbg_out)


def interleave(gens, ratio=None):
    gens = [g for g in gens if g is not None]
    ratio = ratio or [1] * len(gens)
    live = list(zip(gens, ratio))
    while live:
        for item in list(live):
            g, n = item
            for _ in range(n):
                try:
                    next(g)
                except StopIteration:
                    live.remove(item)
                    break


def rwkv_prompt(K, dr, C, l, yT, win, pc, LW, dbg_out):
    P = K.P
    identb, identf, maskU, maskSU, maskSL, blk64 = (C[k] for k in ["identb", "identf", "maskU", "maskSU", "maskSL", "blk64"])
    hc = C["hc"]
    N = 128
    with contextlib.ExitStack() as ph:
        f3 = lambda n: K.sb(ph, n, [128, 2, N], F32)
        Bs = []
        for i in range(2):
            B = {n: f3(f"rb{i}_" + n) for n in ["sig", "aic", "gate", "kk", "t1", "kp", "bonus", "cs", "e1", "e2", "bb"]}
            Bs.append(B)
        MX = K.sb(ph, "MX", [128, 7, N], F32)
        LI = K.sb(ph, "LI", [128, N], BF16)
        for B in Bs:
            B["MX"], B["LI"] = MX, LI
        RW = [K.sb(ph, f"RW{i}", [128, 7, N + 1], F32) for i in range(2)]
        ones_r = K.sb(ph, "ones_r", [128, N], F32)
        bcol = K.sb(ph, "bcol", [128, 2], F32)
        MK2 = K.sb(ph, "MK2", [128, 2, N], F32)
        ARs = [K.sb(ph, f"AR{i}", [128, 2, 2, N], BF16) for i in range(2)]
        BKs = [K.sb(ph, f"BK{i}", [128, 2, 2, N], BF16) for i in range(2)]
        FH = K.sb(ph, "FH", [128, 3, 2, N], BF16)
        TMs = [K.sb(ph, f"TM{i}", [128, 4, 2, N], BF16) for i in range(2)]
        t2 = f3("rb_t2")
        A1 = K.sb(ph, "A1", [128, 4, 2, N], BF16)
        A2 = K.sb(ph, "A2", [128, 4, 2, N], BF16)
        Lb = [K.sb(ph, f"Lb{i}", [128, 4, N], BF16) for i in range(2)]
        Nb = [K.sb(ph, f"Nb{i}", [128, 4, N], BF16) for i in range(2)]
        X32 = K.sb(ph, "X32", [128, 4, 2, 64], F32)
        Xb = K.sb(ph, "Xb", [128, 4, 2, 64], BF16)
        Apf = K.sb(ph, "Apf", [128, 2, N], BF16)
        XAc = K.sb(ph, "XAc", [128, 256], BF16)
        Utm = K.sb(ph, "Utm", [128, 4, 64], BF16)
        ST32 = K.sb(ph, "ST32", [128, 2, N], F32)
        STb = K.sb(ph, "STb", [128, 2, N], BF16)
        tmpS = K.sb(ph, "tmpSr", [128, 2, N], F32)
        gn = (K.sb(ph, "gn_mean", [128, 4], F32), K.sb(ph, "gn_xc", [128, 256], F32),
              K.sb(ph, "gn_sq", [128, 256], F32), K.sb(ph, "gn_var", [128, 4], F32))
        onb = K.sb(ph, "onbr", [128, 256], BF16)
        stT = K.sb(ph, "stT", [128, 2, N], F32)
        pI = K.ps(ph, "pr_I", [128, 512], F32)
        pM = K.ps(ph, "pr_M", [128, 512], F32)
        pT = K.ps(ph, "pr_T", [128, 1024], BF16)
        pT2 = pT
        pAT = K.ps(ph, "pr_AT", [128, 1024], F32)
        pL = K.ps(ph, "pr_L", [128, 512], F32)
        pL2 = K.ps(ph, "pr_L2", [128, 512], F32)
        pX = K.ps(ph, "pr_X", [128, 512], F32)
        pO = pX

        K.memset(ones_r.ap(), 1.0, [ones_r.r()])
        K.cp(MK2.ap()[:, 0, :], maskSU.ap(), [maskSU.r()], [MK2.r()])
        K.cp(MK2.ap()[:, 1, :], maskU.ap(), [maskU.r()], [MK2.r()])
        K.memset(ST32.ap(), 0.0, [ST32.r()])
        K.memset(STb.ap(), 0.0, [STb.r()])
        K.memset(RW[0].ap()[:, :, 0:1], 0.0, [RW[0].r()])

        def s1(c):
            B, AR, BK, TM = Bs[c % 2], ARs[c % 2], BKs[c % 2], TMs[c % 2]
            g = lambda n: B[n].ap()
            h = hc[c % 2]
            make_hc(K, C, l, h, c)
            yield
            rw = RW[c % 2]
            for (t0, t1) in ((0, 4), (4, 7)):
                for t in range(t0, t1):
                    for k in range(KT):
                        K.mm(pI.ap()[:, (t - t0) * N:(t - t0 + 1) * N], win.ap()[:, k, t * N:(t + 1) * N], h.ap()[:, k, :],
                             [win.r(), h.r()], [pI.r()], start=(k == 0), stop=(k == KT - 1))
                    yield
                K.cp(rw.ap()[:, t0:t1, 1:N + 1], pI.ap()[:, 0:(t1 - t0) * N].rearrange("p (t n) -> p t n", t=t1 - t0),
                     [pI.r()], [rw.r()], eng="act")
                yield
            if c + 1 < NCH:
                K.cp(RW[(c + 1) % 2].ap()[:, :, 0:1], rw.ap()[:, :, N:N + 1], [rw.r()], [RW[(c + 1) % 2].r()])
            B["_rw_res"] = [rw.r()]
            yield from rwkv_prep(K, C, pc, LW, N, rw.ap()[:, :, 1:N + 1], rw.ap()[:, :, 0:N], B, pM, pI, pI)
            r, k_, v = B["_rkv"]
            MXr = MX.r()
            for t in range(2):
                K.P.op("dve", lambda e, t=t, B=B: e.tensor_tensor_scan(out=B["cs"].ap()[:, t, :], data0=ones_r.ap(),
                                                                        data1=B["sig"].ap()[:, t, :], initial=0.0,
                                                                        op0=ALU.mult, op1=ALU.add),
                       reads=[ones_r.r(), B["sig"].r()], writes=[B["cs"].r()])
            yield
            K.act(g("e1"), g("cs"), AF.Exp, [B["cs"].r()], [B["e1"].r()], scale=-C0)
            yield
            K.act(g("e2"), g("cs"), AF.Exp, [B["cs"].r()], [B["e2"].r()], scale=C0)
            yield
            K.tt(AR.ap()[:, :, 1, :], r, g("e1"), ALU.mult, [MXr, B["e1"].r()], [AR.r()])
            yield
            K.tt(g("bb"), g("kk"), g("aic"), ALU.mult, [B["kk"].r(), B["aic"].r()], [B["bb"].r()])
            yield
            K.tt(BK.ap()[:, :, 0, :], g("bb"), g("e2"), ALU.mult, [B["bb"].r(), B["e2"].r()], [BK.r()])
            yield
            K.tt(BK.ap()[:, :, 1, :], g("kp"), g("e2"), ALU.mult, [B["kp"].r(), B["e2"].r()], [BK.r()])
            yield
            K.tt(g("t1"), g("cs"), g("sig"), ALU.subtract, [B["cs"].r(), B["sig"].r()], [B["t1"].r()])
            yield
            K.act(g("e2"), g("t1"), AF.Exp, [B["t1"].r()], [B["e2"].r()], scale=-C0)
            yield
            K.stt(AR.ap()[:, :, 0, :], g("kk"), -1.0, g("e2"), ALU.mult, ALU.mult, [B["kk"].r(), B["e2"].r()], [AR.r()])
            yield
            K.ts(bcol.ap(), B["cs"].ap()[:, :, N - 1], -C0, ALU.mult, [B["cs"].r()], [bcol.r()])
            yield
            for t in range(2):
                K.act(B["e2"].ap()[:, t, :], B["cs"].ap()[:, t, :], AF.Exp, [B["cs"].r(), bcol.r()], [B["e2"].r()],
                      scale=C0, bias=bcol.ap()[:, t:t + 1])
            yield
            K.tt(FH.ap()[:, 0], g("bb"), g("e2"), ALU.mult, [B["bb"].r(), B["e2"].r()], [FH.r()])
            yield
            K.tt(FH.ap()[:, 1], g("kp"), g("e2"), ALU.mult, [B["kp"].r(), B["e2"].r()], [FH.r()])
            yield
            K.cp(FH.ap()[:, 2], v, [MXr], [FH.r()], eng="act")
            yield
            for q in range(4):
                for t in range(2):
                    src = FH.ap()[:, q, t, :] if q < 3 else AR.ap()[:, t, 0, :]
                    K.tr(pT.ap()[:, (q * 2 + t) * N:(q * 2 + t + 1) * N], src, identb.ap(),
                         [FH.r(), AR.r(), identb.r()], [pT.r()], inc=(t == 1))
                yield
            K.cp(TM.ap().rearrange("p q t n -> p (q t n)"), pT.ap(), [pT.r()], [TM.r()], eng="act")
            yield

        def s2(c):
            B, AR, BK, TM = Bs[c % 2], ARs[c % 2], BKs[c % 2], TMs[c % 2]
            mk = bc(MK2.ap(), 1, [128, 4, 2, N])
            for which, Adst in ((0, A1), (1, A2)):
                for hd in range(4):
                    t, o = hd // 2, 64 * (hd % 2)
                    sl = slice(o, o + 64)
                    arf = AR.ap()[sl, t].rearrange("p a n -> p (a n)")
                    K.mm(pAT.ap()[:, hd * 256:(hd + 1) * 256], BK.ap()[sl, t, which, :], arf, [BK.r(), AR.r()], [pAT.r()],
                         self_wait=True)
                yield
                K.tt(Adst.ap(), pAT.ap().rearrange("p (h a n) -> p h a n", h=4, a=2), mk, ALU.mult, [pAT.r(), MK2.r()],
                     [Adst.r()])
                yield
            for hd in range(4):
                t, o = hd // 2, 64 * (hd % 2)
                sl = slice(o, o + 64)
                K.mm(pL.ap()[:, hd * N:(hd + 1) * N], AR.ap()[sl, t, 0, :], BK.ap()[sl, t, 0, :], [BK.r(), AR.r()],
                     [pL.r()], self_wait=True)
            yield
            K.tt(Lb[0].ap(), pL.ap().rearrange("p (h n) -> p h n", h=4), bc(maskSL.ap(), 1, [128, 4, N]), ALU.mult,
                 [pL.r(), maskSL.r()], [Lb[0].r()])
            yield
            vtm = TM.ap()[:, 2].rearrange("p t n -> p (t n)")
            for hd in range(4):
                K.mm(pX.ap()[:, hd * 64:(hd + 1) * 64], A2.ap()[:, hd, 0, :], vtm[:, hd * 64:(hd + 1) * 64],
                     [A2.r(), TM.r()], [pX.r()], inc=(hd == 3))
            yield
            K.cp(X32.ap()[:, :, 0, :], TM.ap()[:, 3].rearrange("p t (hh k) -> p (t hh) k", hh=2), [TM.r()], [X32.r()])
            yield
            K.cp(X32.ap()[:, :, 1, :], pX.ap()[:, 0:256].rearrange("p (h v) -> p h v", h=4), [pX.r()], [X32.r()],
                 eng="act")
            yield
            K.cp(Xb.ap(), X32.ap(), [X32.r()], [Xb.r()], eng="act")
            yield
            for i in range(7):
                if i == 0:
                    nref = lambda hd: A1.ap()[:, hd, 0, :]
                    nres = A1.r()
                    lcur = Lb[0]
                else:
                    nprev_ref, nprev_res, lprev = nref, nres, lcur
                    nnew, lnew = Nb[i % 2], Lb[i % 2]
                    for hd in range(4):
                        K.mm(pL.ap()[:, hd * N:(hd + 1) * N], lprev.ap()[:, hd, :], nprev_ref(hd), [lprev.r(), nprev_res],
                             [pL.r()], inc=(hd == 3))
                    yield
                    if i < 6:
                        for hd in range(4):
                            K.mm(pL2.ap()[:, hd * N:(hd + 1) * N], nprev_ref(hd), lprev.ap()[:, hd, :],
                                 [lprev.r(), nprev_res], [pL2.r()], inc=(hd == 3))
                        yield
                    K.cp(nnew.ap(), pL.ap().rearrange("p (h n) -> p h n", h=4), [pL.r()], [nnew.r()], eng="act")
                    yield
                    if i < 6:
                        K.cp(lnew.ap(), pL2.ap().rearrange("p (h n) -> p h n", h=4), [pL2.r()], [lnew.r()])
                        yield
                    nref = lambda hd, nnew=nnew: nnew.ap()[:, hd, :]
                    nres = nnew.r()
                    lcur = lnew
                for hd in range(4):
                    K.mm(pX.ap()[:, hd * N:(hd + 1) * N], nref(hd), Xb.ap()[:, hd].rearrange("p a k -> p (a k)"),
                         [nres, Xb.r()], [pX.r()], inc=(hd == 3))
                yield
                K.tt(X32.ap(), X32.ap(), pX.ap().rearrange("p (h a k) -> p h a k", h=4, a=2), ALU.add,
                     [X32.r(), pX.r()], [X32.r()])
                yield
                K.cp(Xb.ap(), X32.ap(), [X32.r()], [Xb.r()], eng="act")
                yield
            K.cp(XAc.ap().rearrange("p (h k) -> p h k", h=4), X32.ap()[:, :, 0, :], [X32.r()], [XAc.r()])
            yield
            for t in range(2):
                K.tr(pT2.ap()[:, t * N:(t + 1) * N], XAc.ap()[:, t * N:(t + 1) * N], identb.ap(), [XAc.r(), identb.r()],
                     [pT2.r()], inc=(t == 1))
            yield
            K.cp(Apf.ap(), pT2.ap()[:, 0:2 * N].rearrange("p (t n) -> p t n", t=2), [pT2.r()], [Apf.r()], eng="act")
            yield
            for t in range(2):
                K.mm(pX.ap()[:, t * N:(t + 1) * N], Apf.ap()[:, t, :], STb.ap()[:, t, :], [Apf.r(), STb.r()], [pX.r()],
                     inc=(t == 1))
            yield
            K.tt(Utm.ap(), pX.ap()[:, 0:256].rearrange("p (h v) -> p h v", h=4), X32.ap()[:, :, 1, :], ALU.add,
                 [pX.r(), X32.r()], [Utm.r()])
            yield
            for t in range(2):
                K.mm(pO.ap()[:, t * N:(t + 1) * N], AR.ap()[:, t, 1, :], STb.ap()[:, t, :], [AR.r(), STb.r()], [pO.r()],
                     start=(t == 0), stop=False, sgc=True)
            for hd in range(4):
                K.mm(pO.ap()[:, hd * 64:(hd + 1) * 64], A1.ap()[:, hd, 1, :], Utm.ap()[:, hd, :], [A1.r(), Utm.r()],
                     [pO.r()], start=False, stop=False, sgc=True)
                K.mm(pO.ap()[:, hd * 64:(hd + 1) * 64], A2.ap()[:, hd, 1, :], vtm[:, hd * 64:(hd + 1) * 64],
                     [A2.r(), TM.r()], [pO.r()], start=False, stop=False, sgc=True)
            for t in range(2):
                K.mm(pO.ap()[:, 256 + t * N:256 + (t + 1) * N], TM.ap()[:, 0, t, :],
                     Utm.ap()[:, 2 * t:2 * t + 2, :].rearrange("p h v -> p (h v)"), [TM.r(), Utm.r()], [pO.r()],
                     start=False, stop=False, sgc=True)
                K.mm(pO.ap()[:, 256 + t * N:256 + (t + 1) * N], TM.ap()[:, 1, t, :], vtm[:, t * N:(t + 1) * N],
                     [TM.r()], [pO.r()], start=False, stop=(t == 1), inc=(t == 1), sgc=True)
            yield
            K.tt(tmpS.ap(), pO.ap()[:, 256:512].rearrange("p (t n) -> p t n", t=2), bc(blk64.ap(), 1, [128, 2, N]),
                 ALU.mult, [pO.r(), blk64.r()], [tmpS.r()])
            yield
            K.tt(ST32.ap(), ST32.ap(), bc(B["e1"].ap()[:, :, N - 1], 2, [128, 2, N]), ALU.mult, [ST32.r(), B["e1"].r()],
                 [ST32.r()])
            yield
            K.tt(ST32.ap(), ST32.ap(), tmpS.ap(), ALU.add, [ST32.r(), tmpS.r()], [ST32.r()])
            yield
            K.cp(STb.ap(), ST32.ap(), [ST32.r()], [STb.r()], eng="act")
            yield
            groupnorm64(K, pO.ap()[:, 0:256].rearrange("p (h v) -> p h v", h=4), 128, 4, gn, [pO.r()],
                        onb.ap().rearrange("p (h v) -> p h v", h=4), [onb.r()])
            yield
            for t in range(2):
                K.tr(pT2.ap()[:, t * N:(t + 1) * N], onb.ap()[:, t * N:(t + 1) * N], identb.ap(), [onb.r(), identb.r()],
                     [pT2.r()], inc=(t == 1))
            yield
            B2 = dict(B)
            B2["t1"] = t2
            rwkv_epilogue(K, C, pc, B2, N, pT2, yT.ap()[:, 4:6, c * N:(c + 1) * N], xr(yT, c // 4, range(4, 6)))
            yield

        for _ in s1(0):
            pass
        for c in range(NCH):
            interleave([s2(c), s1(c + 1) if c + 1 < NCH else None], ratio=[3, 2])
        rw = RW[(NCH - 1) % 2]
        K.dma(dr["p_shift"][l].rearrange("(t p) -> p t", p=128), rw.ap()[:, :, N], r=[rw.r()])
        for t in range(2):
            K.tr(pL.ap()[:, t * N:(t + 1) * N], ST32.ap()[:, t, :], identf.ap(), [ST32.r(), identf.r()], [pL.r()],
                 inc=(t == 1))
        K.cp(stT.ap(), pL.ap()[:, 0:2 * N].rearrange("p (t n) -> p t n", t=2), [pL.r()], [stT.r()])
        for hd in range(4):
            t, o = hd // 2, 64 * (hd % 2)
            K.dma(dr["p_rwkv"][l, hd], stT.ap()[o:o + 64, t, o:o + 64], r=[stT.r()])
        P.barrier()


def rwkv_sample(K, dr, C, l, yT, win, pc, LW, dbg_out):
    P = K.P
    hs, identb, identf = C["hs"], C["identb"], C["identf"]
    N = NS
    with contextlib.ExitStack() as ph:
        f3 = lambda n: K.sb(ph, n, [128, 2, N], F32)
        B = {n: f3("rs_" + n) for n in ["sig", "aic", "gate", "kk", "t1", "kp", "bonus", "e1", "e2", "bb"]}
        B["MX"] = K.sb(ph, "MXs", [128, 7, N], F32)
        B["LI"] = K.sb(ph, "LIs", [128, N], BF16)
        rws = K.sb(ph, "rws", [128, 7, N], F32)
        prevs = K.sb(ph, "prevs", [128, 7, N], F32)
        shs = K.sb(ph, "shs", [NS, 896], F32)
        rwtm = K.sb(ph, "rwtm", [NS, 896], F32)
        pkT = K.sb(ph, "pkT", [NS, 6, 256], F32)
        pkh = K.sb(ph, "pkhr", [64, 6, 64], F32)
        S = K.sb(ph, "Sr", [64, 64, 64], F32)
        tmp = K.sb(ph, "tmpr", [64, 64, 64], F32)
        sa = K.sb(ph, "sa", [64, 64], F32)
        o = K.sb(ph, "orr", [64, 64], F32)
        on = K.sb(ph, "onr", [64, 64], F32)
        gn = (K.sb(ph, "gns_mean", [64, 1], F32), K.sb(ph, "gns_xc", [64, 64], F32),
              K.sb(ph, "gns_sq", [64, 64], F32), K.sb(ph, "gns_var", [64, 1], F32))
        otm = K.sb(ph, "otmr", [NS, 256], F32)
        otb = K.sb(ph, "otbr", [NS, 256], BF16)
        pA = K.ps(ph, "prs_A", [128, 1024], F32)
        pB = K.ps(ph, "prs_B", [128, 1024], F32)
        pL = K.ps(ph, "prs_L", [128, 512], F32)
        pM = K.ps(ph, "prs_M", [128, 512], F32)
        pT = K.ps(ph, "prs_T", [128, 1024], BF16)
        scr = dram_scratch(K, "rpk", [NS, 4, 6, 64])
        so = dram_scratch(K, "ro", [NS, 256])

        K.dma(shs.ap(), dr["st_shift"][l], w=[shs.r()])
        K.dma(S.ap().rearrange("p v k -> p (v k)"), dr["st_rwkv"][l].rearrange("b h v k -> (b h) (v k)"), w=[S.r()])
        for t in range(7):
            for k in range(KT):
                K.mm(pM.ap()[:, t * N:(t + 1) * N], win.ap()[:, k, t * 128:(t + 1) * 128], hs.ap()[:, k, :],
                     [win.r(), hs.r()], [pM.r()], start=(k == 0), stop=(k == KT - 1), inc=(k == KT - 1 and t == 6))
        K.cp(rws.ap(), pM.ap()[:, 0:7 * N].rearrange("p (t n) -> p t n", t=7), [pM.r()], [rws.r()], eng="act")
        for (c0, c1) in ((0, 512), (512, 896)):
            for k in range(KT):
                K.mm(pA.ap()[0:NS, c0:c1], hs.ap()[:, k, :], win.ap()[:, k, c0:c1], [win.r(), hs.r()], [pA.r()],
                     start=(k == 0), stop=(k == KT - 1))
        K.cp(rwtm.ap(), pA.ap()[0:NS, 0:896], [pA.r()], [rwtm.r()], eng="act")
        K.dma(dr["s_shift"][l], rwtm.ap(), r=[rwtm.r()])
        for t in range(7):
            K.tr(pL.ap()[:, t * N:(t + 1) * N], shs.ap()[:, t * 128:(t + 1) * 128], identf.ap()[0:NS, 0:NS],
                 [shs.r(), identf.r()], [pL.r()], inc=(t == 6))
        K.cp(prevs.ap(), pL.ap()[:, 0:7 * N].rearrange("p (t n) -> p t n", t=7), [pL.r()], [prevs.r()])
        B["_rw_res"] = [rws.r(), prevs.r()]
        for _ in rwkv_prep(K, C, pc, LW, N, rws.ap(), prevs.ap(), B, pB, pL, pM):
            pass
        r, k_, v = B["_rkv"]
        g = lambda n: B[n].ap()
        K.act(g("e1"), g("sig"), AF.Exp, [B["sig"].r()], [B["e1"].r()], scale=-C0)
        K.ts(g("e2"), g("kk"), -1.0, ALU.mult, [B["kk"].r()], [B["e2"].r()])
        K.tt(g("bb"), g("kk"), g("aic"), ALU.mult, [B["kk"].r(), B["aic"].r()], [B["bb"].r()])
        srcs = [(r, B["MX"].r()), (g("e1"), B["e1"].r()), (g("kp"), B["kp"].r()), (v, B["MX"].r()),
                (g("e2"), B["e2"].r()), (g("bb"), B["bb"].r())]
        for q, (ap, res) in enumerate(srcs):
            pp = pA if q < 4 else pB
            for t in range(2):
                col = ((q % 4) * 2 + t) * 128
                K.tr(pp.ap()[0:NS, col:col + 128], ap[:, t, :], identf.ap(), [res, identf.r()], [pp.r()])
        K.cp(pkT.ap()[:, 0:4, :], pA.ap()[0:NS, :].rearrange("p (q n) -> p q n", q=4), [pA.r()], [pkT.r()], eng="act")
        K.cp(pkT.ap()[:, 4:6, :], pB.ap()[0:NS, 0:512].rearrange("p (q n) -> p q n", q=2), [pB.r()], [pkT.r()])
        for q in range(6):
            K.dma(scr.ap()[:, :, q, :], pkT.ap()[:, q, :].rearrange("p (h k) -> p h k", h=4), r=[pkT.r()], w=[scr.r()])
        K.dma(pkh.ap(), scr.ap().rearrange("b h q k -> (b h) q k"), r=[scr.r()], w=[pkh.r()])
        rq, wq, kq, vq, aq, bq = (pkh.ap()[:, i, :] for i in range(6))
        K.tt(tmp.ap(), S.ap(), bc(aq, 1, [64, 64, 64]), ALU.mult, [S.r(), pkh.r()], [tmp.r()])
        K.red(sa.ap(), tmp.ap(), [tmp.r()], [sa.r()])
        K.tt(S.ap(), S.ap(), bc(wq, 1, [64, 64, 64]), ALU.mult, [S.r(), pkh.r()], [S.r()])
        K.tt(tmp.ap(), bc(sa.ap(), 2, [64, 64, 64]), bc(bq, 1, [64, 64, 64]), ALU.mult, [sa.r(), pkh.r()], [tmp.r()])
        K.tt(S.ap(), S.ap(), tmp.ap(), ALU.add, [S.r(), tmp.r()], [S.r()])
        K.tt(tmp.ap(), bc(vq, 2, [64, 64, 64]), bc(kq, 1, [64, 64, 64]), ALU.mult, [pkh.r()], [tmp.r()])
        K.tt(S.ap(), S.ap(), tmp.ap(), ALU.add, [S.r(), tmp.r()], [S.r()])
        K.dma(dr["s_rwkv"][l].rearrange("b h v k -> (b h) (v k)"), S.ap().rearrange("p v k -> p (v k)"), r=[S.r()])
        K.tt(tmp.ap(), S.ap(), bc(rq, 1, [64, 64, 64]), ALU.mult, [S.r(), pkh.r()], [tmp.r()])
        K.red(o.ap(), tmp.ap(), [tmp.r()], [o.r()])
        groupnorm64(K, o.ap().rearrange("p (g e) -> p g e", g=1), 64, 1, gn, [o.r()],
                    on.ap().rearrange("p (g e) -> p g e", g=1), [on.r()])
        K.dma(so.ap().rearrange("b (h v) -> (b h) v", h=4), on.ap(), r=[on.r()], w=[so.r()])
        K.dma(otm.ap(), so.ap(), r=[so.r()], w=[otm.r()])
        K.cp(otb.ap(), otm.ap(), [otm.r()], [otb.r()])
        for t in range(2):
            K.tr(pT.ap()[:, t * N:(t + 1) * N], otb.ap()[:, t * 128:(t + 1) * 128], identb.ap()[0:NS, 0:NS],
                 [otb.r(), identb.r()], [pT.r()], inc=(t == 1))
        rwkv_epilogue(K, C, pc, B, N, pT, yT.ap()[:, 4:6, T:T + NS], xr(yT, 4, range(4, 6)))
        P.barrier()
```

```python
import contextlib
import numpy as np
import concourse.bass as bass
import concourse.mybir as mybir
from concourse.bass_utils import run_bass_kernel_spmd

F32 = mybir.dt.float32
BF16 = mybir.dt.bfloat16
AF = mybir.ActivationFunctionType
ALU = mybir.AluOpType
AX = mybir.AxisListType

NCORES = 8
D = 1024
KT = 8
T = 2048
NS = 16
NT = T + NS
NCH = T // 128
DEPTH = 2
IN_DIM = 2968
OFF = dict(z=0, xbc=512, dt=1280, rw=1288, gq=2184, gk=2312, gv=2440, glo=2696, gg=2712)
F_DENSE = 2816
ALPHA = (2.0 * DEPTH) ** 0.25
LN_EPS = 1e-5
RMS_EPS = 1e-6
RWKV_GN_EPS = 64 * 1e-5
BLOCKS = [(0, 512), (512, 512), (1024, 512), (1536, 512), (2048, 16)]

ENGS = ["pe", "dve", "act", "pool", "sp"]


class Res:
    __slots__ = ("name", "w", "r", "excl")

    def __init__(self, name="", excl=False):
        self.name = name
        self.w = None
        self.r = []
        self.excl = excl


class Prog:
    NDMA = 8

    def __init__(self, nc):
        self.nc = nc
        self.q = {e: [] for e in ENGS}
        self.cnt = {e: 0 for e in ENGS}
        self.seen = {e: {} for e in ENGS}
        self.dma_i = {e: 0 for e in ENGS}
        self.dma_last = {}
        self.sems = {}

    def sem(self, key):
        if key not in self.sems:
            self.sems[key] = self.nc.alloc_semaphore(name="s_" + "_".join(str(k) for k in key))
        return self.sems[key]

    def _collect(self, eng, reads, writes):
        waits = {}

        def add(tok):
            if tok is None:
                return
            key, val = tok
            if self.seen[eng].get(key, 0) >= val:
                return
            if waits.get(key, 0) < val:
                waits[key] = val

        for r in reads:
            add(r.w)
        for w in writes:
            add(w.w)
            for t in w.r:
                add(t)
        if eng == "pe":
            waits.pop(("e", "pe"), None)
        for k, v in waits.items():
            self.seen[eng][k] = v
        return list(waits.items())

    def _commit(self, tok, reads, writes):
        for r in reads:
            r.r.append(tok)
            if len(r.r) > 64:
                r.r = _prune(r.r)
        for w in writes:
            w.w = tok
            w.r = []

    def op(self, eng, fn, reads=(), writes=(), inc=True, self_wait=False):
        assert inc or eng == "pe"
        if any(r.excl for r in reads):
            writes = list(writes) + [r for r in reads if r.excl]
            reads = [r for r in reads if not r.excl]
        waits = self._collect(eng, reads, writes)
        if self_wait and self.cnt[eng] > 0:
            waits.append((("e", eng), self.cnt[eng]))
        tok = (("e", eng), self.cnt[eng] + 1)
        if inc:
            self.cnt[eng] += 1
        self._commit(tok, reads, writes)

        def emit(e, fn=fn, waits=waits, inc=inc, eng=eng):
            for k, v in waits:
                e.wait_ge(self.sem(k), v)
            ins = fn(e)
            if inc:
                ins.then_inc(self.sem(("e", eng)), 1)
        self.q[eng].append(emit)
        return tok

    def dma(self, queue, out, in_, reads=(), writes=(), **kw):
        i = self.dma_i[queue]
        self.dma_i[queue] += 1
        slot = i % self.NDMA
        key = ("d", queue, slot)
        val = 16 * (i // self.NDMA + 1)
        waits = self._collect(queue, reads, writes)
        prev = val - 16
        if prev > 0 and self.seen[queue].get(key, 0) < prev:
            self.seen[queue][key] = prev
            waits.append((key, prev))
        tok = (key, val)
        self.dma_last[key] = val
        self._commit(tok, reads, writes)

        def emit(e, waits=waits, key=key):
            for k, v in waits:
                e.wait_ge(self.sem(k), v)
            e.dma_start(out=out, in_=in_, **kw).then_inc(self.sem(key), 16)
        self.q[queue].append(emit)
        return tok

    def barrier(self, engines=ENGS):
        toks = [(("e", e), self.cnt[e]) for e in ENGS if self.cnt[e] > 0]
        toks += list(self.dma_last.items())
        for eng in engines:
            waits = []
            for k, v in toks:
                if k == ("e", eng) and eng == "pe":
                    continue
                if self.seen[eng].get(k, 0) < v:
                    self.seen[eng][k] = v
                    waits.append((k, v))

            def emit(e, waits=waits):
                for k, v in waits:
                    e.wait_ge(self.sem(k), v)
            self.q[eng].append(emit)

    def emit(self):
        with self.nc.Block() as block:
            @block.tensor
            def _(e):
                for f in self.q["pe"]:
                    f(e)

            @block.vector
            def _(e):
                for f in self.q["dve"]:
                    f(e)

            @block.scalar
            def _(e):
                for f in self.q["act"]:
                    f(e)

            @block.gpsimd
            def _(e):
                for f in self.q["pool"]:
                    f(e)

            @block.sync
            def _(e):
                for f in self.q["sp"]:
                    f(e)


def _prune(toks):
    best = {}
    for k, v in toks:
        if best.get(k, 0) < v:
            best[k] = v
    return list(best.items())


class Tn:
    def __init__(self, h, name, excl=False):
        self.h = h
        self.name = name
        self._res = {}
        self.excl = excl

    def ap(self):
        return self.h.ap()

    def r(self, key=0):
        if key not in self._res:
            self._res[key] = Res(f"{self.name}:{key}", self.excl)
        return self._res[key]


class KB:
    def __init__(self, nc):
        self.nc = nc
        self.P = Prog(nc)
        self.uid = 0

    def sb(self, stack, name, shape, dt=F32):
        self.uid += 1
        h = stack.enter_context(self.nc.sbuf_tensor(f"{name}_{self.uid}", list(shape), dt))
        return Tn(h, name)

    def ps(self, stack, name, shape, dt=F32):
        self.uid += 1
        h = stack.enter_context(self.nc.psum_tensor(f"{name}_{self.uid}", list(shape), dt))
        return Tn(h, name, excl=True)

    def mm(self, out, lhsT, rhs, r, w, start=True, stop=True, inc=None, self_wait=False, sgc=False):
        inc = stop if inc is None else inc
        kw = {"skip_group_check": True} if sgc else {}
        self.P.op("pe", lambda e: e.matmul(out, lhsT=lhsT, rhs=rhs, start=start, stop=stop, **kw),
                  reads=r, writes=w, inc=inc, self_wait=self_wait)

    def tr(self, out, in_, ident, r, w, inc=True):
        self.P.op("pe", lambda e: e.transpose(out, in_, ident), reads=r, writes=w, inc=inc)

    def act(self, out, in_, func, r, w, scale=None, bias=None, accum_out=None):
        kw = {}
        if scale is not None:
            kw["scale"] = scale
        if bias is not None:
            kw["bias"] = bias
        if accum_out is not None:
            kw["accum_out"] = accum_out
        self.P.op("act", lambda e: e.activation(out=out, in_=in_, func=func, **kw), reads=r, writes=w)

    def tt(self, out, in0, in1, op, r, w, eng="dve"):
        self.P.op(eng, lambda e: e.tensor_tensor(out=out, in0=in0, in1=in1, op=op), reads=r, writes=w)

    def ts(self, out, in0, s1, op0, r, w, s2=None, op1=None, eng="dve", accum_out=None):
        kw = {}
        if op1 is not None:
            kw["op1"] = op1
        if accum_out is not None:
            kw["accum_out"] = accum_out
        self.P.op(eng, lambda e: e.tensor_scalar(out=out, in0=in0, scalar1=s1, scalar2=s2, op0=op0, **kw),
                  reads=r, writes=w)

    def stt(self, out, in0, scalar, in1, op0, op1, r, w, eng="dve"):
        self.P.op(eng, lambda e: e.scalar_tensor_tensor(out=out, in0=in0, scalar=scalar, in1=in1, op0=op0, op1=op1),
                  reads=r, writes=w)

    def cp(self, out, in_, r, w, eng="dve"):
        if eng == "act":
            self.P.op("act", lambda e: e.activation(out=out, in_=in_, func=AF.Copy), reads=r, writes=w)
        else:
            self.P.op(eng, lambda e: e.tensor_copy(out=out, in_=in_), reads=r, writes=w)

    def recip(self, out, in_, r, w):
        self.P.op("dve", lambda e: e.reciprocal(out=out, in_=in_), reads=r, writes=w)

    def red(self, out, in_, r, w, op=ALU.add, axis=AX.X, eng="dve"):
        self.P.op(eng, lambda e: e.tensor_reduce(out=out, in_=in_, axis=axis, op=op), reads=r, writes=w)

    def memset(self, ap, val, w, eng="dve"):
        self.P.op(eng, lambda e: e.memset(ap, val), writes=w)

    def dma(self, out, in_, r=(), w=(), q="sp", **kw):
        return self.P.dma(q, out, in_, reads=r, writes=w, **kw)


W_SHAPES = dict(
    w_ada=[DEPTH, D, 6 * D], b_ada=[DEPTH, 6 * D], w_in=[DEPTH, D, IN_DIM], w_out=[DEPTH, D, D],
    ssd_conv_w=[DEPTH, 4, 768], ssd_conv_b=[DEPTH, 768], ssd_dt_bias=[DEPTH, 8], ssd_a_log=[DEPTH, 8],
    ssd_d=[DEPTH, 8], ssd_norm_g=[DEPTH, 512], rwkv_mu=[DEPTH, 896], rwkv_w0=[DEPTH, 256],
    rwkv_w2=[DEPTH, 32, 256], rwkv_a0=[DEPTH, 256], rwkv_a2=[DEPTH, 32, 256], rwkv_g2=[DEPTH, 64, 256],
    rwkv_k_k=[DEPTH, 256], rwkv_k_a=[DEPTH, 256], rwkv_r_k=[DEPTH, 4, 64], rwkv_ln_g=[DEPTH, 256],
    rwkv_ln_b=[DEPTH, 256], gla_w_gk2=[DEPTH, 16, 128], gla_b_gk=[DEPTH, 128], gla_norm_g=[DEPTH, 64],
    ln_mix_g=[DEPTH, D], ln_mix_b=[DEPTH, D], ln_ffn_g=[DEPTH, D], ln_ffn_b=[DEPTH, D],
    ffn_w_gate=[1, D, F_DENSE], ffn_w_up=[1, D, F_DENSE], ffn_w_down=[1, F_DENSE, D],
    moe_router=[1, D, 8], moe_w_gate=[1, 8, D, D], moe_w_up=[1, 8, D, D], moe_w_down=[1, 8, D, D],
)
IN_SHAPES = dict(
    xp=[T, D], xs=[NS, D], cc=[1 + NS, D],
    st_ssd=[DEPTH, NS, 8, 64, 64], st_conv=[DEPTH, NS, 3, 768], st_rwkv=[DEPTH, NS, 4, 64, 64],
    st_shift=[DEPTH, NS, 896], st_gla=[DEPTH, NS, 4, 32, 64],
)
OUT_SHAPES = dict(
    y_p=[T, D], y_s=[NS, D],
    p_ssd=[DEPTH, 8, 64, 64], p_conv=[DEPTH, 3, 768], p_rwkv=[DEPTH, 4, 64, 64], p_shift=[DEPTH, 896],
    p_gla=[DEPTH, 4, 32, 64],
    s_ssd=[DEPTH, NS, 8, 64, 64], s_conv=[DEPTH, NS, 3, 768], s_rwkv=[DEPTH, NS, 4, 64, 64],
    s_shift=[DEPTH, NS, 896], s_gla=[DEPTH, NS, 4, 32, 64],
)

def xr(t, b, tiles=range(KT)):
    return [t.r((d, b)) for d in tiles]


SH1, SC1, GT1, SH2, SC2, GT2 = 0, 8, 16, 24, 32, 40


def build(stub_mixer=False, dbg=None, n_layers=DEPTH):
    nc = bass.Bass("TRN2", target_bir_lowering=False)
    K = KB(nc)
    P = K.P
    dr = {}
    for n, s in IN_SHAPES.items():
        dr[n] = nc.dram_tensor(n, s, F32, kind="ExternalInput").ap()
    for n, s in W_SHAPES.items():
        dr[n] = nc.dram_tensor(n, s, F32, kind="ExternalInput").ap()
    for n, s in OUT_SHAPES.items():
        dr[n] = nc.dram_tensor(n, s, F32, kind="ExternalOutput").ap()
    dbg_out = {}
    if dbg:
        for n, s in dbg.items():
            dbg_out[n] = nc.dram_tensor("dbg_" + n, s, F32, kind="ExternalOutput").ap()
    out_res = Res("outputs")

    with contextlib.ExitStack() as perm, nc.allow_non_contiguous_dma(reason="small param loads"):
        xT = K.sb(perm, "xT", [128, KT, NT], F32)
        modT = [K.sb(perm, f"modT{l}", [128, 48, 1 + NS], F32) for l in range(DEPTH)]
        identf = K.sb(perm, "identf", [128, 128], F32)
        identb = K.sb(perm, "identb", [128, 128], BF16)
        onesM = K.sb(perm, "onesM", [128, 128], F32)
        ones1 = K.sb(perm, "ones1", [128, 128], F32)
        maskU = K.sb(perm, "maskU", [128, 128], F32)
        maskSU = K.sb(perm, "maskSU", [128, 128], F32)
        maskSL = K.sb(perm, "maskSL", [128, 128], F32)
        blk64 = K.sb(perm, "blk64", [128, 128], F32)
        C = dict(xT=xT, modT=modT, identf=identf, identb=identb, onesM=onesM, ones1=ones1,
                 maskU=maskU, maskSU=maskSU, maskSL=maskSL, blk64=blk64)

        def sel(t, val_keep, cmp, fill, base=0, cm=1, pat=None, ap=None):
            ap = t.ap() if ap is None else ap
            pat = [[-1, ap.shape[-1]]] if pat is None else pat
            P.op("pool", lambda e: e.affine_select(out=ap, in_=ap, pattern=pat, compare_op=cmp, fill=fill,
                                                   base=base, channel_multiplier=cm),
                 reads=[t.r()], writes=[t.r()])

        K.memset(identf.ap(), 0.0, [identf.r()], eng="pool")
        sel(identf, 0, ALU.not_equal, 1.0)
        K.cp(identb.ap(), identf.ap(), [identf.r()], [identb.r()], eng="pool")
        K.memset(onesM.ap(), 1.0 / D, [onesM.r()], eng="pool")
        K.memset(ones1.ap(), 1.0, [ones1.r()], eng="pool")
        K.memset(maskU.ap(), 1.0, [maskU.r()], eng="pool")
        sel(maskU, 1, ALU.is_ge, 0.0, cm=-1, pat=[[1, 128]])
        K.memset(maskSU.ap(), 1.0, [maskSU.r()], eng="pool")
        sel(maskSU, 1, ALU.is_gt, 0.0, cm=-1, pat=[[1, 128]])
        K.memset(maskSL.ap(), 1.0, [maskSL.r()], eng="pool")
        sel(maskSL, 1, ALU.is_gt, 0.0)
        K.memset(blk64.ap(), 0.0, [blk64.r()], eng="pool")
        K.memset(blk64.ap()[0:64, 0:64], 1.0, [blk64.r()], eng="pool")
        K.memset(blk64.ap()[64:128, 64:128], 1.0, [blk64.r()], eng="pool")

        with contextlib.ExitStack() as ph:
            stage = [K.sb(ph, f"stg{i}", [128, D], F32) for i in range(2)]
            ctm = K.sb(ph, "ctm", [1 + NS, D], F32)
            scT = K.sb(ph, "scT", [128, KT, 1 + NS], BF16)
            wada = [K.sb(ph, f"wada{i}", [128, KT, 512], BF16) for i in range(2)]
            bB = [K.sb(ph, f"bB{i}", [1 + NS, 512], F32) for i in range(2)]
            modsb = [K.sb(ph, f"modsb{i}", [1 + NS, 512], F32) for i in range(2)]
            pst = [K.ps(ph, f"pst{i}", [128, 1024], F32) for i in range(2)]
            psm = [K.ps(ph, f"psm{i}", [128, 512], F32) for i in range(2)]
            pss = K.ps(ph, "pss", [128, 512], F32)
            for tt in range(NCH + 1):
                st = stage[tt % 2]
                pt = pst[tt % 2]
                n = 128 if tt < NCH else NS
                src = dr["xp"][tt * 128:(tt + 1) * 128, :] if tt < NCH else dr["xs"]
                K.dma(st.ap()[0:n, :], src, w=[st.r()])
                for d in range(KT):
                    K.tr(pt.ap()[:, d * n:(d + 1) * n], st.ap()[0:n, d * 128:(d + 1) * 128],
                         identf.ap()[0:n, 0:n], [st.r(), identf.r()], [pt.r()], inc=(d == KT - 1))
                dst = xT.ap()[:, :, tt * 128:tt * 128 + n]
                srcp = pt.ap()[:, 0:KT * n].rearrange("p (d n) -> p d n", d=KT)
                K.cp(dst, srcp, [pt.r()], xr(xT, min(tt // 4, 4)), eng=("act" if tt % 2 == 0 else "dve"))
            K.dma(ctm.ap(), dr["cc"], w=[ctm.r()])
            for d in range(KT):
                K.tr(pss.ap()[:, d * 17:(d + 1) * 17], ctm.ap()[:, d * 128:(d + 1) * 128],
                     identf.ap()[0:17, 0:17], [ctm.r(), identf.r()], [pss.r()], inc=(d == KT - 1))
            K.act(scT.ap(), pss.ap()[:, 0:KT * 17].rearrange("p (d n) -> p d n", d=KT), AF.Silu,
                  [pss.r()], [scT.r()])
            it = 0
            for l in range(DEPTH):
                for j in range(12):
                    wb = wada[it % 2]
                    bb = bB[it % 2]
                    ms = modsb[it % 2]
                    pm = psm[it % 2]
                    K.dma(wb.ap(), dr["w_ada"][l, :, j * 512:(j + 1) * 512].rearrange("(k p) n -> p k n", p=128),
                          w=[wb.r()], q="pool")
                    K.dma(bb.ap(), dr["b_ada"][l:l + 1, j * 512:(j + 1) * 512].to_broadcast([1 + NS, 512]),
                          w=[bb.r()])
                    for k in range(KT):
                        K.mm(pm.ap()[0:17, :], scT.ap()[:, k, :], wb.ap()[:, k, :], [scT.r(), wb.r()], [pm.r()],
                             start=(k == 0), stop=(k == KT - 1))
                    K.tt(ms.ap(), pm.ap()[0:17, :], bb.ap(), ALU.add, [pm.r(), bb.r()], [ms.r()])
                    for q4 in range(4):
                        K.tr(pss.ap()[:, q4 * 17:(q4 + 1) * 17], ms.ap()[:, q4 * 128:(q4 + 1) * 128],
                             identf.ap()[0:17, 0:17], [ms.r(), identf.r()], [pss.r()], inc=(q4 == 3))
                    K.cp(modT[l].ap()[:, j * 4:(j + 1) * 4, :],
                         pss.ap()[:, 0:4 * 17].rearrange("p (d n) -> p d n", d=4), [pss.r()], [modT[l].r()],
                         eng="act")
                    it += 1
                for seg in (SC1, GT1, SC2, GT2):
                    K.ts(modT[l].ap()[:, seg:seg + 8, :], modT[l].ap()[:, seg:seg + 8, :], 1.0, ALU.add,
                         [modT[l].r()], [modT[l].r()])
            P.barrier()

        for l in range(n_layers):
            layer(K, dr, C, l, stub_mixer, dbg_out)

        with contextlib.ExitStack() as ph:
            stage = [K.sb(ph, f"ostg{i}", [128, D], F32) for i in range(2)]
            pst = [K.ps(ph, f"opst{i}", [128, 1024], F32) for i in range(2)]
            for tt in range(NCH + 1):
                st = stage[tt % 2]
                pt = pst[tt % 2]
                n = 128 if tt < NCH else NS
                for d in range(KT):
                    K.tr(pt.ap()[0:n, d * 128:(d + 1) * 128], xT.ap()[:, d, tt * 128:tt * 128 + n],
                         identf.ap(), [xT.r((d, min(tt // 4, 4))), identf.r()], [pt.r()], inc=(d == KT - 1))
                K.cp(st.ap()[0:n, :], pt.ap()[0:n, :], [pt.r()], [st.r()], eng=("act" if tt % 2 == 0 else "dve"))
                dst = dr["y_p"][tt * 128:(tt + 1) * 128, :] if tt < NCH else dr["y_s"]
                K.dma(dst, st.ap()[0:n, :], r=[st.r()])
            P.barrier()
    with nc.allow_non_contiguous_dma(reason="small param loads"):
        P.emit()
    return nc


def modulate(K, C, l, src, dst, b, sh, sc, dres):
    mod = C["modT"][l]
    c0, n = BLOCKS[b]
    if b < 4:
        for d in range(KT):
            K.act(dst.ap()[:, d, c0:c0 + n], src.ap()[:, d, c0:c0 + n], AF.Identity,
                  [src.r((d, b)), mod.r()], [dres(d)],
                  scale=mod.ap()[:, sc + d, 0:1], bias=mod.ap()[:, sh + d, 0:1])
    else:
        tmp = C["tmp_s"]
        K.tt(tmp.ap(), src.ap()[:, :, c0:c0 + n], mod.ap()[:, sc:sc + 8, 1:1 + NS], ALU.mult,
             xr(src, b) + [mod.r()], [tmp.r()])
        K.tt(dst.ap()[:, :, c0:c0 + n], tmp.ap(), mod.ap()[:, sh:sh + 8, 1:1 + NS], ALU.add,
             [tmp.r(), mod.r()], [dres(d) for d in range(KT)])


def layernorm(K, C, l, gi, psA, psB, scr):
    xT, onesM, lncol = C["xT"], C["onesM"], C["lncol"]
    sq, mean_sb, var, tt_ = scr["sq"], scr["mean"], scr["var"], scr["t"]
    for b, (c0, n) in enumerate(BLOCKS):
        for d in range(KT):
            s = sq[d % 2]
            xs = xT.ap()[:, d, c0:c0 + n]
            K.act(s.ap()[:, :n], xs, AF.Square, [xT.r((d, b))], [s.r()])
            K.mm(psA.ap()[:, :n], onesM.ap(), xs, [onesM.r(), xT.r((d, b))], [psA.r()],
                 start=(d == 0), stop=(d == KT - 1), inc=True)
            K.mm(psB.ap()[:, :n], onesM.ap(), s.ap()[:, :n], [onesM.r(), s.r()], [psB.r()],
                 start=(d == 0), stop=(d == KT - 1), inc=True)
        K.cp(mean_sb.ap()[:, :n], psA.ap()[:, :n], [psA.r()], [mean_sb.r()], eng="act")
        K.tt(var.ap()[:, :n], mean_sb.ap()[:, :n], mean_sb.ap()[:, :n], ALU.mult, [mean_sb.r()], [var.r()])
        K.tt(var.ap()[:, :n], psB.ap()[:, :n], var.ap()[:, :n], ALU.subtract, [psB.r(), var.r()], [var.r()])
        K.act(var.ap()[:, :n], var.ap()[:, :n], AF.Sqrt, [var.r()], [var.r()], bias=LN_EPS)
        K.recip(var.ap()[:, :n], var.ap()[:, :n], [var.r()], [var.r()])
        for d in range(KT):
            t = tt_[d % 2]
            xs = xT.ap()[:, d, c0:c0 + n]
            K.tt(t.ap()[:, :n], xs, mean_sb.ap()[:, :n], ALU.subtract, [xT.r((d, b)), mean_sb.r()], [t.r()])
            K.tt(t.ap()[:, :n], t.ap()[:, :n], var.ap()[:, :n], ALU.mult, [t.r(), var.r()], [t.r()])
            K.act(xs, t.ap()[:, :n], AF.Identity, [t.r(), lncol.r()], [xT.r((d, b))],
                  scale=lncol.ap()[:, gi, d:d + 1], bias=lncol.ap()[:, gi + 1, d:d + 1])


def residual_add(K, C, l, ps, n, dout, b, gt, comb=None):
    xT, mod = C["xT"], C["modT"][l]
    c0, _ = BLOCKS[b]
    xs = xT.ap()[:, dout, c0:c0 + n]
    src = ps.ap()[:, :n]
    rd = [ps.r(), mod.r(), xT.r((dout, b))]
    if comb is not None:
        tmp = C["tmp_c"][dout % 2]
        K.tt(tmp.ap()[:, :n], src, comb.ap()[:, c0:c0 + n], ALU.mult, [ps.r(), comb.r(b)], [tmp.r()])
        src = tmp.ap()[:, :n]
        rd = [tmp.r(), mod.r(), xT.r((dout, b))]
    if b < 4:
        K.stt(xs, src, mod.ap()[:, gt + dout, 0:1], xs, ALU.mult, ALU.add, rd, [xT.r((dout, b))])
    else:
        tmp2 = C["tmp_s2"]
        K.tt(tmp2.ap(), src, mod.ap()[:, gt + dout, 1:1 + NS], ALU.mult, rd[:2], [tmp2.r()])
        K.tt(xs, tmp2.ap(), xs, ALU.add, [tmp2.r(), xT.r((dout, b))], [xT.r((dout, b))])


def scale_x(K, C):
    xT = C["xT"]
    for b, (c0, n) in enumerate(BLOCKS):
        for d in range(KT):
            xs = xT.ap()[:, d, c0:c0 + n]
            K.P.op("act", lambda e, xs=xs: e.mul(out=xs, in_=xs, mul=ALPHA), reads=[xT.r((d, b))],
                   writes=[xT.r((d, b))])


def ffn_gateup(K, C, l, hT, actb, wpool, srcs, nf, ps):
    wg_src, wu_src, wd_src = srcs
    sg = C["sg"]

    def unit():
        u = wpool["bufs"][wpool["i"] % len(wpool["bufs"])]
        wpool["i"] += 1
        return u

    i = 0
    for f0 in range(0, nf, 4):
        nfu = min(4, nf - f0)
        WG, WU = unit(), unit()
        K.dma(WG.ap()[:, :, 0:nfu * 128], wg_src[:, f0 * 128:(f0 + nfu) * 128].rearrange("(k p) n -> p k n", p=128),
              w=[WG.r()], q="pool")
        K.dma(WU.ap()[:, :, 0:nfu * 128], wu_src[:, f0 * 128:(f0 + nfu) * 128].rearrange("(k p) n -> p k n", p=128),
              w=[WU.r()], q="pool")
        for fu in range(nfu):
            f = f0 + fu
            for b, (c0, n) in enumerate(BLOCKS):
                pg, pu = ps["g"][i % 2], ps["u"][i % 2]
                for k in range(KT):
                    K.mm(pg.ap()[:, :n], WG.ap()[:, k, fu * 128:(fu + 1) * 128], hT.ap()[:, k, c0:c0 + n],
                         [WG.r(), hT.r((k, b))], [pg.r()], start=(k == 0), stop=(k == KT - 1))
                for k in range(KT):
                    K.mm(pu.ap()[:, :n], WU.ap()[:, k, fu * 128:(fu + 1) * 128], hT.ap()[:, k, c0:c0 + n],
                         [WU.r(), hT.r((k, b))], [pu.r()], start=(k == 0), stop=(k == KT - 1))
                s_ = sg[i % 2]
                K.act(s_.ap()[:, :n], pg.ap()[:, :n], AF.Silu, [pg.r()], [s_.r()])
                K.tt(actb.ap()[:, f, c0:c0 + n], s_.ap()[:, :n], pu.ap()[:, :n], ALU.mult, [s_.r(), pu.r()],
                     [actb.r((f, b))])
                i += 1
                yield


def ffn_down(K, C, l, actb, wpool, srcs, nf, ps, comb=None):
    wg_src, wu_src, wd_src = srcs

    def unit():
        u = wpool["bufs"][wpool["i"] % len(wpool["bufs"])]
        wpool["i"] += 1
        return u

    i = 0
    for dh in range(2):
        WD = unit()
        K.dma(WD.ap()[:, 0:nf, :], wd_src[:, dh * 512:(dh + 1) * 512].rearrange("(f p) n -> p f n", p=128),
              w=[WD.r()], q="pool")
        for dd in range(4):
            dout = dh * 4 + dd
            for b, (c0, n) in enumerate(BLOCKS):
                pd = ps["d"][i % 2]
                for f in range(nf):
                    K.mm(pd.ap()[:, :n], WD.ap()[:, f, dd * 128:(dd + 1) * 128], actb.ap()[:, f, c0:c0 + n],
                         [WD.r(), actb.r((f, b))], [pd.r()], start=(f == 0), stop=(f == nf - 1))
                residual_add(K, C, l, pd, n, dout, b, GT2, comb=comb)
                i += 1


def ffn_group(K, C, l, hT, actb, wpool, srcs, nf, ps, comb=None):
    for _ in ffn_gateup(K, C, l, hT, actb, wpool, srcs, nf, ps):
        pass
    ffn_down(K, C, l, actb, wpool, srcs, nf, ps, comb=comb)


def moe_routing(K, C, dr, l, ph, ps):
    xT, mod, identf = C["xT"], C["modT"][l], C["identf"]
    router = K.sb(ph, "router", [128, KT, 8], F32)
    K.dma(router.ap(), dr["moe_router"][0].rearrange("(k p) e -> p k e", p=128), w=[router.r()])
    combT = K.sb(ph, "combT", [8, NT], F32)
    tp = C["tpair"]
    sm = {n: K.sb(ph, "rt_" + n, [128, 8], F32) for n in ["lg", "eq1", "l2", "eq2", "cb"]}
    sc1 = {n: K.sb(ph, "rs_" + n, [128, 1], F32) for n in ["m1", "m2", "e", "w1", "w2"]}
    pl, pt = ps["rA"], ps["rB"]
    for tt in range(NCH + 1):
        n = 128 if tt < NCH else NS
        c0 = tt * 128
        b = min(tt // 4, 4)
        hap = tp.ap().rearrange("p a (b c) -> p (a b) c", c=128)
        hres = [tp.r(0), tp.r(1)]
        if tt < NCH:
            for d in range(KT):
                K.act(hap[:, d, :], xT.ap()[:, d, c0:c0 + n], AF.Identity, [xT.r((d, b)), mod.r()], hres,
                      scale=mod.ap()[:, SC2 + d, 0:1], bias=mod.ap()[:, SH2 + d, 0:1])
        else:
            K.tt(hap[:, :, 0:n], xT.ap()[:, :, c0:c0 + n], mod.ap()[:, SC2:SC2 + 8, 1:1 + NS], ALU.mult,
                 xr(xT, b) + [mod.r()], hres)
            K.tt(hap[:, :, 0:n], hap[:, :, 0:n], mod.ap()[:, SH2:SH2 + 8, 1:1 + NS], ALU.add,
                 hres + [mod.r()], hres)
        for d in range(KT):
            K.mm(pl.ap()[0:n, 0:8], hap[:, d, 0:n], router.ap()[:, d, :], hres + [router.r()], [pl.r()],
                 start=(d == 0), stop=(d == KT - 1), inc=True)
        lg, eq1, l2, eq2, cb = (sm[k].ap()[0:n, :] for k in ["lg", "eq1", "l2", "eq2", "cb"])
        m1, m2, ee, w1, w2 = (sc1[k].ap()[0:n, :] for k in ["m1", "m2", "e", "w1", "w2"])
        R = lambda *ks: [(sm[k] if k in sm else sc1[k]).r() for k in ks]
        K.cp(lg, pl.ap()[0:n, 0:8], [pl.r()], R("lg"))
        yield
        K.red(m1, lg, R("lg"), R("m1"), op=ALU.max)
        yield
        K.ts(eq1, lg, m1, ALU.is_equal, R("lg", "m1"), R("eq1"))
        yield
        K.stt(l2, eq1, -1e30, lg, ALU.mult, ALU.add, R("eq1", "lg"), R("l2"))
        yield
        K.red(m2, l2, R("l2"), R("m2"), op=ALU.max)
        yield
        K.ts(eq2, l2, m2, ALU.is_equal, R("l2", "m2"), R("eq2"))
        yield
        K.tt(ee, m2, m1, ALU.subtract, R("m1", "m2"), R("e"))
        yield
        K.act(ee, ee, AF.Exp, R("e"), R("e"))
        yield
        K.ts(w1, ee, 1.0, ALU.add, R("e"), R("w1"))
        yield
        K.recip(w1, w1, R("w1"), R("w1"))
        yield
        K.tt(w2, ee, w1, ALU.mult, R("e", "w1"), R("w2"))
        yield
        K.ts(cb, eq1, w1, ALU.mult, R("eq1", "w1"), R("cb"))
        yield
        K.stt(cb, eq2, w2, cb, ALU.mult, ALU.add, R("eq2", "w2", "cb"), R("cb"))
        yield
        K.tr(pt.ap()[0:8, 0:n], cb, identf.ap()[0:n, 0:n], R("cb") + [identf.r()], [pt.r()])
        yield
        K.cp(combT.ap()[:, c0:c0 + n], pt.ap()[0:8, 0:n], [pt.r()], [combT.r()], eng="act")
        yield
    C["_combT"] = combT
    yield


def layer(K, dr, C, l, stub_mixer, dbg_out):
    nc, P = K.nc, K.P
    xT, mod = C["xT"], C["modT"][l]
    with contextlib.ExitStack() as lay:
        bufA = K.sb(lay, "bufA", [128, KT, NT], BF16)
        lncol = K.sb(lay, "lncol", [128, 4, KT], F32)
        C["lncol"] = lncol
        C["tmp_s"] = K.sb(lay, "tmp_s", [128, KT, NS], F32)
        C["tmp_s2"] = K.sb(lay, "tmp_s2", [128, NS], F32)
        for i, nme in enumerate(["ln_mix_g", "ln_mix_b", "ln_ffn_g", "ln_ffn_b"]):
            K.dma(lncol.ap()[:, i, :], dr[nme][l].rearrange("(d p) -> p d", p=128), w=[lncol.r()])

        with contextlib.ExitStack() as ph:
            if stub_mixer:
                for b in range(5):
                    modulate(K, C, l, xT, bufA, b, SH1, SC1, lambda d, b=b: bufA.r((d, b)))
            else:
                mixers(K, dr, C, l, bufA, dbg_out)
            P.barrier()
            wout = K.sb(ph, "wout", [128, KT, D], BF16)
            K.dma(wout.ap(), dr["w_out"][l].rearrange("(k p) n -> p k n", p=128), w=[wout.r()], q="pool")
            scale_x(K, C)
            with contextlib.ExitStack() as ph2:
                pso = [K.ps(ph2, f"pso{i}", [128, 512], F32) for i in range(4)]
                i = 0
                for dout in range(KT):
                    for b, (c0, n) in enumerate(BLOCKS):
                        pd = pso[i % 4]
                        for k in range(KT):
                            K.mm(pd.ap()[:, :n], wout.ap()[:, k, dout * 128:(dout + 1) * 128],
                                 bufA.ap()[:, k, c0:c0 + n], [wout.r(), bufA.r((k, b))], [pd.r()],
                                 start=(k == 0), stop=(k == KT - 1))
                        residual_add(K, C, l, pd, n, dout, b, GT1)
                        i += 1
                P.barrier()

        with contextlib.ExitStack() as ph:
            hT = K.sb(ph, "hT", [128, KT, NT], BF16)
            tpair = K.sb(ph, "tpair", [128, 2, 512], F32)
            C["tpair"] = tpair

            class _V:
                def __init__(self, i):
                    self.i = i

                def ap(self):
                    return tpair.ap()[:, self.i, :]

                def r(self, key=0):
                    return tpair.r(self.i)
            scr = dict(sq=[K.sb(ph, f"sq{i}", [128, 512], F32) for i in range(2)],
                       mean=K.sb(ph, "mean", [128, 512], F32), var=K.sb(ph, "var", [128, 512], F32),
                       t=[_V(0), _V(1)])
            C["sg"] = scr["sq"]
            C["tmp_c"] = scr["t"]
            W = dict(bufs=[K.sb(ph, f"WP{i}", [128, KT, 512], BF16) for i in range(4)], i=0)
            ps = dict(g=[K.ps(ph, f"pg{i}", [128, 512], F32) for i in range(2)],
                      u=[K.ps(ph, f"pu{i}", [128, 512], F32) for i in range(2)],
                      d=[K.ps(ph, f"pd{i}", [128, 512], F32) for i in range(2)])
            psA = K.ps(ph, "psA", [128, 512], F32)
            psB = K.ps(ph, "psB", [128, 512], F32)
            layernorm(K, C, l, 0, psA, psB, scr)
            for b in range(5):
                modulate(K, C, l, xT, hT, b, SH2, SC2, lambda d, b=b: hT.r((d, b)))
            if l % 2 == 0:
                scale_x(K, C)
                i = l // 2
                for f0 in range(0, F_DENSE // 128, 8):
                    nf = min(8, F_DENSE // 128 - f0)
                    srcs = (dr["ffn_w_gate"][i, :, f0 * 128:(f0 + nf) * 128],
                            dr["ffn_w_up"][i, :, f0 * 128:(f0 + nf) * 128],
                            dr["ffn_w_down"][i, f0 * 128:(f0 + nf) * 128, :])
                    ffn_group(K, C, l, hT, bufA, W, srcs, nf, ps)
            else:
                ps["rA"], ps["rB"] = psA, psB
                i = l // 2
                srcs0 = (dr["moe_w_gate"][i, 0], dr["moe_w_up"][i, 0], dr["moe_w_down"][i, 0])
                interleave([moe_routing(K, C, dr, l, ph, ps), ffn_gateup(K, C, l, hT, bufA, W, srcs0, 8, ps)],
                           ratio=[6, 1])
                combT = C["_combT"]
                scale_x(K, C)
                combB = K.sb(ph, "combB", [128, NT], F32)
                sele = K.sb(ph, "sele", [8, 128], F32)
                i = l // 2
                for e_ in range(8):
                    K.memset(sele.ap(), 0.0, [sele.r()])
                    K.P.op("dve", lambda e, e_=e_: e.memset(sele.ap()[e_:e_ + 1, :], 1.0), reads=[sele.r()],
                           writes=[sele.r()]) if False else K.ts(
                        sele.ap(), C["identf"].ap()[0:8, e_:e_ + 1].to_broadcast([8, 128]), 1.0, ALU.mult,
                        [C["identf"].r()], [sele.r()])
                    for b, (c0, n) in enumerate(BLOCKS):
                        pb = ps["d"][b % 2]
                        K.mm(pb.ap()[:, :n], sele.ap(), combT.ap()[:, c0:c0 + n], [sele.r(), combT.r()],
                             [pb.r()])
                        K.cp(combB.ap()[:, c0:c0 + n], pb.ap()[:, :n], [pb.r()], [combB.r(b)], eng="act")
                    srcs = (dr["moe_w_gate"][i, e_], dr["moe_w_up"][i, e_], dr["moe_w_down"][i, e_])
                    if e_ > 0:
                        for _ in ffn_gateup(K, C, l, hT, bufA, W, srcs, 8, ps):
                            pass
                    ffn_down(K, C, l, bufA, W, srcs, 8, ps, comb=combB)
            layernorm(K, C, l, 2, psA, psB, scr)
            P.barrier()


def make_in_maps(inp):
    g = lambda k: np.ascontiguousarray(np.asarray(inp[k], dtype=np.float32))
    xp, xs, cp, cs = g("x_prompt"), g("x_sample"), g("c_prompt"), g("c_sample")
    st = {k: g(k) for k in ["state_ssd", "state_ssd_conv", "state_rwkv", "state_rwkv_shift", "state_gla"]}
    wts = {k: g(k) for k in W_SHAPES}
    maps = []
    for c in range(NCORES):
        sl = slice(c * NS, (c + 1) * NS)
        m = dict(wts)
        m["xp"] = xp[c]
        m["xs"] = np.ascontiguousarray(xs[sl, 0, :])
        m["cc"] = np.ascontiguousarray(np.concatenate([cp[c:c + 1], cs[sl]], axis=0))
        m["st_ssd"] = np.ascontiguousarray(st["state_ssd"][:, sl])
        m["st_conv"] = np.ascontiguousarray(st["state_ssd_conv"][:, sl])
        m["st_rwkv"] = np.ascontiguousarray(st["state_rwkv"][:, sl])
        m["st_shift"] = np.ascontiguousarray(st["state_rwkv_shift"][:, sl])
        m["st_gla"] = np.ascontiguousarray(st["state_gla"][:, sl])
        maps.append(m)
    return maps


_NC_CACHE = {}


def gather(results):
    R = lambda k: [np.asarray(r[k], dtype=np.float32) for r in results]
    y_p = np.stack(R("y_p"), axis=0)
    y_s = np.concatenate(R("y_s"), axis=0)[:, None, :]
    outs = [y_p, y_s]
    for k in ["p_ssd", "p_conv", "p_rwkv", "p_shift", "p_gla"]:
        outs.append(np.stack(R(k), axis=1))
    for k in ["s_ssd", "s_conv", "s_rwkv", "s_shift", "s_gla"]:
        outs.append(np.concatenate(R(k), axis=1))
    return tuple(np.ascontiguousarray(o) for o in outs)


def kernel(**inputs):
    if "nc" not in _NC_CACHE:
        _NC_CACHE["nc"] = build()
    res = run_bass_kernel_spmd(_NC_CACHE["nc"], make_in_maps(inputs), core_ids=list(range(NCORES)))
    return gather(res.results)


def bc(ap, axis, shape):
    return ap.unsqueeze(axis).to_broadcast(list(shape))


def sigmoid_chain(K, ap, res):
    K.act(ap, ap, AF.Ln, res, res, bias=1.0)
    K.act(ap, ap, AF.Exp, res, res, scale=-1.0)


def rsqrt_(K, ap, res, scale, eps):
    K.act(ap, ap, AF.Ln, res, res, scale=scale, bias=eps)
    K.act(ap, ap, AF.Exp, res, res, scale=-0.5)


def softplus_(K, x, tmp, r, n):
    xa, ta = x[0], tmp[0]
    K.act(ta, xa, AF.Abs, [x[1]], [tmp[1]])
    K.act(ta, ta, AF.Exp, [tmp[1]], [tmp[1]], scale=-1.0)
    K.act(ta, ta, AF.Ln, [tmp[1]], [tmp[1]], bias=1.0)
    K.ts(xa, xa, 0.0, ALU.max, [x[1]], [x[1]])
    K.tt(xa, xa, ta, ALU.add, [x[1], tmp[1]], [x[1]])


def make_hc(K, C, l, hc, c):
    xT, mod = C["xT"], C["modT"][l]
    import os
    if os.environ.get("HC_POOL", "1") == "1":
        tmp = C["hc_tmp"]
        xs = xT.ap()[:, :, c * 128:(c + 1) * 128]
        K.tt(tmp.ap(), xs, bc(mod.ap()[:, SC1:SC1 + 8, 0], 2, [128, KT, 128]), ALU.mult,
             xr(xT, c // 4) + [mod.r()], [tmp.r()], eng="pool")
        K.tt(hc.ap(), tmp.ap(), bc(mod.ap()[:, SH1:SH1 + 8, 0], 2, [128, KT, 128]), ALU.add,
             [tmp.r(), mod.r()], [hc.r()], eng="pool")
        return
    for d in range(KT):
        K.act(hc.ap()[:, d, :], xT.ap()[:, d, c * 128:(c + 1) * 128], AF.Identity,
              [xT.r((d, c // 4)), mod.r()], [hc.r()],
              scale=mod.ap()[:, SC1 + d, 0:1], bias=mod.ap()[:, SH1 + d, 0:1])


def mixers(K, dr, C, l, yT, dbg_out):
    P = K.P
    xT, mod = C["xT"], C["modT"][l]
    en = C.get("enable", ("ssd", "rwkv", "gla"))
    with contextlib.ExitStack() as mx:
        hc = [K.sb(mx, f"hc{i}", [128, KT, 128], BF16) for i in range(2)]
        hs = K.sb(mx, "hs", [128, KT, NS], BF16)
        C["hc"], C["hs"] = hc, hs
        C["hc_tmp"] = K.sb(mx, "hc_tmp", [128, KT, 128], F32)
        modulate(K, C, l, xT, _Shift(hs, 2048), 4, SH1, SC1, lambda d: hs.r())
        for name, tiles in (("ssd", range(0, 4)), ("rwkv", range(4, 6)), ("gla", range(6, 8))):
            if name not in en:
                for d in tiles:
                    for b, (c0, n) in enumerate(BLOCKS):
                        K.memset(yT.ap()[:, d, c0:c0 + n], 0.0, [yT.r((d, b))])
        if "ssd" in en and "gla" in en:
            ssd_gla_phase(K, dr, C, l, yT, dbg_out)
            P.barrier()
        else:
            if "ssd" in en:
                ssd_phase(K, dr, C, l, yT, dbg_out)
                P.barrier()
            if "gla" in en:
                gla_phase(K, dr, C, l, yT, dbg_out)
                P.barrier()
        if "rwkv" in en:
            rwkv_phase(K, dr, C, l, yT, dbg_out)
            P.barrier()


class _Shift:
    def __init__(self, t, off):
        self.t, self.off = t, off

    def ap(self):
        return _ShiftAP(self.t.ap(), self.off)

    def r(self, key=0):
        return self.t.r()


class _ShiftAP:
    def __init__(self, ap, off):
        self._ap, self.off = ap, off

    def __getitem__(self, key):
        p, d, s = key
        return self._ap[p, d, slice(s.start - self.off, s.stop - self.off)]


def ssd_phase(K, dr, C, l, yT, dbg_out):
    P = K.P
    nc = K.nc
    identb, identf, maskU, maskSL, ones1 = C["identb"], C["identf"], C["maskU"], C["maskSL"], C["ones1"]
    hc, hs = C["hc"], C["hs"]
    with contextlib.ExitStack() as ph:
        win = K.sb(ph, "win_ssd", [128, KT, 1288], BF16)
        K.dma(win.ap(), dr["w_in"][l, :, 0:1288].rearrange("(k p) n -> p k n", p=128), w=[win.r()], q="pool")
        convw = K.sb(ph, "convw", [128, 6, 4], F32)
        convb = K.sb(ph, "convb", [128, 6], F32)
        for i in range(4):
            K.dma(convw.ap()[:, :, i], dr["ssd_conv_w"][l, i].rearrange("(t p) -> p t", p=128), w=[convw.r()])
        K.dma(convb.ap(), dr["ssd_conv_b"][l].rearrange("(t p) -> p t", p=128), w=[convb.r()])
        normg = K.sb(ph, "normg", [128, 4], F32)
        K.dma(normg.ap(), dr["ssd_norm_g"][l].rearrange("(t p) -> p t", p=128), w=[normg.r()])
        dtbB = K.sb(ph, "dtbB", [128, 8], F32)
        aB = K.sb(ph, "aB", [128, 8], F32)
        dB = K.sb(ph, "dB", [128, 8], F32)
        K.dma(dtbB.ap(), dr["ssd_dt_bias"][l:l + 1, :].to_broadcast([128, 8]), w=[dtbB.r()])
        K.dma(aB.ap(), dr["ssd_a_log"][l:l + 1, :].to_broadcast([128, 8]), w=[aB.r()])
        K.dma(dB.ap(), dr["ssd_d"][l:l + 1, :].to_broadcast([128, 8]), w=[dB.r()])
        K.act(aB.ap(), aB.ap(), AF.Exp, [aB.r()], [aB.r()])
        K.ts(aB.ap(), aB.ap(), -1.0, ALU.mult, [aB.r()], [aB.r()])
        import os
        if os.environ.get("SKIP_SSD_PROMPT") != "1":
            ssd_prompt(K, dr, C, l, yT, win, convw, convb, normg, dtbB, aB, dB, dbg_out)
        P.barrier()
        if os.environ.get("SKIP_SSD_SAMPLE") != "1":
            ssd_sample(K, dr, C, l, yT, win, aB, dB, dtbB, dbg_out)


def ssd_prompt(K, dr, C, l, yT, win, convw, convb, normg, dtbB, aB, dB, dbg_out):
    P = K.P
    identb, identf, maskU, maskSL, ones1 = C["identb"], C["identf"], C["maskU"], C["maskSL"], C["ones1"]
    hc, hs = C["hc"], C["hs"]
    with contextlib.ExitStack() as ph:
        XB = [K.sb(ph, f"XB{i}", [128, 6, 131], F32) for i in range(2)]
        XC = [K.sb(ph, f"XC{i}", [128, 6, 128], BF16) for i in range(2)]
        cacc = [K.sb(ph, f"cacc{i}", [128, 128], F32) for i in range(2)]
        sz = K.sb(ph, "sz", [128, 512], F32)
        dtt = K.sb(ph, "dtt", [128, 8], F32)
        dtmp = K.sb(ph, "dtmp", [128, 8], F32)
        dtA = K.sb(ph, "dtA", [128, 8], F32)
        csb = K.sb(ph, "csb", [128, 16], F32)
        e1 = K.sb(ph, "e1", [128, 8], F32)
        el = K.sb(ph, "el", [128, 8], F32)
        tail = K.sb(ph, "tail", [128, 8], F32)
        Rt = K.sb(ph, "Rt", [128, 8, 128], F32)
        dec = K.sb(ph, "dec", [128, 8, 128], F32)
        Gs = K.sb(ph, "Gs", [128, 2, 128], F32)
        Mb = K.sb(ph, "Mb", [128, 8, 128], BF16)
        XT = K.sb(ph, "XT", [128, 640], BF16)
        xD = K.sb(ph, "xD", [128, 512], BF16)
        xw = K.sb(ph, "xw", [128, 512], BF16)
        t1 = K.sb(ph, "t1", [128, 512], F32)
        yn = K.sb(ph, "yn", [128, 512], BF16)
        ss = K.sb(ph, "ss", [128, 1], F32)
        HS32 = K.sb(ph, "HS32", [128, 4, 64], F32)
        HSb = K.sb(ph, "HSb", [128, 4, 64], BF16)
        hsT = K.sb(ph, "hsT", [128, 2, 128], F32)

        ps_x = K.ps(ph, "ps_x", [128, 1024], F32)
        ps_z = K.ps(ph, "ps_z", [128, 512], F32)
        ps_c = K.ps(ph, "ps_c", [128, 512], F32)
        ps_t = K.ps(ph, "ps_t", [128, 1024], BF16)
        ps_y = K.ps(ph, "ps_y", [128, 512], F32)
        ps_i = K.ps(ph, "ps_i", [128, 512], F32)
        ps_h = K.ps(ph, "ps_h", [128, 512], F32)

        K.memset(HS32.ap(), 0.0, [HS32.r()])
        K.memset(HSb.ap(), 0.0, [HSb.r()])
        K.memset(XB[0].ap()[:, :, 0:3], 0.0, [XB[0].r()])

        for c in range(NCH):
            h = hc[c % 2]
            make_hc(K, C, l, h, c)
            xb, xc = XB[c % 2], XC[c % 2]
            for ct in range(6):
                for k in range(KT):
                    K.mm(ps_x.ap()[:, ct * 128:(ct + 1) * 128], win.ap()[:, k, 512 + ct * 128:512 + (ct + 1) * 128],
                         h.ap()[:, k, :], [win.r(), h.r()], [ps_x.r()], start=(k == 0), stop=(k == KT - 1),
                         inc=(k == KT - 1 and ct == 5))
            K.cp(xb.ap()[:, :, 3:131], ps_x.ap()[:, 0:768].rearrange("p (t n) -> p t n", t=6), [ps_x.r()], [xb.r()],
                 eng="act")
            if c + 1 < NCH:
                K.cp(XB[(c + 1) % 2].ap()[:, :, 0:3], xb.ap()[:, :, 128:131], [xb.r()], [XB[(c + 1) % 2].r()])
            for ct in range(6):
                ca = cacc[ct % 2]
                K.ts(ca.ap(), xb.ap()[:, ct, 0:128], convw.ap()[:, ct, 0:1], ALU.mult, [xb.r(), convw.r(), convb.r()],
                     [ca.r()], s2=convb.ap()[:, ct:ct + 1], op1=ALU.add)
                for i in range(1, 4):
                    K.stt(ca.ap(), xb.ap()[:, ct, i:i + 128], convw.ap()[:, ct, i:i + 1], ca.ap(), ALU.mult, ALU.add,
                          [xb.r(), convw.r(), ca.r()], [ca.r()])
                K.act(xc.ap()[:, ct, :], ca.ap(), AF.Silu, [ca.r()], [xc.r()])
            for k in range(KT):
                K.mm(ps_z.ap(), h.ap()[:, k, :], win.ap()[:, k, 0:512], [h.r(), win.r()], [ps_z.r()],
                     start=(k == 0), stop=(k == KT - 1))
            for k in range(KT):
                K.mm(ps_c.ap()[:, 0:8], h.ap()[:, k, :], win.ap()[:, k, 1280:1288], [h.r(), win.r()], [ps_c.r()],
                     start=(k == 0), stop=(k == KT - 1))
            K.act(sz.ap(), ps_z.ap(), AF.Silu, [ps_z.r()], [sz.r()])
            K.tt(dtt.ap(), ps_c.ap()[:, 0:8], dtbB.ap(), ALU.add, [ps_c.r(), dtbB.r()], [dtt.r()])
            softplus_(K, (dtt.ap(), dtt.r()), (dtmp.ap(), dtmp.r()), None, None)
            K.tt(dtA.ap(), dtt.ap(), aB.ap(), ALU.mult, [dtt.r(), aB.r()], [dtA.r()])
            K.mm(ps_c.ap()[:, 8:16], maskU.ap(), dtA.ap(), [maskU.r(), dtA.r()], [ps_c.r()])
            K.mm(ps_c.ap()[:, 16:24], ones1.ap(), dtA.ap(), [ones1.r(), dtA.r()], [ps_c.r()])
            K.tt(Rt.ap(), bc(maskU.ap(), 1, [128, 8, 128]), bc(dtA.ap(), 2, [128, 8, 128]), ALU.mult,
                 [maskU.r(), dtA.r()], [Rt.r()])
            for hf in range(2):
                K.mm(ps_x.ap()[:, hf * 512:(hf + 1) * 512], maskSL.ap(),
                     Rt.ap()[:, hf * 4:(hf + 1) * 4, :].rearrange("p h i -> p (h i)"), [maskSL.r(), Rt.r()],
                     [ps_x.r()])
            K.act(dec.ap().rearrange("p h i -> p (h i)"), ps_x.ap(), AF.Exp, [ps_x.r()], [dec.r()])
            K.cp(csb.ap(), ps_c.ap()[:, 8:24], [ps_c.r()], [csb.r()], eng="act")
            for g in range(2):
                K.mm(ps_c.ap()[:, 256 + g * 128:256 + (g + 1) * 128], xc.ap()[64 * g:64 * g + 64, 4, :],
                     xc.ap()[64 * g:64 * g + 64, 5, :], [xc.r()], [ps_c.r()], self_wait=(g == 1))
            K.tt(Gs.ap(), ps_c.ap()[:, 256:512].rearrange("p (g i) -> p g i", g=2), bc(maskU.ap(), 1, [128, 2, 128]),
                 ALU.mult, [ps_c.r(), maskU.r()], [Gs.r()])
            K.tt(dec.ap().rearrange("p (g r) i -> p g r i", g=2), dec.ap().rearrange("p (g r) i -> p g r i", g=2),
                 bc(Gs.ap(), 2, [128, 2, 4, 128]), ALU.mult, [dec.r(), Gs.r()], [dec.r()])
            K.tt(Mb.ap(), dec.ap(), bc(dtt.ap(), 2, [128, 8, 128]), ALU.mult, [dec.r(), dtt.r()], [Mb.r()])
            for ct in range(5):
                K.tr(ps_t.ap()[:, ct * 128:(ct + 1) * 128], xc.ap()[:, ct, :], identb.ap(), [xc.r(), identb.r()],
                     [ps_t.r()], inc=(ct == 4))
            K.cp(XT.ap(), ps_t.ap()[:, 0:640], [ps_t.r()], [XT.r()], eng="act")
            K.tt(xD.ap().rearrange("p (h q) -> p h q", h=8), XT.ap()[:, 0:512].rearrange("p (h q) -> p h q", h=8),
                 bc(dB.ap(), 2, [128, 8, 64]), ALU.mult, [XT.r(), dB.r()], [xD.r()])
            K.mm(ps_y.ap(), identb.ap(), xD.ap(), [identb.r(), xD.r()], [ps_y.r()], start=True, stop=False)
            for hh in range(8):
                K.mm(ps_y.ap()[:, hh * 64:(hh + 1) * 64], Mb.ap()[:, hh, :], XT.ap()[:, hh * 64:(hh + 1) * 64],
                     [Mb.r(), XT.r()], [ps_y.r()], start=False, stop=(hh == 7))
            for g in range(2):
                K.mm(ps_i.ap()[:, g * 256:(g + 1) * 256], xc.ap()[64 * g:64 * g + 64, 5, :],
                     HSb.ap()[64 * g:64 * g + 64, :, :].rearrange("p h q -> p (h q)"), [xc.r(), HSb.r()], [ps_i.r()],
                     self_wait=(g == 1))
            K.act(e1.ap(), csb.ap()[:, 0:8], AF.Exp, [csb.r()], [e1.r()])
            K.tt(t1.ap().rearrange("p (h q) -> p h q", h=8), ps_i.ap().rearrange("p (h q) -> p h q", h=8),
                 bc(e1.ap(), 2, [128, 8, 64]), ALU.mult, [ps_i.r(), e1.r()], [t1.r()])
            K.tt(t1.ap(), t1.ap(), ps_y.ap(), ALU.add, [t1.r(), ps_y.r()], [t1.r()])
            ssd_epilogue(K, C, t1, sz, ss, yn, 128)
            for q in range(4):
                K.tr(ps_t.ap()[:, q * 128:(q + 1) * 128], yn.ap()[:, q * 128:(q + 1) * 128], identb.ap(),
                     [yn.r(), identb.r()], [ps_t.r()], inc=(q == 3))
            K.tt(yT.ap()[:, 0:4, c * 128:(c + 1) * 128], ps_t.ap()[:, 0:512].rearrange("p (t n) -> p t n", t=4),
                 bc(normg.ap(), 2, [128, 4, 128]), ALU.mult, [ps_t.r(), normg.r()], xr(yT, c // 4, range(4)))
            K.act(el.ap(), csb.ap()[:, 8:16], AF.Exp, [csb.r()], [el.r()])
            K.tt(tail.ap(), csb.ap()[:, 8:16], csb.ap()[:, 0:8], ALU.subtract, [csb.r()], [tail.r()])
            K.act(tail.ap(), tail.ap(), AF.Exp, [tail.r()], [tail.r()])
            K.tt(tail.ap(), tail.ap(), dtt.ap(), ALU.mult, [tail.r(), dtt.r()], [tail.r()])
            K.tt(xw.ap().rearrange("p (h q) -> p h q", h=8), XT.ap()[:, 0:512].rearrange("p (h q) -> p h q", h=8),
                 bc(tail.ap(), 2, [128, 8, 64]), ALU.mult, [XT.r(), tail.r()], [xw.r()])
            K.mm(ps_h.ap(), XT.ap()[:, 512:640], xw.ap(), [XT.r(), xw.r()], [ps_h.r()])
            for g in range(2):
                sl = slice(64 * g, 64 * g + 64)
                K.tt(HS32.ap()[sl], HS32.ap()[sl], bc(el.ap()[sl, 4 * g:4 * g + 4], 2, [64, 4, 64]), ALU.mult,
                     [HS32.r(), el.r()], [HS32.r()])
                K.tt(HS32.ap()[sl], HS32.ap()[sl],
                     ps_h.ap()[sl, 256 * g:256 * g + 256].rearrange("p (h q) -> p h q", h=4), ALU.add,
                     [HS32.r(), ps_h.r()], [HS32.r()])
            K.cp(HSb.ap(), HS32.ap(), [HS32.r()], [HSb.r()], eng="act")

        xb = XB[(NCH - 1) % 2]
        for i in range(3):
            K.dma(dr["p_conv"][l, i].rearrange("(t p) -> p t", p=128), xb.ap()[:, :, 128 + i], r=[xb.r()])
        for q in range(2):
            K.tr(ps_y.ap()[:, q * 128:(q + 1) * 128], HS32.ap().rearrange("p h q -> p (h q)")[:, q * 128:(q + 1) * 128],
                 identf.ap(), [HS32.r(), identf.r()], [ps_y.r()], inc=(q == 1))
        K.cp(hsT.ap(), ps_y.ap()[:, 0:256].rearrange("p (q n) -> p q n", q=2), [ps_y.r()], [hsT.r()])
        for g in range(2):
            for q in range(2):
                K.dma(dr["p_ssd"][l, 4 * g + 2 * q:4 * g + 2 * q + 2].rearrange("h p n -> (h p) n"),
                      hsT.ap()[:, q, 64 * g:64 * g + 64], r=[hsT.r()])
        P.barrier()


def ssd_epilogue(K, C, y, sz, ss, yn, n):
    K.tt(y.ap()[0:n], y.ap()[0:n], sz.ap()[0:n], ALU.mult, [y.r(), sz.r()], [y.r()])
    K.act(yn.ap()[0:n], y.ap()[0:n], AF.Square, [y.r()], [yn.r(), ss.r()], accum_out=ss.ap()[0:n])
    rsqrt_(K, ss.ap()[0:n], [ss.r()], 1.0 / 512, RMS_EPS)
    K.ts(yn.ap()[0:n], y.ap()[0:n], ss.ap()[0:n], ALU.mult, [y.r(), ss.r()], [yn.r()])


def ssd_gla_phase(K, dr, C, l, yT, dbg_out):
    P = K.P
    identb, identf, maskU, maskSL, ones1 = C["identb"], C["identf"], C["maskU"], C["maskSL"], C["ones1"]
    hc, hs = C["hc"], C["hs"]
    G0 = OFF["gq"]
    with contextlib.ExitStack() as ph0:
        win = K.sb(ph0, "win_ssd", [128, KT, 1288], BF16)
        K.dma(win.ap(), dr["w_in"][l, :, 0:1288].rearrange("(k p) n -> p k n", p=128), w=[win.r()], q="pool")
        convw = K.sb(ph0, "convw", [128, 6, 4], F32)
        convb = K.sb(ph0, "convb", [128, 6], F32)
        for i in range(4):
            K.dma(convw.ap()[:, :, i], dr["ssd_conv_w"][l, i].rearrange("(t p) -> p t", p=128), w=[convw.r()])
        K.dma(convb.ap(), dr["ssd_conv_b"][l].rearrange("(t p) -> p t", p=128), w=[convb.r()])
        normg = K.sb(ph0, "normg", [128, 4], F32)
        K.dma(normg.ap(), dr["ssd_norm_g"][l].rearrange("(t p) -> p t", p=128), w=[normg.r()])
        dtbB = K.sb(ph0, "dtbB", [128, 8], F32)
        aB = K.sb(ph0, "aB", [128, 8], F32)
        dB = K.sb(ph0, "dB", [128, 8], F32)
        K.dma(dtbB.ap(), dr["ssd_dt_bias"][l:l + 1, :].to_broadcast([128, 8]), w=[dtbB.r()])
        K.dma(aB.ap(), dr["ssd_a_log"][l:l + 1, :].to_broadcast([128, 8]), w=[aB.r()])
        K.dma(dB.ap(), dr["ssd_d"][l:l + 1, :].to_broadcast([128, 8]), w=[dB.r()])
        K.act(aB.ap(), aB.ap(), AF.Exp, [aB.r()], [aB.r()])
        K.ts(aB.ap(), aB.ap(), -1.0, ALU.mult, [aB.r()], [aB.r()])
        wing = K.sb(ph0, "win_gla", [128, KT, 784], BF16)
        K.dma(wing.ap(), dr["w_in"][l, :, G0:G0 + 784].rearrange("(k p) n -> p k n", p=128), w=[wing.r()], q="pool")
        wgk2 = K.sb(ph0, "wgk2", [16, 128], BF16)
        K.dma(wgk2.ap(), dr["gla_w_gk2"][l], w=[wgk2.r()], q="pool")
        bgkB = K.sb(ph0, "bgkB", [128, 128], F32)
        K.dma(bgkB.ap(), dr["gla_b_gk"][l:l + 1, :].to_broadcast([128, 128]), w=[bgkB.r()])
        gcol = K.sb(ph0, "gcol", [128, 1], F32)
        for t in range(2):
            K.dma(gcol.ap()[64 * t:64 * t + 64, :], dr["gla_norm_g"][l].rearrange("(e o) -> e o", o=1), w=[gcol.r()])
        with contextlib.ExitStack() as ph:
            BM = K.sb(ph, "BM", [128, 256], F32)
            hm = K.sb(ph, "hm", [128, 4], F32)
            K.memset(BM.ap(), 1.0, [BM.r()], eng="pool")
            K.memset(hm.ap(), 1.0, [hm.r()], eng="pool")
            for hh in range(4):
                for (t, sl, n) in ((BM, slice(64 * hh, 64 * hh + 64), 64), (hm, slice(hh, hh + 1), 1)):
                    ap = t.ap()[:, sl]
                    K.P.op("pool", lambda e, ap=ap, n=n, hh=hh: e.affine_select(
                        out=ap, in_=ap, pattern=[[0, n]], compare_op=ALU.is_ge, fill=0.0, base=-32 * hh,
                        channel_multiplier=1), reads=[t.r()], writes=[t.r()])
                    K.P.op("pool", lambda e, ap=ap, n=n, hh=hh: e.affine_select(
                        out=ap, in_=ap, pattern=[[0, n]], compare_op=ALU.is_gt, fill=0.0, base=32 * hh + 32,
                        channel_multiplier=-1), reads=[t.r()], writes=[t.r()])
            PX = [K.ps(ph, f"PX{i}", [128, 512], F32) for i in range(2)]
            PZ = K.ps(ph, "PZ", [128, 512], F32)
            PC = K.ps(ph, "PC", [128, 512], F32)
            PF = K.ps(ph, "PF", [128, 512], F32)
            PV = K.ps(ph, "PV", [128, 512], F32)
            PL = K.ps(ph, "PL", [128, 512], F32)
            PT = K.ps(ph, "PT", [128, 1024], BF16)
            XB = [K.sb(ph, f"XB{i}", [128, 6, 131], BF16) for i in range(2)]
            DW = K.sb(ph, "DW", [128, 6, 4, 128], BF16)
            negb = K.sb(ph, "negb", [128, 6], F32)
            e6 = K.sb(ph, "e6", [128, 6, 128], F32)
            xlast = K.sb(ph, "xlast", [128, 6, 3], F32)
            K.ts(negb.ap(), convb.ap(), -1.0, ALU.mult, [convb.r()], [negb.r()])
            for ct in range(6):
                for i in range(4):
                    K.ts(DW.ap()[:, ct, i, :], identf.ap(), convw.ap()[:, ct, i:i + 1], ALU.mult,
                         [identf.r(), convw.r()], [DW.r()])
            XC = [K.sb(ph, f"XC{i}", [128, 6, 128], BF16) for i in range(2)]
            sz = K.sb(ph, "sz", [128, 512], F32)
            dtt = K.sb(ph, "dtt", [128, 8], F32)
            dtmp = K.sb(ph, "dtmp", [128, 8], F32)
            dtA = K.sb(ph, "dtA", [128, 8], F32)
            csb = K.sb(ph, "csb", [128, 16], F32)
            e1 = K.sb(ph, "e1", [128, 8], F32)
            el = K.sb(ph, "el", [128, 8], F32)
            tail = K.sb(ph, "tail", [128, 8], F32)
            Rt = K.sb(ph, "Rt", [128, 8, 128], F32)
            dec = K.sb(ph, "dec", [128, 8, 128], F32)
            Gs = K.sb(ph, "Gs", [128, 2, 128], F32)
            Mb = K.sb(ph, "Mb", [128, 8, 128], BF16)
            XT = K.sb(ph, "XT", [128, 640], BF16)
            xD = K.sb(ph, "xD", [128, 512], BF16)
            xw = K.sb(ph, "xw", [128, 512], BF16)
            t1 = K.sb(ph, "t1", [128, 512], F32)
            yn = K.sb(ph, "yn", [128, 512], BF16)
            ss = K.sb(ph, "ss", [128, 1], F32)
            HS32 = K.sb(ph, "HS32", [128, 4, 64], F32)
            HSb = K.sb(ph, "HSb", [128, 4, 64], BF16)
            glo = K.sb(ph, "glo", [16, 128], BF16)
            lg = K.sb(ph, "lg", [128, 128], F32)
            lgt = K.sb(ph, "lgt", [128, 128], F32)
            Eq = K.sb(ph, "Eq", [128, 128], F32)
            Ek = K.sb(ph, "Ek", [128, 128], F32)
            Ekt = K.sb(ph, "Ekt", [128, 128], F32)
            qt = K.sb(ph, "qt", [128, 128], BF16)
            kf = K.sb(ph, "kf", [128, 128], F32)
            km = K.sb(ph, "km", [128, 4, 128], BF16)
            ktm = K.sb(ph, "ktm", [128, 128], BF16)
            vtm = K.sb(ph, "vtm", [128, 256], BF16)
            sgg = K.sb(ph, "sgg", [128, 256], F32)
            A = K.sb(ph, "A", [128, 4, 128], BF16)
            osq = K.sb(ph, "osq", [128, 256], F32)
            ms = K.sb(ph, "ms", [128, 4], F32)
            on = K.sb(ph, "on", [128, 256], F32)
            onb = K.sb(ph, "onb", [128, 256], BF16)
            tmpS = K.sb(ph, "tmpS", [128, 256], F32)
            S32 = K.sb(ph, "S32", [128, 256], F32)
            Sb = K.sb(ph, "Sb", [128, 256], BF16)

            K.memset(HS32.ap(), 0.0, [HS32.r()])
            K.memset(HSb.ap(), 0.0, [HSb.r()])
            K.memset(XB[0].ap()[:, :, 0:3], 0.0, [XB[0].r()])
            K.memset(S32.ap(), 0.0, [S32.r()])
            K.memset(Sb.ap(), 0.0, [Sb.r()])

            def ssd_body(c):
                h = hc[c % 2]
                xb, xc = XB[c % 2], XC[c % 2]
                for ct in range(6):
                    px = PX[0] if ct < 4 else PX[1]
                    cc = ct if ct < 4 else ct - 4
                    for k in range(KT):
                        K.mm(px.ap()[:, cc * 128:(cc + 1) * 128], win.ap()[:, k, 512 + ct * 128:512 + (ct + 1) * 128],
                             h.ap()[:, k, :], [win.r(), h.r()], [px.r()], start=(k == 0), stop=(k == KT - 1))
                    yield
                K.cp(xb.ap()[:, 0:4, 3:131], PX[0].ap().rearrange("p (t n) -> p t n", t=4), [PX[0].r()], [xb.r()],
                     eng="act")
                yield
                K.cp(xb.ap()[:, 4:6, 3:131], PX[1].ap()[:, 0:256].rearrange("p (t n) -> p t n", t=2), [PX[1].r()],
                     [xb.r()], eng="act")
                yield
                if c + 1 < NCH:
                    K.cp(XB[(c + 1) % 2].ap()[:, :, 0:3], xb.ap()[:, :, 128:131], [xb.r()], [XB[(c + 1) % 2].r()])
                if c == NCH - 1:
                    K.cp(xlast.ap()[:, 0:4, :], PX[0].ap().rearrange("p (t n) -> p t n", t=4)[:, :, 125:128],
                         [PX[0].r()], [xlast.r()])
                    K.cp(xlast.ap()[:, 4:6, :], PX[1].ap()[:, 0:256].rearrange("p (t n) -> p t n", t=2)[:, :, 125:128],
                         [PX[1].r()], [xlast.r()])
                for ct in range(6):
                    px = PX[0] if ct < 4 else PX[1]
                    cc = ct if ct < 4 else ct - 4
                    for i in range(4):
                        K.mm(px.ap()[:, cc * 128:(cc + 1) * 128], DW.ap()[:, ct, i, :], xb.ap()[:, ct, i:i + 128],
                             [DW.r(), xb.r()], [px.r()], start=(i == 0), stop=(i == 3))
                    yield
                for ct in range(6):
                    px = PX[0] if ct < 4 else PX[1]
                    cc = ct if ct < 4 else ct - 4
                    K.act(e6.ap()[:, ct, :], px.ap()[:, cc * 128:(cc + 1) * 128], AF.Exp, [px.r(), negb.r()], [e6.r()],
                          scale=-1.0, bias=negb.ap()[:, ct:ct + 1])
                    yield
                sigmoid_chain(K, e6.ap(), [e6.r()])
                yield
                for ct in range(6):
                    px = PX[0] if ct < 4 else PX[1]
                    cc = ct if ct < 4 else ct - 4
                    K.stt(xc.ap()[:, ct, :], px.ap()[:, cc * 128:(cc + 1) * 128], convb.ap()[:, ct:ct + 1], e6.ap()[:, ct, :],
                          ALU.add, ALU.mult, [px.r(), convb.r(), e6.r()], [xc.r()])
                    yield
                for k in range(KT):
                    K.mm(PZ.ap(), h.ap()[:, k, :], win.ap()[:, k, 0:512], [h.r(), win.r()], [PZ.r()],
                         start=(k == 0), stop=(k == KT - 1))
                for k in range(KT):
                    K.mm(PC.ap()[:, 0:8], h.ap()[:, k, :], win.ap()[:, k, 1280:1288], [h.r(), win.r()], [PC.r()],
                         start=(k == 0), stop=(k == KT - 1))
                yield
                K.act(sz.ap(), PZ.ap(), AF.Exp, [PZ.r()], [sz.r()], scale=-1.0)
                yield
                sigmoid_chain(K, sz.ap(), [sz.r()])
                yield
                K.tt(sz.ap(), sz.ap(), PZ.ap(), ALU.mult, [sz.r(), PZ.r()], [sz.r()])
                yield
                K.tt(dtt.ap(), PC.ap()[:, 0:8], dtbB.ap(), ALU.add, [PC.r(), dtbB.r()], [dtt.r()])
                yield
                softplus_(K, (dtt.ap(), dtt.r()), (dtmp.ap(), dtmp.r()), None, None)
                yield
                K.tt(dtA.ap(), dtt.ap(), aB.ap(), ALU.mult, [dtt.r(), aB.r()], [dtA.r()])
                yield
                K.mm(PC.ap()[:, 8:16], maskU.ap(), dtA.ap(), [maskU.r(), dtA.r()], [PC.r()])
                K.mm(PC.ap()[:, 16:24], ones1.ap(), dtA.ap(), [ones1.r(), dtA.r()], [PC.r()])
                yield
                K.tt(Rt.ap(), bc(maskU.ap(), 1, [128, 8, 128]), bc(dtA.ap(), 2, [128, 8, 128]), ALU.mult,
                     [maskU.r(), dtA.r()], [Rt.r()])
                yield
                for hf in range(2):
                    K.mm(PX[hf].ap(), maskSL.ap(), Rt.ap()[:, hf * 4:(hf + 1) * 4, :].rearrange("p h i -> p (h i)"),
                         [maskSL.r(), Rt.r()], [PX[hf].r()])
                    yield
                for hf in range(2):
                    K.act(dec.ap()[:, hf * 4:(hf + 1) * 4, :].rearrange("p h i -> p (h i)"), PX[hf].ap(), AF.Exp,
                          [PX[hf].r()], [dec.r()])
                    yield
                K.cp(csb.ap(), PC.ap()[:, 8:24], [PC.r()], [csb.r()], eng="act")
                yield
                for g in range(2):
                    K.mm(PC.ap()[:, 256 + g * 128:256 + (g + 1) * 128], xc.ap()[64 * g:64 * g + 64, 4, :],
                         xc.ap()[64 * g:64 * g + 64, 5, :], [xc.r()], [PC.r()], self_wait=(g == 1))
                yield
                K.tt(Gs.ap(), PC.ap()[:, 256:512].rearrange("p (g i) -> p g i", g=2), bc(maskU.ap(), 1, [128, 2, 128]),
                     ALU.mult, [PC.r(), maskU.r()], [Gs.r()])
                yield
                K.tt(dec.ap().rearrange("p (g r) i -> p g r i", g=2), dec.ap().rearrange("p (g r) i -> p g r i", g=2),
                     bc(Gs.ap(), 2, [128, 2, 4, 128]), ALU.mult, [dec.r(), Gs.r()], [dec.r()])
                yield
                K.tt(Mb.ap(), dec.ap(), bc(dtt.ap(), 2, [128, 8, 128]), ALU.mult, [dec.r(), dtt.r()], [Mb.r()])
                yield
                for ct in range(5):
                    K.tr(PT.ap()[:, ct * 128:(ct + 1) * 128], xc.ap()[:, ct, :], identb.ap(), [xc.r(), identb.r()],
                         [PT.r()], inc=(ct == 4))
                yield
                K.cp(XT.ap(), PT.ap()[:, 0:640], [PT.r()], [XT.r()], eng="act")
                yield
                K.tt(xD.ap().rearrange("p (h q) -> p h q", h=8), XT.ap()[:, 0:512].rearrange("p (h q) -> p h q", h=8),
                     bc(dB.ap(), 2, [128, 8, 64]), ALU.mult, [XT.r(), dB.r()], [xD.r()])
                yield
                K.mm(PZ.ap(), identb.ap(), xD.ap(), [identb.r(), xD.r()], [PZ.r()], start=True, stop=False)
                for hh in range(8):
                    K.mm(PZ.ap()[:, hh * 64:(hh + 1) * 64], Mb.ap()[:, hh, :], XT.ap()[:, hh * 64:(hh + 1) * 64],
                         [Mb.r(), XT.r()], [PZ.r()], start=False, stop=(hh == 7))
                for g in range(2):
                    K.mm(PC.ap()[:, g * 256:(g + 1) * 256], xc.ap()[64 * g:64 * g + 64, 5, :],
                         HSb.ap()[64 * g:64 * g + 64, :, :].rearrange("p h q -> p (h q)"), [xc.r(), HSb.r()], [PC.r()],
                         self_wait=(g == 1))
                yield
                K.act(e1.ap(), csb.ap()[:, 0:8], AF.Exp, [csb.r()], [e1.r()])
                yield
                K.tt(t1.ap().rearrange("p (h q) -> p h q", h=8), PC.ap().rearrange("p (h q) -> p h q", h=8),
                     bc(e1.ap(), 2, [128, 8, 64]), ALU.mult, [PC.r(), e1.r()], [t1.r()])
                yield
                K.tt(t1.ap(), t1.ap(), PZ.ap(), ALU.add, [t1.r(), PZ.r()], [t1.r()])
                yield
                ssd_epilogue(K, C, t1, sz, ss, yn, 128)
                yield
                for q in range(4):
                    K.tr(PT.ap()[:, q * 128:(q + 1) * 128], yn.ap()[:, q * 128:(q + 1) * 128], identb.ap(),
                         [yn.r(), identb.r()], [PT.r()], inc=(q == 3))
                yield
                K.tt(yT.ap()[:, 0:4, c * 128:(c + 1) * 128], PT.ap()[:, 0:512].rearrange("p (t n) -> p t n", t=4),
                     bc(normg.ap(), 2, [128, 4, 128]), ALU.mult, [PT.r(), normg.r()], xr(yT, c // 4, range(4)))
                yield
                K.act(el.ap(), csb.ap()[:, 8:16], AF.Exp, [csb.r()], [el.r()])
                yield
                K.tt(tail.ap(), csb.ap()[:, 8:16], csb.ap()[:, 0:8], ALU.subtract, [csb.r()], [tail.r()])
                yield
                K.act(tail.ap(), tail.ap(), AF.Exp, [tail.r()], [tail.r()])
                yield
                K.tt(tail.ap(), tail.ap(), dtt.ap(), ALU.mult, [tail.r(), dtt.r()], [tail.r()])
                yield
                K.tt(xw.ap().rearrange("p (h q) -> p h q", h=8), XT.ap()[:, 0:512].rearrange("p (h q) -> p h q", h=8),
                     bc(tail.ap(), 2, [128, 8, 64]), ALU.mult, [XT.r(), tail.r()], [xw.r()])
                yield
                K.mm(PC.ap(), XT.ap()[:, 512:640], xw.ap(), [XT.r(), xw.r()], [PC.r()])
                yield
                for g in range(2):
                    sl = slice(64 * g, 64 * g + 64)
                    K.tt(HS32.ap()[sl], HS32.ap()[sl], bc(el.ap()[sl, 4 * g:4 * g + 4], 2, [64, 4, 64]), ALU.mult,
                         [HS32.r(), el.r()], [HS32.r()])
                    K.tt(HS32.ap()[sl], HS32.ap()[sl],
                         PC.ap()[sl, 256 * g:256 * g + 256].rearrange("p (h q) -> p h q", h=4), ALU.add,
                         [HS32.r(), PC.r()], [HS32.r()])
                    yield
                K.cp(HSb.ap(), HS32.ap(), [HS32.r()], [HSb.r()], eng="act")
                yield

            def gla_body(c):
                h = hc[c % 2]
                for (dst, cols) in ((PF.ap()[:, 0:128], slice(0, 128)), (PF.ap()[:, 128:256], slice(128, 256)),
                                    (PF.ap()[0:16, 256:384], slice(512, 528))):
                    for k in range(KT):
                        K.mm(dst, wing.ap()[:, k, cols], h.ap()[:, k, :], [wing.r(), h.r()], [PF.r()],
                             start=(k == 0), stop=(k == KT - 1))
                    yield
                for (dst, cols, pst) in ((PV.ap()[:, 0:256], slice(256, 512), PV), (PF.ap()[:, 384:512], slice(128, 256), PF),
                                         (PV.ap()[:, 256:512], slice(528, 784), PV)):
                    for k in range(KT):
                        K.mm(dst, h.ap()[:, k, :], wing.ap()[:, k, cols], [wing.r(), h.r()], [pst.r()],
                             start=(k == 0), stop=(k == KT - 1))
                    yield
                K.cp(glo.ap(), PF.ap()[0:16, 256:384], [PF.r()], [glo.r()], eng="act")
                yield
                K.mm(PL.ap()[:, 0:128], glo.ap(), wgk2.ap(), [glo.r(), wgk2.r()], [PL.r()])
                yield
                K.stt(lg.ap(), PL.ap()[:, 0:128], -1.0, bgkB.ap(), ALU.mult, ALU.subtract, [PL.r(), bgkB.r()], [lg.r()])
                yield
                softplus_(K, (lg.ap(), lg.r()), (lgt.ap(), lgt.r()), None, None)
                yield
                K.ts(lg.ap(), lg.ap(), -1.0 / 16.0, ALU.mult, [lg.r()], [lg.r()])
                yield
                K.mm(PL.ap()[:, 128:256], lg.ap(), maskU.ap(), [lg.r(), maskU.r()], [PL.r()])
                K.mm(PL.ap()[:, 256:384], maskU.ap(), lg.ap(), [lg.r(), maskU.r()], [PL.r()])
                yield
                K.act(Eq.ap(), PL.ap()[:, 128:256], AF.Exp, [PL.r()], [Eq.r()])
                yield
                K.act(Ek.ap(), PL.ap()[:, 128:256], AF.Exp, [PL.r()], [Ek.r()], scale=-1.0)
                yield
                K.act(Ekt.ap(), PL.ap()[:, 256:384], AF.Exp, [PL.r()], [Ekt.r()], scale=-1.0)
                yield
                K.stt(qt.ap(), PF.ap()[:, 0:128], 32.0 ** -0.5, Eq.ap(), ALU.mult, ALU.mult, [PF.r(), Eq.r()], [qt.r()])
                yield
                K.tt(kf.ap(), PF.ap()[:, 128:256], Ek.ap(), ALU.mult, [PF.r(), Ek.r()], [kf.r()])
                yield
                K.tt(km.ap(), bc(kf.ap(), 1, [128, 4, 128]), bc(hm.ap(), 2, [128, 4, 128]), ALU.mult, [kf.r(), hm.r()],
                     [km.r()])
                yield
                K.tt(ktm.ap(), PF.ap()[:, 384:512], Ekt.ap(), ALU.mult, [PF.r(), Ekt.r()], [ktm.r()])
                yield
                K.cp(vtm.ap(), PV.ap()[:, 0:256], [PV.r()], [vtm.r()], eng="act")
                yield
                K.act(sgg.ap(), PV.ap()[:, 256:512], AF.Exp, [PV.r()], [sgg.r()], scale=-1.0)
                yield
                sigmoid_chain(K, sgg.ap(), [sgg.r()])
                yield
                K.tt(sgg.ap(), sgg.ap(), PV.ap()[:, 256:512], ALU.mult, [sgg.r(), PV.r()], [sgg.r()])
                yield
                for hh in range(4):
                    K.mm(PL.ap()[:, hh * 128:(hh + 1) * 128], km.ap()[:, hh, :], qt.ap(), [km.r(), qt.r()], [PL.r()],
                         inc=(hh == 3))
                yield
                K.tt(A.ap(), PL.ap().rearrange("p (h i) -> p h i", h=4), bc(maskU.ap(), 1, [128, 4, 128]), ALU.mult,
                     [PL.r(), maskU.r()], [A.r()])
                yield
                K.mm(PL.ap()[:, 0:256], qt.ap(), Sb.ap(), [qt.r(), Sb.r()], [PL.r()], start=True, stop=False)
                for hh in range(4):
                    K.mm(PL.ap()[:, hh * 64:(hh + 1) * 64], A.ap()[:, hh, :], vtm.ap()[:, hh * 64:(hh + 1) * 64],
                         [A.r(), vtm.r()], [PL.r()], start=False, stop=(hh == 3))
                yield
                K.act(osq.ap(), PL.ap()[:, 0:256], AF.Square, [PL.r()], [osq.r()])
                yield
                K.red(ms.ap(), osq.ap().rearrange("p (h e) -> p h e", h=4), [osq.r()], [ms.r()])
                yield
                rsqrt_(K, ms.ap(), [ms.r()], 1.0 / 64, RMS_EPS)
                yield
                K.tt(on.ap().rearrange("p (h e) -> p h e", h=4), PL.ap()[:, 0:256].rearrange("p (h e) -> p h e", h=4),
                     bc(ms.ap(), 2, [128, 4, 64]), ALU.mult, [PL.r(), ms.r()], [on.r()])
                yield
                K.tt(onb.ap(), on.ap(), sgg.ap(), ALU.mult, [on.r(), sgg.r()], [onb.r()])
                yield
                for q in range(2):
                    K.tr(PT.ap()[:, 768 + q * 128:768 + (q + 1) * 128], onb.ap()[:, q * 128:(q + 1) * 128], identb.ap(),
                         [onb.r(), identb.r()], [PT.r()], inc=(q == 1))
                yield
                K.ts(yT.ap()[:, 6:8, c * 128:(c + 1) * 128], PT.ap()[:, 768:1024].rearrange("p (t n) -> p t n", t=2),
                     gcol.ap(), ALU.mult, [PT.r(), gcol.r()], xr(yT, c // 4, range(6, 8)))
                yield
                K.mm(PL.ap()[:, 256:512], ktm.ap(), vtm.ap(), [ktm.r(), vtm.r()], [PL.r()])
                yield
                K.tt(tmpS.ap(), PL.ap()[:, 256:512], BM.ap(), ALU.mult, [PL.r(), BM.r()], [tmpS.r()])
                yield
                K.tt(S32.ap(), S32.ap(), tmpS.ap(), ALU.add, [S32.r(), tmpS.r()], [S32.r()])
                yield
                K.ts(S32.ap(), S32.ap(), Eq.ap()[:, 127:128], ALU.mult, [S32.r(), Eq.r()], [S32.r()])
                yield
                K.cp(Sb.ap(), S32.ap(), [S32.r()], [Sb.r()], eng="act")
                yield

            for c in range(NCH):
                make_hc(K, C, l, hc[c % 2], c)
                interleave([ssd_body(c), gla_body(c)], ratio=[2, 1])

            xb = XB[(NCH - 1) % 2]
            for i in range(3):
                K.dma(dr["p_conv"][l, i].rearrange("(t p) -> p t", p=128), xlast.ap()[:, :, i], r=[xlast.r()])
            hsT = t1
            for q in range(2):
                K.tr(PZ.ap()[:, q * 128:(q + 1) * 128], HS32.ap().rearrange("p h q -> p (h q)")[:, q * 128:(q + 1) * 128],
                     identf.ap(), [HS32.r(), identf.r()], [PZ.r()], inc=(q == 1))
            K.cp(hsT.ap()[:, 0:256], PZ.ap()[:, 0:256], [PZ.r()], [hsT.r()])
            for g in range(2):
                for q in range(2):
                    K.dma(dr["p_ssd"][l, 4 * g + 2 * q:4 * g + 2 * q + 2].rearrange("h p n -> (h p) n"),
                          hsT.ap()[:, q * 128 + 64 * g:q * 128 + 64 * g + 64], r=[hsT.r()])
            for hh in range(4):
                K.dma(dr["p_gla"][l, hh], S32.ap()[32 * hh:32 * hh + 32, 64 * hh:64 * hh + 64], r=[S32.r()])
            P.barrier()
        ssd_sample(K, dr, C, l, yT, win, aB, dB, dtbB, dbg_out)
        gla_sample(K, dr, C, l, yT, wing, wgk2, bgkB)


def dram_scratch(K, name, shape):
    K.uid += 1
    h = K.nc.dram_tensor(f"scr_{name}_{K.uid}", list(shape), F32)
    return Tn(h, name)


def ssd_sample(K, dr, C, l, yT, win, aB, dB, dtbB, dbg_out):
    P = K.P
    hs, identb = C["hs"], C["identb"]
    with contextlib.ExitStack() as ph:
        cs = K.sb(ph, "cs", [NS, 768], F32)
        wB = K.sb(ph, "wB", [NS, 768], F32)
        gB = K.sb(ph, "gB", [NS, 512], F32)
        xbcs = K.sb(ph, "xbcs", [NS, 768], F32)
        acc = K.sb(ph, "acc", [NS, 768], F32)
        tmpc = K.sb(ph, "tmpc", [NS, 768], F32)
        szs = K.sb(ph, "szs", [NS, 512], F32)
        dts = K.sb(ph, "dts", [NS, 8], F32)
        dtm = K.sb(ph, "dtm", [NS, 8], F32)
        rep = K.sb(ph, "rep", [NS, 2, 8, 64], F32)
        pk = K.sb(ph, "pk", [NS, 8, 3], F32)
        Hs = K.sb(ph, "Hs", [128, 64, 64], F32)
        tmpH = K.sb(ph, "tmpH", [128, 32, 64], F32)
        xh = K.sb(ph, "xh", [128, 64], F32)
        BCh = K.sb(ph, "BCh", [128, 2, 64], F32)
        pkh = K.sb(ph, "pkh", [128, 3], F32)
        dA = K.sb(ph, "dA", [128, 1], F32)
        xdt = K.sb(ph, "xdt", [128, 64], F32)
        yh = K.sb(ph, "yh", [128, 64], F32)
        ysm = K.sb(ph, "ysm", [NS, 512], F32)
        yns = K.sb(ph, "yns", [NS, 512], BF16)
        sss = K.sb(ph, "sss", [NS, 1], F32)
        ps_a = K.ps(ph, "pss_a", [128, 512], F32)
        ps_b = K.ps(ph, "pss_b", [128, 512], F32)
        ps_d = K.ps(ph, "pss_d", [128, 512], F32)
        ps_t = K.ps(ph, "pss_t", [128, 1024], BF16)
        sx = dram_scratch(K, "sx", [NS, 512])
        sbc = dram_scratch(K, "sbc", [2, NS, 512])
        spk = dram_scratch(K, "spk", [NS, 24])
        sy = dram_scratch(K, "sy", [NS, 512])

        K.dma(gB.ap(), dr["ssd_norm_g"][l:l + 1, :].to_broadcast([NS, 512]), w=[gB.r()])
        K.dma(Hs.ap().rearrange("p a b -> p (a b)"), dr["st_ssd"][l].rearrange("b h p n -> (b h) (p n)"), w=[Hs.r()])
        for k in range(KT):
            K.mm(ps_a.ap()[0:NS, :], hs.ap()[:, k, :], win.ap()[:, k, 0:512], [hs.r(), win.r()], [ps_a.r()],
                 start=(k == 0), stop=(k == KT - 1))
        for k in range(KT):
            K.mm(ps_b.ap()[0:NS, :], hs.ap()[:, k, :], win.ap()[:, k, 512:1024], [hs.r(), win.r()], [ps_b.r()],
                 start=(k == 0), stop=(k == KT - 1))
        for k in range(KT):
            K.mm(ps_d.ap()[0:NS, 0:264], hs.ap()[:, k, :], win.ap()[:, k, 1024:1288], [hs.r(), win.r()], [ps_d.r()],
                 start=(k == 0), stop=(k == KT - 1))
        K.act(szs.ap(), ps_a.ap()[0:NS, :], AF.Silu, [ps_a.r()], [szs.r()])
        K.cp(xbcs.ap()[:, 0:512], ps_b.ap()[0:NS, :], [ps_b.r()], [xbcs.r()], eng="act")
        K.cp(xbcs.ap()[:, 512:768], ps_d.ap()[0:NS, 0:256], [ps_d.r()], [xbcs.r()], eng="act")
        K.tt(dts.ap(), ps_d.ap()[0:NS, 256:264], dtbB.ap()[0:NS, :], ALU.add, [ps_d.r(), dtbB.r()], [dts.r()])
        softplus_(K, (dts.ap(), dts.r()), (dtm.ap(), dtm.r()), None, None)
        K.dma(wB.ap(), dr["ssd_conv_w"][l, 3:4, :].to_broadcast([NS, 768]), w=[wB.r()])
        K.tt(acc.ap(), xbcs.ap(), wB.ap(), ALU.mult, [xbcs.r(), wB.r()], [acc.r()])
        for i in range(3):
            K.dma(wB.ap(), dr["ssd_conv_w"][l, i:i + 1, :].to_broadcast([NS, 768]), w=[wB.r()])
            K.dma(cs.ap(), dr["st_conv"][l][:, i, :], w=[cs.r()])
            K.tt(tmpc.ap(), cs.ap(), wB.ap(), ALU.mult, [cs.r(), wB.r()], [tmpc.r()])
            K.tt(acc.ap(), acc.ap(), tmpc.ap(), ALU.add, [acc.r(), tmpc.r()], [acc.r()])
        K.dma(wB.ap(), dr["ssd_conv_b"][l:l + 1, :].to_broadcast([NS, 768]), w=[wB.r()])
        K.tt(acc.ap(), acc.ap(), wB.ap(), ALU.add, [acc.r(), wB.r()], [acc.r()])
        K.act(acc.ap(), acc.ap(), AF.Silu, [acc.r()], [acc.r()])
        K.dma(dr["s_conv"][l][:, 0:2, :], dr["st_conv"][l][:, 1:3, :])
        K.dma(dr["s_conv"][l][:, 2, :], xbcs.ap(), r=[xbcs.r()])
        K.dma(sx.ap(), acc.ap()[:, 0:512], r=[acc.r()], w=[sx.r()])
        K.cp(rep.ap().rearrange("p t (g r) n -> p t g r n", g=2),
             bc(acc.ap()[:, 512:768].rearrange("p (t g n) -> p t g n", t=2, g=2), 3, [NS, 2, 2, 4, 64]),
             [acc.r()], [rep.r()])
        K.cp(pk.ap()[:, :, 0], dts.ap(), [dts.r()], [pk.r()])
        K.cp(pk.ap()[:, :, 1], aB.ap()[0:NS, :], [aB.r()], [pk.r()])
        K.cp(pk.ap()[:, :, 2], dB.ap()[0:NS, :], [dB.r()], [pk.r()])
        K.dma(sbc.ap().rearrange("t b x -> b t x"), rep.ap().rearrange("p t h n -> p t (h n)"), r=[rep.r()],
              w=[sbc.r()])
        K.dma(spk.ap(), pk.ap().rearrange("p h q -> p (h q)"), r=[pk.r()], w=[spk.r()])
        K.dma(xh.ap(), sx.ap().rearrange("b (h p) -> (b h) p", h=8), r=[sx.r()], w=[xh.r()])
        for t in range(2):
            K.dma(BCh.ap()[:, t, :], sbc.ap()[t].rearrange("b (h n) -> (b h) n", h=8), r=[sbc.r()],
                  w=[BCh.r()])
        K.dma(pkh.ap(), spk.ap().rearrange("b (h q) -> (b h) q", h=8), r=[spk.r()], w=[pkh.r()])
        K.act(dA.ap(), pkh.ap()[:, 0:1], AF.Exp, [pkh.r()], [dA.r()], scale=pkh.ap()[:, 1:2])
        K.ts(xdt.ap(), xh.ap(), pkh.ap()[:, 0:1], ALU.mult, [xh.r(), pkh.r()], [xdt.r()])
        K.ts(Hs.ap(), Hs.ap(), dA.ap(), ALU.mult, [Hs.r(), dA.r()], [Hs.r()])
        for hf in range(2):
            sl = slice(32 * hf, 32 * hf + 32)
            K.tt(tmpH.ap(), bc(xdt.ap()[:, sl], 2, [128, 32, 64]), bc(BCh.ap()[:, 0, :], 1, [128, 32, 64]), ALU.mult,
                 [xdt.r(), BCh.r()], [tmpH.r()])
            K.tt(Hs.ap()[:, sl, :], Hs.ap()[:, sl, :], tmpH.ap(), ALU.add, [Hs.r(), tmpH.r()], [Hs.r()])
        K.dma(dr["s_ssd"][l].rearrange("b h p n -> (b h) (p n)"), Hs.ap().rearrange("p a b -> p (a b)"), r=[Hs.r()])
        for hf in range(2):
            sl = slice(32 * hf, 32 * hf + 32)
            K.tt(tmpH.ap(), Hs.ap()[:, sl, :], bc(BCh.ap()[:, 1, :], 1, [128, 32, 64]), ALU.mult, [Hs.r(), BCh.r()],
                 [tmpH.r()])
            K.red(yh.ap()[:, sl], tmpH.ap(), [tmpH.r()], [yh.r()])
        K.stt(yh.ap(), xh.ap(), pkh.ap()[:, 2:3], yh.ap(), ALU.mult, ALU.add, [xh.r(), pkh.r(), yh.r()], [yh.r()])
        K.dma(sy.ap().rearrange("b (h p) -> (b h) p", h=8), yh.ap(), r=[yh.r()], w=[sy.r()])
        K.dma(ysm.ap(), sy.ap(), r=[sy.r()], w=[ysm.r()])
        ssd_epilogue(K, C, ysm, szs, sss, yns, NS)
        K.tt(ysm.ap(), ysm.ap(), gB.ap(), ALU.mult, [ysm.r(), gB.r()], [ysm.r()])
        K.ts(yns.ap(), ysm.ap(), sss.ap(), ALU.mult, [ysm.r(), sss.r()], [yns.r()])
        for q in range(4):
            K.tr(ps_t.ap()[:, q * NS:(q + 1) * NS], yns.ap()[:, q * 128:(q + 1) * 128], identb.ap()[0:NS, 0:NS],
                 [yns.r(), identb.r()], [ps_t.r()], inc=(q == 3))
        K.cp(yT.ap()[:, 0:4, T:T + NS], ps_t.ap()[:, 0:4 * NS].rearrange("p (t n) -> p t n", t=4), [ps_t.r()],
             xr(yT, 4, range(4)))
        P.barrier()


def gla_phase(K, dr, C, l, yT, dbg_out):
    P = K.P
    identb, maskU = C["identb"], C["maskU"]
    hc, hs = C["hc"], C["hs"]
    G0 = OFF["gq"]
    with contextlib.ExitStack() as ph:
        win = K.sb(ph, "win_gla", [128, KT, 784], BF16)
        K.dma(win.ap(), dr["w_in"][l, :, G0:G0 + 784].rearrange("(k p) n -> p k n", p=128), w=[win.r()], q="pool")
        wgk2 = K.sb(ph, "wgk2", [16, 128], BF16)
        K.dma(wgk2.ap(), dr["gla_w_gk2"][l], w=[wgk2.r()], q="pool")
        bgkB = K.sb(ph, "bgkB", [128, 128], F32)
        K.dma(bgkB.ap(), dr["gla_b_gk"][l:l + 1, :].to_broadcast([128, 128]), w=[bgkB.r()])
        gcol = K.sb(ph, "gcol", [128, 1], F32)
        for t in range(2):
            K.dma(gcol.ap()[64 * t:64 * t + 64, :], dr["gla_norm_g"][l].rearrange("(e o) -> e o", o=1), w=[gcol.r()])
        BM = K.sb(ph, "BM", [128, 256], F32)
        hm = K.sb(ph, "hm", [128, 4], F32)
        K.memset(BM.ap(), 1.0, [BM.r()], eng="pool")
        K.memset(hm.ap(), 1.0, [hm.r()], eng="pool")
        for hh in range(4):
            for (t, sl, n) in ((BM, slice(64 * hh, 64 * hh + 64), 64), (hm, slice(hh, hh + 1), 1)):
                ap = t.ap()[:, sl]
                K.P.op("pool", lambda e, ap=ap, n=n, hh=hh: e.affine_select(
                    out=ap, in_=ap, pattern=[[0, n]], compare_op=ALU.is_ge, fill=0.0, base=-32 * hh,
                    channel_multiplier=1), reads=[t.r()], writes=[t.r()])
                K.P.op("pool", lambda e, ap=ap, n=n, hh=hh: e.affine_select(
                    out=ap, in_=ap, pattern=[[0, n]], compare_op=ALU.is_gt, fill=0.0, base=32 * hh + 32,
                    channel_multiplier=-1), reads=[t.r()], writes=[t.r()])
        gla_prompt(K, dr, C, l, yT, win, wgk2, bgkB, gcol, BM, hm)
        P.barrier()
        gla_sample(K, dr, C, l, yT, win, wgk2, bgkB)


def gla_prompt(K, dr, C, l, yT, win, wgk2, bgkB, gcol, BM, hm):
    P = K.P
    identb, maskU = C["identb"], C["maskU"]
    hc = C["hc"]
    with contextlib.ExitStack() as ph:
        glo = K.sb(ph, "glo", [16, 128], BF16)
        lg = K.sb(ph, "lg", [128, 128], F32)
        lgt = K.sb(ph, "lgt", [128, 128], F32)
        Eq = K.sb(ph, "Eq", [128, 128], F32)
        Ek = K.sb(ph, "Ek", [128, 128], F32)
        Ekt = K.sb(ph, "Ekt", [128, 128], F32)
        qt = K.sb(ph, "qt", [128, 128], BF16)
        kf = K.sb(ph, "kf", [128, 128], F32)
        km = K.sb(ph, "km", [128, 4, 128], BF16)
        ktm = K.sb(ph, "ktm", [128, 128], BF16)
        vtm = K.sb(ph, "vtm", [128, 256], BF16)
        sgg = K.sb(ph, "sgg", [128, 256], F32)
        A = K.sb(ph, "A", [128, 4, 128], BF16)
        osq = K.sb(ph, "osq", [128, 256], F32)
        ms = K.sb(ph, "ms", [128, 4], F32)
        on = K.sb(ph, "on", [128, 256], F32)
        onb = K.sb(ph, "onb", [128, 256], BF16)
        tmpS = K.sb(ph, "tmpS", [128, 256], F32)
        S32 = K.sb(ph, "S32", [128, 256], F32)
        Sb = K.sb(ph, "Sb", [128, 256], BF16)
        ps_f = K.ps(ph, "psg_f", [128, 512], F32)
        ps_m = K.ps(ph, "psg_m", [128, 512], F32)
        ps_g = K.ps(ph, "psg_g", [128, 512], F32)
        ps_l = K.ps(ph, "psg_l", [128, 512], F32)
        ps_a = K.ps(ph, "psg_a", [128, 512], F32)
        ps_o = K.ps(ph, "psg_o", [128, 512], F32)
        ps_t = K.ps(ph, "psg_t", [128, 1024], BF16)
        K.memset(S32.ap(), 0.0, [S32.r()])
        K.memset(Sb.ap(), 0.0, [Sb.r()])
        for c in range(NCH):
            h = hc[c % 2]
            make_hc(K, C, l, h, c)
            for (dst, cols, M) in ((ps_f.ap()[:, 0:128], slice(0, 128), 128), (ps_f.ap()[:, 128:256], slice(128, 256), 128),
                                   (ps_f.ap()[0:16, 256:384], slice(512, 528), 16)):
                for k in range(KT):
                    K.mm(dst, win.ap()[:, k, cols], h.ap()[:, k, :], [win.r(), h.r()], [ps_f.r()],
                         start=(k == 0), stop=(k == KT - 1))
            for (dst, cols, pst) in ((ps_m.ap()[:, 0:256], slice(256, 512), ps_m), (ps_m.ap()[:, 256:384], slice(128, 256), ps_m),
                                     (ps_g.ap()[:, 0:256], slice(528, 784), ps_g)):
                for k in range(KT):
                    K.mm(dst, h.ap()[:, k, :], win.ap()[:, k, cols], [win.r(), h.r()], [pst.r()],
                         start=(k == 0), stop=(k == KT - 1))
            K.cp(glo.ap(), ps_f.ap()[0:16, 256:384], [ps_f.r()], [glo.r()], eng="act")
            K.mm(ps_l.ap()[:, 0:128], glo.ap(), wgk2.ap(), [glo.r(), wgk2.r()], [ps_l.r()])
            K.stt(lg.ap(), ps_l.ap()[:, 0:128], -1.0, bgkB.ap(), ALU.mult, ALU.subtract, [ps_l.r(), bgkB.r()], [lg.r()])
            softplus_(K, (lg.ap(), lg.r()), (lgt.ap(), lgt.r()), None, None)
            K.ts(lg.ap(), lg.ap(), -1.0 / 16.0, ALU.mult, [lg.r()], [lg.r()])
            K.mm(ps_l.ap()[:, 128:256], lg.ap(), maskU.ap(), [lg.r(), maskU.r()], [ps_l.r()])
            K.mm(ps_l.ap()[:, 256:384], maskU.ap(), lg.ap(), [lg.r(), maskU.r()], [ps_l.r()])
            K.act(Eq.ap(), ps_l.ap()[:, 128:256], AF.Exp, [ps_l.r()], [Eq.r()])
            K.act(Ek.ap(), ps_l.ap()[:, 128:256], AF.Exp, [ps_l.r()], [Ek.r()], scale=-1.0)
            K.act(Ekt.ap(), ps_l.ap()[:, 256:384], AF.Exp, [ps_l.r()], [Ekt.r()], scale=-1.0)
            K.stt(qt.ap(), ps_f.ap()[:, 0:128], 32.0 ** -0.5, Eq.ap(), ALU.mult, ALU.mult, [ps_f.r(), Eq.r()], [qt.r()])
            K.tt(kf.ap(), ps_f.ap()[:, 128:256], Ek.ap(), ALU.mult, [ps_f.r(), Ek.r()], [kf.r()])
            K.tt(km.ap(), bc(kf.ap(), 1, [128, 4, 128]), bc(hm.ap(), 2, [128, 4, 128]), ALU.mult, [kf.r(), hm.r()],
                 [km.r()])
            K.tt(ktm.ap(), ps_m.ap()[:, 256:384], Ekt.ap(), ALU.mult, [ps_m.r(), Ekt.r()], [ktm.r()])
            K.cp(vtm.ap(), ps_m.ap()[:, 0:256], [ps_m.r()], [vtm.r()], eng="act")
            K.act(sgg.ap(), ps_g.ap()[:, 0:256], AF.Silu, [ps_g.r()], [sgg.r()])
            for hh in range(4):
                K.mm(ps_a.ap()[:, hh * 128:(hh + 1) * 128], km.ap()[:, hh, :], qt.ap(), [km.r(), qt.r()], [ps_a.r()],
                     inc=(hh == 3))
            K.tt(A.ap(), ps_a.ap().rearrange("p (h i) -> p h i", h=4), bc(maskU.ap(), 1, [128, 4, 128]), ALU.mult,
                 [ps_a.r(), maskU.r()], [A.r()])
            K.mm(ps_o.ap()[:, 0:256], qt.ap(), Sb.ap(), [qt.r(), Sb.r()], [ps_o.r()], start=True, stop=False)
            for hh in range(4):
                K.mm(ps_o.ap()[:, hh * 64:(hh + 1) * 64], A.ap()[:, hh, :], vtm.ap()[:, hh * 64:(hh + 1) * 64],
                     [A.r(), vtm.r()], [ps_o.r()], start=False, stop=(hh == 3))
            K.act(osq.ap(), ps_o.ap()[:, 0:256], AF.Square, [ps_o.r()], [osq.r()])
            K.red(ms.ap(), osq.ap().rearrange("p (h e) -> p h e", h=4), [osq.r()], [ms.r()])
            K.act(ms.ap(), ms.ap(), AF.Sqrt, [ms.r()], [ms.r()], scale=1.0 / 64, bias=RMS_EPS)
            K.recip(ms.ap(), ms.ap(), [ms.r()], [ms.r()])
            K.tt(on.ap().rearrange("p (h e) -> p h e", h=4), ps_o.ap()[:, 0:256].rearrange("p (h e) -> p h e", h=4),
                 bc(ms.ap(), 2, [128, 4, 64]), ALU.mult, [ps_o.r(), ms.r()], [on.r()])
            K.tt(onb.ap(), on.ap(), sgg.ap(), ALU.mult, [on.r(), sgg.r()], [onb.r()])
            for q in range(2):
                K.tr(ps_t.ap()[:, q * 128:(q + 1) * 128], onb.ap()[:, q * 128:(q + 1) * 128], identb.ap(),
                     [onb.r(), identb.r()], [ps_t.r()], inc=(q == 1))
            K.ts(yT.ap()[:, 6:8, c * 128:(c + 1) * 128], ps_t.ap()[:, 0:256].rearrange("p (t n) -> p t n", t=2),
                 gcol.ap(), ALU.mult, [ps_t.r(), gcol.r()], xr(yT, c // 4, range(6, 8)))
            K.mm(ps_o.ap()[:, 256:512], ktm.ap(), vtm.ap(), [ktm.r(), vtm.r()], [ps_o.r()])
            K.tt(tmpS.ap(), ps_o.ap()[:, 256:512], BM.ap(), ALU.mult, [ps_o.r(), BM.r()], [tmpS.r()])
            K.tt(S32.ap(), S32.ap(), tmpS.ap(), ALU.add, [S32.r(), tmpS.r()], [S32.r()])
            K.ts(S32.ap(), S32.ap(), Eq.ap()[:, 127:128], ALU.mult, [S32.r(), Eq.r()], [S32.r()])
            K.cp(Sb.ap(), S32.ap(), [S32.r()], [Sb.r()], eng="act")
        for hh in range(4):
            K.dma(dr["p_gla"][l, hh], S32.ap()[32 * hh:32 * hh + 32, 64 * hh:64 * hh + 64], r=[S32.r()])
        P.barrier()


def gla_sample(K, dr, C, l, yT, win, wgk2, bgkB):
    P = K.P
    hs, identb = C["hs"], C["identb"]
    with contextlib.ExitStack() as ph:
        glo = K.sb(ph, "glos", [16, NS], BF16)
        lg = K.sb(ph, "lgs", [NS, 128], F32)
        lgt = K.sb(ph, "lgts", [NS, 128], F32)
        pk = K.sb(ph, "pkg", [NS, 4, 160], F32)
        sgg = K.sb(ph, "sggs", [NS, 256], F32)
        gB = K.sb(ph, "gBg", [64, 64], F32)
        S = K.sb(ph, "Sg", [64, 32, 64], F32)
        tmp = K.sb(ph, "tmpg", [64, 32, 64], F32)
        pkh = K.sb(ph, "pkhg", [64, 160], F32)
        o = K.sb(ph, "og", [64, 64], F32)
        junk = K.sb(ph, "junkg", [64, 64], F32)
        ss = K.sb(ph, "ssg", [64, 1], F32)
        otm = K.sb(ph, "otm", [NS, 256], F32)
        otb = K.sb(ph, "otb", [NS, 256], BF16)
        ps_a = K.ps(ph, "psgs_a", [128, 512], F32)
        ps_b = K.ps(ph, "psgs_b", [128, 512], F32)
        ps_c = K.ps(ph, "psgs_c", [128, 512], F32)
        ps_t = K.ps(ph, "psgs_t", [128, 1024], BF16)
        spk = dram_scratch(K, "gpk", [NS, 640])
        so = dram_scratch(K, "go", [NS, 256])
        K.dma(gB.ap(), dr["gla_norm_g"][l:l + 1, :].to_broadcast([64, 64]), w=[gB.r()])
        K.dma(S.ap().rearrange("p d e -> p (d e)"), dr["st_gla"][l].rearrange("b h d e -> (b h) (d e)"), w=[S.r()])
        for k in range(KT):
            K.mm(ps_a.ap()[0:NS, :], hs.ap()[:, k, :], win.ap()[:, k, 0:512], [hs.r(), win.r()], [ps_a.r()],
                 start=(k == 0), stop=(k == KT - 1))
        for k in range(KT):
            K.mm(ps_b.ap()[0:NS, 0:256], hs.ap()[:, k, :], win.ap()[:, k, 528:784], [hs.r(), win.r()], [ps_b.r()],
                 start=(k == 0), stop=(k == KT - 1))
        for k in range(KT):
            K.mm(ps_c.ap()[0:16, 0:NS], win.ap()[:, k, 512:528], hs.ap()[:, k, :], [hs.r(), win.r()], [ps_c.r()],
                 start=(k == 0), stop=(k == KT - 1))
        K.cp(glo.ap(), ps_c.ap()[0:16, 0:NS], [ps_c.r()], [glo.r()], eng="act")
        K.mm(ps_c.ap()[0:NS, 128:256], glo.ap(), wgk2.ap(), [glo.r(), wgk2.r()], [ps_c.r()])
        K.stt(lg.ap(), ps_c.ap()[0:NS, 128:256], -1.0, bgkB.ap()[0:NS, :], ALU.mult, ALU.subtract,
              [ps_c.r(), bgkB.r()], [lg.r()])
        softplus_(K, (lg.ap(), lg.r()), (lgt.ap(), lgt.r()), None, None)
        K.act(lg.ap(), lg.ap(), AF.Exp, [lg.r()], [lg.r()], scale=-1.0 / 16.0)
        K.act(sgg.ap(), ps_b.ap()[0:NS, 0:256], AF.Silu, [ps_b.r()], [sgg.r()])
        K.ts(pk.ap()[:, :, 0:32], ps_a.ap()[0:NS, 0:128].rearrange("p (h d) -> p h d", h=4), 32.0 ** -0.5, ALU.mult,
             [ps_a.r()], [pk.r()])
        K.cp(pk.ap()[:, :, 32:64], ps_a.ap()[0:NS, 128:256].rearrange("p (h d) -> p h d", h=4), [ps_a.r()], [pk.r()])
        K.cp(pk.ap()[:, :, 64:96], lg.ap().rearrange("p (h d) -> p h d", h=4), [lg.r()], [pk.r()])
        K.cp(pk.ap()[:, :, 96:160], ps_a.ap()[0:NS, 256:512].rearrange("p (h e) -> p h e", h=4), [ps_a.r()], [pk.r()])
        K.dma(spk.ap(), pk.ap().rearrange("p h x -> p (h x)"), r=[pk.r()], w=[spk.r()])
        K.dma(pkh.ap(), spk.ap().rearrange("b (h x) -> (b h) x", h=4), r=[spk.r()], w=[pkh.r()])
        qh, kh, eh, vh = pkh.ap()[:, 0:32], pkh.ap()[:, 32:64], pkh.ap()[:, 64:96], pkh.ap()[:, 96:160]
        K.tt(S.ap(), S.ap(), bc(eh, 2, [64, 32, 64]), ALU.mult, [S.r(), pkh.r()], [S.r()])
        K.tt(tmp.ap(), bc(kh, 2, [64, 32, 64]), bc(vh, 1, [64, 32, 64]), ALU.mult, [pkh.r()], [tmp.r()])
        K.tt(S.ap(), S.ap(), tmp.ap(), ALU.add, [S.r(), tmp.r()], [S.r()])
        K.dma(dr["s_gla"][l].rearrange("b h d e -> (b h) (d e)"), S.ap().rearrange("p d e -> p (d e)"), r=[S.r()])
        K.tt(tmp.ap(), S.ap(), bc(qh, 2, [64, 32, 64]), ALU.mult, [S.r(), pkh.r()], [tmp.r()])
        K.red(o.ap(), tmp.ap().rearrange("p d e -> p e d"), [tmp.r()], [o.r()])
        K.act(junk.ap(), o.ap(), AF.Square, [o.r()], [junk.r(), ss.r()], accum_out=ss.ap())
        K.act(ss.ap(), ss.ap(), AF.Sqrt, [ss.r()], [ss.r()], scale=1.0 / 64, bias=RMS_EPS)
        K.recip(ss.ap(), ss.ap(), [ss.r()], [ss.r()])
        K.stt(o.ap(), o.ap(), ss.ap(), gB.ap(), ALU.mult, ALU.mult, [o.r(), ss.r(), gB.r()], [o.r()])
        K.dma(so.ap().rearrange("b (h e) -> (b h) e", h=4), o.ap(), r=[o.r()], w=[so.r()])
        K.dma(otm.ap(), so.ap(), r=[so.r()], w=[otm.r()])
        K.tt(otb.ap(), otm.ap(), sgg.ap(), ALU.mult, [otm.r(), sgg.r()], [otb.r()])
        for q in range(2):
            K.tr(ps_t.ap()[:, q * NS:(q + 1) * NS], otb.ap()[:, q * 128:(q + 1) * 128], identb.ap()[0:NS, 0:NS],
                 [otb.r(), identb.r()], [ps_t.r()], inc=(q == 1))
        K.cp(yT.ap()[:, 6:8, T:T + NS], ps_t.ap()[:, 0:2 * NS].rearrange("p (t n) -> p t n", t=2), [ps_t.r()],
             xr(yT, 4, range(6, 8)))
        P.barrier()


C0 = float(np.exp(-0.5))


def rwkv_prep(K, C, pc, LW, N, rw, prev, B, pl, pg, pn, ee="dve"):
    blk64 = C["blk64"]
    MX, LI = B["MX"], B["LI"]
    mxa = MX.ap()[:, :, 0:N]
    K.tt(mxa, prev, rw, ALU.subtract, B["_rw_res"], [MX.r()], eng=ee)
    yield
    K.tt(mxa, mxa, bc(pc["mu"].ap(), 2, [128, 7, N]), ALU.mult, [MX.r(), pc["mu"].r()], [MX.r()], eng=ee)
    yield
    K.tt(mxa, mxa, rw, ALU.add, [MX.r()] + B["_rw_res"], [MX.r()], eng=ee)
    yield
    r, k, v = (MX.ap()[:, 0:2, 0:N], MX.ap()[:, 2:4, 0:N], MX.ap()[:, 4:6, 0:N])
    lia = LI.ap()[:, 0:N]
    lif = B["t1"].ap()[:, 0, 0:N]
    K.act(lif, MX.ap()[:, 6, 0:N], AF.Exp, [MX.r(), pc["lisc"].r()], [B["t1"].r()], scale=pc["lisc"].ap())
    yield
    sigmoid_chain(K, lif, [B["t1"].r()])
    yield
    K.ts(lia[0:32], lif[0:32], 2.0, ALU.mult, [B["t1"].r()], [LI.r()], s2=-1.0, op1=ALU.add)
    K.cp(lia[32:64], MX.ap()[32:64, 6, 0:N], [MX.r()], [LI.r()], eng="act")
    K.cp(lia[64:128], lif[64:128], [B["t1"].r()], [LI.r()])
    yield
    for t in range(2):
        cs = slice(t * 128, (t + 1) * 128)
        K.mm(pl.ap()[:, t * N:(t + 1) * N], LW.ap()[0:32, cs], lia[0:32], [LW.r(), LI.r()], [pl.r()], self_wait=True)
        K.mm(pl.ap()[:, (2 + t) * N:(3 + t) * N], LW.ap()[32:64, cs], lia[32:64], [LW.r(), LI.r()], [pl.r()],
             self_wait=True)
        K.mm(pg.ap()[:, t * N:(t + 1) * N], LW.ap()[64:128, cs], lia[64:128], [LW.r(), LI.r()], [pg.r()],
             self_wait=True)
    g = lambda n: B[n].ap()[:, :, 0:N]
    for t in range(2):
        K.act(B["sig"].ap()[:, t, 0:N], pl.ap()[:, t * N:(t + 1) * N], AF.Exp, [pl.r(), pc["nw0"].r()],
              [B["sig"].r()], scale=-1.0, bias=pc["nw0"].ap()[:, t:t + 1])
        K.act(B["aic"].ap()[:, t, 0:N], pl.ap()[:, (2 + t) * N:(3 + t) * N], AF.Exp, [pl.r(), pc["na0"].r()],
              [B["aic"].r()], scale=-1.0, bias=pc["na0"].ap()[:, t:t + 1])
    sigmoid_chain(K, g("sig"), [B["sig"].r()])
    sigmoid_chain(K, g("aic"), [B["aic"].r()])
    for t in range(0):
        pass
    K.cp(g("gate"), pg.ap()[:, 0:2 * N].rearrange("p (t n) -> p t n", t=2), [pg.r()], [B["gate"].r()], eng="act")
    yield
    K.tt(g("kk"), k, bc(pc["k_k"].ap(), 2, [128, 2, N]), ALU.mult, [MX.r(), pc["k_k"].r()], [B["kk"].r()], eng=ee)
    yield
    K.tt(g("t1"), g("kk"), g("kk"), ALU.mult, [B["kk"].r()], [B["t1"].r()], eng=ee)
    yield
    for t in range(2):
        K.mm(pn.ap()[:, t * N:(t + 1) * N], blk64.ap(), B["t1"].ap()[:, t, 0:N], [blk64.r(), B["t1"].r()], [pn.r()])
    K.act(g("t1"), pn.ap()[:, 0:2 * N].rearrange("p (t n) -> p t n", t=2), AF.Ln, [pn.r()], [B["t1"].r()],
          bias=1e-12)
    K.act(g("t1"), g("t1"), AF.Exp, [B["t1"].r()], [B["t1"].r()], scale=-0.5)
    yield
    K.tt(g("kk"), g("kk"), g("t1"), ALU.mult, [B["kk"].r(), B["t1"].r()], [B["kk"].r()], eng=ee)
    yield
    yield
    K.tt(g("t1"), g("aic"), bc(pc["k_a"].ap(), 2, [128, 2, N]), ALU.mult, [B["aic"].r(), pc["k_a"].r()], [B["t1"].r()], eng=ee)
    yield
    K.tt(g("t1"), g("t1"), bc(pc["omka"].ap(), 2, [128, 2, N]), ALU.add, [B["t1"].r(), pc["omka"].r()], [B["t1"].r()], eng=ee)
    yield
    K.tt(g("kp"), k, g("t1"), ALU.mult, [MX.r(), B["t1"].r()], [B["kp"].r()], eng=ee)
    yield
    K.tt(g("t1"), r, g("kp"), ALU.mult, [MX.r(), B["kp"].r()], [B["t1"].r()], eng=ee)
    yield
    K.tt(g("t1"), g("t1"), bc(pc["r_k"].ap(), 2, [128, 2, N]), ALU.mult, [B["t1"].r(), pc["r_k"].r()], [B["t1"].r()], eng=ee)
    yield
    for t in range(2):
        K.mm(pn.ap()[:, t * N:(t + 1) * N], blk64.ap(), B["t1"].ap()[:, t, 0:N], [blk64.r(), B["t1"].r()], [pn.r()])
    K.tt(g("bonus"), pn.ap()[:, 0:2 * N].rearrange("p (t n) -> p t n", t=2), v, ALU.mult, [pn.r(), MX.r()],
         [B["bonus"].r()])
    B['_rkv'] = (r, k, v)
    yield


def rwkv_params(K, dr, l, ph):
    pc = {}
    mu = K.sb(ph, "mu", [128, 7], F32)
    K.dma(mu.ap(), dr["rwkv_mu"][l].rearrange("(t p) -> p t", p=128), w=[mu.r()])
    pc["mu"] = mu
    for n, src in (("w0", dr["rwkv_w0"][l]), ("a0", dr["rwkv_a0"][l]), ("k_k", dr["rwkv_k_k"][l]),
                   ("k_a", dr["rwkv_k_a"][l]), ("r_k", dr["rwkv_r_k"][l].rearrange("h n -> (h n)")),
                   ("ln_g", dr["rwkv_ln_g"][l]), ("ln_b", dr["rwkv_ln_b"][l])):
        t = K.sb(ph, "pc_" + n, [128, 2], F32)
        K.dma(t.ap(), src.rearrange("(t p) -> p t", p=128), w=[t.r()])
        pc[n] = t
    for n in ("w0", "a0"):
        t = K.sb(ph, "pc_n" + n, [128, 2], F32)
        K.ts(t.ap(), pc[n].ap(), -1.0, ALU.mult, [pc[n].r()], [t.r()])
        pc["n" + n] = t
    lisc = K.sb(ph, "lisc", [128, 1], F32)
    K.memset(lisc.ap()[0:32], -2.0, [lisc.r()])
    K.memset(lisc.ap()[32:64], 0.0, [lisc.r()])
    K.memset(lisc.ap()[64:128], -1.0, [lisc.r()])
    pc["lisc"] = lisc
    omka = K.sb(ph, "omka", [128, 2], F32)
    K.ts(omka.ap(), pc["k_a"].ap(), -1.0, ALU.mult, [pc["k_a"].r()], [omka.r()], s2=1.0, op1=ALU.add)
    pc["omka"] = omka
    LW = K.sb(ph, "LW", [128, 256], BF16)
    K.dma(LW.ap()[0:32, :], dr["rwkv_w2"][l], w=[LW.r()], q="pool")
    K.dma(LW.ap()[32:64, :], dr["rwkv_a2"][l], w=[LW.r()], q="pool")
    K.dma(LW.ap()[64:128, :], dr["rwkv_g2"][l], w=[LW.r()], q="pool")
    return pc, LW


def rwkv_epilogue(K, C, pc, B, N, pT, ydst, yres):
    for t in range(2):
        K.act(B["t1"].ap()[:, t, 0:N], pT.ap()[:, t * N:(t + 1) * N], AF.Identity, [pT.r(), pc["ln_g"].r(), pc["ln_b"].r()],
              [B["t1"].r()], scale=pc["ln_g"].ap()[:, t:t + 1], bias=pc["ln_b"].ap()[:, t:t + 1])
    g = lambda n: B[n].ap()[:, :, 0:N]
    K.tt(g("t1"), g("t1"), g("bonus"), ALU.add, [B["t1"].r(), B["bonus"].r()], [B["t1"].r()])
    K.tt(ydst, g("t1"), g("gate"), ALU.mult, [B["t1"].r(), B["gate"].r()], yres)


def groupnorm64(K, o_ap, n, G, scr, res_in, out_ap, out_res):
    mean, xc, sq, var = scr
    K.red(mean.ap()[0:n, 0:G], o_ap, res_in, [mean.r()])
    K.ts(mean.ap()[0:n, 0:G], mean.ap()[0:n, 0:G], 1.0 / 64, ALU.mult, [mean.r()], [mean.r()])
    xca = xc.ap()[0:n, 0:G * 64].rearrange("p (g e) -> p g e", g=G)
    K.tt(xca, o_ap, bc(mean.ap()[0:n, 0:G], 2, [n, G, 64]), ALU.subtract, res_in + [mean.r()], [xc.r()])
    sqa = sq.ap()[0:n, 0:G * 64].rearrange("p (g e) -> p g e", g=G)
    K.tt(sqa, xca, xca, ALU.mult, [xc.r()], [sq.r()])
    K.red(var.ap()[0:n, 0:G], sqa, [sq.r()], [var.r()])
    rsqrt_(K, var.ap()[0:n, 0:G], [var.r()], 1.0 / 64, RWKV_GN_EPS)
    K.tt(out_ap, xca, bc(var.ap()[0:n, 0:G], 2, [n, G, 64]), ALU.mult, [xc.r(), var.r()], out_res)


def rwkv_phase(K, dr, C, l, yT, dbg_out):
    P = K.P
    R0 = OFF["rw"]
    with contextlib.ExitStack() as ph:
        win = K.sb(ph, "win_rwkv", [128, KT, 896], BF16)
        K.dma(win.ap(), dr["w_in"][l, :, R0:R0 + 896].rearrange("(k p) n -> p k n", p=128), w=[win.r()], q="pool")
        pc, LW = rwkv_params(K, dr, l, ph)
        import os
        if os.environ.get("SKIP_RWKV_PROMPT") != "1":
            rwkv_prompt(K, dr, C, l, yT, win, pc, LW, dbg_out)
        P.barrier()
        if os.environ.get("SKIP_RWKV_SAMPLE") != "1":
            rwkv_sample(K, dr, C, l, yT, win, pc, LW, dbg_out)


def interleave(gens, ratio=None):
    gens = [g for g in gens if g is not None]
    ratio = ratio or [1] * len(gens)
    live = list(zip(gens, ratio))
    while live:
        for item in list(live):
            g, n = item
            for _ in range(n):
                try:
                    next(g)
                except StopIteration:
                    live.remove(item)
                    break


def rwkv_prompt(K, dr, C, l, yT, win, pc, LW, dbg_out):
    P = K.P
    identb, identf, maskU, maskSU, maskSL, blk64 = (C[k] for k in ["identb", "identf", "maskU", "maskSU", "maskSL", "blk64"])
    hc = C["hc"]
    N = 128
    with contextlib.ExitStack() as ph:
        f3 = lambda n: K.sb(ph, n, [128, 2, N], F32)
        Bs = []
        for i in range(2):
            B = {n: f3(f"rb{i}_" + n) for n in ["sig", "aic", "gate", "kk", "t1", "kp", "bonus", "cs", "e1", "e2", "bb"]}
            Bs.append(B)
        MX = K.sb(ph, "MX", [128, 7, N], F32)
        LI = K.sb(ph, "LI", [128, N], BF16)
        for B in Bs:
            B["MX"], B["LI"] = MX, LI
        RW = [K.sb(ph, f"RW{i}", [128, 7, N + 1], F32) for i in range(2)]
        ones_r = K.sb(ph, "ones_r", [128, N], F32)
        bcol = K.sb(ph, "bcol", [128, 2], F32)
        MK2 = K.sb(ph, "MK2", [128, 2, N], F32)
        ARs = [K.sb(ph, f"AR{i}", [128, 2, 2, N], BF16) for i in range(2)]
        BKs = [K.sb(ph, f"BK{i}", [128, 2, 2, N], BF16) for i in range(2)]
        FH = K.sb(ph, "FH", [128, 3, 2, N], BF16)
        TMs = [K.sb(ph, f"TM{i}", [128, 4, 2, N], BF16) for i in range(2)]
        t2 = f3("rb_t2")
        A1 = K.sb(ph, "A1", [128, 4, 2, N], BF16)
        A2 = K.sb(ph, "A2", [128, 4, 2, N], BF16)
        Lb = [K.sb(ph, f"Lb{i}", [128, 4, N], BF16) for i in range(2)]
        Nb = [K.sb(ph, f"Nb{i}", [128, 4, N], BF16) for i in range(2)]
        X32 = K.sb(ph, "X32", [128, 4, 2, 64], F32)
        Xb = K.sb(ph, "Xb", [128, 4, 2, 64], BF16)
        Apf = K.sb(ph, "Apf", [128, 2, N], BF16)
        XAc = K.sb(ph, "XAc", [128, 256], BF16)
        Utm = K.sb(ph, "Utm", [128, 4, 64], BF16)
        ST32 = K.sb(ph, "ST32", [128, 2, N], F32)
        STb = K.sb(ph, "STb", [128, 2, N], BF16)
        tmpS = K.sb(ph, "tmpSr", [128, 2, N], F32)
        gn = (K.sb(ph, "gn_mean", [128, 4], F32), K.sb(ph, "gn_xc", [128, 256], F32),
              K.sb(ph, "gn_sq", [128, 256], F32), K.sb(ph, "gn_var", [128, 4], F32))
        onb = K.sb(ph, "onbr", [128, 256], BF16)
        stT = K.sb(ph, "stT", [128, 2, N], F32)
        pI = K.ps(ph, "pr_I", [128, 512], F32)
        pM = K.ps(ph, "pr_M", [128, 512], F32)
        pT = K.ps(ph, "pr_T", [128, 1024], BF16)
        pT2 = pT
        pAT = K.ps(ph, "pr_AT", [128, 1024], F32)
        pL = K.ps(ph, "pr_L", [128, 512], F32)
        pL2 = K.ps(ph, "pr_L2", [128, 512], F32)
        pX = K.ps(ph, "pr_X", [128, 512], F32)
        pO = pX

        K.memset(ones_r.ap(), 1.0, [ones_r.r()])
        K.cp(MK2.ap()[:, 0, :], maskSU.ap(), [maskSU.r()], [MK2.r()])
        K.cp(MK2.ap()[:, 1, :], maskU.ap(), [maskU.r()], [MK2.r()])
        K.memset(ST32.ap(), 0.0, [ST32.r()])
        K.memset(STb.ap(), 0.0, [STb.r()])
        K.memset(RW[0].ap()[:, :, 0:1], 0.0, [RW[0].r()])

        import os
        PE1 = os.environ.get("RWKV_S1_ENG", "dve")

        def s1(c):
            B, AR, BK, TM = Bs[c % 2], ARs[c % 2], BKs[c % 2], TMs[c % 2]
            g = lambda n: B[n].ap()
            h = hc[c % 2]
            make_hc(K, C, l, h, c)
            yield
            rw = RW[c % 2]
            for (t0, t1) in ((0, 4), (4, 7)):
                for t in range(t0, t1):
                    for k in range(KT):
                        K.mm(pI.ap()[:, (t - t0) * N:(t - t0 + 1) * N], win.ap()[:, k, t * N:(t + 1) * N], h.ap()[:, k, :],
                             [win.r(), h.r()], [pI.r()], start=(k == 0), stop=(k == KT - 1))
                    yield
                K.cp(rw.ap()[:, t0:t1, 1:N + 1], pI.ap()[:, 0:(t1 - t0) * N].rearrange("p (t n) -> p t n", t=t1 - t0),
                     [pI.r()], [rw.r()], eng="act")
                yield
            if c + 1 < NCH:
                K.cp(RW[(c + 1) % 2].ap()[:, :, 0:1], rw.ap()[:, :, N:N + 1], [rw.r()], [RW[(c + 1) % 2].r()])
            B["_rw_res"] = [rw.r()]
            yield from rwkv_prep(K, C, pc, LW, N, rw.ap()[:, :, 1:N + 1], rw.ap()[:, :, 0:N], B, pM, pI, pI, ee=PE1)
            r, k_, v = B["_rkv"]
            MXr = MX.r()
            for t in range(2):
                K.P.op("dve", lambda e, t=t, B=B: e.tensor_tensor_scan(out=B["cs"].ap()[:, t, :], data0=ones_r.ap(),
                                                                        data1=B["sig"].ap()[:, t, :], initial=0.0,
                                                                        op0=ALU.mult, op1=ALU.add),
                       reads=[ones_r.r(), B["sig"].r()], writes=[B["cs"].r()])
            yield
            K.act(g("e1"), g("cs"), AF.Exp, [B["cs"].r()], [B["e1"].r()], scale=-C0)
            yield
            K.act(g("e2"), g("cs"), AF.Exp, [B["cs"].r()], [B["e2"].r()], scale=C0)
            yield
            K.tt(AR.ap()[:, :, 1, :], r, g("e1"), ALU.mult, [MXr, B["e1"].r()], [AR.r()], eng=PE1)
            yield
            K.tt(g("bb"), g("kk"), g("aic"), ALU.mult, [B["kk"].r(), B["aic"].r()], [B["bb"].r()], eng=PE1)
            yield
            K.tt(BK.ap()[:, :, 0, :], g("bb"), g("e2"), ALU.mult, [B["bb"].r(), B["e2"].r()], [BK.r()], eng=PE1)
            yield
            K.tt(BK.ap()[:, :, 1, :], g("kp"), g("e2"), ALU.mult, [B["kp"].r(), B["e2"].r()], [BK.r()], eng=PE1)
            yield
            K.tt(g("t1"), g("cs"), g("sig"), ALU.subtract, [B["cs"].r(), B["sig"].r()], [B["t1"].r()], eng=PE1)
            yield
            K.act(g("e2"), g("t1"), AF.Exp, [B["t1"].r()], [B["e2"].r()], scale=-C0)
            yield
            K.stt(AR.ap()[:, :, 0, :], g("kk"), -1.0, g("e2"), ALU.mult, ALU.mult, [B["kk"].r(), B["e2"].r()], [AR.r()])
            yield
            K.ts(bcol.ap(), B["cs"].ap()[:, :, N - 1], -C0, ALU.mult, [B["cs"].r()], [bcol.r()])
            yield
            for t in range(2):
                K.act(B["e2"].ap()[:, t, :], B["cs"].ap()[:, t, :], AF.Exp, [B["cs"].r(), bcol.r()], [B["e2"].r()],
                      scale=C0, bias=bcol.ap()[:, t:t + 1])
            yield
            K.tt(FH.ap()[:, 0], g("bb"), g("e2"), ALU.mult, [B["bb"].r(), B["e2"].r()], [FH.r()], eng=PE1)
            yield
            K.tt(FH.ap()[:, 1], g("kp"), g("e2"), ALU.mult, [B["kp"].r(), B["e2"].r()], [FH.r()], eng=PE1)
            yield
            K.cp(FH.ap()[:, 2], v, [MXr], [FH.r()], eng="act")
            yield
            for q in range(4):
                for t in range(2):
                    src = FH.ap()[:, q, t, :] if q < 3 else AR.ap()[:, t, 0, :]
                    K.tr(pT.ap()[:, (q * 2 + t) * N:(q * 2 + t + 1) * N], src, identb.ap(),
                         [FH.r(), AR.r(), identb.r()], [pT.r()], inc=(t == 1))
                yield
            K.cp(TM.ap().rearrange("p q t n -> p (q t n)"), pT.ap(), [pT.r()], [TM.r()], eng="act")
            yield

        def s2(c):
            B, AR, BK, TM = Bs[c % 2], ARs[c % 2], BKs[c % 2], TMs[c % 2]
            mk = bc(MK2.ap(), 1, [128, 4, 2, N])
            for which, Adst in ((0, A1), (1, A2)):
                for hd in range(4):
                    t, o = hd // 2, 64 * (hd % 2)
                    sl = slice(o, o + 64)
                    arf = AR.ap()[sl, t].rearrange("p a n -> p (a n)")
                    K.mm(pAT.ap()[:, hd * 256:(hd + 1) * 256], BK.ap()[sl, t, which, :], arf, [BK.r(), AR.r()], [pAT.r()],
                         self_wait=True)
                yield
                K.tt(Adst.ap(), pAT.ap().rearrange("p (h a n) -> p h a n", h=4, a=2), mk, ALU.mult, [pAT.r(), MK2.r()],
                     [Adst.r()])
                yield
            for hd in range(4):
                t, o = hd // 2, 64 * (hd % 2)
                sl = slice(o, o + 64)
                K.mm(pL.ap()[:, hd * N:(hd + 1) * N], AR.ap()[sl, t, 0, :], BK.ap()[sl, t, 0, :], [BK.r(), AR.r()],
                     [pL.r()], self_wait=True)
            yield
            K.tt(Lb[0].ap(), pL.ap().rearrange("p (h n) -> p h n", h=4), bc(maskSL.ap(), 1, [128, 4, N]), ALU.mult,
                 [pL.r(), maskSL.r()], [Lb[0].r()])
            yield
            vtm = TM.ap()[:, 2].rearrange("p t n -> p (t n)")
            for hd in range(4):
                K.mm(pX.ap()[:, hd * 64:(hd + 1) * 64], A2.ap()[:, hd, 0, :], vtm[:, hd * 64:(hd + 1) * 64],
                     [A2.r(), TM.r()], [pX.r()], inc=(hd == 3))
            yield
            K.cp(X32.ap()[:, :, 0, :], TM.ap()[:, 3].rearrange("p t (hh k) -> p (t hh) k", hh=2), [TM.r()], [X32.r()])
            yield
            K.cp(X32.ap()[:, :, 1, :], pX.ap()[:, 0:256].rearrange("p (h v) -> p h v", h=4), [pX.r()], [X32.r()],
                 eng="act")
            yield
            K.cp(Xb.ap(), X32.ap(), [X32.r()], [Xb.r()], eng="act")
            yield
            for i in range(7):
                if i == 0:
                    nref = lambda hd: A1.ap()[:, hd, 0, :]
                    nres = A1.r()
                    lcur = Lb[0]
                else:
                    nprev_ref, nprev_res, lprev = nref, nres, lcur
                    nnew, lnew = Nb[i % 2], Lb[i % 2]
                    for hd in range(4):
                        K.mm(pL.ap()[:, hd * N:(hd + 1) * N], lprev.ap()[:, hd, :], nprev_ref(hd), [lprev.r(), nprev_res],
                             [pL.r()], inc=(hd == 3))
                    yield
                    if i < 6:
                        for hd in range(4):
                            K.mm(pL2.ap()[:, hd * N:(hd + 1) * N], nprev_ref(hd), lprev.ap()[:, hd, :],
                                 [lprev.r(), nprev_res], [pL2.r()], inc=(hd == 3))
                        yield
                    K.cp(nnew.ap(), pL.ap().rearrange("p (h n) -> p h n", h=4), [pL.r()], [nnew.r()], eng="act")
                    yield
                    if i < 6:
                        K.cp(lnew.ap(), pL2.ap().rearrange("p (h n) -> p h n", h=4), [pL2.r()], [lnew.r()])
                        yield
                    nref = lambda hd, nnew=nnew: nnew.ap()[:, hd, :]
                    nres = nnew.r()
                    lcur = lnew
                for hd in range(4):
                    K.mm(pX.ap()[:, hd * N:(hd + 1) * N], nref(hd), Xb.ap()[:, hd].rearrange("p a k -> p (a k)"),
                         [nres, Xb.r()], [pX.r()], inc=(hd == 3))
                yield
                K.tt(X32.ap(), X32.ap(), pX.ap().rearrange("p (h a k) -> p h a k", h=4, a=2), ALU.add,
                     [X32.r(), pX.r()], [X32.r()])
                yield
                K.cp(Xb.ap(), X32.ap(), [X32.r()], [Xb.r()], eng="act")
                yield
            K.cp(XAc.ap().rearrange("p (h k) -> p h k", h=4), X32.ap()[:, :, 0, :], [X32.r()], [XAc.r()])
            yield
            for t in range(2):
                K.tr(pT2.ap()[:, t * N:(t + 1) * N], XAc.ap()[:, t * N:(t + 1) * N], identb.ap(), [XAc.r(), identb.r()],
                     [pT2.r()], inc=(t == 1))
            yield
            K.cp(Apf.ap(), pT2.ap()[:, 0:2 * N].rearrange("p (t n) -> p t n", t=2), [pT2.r()], [Apf.r()], eng="act")
            yield
            for t in range(2):
                K.mm(pX.ap()[:, t * N:(t + 1) * N], Apf.ap()[:, t, :], STb.ap()[:, t, :], [Apf.r(), STb.r()], [pX.r()],
                     inc=(t == 1))
            yield
            K.tt(Utm.ap(), pX.ap()[:, 0:256].rearrange("p (h v) -> p h v", h=4), X32.ap()[:, :, 1, :], ALU.add,
                 [pX.r(), X32.r()], [Utm.r()])
            yield
            for t in range(2):
                K.mm(pO.ap()[:, t * N:(t + 1) * N], AR.ap()[:, t, 1, :], STb.ap()[:, t, :], [AR.r(), STb.r()], [pO.r()],
                     start=(t == 0), stop=False, sgc=True)
            for hd in range(4):
                K.mm(pO.ap()[:, hd * 64:(hd + 1) * 64], A1.ap()[:, hd, 1, :], Utm.ap()[:, hd, :], [A1.r(), Utm.r()],
                     [pO.r()], start=False, stop=False, sgc=True)
                K.mm(pO.ap()[:, hd * 64:(hd + 1) * 64], A2.ap()[:, hd, 1, :], vtm[:, hd * 64:(hd + 1) * 64],
                     [A2.r(), TM.r()], [pO.r()], start=False, stop=False, sgc=True)
            for t in range(2):
                K.mm(pO.ap()[:, 256 + t * N:256 + (t + 1) * N], TM.ap()[:, 0, t, :],
                     Utm.ap()[:, 2 * t:2 * t + 2, :].rearrange("p h v -> p (h v)"), [TM.r(), Utm.r()], [pO.r()],
                     start=False, stop=False, sgc=True)
                K.mm(pO.ap()[:, 256 + t * N:256 + (t + 1) * N], TM.ap()[:, 1, t, :], vtm[:, t * N:(t + 1) * N],
                     [TM.r()], [pO.r()], start=False, stop=(t == 1), inc=(t == 1), sgc=True)
            yield
            K.tt(tmpS.ap(), pO.ap()[:, 256:512].rearrange("p (t n) -> p t n", t=2), bc(blk64.ap(), 1, [128, 2, N]),
                 ALU.mult, [pO.r(), blk64.r()], [tmpS.r()])
            yield
            K.tt(ST32.ap(), ST32.ap(), bc(B["e1"].ap()[:, :, N - 1], 2, [128, 2, N]), ALU.mult, [ST32.r(), B["e1"].r()],
                 [ST32.r()])
            yield
            K.tt(ST32.ap(), ST32.ap(), tmpS.ap(), ALU.add, [ST32.r(), tmpS.r()], [ST32.r()])
            yield
            K.cp(STb.ap(), ST32.ap(), [ST32.r()], [STb.r()], eng="act")
            yield
            groupnorm64(K, pO.ap()[:, 0:256].rearrange("p (h v) -> p h v", h=4), 128, 4, gn, [pO.r()],
                        onb.ap().rearrange("p (h v) -> p h v", h=4), [onb.r()])
            yield
            for t in range(2):
                K.tr(pT2.ap()[:, t * N:(t + 1) * N], onb.ap()[:, t * N:(t + 1) * N], identb.ap(), [onb.r(), identb.r()],
                     [pT2.r()], inc=(t == 1))
            yield
            B2 = dict(B)
            B2["t1"] = t2
            rwkv_epilogue(K, C, pc, B2, N, pT2, yT.ap()[:, 4:6, c * N:(c + 1) * N], xr(yT, c // 4, range(4, 6)))
            yield

        for _ in s1(0):
            pass
        for c in range(NCH):
            interleave([s2(c), s1(c + 1) if c + 1 < NCH else None], ratio=[3, 2])
        rw = RW[(NCH - 1) % 2]
        K.dma(dr["p_shift"][l].rearrange("(t p) -> p t", p=128), rw.ap()[:, :, N], r=[rw.r()])
        for t in range(2):
            K.tr(pL.ap()[:, t * N:(t + 1) * N], ST32.ap()[:, t, :], identf.ap(), [ST32.r(), identf.r()], [pL.r()],
                 inc=(t == 1))
        K.cp(stT.ap(), pL.ap()[:, 0:2 * N].rearrange("p (t n) -> p t n", t=2), [pL.r()], [stT.r()])
        for hd in range(4):
            t, o = hd // 2, 64 * (hd % 2)
            K.dma(dr["p_rwkv"][l, hd], stT.ap()[o:o + 64, t, o:o + 64], r=[stT.r()])
        P.barrier()


def rwkv_sample(K, dr, C, l, yT, win, pc, LW, dbg_out):
    P = K.P
    hs, identb, identf = C["hs"], C["identb"], C["identf"]
    N = NS
    with contextlib.ExitStack() as ph:
        f3 = lambda n: K.sb(ph, n, [128, 2, N], F32)
        B = {n: f3("rs_" + n) for n in ["sig", "aic", "gate", "kk", "t1", "kp", "bonus", "e1", "e2", "bb"]}
        B["MX"] = K.sb(ph, "MXs", [128, 7, N], F32)
        B["LI"] = K.sb(ph, "LIs", [128, N], BF16)
        rws = K.sb(ph, "rws", [128, 7, N], F32)
        prevs = K.sb(ph, "prevs", [128, 7, N], F32)
        shs = K.sb(ph, "shs", [NS, 896], F32)
        rwtm = K.sb(ph, "rwtm", [NS, 896], F32)
        pkT = K.sb(ph, "pkT", [NS, 6, 256], F32)
        pkh = K.sb(ph, "pkhr", [64, 6, 64], F32)
        S = K.sb(ph, "Sr", [64, 64, 64], F32)
        tmp = K.sb(ph, "tmpr", [64, 64, 64], F32)
        sa = K.sb(ph, "sa", [64, 64], F32)
        o = K.sb(ph, "orr", [64, 64], F32)
        on = K.sb(ph, "onr", [64, 64], F32)
        gn = (K.sb(ph, "gns_mean", [64, 1], F32), K.sb(ph, "gns_xc", [64, 64], F32),
              K.sb(ph, "gns_sq", [64, 64], F32), K.sb(ph, "gns_var", [64, 1], F32))
        otm = K.sb(ph, "otmr", [NS, 256], F32)
        otb = K.sb(ph, "otbr", [NS, 256], BF16)
        pA = K.ps(ph, "prs_A", [128, 1024], F32)
        pB = K.ps(ph, "prs_B", [128, 1024], F32)
        pL = K.ps(ph, "prs_L", [128, 512], F32)
        pM = K.ps(ph, "prs_M", [128, 512], F32)
        pT = K.ps(ph, "prs_T", [128, 1024], BF16)
        scr = dram_scratch(K, "rpk", [NS, 4, 6, 64])
        so = dram_scratch(K, "ro", [NS, 256])

        K.dma(shs.ap(), dr["st_shift"][l], w=[shs.r()])
        K.dma(S.ap().rearrange("p v k -> p (v k)"), dr["st_rwkv"][l].rearrange("b h v k -> (b h) (v k)"), w=[S.r()])
        for t in range(7):
            for k in range(KT):
                K.mm(pM.ap()[:, t * N:(t + 1) * N], win.ap()[:, k, t * 128:(t + 1) * 128], hs.ap()[:, k, :],
                     [win.r(), hs.r()], [pM.r()], start=(k == 0), stop=(k == KT - 1), inc=(k == KT - 1 and t == 6))
        K.cp(rws.ap(), pM.ap()[:, 0:7 * N].rearrange("p (t n) -> p t n", t=7), [pM.r()], [rws.r()], eng="act")
        for (c0, c1) in ((0, 512), (512, 896)):
            for k in range(KT):
                K.mm(pA.ap()[0:NS, c0:c1], hs.ap()[:, k, :], win.ap()[:, k, c0:c1], [win.r(), hs.r()], [pA.r()],
                     start=(k == 0), stop=(k == KT - 1))
        K.cp(rwtm.ap(), pA.ap()[0:NS, 0:896], [pA.r()], [rwtm.r()], eng="act")
        K.dma(dr["s_shift"][l], rwtm.ap(), r=[rwtm.r()])
        for t in range(7):
            K.tr(pL.ap()[:, t * N:(t + 1) * N], shs.ap()[:, t * 128:(t + 1) * 128], identf.ap()[0:NS, 0:NS],
                 [shs.r(), identf.r()], [pL.r()], inc=(t == 6))
        K.cp(prevs.ap(), pL.ap()[:, 0:7 * N].rearrange("p (t n) -> p t n", t=7), [pL.r()], [prevs.r()])
        B["_rw_res"] = [rws.r(), prevs.r()]
        for _ in rwkv_prep(K, C, pc, LW, N, rws.ap(), prevs.ap(), B, pB, pL, pM):
            pass
        r, k_, v = B["_rkv"]
        g = lambda n: B[n].ap()
        K.act(g("e1"), g("sig"), AF.Exp, [B["sig"].r()], [B["e1"].r()], scale=-C0)
        K.ts(g("e2"), g("kk"), -1.0, ALU.mult, [B["kk"].r()], [B["e2"].r()])
        K.tt(g("bb"), g("kk"), g("aic"), ALU.mult, [B["kk"].r(), B["aic"].r()], [B["bb"].r()])
        srcs = [(r, B["MX"].r()), (g("e1"), B["e1"].r()), (g("kp"), B["kp"].r()), (v, B["MX"].r()),
                (g("e2"), B["e2"].r()), (g("bb"), B["bb"].r())]
        for q, (ap, res) in enumerate(srcs):
            pp = pA if q < 4 else pB
            for t in range(2):
                col = ((q % 4) * 2 + t) * 128
                K.tr(pp.ap()[0:NS, col:col + 128], ap[:, t, :], identf.ap(), [res, identf.r()], [pp.r()])
        K.cp(pkT.ap()[:, 0:4, :], pA.ap()[0:NS, :].rearrange("p (q n) -> p q n", q=4), [pA.r()], [pkT.r()], eng="act")
        K.cp(pkT.ap()[:, 4:6, :], pB.ap()[0:NS, 0:512].rearrange("p (q n) -> p q n", q=2), [pB.r()], [pkT.r()])
        for q in range(6):
            K.dma(scr.ap()[:, :, q, :], pkT.ap()[:, q, :].rearrange("p (h k) -> p h k", h=4), r=[pkT.r()], w=[scr.r()])
        K.dma(pkh.ap(), scr.ap().rearrange("b h q k -> (b h) q k"), r=[scr.r()], w=[pkh.r()])
        rq, wq, kq, vq, aq, bq = (pkh.ap()[:, i, :] for i in range(6))
        K.tt(tmp.ap(), S.ap(), bc(aq, 1, [64, 64, 64]), ALU.mult, [S.r(), pkh.r()], [tmp.r()])
        K.red(sa.ap(), tmp.ap(), [tmp.r()], [sa.r()])
        K.tt(S.ap(), S.ap(), bc(wq, 1, [64, 64, 64]), ALU.mult, [S.r(), pkh.r()], [S.r()])
        K.tt(tmp.ap(), bc(sa.ap(), 2, [64, 64, 64]), bc(bq, 1, [64, 64, 64]), ALU.mult, [sa.r(), pkh.r()], [tmp.r()])
        K.tt(S.ap(), S.ap(), tmp.ap(), ALU.add, [S.r(), tmp.r()], [S.r()])
        K.tt(tmp.ap(), bc(vq, 2, [64, 64, 64]), bc(kq, 1, [64, 64, 64]), ALU.mult, [pkh.r()], [tmp.r()])
        K.tt(S.ap(), S.ap(), tmp.ap(), ALU.add, [S.r(), tmp.r()], [S.r()])
        K.dma(dr["s_rwkv"][l].rearrange("b h v k -> (b h) (v k)"), S.ap().rearrange("p v k -> p (v k)"), r=[S.r()])
        K.tt(tmp.ap(), S.ap(), bc(rq, 1, [64, 64, 64]), ALU.mult, [S.r(), pkh.r()], [tmp.r()])
        K.red(o.ap(), tmp.ap(), [tmp.r()], [o.r()])
        groupnorm64(K, o.ap().rearrange("p (g e) -> p g e", g=1), 64, 1, gn, [o.r()],
                    on.ap().rearrange("p (g e) -> p g e", g=1), [on.r()])
        K.dma(so.ap().rearrange("b (h v) -> (b h) v", h=4), on.ap(), r=[on.r()], w=[so.r()])
        K.dma(otm.ap(), so.ap(), r=[so.r()], w=[otm.r()])
        K.cp(otb.ap(), otm.ap(), [otm.r()], [otb.r()])
        for t in range(2):
            K.tr(pT.ap()[:, t * N:(t + 1) * N], otb.ap()[:, t * 128:(t + 1) * 128], identb.ap()[0:NS, 0:NS],
                 [otb.r(), identb.r()], [pT.r()], inc=(t == 1))
        rwkv_epilogue(K, C, pc, B, N, pT, yT.ap()[:, 4:6, T:T + NS], xr(yT, 4, range(4, 6)))
        P.barrier()
```

```python
import contextlib
import numpy as np
import concourse.bass as bass
import concourse.mybir as mybir
from concourse.bass_utils import run_bass_kernel_spmd

F32 = mybir.dt.float32
BF16 = mybir.dt.bfloat16
AF = mybir.ActivationFunctionType
ALU = mybir.AluOpType
AX = mybir.AxisListType

NCORES = 8
D = 1024
KT = 8
T = 2048
NS = 16
NT = T + NS
NCH = T // 128
DEPTH = 2
IN_DIM = 2968
OFF = dict(z=0, xbc=512, dt=1280, rw=1288, gq=2184, gk=2312, gv=2440, glo=2696, gg=2712)
F_DENSE = 2816
ALPHA = (2.0 * DEPTH) ** 0.25
LN_EPS = 1e-5
RMS_EPS = 1e-6
RWKV_GN_EPS = 64 * 1e-5
BLOCKS = [(0, 512), (512, 512), (1024, 512), (1536, 512), (2048, 16)]

ENGS = ["pe", "dve", "act", "pool", "sp"]


class Res:
    __slots__ = ("name", "w", "r", "excl")

    def __init__(self, name="", excl=False):
        self.name = name
        self.w = None
        self.r = []
        self.excl = excl


class Prog:
    NDMA = 8

    def __init__(self, nc):
        self.nc = nc
        self.q = {e: [] for e in ENGS}
        self.cnt = {e: 0 for e in ENGS}
        self.seen = {e: {} for e in ENGS}
        self.dma_i = {e: 0 for e in ENGS}
        self.dma_last = {}
        self.sems = {}

    def sem(self, key):
        if key not in self.sems:
            self.sems[key] = self.nc.alloc_semaphore(name="s_" + "_".join(str(k) for k in key))
        return self.sems[key]

    def _collect(self, eng, reads, writes):
        waits = {}

        def add(tok):
            if tok is None:
                return
            key, val = tok
            if self.seen[eng].get(key, 0) >= val:
                return
            if waits.get(key, 0) < val:
                waits[key] = val

        for r in reads:
            add(r.w)
        for w in writes:
            add(w.w)
            for t in w.r:
                add(t)
        if eng == "pe":
            waits.pop(("e", "pe"), None)
        for k, v in waits.items():
            self.seen[eng][k] = v
        return list(waits.items())

    def _commit(self, tok, reads, writes):
        for r in reads:
            r.r.append(tok)
            if len(r.r) > 64:
                r.r = _prune(r.r)
        for w in writes:
            w.w = tok
            w.r = []

    def op(self, eng, fn, reads=(), writes=(), inc=True, self_wait=False):
        assert inc or eng == "pe"
        if any(r.excl for r in reads):
            writes = list(writes) + [r for r in reads if r.excl]
            reads = [r for r in reads if not r.excl]
        waits = self._collect(eng, reads, writes)
        if self_wait and self.cnt[eng] > 0:
            waits.append((("e", eng), self.cnt[eng]))
        tok = (("e", eng), self.cnt[eng] + 1)
        if inc:
            self.cnt[eng] += 1
        self._commit(tok, reads, writes)

        def emit(e, fn=fn, waits=waits, inc=inc, eng=eng):
            for k, v in waits:
                e.wait_ge(self.sem(k), v)
            ins = fn(e)
            if inc:
                ins.then_inc(self.sem(("e", eng)), 1)
        self.q[eng].append(emit)
        return tok

    def dma(self, queue, out, in_, reads=(), writes=(), **kw):
        i = self.dma_i[queue]
        self.dma_i[queue] += 1
        slot = i % self.NDMA
        key = ("d", queue, slot)
        val = 16 * (i // self.NDMA + 1)
        waits = self._collect(queue, reads, writes)
        prev = val - 16
        if prev > 0 and self.seen[queue].get(key, 0) < prev:
            self.seen[queue][key] = prev
            waits.append((key, prev))
        tok = (key, val)
        self.dma_last[key] = val
        self._commit(tok, reads, writes)

        def emit(e, waits=waits, key=key):
            for k, v in waits:
                e.wait_ge(self.sem(k), v)
            e.dma_start(out=out, in_=in_, **kw).then_inc(self.sem(key), 16)
        self.q[queue].append(emit)
        return tok

    def barrier(self, engines=ENGS):
        toks = [(("e", e), self.cnt[e]) for e in ENGS if self.cnt[e] > 0]
        toks += list(self.dma_last.items())
        for eng in engines:
            waits = []
            for k, v in toks:
                if k == ("e", eng) and eng == "pe":
                    continue
                if self.seen[eng].get(k, 0) < v:
                    self.seen[eng][k] = v
                    waits.append((k, v))

            def emit(e, waits=waits):
                for k, v in waits:
                    e.wait_ge(self.sem(k), v)
            self.q[eng].append(emit)

    def emit(self):
        with self.nc.Block() as block:
            @block.tensor
            def _(e):
                for f in self.q["pe"]:
                    f(e)

            @block.vector
            def _(e):
                for f in self.q["dve"]:
                    f(e)

            @block.scalar
            def _(e):
                for f in self.q["act"]:
                    f(e)

            @block.gpsimd
            def _(e):
                for f in self.q["pool"]:
                    f(e)

            @block.sync
            def _(e):
                for f in self.q["sp"]:
                    f(e)


def _prune(toks):
    best = {}
    for k, v in toks:
        if best.get(k, 0) < v:
            best[k] = v
    return list(best.items())


class Tn:
    def __init__(self, h, name, excl=False):
        self.h = h
        self.name = name
        self._res = {}
        self.excl = excl

    def ap(self):
        return self.h.ap()

    def r(self, key=0):
        if key not in self._res:
            self._res[key] = Res(f"{self.name}:{key}", self.excl)
        return self._res[key]


class KB:
    def __init__(self, nc):
        self.nc = nc
        self.P = Prog(nc)
        self.uid = 0

    def sb(self, stack, name, shape, dt=F32):
        self.uid += 1
        h = stack.enter_context(self.nc.sbuf_tensor(f"{name}_{self.uid}", list(shape), dt))
        return Tn(h, name)

    def ps(self, stack, name, shape, dt=F32):
        self.uid += 1
        h = stack.enter_context(self.nc.psum_tensor(f"{name}_{self.uid}", list(shape), dt))
        return Tn(h, name, excl=True)

    def mm(self, out, lhsT, rhs, r, w, start=True, stop=True, inc=None, self_wait=False, sgc=False):
        inc = stop if inc is None else inc
        kw = {"skip_group_check": True} if sgc else {}
        self.P.op("pe", lambda e: e.matmul(out, lhsT=lhsT, rhs=rhs, start=start, stop=stop, **kw),
                  reads=r, writes=w, inc=inc, self_wait=self_wait)

    def tr(self, out, in_, ident, r, w, inc=True):
        self.P.op("pe", lambda e: e.transpose(out, in_, ident), reads=r, writes=w, inc=inc)

    def act(self, out, in_, func, r, w, scale=None, bias=None, accum_out=None):
        kw = {}
        if scale is not None:
            kw["scale"] = scale
        if bias is not None:
            kw["bias"] = bias
        if accum_out is not None:
            kw["accum_out"] = accum_out
        self.P.op("act", lambda e: e.activation(out=out, in_=in_, func=func, **kw), reads=r, writes=w)

    def tt(self, out, in0, in1, op, r, w, eng="dve"):
        self.P.op(eng, lambda e: e.tensor_tensor(out=out, in0=in0, in1=in1, op=op), reads=r, writes=w)

    def ts(self, out, in0, s1, op0, r, w, s2=None, op1=None, eng="dve", accum_out=None):
        kw = {}
        if op1 is not None:
            kw["op1"] = op1
        if accum_out is not None:
            kw["accum_out"] = accum_out
        self.P.op(eng, lambda e: e.tensor_scalar(out=out, in0=in0, scalar1=s1, scalar2=s2, op0=op0, **kw),
                  reads=r, writes=w)

    def stt(self, out, in0, scalar, in1, op0, op1, r, w, eng="dve"):
        self.P.op(eng, lambda e: e.scalar_tensor_tensor(out=out, in0=in0, scalar=scalar, in1=in1, op0=op0, op1=op1),
                  reads=r, writes=w)

    def cp(self, out, in_, r, w, eng="dve"):
        if eng == "act":
            self.P.op("act", lambda e: e.activation(out=out, in_=in_, func=AF.Copy), reads=r, writes=w)
        else:
            self.P.op(eng, lambda e: e.tensor_copy(out=out, in_=in_), reads=r, writes=w)

    def recip(self, out, in_, r, w):
        self.P.op("dve", lambda e: e.reciprocal(out=out, in_=in_), reads=r, writes=w)

    def red(self, out, in_, r, w, op=ALU.add, axis=AX.X, eng="dve"):
        self.P.op(eng, lambda e: e.tensor_reduce(out=out, in_=in_, axis=axis, op=op), reads=r, writes=w)

    def memset(self, ap, val, w, eng="dve"):
        self.P.op(eng, lambda e: e.memset(ap, val), writes=w)

    def dma(self, out, in_, r=(), w=(), q="sp", **kw):
        return self.P.dma(q, out, in_, reads=r, writes=w, **kw)


W_SHAPES = dict(
    w_ada=[DEPTH, D, 6 * D], b_ada=[DEPTH, 6 * D], w_in=[DEPTH, D, IN_DIM], w_out=[DEPTH, D, D],
    ssd_conv_w=[DEPTH, 4, 768], ssd_conv_b=[DEPTH, 768], ssd_dt_bias=[DEPTH, 8], ssd_a_log=[DEPTH, 8],
    ssd_d=[DEPTH, 8], ssd_norm_g=[DEPTH, 512], rwkv_mu=[DEPTH, 896], rwkv_w0=[DEPTH, 256],
    rwkv_w2=[DEPTH, 32, 256], rwkv_a0=[DEPTH, 256], rwkv_a2=[DEPTH, 32, 256], rwkv_g2=[DEPTH, 64, 256],
    rwkv_k_k=[DEPTH, 256], rwkv_k_a=[DEPTH, 256], rwkv_r_k=[DEPTH, 4, 64], rwkv_ln_g=[DEPTH, 256],
    rwkv_ln_b=[DEPTH, 256], gla_w_gk2=[DEPTH, 16, 128], gla_b_gk=[DEPTH, 128], gla_norm_g=[DEPTH, 64],
    ln_mix_g=[DEPTH, D], ln_mix_b=[DEPTH, D], ln_ffn_g=[DEPTH, D], ln_ffn_b=[DEPTH, D],
    ffn_w_gate=[1, D, F_DENSE], ffn_w_up=[1, D, F_DENSE], ffn_w_down=[1, F_DENSE, D],
    moe_router=[1, D, 8], moe_w_gate=[1, 8, D, D], moe_w_up=[1, 8, D, D], moe_w_down=[1, 8, D, D],
)
IN_SHAPES = dict(
    xp=[T, D], xs=[NS, D], cc=[1 + NS, D],
    st_ssd=[DEPTH, NS, 8, 64, 64], st_conv=[DEPTH, NS, 3, 768], st_rwkv=[DEPTH, NS, 4, 64, 64],
    st_shift=[DEPTH, NS, 896], st_gla=[DEPTH, NS, 4, 32, 64],
)
OUT_SHAPES = dict(
    y_p=[T, D], y_s=[NS, D],
    p_ssd=[DEPTH, 8, 64, 64], p_conv=[DEPTH, 3, 768], p_rwkv=[DEPTH, 4, 64, 64], p_shift=[DEPTH, 896],
    p_gla=[DEPTH, 4, 32, 64],
    s_ssd=[DEPTH, NS, 8, 64, 64], s_conv=[DEPTH, NS, 3, 768], s_rwkv=[DEPTH, NS, 4, 64, 64],
    s_shift=[DEPTH, NS, 896], s_gla=[DEPTH, NS, 4, 32, 64],
)

def xr(t, b, tiles=range(KT)):
    return [t.r((d, b)) for d in tiles]


SH1, SC1, GT1, SH2, SC2, GT2 = 0, 8, 16, 24, 32, 40


def build(stub_mixer=False, dbg=None, n_layers=DEPTH):
    nc = bass.Bass("TRN2", target_bir_lowering=False)
    K = KB(nc)
    P = K.P
    dr = {}
    for n, s in IN_SHAPES.items():
        dr[n] = nc.dram_tensor(n, s, F32, kind="ExternalInput").ap()
    for n, s in W_SHAPES.items():
        dr[n] = nc.dram_tensor(n, s, F32, kind="ExternalInput").ap()
    for n, s in OUT_SHAPES.items():
        dr[n] = nc.dram_tensor(n, s, F32, kind="ExternalOutput").ap()
    dbg_out = {}
    if dbg:
        for n, s in dbg.items():
            dbg_out[n] = nc.dram_tensor("dbg_" + n, s, F32, kind="ExternalOutput").ap()
    out_res = Res("outputs")

    with contextlib.ExitStack() as perm, nc.allow_non_contiguous_dma(reason="small param loads"):
        xT = K.sb(perm, "xT", [128, KT, NT], F32)
        modT = [K.sb(perm, f"modT{l}", [128, 48, 1 + NS], F32) for l in range(DEPTH)]
        identf = K.sb(perm, "identf", [128, 128], F32)
        identb = K.sb(perm, "identb", [128, 128], BF16)
        onesM = K.sb(perm, "onesM", [128, 128], F32)
        ones1 = K.sb(perm, "ones1", [128, 128], F32)
        maskU = K.sb(perm, "maskU", [128, 128], F32)
        maskSU = K.sb(perm, "maskSU", [128, 128], F32)
        maskSL = K.sb(perm, "maskSL", [128, 128], F32)
        blk64 = K.sb(perm, "blk64", [128, 128], F32)
        C = dict(xT=xT, modT=modT, identf=identf, identb=identb, onesM=onesM, ones1=ones1,
                 maskU=maskU, maskSU=maskSU, maskSL=maskSL, blk64=blk64)

        def sel(t, val_keep, cmp, fill, base=0, cm=1, pat=None, ap=None):
            ap = t.ap() if ap is None else ap
            pat = [[-1, ap.shape[-1]]] if pat is None else pat
            P.op("pool", lambda e: e.affine_select(out=ap, in_=ap, pattern=pat, compare_op=cmp, fill=fill,
                                                   base=base, channel_multiplier=cm),
                 reads=[t.r()], writes=[t.r()])

        K.memset(identf.ap(), 0.0, [identf.r()], eng="pool")
        sel(identf, 0, ALU.not_equal, 1.0)
        K.cp(identb.ap(), identf.ap(), [identf.r()], [identb.r()], eng="pool")
        K.memset(onesM.ap(), 1.0 / D, [onesM.r()], eng="pool")
        K.memset(ones1.ap(), 1.0, [ones1.r()], eng="pool")
        K.memset(maskU.ap(), 1.0, [maskU.r()], eng="pool")
        sel(maskU, 1, ALU.is_ge, 0.0, cm=-1, pat=[[1, 128]])
        K.memset(maskSU.ap(), 1.0, [maskSU.r()], eng="pool")
        sel(maskSU, 1, ALU.is_gt, 0.0, cm=-1, pat=[[1, 128]])
        K.memset(maskSL.ap(), 1.0, [maskSL.r()], eng="pool")
        sel(maskSL, 1, ALU.is_gt, 0.0)
        K.memset(blk64.ap(), 0.0, [blk64.r()], eng="pool")
        K.memset(blk64.ap()[0:64, 0:64], 1.0, [blk64.r()], eng="pool")
        K.memset(blk64.ap()[64:128, 64:128], 1.0, [blk64.r()], eng="pool")

        with contextlib.ExitStack() as ph:
            stage = [K.sb(ph, f"stg{i}", [128, D], F32) for i in range(2)]
            ctm = K.sb(ph, "ctm", [1 + NS, D], F32)
            scT = K.sb(ph, "scT", [128, KT, 1 + NS], BF16)
            wada = [K.sb(ph, f"wada{i}", [128, KT, 512], BF16) for i in range(2)]
            bB = [K.sb(ph, f"bB{i}", [1 + NS, 512], F32) for i in range(2)]
            modsb = [K.sb(ph, f"modsb{i}", [1 + NS, 512], F32) for i in range(2)]
            pst = [K.ps(ph, f"pst{i}", [128, 1024], F32) for i in range(2)]
            psm = [K.ps(ph, f"psm{i}", [128, 512], F32) for i in range(2)]
            pss = K.ps(ph, "pss", [128, 512], F32)
            for tt in range(NCH + 1):
                st = stage[tt % 2]
                pt = pst[tt % 2]
                n = 128 if tt < NCH else NS
                src = dr["xp"][tt * 128:(tt + 1) * 128, :] if tt < NCH else dr["xs"]
                K.dma(st.ap()[0:n, :], src, w=[st.r()])
                for d in range(KT):
                    K.tr(pt.ap()[:, d * n:(d + 1) * n], st.ap()[0:n, d * 128:(d + 1) * 128],
                         identf.ap()[0:n, 0:n], [st.r(), identf.r()], [pt.r()], inc=(d == KT - 1))
                dst = xT.ap()[:, :, tt * 128:tt * 128 + n]
                srcp = pt.ap()[:, 0:KT * n].rearrange("p (d n) -> p d n", d=KT)
                K.cp(dst, srcp, [pt.r()], xr(xT, min(tt // 4, 4)), eng=("act" if tt % 2 == 0 else "dve"))
            K.dma(ctm.ap(), dr["cc"], w=[ctm.r()])
            for d in range(KT):
                K.tr(pss.ap()[:, d * 17:(d + 1) * 17], ctm.ap()[:, d * 128:(d + 1) * 128],
                     identf.ap()[0:17, 0:17], [ctm.r(), identf.r()], [pss.r()], inc=(d == KT - 1))
            K.act(scT.ap(), pss.ap()[:, 0:KT * 17].rearrange("p (d n) -> p d n", d=KT), AF.Silu,
                  [pss.r()], [scT.r()])
            it = 0
            for l in range(DEPTH):
                for j in range(12):
                    wb = wada[it % 2]
                    bb = bB[it % 2]
                    ms = modsb[it % 2]
                    pm = psm[it % 2]
                    K.dma(wb.ap(), dr["w_ada"][l, :, j * 512:(j + 1) * 512].rearrange("(k p) n -> p k n", p=128),
                          w=[wb.r()], q="pool")
                    K.dma(bb.ap(), dr["b_ada"][l:l + 1, j * 512:(j + 1) * 512].to_broadcast([1 + NS, 512]),
                          w=[bb.r()])
                    for k in range(KT):
                        K.mm(pm.ap()[0:17, :], scT.ap()[:, k, :], wb.ap()[:, k, :], [scT.r(), wb.r()], [pm.r()],
                             start=(k == 0), stop=(k == KT - 1))
                    K.tt(ms.ap(), pm.ap()[0:17, :], bb.ap(), ALU.add, [pm.r(), bb.r()], [ms.r()])
                    for q4 in range(4):
                        K.tr(pss.ap()[:, q4 * 17:(q4 + 1) * 17], ms.ap()[:, q4 * 128:(q4 + 1) * 128],
                             identf.ap()[0:17, 0:17], [ms.r(), identf.r()], [pss.r()], inc=(q4 == 3))
                    K.cp(modT[l].ap()[:, j * 4:(j + 1) * 4, :],
                         pss.ap()[:, 0:4 * 17].rearrange("p (d n) -> p d n", d=4), [pss.r()], [modT[l].r()],
                         eng="act")
                    it += 1
                for seg in (SC1, GT1, SC2, GT2):
                    K.ts(modT[l].ap()[:, seg:seg + 8, :], modT[l].ap()[:, seg:seg + 8, :], 1.0, ALU.add,
                         [modT[l].r()], [modT[l].r()])
            P.barrier()

        for l in range(n_layers):
            layer(K, dr, C, l, stub_mixer, dbg_out)

        with contextlib.ExitStack() as ph:
            stage = [K.sb(ph, f"ostg{i}", [128, D], F32) for i in range(2)]
            pst = [K.ps(ph, f"opst{i}", [128, 1024], F32) for i in range(2)]
            for tt in range(NCH + 1):
                st = stage[tt % 2]
                pt = pst[tt % 2]
                n = 128 if tt < NCH else NS
                for d in range(KT):
                    K.tr(pt.ap()[0:n, d * 128:(d + 1) * 128], xT.ap()[:, d, tt * 128:tt * 128 + n],
                         identf.ap(), [xT.r((d, min(tt // 4, 4))), identf.r()], [pt.r()], inc=(d == KT - 1))
                K.cp(st.ap()[0:n, :], pt.ap()[0:n, :], [pt.r()], [st.r()], eng=("act" if tt % 2 == 0 else "dve"))
                dst = dr["y_p"][tt * 128:(tt + 1) * 128, :] if tt < NCH else dr["y_s"]
                K.dma(dst, st.ap()[0:n, :], r=[st.r()])
            P.barrier()
    with nc.allow_non_contiguous_dma(reason="small param loads"):
        P.emit()
    return nc


def modulate(K, C, l, src, dst, b, sh, sc, dres):
    mod = C["modT"][l]
    c0, n = BLOCKS[b]
    if b < 4:
        for d in range(KT):
            K.act(dst.ap()[:, d, c0:c0 + n], src.ap()[:, d, c0:c0 + n], AF.Identity,
                  [src.r((d, b)), mod.r()], [dres(d)],
                  scale=mod.ap()[:, sc + d, 0:1], bias=mod.ap()[:, sh + d, 0:1])
    else:
        tmp = C["tmp_s"]
        K.tt(tmp.ap(), src.ap()[:, :, c0:c0 + n], mod.ap()[:, sc:sc + 8, 1:1 + NS], ALU.mult,
             xr(src, b) + [mod.r()], [tmp.r()])
        K.tt(dst.ap()[:, :, c0:c0 + n], tmp.ap(), mod.ap()[:, sh:sh + 8, 1:1 + NS], ALU.add,
             [tmp.r(), mod.r()], [dres(d) for d in range(KT)])


def layernorm(K, C, l, gi, psA, psB, scr):
    xT, onesM, lncol = C["xT"], C["onesM"], C["lncol"]
    sq, mean_sb, var, tt_ = scr["sq"], scr["mean"], scr["var"], scr["t"]
    for b, (c0, n) in enumerate(BLOCKS):
        for d in range(KT):
            s = sq[d % 2]
            xs = xT.ap()[:, d, c0:c0 + n]
            K.act(s.ap()[:, :n], xs, AF.Square, [xT.r((d, b))], [s.r()])
            K.mm(psA.ap()[:, :n], onesM.ap(), xs, [onesM.r(), xT.r((d, b))], [psA.r()],
                 start=(d == 0), stop=(d == KT - 1), inc=True)
            K.mm(psB.ap()[:, :n], onesM.ap(), s.ap()[:, :n], [onesM.r(), s.r()], [psB.r()],
                 start=(d == 0), stop=(d == KT - 1), inc=True)
        K.cp(mean_sb.ap()[:, :n], psA.ap()[:, :n], [psA.r()], [mean_sb.r()], eng="act")
        K.tt(var.ap()[:, :n], mean_sb.ap()[:, :n], mean_sb.ap()[:, :n], ALU.mult, [mean_sb.r()], [var.r()])
        K.tt(var.ap()[:, :n], psB.ap()[:, :n], var.ap()[:, :n], ALU.subtract, [psB.r(), var.r()], [var.r()])
        K.act(var.ap()[:, :n], var.ap()[:, :n], AF.Sqrt, [var.r()], [var.r()], bias=LN_EPS)
        K.recip(var.ap()[:, :n], var.ap()[:, :n], [var.r()], [var.r()])
        for d in range(KT):
            t = tt_[d % 2]
            xs = xT.ap()[:, d, c0:c0 + n]
            K.tt(t.ap()[:, :n], xs, mean_sb.ap()[:, :n], ALU.subtract, [xT.r((d, b)), mean_sb.r()], [t.r()])
            K.tt(t.ap()[:, :n], t.ap()[:, :n], var.ap()[:, :n], ALU.mult, [t.r(), var.r()], [t.r()])
            K.act(xs, t.ap()[:, :n], AF.Identity, [t.r(), lncol.r()], [xT.r((d, b))],
                  scale=lncol.ap()[:, gi, d:d + 1], bias=lncol.ap()[:, gi + 1, d:d + 1])


def residual_add(K, C, l, ps, n, dout, b, gt, comb=None):
    xT, mod = C["xT"], C["modT"][l]
    c0, _ = BLOCKS[b]
    xs = xT.ap()[:, dout, c0:c0 + n]
    src = ps.ap()[:, :n]
    rd = [ps.r(), mod.r(), xT.r((dout, b))]
    if comb is not None:
        tmp = C["tmp_c"][dout % 2]
        K.tt(tmp.ap()[:, :n], src, comb.ap()[:, c0:c0 + n], ALU.mult, [ps.r(), comb.r(b)], [tmp.r()])
        src = tmp.ap()[:, :n]
        rd = [tmp.r(), mod.r(), xT.r((dout, b))]
    if b < 4:
        K.stt(xs, src, mod.ap()[:, gt + dout, 0:1], xs, ALU.mult, ALU.add, rd, [xT.r((dout, b))])
    else:
        tmp2 = C["tmp_s2"]
        K.tt(tmp2.ap(), src, mod.ap()[:, gt + dout, 1:1 + NS], ALU.mult, rd[:2], [tmp2.r()])
        K.tt(xs, tmp2.ap(), xs, ALU.add, [tmp2.r(), xT.r((dout, b))], [xT.r((dout, b))])


def scale_x(K, C):
    xT = C["xT"]
    for b, (c0, n) in enumerate(BLOCKS):
        for d in range(KT):
            xs = xT.ap()[:, d, c0:c0 + n]
            K.P.op("act", lambda e, xs=xs: e.mul(out=xs, in_=xs, mul=ALPHA), reads=[xT.r((d, b))],
                   writes=[xT.r((d, b))])


def ffn_gateup(K, C, l, hT, actb, wpool, srcs, nf, ps):
    wg_src, wu_src, wd_src = srcs
    sg = C["sg"]

    def unit():
        u = wpool["bufs"][wpool["i"] % len(wpool["bufs"])]
        wpool["i"] += 1
        return u

    i = 0
    for f0 in range(0, nf, 4):
        nfu = min(4, nf - f0)
        WG, WU = unit(), unit()
        K.dma(WG.ap()[:, :, 0:nfu * 128], wg_src[:, f0 * 128:(f0 + nfu) * 128].rearrange("(k p) n -> p k n", p=128),
              w=[WG.r()], q="pool")
        K.dma(WU.ap()[:, :, 0:nfu * 128], wu_src[:, f0 * 128:(f0 + nfu) * 128].rearrange("(k p) n -> p k n", p=128),
              w=[WU.r()], q="pool")
        for fu in range(nfu):
            f = f0 + fu
            for b, (c0, n) in enumerate(BLOCKS):
                pg, pu = ps["g"][i % 2], ps["u"][i % 2]
                for k in range(KT):
                    K.mm(pg.ap()[:, :n], WG.ap()[:, k, fu * 128:(fu + 1) * 128], hT.ap()[:, k, c0:c0 + n],
                         [WG.r(), hT.r((k, b))], [pg.r()], start=(k == 0), stop=(k == KT - 1))
                for k in range(KT):
                    K.mm(pu.ap()[:, :n], WU.ap()[:, k, fu * 128:(fu + 1) * 128], hT.ap()[:, k, c0:c0 + n],
                         [WU.r(), hT.r((k, b))], [pu.r()], start=(k == 0), stop=(k == KT - 1))
                s_ = sg[i % 2]
                K.act(s_.ap()[:, :n], pg.ap()[:, :n], AF.Silu, [pg.r()], [s_.r()])
                K.tt(actb.ap()[:, f, c0:c0 + n], s_.ap()[:, :n], pu.ap()[:, :n], ALU.mult, [s_.r(), pu.r()],
                     [actb.r((f, b))])
                i += 1
                yield


def ffn_down(K, C, l, actb, wpool, srcs, nf, ps, comb=None):
    wg_src, wu_src, wd_src = srcs

    def unit():
        u = wpool["bufs"][wpool["i"] % len(wpool["bufs"])]
        wpool["i"] += 1
        return u

    i = 0
    for dh in range(2):
        WD = unit()
        K.dma(WD.ap()[:, 0:nf, :], wd_src[:, dh * 512:(dh + 1) * 512].rearrange("(f p) n -> p f n", p=128),
              w=[WD.r()], q="pool")
        for dd in range(4):
            dout = dh * 4 + dd
            for b, (c0, n) in enumerate(BLOCKS):
                pd = ps["d"][i % 2]
                for f in range(nf):
                    K.mm(pd.ap()[:, :n], WD.ap()[:, f, dd * 128:(dd + 1) * 128], actb.ap()[:, f, c0:c0 + n],
                         [WD.r(), actb.r((f, b))], [pd.r()], start=(f == 0), stop=(f == nf - 1))
                residual_add(K, C, l, pd, n, dout, b, GT2, comb=comb)
                i += 1


def ffn_group(K, C, l, hT, actb, wpool, srcs, nf, ps, comb=None):
    for _ in ffn_gateup(K, C, l, hT, actb, wpool, srcs, nf, ps):
        pass
    ffn_down(K, C, l, actb, wpool, srcs, nf, ps, comb=comb)


def moe_routing(K, C, dr, l, ph, ps):
    xT, mod, identf = C["xT"], C["modT"][l], C["identf"]
    router = K.sb(ph, "router", [128, KT, 8], F32)
    K.dma(router.ap(), dr["moe_router"][0].rearrange("(k p) e -> p k e", p=128), w=[router.r()])
    combT = K.sb(ph, "combT", [8, NT], F32)
    tp = C["tpair"]
    sm = {n: K.sb(ph, "rt_" + n, [128, 8], F32) for n in ["lg", "eq1", "l2", "eq2", "cb"]}
    sc1 = {n: K.sb(ph, "rs_" + n, [128, 1], F32) for n in ["m1", "m2", "e", "w1", "w2"]}
    pl, pt = ps["rA"], ps["rB"]
    for tt in range(NCH + 1):
        n = 128 if tt < NCH else NS
        c0 = tt * 128
        b = min(tt // 4, 4)
        hap = tp.ap().rearrange("p a (b c) -> p (a b) c", c=128)
        hres = [tp.r(0), tp.r(1)]
        if tt < NCH:
            for d in range(KT):
                K.act(hap[:, d, :], xT.ap()[:, d, c0:c0 + n], AF.Identity, [xT.r((d, b)), mod.r()], hres,
                      scale=mod.ap()[:, SC2 + d, 0:1], bias=mod.ap()[:, SH2 + d, 0:1])
        else:
            K.tt(hap[:, :, 0:n], xT.ap()[:, :, c0:c0 + n], mod.ap()[:, SC2:SC2 + 8, 1:1 + NS], ALU.mult,
                 xr(xT, b) + [mod.r()], hres)
            K.tt(hap[:, :, 0:n], hap[:, :, 0:n], mod.ap()[:, SH2:SH2 + 8, 1:1 + NS], ALU.add,
                 hres + [mod.r()], hres)
        for d in range(KT):
            K.mm(pl.ap()[0:n, 0:8], hap[:, d, 0:n], router.ap()[:, d, :], hres + [router.r()], [pl.r()],
                 start=(d == 0), stop=(d == KT - 1), inc=True)
        lg, eq1, l2, eq2, cb = (sm[k].ap()[0:n, :] for k in ["lg", "eq1", "l2", "eq2", "cb"])
        m1, m2, ee, w1, w2 = (sc1[k].ap()[0:n, :] for k in ["m1", "m2", "e", "w1", "w2"])
        R = lambda *ks: [(sm[k] if k in sm else sc1[k]).r() for k in ks]
        K.cp(lg, pl.ap()[0:n, 0:8], [pl.r()], R("lg"))
        yield
        K.red(m1, lg, R("lg"), R("m1"), op=ALU.max)
        yield
        K.ts(eq1, lg, m1, ALU.is_equal, R("lg", "m1"), R("eq1"))
        yield
        K.stt(l2, eq1, -1e30, lg, ALU.mult, ALU.add, R("eq1", "lg"), R("l2"))
        yield
        K.red(m2, l2, R("l2"), R("m2"), op=ALU.max)
        yield
        K.ts(eq2, l2, m2, ALU.is_equal, R("l2", "m2"), R("eq2"))
        yield
        K.tt(ee, m2, m1, ALU.subtract, R("m1", "m2"), R("e"))
        yield
        K.act(ee, ee, AF.Exp, R("e"), R("e"))
        yield
        K.ts(w1, ee, 1.0, ALU.add, R("e"), R("w1"))
        yield
        K.recip(w1, w1, R("w1"), R("w1"))
        yield
        K.tt(w2, ee, w1, ALU.mult, R("e", "w1"), R("w2"))
        yield
        K.ts(cb, eq1, w1, ALU.mult, R("eq1", "w1"), R("cb"))
        yield
        K.stt(cb, eq2, w2, cb, ALU.mult, ALU.add, R("eq2", "w2", "cb"), R("cb"))
        yield
        K.tr(pt.ap()[0:8, 0:n], cb, identf.ap()[0:n, 0:n], R("cb") + [identf.r()], [pt.r()])
        yield
        K.cp(combT.ap()[:, c0:c0 + n], pt.ap()[0:8, 0:n], [pt.r()], [combT.r()], eng="act")
        yield
    C["_combT"] = combT
    yield


def layer(K, dr, C, l, stub_mixer, dbg_out):
    nc, P = K.nc, K.P
    xT, mod = C["xT"], C["modT"][l]
    with contextlib.ExitStack() as lay:
        bufA = K.sb(lay, "bufA", [128, KT, NT], BF16)
        lncol = K.sb(lay, "lncol", [128, 4, KT], F32)
        C["lncol"] = lncol
        C["tmp_s"] = K.sb(lay, "tmp_s", [128, KT, NS], F32)
        C["tmp_s2"] = K.sb(lay, "tmp_s2", [128, NS], F32)
        for i, nme in enumerate(["ln_mix_g", "ln_mix_b", "ln_ffn_g", "ln_ffn_b"]):
            K.dma(lncol.ap()[:, i, :], dr[nme][l].rearrange("(d p) -> p d", p=128), w=[lncol.r()])

        with contextlib.ExitStack() as ph:
            if stub_mixer:
                for b in range(5):
                    modulate(K, C, l, xT, bufA, b, SH1, SC1, lambda d, b=b: bufA.r((d, b)))
            else:
                mixers(K, dr, C, l, bufA, dbg_out)
            P.barrier()
            wout = K.sb(ph, "wout", [128, KT, D], BF16)
            K.dma(wout.ap(), dr["w_out"][l].rearrange("(k p) n -> p k n", p=128), w=[wout.r()], q="pool")
            scale_x(K, C)
            with contextlib.ExitStack() as ph2:
                pso = [K.ps(ph2, f"pso{i}", [128, 512], F32) for i in range(4)]
                i = 0
                for dout in range(KT):
                    for b, (c0, n) in enumerate(BLOCKS):
                        pd = pso[i % 4]
                        for k in range(KT):
                            K.mm(pd.ap()[:, :n], wout.ap()[:, k, dout * 128:(dout + 1) * 128],
                                 bufA.ap()[:, k, c0:c0 + n], [wout.r(), bufA.r((k, b))], [pd.r()],
                                 start=(k == 0), stop=(k == KT - 1))
                        residual_add(K, C, l, pd, n, dout, b, GT1)
                        i += 1
                P.barrier()

        with contextlib.ExitStack() as ph:
            hT = K.sb(ph, "hT", [128, KT, NT], BF16)
            tpair = K.sb(ph, "tpair", [128, 2, 512], F32)
            C["tpair"] = tpair

            class _V:
                def __init__(self, i):
                    self.i = i

                def ap(self):
                    return tpair.ap()[:, self.i, :]

                def r(self, key=0):
                    return tpair.r(self.i)
            scr = dict(sq=[K.sb(ph, f"sq{i}", [128, 512], F32) for i in range(2)],
                       mean=K.sb(ph, "mean", [128, 512], F32), var=K.sb(ph, "var", [128, 512], F32),
                       t=[_V(0), _V(1)])
            C["sg"] = scr["sq"]
            C["tmp_c"] = scr["t"]
            W = dict(bufs=[K.sb(ph, f"WP{i}", [128, KT, 512], BF16) for i in range(4)], i=0)
            ps = dict(g=[K.ps(ph, f"pg{i}", [128, 512], F32) for i in range(2)],
                      u=[K.ps(ph, f"pu{i}", [128, 512], F32) for i in range(2)],
                      d=[K.ps(ph, f"pd{i}", [128, 512], F32) for i in range(2)])
            psA = K.ps(ph, "psA", [128, 512], F32)
            psB = K.ps(ph, "psB", [128, 512], F32)
            layernorm(K, C, l, 0, psA, psB, scr)
            for b in range(5):
                modulate(K, C, l, xT, hT, b, SH2, SC2, lambda d, b=b: hT.r((d, b)))
            if l % 2 == 0:
                scale_x(K, C)
                i = l // 2
                for f0 in range(0, F_DENSE // 128, 8):
                    nf = min(8, F_DENSE // 128 - f0)
                    srcs = (dr["ffn_w_gate"][i, :, f0 * 128:(f0 + nf) * 128],
                            dr["ffn_w_up"][i, :, f0 * 128:(f0 + nf) * 128],
                            dr["ffn_w_down"][i, f0 * 128:(f0 + nf) * 128, :])
                    ffn_group(K, C, l, hT, bufA, W, srcs, nf, ps)
            else:
                ps["rA"], ps["rB"] = psA, psB
                i = l // 2
                srcs0 = (dr["moe_w_gate"][i, 0], dr["moe_w_up"][i, 0], dr["moe_w_down"][i, 0])
                interleave([moe_routing(K, C, dr, l, ph, ps), ffn_gateup(K, C, l, hT, bufA, W, srcs0, 8, ps)],
                           ratio=[6, 1])
                combT = C["_combT"]
                scale_x(K, C)
                combB = K.sb(ph, "combB", [128, NT], F32)
                sele = K.sb(ph, "sele", [8, 128], F32)
                i = l // 2
                for e_ in range(8):
                    K.memset(sele.ap(), 0.0, [sele.r()])
                    K.P.op("dve", lambda e, e_=e_: e.memset(sele.ap()[e_:e_ + 1, :], 1.0), reads=[sele.r()],
                           writes=[sele.r()]) if False else K.ts(
                        sele.ap(), C["identf"].ap()[0:8, e_:e_ + 1].to_broadcast([8, 128]), 1.0, ALU.mult,
                        [C["identf"].r()], [sele.r()])
                    for b, (c0, n) in enumerate(BLOCKS):
                        pb = ps["d"][b % 2]
                        K.mm(pb.ap()[:, :n], sele.ap(), combT.ap()[:, c0:c0 + n], [sele.r(), combT.r()],
                             [pb.r()])
                        K.cp(combB.ap()[:, c0:c0 + n], pb.ap()[:, :n], [pb.r()], [combB.r(b)], eng="act")
                    srcs = (dr["moe_w_gate"][i, e_], dr["moe_w_up"][i, e_], dr["moe_w_down"][i, e_])
                    if e_ > 0:
                        for _ in ffn_gateup(K, C, l, hT, bufA, W, srcs, 8, ps):
                            pass
                    ffn_down(K, C, l, bufA, W, srcs, 8, ps, comb=combB)
            layernorm(K, C, l, 2, psA, psB, scr)
            P.barrier()


def make_in_maps(inp):
    g = lambda k: np.ascontiguousarray(np.asarray(inp[k], dtype=np.float32))
    xp, xs, cp, cs = g("x_prompt"), g("x_sample"), g("c_prompt"), g("c_sample")
    st = {k: g(k) for k in ["state_ssd", "state_ssd_conv", "state_rwkv", "state_rwkv_shift", "state_gla"]}
    wts = {k: g(k) for k in W_SHAPES}
    maps = []
    for c in range(NCORES):
        sl = slice(c * NS, (c + 1) * NS)
        m = dict(wts)
        m["xp"] = xp[c]
        m["xs"] = np.ascontiguousarray(xs[sl, 0, :])
        m["cc"] = np.ascontiguousarray(np.concatenate([cp[c:c + 1], cs[sl]], axis=0))
        m["st_ssd"] = np.ascontiguousarray(st["state_ssd"][:, sl])
        m["st_conv"] = np.ascontiguousarray(st["state_ssd_conv"][:, sl])
        m["st_rwkv"] = np.ascontiguousarray(st["state_rwkv"][:, sl])
        m["st_shift"] = np.ascontiguousarray(st["state_rwkv_shift"][:, sl])
        m["st_gla"] = np.ascontiguousarray(st["state_gla"][:, sl])
        maps.append(m)
    return maps


_NC_CACHE = {}


def gather(results):
    R = lambda k: [np.asarray(r[k], dtype=np.float32) for r in results]
    y_p = np.stack(R("y_p"), axis=0)
    y_s = np.concatenate(R("y_s"), axis=0)[:, None, :]
    outs = [y_p, y_s]
    for k in ["p_ssd", "p_conv", "p_rwkv", "p_shift", "p_gla"]:
        outs.append(np.stack(R(k), axis=1))
    for k in ["s_ssd", "s_conv", "s_rwkv", "s_shift", "s_gla"]:
        outs.append(np.concatenate(R(k), axis=1))
    return tuple(np.ascontiguousarray(o) for o in outs)


def kernel(**inputs):
    if "nc" not in _NC_CACHE:
        _NC_CACHE["nc"] = build()
    res = run_bass_kernel_spmd(_NC_CACHE["nc"], make_in_maps(inputs), core_ids=list(range(NCORES)))
    return gather(res.results)


def bc(ap, axis, shape):
    return ap.unsqueeze(axis).to_broadcast(list(shape))


def sigmoid_chain(K, ap, res):
    K.act(ap, ap, AF.Ln, res, res, bias=1.0)
    K.act(ap, ap, AF.Exp, res, res, scale=-1.0)


def rsqrt_(K, ap, res, scale, eps):
    K.act(ap, ap, AF.Ln, res, res, scale=scale, bias=eps)
    K.act(ap, ap, AF.Exp, res, res, scale=-0.5)


def softplus_(K, x, tmp, r, n):
    xa, ta = x[0], tmp[0]
    K.act(ta, xa, AF.Abs, [x[1]], [tmp[1]])
    K.act(ta, ta, AF.Exp, [tmp[1]], [tmp[1]], scale=-1.0)
    K.act(ta, ta, AF.Ln, [tmp[1]], [tmp[1]], bias=1.0)
    K.ts(xa, xa, 0.0, ALU.max, [x[1]], [x[1]])
    K.tt(xa, xa, ta, ALU.add, [x[1], tmp[1]], [x[1]])


def make_hc(K, C, l, hc, c):
    xT, mod = C["xT"], C["modT"][l]
    import os
    if os.environ.get("HC_POOL", "1") == "1":
        tmp = C["hc_tmp"]
        xs = xT.ap()[:, :, c * 128:(c + 1) * 128]
        K.tt(tmp.ap(), xs, bc(mod.ap()[:, SC1:SC1 + 8, 0], 2, [128, KT, 128]), ALU.mult,
             xr(xT, c // 4) + [mod.r()], [tmp.r()], eng="pool")
        K.tt(hc.ap(), tmp.ap(), bc(mod.ap()[:, SH1:SH1 + 8, 0], 2, [128, KT, 128]), ALU.add,
             [tmp.r(), mod.r()], [hc.r()], eng="pool")
        return
    for d in range(KT):
        K.act(hc.ap()[:, d, :], xT.ap()[:, d, c * 128:(c + 1) * 128], AF.Identity,
              [xT.r((d, c // 4)), mod.r()], [hc.r()],
              scale=mod.ap()[:, SC1 + d, 0:1], bias=mod.ap()[:, SH1 + d, 0:1])


def mixers(K, dr, C, l, yT, dbg_out):
    P = K.P
    xT, mod = C["xT"], C["modT"][l]
    en = C.get("enable", ("ssd", "rwkv", "gla"))
    with contextlib.ExitStack() as mx:
        hc = [K.sb(mx, f"hc{i}", [128, KT, 128], BF16) for i in range(2)]
        hs = K.sb(mx, "hs", [128, KT, NS], BF16)
        C["hc"], C["hs"] = hc, hs
        C["hc_tmp"] = K.sb(mx, "hc_tmp", [128, KT, 128], F32)
        modulate(K, C, l, xT, _Shift(hs, 2048), 4, SH1, SC1, lambda d: hs.r())
        for name, tiles in (("ssd", range(0, 4)), ("rwkv", range(4, 6)), ("gla", range(6, 8))):
            if name not in en:
                for d in tiles:
                    for b, (c0, n) in enumerate(BLOCKS):
                        K.memset(yT.ap()[:, d, c0:c0 + n], 0.0, [yT.r((d, b))])
        if "ssd" in en and "gla" in en:
            ssd_gla_phase(K, dr, C, l, yT, dbg_out)
            P.barrier()
        else:
            if "ssd" in en:
                ssd_phase(K, dr, C, l, yT, dbg_out)
                P.barrier()
            if "gla" in en:
                gla_phase(K, dr, C, l, yT, dbg_out)
                P.barrier()
        if "rwkv" in en:
            rwkv_phase(K, dr, C, l, yT, dbg_out)
            P.barrier()


class _Shift:
    def __init__(self, t, off):
        self.t, self.off = t, off

    def ap(self):
        return _ShiftAP(self.t.ap(), self.off)

    def r(self, key=0):
        return self.t.r()


class _ShiftAP:
    def __init__(self, ap, off):
        self._ap, self.off = ap, off

    def __getitem__(self, key):
        p, d, s = key
        return self._ap[p, d, slice(s.start - self.off, s.stop - self.off)]


def ssd_phase(K, dr, C, l, yT, dbg_out):
    P = K.P
    nc = K.nc
    identb, identf, maskU, maskSL, ones1 = C["identb"], C["identf"], C["maskU"], C["maskSL"], C["ones1"]
    hc, hs = C["hc"], C["hs"]
    with contextlib.ExitStack() as ph:
        win = K.sb(ph, "win_ssd", [128, KT, 1288], BF16)
        K.dma(win.ap(), dr["w_in"][l, :, 0:1288].rearrange("(k p) n -> p k n", p=128), w=[win.r()], q="pool")
        convw = K.sb(ph, "convw", [128, 6, 4], F32)
        convb = K.sb(ph, "convb", [128, 6], F32)
        for i in range(4):
            K.dma(convw.ap()[:, :, i], dr["ssd_conv_w"][l, i].rearrange("(t p) -> p t", p=128), w=[convw.r()])
        K.dma(convb.ap(), dr["ssd_conv_b"][l].rearrange("(t p) -> p t", p=128), w=[convb.r()])
        normg = K.sb(ph, "normg", [128, 4], F32)
        K.dma(normg.ap(), dr["ssd_norm_g"][l].rearrange("(t p) -> p t", p=128), w=[normg.r()])
        dtbB = K.sb(ph, "dtbB", [128, 8], F32)
        aB = K.sb(ph, "aB", [128, 8], F32)
        dB = K.sb(ph, "dB", [128, 8], F32)
        K.dma(dtbB.ap(), dr["ssd_dt_bias"][l:l + 1, :].to_broadcast([128, 8]), w=[dtbB.r()])
        K.dma(aB.ap(), dr["ssd_a_log"][l:l + 1, :].to_broadcast([128, 8]), w=[aB.r()])
        K.dma(dB.ap(), dr["ssd_d"][l:l + 1, :].to_broadcast([128, 8]), w=[dB.r()])
        K.act(aB.ap(), aB.ap(), AF.Exp, [aB.r()], [aB.r()])
        K.ts(aB.ap(), aB.ap(), -1.0, ALU.mult, [aB.r()], [aB.r()])
        import os
        if os.environ.get("SKIP_SSD_PROMPT") != "1":
            ssd_prompt(K, dr, C, l, yT, win, convw, convb, normg, dtbB, aB, dB, dbg_out)
        P.barrier()
        if os.environ.get("SKIP_SSD_SAMPLE") != "1":
            ssd_sample(K, dr, C, l, yT, win, aB, dB, dtbB, dbg_out)


def ssd_prompt(K, dr, C, l, yT, win, convw, convb, normg, dtbB, aB, dB, dbg_out):
    P = K.P
    identb, identf, maskU, maskSL, ones1 = C["identb"], C["identf"], C["maskU"], C["maskSL"], C["ones1"]
    hc, hs = C["hc"], C["hs"]
    with contextlib.ExitStack() as ph:
        XB = [K.sb(ph, f"XB{i}", [128, 6, 131], F32) for i in range(2)]
        XC = [K.sb(ph, f"XC{i}", [128, 6, 128], BF16) for i in range(2)]
        cacc = [K.sb(ph, f"cacc{i}", [128, 128], F32) for i in range(2)]
        sz = K.sb(ph, "sz", [128, 512], F32)
        dtt = K.sb(ph, "dtt", [128, 8], F32)
        dtmp = K.sb(ph, "dtmp", [128, 8], F32)
        dtA = K.sb(ph, "dtA", [128, 8], F32)
        csb = K.sb(ph, "csb", [128, 16], F32)
        e1 = K.sb(ph, "e1", [128, 8], F32)
        el = K.sb(ph, "el", [128, 8], F32)
        tail = K.sb(ph, "tail", [128, 8], F32)
        Rt = K.sb(ph, "Rt", [128, 8, 128], F32)
        dec = K.sb(ph, "dec", [128, 8, 128], F32)
        Gs = K.sb(ph, "Gs", [128, 2, 128], F32)
        Mb = K.sb(ph, "Mb", [128, 8, 128], BF16)
        XT = K.sb(ph, "XT", [128, 640], BF16)
        xD = K.sb(ph, "xD", [128, 512], BF16)
        xw = K.sb(ph, "xw", [128, 512], BF16)
        t1 = K.sb(ph, "t1", [128, 512], F32)
        yn = K.sb(ph, "yn", [128, 512], BF16)
        ss = K.sb(ph, "ss", [128, 1], F32)
        HS32 = K.sb(ph, "HS32", [128, 4, 64], F32)
        HSb = K.sb(ph, "HSb", [128, 4, 64], BF16)
        hsT = K.sb(ph, "hsT", [128, 2, 128], F32)

        ps_x = K.ps(ph, "ps_x", [128, 1024], F32)
        ps_z = K.ps(ph, "ps_z", [128, 512], F32)
        ps_c = K.ps(ph, "ps_c", [128, 512], F32)
        ps_t = K.ps(ph, "ps_t", [128, 1024], BF16)
        ps_y = K.ps(ph, "ps_y", [128, 512], F32)
        ps_i = K.ps(ph, "ps_i", [128, 512], F32)
        ps_h = K.ps(ph, "ps_h", [128, 512], F32)

        K.memset(HS32.ap(), 0.0, [HS32.r()])
        K.memset(HSb.ap(), 0.0, [HSb.r()])
        K.memset(XB[0].ap()[:, :, 0:3], 0.0, [XB[0].r()])

        for c in range(NCH):
            h = hc[c % 2]
            make_hc(K, C, l, h, c)
            xb, xc = XB[c % 2], XC[c % 2]
            for ct in range(6):
                for k in range(KT):
                    K.mm(ps_x.ap()[:, ct * 128:(ct + 1) * 128], win.ap()[:, k, 512 + ct * 128:512 + (ct + 1) * 128],
                         h.ap()[:, k, :], [win.r(), h.r()], [ps_x.r()], start=(k == 0), stop=(k == KT - 1),
                         inc=(k == KT - 1 and ct == 5))
            K.cp(xb.ap()[:, :, 3:131], ps_x.ap()[:, 0:768].rearrange("p (t n) -> p t n", t=6), [ps_x.r()], [xb.r()],
                 eng="act")
            if c + 1 < NCH:
                K.cp(XB[(c + 1) % 2].ap()[:, :, 0:3], xb.ap()[:, :, 128:131], [xb.r()], [XB[(c + 1) % 2].r()])
            for ct in range(6):
                ca = cacc[ct % 2]
                K.ts(ca.ap(), xb.ap()[:, ct, 0:128], convw.ap()[:, ct, 0:1], ALU.mult, [xb.r(), convw.r(), convb.r()],
                     [ca.r()], s2=convb.ap()[:, ct:ct + 1], op1=ALU.add)
                for i in range(1, 4):
                    K.stt(ca.ap(), xb.ap()[:, ct, i:i + 128], convw.ap()[:, ct, i:i + 1], ca.ap(), ALU.mult, ALU.add,
                          [xb.r(), convw.r(), ca.r()], [ca.r()])
                K.act(xc.ap()[:, ct, :], ca.ap(), AF.Silu, [ca.r()], [xc.r()])
            for k in range(KT):
                K.mm(ps_z.ap(), h.ap()[:, k, :], win.ap()[:, k, 0:512], [h.r(), win.r()], [ps_z.r()],
                     start=(k == 0), stop=(k == KT - 1))
            for k in range(KT):
                K.mm(ps_c.ap()[:, 0:8], h.ap()[:, k, :], win.ap()[:, k, 1280:1288], [h.r(), win.r()], [ps_c.r()],
                     start=(k == 0), stop=(k == KT - 1))
            K.act(sz.ap(), ps_z.ap(), AF.Silu, [ps_z.r()], [sz.r()])
            K.tt(dtt.ap(), ps_c.ap()[:, 0:8], dtbB.ap(), ALU.add, [ps_c.r(), dtbB.r()], [dtt.r()])
            softplus_(K, (dtt.ap(), dtt.r()), (dtmp.ap(), dtmp.r()), None, None)
            K.tt(dtA.ap(), dtt.ap(), aB.ap(), ALU.mult, [dtt.r(), aB.r()], [dtA.r()])
            K.mm(ps_c.ap()[:, 8:16], maskU.ap(), dtA.ap(), [maskU.r(), dtA.r()], [ps_c.r()])
            K.mm(ps_c.ap()[:, 16:24], ones1.ap(), dtA.ap(), [ones1.r(), dtA.r()], [ps_c.r()])
            K.tt(Rt.ap(), bc(maskU.ap(), 1, [128, 8, 128]), bc(dtA.ap(), 2, [128, 8, 128]), ALU.mult,
                 [maskU.r(), dtA.r()], [Rt.r()])
            for hf in range(2):
                K.mm(ps_x.ap()[:, hf * 512:(hf + 1) * 512], maskSL.ap(),
                     Rt.ap()[:, hf * 4:(hf + 1) * 4, :].rearrange("p h i -> p (h i)"), [maskSL.r(), Rt.r()],
                     [ps_x.r()])
            K.act(dec.ap().rearrange("p h i -> p (h i)"), ps_x.ap(), AF.Exp, [ps_x.r()], [dec.r()])
            K.cp(csb.ap(), ps_c.ap()[:, 8:24], [ps_c.r()], [csb.r()], eng="act")
            for g in range(2):
                K.mm(ps_c.ap()[:, 256 + g * 128:256 + (g + 1) * 128], xc.ap()[64 * g:64 * g + 64, 4, :],
                     xc.ap()[64 * g:64 * g + 64, 5, :], [xc.r()], [ps_c.r()], self_wait=(g == 1))
            K.tt(Gs.ap(), ps_c.ap()[:, 256:512].rearrange("p (g i) -> p g i", g=2), bc(maskU.ap(), 1, [128, 2, 128]),
                 ALU.mult, [ps_c.r(), maskU.r()], [Gs.r()])
            K.tt(dec.ap().rearrange("p (g r) i -> p g r i", g=2), dec.ap().rearrange("p (g r) i -> p g r i", g=2),
                 bc(Gs.ap(), 2, [128, 2, 4, 128]), ALU.mult, [dec.r(), Gs.r()], [dec.r()])
            K.tt(Mb.ap(), dec.ap(), bc(dtt.ap(), 2, [128, 8, 128]), ALU.mult, [dec.r(), dtt.r()], [Mb.r()])
            for ct in range(5):
                K.tr(ps_t.ap()[:, ct * 128:(ct + 1) * 128], xc.ap()[:, ct, :], identb.ap(), [xc.r(), identb.r()],
                     [ps_t.r()], inc=(ct == 4))
            K.cp(XT.ap(), ps_t.ap()[:, 0:640], [ps_t.r()], [XT.r()], eng="act")
            K.tt(xD.ap().rearrange("p (h q) -> p h q", h=8), XT.ap()[:, 0:512].rearrange("p (h q) -> p h q", h=8),
                 bc(dB.ap(), 2, [128, 8, 64]), ALU.mult, [XT.r(), dB.r()], [xD.r()])
            K.mm(ps_y.ap(), identb.ap(), xD.ap(), [identb.r(), xD.r()], [ps_y.r()], start=True, stop=False)
            for hh in range(8):
                K.mm(ps_y.ap()[:, hh * 64:(hh + 1) * 64], Mb.ap()[:, hh, :], XT.ap()[:, hh * 64:(hh + 1) * 64],
                     [Mb.r(), XT.r()], [ps_y.r()], start=False, stop=(hh == 7))
            for g in range(2):
                K.mm(ps_i.ap()[:, g * 256:(g + 1) * 256], xc.ap()[64 * g:64 * g + 64, 5, :],
                     HSb.ap()[64 * g:64 * g + 64, :, :].rearrange("p h q -> p (h q)"), [xc.r(), HSb.r()], [ps_i.r()],
                     self_wait=(g == 1))
            K.act(e1.ap(), csb.ap()[:, 0:8], AF.Exp, [csb.r()], [e1.r()])
            K.tt(t1.ap().rearrange("p (h q) -> p h q", h=8), ps_i.ap().rearrange("p (h q) -> p h q", h=8),
                 bc(e1.ap(), 2, [128, 8, 64]), ALU.mult, [ps_i.r(), e1.r()], [t1.r()])
            K.tt(t1.ap(), t1.ap(), ps_y.ap(), ALU.add, [t1.r(), ps_y.r()], [t1.r()])
            ssd_epilogue(K, C, t1, sz, ss, yn, 128)
            for q in range(4):
                K.tr(ps_t.ap()[:, q * 128:(q + 1) * 128], yn.ap()[:, q * 128:(q + 1) * 128], identb.ap(),
                     [yn.r(), identb.r()], [ps_t.r()], inc=(q == 3))
            K.tt(yT.ap()[:, 0:4, c * 128:(c + 1) * 128], ps_t.ap()[:, 0:512].rearrange("p (t n) -> p t n", t=4),
                 bc(normg.ap(), 2, [128, 4, 128]), ALU.mult, [ps_t.r(), normg.r()], xr(yT, c // 4, range(4)))
            K.act(el.ap(), csb.ap()[:, 8:16], AF.Exp, [csb.r()], [el.r()])
            K.tt(tail.ap(), csb.ap()[:, 8:16], csb.ap()[:, 0:8], ALU.subtract, [csb.r()], [tail.r()])
            K.act(tail.ap(), tail.ap(), AF.Exp, [tail.r()], [tail.r()])
            K.tt(tail.ap(), tail.ap(), dtt.ap(), ALU.mult, [tail.r(), dtt.r()], [tail.r()])
            K.tt(xw.ap().rearrange("p (h q) -> p h q", h=8), XT.ap()[:, 0:512].rearrange("p (h q) -> p h q", h=8),
                 bc(tail.ap(), 2, [128, 8, 64]), ALU.mult, [XT.r(), tail.r()], [xw.r()])
            K.mm(ps_h.ap(), XT.ap()[:, 512:640], xw.ap(), [XT.r(), xw.r()], [ps_h.r()])
            for g in range(2):
                sl = slice(64 * g, 64 * g + 64)
                K.tt(HS32.ap()[sl], HS32.ap()[sl], bc(el.ap()[sl, 4 * g:4 * g + 4], 2, [64, 4, 64]), ALU.mult,
                     [HS32.r(), el.r()], [HS32.r()])
                K.tt(HS32.ap()[sl], HS32.ap()[sl],
                     ps_h.ap()[sl, 256 * g:256 * g + 256].rearrange("p (h q) -> p h q", h=4), ALU.add,
                     [HS32.r(), ps_h.r()], [HS32.r()])
            K.cp(HSb.ap(), HS32.ap(), [HS32.r()], [HSb.r()], eng="act")

        xb = XB[(NCH - 1) % 2]
        for i in range(3):
            K.dma(dr["p_conv"][l, i].rearrange("(t p) -> p t", p=128), xb.ap()[:, :, 128 + i], r=[xb.r()])
        for q in range(2):
            K.tr(ps_y.ap()[:, q * 128:(q + 1) * 128], HS32.ap().rearrange("p h q -> p (h q)")[:, q * 128:(q + 1) * 128],
                 identf.ap(), [HS32.r(), identf.r()], [ps_y.r()], inc=(q == 1))
        K.cp(hsT.ap(), ps_y.ap()[:, 0:256].rearrange("p (q n) -> p q n", q=2), [ps_y.r()], [hsT.r()])
        for g in range(2):
            for q in range(2):
                K.dma(dr["p_ssd"][l, 4 * g + 2 * q:4 * g + 2 * q + 2].rearrange("h p n -> (h p) n"),
                      hsT.ap()[:, q, 64 * g:64 * g + 64], r=[hsT.r()])
        P.barrier()


def ssd_epilogue(K, C, y, sz, ss, yn, n):
    K.tt(y.ap()[0:n], y.ap()[0:n], sz.ap()[0:n], ALU.mult, [y.r(), sz.r()], [y.r()])
    K.act(yn.ap()[0:n], y.ap()[0:n], AF.Square, [y.r()], [yn.r(), ss.r()], accum_out=ss.ap()[0:n])
    rsqrt_(K, ss.ap()[0:n], [ss.r()], 1.0 / 512, RMS_EPS)
    K.ts(yn.ap()[0:n], y.ap()[0:n], ss.ap()[0:n], ALU.mult, [y.r(), ss.r()], [yn.r()])


def ssd_gla_phase(K, dr, C, l, yT, dbg_out):
    P = K.P
    identb, identf, maskU, maskSL, ones1 = C["identb"], C["identf"], C["maskU"], C["maskSL"], C["ones1"]
    hc, hs = C["hc"], C["hs"]
    G0 = OFF["gq"]
    with contextlib.ExitStack() as ph0:
        win = K.sb(ph0, "win_ssd", [128, KT, 1288], BF16)
        K.dma(win.ap(), dr["w_in"][l, :, 0:1288].rearrange("(k p) n -> p k n", p=128), w=[win.r()], q="pool")
        convw = K.sb(ph0, "convw", [128, 6, 4], F32)
        convb = K.sb(ph0, "convb", [128, 6], F32)
        for i in range(4):
            K.dma(convw.ap()[:, :, i], dr["ssd_conv_w"][l, i].rearrange("(t p) -> p t", p=128), w=[convw.r()])
        K.dma(convb.ap(), dr["ssd_conv_b"][l].rearrange("(t p) -> p t", p=128), w=[convb.r()])
        normg = K.sb(ph0, "normg", [128, 4], F32)
        K.dma(normg.ap(), dr["ssd_norm_g"][l].rearrange("(t p) -> p t", p=128), w=[normg.r()])
        dtbB = K.sb(ph0, "dtbB", [128, 8], F32)
        aB = K.sb(ph0, "aB", [128, 8], F32)
        dB = K.sb(ph0, "dB", [128, 8], F32)
        K.dma(dtbB.ap(), dr["ssd_dt_bias"][l:l + 1, :].to_broadcast([128, 8]), w=[dtbB.r()])
        K.dma(aB.ap(), dr["ssd_a_log"][l:l + 1, :].to_broadcast([128, 8]), w=[aB.r()])
        K.dma(dB.ap(), dr["ssd_d"][l:l + 1, :].to_broadcast([128, 8]), w=[dB.r()])
        K.act(aB.ap(), aB.ap(), AF.Exp, [aB.r()], [aB.r()])
        K.ts(aB.ap(), aB.ap(), -1.0, ALU.mult, [aB.r()], [aB.r()])
        wing = K.sb(ph0, "win_gla", [128, KT, 784], BF16)
        K.dma(wing.ap(), dr["w_in"][l, :, G0:G0 + 784].rearrange("(k p) n -> p k n", p=128), w=[wing.r()], q="pool")
        wgk2 = K.sb(ph0, "wgk2", [16, 128], BF16)
        K.dma(wgk2.ap(), dr["gla_w_gk2"][l], w=[wgk2.r()], q="pool")
        bgkB = K.sb(ph0, "bgkB", [128, 128], F32)
        K.dma(bgkB.ap(), dr["gla_b_gk"][l:l + 1, :].to_broadcast([128, 128]), w=[bgkB.r()])
        gcol = K.sb(ph0, "gcol", [128, 1], F32)
        for t in range(2):
            K.dma(gcol.ap()[64 * t:64 * t + 64, :], dr["gla_norm_g"][l].rearrange("(e o) -> e o", o=1), w=[gcol.r()])
        with contextlib.ExitStack() as ph:
            BM = K.sb(ph, "BM", [128, 256], F32)
            hm = K.sb(ph, "hm", [128, 4], F32)
            K.memset(BM.ap(), 1.0, [BM.r()], eng="pool")
            K.memset(hm.ap(), 1.0, [hm.r()], eng="pool")
            for hh in range(4):
                for (t, sl, n) in ((BM, slice(64 * hh, 64 * hh + 64), 64), (hm, slice(hh, hh + 1), 1)):
                    ap = t.ap()[:, sl]
                    K.P.op("pool", lambda e, ap=ap, n=n, hh=hh: e.affine_select(
                        out=ap, in_=ap, pattern=[[0, n]], compare_op=ALU.is_ge, fill=0.0, base=-32 * hh,
                        channel_multiplier=1), reads=[t.r()], writes=[t.r()])
                    K.P.op("pool", lambda e, ap=ap, n=n, hh=hh: e.affine_select(
                        out=ap, in_=ap, pattern=[[0, n]], compare_op=ALU.is_gt, fill=0.0, base=32 * hh + 32,
                        channel_multiplier=-1), reads=[t.r()], writes=[t.r()])
            PX = [K.ps(ph, f"PX{i}", [128, 512], F32) for i in range(2)]
            PZ = K.ps(ph, "PZ", [128, 512], F32)
            PC = K.ps(ph, "PC", [128, 512], F32)
            PF = K.ps(ph, "PF", [128, 512], F32)
            PV = K.ps(ph, "PV", [128, 512], F32)
            PL = K.ps(ph, "PL", [128, 512], F32)
            PT = K.ps(ph, "PT", [128, 1024], BF16)
            XB = [K.sb(ph, f"XB{i}", [128, 6, 131], BF16) for i in range(2)]
            DW = K.sb(ph, "DW", [128, 6, 4, 128], BF16)
            negb = K.sb(ph, "negb", [128, 6], F32)
            e6 = K.sb(ph, "e6", [128, 6, 128], F32)
            xlast = K.sb(ph, "xlast", [128, 6, 3], F32)
            K.ts(negb.ap(), convb.ap(), -1.0, ALU.mult, [convb.r()], [negb.r()])
            for ct in range(6):
                for i in range(4):
                    K.ts(DW.ap()[:, ct, i, :], identf.ap(), convw.ap()[:, ct, i:i + 1], ALU.mult,
                         [identf.r(), convw.r()], [DW.r()])
            XC = [K.sb(ph, f"XC{i}", [128, 6, 128], BF16) for i in range(2)]
            sz = K.sb(ph, "sz", [128, 512], F32)
            dtt = K.sb(ph, "dtt", [128, 8], F32)
            dtmp = K.sb(ph, "dtmp", [128, 8], F32)
            dtA = K.sb(ph, "dtA", [128, 8], F32)
            csb = K.sb(ph, "csb", [128, 16], F32)
            e1 = K.sb(ph, "e1", [128, 8], F32)
            el = K.sb(ph, "el", [128, 8], F32)
            tail = K.sb(ph, "tail", [128, 8], F32)
            Rt = K.sb(ph, "Rt", [128, 8, 128], F32)
            dec = K.sb(ph, "dec", [128, 8, 128], F32)
            Gs = K.sb(ph, "Gs", [128, 2, 128], F32)
            Mb = K.sb(ph, "Mb", [128, 8, 128], BF16)
            XT = K.sb(ph, "XT", [128, 640], BF16)
            xD = K.sb(ph, "xD", [128, 512], BF16)
            xw = K.sb(ph, "xw", [128, 512], BF16)
            t1 = K.sb(ph, "t1", [128, 512], F32)
            yn = K.sb(ph, "yn", [128, 512], BF16)
            ss = K.sb(ph, "ss", [128, 1], F32)
            HS32 = K.sb(ph, "HS32", [128, 4, 64], F32)
            HSb = K.sb(ph, "HSb", [128, 4, 64], BF16)
            glo = K.sb(ph, "glo", [16, 128], BF16)
            lg = K.sb(ph, "lg", [128, 128], F32)
            lgt = K.sb(ph, "lgt", [128, 128], F32)
            Eq = K.sb(ph, "Eq", [128, 128], F32)
            Ek = K.sb(ph, "Ek", [128, 128], F32)
            Ekt = K.sb(ph, "Ekt", [128, 128], F32)
            qt = K.sb(ph, "qt", [128, 128], BF16)
            kf = K.sb(ph, "kf", [128, 128], F32)
            km = K.sb(ph, "km", [128, 4, 128], BF16)
            ktm = K.sb(ph, "ktm", [128, 128], BF16)
            vtm = K.sb(ph, "vtm", [128, 256], BF16)
            sgg = K.sb(ph, "sgg", [128, 256], F32)
            A = K.sb(ph, "A", [128, 4, 128], BF16)
            osq = K.sb(ph, "osq", [128, 256], F32)
            ms = K.sb(ph, "ms", [128, 4], F32)
            on = K.sb(ph, "on", [128, 256], F32)
            onb = K.sb(ph, "onb", [128, 256], BF16)
            tmpS = K.sb(ph, "tmpS", [128, 256], F32)
            S32 = K.sb(ph, "S32", [128, 256], F32)
            Sb = K.sb(ph, "Sb", [128, 256], BF16)

            K.memset(HS32.ap(), 0.0, [HS32.r()])
            K.memset(HSb.ap(), 0.0, [HSb.r()])
            K.memset(XB[0].ap()[:, :, 0:3], 0.0, [XB[0].r()])
            K.memset(S32.ap(), 0.0, [S32.r()])
            K.memset(Sb.ap(), 0.0, [Sb.r()])

            def ssd_body(c):
                h = hc[c % 2]
                xb, xc = XB[c % 2], XC[c % 2]
                def chain_a():
                    for ct in range(6):
                        px = PX[0] if ct < 4 else PX[1]
                        cc = ct if ct < 4 else ct - 4
                        for k in range(KT):
                            K.mm(px.ap()[:, cc * 128:(cc + 1) * 128], win.ap()[:, k, 512 + ct * 128:512 + (ct + 1) * 128],
                                 h.ap()[:, k, :], [win.r(), h.r()], [px.r()], start=(k == 0), stop=(k == KT - 1))
                        yield
                    K.cp(xb.ap()[:, 0:4, 3:131], PX[0].ap().rearrange("p (t n) -> p t n", t=4), [PX[0].r()], [xb.r()],
                         eng="act")
                    yield
                    K.cp(xb.ap()[:, 4:6, 3:131], PX[1].ap()[:, 0:256].rearrange("p (t n) -> p t n", t=2), [PX[1].r()],
                         [xb.r()], eng="act")
                    yield
                    if c + 1 < NCH:
                        K.cp(XB[(c + 1) % 2].ap()[:, :, 0:3], xb.ap()[:, :, 128:131], [xb.r()], [XB[(c + 1) % 2].r()])
                    if c == NCH - 1:
                        K.cp(xlast.ap()[:, 0:4, :], PX[0].ap().rearrange("p (t n) -> p t n", t=4)[:, :, 125:128],
                             [PX[0].r()], [xlast.r()])
                        K.cp(xlast.ap()[:, 4:6, :], PX[1].ap()[:, 0:256].rearrange("p (t n) -> p t n", t=2)[:, :, 125:128],
                             [PX[1].r()], [xlast.r()])
                    for ct in range(6):
                        px = PX[0] if ct < 4 else PX[1]
                        cc = ct if ct < 4 else ct - 4
                        for i in range(4):
                            K.mm(px.ap()[:, cc * 128:(cc + 1) * 128], DW.ap()[:, ct, i, :], xb.ap()[:, ct, i:i + 128],
                                 [DW.r(), xb.r()], [px.r()], start=(i == 0), stop=(i == 3))
                        yield
                    for ct in range(6):
                        px = PX[0] if ct < 4 else PX[1]
                        cc = ct if ct < 4 else ct - 4
                        K.act(e6.ap()[:, ct, :], px.ap()[:, cc * 128:(cc + 1) * 128], AF.Exp, [px.r(), negb.r()], [e6.r()],
                              scale=-1.0, bias=negb.ap()[:, ct:ct + 1])
                        yield
                    sigmoid_chain(K, e6.ap(), [e6.r()])
                    yield
                    for ct in range(6):
                        px = PX[0] if ct < 4 else PX[1]
                        cc = ct if ct < 4 else ct - 4
                        K.stt(xc.ap()[:, ct, :], px.ap()[:, cc * 128:(cc + 1) * 128], convb.ap()[:, ct:ct + 1], e6.ap()[:, ct, :],
                              ALU.add, ALU.mult, [px.r(), convb.r(), e6.r()], [xc.r()])
                        yield
                    for g in range(2):
                        K.mm(PC.ap()[:, 256 + g * 128:256 + (g + 1) * 128], xc.ap()[64 * g:64 * g + 64, 4, :],
                             xc.ap()[64 * g:64 * g + 64, 5, :], [xc.r()], [PC.r()], self_wait=(g == 1))
                    yield
                    K.tt(Gs.ap(), PC.ap()[:, 256:512].rearrange("p (g i) -> p g i", g=2), bc(maskU.ap(), 1, [128, 2, 128]),
                         ALU.mult, [PC.r(), maskU.r()], [Gs.r()])
                    yield
                    for ct in range(5):
                        K.tr(PT.ap()[:, ct * 128:(ct + 1) * 128], xc.ap()[:, ct, :], identb.ap(), [xc.r(), identb.r()],
                             [PT.r()], inc=(ct == 4))
                    yield
                    K.cp(XT.ap(), PT.ap()[:, 0:640], [PT.r()], [XT.r()], eng="act")
                    yield

                def chain_b():
                    for k in range(KT):
                        K.mm(PZ.ap(), h.ap()[:, k, :], win.ap()[:, k, 0:512], [h.r(), win.r()], [PZ.r()],
                             start=(k == 0), stop=(k == KT - 1))
                    for k in range(KT):
                        K.mm(PC.ap()[:, 0:8], h.ap()[:, k, :], win.ap()[:, k, 1280:1288], [h.r(), win.r()], [PC.r()],
                             start=(k == 0), stop=(k == KT - 1))
                    yield
                    K.act(sz.ap(), PZ.ap(), AF.Exp, [PZ.r()], [sz.r()], scale=-1.0)
                    yield
                    sigmoid_chain(K, sz.ap(), [sz.r()])
                    yield
                    K.tt(sz.ap(), sz.ap(), PZ.ap(), ALU.mult, [sz.r(), PZ.r()], [sz.r()])
                    yield
                    K.tt(dtt.ap(), PC.ap()[:, 0:8], dtbB.ap(), ALU.add, [PC.r(), dtbB.r()], [dtt.r()])
                    yield
                    softplus_(K, (dtt.ap(), dtt.r()), (dtmp.ap(), dtmp.r()), None, None)
                    yield
                    K.tt(dtA.ap(), dtt.ap(), aB.ap(), ALU.mult, [dtt.r(), aB.r()], [dtA.r()])
                    yield
                    K.mm(PC.ap()[:, 8:16], maskU.ap(), dtA.ap(), [maskU.r(), dtA.r()], [PC.r()])
                    K.mm(PC.ap()[:, 16:24], ones1.ap(), dtA.ap(), [ones1.r(), dtA.r()], [PC.r()])
                    yield
                    K.tt(Rt.ap(), bc(maskU.ap(), 1, [128, 8, 128]), bc(dtA.ap(), 2, [128, 8, 128]), ALU.mult,
                         [maskU.r(), dtA.r()], [Rt.r()])
                    yield
                    for hf in range(2):
                        K.mm(PZ.ap(), maskSL.ap(), Rt.ap()[:, hf * 4:(hf + 1) * 4, :].rearrange("p h i -> p (h i)"),
                             [maskSL.r(), Rt.r()], [PZ.r()])
                        yield
                        K.act(dec.ap()[:, hf * 4:(hf + 1) * 4, :].rearrange("p h i -> p (h i)"), PZ.ap(), AF.Exp,
                              [PZ.r()], [dec.r()])
                        yield
                    K.cp(csb.ap(), PC.ap()[:, 8:24], [PC.r()], [csb.r()], eng="act")
                    yield

                yield from interleave_gen([chain_a(), chain_b()], [2, 1])
                K.tt(dec.ap().rearrange("p (g r) i -> p g r i", g=2), dec.ap().rearrange("p (g r) i -> p g r i", g=2),
                     bc(Gs.ap(), 2, [128, 2, 4, 128]), ALU.mult, [dec.r(), Gs.r()], [dec.r()])
                yield
                K.tt(Mb.ap(), dec.ap(), bc(dtt.ap(), 2, [128, 8, 128]), ALU.mult, [dec.r(), dtt.r()], [Mb.r()])
                yield
                K.tt(xD.ap().rearrange("p (h q) -> p h q", h=8), XT.ap()[:, 0:512].rearrange("p (h q) -> p h q", h=8),
                     bc(dB.ap(), 2, [128, 8, 64]), ALU.mult, [XT.r(), dB.r()], [xD.r()])
                yield
                K.mm(PZ.ap(), identb.ap(), xD.ap(), [identb.r(), xD.r()], [PZ.r()], start=True, stop=False)
                for hh in range(8):
                    K.mm(PZ.ap()[:, hh * 64:(hh + 1) * 64], Mb.ap()[:, hh, :], XT.ap()[:, hh * 64:(hh + 1) * 64],
                         [Mb.r(), XT.r()], [PZ.r()], start=False, stop=(hh == 7))
                for g in range(2):
                    K.mm(PC.ap()[:, g * 256:(g + 1) * 256], xc.ap()[64 * g:64 * g + 64, 5, :],
                         HSb.ap()[64 * g:64 * g + 64, :, :].rearrange("p h q -> p (h q)"), [xc.r(), HSb.r()], [PC.r()],
                         self_wait=(g == 1))
                yield
                K.act(e1.ap(), csb.ap()[:, 0:8], AF.Exp, [csb.r()], [e1.r()])
                yield
                K.tt(t1.ap().rearrange("p (h q) -> p h q", h=8), PC.ap().rearrange("p (h q) -> p h q", h=8),
                     bc(e1.ap(), 2, [128, 8, 64]), ALU.mult, [PC.r(), e1.r()], [t1.r()])
                yield
                K.tt(t1.ap(), t1.ap(), PZ.ap(), ALU.add, [t1.r(), PZ.r()], [t1.r()])
                yield
                ssd_epilogue(K, C, t1, sz, ss, yn, 128)
                yield
                for q in range(4):
                    K.tr(PT.ap()[:, q * 128:(q + 1) * 128], yn.ap()[:, q * 128:(q + 1) * 128], identb.ap(),
                         [yn.r(), identb.r()], [PT.r()], inc=(q == 3))
                yield
                K.tt(yT.ap()[:, 0:4, c * 128:(c + 1) * 128], PT.ap()[:, 0:512].rearrange("p (t n) -> p t n", t=4),
                     bc(normg.ap(), 2, [128, 4, 128]), ALU.mult, [PT.r(), normg.r()], xr(yT, c // 4, range(4)))
                yield
                K.act(el.ap(), csb.ap()[:, 8:16], AF.Exp, [csb.r()], [el.r()])
                yield
                K.tt(tail.ap(), csb.ap()[:, 8:16], csb.ap()[:, 0:8], ALU.subtract, [csb.r()], [tail.r()])
                yield
                K.act(tail.ap(), tail.ap(), AF.Exp, [tail.r()], [tail.r()])
                yield
                K.tt(tail.ap(), tail.ap(), dtt.ap(), ALU.mult, [tail.r(), dtt.r()], [tail.r()])
                yield
                K.tt(xw.ap().rearrange("p (h q) -> p h q", h=8), XT.ap()[:, 0:512].rearrange("p (h q) -> p h q", h=8),
                     bc(tail.ap(), 2, [128, 8, 64]), ALU.mult, [XT.r(), tail.r()], [xw.r()])
                yield
                K.mm(PC.ap(), XT.ap()[:, 512:640], xw.ap(), [XT.r(), xw.r()], [PC.r()])
                yield
                for g in range(2):
                    sl = slice(64 * g, 64 * g + 64)
                    K.tt(HS32.ap()[sl], HS32.ap()[sl], bc(el.ap()[sl, 4 * g:4 * g + 4], 2, [64, 4, 64]), ALU.mult,
                         [HS32.r(), el.r()], [HS32.r()])
                    K.tt(HS32.ap()[sl], HS32.ap()[sl],
                         PC.ap()[sl, 256 * g:256 * g + 256].rearrange("p (h q) -> p h q", h=4), ALU.add,
                         [HS32.r(), PC.r()], [HS32.r()])
                    yield
                K.cp(HSb.ap(), HS32.ap(), [HS32.r()], [HSb.r()], eng="act")
                yield

            def gla_body(c):
                h = hc[c % 2]
                for (dst, cols) in ((PF.ap()[:, 0:128], slice(0, 128)), (PF.ap()[:, 128:256], slice(128, 256)),
                                    (PF.ap()[0:16, 256:384], slice(512, 528))):
                    for k in range(KT):
                        K.mm(dst, wing.ap()[:, k, cols], h.ap()[:, k, :], [wing.r(), h.r()], [PF.r()],
                             start=(k == 0), stop=(k == KT - 1))
                    yield
                for (dst, cols, pst) in ((PV.ap()[:, 0:256], slice(256, 512), PV), (PF.ap()[:, 384:512], slice(128, 256), PF),
                                         (PV.ap()[:, 256:512], slice(528, 784), PV)):
                    for k in range(KT):
                        K.mm(dst, h.ap()[:, k, :], wing.ap()[:, k, cols], [wing.r(), h.r()], [pst.r()],
                             start=(k == 0), stop=(k == KT - 1))
                    yield
                K.cp(glo.ap(), PF.ap()[0:16, 256:384], [PF.r()], [glo.r()], eng="act")
                yield
                K.mm(PL.ap()[:, 0:128], glo.ap(), wgk2.ap(), [glo.r(), wgk2.r()], [PL.r()])
                yield
                K.stt(lg.ap(), PL.ap()[:, 0:128], -1.0, bgkB.ap(), ALU.mult, ALU.subtract, [PL.r(), bgkB.r()], [lg.r()])
                yield
                softplus_(K, (lg.ap(), lg.r()), (lgt.ap(), lgt.r()), None, None)
                yield
                K.ts(lg.ap(), lg.ap(), -1.0 / 16.0, ALU.mult, [lg.r()], [lg.r()])
                yield
                K.mm(PL.ap()[:, 128:256], lg.ap(), maskU.ap(), [lg.r(), maskU.r()], [PL.r()])
                K.mm(PL.ap()[:, 256:384], maskU.ap(), lg.ap(), [lg.r(), maskU.r()], [PL.r()])
                yield
                K.act(Eq.ap(), PL.ap()[:, 128:256], AF.Exp, [PL.r()], [Eq.r()])
                yield
                K.act(Ek.ap(), PL.ap()[:, 128:256], AF.Exp, [PL.r()], [Ek.r()], scale=-1.0)
                yield
                K.act(Ekt.ap(), PL.ap()[:, 256:384], AF.Exp, [PL.r()], [Ekt.r()], scale=-1.0)
                yield
                K.stt(qt.ap(), PF.ap()[:, 0:128], 32.0 ** -0.5, Eq.ap(), ALU.mult, ALU.mult, [PF.r(), Eq.r()], [qt.r()])
                yield
                K.tt(kf.ap(), PF.ap()[:, 128:256], Ek.ap(), ALU.mult, [PF.r(), Ek.r()], [kf.r()])
                yield
                K.tt(km.ap(), bc(kf.ap(), 1, [128, 4, 128]), bc(hm.ap(), 2, [128, 4, 128]), ALU.mult, [kf.r(), hm.r()],
                     [km.r()])
                yield
                K.tt(ktm.ap(), PF.ap()[:, 384:512], Ekt.ap(), ALU.mult, [PF.r(), Ekt.r()], [ktm.r()])
                yield
                K.cp(vtm.ap(), PV.ap()[:, 0:256], [PV.r()], [vtm.r()], eng="act")
                yield
                K.act(sgg.ap(), PV.ap()[:, 256:512], AF.Exp, [PV.r()], [sgg.r()], scale=-1.0)
                yield
                sigmoid_chain(K, sgg.ap(), [sgg.r()])
                yield
                K.tt(sgg.ap(), sgg.ap(), PV.ap()[:, 256:512], ALU.mult, [sgg.r(), PV.r()], [sgg.r()])
                yield
                for hh in range(4):
                    K.mm(PL.ap()[:, hh * 128:(hh + 1) * 128], km.ap()[:, hh, :], qt.ap(), [km.r(), qt.r()], [PL.r()],
                         inc=(hh == 3))
                yield
                K.tt(A.ap(), PL.ap().rearrange("p (h i) -> p h i", h=4), bc(maskU.ap(), 1, [128, 4, 128]), ALU.mult,
                     [PL.r(), maskU.r()], [A.r()])
                yield
                K.mm(PL.ap()[:, 0:256], qt.ap(), Sb.ap(), [qt.r(), Sb.r()], [PL.r()], start=True, stop=False)
                for hh in range(4):
                    K.mm(PL.ap()[:, hh * 64:(hh + 1) * 64], A.ap()[:, hh, :], vtm.ap()[:, hh * 64:(hh + 1) * 64],
                         [A.r(), vtm.r()], [PL.r()], start=False, stop=(hh == 3))
                yield
                K.act(osq.ap(), PL.ap()[:, 0:256], AF.Square, [PL.r()], [osq.r()])
                yield
                K.red(ms.ap(), osq.ap().rearrange("p (h e) -> p h e", h=4), [osq.r()], [ms.r()])
                yield
                rsqrt_(K, ms.ap(), [ms.r()], 1.0 / 64, RMS_EPS)
                yield
                K.tt(on.ap().rearrange("p (h e) -> p h e", h=4), PL.ap()[:, 0:256].rearrange("p (h e) -> p h e", h=4),
                     bc(ms.ap(), 2, [128, 4, 64]), ALU.mult, [PL.r(), ms.r()], [on.r()])
                yield
                K.tt(onb.ap(), on.ap(), sgg.ap(), ALU.mult, [on.r(), sgg.r()], [onb.r()])
                yield
                for q in range(2):
                    K.tr(PT.ap()[:, 768 + q * 128:768 + (q + 1) * 128], onb.ap()[:, q * 128:(q + 1) * 128], identb.ap(),
                         [onb.r(), identb.r()], [PT.r()], inc=(q == 1))
                yield
                K.ts(yT.ap()[:, 6:8, c * 128:(c + 1) * 128], PT.ap()[:, 768:1024].rearrange("p (t n) -> p t n", t=2),
                     gcol.ap(), ALU.mult, [PT.r(), gcol.r()], xr(yT, c // 4, range(6, 8)))
                yield
                K.mm(PL.ap()[:, 256:512], ktm.ap(), vtm.ap(), [ktm.r(), vtm.r()], [PL.r()])
                yield
                K.tt(tmpS.ap(), PL.ap()[:, 256:512], BM.ap(), ALU.mult, [PL.r(), BM.r()], [tmpS.r()])
                yield
                K.tt(S32.ap(), S32.ap(), tmpS.ap(), ALU.add, [S32.r(), tmpS.r()], [S32.r()])
                yield
                K.ts(S32.ap(), S32.ap(), Eq.ap()[:, 127:128], ALU.mult, [S32.r(), Eq.r()], [S32.r()])
                yield
                K.cp(Sb.ap(), S32.ap(), [S32.r()], [Sb.r()], eng="act")
                yield

            for c in range(NCH):
                make_hc(K, C, l, hc[c % 2], c)
                interleave([ssd_body(c), gla_body(c)], ratio=[2, 1])

            xb = XB[(NCH - 1) % 2]
            for i in range(3):
                K.dma(dr["p_conv"][l, i].rearrange("(t p) -> p t", p=128), xlast.ap()[:, :, i], r=[xlast.r()])
            hsT = t1
            for q in range(2):
                K.tr(PZ.ap()[:, q * 128:(q + 1) * 128], HS32.ap().rearrange("p h q -> p (h q)")[:, q * 128:(q + 1) * 128],
                     identf.ap(), [HS32.r(), identf.r()], [PZ.r()], inc=(q == 1))
            K.cp(hsT.ap()[:, 0:256], PZ.ap()[:, 0:256], [PZ.r()], [hsT.r()])
            for g in range(2):
                for q in range(2):
                    K.dma(dr["p_ssd"][l, 4 * g + 2 * q:4 * g + 2 * q + 2].rearrange("h p n -> (h p) n"),
                          hsT.ap()[:, q * 128 + 64 * g:q * 128 + 64 * g + 64], r=[hsT.r()])
            for hh in range(4):
                K.dma(dr["p_gla"][l, hh], S32.ap()[32 * hh:32 * hh + 32, 64 * hh:64 * hh + 64], r=[S32.r()])
            P.barrier()
        ssd_sample(K, dr, C, l, yT, win, aB, dB, dtbB, dbg_out)
        gla_sample(K, dr, C, l, yT, wing, wgk2, bgkB)


def dram_scratch(K, name, shape):
    K.uid += 1
    h = K.nc.dram_tensor(f"scr_{name}_{K.uid}", list(shape), F32)
    return Tn(h, name)


def ssd_sample(K, dr, C, l, yT, win, aB, dB, dtbB, dbg_out):
    P = K.P
    hs, identb = C["hs"], C["identb"]
    with contextlib.ExitStack() as ph:
        cs = K.sb(ph, "cs", [NS, 768], F32)
        wB = K.sb(ph, "wB", [NS, 768], F32)
        gB = K.sb(ph, "gB", [NS, 512], F32)
        xbcs = K.sb(ph, "xbcs", [NS, 768], F32)
        acc = K.sb(ph, "acc", [NS, 768], F32)
        tmpc = K.sb(ph, "tmpc", [NS, 768], F32)
        szs = K.sb(ph, "szs", [NS, 512], F32)
        dts = K.sb(ph, "dts", [NS, 8], F32)
        dtm = K.sb(ph, "dtm", [NS, 8], F32)
        rep = K.sb(ph, "rep", [NS, 2, 8, 64], F32)
        pk = K.sb(ph, "pk", [NS, 8, 3], F32)
        Hs = K.sb(ph, "Hs", [128, 64, 64], F32)
        tmpH = K.sb(ph, "tmpH", [128, 32, 64], F32)
        xh = K.sb(ph, "xh", [128, 64], F32)
        BCh = K.sb(ph, "BCh", [128, 2, 64], F32)
        pkh = K.sb(ph, "pkh", [128, 3], F32)
        dA = K.sb(ph, "dA", [128, 1], F32)
        xdt = K.sb(ph, "xdt", [128, 64], F32)
        yh = K.sb(ph, "yh", [128, 64], F32)
        ysm = K.sb(ph, "ysm", [NS, 512], F32)
        yns = K.sb(ph, "yns", [NS, 512], BF16)
        sss = K.sb(ph, "sss", [NS, 1], F32)
        ps_a = K.ps(ph, "pss_a", [128, 512], F32)
        ps_b = K.ps(ph, "pss_b", [128, 512], F32)
        ps_d = K.ps(ph, "pss_d", [128, 512], F32)
        ps_t = K.ps(ph, "pss_t", [128, 1024], BF16)
        sx = dram_scratch(K, "sx", [NS, 512])
        sbc = dram_scratch(K, "sbc", [2, NS, 512])
        spk = dram_scratch(K, "spk", [NS, 24])
        sy = dram_scratch(K, "sy", [NS, 512])

        K.dma(gB.ap(), dr["ssd_norm_g"][l:l + 1, :].to_broadcast([NS, 512]), w=[gB.r()])
        K.dma(Hs.ap().rearrange("p a b -> p (a b)"), dr["st_ssd"][l].rearrange("b h p n -> (b h) (p n)"), w=[Hs.r()])
        for k in range(KT):
            K.mm(ps_a.ap()[0:NS, :], hs.ap()[:, k, :], win.ap()[:, k, 0:512], [hs.r(), win.r()], [ps_a.r()],
                 start=(k == 0), stop=(k == KT - 1))
        for k in range(KT):
            K.mm(ps_b.ap()[0:NS, :], hs.ap()[:, k, :], win.ap()[:, k, 512:1024], [hs.r(), win.r()], [ps_b.r()],
                 start=(k == 0), stop=(k == KT - 1))
        for k in range(KT):
            K.mm(ps_d.ap()[0:NS, 0:264], hs.ap()[:, k, :], win.ap()[:, k, 1024:1288], [hs.r(), win.r()], [ps_d.r()],
                 start=(k == 0), stop=(k == KT - 1))
        K.act(szs.ap(), ps_a.ap()[0:NS, :], AF.Silu, [ps_a.r()], [szs.r()])
        K.cp(xbcs.ap()[:, 0:512], ps_b.ap()[0:NS, :], [ps_b.r()], [xbcs.r()], eng="act")
        K.cp(xbcs.ap()[:, 512:768], ps_d.ap()[0:NS, 0:256], [ps_d.r()], [xbcs.r()], eng="act")
        K.tt(dts.ap(), ps_d.ap()[0:NS, 256:264], dtbB.ap()[0:NS, :], ALU.add, [ps_d.r(), dtbB.r()], [dts.r()])
        softplus_(K, (dts.ap(), dts.r()), (dtm.ap(), dtm.r()), None, None)
        K.dma(wB.ap(), dr["ssd_conv_w"][l, 3:4, :].to_broadcast([NS, 768]), w=[wB.r()])
        K.tt(acc.ap(), xbcs.ap(), wB.ap(), ALU.mult, [xbcs.r(), wB.r()], [acc.r()])
        for i in range(3):
            K.dma(wB.ap(), dr["ssd_conv_w"][l, i:i + 1, :].to_broadcast([NS, 768]), w=[wB.r()])
            K.dma(cs.ap(), dr["st_conv"][l][:, i, :], w=[cs.r()])
            K.tt(tmpc.ap(), cs.ap(), wB.ap(), ALU.mult, [cs.r(), wB.r()], [tmpc.r()])
            K.tt(acc.ap(), acc.ap(), tmpc.ap(), ALU.add, [acc.r(), tmpc.r()], [acc.r()])
        K.dma(wB.ap(), dr["ssd_conv_b"][l:l + 1, :].to_broadcast([NS, 768]), w=[wB.r()])
        K.tt(acc.ap(), acc.ap(), wB.ap(), ALU.add, [acc.r(), wB.r()], [acc.r()])
        K.act(acc.ap(), acc.ap(), AF.Silu, [acc.r()], [acc.r()])
        K.dma(dr["s_conv"][l][:, 0:2, :], dr["st_conv"][l][:, 1:3, :])
        K.dma(dr["s_conv"][l][:, 2, :], xbcs.ap(), r=[xbcs.r()])
        K.dma(sx.ap(), acc.ap()[:, 0:512], r=[acc.r()], w=[sx.r()])
        K.cp(rep.ap().rearrange("p t (g r) n -> p t g r n", g=2),
             bc(acc.ap()[:, 512:768].rearrange("p (t g n) -> p t g n", t=2, g=2), 3, [NS, 2, 2, 4, 64]),
             [acc.r()], [rep.r()])
        K.cp(pk.ap()[:, :, 0], dts.ap(), [dts.r()], [pk.r()])
        K.cp(pk.ap()[:, :, 1], aB.ap()[0:NS, :], [aB.r()], [pk.r()])
        K.cp(pk.ap()[:, :, 2], dB.ap()[0:NS, :], [dB.r()], [pk.r()])
        K.dma(sbc.ap().rearrange("t b x -> b t x"), rep.ap().rearrange("p t h n -> p t (h n)"), r=[rep.r()],
              w=[sbc.r()])
        K.dma(spk.ap(), pk.ap().rearrange("p h q -> p (h q)"), r=[pk.r()], w=[spk.r()])
        K.dma(xh.ap(), sx.ap().rearrange("b (h p) -> (b h) p", h=8), r=[sx.r()], w=[xh.r()])
        for t in range(2):
            K.dma(BCh.ap()[:, t, :], sbc.ap()[t].rearrange("b (h n) -> (b h) n", h=8), r=[sbc.r()],
                  w=[BCh.r()])
        K.dma(pkh.ap(), spk.ap().rearrange("b (h q) -> (b h) q", h=8), r=[spk.r()], w=[pkh.r()])
        K.act(dA.ap(), pkh.ap()[:, 0:1], AF.Exp, [pkh.r()], [dA.r()], scale=pkh.ap()[:, 1:2])
        K.ts(xdt.ap(), xh.ap(), pkh.ap()[:, 0:1], ALU.mult, [xh.r(), pkh.r()], [xdt.r()])
        K.ts(Hs.ap(), Hs.ap(), dA.ap(), ALU.mult, [Hs.r(), dA.r()], [Hs.r()])
        for hf in range(2):
            sl = slice(32 * hf, 32 * hf + 32)
            K.tt(tmpH.ap(), bc(xdt.ap()[:, sl], 2, [128, 32, 64]), bc(BCh.ap()[:, 0, :], 1, [128, 32, 64]), ALU.mult,
                 [xdt.r(), BCh.r()], [tmpH.r()])
            K.tt(Hs.ap()[:, sl, :], Hs.ap()[:, sl, :], tmpH.ap(), ALU.add, [Hs.r(), tmpH.r()], [Hs.r()])
        K.dma(dr["s_ssd"][l].rearrange("b h p n -> (b h) (p n)"), Hs.ap().rearrange("p a b -> p (a b)"), r=[Hs.r()])
        for hf in range(2):
            sl = slice(32 * hf, 32 * hf + 32)
            K.tt(tmpH.ap(), Hs.ap()[:, sl, :], bc(BCh.ap()[:, 1, :], 1, [128, 32, 64]), ALU.mult, [Hs.r(), BCh.r()],
                 [tmpH.r()])
            K.red(yh.ap()[:, sl], tmpH.ap(), [tmpH.r()], [yh.r()])
        K.stt(yh.ap(), xh.ap(), pkh.ap()[:, 2:3], yh.ap(), ALU.mult, ALU.add, [xh.r(), pkh.r(), yh.r()], [yh.r()])
        K.dma(sy.ap().rearrange("b (h p) -> (b h) p", h=8), yh.ap(), r=[yh.r()], w=[sy.r()])
        K.dma(ysm.ap(), sy.ap(), r=[sy.r()], w=[ysm.r()])
        ssd_epilogue(K, C, ysm, szs, sss, yns, NS)
        K.tt(ysm.ap(), ysm.ap(), gB.ap(), ALU.mult, [ysm.r(), gB.r()], [ysm.r()])
        K.ts(yns.ap(), ysm.ap(), sss.ap(), ALU.mult, [ysm.r(), sss.r()], [yns.r()])
        for q in range(4):
            K.tr(ps_t.ap()[:, q * NS:(q + 1) * NS], yns.ap()[:, q * 128:(q + 1) * 128], identb.ap()[0:NS, 0:NS],
                 [yns.r(), identb.r()], [ps_t.r()], inc=(q == 3))
        K.cp(yT.ap()[:, 0:4, T:T + NS], ps_t.ap()[:, 0:4 * NS].rearrange("p (t n) -> p t n", t=4), [ps_t.r()],
             xr(yT, 4, range(4)))
        P.barrier()


def gla_phase(K, dr, C, l, yT, dbg_out):
    P = K.P
    identb, maskU = C["identb"], C["maskU"]
    hc, hs = C["hc"], C["hs"]
    G0 = OFF["gq"]
    with contextlib.ExitStack() as ph:
        win = K.sb(ph, "win_gla", [128, KT, 784], BF16)
        K.dma(win.ap(), dr["w_in"][l, :, G0:G0 + 784].rearrange("(k p) n -> p k n", p=128), w=[win.r()], q="pool")
        wgk2 = K.sb(ph, "wgk2", [16, 128], BF16)
        K.dma(wgk2.ap(), dr["gla_w_gk2"][l], w=[wgk2.r()], q="pool")
        bgkB = K.sb(ph, "bgkB", [128, 128], F32)
        K.dma(bgkB.ap(), dr["gla_b_gk"][l:l + 1, :].to_broadcast([128, 128]), w=[bgkB.r()])
        gcol = K.sb(ph, "gcol", [128, 1], F32)
        for t in range(2):
            K.dma(gcol.ap()[64 * t:64 * t + 64, :], dr["gla_norm_g"][l].rearrange("(e o) -> e o", o=1), w=[gcol.r()])
        BM = K.sb(ph, "BM", [128, 256], F32)
        hm = K.sb(ph, "hm", [128, 4], F32)
        K.memset(BM.ap(), 1.0, [BM.r()], eng="pool")
        K.memset(hm.ap(), 1.0, [hm.r()], eng="pool")
        for hh in range(4):
            for (t, sl, n) in ((BM, slice(64 * hh, 64 * hh + 64), 64), (hm, slice(hh, hh + 1), 1)):
                ap = t.ap()[:, sl]
                K.P.op("pool", lambda e, ap=ap, n=n, hh=hh: e.affine_select(
                    out=ap, in_=ap, pattern=[[0, n]], compare_op=ALU.is_ge, fill=0.0, base=-32 * hh,
                    channel_multiplier=1), reads=[t.r()], writes=[t.r()])
                K.P.op("pool", lambda e, ap=ap, n=n, hh=hh: e.affine_select(
                    out=ap, in_=ap, pattern=[[0, n]], compare_op=ALU.is_gt, fill=0.0, base=32 * hh + 32,
                    channel_multiplier=-1), reads=[t.r()], writes=[t.r()])
        gla_prompt(K, dr, C, l, yT, win, wgk2, bgkB, gcol, BM, hm)
        P.barrier()
        gla_sample(K, dr, C, l, yT, win, wgk2, bgkB)


def gla_prompt(K, dr, C, l, yT, win, wgk2, bgkB, gcol, BM, hm):
    P = K.P
    identb, maskU = C["identb"], C["maskU"]
    hc = C["hc"]
    with contextlib.ExitStack() as ph:
        glo = K.sb(ph, "glo", [16, 128], BF16)
        lg = K.sb(ph, "lg", [128, 128], F32)
        lgt = K.sb(ph, "lgt", [128, 128], F32)
        Eq = K.sb(ph, "Eq", [128, 128], F32)
        Ek = K.sb(ph, "Ek", [128, 128], F32)
        Ekt = K.sb(ph, "Ekt", [128, 128], F32)
        qt = K.sb(ph, "qt", [128, 128], BF16)
        kf = K.sb(ph, "kf", [128, 128], F32)
        km = K.sb(ph, "km", [128, 4, 128], BF16)
        ktm = K.sb(ph, "ktm", [128, 128], BF16)
        vtm = K.sb(ph, "vtm", [128, 256], BF16)
        sgg = K.sb(ph, "sgg", [128, 256], F32)
        A = K.sb(ph, "A", [128, 4, 128], BF16)
        osq = K.sb(ph, "osq", [128, 256], F32)
        ms = K.sb(ph, "ms", [128, 4], F32)
        on = K.sb(ph, "on", [128, 256], F32)
        onb = K.sb(ph, "onb", [128, 256], BF16)
        tmpS = K.sb(ph, "tmpS", [128, 256], F32)
        S32 = K.sb(ph, "S32", [128, 256], F32)
        Sb = K.sb(ph, "Sb", [128, 256], BF16)
        ps_f = K.ps(ph, "psg_f", [128, 512], F32)
        ps_m = K.ps(ph, "psg_m", [128, 512], F32)
        ps_g = K.ps(ph, "psg_g", [128, 512], F32)
        ps_l = K.ps(ph, "psg_l", [128, 512], F32)
        ps_a = K.ps(ph, "psg_a", [128, 512], F32)
        ps_o = K.ps(ph, "psg_o", [128, 512], F32)
        ps_t = K.ps(ph, "psg_t", [128, 1024], BF16)
        K.memset(S32.ap(), 0.0, [S32.r()])
        K.memset(Sb.ap(), 0.0, [Sb.r()])
        for c in range(NCH):
            h = hc[c % 2]
            make_hc(K, C, l, h, c)
            for (dst, cols, M) in ((ps_f.ap()[:, 0:128], slice(0, 128), 128), (ps_f.ap()[:, 128:256], slice(128, 256), 128),
                                   (ps_f.ap()[0:16, 256:384], slice(512, 528), 16)):
                for k in range(KT):
                    K.mm(dst, win.ap()[:, k, cols], h.ap()[:, k, :], [win.r(), h.r()], [ps_f.r()],
                         start=(k == 0), stop=(k == KT - 1))
            for (dst, cols, pst) in ((ps_m.ap()[:, 0:256], slice(256, 512), ps_m), (ps_m.ap()[:, 256:384], slice(128, 256), ps_m),
                                     (ps_g.ap()[:, 0:256], slice(528, 784), ps_g)):
                for k in range(KT):
                    K.mm(dst, h.ap()[:, k, :], win.ap()[:, k, cols], [win.r(), h.r()], [pst.r()],
                         start=(k == 0), stop=(k == KT - 1))
            K.cp(glo.ap(), ps_f.ap()[0:16, 256:384], [ps_f.r()], [glo.r()], eng="act")
            K.mm(ps_l.ap()[:, 0:128], glo.ap(), wgk2.ap(), [glo.r(), wgk2.r()], [ps_l.r()])
            K.stt(lg.ap(), ps_l.ap()[:, 0:128], -1.0, bgkB.ap(), ALU.mult, ALU.subtract, [ps_l.r(), bgkB.r()], [lg.r()])
            softplus_(K, (lg.ap(), lg.r()), (lgt.ap(), lgt.r()), None, None)
            K.ts(lg.ap(), lg.ap(), -1.0 / 16.0, ALU.mult, [lg.r()], [lg.r()])
            K.mm(ps_l.ap()[:, 128:256], lg.ap(), maskU.ap(), [lg.r(), maskU.r()], [ps_l.r()])
            K.mm(ps_l.ap()[:, 256:384], maskU.ap(), lg.ap(), [lg.r(), maskU.r()], [ps_l.r()])
            K.act(Eq.ap(), ps_l.ap()[:, 128:256], AF.Exp, [ps_l.r()], [Eq.r()])
            K.act(Ek.ap(), ps_l.ap()[:, 128:256], AF.Exp, [ps_l.r()], [Ek.r()], scale=-1.0)
            K.act(Ekt.ap(), ps_l.ap()[:, 256:384], AF.Exp, [ps_l.r()], [Ekt.r()], scale=-1.0)
            K.stt(qt.ap(), ps_f.ap()[:, 0:128], 32.0 ** -0.5, Eq.ap(), ALU.mult, ALU.mult, [ps_f.r(), Eq.r()], [qt.r()])
            K.tt(kf.ap(), ps_f.ap()[:, 128:256], Ek.ap(), ALU.mult, [ps_f.r(), Ek.r()], [kf.r()])
            K.tt(km.ap(), bc(kf.ap(), 1, [128, 4, 128]), bc(hm.ap(), 2, [128, 4, 128]), ALU.mult, [kf.r(), hm.r()],
                 [km.r()])
            K.tt(ktm.ap(), ps_m.ap()[:, 256:384], Ekt.ap(), ALU.mult, [ps_m.r(), Ekt.r()], [ktm.r()])
            K.cp(vtm.ap(), ps_m.ap()[:, 0:256], [ps_m.r()], [vtm.r()], eng="act")
            K.act(sgg.ap(), ps_g.ap()[:, 0:256], AF.Silu, [ps_g.r()], [sgg.r()])
            for hh in range(4):
                K.mm(ps_a.ap()[:, hh * 128:(hh + 1) * 128], km.ap()[:, hh, :], qt.ap(), [km.r(), qt.r()], [ps_a.r()],
                     inc=(hh == 3))
            K.tt(A.ap(), ps_a.ap().rearrange("p (h i) -> p h i", h=4), bc(maskU.ap(), 1, [128, 4, 128]), ALU.mult,
                 [ps_a.r(), maskU.r()], [A.r()])
            K.mm(ps_o.ap()[:, 0:256], qt.ap(), Sb.ap(), [qt.r(), Sb.r()], [ps_o.r()], start=True, stop=False)
            for hh in range(4):
                K.mm(ps_o.ap()[:, hh * 64:(hh + 1) * 64], A.ap()[:, hh, :], vtm.ap()[:, hh * 64:(hh + 1) * 64],
                     [A.r(), vtm.r()], [ps_o.r()], start=False, stop=(hh == 3))
            K.act(osq.ap(), ps_o.ap()[:, 0:256], AF.Square, [ps_o.r()], [osq.r()])
            K.red(ms.ap(), osq.ap().rearrange("p (h e) -> p h e", h=4), [osq.r()], [ms.r()])
            K.act(ms.ap(), ms.ap(), AF.Sqrt, [ms.r()], [ms.r()], scale=1.0 / 64, bias=RMS_EPS)
            K.recip(ms.ap(), ms.ap(), [ms.r()], [ms.r()])
            K.tt(on.ap().rearrange("p (h e) -> p h e", h=4), ps_o.ap()[:, 0:256].rearrange("p (h e) -> p h e", h=4),
                 bc(ms.ap(), 2, [128, 4, 64]), ALU.mult, [ps_o.r(), ms.r()], [on.r()])
            K.tt(onb.ap(), on.ap(), sgg.ap(), ALU.mult, [on.r(), sgg.r()], [onb.r()])
            for q in range(2):
                K.tr(ps_t.ap()[:, q * 128:(q + 1) * 128], onb.ap()[:, q * 128:(q + 1) * 128], identb.ap(),
                     [onb.r(), identb.r()], [ps_t.r()], inc=(q == 1))
            K.ts(yT.ap()[:, 6:8, c * 128:(c + 1) * 128], ps_t.ap()[:, 0:256].rearrange("p (t n) -> p t n", t=2),
                 gcol.ap(), ALU.mult, [ps_t.r(), gcol.r()], xr(yT, c // 4, range(6, 8)))
            K.mm(ps_o.ap()[:, 256:512], ktm.ap(), vtm.ap(), [ktm.r(), vtm.r()], [ps_o.r()])
            K.tt(tmpS.ap(), ps_o.ap()[:, 256:512], BM.ap(), ALU.mult, [ps_o.r(), BM.r()], [tmpS.r()])
            K.tt(S32.ap(), S32.ap(), tmpS.ap(), ALU.add, [S32.r(), tmpS.r()], [S32.r()])
            K.ts(S32.ap(), S32.ap(), Eq.ap()[:, 127:128], ALU.mult, [S32.r(), Eq.r()], [S32.r()])
            K.cp(Sb.ap(), S32.ap(), [S32.r()], [Sb.r()], eng="act")
        for hh in range(4):
            K.dma(dr["p_gla"][l, hh], S32.ap()[32 * hh:32 * hh + 32, 64 * hh:64 * hh + 64], r=[S32.r()])
        P.barrier()


def gla_sample(K, dr, C, l, yT, win, wgk2, bgkB):
    P = K.P
    hs, identb = C["hs"], C["identb"]
    with contextlib.ExitStack() as ph:
        glo = K.sb(ph, "glos", [16, NS], BF16)
        lg = K.sb(ph, "lgs", [NS, 128], F32)
        lgt = K.sb(ph, "lgts", [NS, 128], F32)
        pk = K.sb(ph, "pkg", [NS, 4, 160], F32)
        sgg = K.sb(ph, "sggs", [NS, 256], F32)
        gB = K.sb(ph, "gBg", [64, 64], F32)
        S = K.sb(ph, "Sg", [64, 32, 64], F32)
        tmp = K.sb(ph, "tmpg", [64, 32, 64], F32)
        pkh = K.sb(ph, "pkhg", [64, 160], F32)
        o = K.sb(ph, "og", [64, 64], F32)
        junk = K.sb(ph, "junkg", [64, 64], F32)
        ss = K.sb(ph, "ssg", [64, 1], F32)
        otm = K.sb(ph, "otm", [NS, 256], F32)
        otb = K.sb(ph, "otb", [NS, 256], BF16)
        ps_a = K.ps(ph, "psgs_a", [128, 512], F32)
        ps_b = K.ps(ph, "psgs_b", [128, 512], F32)
        ps_c = K.ps(ph, "psgs_c", [128, 512], F32)
        ps_t = K.ps(ph, "psgs_t", [128, 1024], BF16)
        spk = dram_scratch(K, "gpk", [NS, 640])
        so = dram_scratch(K, "go", [NS, 256])
        K.dma(gB.ap(), dr["gla_norm_g"][l:l + 1, :].to_broadcast([64, 64]), w=[gB.r()])
        K.dma(S.ap().rearrange("p d e -> p (d e)"), dr["st_gla"][l].rearrange("b h d e -> (b h) (d e)"), w=[S.r()])
        for k in range(KT):
            K.mm(ps_a.ap()[0:NS, :], hs.ap()[:, k, :], win.ap()[:, k, 0:512], [hs.r(), win.r()], [ps_a.r()],
                 start=(k == 0), stop=(k == KT - 1))
        for k in range(KT):
            K.mm(ps_b.ap()[0:NS, 0:256], hs.ap()[:, k, :], win.ap()[:, k, 528:784], [hs.r(), win.r()], [ps_b.r()],
                 start=(k == 0), stop=(k == KT - 1))
        for k in range(KT):
            K.mm(ps_c.ap()[0:16, 0:NS], win.ap()[:, k, 512:528], hs.ap()[:, k, :], [hs.r(), win.r()], [ps_c.r()],
                 start=(k == 0), stop=(k == KT - 1))
        K.cp(glo.ap(), ps_c.ap()[0:16, 0:NS], [ps_c.r()], [glo.r()], eng="act")
        K.mm(ps_c.ap()[0:NS, 128:256], glo.ap(), wgk2.ap(), [glo.r(), wgk2.r()], [ps_c.r()])
        K.stt(lg.ap(), ps_c.ap()[0:NS, 128:256], -1.0, bgkB.ap()[0:NS, :], ALU.mult, ALU.subtract,
              [ps_c.r(), bgkB.r()], [lg.r()])
        softplus_(K, (lg.ap(), lg.r()), (lgt.ap(), lgt.r()), None, None)
        K.act(lg.ap(), lg.ap(), AF.Exp, [lg.r()], [lg.r()], scale=-1.0 / 16.0)
        K.act(sgg.ap(), ps_b.ap()[0:NS, 0:256], AF.Silu, [ps_b.r()], [sgg.r()])
        K.ts(pk.ap()[:, :, 0:32], ps_a.ap()[0:NS, 0:128].rearrange("p (h d) -> p h d", h=4), 32.0 ** -0.5, ALU.mult,
             [ps_a.r()], [pk.r()])
        K.cp(pk.ap()[:, :, 32:64], ps_a.ap()[0:NS, 128:256].rearrange("p (h d) -> p h d", h=4), [ps_a.r()], [pk.r()])
        K.cp(pk.ap()[:, :, 64:96], lg.ap().rearrange("p (h d) -> p h d", h=4), [lg.r()], [pk.r()])
        K.cp(pk.ap()[:, :, 96:160], ps_a.ap()[0:NS, 256:512].rearrange("p (h e) -> p h e", h=4), [ps_a.r()], [pk.r()])
        K.dma(spk.ap(), pk.ap().rearrange("p h x -> p (h x)"), r=[pk.r()], w=[spk.r()])
        K.dma(pkh.ap(), spk.ap().rearrange("b (h x) -> (b h) x", h=4), r=[spk.r()], w=[pkh.r()])
        qh, kh, eh, vh = pkh.ap()[:, 0:32], pkh.ap()[:, 32:64], pkh.ap()[:, 64:96], pkh.ap()[:, 96:160]
        K.tt(S.ap(), S.ap(), bc(eh, 2, [64, 32, 64]), ALU.mult, [S.r(), pkh.r()], [S.r()])
        K.tt(tmp.ap(), bc(kh, 2, [64, 32, 64]), bc(vh, 1, [64, 32, 64]), ALU.mult, [pkh.r()], [tmp.r()])
        K.tt(S.ap(), S.ap(), tmp.ap(), ALU.add, [S.r(), tmp.r()], [S.r()])
        K.dma(dr["s_gla"][l].rearrange("b h d e -> (b h) (d e)"), S.ap().rearrange("p d e -> p (d e)"), r=[S.r()])
        K.tt(tmp.ap(), S.ap(), bc(qh, 2, [64, 32, 64]), ALU.mult, [S.r(), pkh.r()], [tmp.r()])
        K.red(o.ap(), tmp.ap().rearrange("p d e -> p e d"), [tmp.r()], [o.r()])
        K.act(junk.ap(), o.ap(), AF.Square, [o.r()], [junk.r(), ss.r()], accum_out=ss.ap())
        K.act(ss.ap(), ss.ap(), AF.Sqrt, [ss.r()], [ss.r()], scale=1.0 / 64, bias=RMS_EPS)
        K.recip(ss.ap(), ss.ap(), [ss.r()], [ss.r()])
        K.stt(o.ap(), o.ap(), ss.ap(), gB.ap(), ALU.mult, ALU.mult, [o.r(), ss.r(), gB.r()], [o.r()])
        K.dma(so.ap().rearrange("b (h e) -> (b h) e", h=4), o.ap(), r=[o.r()], w=[so.r()])
        K.dma(otm.ap(), so.ap(), r=[so.r()], w=[otm.r()])
        K.tt(otb.ap(), otm.ap(), sgg.ap(), ALU.mult, [otm.r(), sgg.r()], [otb.r()])
        for q in range(2):
            K.tr(ps_t.ap()[:, q * NS:(q + 1) * NS], otb.ap()[:, q * 128:(q + 1) * 128], identb.ap()[0:NS, 0:NS],
                 [otb.r(), identb.r()], [ps_t.r()], inc=(q == 1))
        K.cp(yT.ap()[:, 6:8, T:T + NS], ps_t.ap()[:, 0:2 * NS].rearrange("p (t n) -> p t n", t=2), [ps_t.r()],
             xr(yT, 4, range(6, 8)))
        P.barrier()


C0 = float(np.exp(-0.5))


def rwkv_prep(K, C, pc, LW, N, rw, prev, B, pl, pg, pn, ee="dve"):
    blk64 = C["blk64"]
    MX, LI = B["MX"], B["LI"]
    mxa = MX.ap()[:, :, 0:N]
    K.tt(mxa, prev, rw, ALU.subtract, B["_rw_res"], [MX.r()], eng=ee)
    yield
    K.tt(mxa, mxa, bc(pc["mu"].ap(), 2, [128, 7, N]), ALU.mult, [MX.r(), pc["mu"].r()], [MX.r()], eng=ee)
    yield
    K.tt(mxa, mxa, rw, ALU.add, [MX.r()] + B["_rw_res"], [MX.r()], eng=ee)
    yield
    r, k, v = (MX.ap()[:, 0:2, 0:N], MX.ap()[:, 2:4, 0:N], MX.ap()[:, 4:6, 0:N])
    lia = LI.ap()[:, 0:N]
    lif = B["t1"].ap()[:, 0, 0:N]
    K.act(lif, MX.ap()[:, 6, 0:N], AF.Exp, [MX.r(), pc["lisc"].r()], [B["t1"].r()], scale=pc["lisc"].ap())
    yield
    sigmoid_chain(K, lif, [B["t1"].r()])
    yield
    K.ts(lia[0:32], lif[0:32], 2.0, ALU.mult, [B["t1"].r()], [LI.r()], s2=-1.0, op1=ALU.add)
    K.cp(lia[32:64], MX.ap()[32:64, 6, 0:N], [MX.r()], [LI.r()], eng="act")
    K.cp(lia[64:128], lif[64:128], [B["t1"].r()], [LI.r()])
    yield
    for t in range(2):
        cs = slice(t * 128, (t + 1) * 128)
        K.mm(pl.ap()[:, t * N:(t + 1) * N], LW.ap()[0:32, cs], lia[0:32], [LW.r(), LI.r()], [pl.r()], self_wait=True)
        K.mm(pl.ap()[:, (2 + t) * N:(3 + t) * N], LW.ap()[32:64, cs], lia[32:64], [LW.r(), LI.r()], [pl.r()],
             self_wait=True)
        K.mm(pg.ap()[:, t * N:(t + 1) * N], LW.ap()[64:128, cs], lia[64:128], [LW.r(), LI.r()], [pg.r()],
             self_wait=True)
    g = lambda n: B[n].ap()[:, :, 0:N]
    for t in range(2):
        K.act(B["sig"].ap()[:, t, 0:N], pl.ap()[:, t * N:(t + 1) * N], AF.Exp, [pl.r(), pc["nw0"].r()],
              [B["sig"].r()], scale=-1.0, bias=pc["nw0"].ap()[:, t:t + 1])
        K.act(B["aic"].ap()[:, t, 0:N], pl.ap()[:, (2 + t) * N:(3 + t) * N], AF.Exp, [pl.r(), pc["na0"].r()],
              [B["aic"].r()], scale=-1.0, bias=pc["na0"].ap()[:, t:t + 1])
    sigmoid_chain(K, g("sig"), [B["sig"].r()])
    sigmoid_chain(K, g("aic"), [B["aic"].r()])
    for t in range(0):
        pass
    K.cp(g("gate"), pg.ap()[:, 0:2 * N].rearrange("p (t n) -> p t n", t=2), [pg.r()], [B["gate"].r()], eng="act")
    yield
    K.tt(g("kk"), k, bc(pc["k_k"].ap(), 2, [128, 2, N]), ALU.mult, [MX.r(), pc["k_k"].r()], [B["kk"].r()], eng=ee)
    yield
    K.tt(g("t1"), g("kk"), g("kk"), ALU.mult, [B["kk"].r()], [B["t1"].r()], eng=ee)
    yield
    for t in range(2):
        K.mm(pn.ap()[:, t * N:(t + 1) * N], blk64.ap(), B["t1"].ap()[:, t, 0:N], [blk64.r(), B["t1"].r()], [pn.r()])
    K.act(g("t1"), pn.ap()[:, 0:2 * N].rearrange("p (t n) -> p t n", t=2), AF.Ln, [pn.r()], [B["t1"].r()],
          bias=1e-12)
    K.act(g("t1"), g("t1"), AF.Exp, [B["t1"].r()], [B["t1"].r()], scale=-0.5)
    yield
    K.tt(g("kk"), g("kk"), g("t1"), ALU.mult, [B["kk"].r(), B["t1"].r()], [B["kk"].r()], eng=ee)
    yield
    yield
    K.tt(g("t1"), g("aic"), bc(pc["k_a"].ap(), 2, [128, 2, N]), ALU.mult, [B["aic"].r(), pc["k_a"].r()], [B["t1"].r()], eng=ee)
    yield
    K.tt(g("t1"), g("t1"), bc(pc["omka"].ap(), 2, [128, 2, N]), ALU.add, [B["t1"].r(), pc["omka"].r()], [B["t1"].r()], eng=ee)
    yield
    K.tt(g("kp"), k, g("t1"), ALU.mult, [MX.r(), B["t1"].r()], [B["kp"].r()], eng=ee)
    yield
    K.tt(g("t1"), r, g("kp"), ALU.mult, [MX.r(), B["kp"].r()], [B["t1"].r()], eng=ee)
    yield
    K.tt(g("t1"), g("t1"), bc(pc["r_k"].ap(), 2, [128, 2, N]), ALU.mult, [B["t1"].r(), pc["r_k"].r()], [B["t1"].r()], eng=ee)
    yield
    for t in range(2):
        K.mm(pn.ap()[:, t * N:(t + 1) * N], blk64.ap(), B["t1"].ap()[:, t, 0:N], [blk64.r(), B["t1"].r()], [pn.r()])
    K.tt(g("bonus"), pn.ap()[:, 0:2 * N].rearrange("p (t n) -> p t n", t=2), v, ALU.mult, [pn.r(), MX.r()],
         [B["bonus"].r()])
    B['_rkv'] = (r, k, v)
    yield


def rwkv_params(K, dr, l, ph):
    pc = {}
    mu = K.sb(ph, "mu", [128, 7], F32)
    K.dma(mu.ap(), dr["rwkv_mu"][l].rearrange("(t p) -> p t", p=128), w=[mu.r()])
    pc["mu"] = mu
    for n, src in (("w0", dr["rwkv_w0"][l]), ("a0", dr["rwkv_a0"][l]), ("k_k", dr["rwkv_k_k"][l]),
                   ("k_a", dr["rwkv_k_a"][l]), ("r_k", dr["rwkv_r_k"][l].rearrange("h n -> (h n)")),
                   ("ln_g", dr["rwkv_ln_g"][l]), ("ln_b", dr["rwkv_ln_b"][l])):
        t = K.sb(ph, "pc_" + n, [128, 2], F32)
        K.dma(t.ap(), src.rearrange("(t p) -> p t", p=128), w=[t.r()])
        pc[n] = t
    for n in ("w0", "a0"):
        t = K.sb(ph, "pc_n" + n, [128, 2], F32)
        K.ts(t.ap(), pc[n].ap(), -1.0, ALU.mult, [pc[n].r()], [t.r()])
        pc["n" + n] = t
    lisc = K.sb(ph, "lisc", [128, 1], F32)
    K.memset(lisc.ap()[0:32], -2.0, [lisc.r()])
    K.memset(lisc.ap()[32:64], 0.0, [lisc.r()])
    K.memset(lisc.ap()[64:128], -1.0, [lisc.r()])
    pc["lisc"] = lisc
    omka = K.sb(ph, "omka", [128, 2], F32)
    K.ts(omka.ap(), pc["k_a"].ap(), -1.0, ALU.mult, [pc["k_a"].r()], [omka.r()], s2=1.0, op1=ALU.add)
    pc["omka"] = omka
    LW = K.sb(ph, "LW", [128, 256], BF16)
    K.dma(LW.ap()[0:32, :], dr["rwkv_w2"][l], w=[LW.r()], q="pool")
    K.dma(LW.ap()[32:64, :], dr["rwkv_a2"][l], w=[LW.r()], q="pool")
    K.dma(LW.ap()[64:128, :], dr["rwkv_g2"][l], w=[LW.r()], q="pool")
    return pc, LW


def rwkv_epilogue(K, C, pc, B, N, pT, ydst, yres):
    for t in range(2):
        K.act(B["t1"].ap()[:, t, 0:N], pT.ap()[:, t * N:(t + 1) * N], AF.Identity, [pT.r(), pc["ln_g"].r(), pc["ln_b"].r()],
              [B["t1"].r()], scale=pc["ln_g"].ap()[:, t:t + 1], bias=pc["ln_b"].ap()[:, t:t + 1])
    g = lambda n: B[n].ap()[:, :, 0:N]
    K.tt(g("t1"), g("t1"), g("bonus"), ALU.add, [B["t1"].r(), B["bonus"].r()], [B["t1"].r()])
    K.tt(ydst, g("t1"), g("gate"), ALU.mult, [B["t1"].r(), B["gate"].r()], yres)


def groupnorm64(K, o_ap, n, G, scr, res_in, out_ap, out_res):
    mean, xc, sq, var = scr
    K.red(mean.ap()[0:n, 0:G], o_ap, res_in, [mean.r()])
    K.ts(mean.ap()[0:n, 0:G], mean.ap()[0:n, 0:G], 1.0 / 64, ALU.mult, [mean.r()], [mean.r()])
    xca = xc.ap()[0:n, 0:G * 64].rearrange("p (g e) -> p g e", g=G)
    K.tt(xca, o_ap, bc(mean.ap()[0:n, 0:G], 2, [n, G, 64]), ALU.subtract, res_in + [mean.r()], [xc.r()])
    sqa = sq.ap()[0:n, 0:G * 64].rearrange("p (g e) -> p g e", g=G)
    K.tt(sqa, xca, xca, ALU.mult, [xc.r()], [sq.r()])
    K.red(var.ap()[0:n, 0:G], sqa, [sq.r()], [var.r()])
    rsqrt_(K, var.ap()[0:n, 0:G], [var.r()], 1.0 / 64, RWKV_GN_EPS)
    K.tt(out_ap, xca, bc(var.ap()[0:n, 0:G], 2, [n, G, 64]), ALU.mult, [xc.r(), var.r()], out_res)


def rwkv_phase(K, dr, C, l, yT, dbg_out):
    P = K.P
    R0 = OFF["rw"]
    with contextlib.ExitStack() as ph:
        win = K.sb(ph, "win_rwkv", [128, KT, 896], BF16)
        K.dma(win.ap(), dr["w_in"][l, :, R0:R0 + 896].rearrange("(k p) n -> p k n", p=128), w=[win.r()], q="pool")
        pc, LW = rwkv_params(K, dr, l, ph)
        import os
        if os.environ.get("SKIP_RWKV_PROMPT") != "1":
            rwkv_prompt(K, dr, C, l, yT, win, pc, LW, dbg_out)
        P.barrier()
        if os.environ.get("SKIP_RWKV_SAMPLE") != "1":
            rwkv_sample(K, dr, C, l, yT, win, pc, LW, dbg_out)


def interleave(gens, ratio=None):
    gens = [g for g in gens if g is not None]
    ratio = ratio or [1] * len(gens)
    live = list(zip(gens, ratio))
    while live:
        for item in list(live):
            g, n = item
            for _ in range(n):
                try:
                    next(g)
                except StopIteration:
                    live.remove(item)
                    break


def interleave_gen(gens, ratio):
    live = list(zip(gens, ratio))
    while live:
        for item in list(live):
            g, n = item
            for _ in range(n):
                try:
                    next(g)
                except StopIteration:
                    live.remove(item)
                    break
                yield


def rwkv_prompt(K, dr, C, l, yT, win, pc, LW, dbg_out):
    P = K.P
    identb, identf, maskU, maskSU, maskSL, blk64 = (C[k] for k in ["identb", "identf", "maskU", "maskSU", "maskSL", "blk64"])
    hc = C["hc"]
    N = 128
    with contextlib.ExitStack() as ph:
        f3 = lambda n: K.sb(ph, n, [128, 2, N], F32)
        Bs = []
        for i in range(2):
            B = {n: f3(f"rb{i}_" + n) for n in ["sig", "aic", "gate", "kk", "t1", "kp", "bonus", "cs", "e1", "e2", "bb"]}
            Bs.append(B)
        MX = K.sb(ph, "MX", [128, 7, N], F32)
        LI = K.sb(ph, "LI", [128, N], BF16)
        for B in Bs:
            B["MX"], B["LI"] = MX, LI
        RW = [K.sb(ph, f"RW{i}", [128, 7, N + 1], F32) for i in range(2)]
        ones_r = K.sb(ph, "ones_r", [128, N], F32)
        bcol = K.sb(ph, "bcol", [128, 2], F32)
        MK2 = K.sb(ph, "MK2", [128, 2, N], F32)
        ARs = [K.sb(ph, f"AR{i}", [128, 2, 2, N], BF16) for i in range(2)]
        BKs = [K.sb(ph, f"BK{i}", [128, 2, 2, N], BF16) for i in range(2)]
        FH = K.sb(ph, "FH", [128, 3, 2, N], BF16)
        TMs = [K.sb(ph, f"TM{i}", [128, 4, 2, N], BF16) for i in range(2)]
        t2 = f3("rb_t2")
        A1 = K.sb(ph, "A1", [128, 4, 2, N], BF16)
        A2 = K.sb(ph, "A2", [128, 4, 2, N], BF16)
        Lb = [K.sb(ph, f"Lb{i}", [128, 4, N], BF16) for i in range(2)]
        Nb = [K.sb(ph, f"Nb{i}", [128, 4, N], BF16) for i in range(2)]
        X32 = K.sb(ph, "X32", [128, 4, 2, 64], F32)
        Xb = K.sb(ph, "Xb", [128, 4, 2, 64], BF16)
        Apf = K.sb(ph, "Apf", [128, 2, N], BF16)
        XAc = K.sb(ph, "XAc", [128, 256], BF16)
        Utm = K.sb(ph, "Utm", [128, 4, 64], BF16)
        ST32 = K.sb(ph, "ST32", [128, 2, N], F32)
        STb = K.sb(ph, "STb", [128, 2, N], BF16)
        tmpS = K.sb(ph, "tmpSr", [128, 2, N], F32)
        gn = (K.sb(ph, "gn_mean", [128, 4], F32), K.sb(ph, "gn_xc", [128, 256], F32),
              K.sb(ph, "gn_sq", [128, 256], F32), K.sb(ph, "gn_var", [128, 4], F32))
        onb = K.sb(ph, "onbr", [128, 256], BF16)
        stT = K.sb(ph, "stT", [128, 2, N], F32)
        pI = K.ps(ph, "pr_I", [128, 512], F32)
        pM = K.ps(ph, "pr_M", [128, 512], F32)
        pT = K.ps(ph, "pr_T", [128, 1024], BF16)
        pT2 = pT
        pAT = K.ps(ph, "pr_AT", [128, 1024], F32)
        pL = K.ps(ph, "pr_L", [128, 512], F32)
        pL2 = K.ps(ph, "pr_L2", [128, 512], F32)
        pX = K.ps(ph, "pr_X", [128, 512], F32)
        pO = pX

        K.memset(ones_r.ap(), 1.0, [ones_r.r()])
        K.cp(MK2.ap()[:, 0, :], maskSU.ap(), [maskSU.r()], [MK2.r()])
        K.cp(MK2.ap()[:, 1, :], maskU.ap(), [maskU.r()], [MK2.r()])
        K.memset(ST32.ap(), 0.0, [ST32.r()])
        K.memset(STb.ap(), 0.0, [STb.r()])
        K.memset(RW[0].ap()[:, :, 0:1], 0.0, [RW[0].r()])

        import os
        PE1 = os.environ.get("RWKV_S1_ENG", "dve")

        def s1(c):
            B, AR, BK, TM = Bs[c % 2], ARs[c % 2], BKs[c % 2], TMs[c % 2]
            g = lambda n: B[n].ap()
            h = hc[c % 2]
            make_hc(K, C, l, h, c)
            yield
            rw = RW[c % 2]
            for (t0, t1) in ((0, 4), (4, 7)):
                for t in range(t0, t1):
                    for k in range(KT):
                        K.mm(pI.ap()[:, (t - t0) * N:(t - t0 + 1) * N], win.ap()[:, k, t * N:(t + 1) * N], h.ap()[:, k, :],
                             [win.r(), h.r()], [pI.r()], start=(k == 0), stop=(k == KT - 1))
                    yield
                K.cp(rw.ap()[:, t0:t1, 1:N + 1], pI.ap()[:, 0:(t1 - t0) * N].rearrange("p (t n) -> p t n", t=t1 - t0),
                     [pI.r()], [rw.r()], eng="act")
                yield
            if c + 1 < NCH:
                K.cp(RW[(c + 1) % 2].ap()[:, :, 0:1], rw.ap()[:, :, N:N + 1], [rw.r()], [RW[(c + 1) % 2].r()])
            B["_rw_res"] = [rw.r()]
            yield from rwkv_prep(K, C, pc, LW, N, rw.ap()[:, :, 1:N + 1], rw.ap()[:, :, 0:N], B, pM, pI, pI, ee=PE1)
            r, k_, v = B["_rkv"]
            MXr = MX.r()
            for t in range(2):
                K.P.op("dve", lambda e, t=t, B=B: e.tensor_tensor_scan(out=B["cs"].ap()[:, t, :], data0=ones_r.ap(),
                                                                        data1=B["sig"].ap()[:, t, :], initial=0.0,
                                                                        op0=ALU.mult, op1=ALU.add),
                       reads=[ones_r.r(), B["sig"].r()], writes=[B["cs"].r()])
            yield
            K.act(g("e1"), g("cs"), AF.Exp, [B["cs"].r()], [B["e1"].r()], scale=-C0)
            yield
            K.act(g("e2"), g("cs"), AF.Exp, [B["cs"].r()], [B["e2"].r()], scale=C0)
            yield
            K.tt(AR.ap()[:, :, 1, :], r, g("e1"), ALU.mult, [MXr, B["e1"].r()], [AR.r()], eng=PE1)
            yield
            K.tt(g("bb"), g("kk"), g("aic"), ALU.mult, [B["kk"].r(), B["aic"].r()], [B["bb"].r()], eng=PE1)
            yield
            K.tt(BK.ap()[:, :, 0, :], g("bb"), g("e2"), ALU.mult, [B["bb"].r(), B["e2"].r()], [BK.r()], eng=PE1)
            yield
            K.tt(BK.ap()[:, :, 1, :], g("kp"), g("e2"), ALU.mult, [B["kp"].r(), B["e2"].r()], [BK.r()], eng=PE1)
            yield
            K.tt(g("t1"), g("cs"), g("sig"), ALU.subtract, [B["cs"].r(), B["sig"].r()], [B["t1"].r()], eng=PE1)
            yield
            K.act(g("e2"), g("t1"), AF.Exp, [B["t1"].r()], [B["e2"].r()], scale=-C0)
            yield
            K.stt(AR.ap()[:, :, 0, :], g("kk"), -1.0, g("e2"), ALU.mult, ALU.mult, [B["kk"].r(), B["e2"].r()], [AR.r()])
            yield
            K.ts(bcol.ap(), B["cs"].ap()[:, :, N - 1], -C0, ALU.mult, [B["cs"].r()], [bcol.r()])
            yield
            for t in range(2):
                K.act(B["e2"].ap()[:, t, :], B["cs"].ap()[:, t, :], AF.Exp, [B["cs"].r(), bcol.r()], [B["e2"].r()],
                      scale=C0, bias=bcol.ap()[:, t:t + 1])
            yield
            K.tt(FH.ap()[:, 0], g("bb"), g("e2"), ALU.mult, [B["bb"].r(), B["e2"].r()], [FH.r()], eng=PE1)
            yield
            K.tt(FH.ap()[:, 1], g("kp"), g("e2"), ALU.mult, [B["kp"].r(), B["e2"].r()], [FH.r()], eng=PE1)
            yield
            K.cp(FH.ap()[:, 2], v, [MXr], [FH.r()], eng="act")
            yield
            for q in range(4):
                for t in range(2):
                    src = FH.ap()[:, q, t, :] if q < 3 else AR.ap()[:, t, 0, :]
                    K.tr(pT.ap()[:, (q * 2 + t) * N:(q * 2 + t + 1) * N], src, identb.ap(),
                         [FH.r(), AR.r(), identb.r()], [pT.r()], inc=(t == 1))
                yield
            K.cp(TM.ap().rearrange("p q t n -> p (q t n)"), pT.ap(), [pT.r()], [TM.r()], eng="act")
            yield

        def s2(c):
            B, AR, BK, TM = Bs[c % 2], ARs[c % 2], BKs[c % 2], TMs[c % 2]
            mk = bc(MK2.ap(), 1, [128, 4, 2, N])
            for which, Adst in ((0, A1), (1, A2)):
                for hd in range(4):
                    t, o = hd // 2, 64 * (hd % 2)
                    sl = slice(o, o + 64)
                    arf = AR.ap()[sl, t].rearrange("p a n -> p (a n)")
                    K.mm(pAT.ap()[:, hd * 256:(hd + 1) * 256], BK.ap()[sl, t, which, :], arf, [BK.r(), AR.r()], [pAT.r()],
                         self_wait=True)
                yield
                K.tt(Adst.ap(), pAT.ap().rearrange("p (h a n) -> p h a n", h=4, a=2), mk, ALU.mult, [pAT.r(), MK2.r()],
                     [Adst.r()])
                yield
            for hd in range(4):
                t, o = hd // 2, 64 * (hd % 2)
                sl = slice(o, o + 64)
                K.mm(pL.ap()[:, hd * N:(hd + 1) * N], AR.ap()[sl, t, 0, :], BK.ap()[sl, t, 0, :], [BK.r(), AR.r()],
                     [pL.r()], self_wait=True)
            yield
            K.tt(Lb[0].ap(), pL.ap().rearrange("p (h n) -> p h n", h=4), bc(maskSL.ap(), 1, [128, 4, N]), ALU.mult,
                 [pL.r(), maskSL.r()], [Lb[0].r()])
            yield
            vtm = TM.ap()[:, 2].rearrange("p t n -> p (t n)")
            for hd in range(4):
                K.mm(pX.ap()[:, hd * 64:(hd + 1) * 64], A2.ap()[:, hd, 0, :], vtm[:, hd * 64:(hd + 1) * 64],
                     [A2.r(), TM.r()], [pX.r()], inc=(hd == 3))
            yield
            K.cp(X32.ap()[:, :, 0, :], TM.ap()[:, 3].rearrange("p t (hh k) -> p (t hh) k", hh=2), [TM.r()], [X32.r()])
            yield
            K.cp(X32.ap()[:, :, 1, :], pX.ap()[:, 0:256].rearrange("p (h v) -> p h v", h=4), [pX.r()], [X32.r()],
                 eng="act")
            yield
            K.cp(Xb.ap(), X32.ap(), [X32.r()], [Xb.r()], eng="act")
            yield
            for i in range(7):
                if i == 0:
                    nref = lambda hd: A1.ap()[:, hd, 0, :]
                    nres = A1.r()
                    lcur = Lb[0]
                else:
                    nprev_ref, nprev_res, lprev = nref, nres, lcur
                    nnew, lnew = Nb[i % 2], Lb[i % 2]
                    for hd in range(4):
                        K.mm(pL.ap()[:, hd * N:(hd + 1) * N], lprev.ap()[:, hd, :], nprev_ref(hd), [lprev.r(), nprev_res],
                             [pL.r()], inc=(hd == 3))
                    yield
                    if i < 6:
                        for hd in range(4):
                            K.mm(pL2.ap()[:, hd * N:(hd + 1) * N], nprev_ref(hd), lprev.ap()[:, hd, :],
                                 [lprev.r(), nprev_res], [pL2.r()], inc=(hd == 3))
                        yield
                    K.cp(nnew.ap(), pL.ap().rearrange("p (h n) -> p h n", h=4), [pL.r()], [nnew.r()], eng="act")
                    yield
                    if i < 6:
                        K.cp(lnew.ap(), pL2.ap().rearrange("p (h n) -> p h n", h=4), [pL2.r()], [lnew.r()])
                        yield
                    nref = lambda hd, nnew=nnew: nnew.ap()[:, hd, :]
                    nres = nnew.r()
                    lcur = lnew
                for hd in range(4):
                    K.mm(pX.ap()[:, hd * N:(hd + 1) * N], nref(hd), Xb.ap()[:, hd].rearrange("p a k -> p (a k)"),
                         [nres, Xb.r()], [pX.r()], inc=(hd == 3))
                yield
                K.tt(X32.ap(), X32.ap(), pX.ap().rearrange("p (h a k) -> p h a k", h=4, a=2), ALU.add,
                     [X32.r(), pX.r()], [X32.r()])
                yield
                K.cp(Xb.ap(), X32.ap(), [X32.r()], [Xb.r()], eng="act")
                yield
            K.cp(XAc.ap().rearrange("p (h k) -> p h k", h=4), X32.ap()[:, :, 0, :], [X32.r()], [XAc.r()])
            yield
            for t in range(2):
                K.tr(pT2.ap()[:, t * N:(t + 1) * N], XAc.ap()[:, t * N:(t + 1) * N], identb.ap(), [XAc.r(), identb.r()],
                     [pT2.r()], inc=(t == 1))
            yield
            K.cp(Apf.ap(), pT2.ap()[:, 0:2 * N].rearrange("p (t n) -> p t n", t=2), [pT2.r()], [Apf.r()], eng="act")
            yield
            for t in range(2):
                K.mm(pX.ap()[:, t * N:(t + 1) * N], Apf.ap()[:, t, :], STb.ap()[:, t, :], [Apf.r(), STb.r()], [pX.r()],
                     inc=(t == 1))
            yield
            K.tt(Utm.ap(), pX.ap()[:, 0:256].rearrange("p (h v) -> p h v", h=4), X32.ap()[:, :, 1, :], ALU.add,
                 [pX.r(), X32.r()], [Utm.r()])
            yield
            for t in range(2):
                K.mm(pO.ap()[:, t * N:(t + 1) * N], AR.ap()[:, t, 1, :], STb.ap()[:, t, :], [AR.r(), STb.r()], [pO.r()],
                     start=(t == 0), stop=False, sgc=True)
            for hd in range(4):
                K.mm(pO.ap()[:, hd * 64:(hd + 1) * 64], A1.ap()[:, hd, 1, :], Utm.ap()[:, hd, :], [A1.r(), Utm.r()],
                     [pO.r()], start=False, stop=False, sgc=True)
                K.mm(pO.ap()[:, hd * 64:(hd + 1) * 64], A2.ap()[:, hd, 1, :], vtm[:, hd * 64:(hd + 1) * 64],
                     [A2.r(), TM.r()], [pO.r()], start=False, stop=False, sgc=True)
            for t in range(2):
                K.mm(pO.ap()[:, 256 + t * N:256 + (t + 1) * N], TM.ap()[:, 0, t, :],
                     Utm.ap()[:, 2 * t:2 * t + 2, :].rearrange("p h v -> p (h v)"), [TM.r(), Utm.r()], [pO.r()],
                     start=False, stop=False, sgc=True)
                K.mm(pO.ap()[:, 256 + t * N:256 + (t + 1) * N], TM.ap()[:, 1, t, :], vtm[:, t * N:(t + 1) * N],
                     [TM.r()], [pO.r()], start=False, stop=(t == 1), inc=(t == 1), sgc=True)
            yield
            K.tt(tmpS.ap(), pO.ap()[:, 256:512].rearrange("p (t n) -> p t n", t=2), bc(blk64.ap(), 1, [128, 2, N]),
                 ALU.mult, [pO.r(), blk64.r()], [tmpS.r()])
            yield
            K.tt(ST32.ap(), ST32.ap(), bc(B["e1"].ap()[:, :, N - 1], 2, [128, 2, N]), ALU.mult, [ST32.r(), B["e1"].r()],
                 [ST32.r()])
            yield
            K.tt(ST32.ap(), ST32.ap(), tmpS.ap(), ALU.add, [ST32.r(), tmpS.r()], [ST32.r()])
            yield
            K.cp(STb.ap(), ST32.ap(), [ST32.r()], [STb.r()], eng="act")
            yield
            groupnorm64(K, pO.ap()[:, 0:256].rearrange("p (h v) -> p h v", h=4), 128, 4, gn, [pO.r()],
                        onb.ap().rearrange("p (h v) -> p h v", h=4), [onb.r()])
            yield
            for t in range(2):
                K.tr(pT2.ap()[:, t * N:(t + 1) * N], onb.ap()[:, t * N:(t + 1) * N], identb.ap(), [onb.r(), identb.r()],
                     [pT2.r()], inc=(t == 1))
            yield
            B2 = dict(B)
            B2["t1"] = t2
            rwkv_epilogue(K, C, pc, B2, N, pT2, yT.ap()[:, 4:6, c * N:(c + 1) * N], xr(yT, c // 4, range(4, 6)))
            yield

        for _ in s1(0):
            pass
        for c in range(NCH):
            interleave([s2(c), s1(c + 1) if c + 1 < NCH else None], ratio=[3, 2])
        rw = RW[(NCH - 1) % 2]
        K.dma(dr["p_shift"][l].rearrange("(t p) -> p t", p=128), rw.ap()[:, :, N], r=[rw.r()])
        for t in range(2):
            K.tr(pL.ap()[:, t * N:(t + 1) * N], ST32.ap()[:, t, :], identf.ap(), [ST32.r(), identf.r()], [pL.r()],
                 inc=(t == 1))
        K.cp(stT.ap(), pL.ap()[:, 0:2 * N].rearrange("p (t n) -> p t n", t=2), [pL.r()], [stT.r()])
        for hd in range(4):
            t, o = hd // 2, 64 * (hd % 2)
            K.dma(dr["p_rwkv"][l, hd], stT.ap()[o:o + 64, t, o:o + 64], r=[stT.r()])
        P.barrier()


def rwkv_sample(K, dr, C, l, yT, win, pc, LW, dbg_out):
    P = K.P
    hs, identb, identf = C["hs"], C["identb"], C["identf"]
    N = NS
    with contextlib.ExitStack() as ph:
        f3 = lambda n: K.sb(ph, n, [128, 2, N], F32)
        B = {n: f3("rs_" + n) for n in ["sig", "aic", "gate", "kk", "t1", "kp", "bonus", "e1", "e2", "bb"]}
        B["MX"] = K.sb(ph, "MXs", [128, 7, N], F32)
        B["LI"] = K.sb(ph, "LIs", [128, N], BF16)
        rws = K.sb(ph, "rws", [128, 7, N], F32)
        prevs = K.sb(ph, "prevs", [128, 7, N], F32)
        shs = K.sb(ph, "shs", [NS, 896], F32)
        rwtm = K.sb(ph, "rwtm", [NS, 896], F32)
        pkT = K.sb(ph, "pkT", [NS, 6, 256], F32)
        pkh = K.sb(ph, "pkhr", [64, 6, 64], F32)
        S = K.sb(ph, "Sr", [64, 64, 64], F32)
        tmp = K.sb(ph, "tmpr", [64, 64, 64], F32)
        sa = K.sb(ph, "sa", [64, 64], F32)
        o = K.sb(ph, "orr", [64, 64], F32)
        on = K.sb(ph, "onr", [64, 64], F32)
        gn = (K.sb(ph, "gns_mean", [64, 1], F32), K.sb(ph, "gns_xc", [64, 64], F32),
              K.sb(ph, "gns_sq", [64, 64], F32), K.sb(ph, "gns_var", [64, 1], F32))
        otm = K.sb(ph, "otmr", [NS, 256], F32)
        otb = K.sb(ph, "otbr", [NS, 256], BF16)
        pA = K.ps(ph, "prs_A", [128, 1024], F32)
        pB = K.ps(ph, "prs_B", [128, 1024], F32)
        pL = K.ps(ph, "prs_L", [128, 512], F32)
        pM = K.ps(ph, "prs_M", [128, 512], F32)
        pT = K.ps(ph, "prs_T", [128, 1024], BF16)
        scr = dram_scratch(K, "rpk", [NS, 4, 6, 64])
        so = dram_scratch(K, "ro", [NS, 256])

        K.dma(shs.ap(), dr["st_shift"][l], w=[shs.r()])
        K.dma(S.ap().rearrange("p v k -> p (v k)"), dr["st_rwkv"][l].rearrange("b h v k -> (b h) (v k)"), w=[S.r()])
        for t in range(7):
            for k in range(KT):
                K.mm(pM.ap()[:, t * N:(t + 1) * N], win.ap()[:, k, t * 128:(t + 1) * 128], hs.ap()[:, k, :],
                     [win.r(), hs.r()], [pM.r()], start=(k == 0), stop=(k == KT - 1), inc=(k == KT - 1 and t == 6))
        K.cp(rws.ap(), pM.ap()[:, 0:7 * N].rearrange("p (t n) -> p t n", t=7), [pM.r()], [rws.r()], eng="act")
        for (c0, c1) in ((0, 512), (512, 896)):
            for k in range(KT):
                K.mm(pA.ap()[0:NS, c0:c1], hs.ap()[:, k, :], win.ap()[:, k, c0:c1], [win.r(), hs.r()], [pA.r()],
                     start=(k == 0), stop=(k == KT - 1))
        K.cp(rwtm.ap(), pA.ap()[0:NS, 0:896], [pA.r()], [rwtm.r()], eng="act")
        K.dma(dr["s_shift"][l], rwtm.ap(), r=[rwtm.r()])
        for t in range(7):
            K.tr(pL.ap()[:, t * N:(t + 1) * N], shs.ap()[:, t * 128:(t + 1) * 128], identf.ap()[0:NS, 0:NS],
                 [shs.r(), identf.r()], [pL.r()], inc=(t == 6))
        K.cp(prevs.ap(), pL.ap()[:, 0:7 * N].rearrange("p (t n) -> p t n", t=7), [pL.r()], [prevs.r()])
        B["_rw_res"] = [rws.r(), prevs.r()]
        for _ in rwkv_prep(K, C, pc, LW, N, rws.ap(), prevs.ap(), B, pB, pL, pM):
            pass
        r, k_, v = B["_rkv"]
        g = lambda n: B[n].ap()
        K.act(g("e1"), g("sig"), AF.Exp, [B["sig"].r()], [B["e1"].r()], scale=-C0)
        K.ts(g("e2"), g("kk"), -1.0, ALU.mult, [B["kk"].r()], [B["e2"].r()])
        K.tt(g("bb"), g("kk"), g("aic"), ALU.mult, [B["kk"].r(), B["aic"].r()], [B["bb"].r()])
        srcs = [(r, B["MX"].r()), (g("e1"), B["e1"].r()), (g("kp"), B["kp"].r()), (v, B["MX"].r()),
                (g("e2"), B["e2"].r()), (g("bb"), B["bb"].r())]
        for q, (ap, res) in enumerate(srcs):
            pp = pA if q < 4 else pB
            for t in range(2):
                col = ((q % 4) * 2 + t) * 128
                K.tr(pp.ap()[0:NS, col:col + 128], ap[:, t, :], identf.ap(), [res, identf.r()], [pp.r()])
        K.cp(pkT.ap()[:, 0:4, :], pA.ap()[0:NS, :].rearrange("p (q n) -> p q n", q=4), [pA.r()], [pkT.r()], eng="act")
        K.cp(pkT.ap()[:, 4:6, :], pB.ap()[0:NS, 0:512].rearrange("p (q n) -> p q n", q=2), [pB.r()], [pkT.r()])
        for q in range(6):
            K.dma(scr.ap()[:, :, q, :], pkT.ap()[:, q, :].rearrange("p (h k) -> p h k", h=4), r=[pkT.r()], w=[scr.r()])
        K.dma(pkh.ap(), scr.ap().rearrange("b h q k -> (b h) q k"), r=[scr.r()], w=[pkh.r()])
        rq, wq, kq, vq, aq, bq = (pkh.ap()[:, i, :] for i in range(6))
        K.tt(tmp.ap(), S.ap(), bc(aq, 1, [64, 64, 64]), ALU.mult, [S.r(), pkh.r()], [tmp.r()])
        K.red(sa.ap(), tmp.ap(), [tmp.r()], [sa.r()])
        K.tt(S.ap(), S.ap(), bc(wq, 1, [64, 64, 64]), ALU.mult, [S.r(), pkh.r()], [S.r()])
        K.tt(tmp.ap(), bc(sa.ap(), 2, [64, 64, 64]), bc(bq, 1, [64, 64, 64]), ALU.mult, [sa.r(), pkh.r()], [tmp.r()])
        K.tt(S.ap(), S.ap(), tmp.ap(), ALU.add, [S.r(), tmp.r()], [S.r()])
        K.tt(tmp.ap(), bc(vq, 2, [64, 64, 64]), bc(kq, 1, [64, 64, 64]), ALU.mult, [pkh.r()], [tmp.r()])
        K.tt(S.ap(), S.ap(), tmp.ap(), ALU.add, [S.r(), tmp.r()], [S.r()])
        K.dma(dr["s_rwkv"][l].rearrange("b h v k -> (b h) (v k)"), S.ap().rearrange("p v k -> p (v k)"), r=[S.r()])
        K.tt(tmp.ap(), S.ap(), bc(rq, 1, [64, 64, 64]), ALU.mult, [S.r(), pkh.r()], [tmp.r()])
        K.red(o.ap(), tmp.ap(), [tmp.r()], [o.r()])
        groupnorm64(K, o.ap().rearrange("p (g e) -> p g e", g=1), 64, 1, gn, [o.r()],
                    on.ap().rearrange("p (g e) -> p g e", g=1), [on.r()])
        K.dma(so.ap().rearrange("b (h v) -> (b h) v", h=4), on.ap(), r=[on.r()], w=[so.r()])
        K.dma(otm.ap(), so.ap(), r=[so.r()], w=[otm.r()])
        K.cp(otb.ap(), otm.ap(), [otm.r()], [otb.r()])
        for t in range(2):
            K.tr(pT.ap()[:, t * N:(t + 1) * N], otb.ap()[:, t * 128:(t + 1) * 128], identb.ap()[0:NS, 0:NS],
                 [otb.r(), identb.r()], [pT.r()], inc=(t == 1))
        rwkv_epilogue(K, C, pc, B, N, pT, yT.ap()[:, 4:6, T:T + NS], xr(yT, 4, range(4, 6)))
        P.barrier()
```

```python
import contextlib
import numpy as np
import concourse.bass as bass
import concourse.mybir as mybir
from concourse.bass_utils import run_bass_kernel_spmd

F32 = mybir.dt.float32
BF16 = mybir.dt.bfloat16
AF = mybir.ActivationFunctionType
ALU = mybir.AluOpType
AX = mybir.AxisListType

NCORES = 8
D = 1024
KT = 8
T = 2048
NS = 16
NT = T + NS
NCH = T // 128
DEPTH = 2
IN_DIM = 2968
OFF = dict(z=0, xbc=512, dt=1280, rw=1288, gq=2184, gk=2312, gv=2440, glo=2696, gg=2712)
F_DENSE = 2816
ALPHA = (2.0 * DEPTH) ** 0.25
LN_EPS = 1e-5
RMS_EPS = 1e-6
RWKV_GN_EPS = 64 * 1e-5
BLOCKS = [(0, 512), (512, 512), (1024, 512), (1536, 512), (2048, 16)]

ENGS = ["pe", "dve", "act", "pool", "sp"]


class Res:
    __slots__ = ("name", "w", "r", "excl")

    def __init__(self, name="", excl=False):
        self.name = name
        self.w = None
        self.r = []
        self.excl = excl


class Prog:
    NDMA = 8

    def __init__(self, nc):
        self.nc = nc
        self.q = {e: [] for e in ENGS}
        self.cnt = {e: 0 for e in ENGS}
        self.seen = {e: {} for e in ENGS}
        self.dma_i = {e: 0 for e in ENGS}
        self.dma_last = {}
        self.sems = {}

    def sem(self, key):
        if key not in self.sems:
            self.sems[key] = self.nc.alloc_semaphore(name="s_" + "_".join(str(k) for k in key))
        return self.sems[key]

    def _collect(self, eng, reads, writes):
        waits = {}

        def add(tok):
            if tok is None:
                return
            key, val = tok
            if self.seen[eng].get(key, 0) >= val:
                return
            if waits.get(key, 0) < val:
                waits[key] = val

        for r in reads:
            add(r.w)
        for w in writes:
            add(w.w)
            for t in w.r:
                add(t)
        if eng == "pe":
            waits.pop(("e", "pe"), None)
        for k, v in waits.items():
            self.seen[eng][k] = v
        return list(waits.items())

    def _commit(self, tok, reads, writes):
        for r in reads:
            r.r.append(tok)
            if len(r.r) > 64:
                r.r = _prune(r.r)
        for w in writes:
            w.w = tok
            w.r = []

    def op(self, eng, fn, reads=(), writes=(), inc=True, self_wait=False):
        assert inc or eng == "pe"
        if any(r.excl for r in reads):
            writes = list(writes) + [r for r in reads if r.excl]
            reads = [r for r in reads if not r.excl]
        waits = self._collect(eng, reads, writes)
        if self_wait and self.cnt[eng] > 0:
            waits.append((("e", eng), self.cnt[eng]))
        tok = (("e", eng), self.cnt[eng] + 1)
        if inc:
            self.cnt[eng] += 1
        self._commit(tok, reads, writes)

        def emit(e, fn=fn, waits=waits, inc=inc, eng=eng):
            for k, v in waits:
                e.wait_ge(self.sem(k), v)
            ins = fn(e)
            if inc:
                ins.then_inc(self.sem(("e", eng)), 1)
        self.q[eng].append(emit)
        return tok

    def dma(self, queue, out, in_, reads=(), writes=(), **kw):
        i = self.dma_i[queue]
        self.dma_i[queue] += 1
        slot = i % self.NDMA
        key = ("d", queue, slot)
        val = 16 * (i // self.NDMA + 1)
        waits = self._collect(queue, reads, writes)
        prev = val - 16
        if prev > 0 and self.seen[queue].get(key, 0) < prev:
            self.seen[queue][key] = prev
            waits.append((key, prev))
        tok = (key, val)
        self.dma_last[key] = val
        self._commit(tok, reads, writes)

        def emit(e, waits=waits, key=key):
            for k, v in waits:
                e.wait_ge(self.sem(k), v)
            e.dma_start(out=out, in_=in_, **kw).then_inc(self.sem(key), 16)
        self.q[queue].append(emit)
        return tok

    def barrier(self, engines=ENGS):
        toks = [(("e", e), self.cnt[e]) for e in ENGS if self.cnt[e] > 0]
        toks += list(self.dma_last.items())
        for eng in engines:
            waits = []
            for k, v in toks:
                if k == ("e", eng) and eng == "pe":
                    continue
                if self.seen[eng].get(k, 0) < v:
                    self.seen[eng][k] = v
                    waits.append((k, v))

            def emit(e, waits=waits):
                for k, v in waits:
                    e.wait_ge(self.sem(k), v)
            self.q[eng].append(emit)

    def emit(self):
        with self.nc.Block() as block:
            @block.tensor
            def _(e):
                for f in self.q["pe"]:
                    f(e)

            @block.vector
            def _(e):
                for f in self.q["dve"]:
                    f(e)

            @block.scalar
            def _(e):
                for f in self.q["act"]:
                    f(e)

            @block.gpsimd
            def _(e):
                for f in self.q["pool"]:
                    f(e)

            @block.sync
            def _(e):
                for f in self.q["sp"]:
                    f(e)


def _prune(toks):
    best = {}
    for k, v in toks:
        if best.get(k, 0) < v:
            best[k] = v
    return list(best.items())


class Tn:
    def __init__(self, h, name, excl=False):
        self.h = h
        self.name = name
        self._res = {}
        self.excl = excl

    def ap(self):
        return self.h.ap()

    def r(self, key=0):
        if key not in self._res:
            self._res[key] = Res(f"{self.name}:{key}", self.excl)
        return self._res[key]


class KB:
    def __init__(self, nc):
        self.nc = nc
        self.P = Prog(nc)
        self.uid = 0

    def sb(self, stack, name, shape, dt=F32):
        self.uid += 1
        h = stack.enter_context(self.nc.sbuf_tensor(f"{name}_{self.uid}", list(shape), dt))
        return Tn(h, name)

    def ps(self, stack, name, shape, dt=F32):
        self.uid += 1
        h = stack.enter_context(self.nc.psum_tensor(f"{name}_{self.uid}", list(shape), dt))
        return Tn(h, name, excl=True)

    def mm(self, out, lhsT, rhs, r, w, start=True, stop=True, inc=None, self_wait=False, sgc=False):
        inc = stop if inc is None else inc
        kw = {"skip_group_check": True} if sgc else {}
        self.P.op("pe", lambda e: e.matmul(out, lhsT=lhsT, rhs=rhs, start=start, stop=stop, **kw),
                  reads=r, writes=w, inc=inc, self_wait=self_wait)

    def tr(self, out, in_, ident, r, w, inc=True):
        self.P.op("pe", lambda e: e.transpose(out, in_, ident), reads=r, writes=w, inc=inc)

    def act(self, out, in_, func, r, w, scale=None, bias=None, accum_out=None):
        kw = {}
        if scale is not None:
            kw["scale"] = scale
        if bias is not None:
            kw["bias"] = bias
        if accum_out is not None:
            kw["accum_out"] = accum_out
        self.P.op("act", lambda e: e.activation(out=out, in_=in_, func=func, **kw), reads=r, writes=w)

    def tt(self, out, in0, in1, op, r, w, eng="dve"):
        self.P.op(eng, lambda e: e.tensor_tensor(out=out, in0=in0, in1=in1, op=op), reads=r, writes=w)

    def ts(self, out, in0, s1, op0, r, w, s2=None, op1=None, eng="dve", accum_out=None):
        kw = {}
        if op1 is not None:
            kw["op1"] = op1
        if accum_out is not None:
            kw["accum_out"] = accum_out
        self.P.op(eng, lambda e: e.tensor_scalar(out=out, in0=in0, scalar1=s1, scalar2=s2, op0=op0, **kw),
                  reads=r, writes=w)

    def stt(self, out, in0, scalar, in1, op0, op1, r, w, eng="dve"):
        self.P.op(eng, lambda e: e.scalar_tensor_tensor(out=out, in0=in0, scalar=scalar, in1=in1, op0=op0, op1=op1),
                  reads=r, writes=w)

    def cp(self, out, in_, r, w, eng="dve"):
        if eng == "act":
            self.P.op("act", lambda e: e.activation(out=out, in_=in_, func=AF.Copy), reads=r, writes=w)
        else:
            self.P.op(eng, lambda e: e.tensor_copy(out=out, in_=in_), reads=r, writes=w)

    def recip(self, out, in_, r, w):
        self.P.op("dve", lambda e: e.reciprocal(out=out, in_=in_), reads=r, writes=w)

    def red(self, out, in_, r, w, op=ALU.add, axis=AX.X, eng="dve"):
        self.P.op(eng, lambda e: e.tensor_reduce(out=out, in_=in_, axis=axis, op=op), reads=r, writes=w)

    def memset(self, ap, val, w, eng="dve"):
        self.P.op(eng, lambda e: e.memset(ap, val), writes=w)

    def dma(self, out, in_, r=(), w=(), q="sp", **kw):
        return self.P.dma(q, out, in_, reads=r, writes=w, **kw)


W_SHAPES = dict(
    w_ada=[DEPTH, D, 6 * D], b_ada=[DEPTH, 6 * D], w_in=[DEPTH, D, IN_DIM], w_out=[DEPTH, D, D],
    ssd_conv_w=[DEPTH, 4, 768], ssd_conv_b=[DEPTH, 768], ssd_dt_bias=[DEPTH, 8], ssd_a_log=[DEPTH, 8],
    ssd_d=[DEPTH, 8], ssd_norm_g=[DEPTH, 512], rwkv_mu=[DEPTH, 896], rwkv_w0=[DEPTH, 256],
    rwkv_w2=[DEPTH, 32, 256], rwkv_a0=[DEPTH, 256], rwkv_a2=[DEPTH, 32, 256], rwkv_g2=[DEPTH, 64, 256],
    rwkv_k_k=[DEPTH, 256], rwkv_k_a=[DEPTH, 256], rwkv_r_k=[DEPTH, 4, 64], rwkv_ln_g=[DEPTH, 256],
    rwkv_ln_b=[DEPTH, 256], gla_w_gk2=[DEPTH, 16, 128], gla_b_gk=[DEPTH, 128], gla_norm_g=[DEPTH, 64],
    ln_mix_g=[DEPTH, D], ln_mix_b=[DEPTH, D], ln_ffn_g=[DEPTH, D], ln_ffn_b=[DEPTH, D],
    ffn_w_gate=[1, D, F_DENSE], ffn_w_up=[1, D, F_DENSE], ffn_w_down=[1, F_DENSE, D],
    moe_router=[1, D, 8], moe_w_gate=[1, 8, D, D], moe_w_up=[1, 8, D, D], moe_w_down=[1, 8, D, D],
)
IN_SHAPES = dict(
    xp=[T, D], xs=[NS, D], cc=[1 + NS, D],
    st_ssd=[DEPTH, NS, 8, 64, 64], st_conv=[DEPTH, NS, 3, 768], st_rwkv=[DEPTH, NS, 4, 64, 64],
    st_shift=[DEPTH, NS, 896], st_gla=[DEPTH, NS, 4, 32, 64],
)
OUT_SHAPES = dict(
    y_p=[T, D], y_s=[NS, D],
    p_ssd=[DEPTH, 8, 64, 64], p_conv=[DEPTH, 3, 768], p_rwkv=[DEPTH, 4, 64, 64], p_shift=[DEPTH, 896],
    p_gla=[DEPTH, 4, 32, 64],
    s_ssd=[DEPTH, NS, 8, 64, 64], s_conv=[DEPTH, NS, 3, 768], s_rwkv=[DEPTH, NS, 4, 64, 64],
    s_shift=[DEPTH, NS, 896], s_gla=[DEPTH, NS, 4, 32, 64],
)

def xr(t, b, tiles=range(KT)):
    return [t.r((d, b)) for d in tiles]


SH1, SC1, GT1, SH2, SC2, GT2 = 0, 8, 16, 24, 32, 40


def build(stub_mixer=False, dbg=None, n_layers=DEPTH):
    nc = bass.Bass("TRN2", target_bir_lowering=False)
    K = KB(nc)
    P = K.P
    dr = {}
    for n, s in IN_SHAPES.items():
        dr[n] = nc.dram_tensor(n, s, F32, kind="ExternalInput").ap()
    for n, s in W_SHAPES.items():
        dr[n] = nc.dram_tensor(n, s, F32, kind="ExternalInput").ap()
    for n, s in OUT_SHAPES.items():
        dr[n] = nc.dram_tensor(n, s, F32, kind="ExternalOutput").ap()
    dbg_out = {}
    if dbg:
        for n, s in dbg.items():
            dbg_out[n] = nc.dram_tensor("dbg_" + n, s, F32, kind="ExternalOutput").ap()
    out_res = Res("outputs")

    with contextlib.ExitStack() as perm, nc.allow_non_contiguous_dma(reason="small param loads"):
        xT = K.sb(perm, "xT", [128, KT, NT], F32)
        modT = [K.sb(perm, f"modT{l}", [128, 48, 1 + NS], F32) for l in range(DEPTH)]
        identf = K.sb(perm, "identf", [128, 128], F32)
        identb = K.sb(perm, "identb", [128, 128], BF16)
        onesM = K.sb(perm, "onesM", [128, 128], F32)
        ones1 = K.sb(perm, "ones1", [128, 128], F32)
        maskU = K.sb(perm, "maskU", [128, 128], F32)
        maskSU = K.sb(perm, "maskSU", [128, 128], F32)
        maskSL = K.sb(perm, "maskSL", [128, 128], F32)
        blk64 = K.sb(perm, "blk64", [128, 128], F32)
        C = dict(ones1f=ones1, xT=xT, modT=modT, identf=identf, identb=identb, onesM=onesM, ones1=ones1,
                 maskU=maskU, maskSU=maskSU, maskSL=maskSL, blk64=blk64)

        def sel(t, val_keep, cmp, fill, base=0, cm=1, pat=None, ap=None):
            ap = t.ap() if ap is None else ap
            pat = [[-1, ap.shape[-1]]] if pat is None else pat
            P.op("pool", lambda e: e.affine_select(out=ap, in_=ap, pattern=pat, compare_op=cmp, fill=fill,
                                                   base=base, channel_multiplier=cm),
                 reads=[t.r()], writes=[t.r()])

        K.memset(identf.ap(), 0.0, [identf.r()], eng="pool")
        sel(identf, 0, ALU.not_equal, 1.0)
        K.cp(identb.ap(), identf.ap(), [identf.r()], [identb.r()], eng="pool")
        K.memset(onesM.ap(), 1.0 / D, [onesM.r()], eng="pool")
        K.memset(ones1.ap(), 1.0, [ones1.r()], eng="pool")
        K.memset(maskU.ap(), 1.0, [maskU.r()], eng="pool")
        sel(maskU, 1, ALU.is_ge, 0.0, cm=-1, pat=[[1, 128]])
        K.memset(maskSU.ap(), 1.0, [maskSU.r()], eng="pool")
        sel(maskSU, 1, ALU.is_gt, 0.0, cm=-1, pat=[[1, 128]])
        K.memset(maskSL.ap(), 1.0, [maskSL.r()], eng="pool")
        sel(maskSL, 1, ALU.is_gt, 0.0)
        K.memset(blk64.ap(), 0.0, [blk64.r()], eng="pool")
        K.memset(blk64.ap()[0:64, 0:64], 1.0, [blk64.r()], eng="pool")
        K.memset(blk64.ap()[64:128, 64:128], 1.0, [blk64.r()], eng="pool")

        with contextlib.ExitStack() as ph:
            stage = [K.sb(ph, f"stg{i}", [128, D], F32) for i in range(2)]
            ctm = K.sb(ph, "ctm", [1 + NS, D], F32)
            scT = K.sb(ph, "scT", [128, KT, 1 + NS], BF16)
            wada = [K.sb(ph, f"wada{i}", [128, KT, 512], BF16) for i in range(2)]
            bB = [K.sb(ph, f"bB{i}", [1 + NS, 512], F32) for i in range(2)]
            modsb = [K.sb(ph, f"modsb{i}", [1 + NS, 512], F32) for i in range(2)]
            pst = [K.ps(ph, f"pst{i}", [128, 1024], F32) for i in range(2)]
            psm = [K.ps(ph, f"psm{i}", [128, 512], F32) for i in range(2)]
            pss = K.ps(ph, "pss", [128, 512], F32)
            for tt in range(NCH + 1):
                st = stage[tt % 2]
                pt = pst[tt % 2]
                n = 128 if tt < NCH else NS
                src = dr["xp"][tt * 128:(tt + 1) * 128, :] if tt < NCH else dr["xs"]
                K.dma(st.ap()[0:n, :], src, w=[st.r()])
                for d in range(KT):
                    K.tr(pt.ap()[:, d * n:(d + 1) * n], st.ap()[0:n, d * 128:(d + 1) * 128],
                         identf.ap()[0:n, 0:n], [st.r(), identf.r()], [pt.r()], inc=(d == KT - 1))
                dst = xT.ap()[:, :, tt * 128:tt * 128 + n]
                srcp = pt.ap()[:, 0:KT * n].rearrange("p (d n) -> p d n", d=KT)
                K.cp(dst, srcp, [pt.r()], xr(xT, min(tt // 4, 4)), eng=("act" if tt % 2 == 0 else "dve"))
            K.dma(ctm.ap(), dr["cc"], w=[ctm.r()])
            for d in range(KT):
                K.tr(pss.ap()[:, d * 17:(d + 1) * 17], ctm.ap()[:, d * 128:(d + 1) * 128],
                     identf.ap()[0:17, 0:17], [ctm.r(), identf.r()], [pss.r()], inc=(d == KT - 1))
            K.act(scT.ap(), pss.ap()[:, 0:KT * 17].rearrange("p (d n) -> p d n", d=KT), AF.Silu,
                  [pss.r()], [scT.r()])
            it = 0
            for l in range(DEPTH):
                for j in range(12):
                    wb = wada[it % 2]
                    bb = bB[it % 2]
                    ms = modsb[it % 2]
                    pm = psm[it % 2]
                    K.dma(wb.ap(), dr["w_ada"][l, :, j * 512:(j + 1) * 512].rearrange("(k p) n -> p k n", p=128),
                          w=[wb.r()], q="pool")
                    K.dma(bb.ap(), dr["b_ada"][l:l + 1, j * 512:(j + 1) * 512].to_broadcast([1 + NS, 512]),
                          w=[bb.r()])
                    for k in range(KT):
                        K.mm(pm.ap()[0:17, :], scT.ap()[:, k, :], wb.ap()[:, k, :], [scT.r(), wb.r()], [pm.r()],
                             start=(k == 0), stop=(k == KT - 1))
                    K.tt(ms.ap(), pm.ap()[0:17, :], bb.ap(), ALU.add, [pm.r(), bb.r()], [ms.r()])
                    for q4 in range(4):
                        K.tr(pss.ap()[:, q4 * 17:(q4 + 1) * 17], ms.ap()[:, q4 * 128:(q4 + 1) * 128],
                             identf.ap()[0:17, 0:17], [ms.r(), identf.r()], [pss.r()], inc=(q4 == 3))
                    K.cp(modT[l].ap()[:, j * 4:(j + 1) * 4, :],
                         pss.ap()[:, 0:4 * 17].rearrange("p (d n) -> p d n", d=4), [pss.r()], [modT[l].r()],
                         eng="act")
                    it += 1
                for seg in (SC1, GT1, SC2, GT2):
                    K.ts(modT[l].ap()[:, seg:seg + 8, :], modT[l].ap()[:, seg:seg + 8, :], 1.0, ALU.add,
                         [modT[l].r()], [modT[l].r()])
            P.barrier()

        for l in range(n_layers):
            layer(K, dr, C, l, stub_mixer, dbg_out)

        with contextlib.ExitStack() as ph:
            stage = [K.sb(ph, f"ostg{i}", [128, D], F32) for i in range(2)]
            pst = [K.ps(ph, f"opst{i}", [128, 1024], F32) for i in range(2)]
            for tt in range(NCH + 1):
                st = stage[tt % 2]
                pt = pst[tt % 2]
                n = 128 if tt < NCH else NS
                for d in range(KT):
                    K.tr(pt.ap()[0:n, d * 128:(d + 1) * 128], xT.ap()[:, d, tt * 128:tt * 128 + n],
                         identf.ap(), [xT.r((d, min(tt // 4, 4))), identf.r()], [pt.r()], inc=(d == KT - 1))
                K.cp(st.ap()[0:n, :], pt.ap()[0:n, :], [pt.r()], [st.r()], eng=("act" if tt % 2 == 0 else "dve"))
                dst = dr["y_p"][tt * 128:(tt + 1) * 128, :] if tt < NCH else dr["y_s"]
                K.dma(dst, st.ap()[0:n, :], r=[st.r()])
            P.barrier()
    with nc.allow_non_contiguous_dma(reason="small param loads"):
        P.emit()
    return nc


def modulate(K, C, l, src, dst, b, sh, sc, dres):
    mod = C["modT"][l]
    c0, n = BLOCKS[b]
    if b < 4:
        for d in range(KT):
            K.act(dst.ap()[:, d, c0:c0 + n], src.ap()[:, d, c0:c0 + n], AF.Identity,
                  [src.r((d, b)), mod.r()], [dres(d)],
                  scale=mod.ap()[:, sc + d, 0:1], bias=mod.ap()[:, sh + d, 0:1])
    else:
        tmp = C["tmp_s"]
        K.tt(tmp.ap(), src.ap()[:, :, c0:c0 + n], mod.ap()[:, sc:sc + 8, 1:1 + NS], ALU.mult,
             xr(src, b) + [mod.r()], [tmp.r()])
        K.tt(dst.ap()[:, :, c0:c0 + n], tmp.ap(), mod.ap()[:, sh:sh + 8, 1:1 + NS], ALU.add,
             [tmp.r(), mod.r()], [dres(d) for d in range(KT)])


def layernorm(K, C, l, gi, psA, psB, scr):
    xT, onesM, lncol = C["xT"], C["onesM"], C["lncol"]
    sq, mean_sb, var, tt_ = scr["sq"], scr["mean"], scr["var"], scr["t"]
    for b, (c0, n) in enumerate(BLOCKS):
        for d in range(KT):
            s = sq[d % 2]
            xs = xT.ap()[:, d, c0:c0 + n]
            K.act(s.ap()[:, :n], xs, AF.Square, [xT.r((d, b))], [s.r()])
            K.mm(psA.ap()[:, :n], onesM.ap(), xs, [onesM.r(), xT.r((d, b))], [psA.r()],
                 start=(d == 0), stop=(d == KT - 1), inc=True)
            K.mm(psB.ap()[:, :n], onesM.ap(), s.ap()[:, :n], [onesM.r(), s.r()], [psB.r()],
                 start=(d == 0), stop=(d == KT - 1), inc=True)
        K.cp(mean_sb.ap()[:, :n], psA.ap()[:, :n], [psA.r()], [mean_sb.r()], eng="act")
        K.tt(var.ap()[:, :n], mean_sb.ap()[:, :n], mean_sb.ap()[:, :n], ALU.mult, [mean_sb.r()], [var.r()])
        K.tt(var.ap()[:, :n], psB.ap()[:, :n], var.ap()[:, :n], ALU.subtract, [psB.r(), var.r()], [var.r()])
        K.act(var.ap()[:, :n], var.ap()[:, :n], AF.Sqrt, [var.r()], [var.r()], bias=LN_EPS)
        K.recip(var.ap()[:, :n], var.ap()[:, :n], [var.r()], [var.r()])
        for d in range(KT):
            t = tt_[d % 2]
            xs = xT.ap()[:, d, c0:c0 + n]
            K.tt(t.ap()[:, :n], xs, mean_sb.ap()[:, :n], ALU.subtract, [xT.r((d, b)), mean_sb.r()], [t.r()])
            K.tt(t.ap()[:, :n], t.ap()[:, :n], var.ap()[:, :n], ALU.mult, [t.r(), var.r()], [t.r()])
            K.act(xs, t.ap()[:, :n], AF.Identity, [t.r(), lncol.r()], [xT.r((d, b))],
                  scale=lncol.ap()[:, gi, d:d + 1], bias=lncol.ap()[:, gi + 1, d:d + 1])


def residual_add(K, C, l, ps, n, dout, b, gt, comb=None):
    xT, mod = C["xT"], C["modT"][l]
    c0, _ = BLOCKS[b]
    xs = xT.ap()[:, dout, c0:c0 + n]
    src = ps.ap()[:, :n]
    rd = [ps.r(), mod.r(), xT.r((dout, b))]
    if comb is not None:
        tmp = C["tmp_c"][dout % 2]
        K.tt(tmp.ap()[:, :n], src, comb.ap()[:, c0:c0 + n], ALU.mult, [ps.r(), comb.r(b)], [tmp.r()])
        src = tmp.ap()[:, :n]
        rd = [tmp.r(), mod.r(), xT.r((dout, b))]
    if b < 4:
        K.stt(xs, src, mod.ap()[:, gt + dout, 0:1], xs, ALU.mult, ALU.add, rd, [xT.r((dout, b))])
    else:
        tmp2 = C["tmp_s2"]
        K.tt(tmp2.ap(), src, mod.ap()[:, gt + dout, 1:1 + NS], ALU.mult, rd[:2], [tmp2.r()])
        K.tt(xs, tmp2.ap(), xs, ALU.add, [tmp2.r(), xT.r((dout, b))], [xT.r((dout, b))])


def scale_x(K, C):
    xT = C["xT"]
    for b, (c0, n) in enumerate(BLOCKS):
        for d in range(KT):
            xs = xT.ap()[:, d, c0:c0 + n]
            K.P.op("act", lambda e, xs=xs: e.mul(out=xs, in_=xs, mul=ALPHA), reads=[xT.r((d, b))],
                   writes=[xT.r((d, b))])


def ffn_gateup(K, C, l, hT, actb, wpool, srcs, nf, ps):
    wg_src, wu_src, wd_src = srcs
    sg = C["sg"]

    def unit():
        u = wpool["bufs"][wpool["i"] % len(wpool["bufs"])]
        wpool["i"] += 1
        return u

    i = 0
    for f0 in range(0, nf, 4):
        nfu = min(4, nf - f0)
        WG, WU = unit(), unit()
        K.dma(WG.ap()[:, :, 0:nfu * 128], wg_src[:, f0 * 128:(f0 + nfu) * 128].rearrange("(k p) n -> p k n", p=128),
              w=[WG.r()], q="pool")
        K.dma(WU.ap()[:, :, 0:nfu * 128], wu_src[:, f0 * 128:(f0 + nfu) * 128].rearrange("(k p) n -> p k n", p=128),
              w=[WU.r()], q="pool")
        for fu in range(nfu):
            f = f0 + fu
            for b, (c0, n) in enumerate(BLOCKS):
                pg, pu = ps["g"][i % 2], ps["u"][i % 2]
                for k in range(KT):
                    K.mm(pg.ap()[:, :n], WG.ap()[:, k, fu * 128:(fu + 1) * 128], hT.ap()[:, k, c0:c0 + n],
                         [WG.r(), hT.r((k, b))], [pg.r()], start=(k == 0), stop=(k == KT - 1))
                for k in range(KT):
                    K.mm(pu.ap()[:, :n], WU.ap()[:, k, fu * 128:(fu + 1) * 128], hT.ap()[:, k, c0:c0 + n],
                         [WU.r(), hT.r((k, b))], [pu.r()], start=(k == 0), stop=(k == KT - 1))
                s_ = sg[i % 2]
                K.act(s_.ap()[:, :n], pg.ap()[:, :n], AF.Silu, [pg.r()], [s_.r()])
                K.tt(actb.ap()[:, f, c0:c0 + n], s_.ap()[:, :n], pu.ap()[:, :n], ALU.mult, [s_.r(), pu.r()],
                     [actb.r((f, b))])
                i += 1
                yield


def ffn_down(K, C, l, actb, wpool, srcs, nf, ps, comb=None):
    wg_src, wu_src, wd_src = srcs

    def unit():
        u = wpool["bufs"][wpool["i"] % len(wpool["bufs"])]
        wpool["i"] += 1
        return u

    i = 0
    for dh in range(2):
        WD = unit()
        K.dma(WD.ap()[:, 0:nf, :], wd_src[:, dh * 512:(dh + 1) * 512].rearrange("(f p) n -> p f n", p=128),
              w=[WD.r()], q="pool")
        for dd in range(4):
            dout = dh * 4 + dd
            for b, (c0, n) in enumerate(BLOCKS):
                pd = ps["d"][i % 2]
                for f in range(nf):
                    K.mm(pd.ap()[:, :n], WD.ap()[:, f, dd * 128:(dd + 1) * 128], actb.ap()[:, f, c0:c0 + n],
                         [WD.r(), actb.r((f, b))], [pd.r()], start=(f == 0), stop=(f == nf - 1))
                residual_add(K, C, l, pd, n, dout, b, GT2, comb=comb)
                i += 1


def ffn_group(K, C, l, hT, actb, wpool, srcs, nf, ps, comb=None):
    for _ in ffn_gateup(K, C, l, hT, actb, wpool, srcs, nf, ps):
        pass
    ffn_down(K, C, l, actb, wpool, srcs, nf, ps, comb=comb)


def moe_routing(K, C, dr, l, ph, ps):
    xT, mod, identf = C["xT"], C["modT"][l], C["identf"]
    router = K.sb(ph, "router", [128, KT, 8], F32)
    K.dma(router.ap(), dr["moe_router"][0].rearrange("(k p) e -> p k e", p=128), w=[router.r()])
    combT = K.sb(ph, "combT", [8, NT], F32)
    tp = C["tpair"]
    sm = {n: K.sb(ph, "rt_" + n, [128, 8], F32) for n in ["lg", "eq1", "l2", "eq2", "cb"]}
    sc1 = {n: K.sb(ph, "rs_" + n, [128, 1], F32) for n in ["m1", "m2", "e", "w1", "w2"]}
    pl, pt = ps["rA"], ps["rB"]
    rmod = K.sb(ph, "rmod", [128, KT, 8], F32)
    crow = K.sb(ph, "crow", [1, 8], F32)
    K.tt(rmod.ap(), router.ap(), bc(mod.ap()[:, SC2:SC2 + 8, 0], 2, [128, KT, 8]), ALU.mult, [router.r(), mod.r()],
         [rmod.r()])
    for d in range(KT):
        K.mm(pt.ap()[0:1, 0:8], mod.ap()[:, SH2 + d, 0:1], router.ap()[:, d, :], [mod.r(), router.r()], [pt.r()],
             start=(d == 0), stop=(d == KT - 1), inc=True)
    K.cp(crow.ap(), pt.ap()[0:1, 0:8], [pt.r()], [crow.r()])
    yield
    for tt in range(NCH + 1):
        n = 128 if tt < NCH else NS
        c0 = tt * 128
        b = min(tt // 4, 4)
        hap = tp.ap().rearrange("p a (b c) -> p (a b) c", c=128)
        hres = [tp.r(0), tp.r(1)]
        if tt < NCH:
            for d in range(KT):
                K.mm(pl.ap()[0:n, 0:8], xT.ap()[:, d, c0:c0 + n], rmod.ap()[:, d, :], [xT.r((d, b)), rmod.r()], [pl.r()],
                     start=(d == 0), stop=False, inc=True)
            K.mm(pl.ap()[0:n, 0:8], C["ones1f"].ap()[0:1, 0:n], crow.ap(), [C["ones1f"].r(), crow.r()], [pl.r()],
                 start=False, stop=True, inc=True)
        else:
            K.tt(hap[:, :, 0:n], xT.ap()[:, :, c0:c0 + n], mod.ap()[:, SC2:SC2 + 8, 1:1 + NS], ALU.mult,
                 xr(xT, b) + [mod.r()], hres)
            K.tt(hap[:, :, 0:n], hap[:, :, 0:n], mod.ap()[:, SH2:SH2 + 8, 1:1 + NS], ALU.add,
                 hres + [mod.r()], hres)
        if tt == NCH:
            for d in range(KT):
                K.mm(pl.ap()[0:n, 0:8], hap[:, d, 0:n], router.ap()[:, d, :], hres + [router.r()], [pl.r()],
                     start=(d == 0), stop=(d == KT - 1), inc=True)
        lg, eq1, l2, eq2, cb = (sm[k].ap()[0:n, :] for k in ["lg", "eq1", "l2", "eq2", "cb"])
        m1, m2, ee, w1, w2 = (sc1[k].ap()[0:n, :] for k in ["m1", "m2", "e", "w1", "w2"])
        R = lambda *ks: [(sm[k] if k in sm else sc1[k]).r() for k in ks]
        K.cp(lg, pl.ap()[0:n, 0:8], [pl.r()], R("lg"))
        yield
        K.red(m1, lg, R("lg"), R("m1"), op=ALU.max)
        yield
        K.ts(eq1, lg, m1, ALU.is_equal, R("lg", "m1"), R("eq1"))
        yield
        K.stt(l2, eq1, -1e30, lg, ALU.mult, ALU.add, R("eq1", "lg"), R("l2"))
        yield
        K.red(m2, l2, R("l2"), R("m2"), op=ALU.max)
        yield
        K.ts(eq2, l2, m2, ALU.is_equal, R("l2", "m2"), R("eq2"))
        yield
        K.tt(ee, m2, m1, ALU.subtract, R("m1", "m2"), R("e"))
        yield
        K.act(ee, ee, AF.Exp, R("e"), R("e"))
        yield
        K.ts(w1, ee, 1.0, ALU.add, R("e"), R("w1"))
        yield
        K.recip(w1, w1, R("w1"), R("w1"))
        yield
        K.tt(w2, ee, w1, ALU.mult, R("e", "w1"), R("w2"))
        yield
        K.ts(cb, eq1, w1, ALU.mult, R("eq1", "w1"), R("cb"))
        yield
        K.stt(cb, eq2, w2, cb, ALU.mult, ALU.add, R("eq2", "w2", "cb"), R("cb"))
        yield
        K.tr(pt.ap()[0:8, 0:n], cb, identf.ap()[0:n, 0:n], R("cb") + [identf.r()], [pt.r()])
        yield
        K.cp(combT.ap()[:, c0:c0 + n], pt.ap()[0:8, 0:n], [pt.r()], [combT.r()], eng="act")
        yield
    C["_combT"] = combT
    yield


def layer(K, dr, C, l, stub_mixer, dbg_out):
    nc, P = K.nc, K.P
    xT, mod = C["xT"], C["modT"][l]
    with contextlib.ExitStack() as lay:
        bufA = K.sb(lay, "bufA", [128, KT, NT], BF16)
        lncol = K.sb(lay, "lncol", [128, 4, KT], F32)
        C["lncol"] = lncol
        C["tmp_s"] = K.sb(lay, "tmp_s", [128, KT, NS], F32)
        C["tmp_s2"] = K.sb(lay, "tmp_s2", [128, NS], F32)
        for i, nme in enumerate(["ln_mix_g", "ln_mix_b", "ln_ffn_g", "ln_ffn_b"]):
            K.dma(lncol.ap()[:, i, :], dr[nme][l].rearrange("(d p) -> p d", p=128), w=[lncol.r()])

        with contextlib.ExitStack() as ph:
            if stub_mixer:
                for b in range(5):
                    modulate(K, C, l, xT, bufA, b, SH1, SC1, lambda d, b=b: bufA.r((d, b)))
            else:
                mixers(K, dr, C, l, bufA, dbg_out)
            P.barrier()
            wout = K.sb(ph, "wout", [128, KT, D], BF16)
            K.dma(wout.ap(), dr["w_out"][l].rearrange("(k p) n -> p k n", p=128), w=[wout.r()], q="pool")
            scale_x(K, C)
            with contextlib.ExitStack() as ph2:
                pso = [K.ps(ph2, f"pso{i}", [128, 512], F32) for i in range(4)]
                i = 0
                for dout in range(KT):
                    for b, (c0, n) in enumerate(BLOCKS):
                        pd = pso[i % 4]
                        for k in range(KT):
                            K.mm(pd.ap()[:, :n], wout.ap()[:, k, dout * 128:(dout + 1) * 128],
                                 bufA.ap()[:, k, c0:c0 + n], [wout.r(), bufA.r((k, b))], [pd.r()],
                                 start=(k == 0), stop=(k == KT - 1))
                        residual_add(K, C, l, pd, n, dout, b, GT1)
                        i += 1
                P.barrier()

        with contextlib.ExitStack() as ph:
            hT = K.sb(ph, "hT", [128, KT, NT], BF16)
            tpair = K.sb(ph, "tpair", [128, 2, 512], F32)
            C["tpair"] = tpair

            class _V:
                def __init__(self, i):
                    self.i = i

                def ap(self):
                    return tpair.ap()[:, self.i, :]

                def r(self, key=0):
                    return tpair.r(self.i)
            scr = dict(sq=[K.sb(ph, f"sq{i}", [128, 512], F32) for i in range(2)],
                       mean=K.sb(ph, "mean", [128, 512], F32), var=K.sb(ph, "var", [128, 512], F32),
                       t=[_V(0), _V(1)])
            C["sg"] = scr["sq"]
            C["tmp_c"] = scr["t"]
            W = dict(bufs=[K.sb(ph, f"WP{i}", [128, KT, 512], BF16) for i in range(4)], i=0)
            ps = dict(g=[K.ps(ph, f"pg{i}", [128, 512], F32) for i in range(2)],
                      u=[K.ps(ph, f"pu{i}", [128, 512], F32) for i in range(2)],
                      d=[K.ps(ph, f"pd{i}", [128, 512], F32) for i in range(2)])
            psA = K.ps(ph, "psA", [128, 512], F32)
            psB = K.ps(ph, "psB", [128, 512], F32)
            layernorm(K, C, l, 0, psA, psB, scr)
            for b in range(5):
                modulate(K, C, l, xT, hT, b, SH2, SC2, lambda d, b=b: hT.r((d, b)))
            if l % 2 == 0:
                scale_x(K, C)
                i = l // 2
                for f0 in range(0, F_DENSE // 128, 8):
                    nf = min(8, F_DENSE // 128 - f0)
                    srcs = (dr["ffn_w_gate"][i, :, f0 * 128:(f0 + nf) * 128],
                            dr["ffn_w_up"][i, :, f0 * 128:(f0 + nf) * 128],
                            dr["ffn_w_down"][i, f0 * 128:(f0 + nf) * 128, :])
                    ffn_group(K, C, l, hT, bufA, W, srcs, nf, ps)
            else:
                ps["rA"], ps["rB"] = psA, psB
                i = l // 2
                srcs0 = (dr["moe_w_gate"][i, 0], dr["moe_w_up"][i, 0], dr["moe_w_down"][i, 0])
                interleave([moe_routing(K, C, dr, l, ph, ps), ffn_gateup(K, C, l, hT, bufA, W, srcs0, 8, ps)],
                           ratio=[6, 1])
                combT = C["_combT"]
                scale_x(K, C)
                combB = K.sb(ph, "combB", [128, NT], F32)
                sele = K.sb(ph, "sele", [8, 128], F32)
                i = l // 2
                for e_ in range(8):
                    K.memset(sele.ap(), 0.0, [sele.r()])
                    K.P.op("dve", lambda e, e_=e_: e.memset(sele.ap()[e_:e_ + 1, :], 1.0), reads=[sele.r()],
                           writes=[sele.r()]) if False else K.ts(
                        sele.ap(), C["identf"].ap()[0:8, e_:e_ + 1].to_broadcast([8, 128]), 1.0, ALU.mult,
                        [C["identf"].r()], [sele.r()])
                    for b, (c0, n) in enumerate(BLOCKS):
                        pb = ps["d"][b % 2]
                        K.mm(pb.ap()[:, :n], sele.ap(), combT.ap()[:, c0:c0 + n], [sele.r(), combT.r()],
                             [pb.r()])
                        K.cp(combB.ap()[:, c0:c0 + n], pb.ap()[:, :n], [pb.r()], [combB.r(b)], eng="act")
                    srcs = (dr["moe_w_gate"][i, e_], dr["moe_w_up"][i, e_], dr["moe_w_down"][i, e_])
                    if e_ > 0:
                        for _ in ffn_gateup(K, C, l, hT, bufA, W, srcs, 8, ps):
                            pass
                    ffn_down(K, C, l, bufA, W, srcs, 8, ps, comb=combB)
            layernorm(K, C, l, 2, psA, psB, scr)
            P.barrier()


def make_in_maps(inp):
    g = lambda k: np.ascontiguousarray(np.asarray(inp[k], dtype=np.float32))
    xp, xs, cp, cs = g("x_prompt"), g("x_sample"), g("c_prompt"), g("c_sample")
    st = {k: g(k) for k in ["state_ssd", "state_ssd_conv", "state_rwkv", "state_rwkv_shift", "state_gla"]}
    wts = {k: g(k) for k in W_SHAPES}
    maps = []
    for c in range(NCORES):
        sl = slice(c * NS, (c + 1) * NS)
        m = dict(wts)
        m["xp"] = xp[c]
        m["xs"] = np.ascontiguousarray(xs[sl, 0, :])
        m["cc"] = np.ascontiguousarray(np.concatenate([cp[c:c + 1], cs[sl]], axis=0))
        m["st_ssd"] = np.ascontiguousarray(st["state_ssd"][:, sl])
        m["st_conv"] = np.ascontiguousarray(st["state_ssd_conv"][:, sl])
        m["st_rwkv"] = np.ascontiguousarray(st["state_rwkv"][:, sl])
        m["st_shift"] = np.ascontiguousarray(st["state_rwkv_shift"][:, sl])
        m["st_gla"] = np.ascontiguousarray(st["state_gla"][:, sl])
        maps.append(m)
    return maps


_NC_CACHE = {}


def gather(results):
    R = lambda k: [np.asarray(r[k], dtype=np.float32) for r in results]
    y_p = np.stack(R("y_p"), axis=0)
    y_s = np.concatenate(R("y_s"), axis=0)[:, None, :]
    outs = [y_p, y_s]
    for k in ["p_ssd", "p_conv", "p_rwkv", "p_shift", "p_gla"]:
        outs.append(np.stack(R(k), axis=1))
    for k in ["s_ssd", "s_conv", "s_rwkv", "s_shift", "s_gla"]:
        outs.append(np.concatenate(R(k), axis=1))
    return tuple(np.ascontiguousarray(o) for o in outs)


def kernel(**inputs):
    if "nc" not in _NC_CACHE:
        _NC_CACHE["nc"] = build()
    res = run_bass_kernel_spmd(_NC_CACHE["nc"], make_in_maps(inputs), core_ids=list(range(NCORES)))
    return gather(res.results)


def bc(ap, axis, shape):
    return ap.unsqueeze(axis).to_broadcast(list(shape))


def sigmoid_chain(K, ap, res):
    K.act(ap, ap, AF.Ln, res, res, bias=1.0)
    K.act(ap, ap, AF.Exp, res, res, scale=-1.0)


def rsqrt_(K, ap, res, scale, eps):
    K.act(ap, ap, AF.Ln, res, res, scale=scale, bias=eps)
    K.act(ap, ap, AF.Exp, res, res, scale=-0.5)


def softplus_(K, x, tmp, r, n):
    xa, ta = x[0], tmp[0]
    K.act(ta, xa, AF.Abs, [x[1]], [tmp[1]])
    K.act(ta, ta, AF.Exp, [tmp[1]], [tmp[1]], scale=-1.0)
    K.act(ta, ta, AF.Ln, [tmp[1]], [tmp[1]], bias=1.0)
    K.ts(xa, xa, 0.0, ALU.max, [x[1]], [x[1]])
    K.tt(xa, xa, ta, ALU.add, [x[1], tmp[1]], [x[1]])


def make_hc(K, C, l, hc, c):
    xT, mod = C["xT"], C["modT"][l]
    import os
    if os.environ.get("HC_POOL", "1") == "1":
        tmp = C["hc_tmp"]
        xs = xT.ap()[:, :, c * 128:(c + 1) * 128]
        K.tt(tmp.ap(), xs, bc(mod.ap()[:, SC1:SC1 + 8, 0], 2, [128, KT, 128]), ALU.mult,
             xr(xT, c // 4) + [mod.r()], [tmp.r()], eng="pool")
        K.tt(hc.ap(), tmp.ap(), bc(mod.ap()[:, SH1:SH1 + 8, 0], 2, [128, KT, 128]), ALU.add,
             [tmp.r(), mod.r()], [hc.r()], eng="pool")
        return
    for d in range(KT):
        K.act(hc.ap()[:, d, :], xT.ap()[:, d, c * 128:(c + 1) * 128], AF.Identity,
              [xT.r((d, c // 4)), mod.r()], [hc.r()],
              scale=mod.ap()[:, SC1 + d, 0:1], bias=mod.ap()[:, SH1 + d, 0:1])


def mixers(K, dr, C, l, yT, dbg_out):
    P = K.P
    xT, mod = C["xT"], C["modT"][l]
    en = C.get("enable", ("ssd", "rwkv", "gla"))
    with contextlib.ExitStack() as mx:
        hc = [K.sb(mx, f"hc{i}", [128, KT, 128], BF16) for i in range(2)]
        hs = K.sb(mx, "hs", [128, KT, NS], BF16)
        C["hc"], C["hs"] = hc, hs
        C["hc_tmp"] = K.sb(mx, "hc_tmp", [128, KT, 128], F32)
        modulate(K, C, l, xT, _Shift(hs, 2048), 4, SH1, SC1, lambda d: hs.r())
        for name, tiles in (("ssd", range(0, 4)), ("rwkv", range(4, 6)), ("gla", range(6, 8))):
            if name not in en:
                for d in tiles:
                    for b, (c0, n) in enumerate(BLOCKS):
                        K.memset(yT.ap()[:, d, c0:c0 + n], 0.0, [yT.r((d, b))])
        if "ssd" in en and "gla" in en:
            ssd_gla_phase(K, dr, C, l, yT, dbg_out)
            P.barrier()
        else:
            if "ssd" in en:
                ssd_phase(K, dr, C, l, yT, dbg_out)
                P.barrier()
            if "gla" in en:
                gla_phase(K, dr, C, l, yT, dbg_out)
                P.barrier()
        if "rwkv" in en:
            rwkv_phase(K, dr, C, l, yT, dbg_out)
            P.barrier()


class _Shift:
    def __init__(self, t, off):
        self.t, self.off = t, off

    def ap(self):
        return _ShiftAP(self.t.ap(), self.off)

    def r(self, key=0):
        return self.t.r()


class _ShiftAP:
    def __init__(self, ap, off):
        self._ap, self.off = ap, off

    def __getitem__(self, key):
        p, d, s = key
        return self._ap[p, d, slice(s.start - self.off, s.stop - self.off)]


def ssd_phase(K, dr, C, l, yT, dbg_out):
    P = K.P
    nc = K.nc
    identb, identf, maskU, maskSL, ones1 = C["identb"], C["identf"], C["maskU"], C["maskSL"], C["ones1"]
    hc, hs = C["hc"], C["hs"]
    with contextlib.ExitStack() as ph:
        win = K.sb(ph, "win_ssd", [128, KT, 1288], BF16)
        K.dma(win.ap(), dr["w_in"][l, :, 0:1288].rearrange("(k p) n -> p k n", p=128), w=[win.r()], q="pool")
        convw = K.sb(ph, "convw", [128, 6, 4], F32)
        convb = K.sb(ph, "convb", [128, 6], F32)
        for i in range(4):
            K.dma(convw.ap()[:, :, i], dr["ssd_conv_w"][l, i].rearrange("(t p) -> p t", p=128), w=[convw.r()])
        K.dma(convb.ap(), dr["ssd_conv_b"][l].rearrange("(t p) -> p t", p=128), w=[convb.r()])
        normg = K.sb(ph, "normg", [128, 4], F32)
        K.dma(normg.ap(), dr["ssd_norm_g"][l].rearrange("(t p) -> p t", p=128), w=[normg.r()])
        dtbB = K.sb(ph, "dtbB", [128, 8], F32)
        aB = K.sb(ph, "aB", [128, 8], F32)
        dB = K.sb(ph, "dB", [128, 8], F32)
        K.dma(dtbB.ap(), dr["ssd_dt_bias"][l:l + 1, :].to_broadcast([128, 8]), w=[dtbB.r()])
        K.dma(aB.ap(), dr["ssd_a_log"][l:l + 1, :].to_broadcast([128, 8]), w=[aB.r()])
        K.dma(dB.ap(), dr["ssd_d"][l:l + 1, :].to_broadcast([128, 8]), w=[dB.r()])
        K.act(aB.ap(), aB.ap(), AF.Exp, [aB.r()], [aB.r()])
        K.ts(aB.ap(), aB.ap(), -1.0, ALU.mult, [aB.r()], [aB.r()])
        import os
        if os.environ.get("SKIP_SSD_PROMPT") != "1":
            ssd_prompt(K, dr, C, l, yT, win, convw, convb, normg, dtbB, aB, dB, dbg_out)
        P.barrier()
        if os.environ.get("SKIP_SSD_SAMPLE") != "1":
            ssd_sample(K, dr, C, l, yT, win, aB, dB, dtbB, dbg_out)


def ssd_prompt(K, dr, C, l, yT, win, convw, convb, normg, dtbB, aB, dB, dbg_out):
    P = K.P
    identb, identf, maskU, maskSL, ones1 = C["identb"], C["identf"], C["maskU"], C["maskSL"], C["ones1"]
    hc, hs = C["hc"], C["hs"]
    with contextlib.ExitStack() as ph:
        XB = [K.sb(ph, f"XB{i}", [128, 6, 131], F32) for i in range(2)]
        XC = [K.sb(ph, f"XC{i}", [128, 6, 128], BF16) for i in range(2)]
        cacc = [K.sb(ph, f"cacc{i}", [128, 128], F32) for i in range(2)]
        sz = K.sb(ph, "sz", [128, 512], F32)
        dtt = K.sb(ph, "dtt", [128, 8], F32)
        dtmp = K.sb(ph, "dtmp", [128, 8], F32)
        dtA = K.sb(ph, "dtA", [128, 8], F32)
        csb = K.sb(ph, "csb", [128, 16], F32)
        e1 = K.sb(ph, "e1", [128, 8], F32)
        el = K.sb(ph, "el", [128, 8], F32)
        tail = K.sb(ph, "tail", [128, 8], F32)
        Rt = K.sb(ph, "Rt", [128, 8, 128], F32)
        dec = K.sb(ph, "dec", [128, 8, 128], F32)
        Gs = K.sb(ph, "Gs", [128, 2, 128], F32)
        Mb = K.sb(ph, "Mb", [128, 8, 128], BF16)
        XT = K.sb(ph, "XT", [128, 640], BF16)
        xD = K.sb(ph, "xD", [128, 512], BF16)
        xw = K.sb(ph, "xw", [128, 512], BF16)
        t1 = K.sb(ph, "t1", [128, 512], F32)
        yn = K.sb(ph, "yn", [128, 512], BF16)
        ss = K.sb(ph, "ss", [128, 1], F32)
        HS32 = K.sb(ph, "HS32", [128, 4, 64], F32)
        HSb = K.sb(ph, "HSb", [128, 4, 64], BF16)
        hsT = K.sb(ph, "hsT", [128, 2, 128], F32)

        ps_x = K.ps(ph, "ps_x", [128, 1024], F32)
        ps_z = K.ps(ph, "ps_z", [128, 512], F32)
        ps_c = K.ps(ph, "ps_c", [128, 512], F32)
        ps_t = K.ps(ph, "ps_t", [128, 1024], BF16)
        ps_y = K.ps(ph, "ps_y", [128, 512], F32)
        ps_i = K.ps(ph, "ps_i", [128, 512], F32)
        ps_h = K.ps(ph, "ps_h", [128, 512], F32)

        K.memset(HS32.ap(), 0.0, [HS32.r()])
        K.memset(HSb.ap(), 0.0, [HSb.r()])
        K.memset(XB[0].ap()[:, :, 0:3], 0.0, [XB[0].r()])

        for c in range(NCH):
            h = hc[c % 2]
            make_hc(K, C, l, h, c)
            xb, xc = XB[c % 2], XC[c % 2]
            for ct in range(6):
                for k in range(KT):
                    K.mm(ps_x.ap()[:, ct * 128:(ct + 1) * 128], win.ap()[:, k, 512 + ct * 128:512 + (ct + 1) * 128],
                         h.ap()[:, k, :], [win.r(), h.r()], [ps_x.r()], start=(k == 0), stop=(k == KT - 1),
                         inc=(k == KT - 1 and ct == 5))
            K.cp(xb.ap()[:, :, 3:131], ps_x.ap()[:, 0:768].rearrange("p (t n) -> p t n", t=6), [ps_x.r()], [xb.r()],
                 eng="act")
            if c + 1 < NCH:
                K.cp(XB[(c + 1) % 2].ap()[:, :, 0:3], xb.ap()[:, :, 128:131], [xb.r()], [XB[(c + 1) % 2].r()])
            for ct in range(6):
                ca = cacc[ct % 2]
                K.ts(ca.ap(), xb.ap()[:, ct, 0:128], convw.ap()[:, ct, 0:1], ALU.mult, [xb.r(), convw.r(), convb.r()],
                     [ca.r()], s2=convb.ap()[:, ct:ct + 1], op1=ALU.add)
                for i in range(1, 4):
                    K.stt(ca.ap(), xb.ap()[:, ct, i:i + 128], convw.ap()[:, ct, i:i + 1], ca.ap(), ALU.mult, ALU.add,
                          [xb.r(), convw.r(), ca.r()], [ca.r()])
                K.act(xc.ap()[:, ct, :], ca.ap(), AF.Silu, [ca.r()], [xc.r()])
            for k in range(KT):
                K.mm(ps_z.ap(), h.ap()[:, k, :], win.ap()[:, k, 0:512], [h.r(), win.r()], [ps_z.r()],
                     start=(k == 0), stop=(k == KT - 1))
            for k in range(KT):
                K.mm(ps_c.ap()[:, 0:8], h.ap()[:, k, :], win.ap()[:, k, 1280:1288], [h.r(), win.r()], [ps_c.r()],
                     start=(k == 0), stop=(k == KT - 1))
            K.act(sz.ap(), ps_z.ap(), AF.Silu, [ps_z.r()], [sz.r()])
            K.tt(dtt.ap(), ps_c.ap()[:, 0:8], dtbB.ap(), ALU.add, [ps_c.r(), dtbB.r()], [dtt.r()])
            softplus_(K, (dtt.ap(), dtt.r()), (dtmp.ap(), dtmp.r()), None, None)
            K.tt(dtA.ap(), dtt.ap(), aB.ap(), ALU.mult, [dtt.r(), aB.r()], [dtA.r()])
            K.mm(ps_c.ap()[:, 8:16], maskU.ap(), dtA.ap(), [maskU.r(), dtA.r()], [ps_c.r()])
            K.mm(ps_c.ap()[:, 16:24], ones1.ap(), dtA.ap(), [ones1.r(), dtA.r()], [ps_c.r()])
            K.tt(Rt.ap(), bc(maskU.ap(), 1, [128, 8, 128]), bc(dtA.ap(), 2, [128, 8, 128]), ALU.mult,
                 [maskU.r(), dtA.r()], [Rt.r()])
            for hf in range(2):
                K.mm(ps_x.ap()[:, hf * 512:(hf + 1) * 512], maskSL.ap(),
                     Rt.ap()[:, hf * 4:(hf + 1) * 4, :].rearrange("p h i -> p (h i)"), [maskSL.r(), Rt.r()],
                     [ps_x.r()])
            K.act(dec.ap().rearrange("p h i -> p (h i)"), ps_x.ap(), AF.Exp, [ps_x.r()], [dec.r()])
            K.cp(csb.ap(), ps_c.ap()[:, 8:24], [ps_c.r()], [csb.r()], eng="act")
            for g in range(2):
                K.mm(ps_c.ap()[:, 256 + g * 128:256 + (g + 1) * 128], xc.ap()[64 * g:64 * g + 64, 4, :],
                     xc.ap()[64 * g:64 * g + 64, 5, :], [xc.r()], [ps_c.r()], self_wait=(g == 1))
            K.tt(Gs.ap(), ps_c.ap()[:, 256:512].rearrange("p (g i) -> p g i", g=2), bc(maskU.ap(), 1, [128, 2, 128]),
                 ALU.mult, [ps_c.r(), maskU.r()], [Gs.r()])
            K.tt(dec.ap().rearrange("p (g r) i -> p g r i", g=2), dec.ap().rearrange("p (g r) i -> p g r i", g=2),
                 bc(Gs.ap(), 2, [128, 2, 4, 128]), ALU.mult, [dec.r(), Gs.r()], [dec.r()])
            K.tt(Mb.ap(), dec.ap(), bc(dtt.ap(), 2, [128, 8, 128]), ALU.mult, [dec.r(), dtt.r()], [Mb.r()])
            for ct in range(5):
                K.tr(ps_t.ap()[:, ct * 128:(ct + 1) * 128], xc.ap()[:, ct, :], identb.ap(), [xc.r(), identb.r()],
                     [ps_t.r()], inc=(ct == 4))
            K.cp(XT.ap(), ps_t.ap()[:, 0:640], [ps_t.r()], [XT.r()], eng="act")
            K.tt(xD.ap().rearrange("p (h q) -> p h q", h=8), XT.ap()[:, 0:512].rearrange("p (h q) -> p h q", h=8),
                 bc(dB.ap(), 2, [128, 8, 64]), ALU.mult, [XT.r(), dB.r()], [xD.r()])
            K.mm(ps_y.ap(), identb.ap(), xD.ap(), [identb.r(), xD.r()], [ps_y.r()], start=True, stop=False)
            for hh in range(8):
                K.mm(ps_y.ap()[:, hh * 64:(hh + 1) * 64], Mb.ap()[:, hh, :], XT.ap()[:, hh * 64:(hh + 1) * 64],
                     [Mb.r(), XT.r()], [ps_y.r()], start=False, stop=(hh == 7))
            for g in range(2):
                K.mm(ps_i.ap()[:, g * 256:(g + 1) * 256], xc.ap()[64 * g:64 * g + 64, 5, :],
                     HSb.ap()[64 * g:64 * g + 64, :, :].rearrange("p h q -> p (h q)"), [xc.r(), HSb.r()], [ps_i.r()],
                     self_wait=(g == 1))
            K.act(e1.ap(), csb.ap()[:, 0:8], AF.Exp, [csb.r()], [e1.r()])
            K.tt(t1.ap().rearrange("p (h q) -> p h q", h=8), ps_i.ap().rearrange("p (h q) -> p h q", h=8),
                 bc(e1.ap(), 2, [128, 8, 64]), ALU.mult, [ps_i.r(), e1.r()], [t1.r()])
            K.tt(t1.ap(), t1.ap(), ps_y.ap(), ALU.add, [t1.r(), ps_y.r()], [t1.r()])
            ssd_epilogue(K, C, t1, sz, ss, yn, 128)
            for q in range(4):
                K.tr(ps_t.ap()[:, q * 128:(q + 1) * 128], yn.ap()[:, q * 128:(q + 1) * 128], identb.ap(),
                     [yn.r(), identb.r()], [ps_t.r()], inc=(q == 3))
            K.tt(yT.ap()[:, 0:4, c * 128:(c + 1) * 128], ps_t.ap()[:, 0:512].rearrange("p (t n) -> p t n", t=4),
                 bc(normg.ap(), 2, [128, 4, 128]), ALU.mult, [ps_t.r(), normg.r()], xr(yT, c // 4, range(4)))
            K.act(el.ap(), csb.ap()[:, 8:16], AF.Exp, [csb.r()], [el.r()])
            K.tt(tail.ap(), csb.ap()[:, 8:16], csb.ap()[:, 0:8], ALU.subtract, [csb.r()], [tail.r()])
            K.act(tail.ap(), tail.ap(), AF.Exp, [tail.r()], [tail.r()])
            K.tt(tail.ap(), tail.ap(), dtt.ap(), ALU.mult, [tail.r(), dtt.r()], [tail.r()])
            K.tt(xw.ap().rearrange("p (h q) -> p h q", h=8), XT.ap()[:, 0:512].rearrange("p (h q) -> p h q", h=8),
                 bc(tail.ap(), 2, [128, 8, 64]), ALU.mult, [XT.r(), tail.r()], [xw.r()])
            K.mm(ps_h.ap(), XT.ap()[:, 512:640], xw.ap(), [XT.r(), xw.r()], [ps_h.r()])
            for g in range(2):
                sl = slice(64 * g, 64 * g + 64)
                K.tt(HS32.ap()[sl], HS32.ap()[sl], bc(el.ap()[sl, 4 * g:4 * g + 4], 2, [64, 4, 64]), ALU.mult,
                     [HS32.r(), el.r()], [HS32.r()])
                K.tt(HS32.ap()[sl], HS32.ap()[sl],
                     ps_h.ap()[sl, 256 * g:256 * g + 256].rearrange("p (h q) -> p h q", h=4), ALU.add,
                     [HS32.r(), ps_h.r()], [HS32.r()])
            K.cp(HSb.ap(), HS32.ap(), [HS32.r()], [HSb.r()], eng="act")

        xb = XB[(NCH - 1) % 2]
        for i in range(3):
            K.dma(dr["p_conv"][l, i].rearrange("(t p) -> p t", p=128), xb.ap()[:, :, 128 + i], r=[xb.r()])
        for q in range(2):
            K.tr(ps_y.ap()[:, q * 128:(q + 1) * 128], HS32.ap().rearrange("p h q -> p (h q)")[:, q * 128:(q + 1) * 128],
                 identf.ap(), [HS32.r(), identf.r()], [ps_y.r()], inc=(q == 1))
        K.cp(hsT.ap(), ps_y.ap()[:, 0:256].rearrange("p (q n) -> p q n", q=2), [ps_y.r()], [hsT.r()])
        for g in range(2):
            for q in range(2):
                K.dma(dr["p_ssd"][l, 4 * g + 2 * q:4 * g + 2 * q + 2].rearrange("h p n -> (h p) n"),
                      hsT.ap()[:, q, 64 * g:64 * g + 64], r=[hsT.r()])
        P.barrier()


def ssd_epilogue(K, C, y, sz, ss, yn, n):
    K.tt(y.ap()[0:n], y.ap()[0:n], sz.ap()[0:n], ALU.mult, [y.r(), sz.r()], [y.r()])
    K.act(yn.ap()[0:n], y.ap()[0:n], AF.Square, [y.r()], [yn.r(), ss.r()], accum_out=ss.ap()[0:n])
    rsqrt_(K, ss.ap()[0:n], [ss.r()], 1.0 / 512, RMS_EPS)
    K.ts(yn.ap()[0:n], y.ap()[0:n], ss.ap()[0:n], ALU.mult, [y.r(), ss.r()], [yn.r()])


def ssd_gla_phase(K, dr, C, l, yT, dbg_out):
    P = K.P
    identb, identf, maskU, maskSL, ones1 = C["identb"], C["identf"], C["maskU"], C["maskSL"], C["ones1"]
    hc, hs = C["hc"], C["hs"]
    G0 = OFF["gq"]
    with contextlib.ExitStack() as ph0:
        win = K.sb(ph0, "win_ssd", [128, KT, 1288], BF16)
        K.dma(win.ap(), dr["w_in"][l, :, 0:1288].rearrange("(k p) n -> p k n", p=128), w=[win.r()], q="pool")
        convw = K.sb(ph0, "convw", [128, 6, 4], F32)
        convb = K.sb(ph0, "convb", [128, 6], F32)
        for i in range(4):
            K.dma(convw.ap()[:, :, i], dr["ssd_conv_w"][l, i].rearrange("(t p) -> p t", p=128), w=[convw.r()])
        K.dma(convb.ap(), dr["ssd_conv_b"][l].rearrange("(t p) -> p t", p=128), w=[convb.r()])
        normg = K.sb(ph0, "normg", [128, 4], F32)
        K.dma(normg.ap(), dr["ssd_norm_g"][l].rearrange("(t p) -> p t", p=128), w=[normg.r()])
        dtbB = K.sb(ph0, "dtbB", [128, 8], F32)
        aB = K.sb(ph0, "aB", [128, 8], F32)
        dB = K.sb(ph0, "dB", [128, 8], F32)
        K.dma(dtbB.ap(), dr["ssd_dt_bias"][l:l + 1, :].to_broadcast([128, 8]), w=[dtbB.r()])
        K.dma(aB.ap(), dr["ssd_a_log"][l:l + 1, :].to_broadcast([128, 8]), w=[aB.r()])
        K.dma(dB.ap(), dr["ssd_d"][l:l + 1, :].to_broadcast([128, 8]), w=[dB.r()])
        K.act(aB.ap(), aB.ap(), AF.Exp, [aB.r()], [aB.r()])
        K.ts(aB.ap(), aB.ap(), -1.0, ALU.mult, [aB.r()], [aB.r()])
        wing = K.sb(ph0, "win_gla", [128, KT, 784], BF16)
        K.dma(wing.ap(), dr["w_in"][l, :, G0:G0 + 784].rearrange("(k p) n -> p k n", p=128), w=[wing.r()], q="pool")
        wgk2 = K.sb(ph0, "wgk2", [16, 128], BF16)
        K.dma(wgk2.ap(), dr["gla_w_gk2"][l], w=[wgk2.r()], q="pool")
        bgkB = K.sb(ph0, "bgkB", [128, 128], F32)
        K.dma(bgkB.ap(), dr["gla_b_gk"][l:l + 1, :].to_broadcast([128, 128]), w=[bgkB.r()])
        gcol = K.sb(ph0, "gcol", [128, 1], F32)
        for t in range(2):
            K.dma(gcol.ap()[64 * t:64 * t + 64, :], dr["gla_norm_g"][l].rearrange("(e o) -> e o", o=1), w=[gcol.r()])
        with contextlib.ExitStack() as ph:
            BM = K.sb(ph, "BM", [128, 256], F32)
            hm = K.sb(ph, "hm", [128, 4], F32)
            K.memset(BM.ap(), 1.0, [BM.r()], eng="pool")
            K.memset(hm.ap(), 1.0, [hm.r()], eng="pool")
            for hh in range(4):
                for (t, sl, n) in ((BM, slice(64 * hh, 64 * hh + 64), 64), (hm, slice(hh, hh + 1), 1)):
                    ap = t.ap()[:, sl]
                    K.P.op("pool", lambda e, ap=ap, n=n, hh=hh: e.affine_select(
                        out=ap, in_=ap, pattern=[[0, n]], compare_op=ALU.is_ge, fill=0.0, base=-32 * hh,
                        channel_multiplier=1), reads=[t.r()], writes=[t.r()])
                    K.P.op("pool", lambda e, ap=ap, n=n, hh=hh: e.affine_select(
                        out=ap, in_=ap, pattern=[[0, n]], compare_op=ALU.is_gt, fill=0.0, base=32 * hh + 32,
                        channel_multiplier=-1), reads=[t.r()], writes=[t.r()])
            PX = [K.ps(ph, f"PX{i}", [128, 512], F32) for i in range(2)]
            PZ = K.ps(ph, "PZ", [128, 512], F32)
            PC = K.ps(ph, "PC", [128, 512], F32)
            PF = K.ps(ph, "PF", [128, 512], F32)
            PV = K.ps(ph, "PV", [128, 512], F32)
            PL = K.ps(ph, "PL", [128, 512], F32)
            PT = K.ps(ph, "PT", [128, 1024], BF16)
            XB = [K.sb(ph, f"XB{i}", [128, 6, 131], BF16) for i in range(2)]
            DW = K.sb(ph, "DW", [128, 6, 4, 128], BF16)
            negb = K.sb(ph, "negb", [128, 6], F32)
            e6 = K.sb(ph, "e6", [128, 6, 128], F32)
            xlast = K.sb(ph, "xlast", [128, 6, 3], F32)
            K.ts(negb.ap(), convb.ap(), -1.0, ALU.mult, [convb.r()], [negb.r()])
            for ct in range(6):
                for i in range(4):
                    K.ts(DW.ap()[:, ct, i, :], identf.ap(), convw.ap()[:, ct, i:i + 1], ALU.mult,
                         [identf.r(), convw.r()], [DW.r()])
            XC = [K.sb(ph, f"XC{i}", [128, 6, 128], BF16) for i in range(2)]
            sz = K.sb(ph, "sz", [128, 512], F32)
            dtt = K.sb(ph, "dtt", [128, 8], F32)
            dtmp = K.sb(ph, "dtmp", [128, 8], F32)
            dtA = K.sb(ph, "dtA", [128, 8], F32)
            csb = K.sb(ph, "csb", [128, 16], F32)
            e1 = K.sb(ph, "e1", [128, 8], F32)
            el = K.sb(ph, "el", [128, 8], F32)
            tail = K.sb(ph, "tail", [128, 8], F32)
            Rt = K.sb(ph, "Rt", [128, 8, 128], F32)
            dec = K.sb(ph, "dec", [128, 8, 128], F32)
            Gs = K.sb(ph, "Gs", [128, 2, 128], F32)
            Mb = K.sb(ph, "Mb", [128, 8, 128], BF16)
            XT = K.sb(ph, "XT", [128, 640], BF16)
            xD = K.sb(ph, "xD", [128, 512], BF16)
            xw = K.sb(ph, "xw", [128, 512], BF16)
            t1 = K.sb(ph, "t1", [128, 512], F32)
            yn = K.sb(ph, "yn", [128, 512], BF16)
            ss = K.sb(ph, "ss", [128, 1], F32)
            HS32 = K.sb(ph, "HS32", [128, 4, 64], F32)
            HSb = K.sb(ph, "HSb", [128, 4, 64], BF16)
            glo = K.sb(ph, "glo", [16, 128], BF16)
            lg = K.sb(ph, "lg", [128, 128], F32)
            lgt = K.sb(ph, "lgt", [128, 128], F32)
            Eq = K.sb(ph, "Eq", [128, 128], F32)
            Ek = K.sb(ph, "Ek", [128, 128], F32)
            Ekt = K.sb(ph, "Ekt", [128, 128], F32)
            qt = K.sb(ph, "qt", [128, 128], BF16)
            kf = K.sb(ph, "kf", [128, 128], F32)
            km = K.sb(ph, "km", [128, 4, 128], BF16)
            ktm = K.sb(ph, "ktm", [128, 128], BF16)
            vtm = K.sb(ph, "vtm", [128, 256], BF16)
            sgg = K.sb(ph, "sgg", [128, 256], F32)
            A = K.sb(ph, "A", [128, 4, 128], BF16)
            osq = K.sb(ph, "osq", [128, 256], F32)
            ms = K.sb(ph, "ms", [128, 4], F32)
            on = K.sb(ph, "on", [128, 256], F32)
            onb = K.sb(ph, "onb", [128, 256], BF16)
            tmpS = K.sb(ph, "tmpS", [128, 256], F32)
            S32 = K.sb(ph, "S32", [128, 256], F32)
            Sb = K.sb(ph, "Sb", [128, 256], BF16)

            K.memset(HS32.ap(), 0.0, [HS32.r()])
            K.memset(HSb.ap(), 0.0, [HSb.r()])
            K.memset(XB[0].ap()[:, :, 0:3], 0.0, [XB[0].r()])
            K.memset(S32.ap(), 0.0, [S32.r()])
            K.memset(Sb.ap(), 0.0, [Sb.r()])

            def ssd_body(c):
                h = hc[c % 2]
                xb, xc = XB[c % 2], XC[c % 2]
                def chain_a():
                    for ct in range(6):
                        px = PX[0] if ct < 4 else PX[1]
                        cc = ct if ct < 4 else ct - 4
                        for k in range(KT):
                            K.mm(px.ap()[:, cc * 128:(cc + 1) * 128], win.ap()[:, k, 512 + ct * 128:512 + (ct + 1) * 128],
                                 h.ap()[:, k, :], [win.r(), h.r()], [px.r()], start=(k == 0), stop=(k == KT - 1))
                        yield
                    K.cp(xb.ap()[:, 0:4, 3:131], PX[0].ap().rearrange("p (t n) -> p t n", t=4), [PX[0].r()], [xb.r()],
                         eng="act")
                    yield
                    K.cp(xb.ap()[:, 4:6, 3:131], PX[1].ap()[:, 0:256].rearrange("p (t n) -> p t n", t=2), [PX[1].r()],
                         [xb.r()], eng="act")
                    yield
                    if c + 1 < NCH:
                        K.cp(XB[(c + 1) % 2].ap()[:, :, 0:3], xb.ap()[:, :, 128:131], [xb.r()], [XB[(c + 1) % 2].r()])
                    if c == NCH - 1:
                        K.cp(xlast.ap()[:, 0:4, :], PX[0].ap().rearrange("p (t n) -> p t n", t=4)[:, :, 125:128],
                             [PX[0].r()], [xlast.r()])
                        K.cp(xlast.ap()[:, 4:6, :], PX[1].ap()[:, 0:256].rearrange("p (t n) -> p t n", t=2)[:, :, 125:128],
                             [PX[1].r()], [xlast.r()])
                    for ct in range(6):
                        px = PX[0] if ct < 4 else PX[1]
                        cc = ct if ct < 4 else ct - 4
                        for i in range(4):
                            K.mm(px.ap()[:, cc * 128:(cc + 1) * 128], DW.ap()[:, ct, i, :], xb.ap()[:, ct, i:i + 128],
                                 [DW.r(), xb.r()], [px.r()], start=(i == 0), stop=(i == 3))
                        yield
                    for ct in range(6):
                        px = PX[0] if ct < 4 else PX[1]
                        cc = ct if ct < 4 else ct - 4
                        K.act(e6.ap()[:, ct, :], px.ap()[:, cc * 128:(cc + 1) * 128], AF.Exp, [px.r(), negb.r()], [e6.r()],
                              scale=-1.0, bias=negb.ap()[:, ct:ct + 1])
                        yield
                    sigmoid_chain(K, e6.ap(), [e6.r()])
                    yield
                    for ct in range(6):
                        px = PX[0] if ct < 4 else PX[1]
                        cc = ct if ct < 4 else ct - 4
                        K.stt(xc.ap()[:, ct, :], px.ap()[:, cc * 128:(cc + 1) * 128], convb.ap()[:, ct:ct + 1], e6.ap()[:, ct, :],
                              ALU.add, ALU.mult, [px.r(), convb.r(), e6.r()], [xc.r()])
                        yield
                    for g in range(2):
                        K.mm(PC.ap()[:, 256 + g * 128:256 + (g + 1) * 128], xc.ap()[64 * g:64 * g + 64, 4, :],
                             xc.ap()[64 * g:64 * g + 64, 5, :], [xc.r()], [PC.r()], self_wait=(g == 1))
                    yield
                    K.tt(Gs.ap(), PC.ap()[:, 256:512].rearrange("p (g i) -> p g i", g=2), bc(maskU.ap(), 1, [128, 2, 128]),
                         ALU.mult, [PC.r(), maskU.r()], [Gs.r()])
                    yield
                    for ct in range(5):
                        K.tr(PT.ap()[:, ct * 128:(ct + 1) * 128], xc.ap()[:, ct, :], identb.ap(), [xc.r(), identb.r()],
                             [PT.r()], inc=(ct == 4))
                    yield
                    K.cp(XT.ap(), PT.ap()[:, 0:640], [PT.r()], [XT.r()], eng="act")
                    yield

                def chain_b():
                    for k in range(KT):
                        K.mm(PZ.ap(), h.ap()[:, k, :], win.ap()[:, k, 0:512], [h.r(), win.r()], [PZ.r()],
                             start=(k == 0), stop=(k == KT - 1))
                    for k in range(KT):
                        K.mm(PC.ap()[:, 0:8], h.ap()[:, k, :], win.ap()[:, k, 1280:1288], [h.r(), win.r()], [PC.r()],
                             start=(k == 0), stop=(k == KT - 1))
                    yield
                    K.act(sz.ap(), PZ.ap(), AF.Exp, [PZ.r()], [sz.r()], scale=-1.0)
                    yield
                    sigmoid_chain(K, sz.ap(), [sz.r()])
                    yield
                    K.tt(sz.ap(), sz.ap(), PZ.ap(), ALU.mult, [sz.r(), PZ.r()], [sz.r()])
                    yield
                    K.tt(dtt.ap(), PC.ap()[:, 0:8], dtbB.ap(), ALU.add, [PC.r(), dtbB.r()], [dtt.r()])
                    yield
                    softplus_(K, (dtt.ap(), dtt.r()), (dtmp.ap(), dtmp.r()), None, None)
                    yield
                    K.tt(dtA.ap(), dtt.ap(), aB.ap(), ALU.mult, [dtt.r(), aB.r()], [dtA.r()])
                    yield
                    K.mm(PC.ap()[:, 8:16], maskU.ap(), dtA.ap(), [maskU.r(), dtA.r()], [PC.r()])
                    K.mm(PC.ap()[:, 16:24], ones1.ap(), dtA.ap(), [ones1.r(), dtA.r()], [PC.r()])
                    yield
                    K.tt(Rt.ap(), bc(maskU.ap(), 1, [128, 8, 128]), bc(dtA.ap(), 2, [128, 8, 128]), ALU.mult,
                         [maskU.r(), dtA.r()], [Rt.r()])
                    yield
                    for hf in range(2):
                        K.mm(PZ.ap(), maskSL.ap(), Rt.ap()[:, hf * 4:(hf + 1) * 4, :].rearrange("p h i -> p (h i)"),
                             [maskSL.r(), Rt.r()], [PZ.r()])
                        yield
                        K.act(dec.ap()[:, hf * 4:(hf + 1) * 4, :].rearrange("p h i -> p (h i)"), PZ.ap(), AF.Exp,
                              [PZ.r()], [dec.r()])
                        yield
                    K.cp(csb.ap(), PC.ap()[:, 8:24], [PC.r()], [csb.r()], eng="act")
                    yield

                yield from interleave_gen([chain_a(), chain_b()], [2, 1])
                K.tt(dec.ap().rearrange("p (g r) i -> p g r i", g=2), dec.ap().rearrange("p (g r) i -> p g r i", g=2),
                     bc(Gs.ap(), 2, [128, 2, 4, 128]), ALU.mult, [dec.r(), Gs.r()], [dec.r()])
                yield
                K.tt(Mb.ap(), dec.ap(), bc(dtt.ap(), 2, [128, 8, 128]), ALU.mult, [dec.r(), dtt.r()], [Mb.r()])
                yield
                K.tt(xD.ap().rearrange("p (h q) -> p h q", h=8), XT.ap()[:, 0:512].rearrange("p (h q) -> p h q", h=8),
                     bc(dB.ap(), 2, [128, 8, 64]), ALU.mult, [XT.r(), dB.r()], [xD.r()])
                yield
                K.mm(PZ.ap(), identb.ap(), xD.ap(), [identb.r(), xD.r()], [PZ.r()], start=True, stop=False)
                for hh in range(8):
                    K.mm(PZ.ap()[:, hh * 64:(hh + 1) * 64], Mb.ap()[:, hh, :], XT.ap()[:, hh * 64:(hh + 1) * 64],
                         [Mb.r(), XT.r()], [PZ.r()], start=False, stop=(hh == 7))
                for g in range(2):
                    K.mm(PC.ap()[:, g * 256:(g + 1) * 256], xc.ap()[64 * g:64 * g + 64, 5, :],
                         HSb.ap()[64 * g:64 * g + 64, :, :].rearrange("p h q -> p (h q)"), [xc.r(), HSb.r()], [PC.r()],
                         self_wait=(g == 1))
                yield
                K.act(e1.ap(), csb.ap()[:, 0:8], AF.Exp, [csb.r()], [e1.r()])
                yield
                K.tt(t1.ap().rearrange("p (h q) -> p h q", h=8), PC.ap().rearrange("p (h q) -> p h q", h=8),
                     bc(e1.ap(), 2, [128, 8, 64]), ALU.mult, [PC.r(), e1.r()], [t1.r()])
                yield
                K.tt(t1.ap(), t1.ap(), PZ.ap(), ALU.add, [t1.r(), PZ.r()], [t1.r()])
                yield
                ssd_epilogue(K, C, t1, sz, ss, yn, 128)
                yield
                for q in range(4):
                    K.tr(PT.ap()[:, q * 128:(q + 1) * 128], yn.ap()[:, q * 128:(q + 1) * 128], identb.ap(),
                         [yn.r(), identb.r()], [PT.r()], inc=(q == 3))
                yield
                K.tt(yT.ap()[:, 0:4, c * 128:(c + 1) * 128], PT.ap()[:, 0:512].rearrange("p (t n) -> p t n", t=4),
                     bc(normg.ap(), 2, [128, 4, 128]), ALU.mult, [PT.r(), normg.r()], xr(yT, c // 4, range(4)))
                yield
                K.act(el.ap(), csb.ap()[:, 8:16], AF.Exp, [csb.r()], [el.r()])
                yield
                K.tt(tail.ap(), csb.ap()[:, 8:16], csb.ap()[:, 0:8], ALU.subtract, [csb.r()], [tail.r()])
                yield
                K.act(tail.ap(), tail.ap(), AF.Exp, [tail.r()], [tail.r()])
                yield
                K.tt(tail.ap(), tail.ap(), dtt.ap(), ALU.mult, [tail.r(), dtt.r()], [tail.r()])
                yield
                K.tt(xw.ap().rearrange("p (h q) -> p h q", h=8), XT.ap()[:, 0:512].rearrange("p (h q) -> p h q", h=8),
                     bc(tail.ap(), 2, [128, 8, 64]), ALU.mult, [XT.r(), tail.r()], [xw.r()])
                yield
                K.mm(PC.ap(), XT.ap()[:, 512:640], xw.ap(), [XT.r(), xw.r()], [PC.r()])
                yield
                for g in range(2):
                    sl = slice(64 * g, 64 * g + 64)
                    K.tt(HS32.ap()[sl], HS32.ap()[sl], bc(el.ap()[sl, 4 * g:4 * g + 4], 2, [64, 4, 64]), ALU.mult,
                         [HS32.r(), el.r()], [HS32.r()])
                    K.tt(HS32.ap()[sl], HS32.ap()[sl],
                         PC.ap()[sl, 256 * g:256 * g + 256].rearrange("p (h q) -> p h q", h=4), ALU.add,
                         [HS32.r(), PC.r()], [HS32.r()])
                    yield
                K.cp(HSb.ap(), HS32.ap(), [HS32.r()], [HSb.r()], eng="act")
                yield

            def gla_body(c):
                h = hc[c % 2]
                for (dst, cols) in ((PF.ap()[:, 0:128], slice(0, 128)), (PF.ap()[:, 128:256], slice(128, 256)),
                                    (PF.ap()[0:16, 256:384], slice(512, 528))):
                    for k in range(KT):
                        K.mm(dst, wing.ap()[:, k, cols], h.ap()[:, k, :], [wing.r(), h.r()], [PF.r()],
                             start=(k == 0), stop=(k == KT - 1))
                    yield
                for (dst, cols, pst) in ((PV.ap()[:, 0:256], slice(256, 512), PV), (PF.ap()[:, 384:512], slice(128, 256), PF),
                                         (PV.ap()[:, 256:512], slice(528, 784), PV)):
                    for k in range(KT):
                        K.mm(dst, h.ap()[:, k, :], wing.ap()[:, k, cols], [wing.r(), h.r()], [pst.r()],
                             start=(k == 0), stop=(k == KT - 1))
                    yield
                K.cp(glo.ap(), PF.ap()[0:16, 256:384], [PF.r()], [glo.r()], eng="act")
                yield
                K.mm(PL.ap()[:, 0:128], glo.ap(), wgk2.ap(), [glo.r(), wgk2.r()], [PL.r()])
                yield
                K.stt(lg.ap(), PL.ap()[:, 0:128], -1.0, bgkB.ap(), ALU.mult, ALU.subtract, [PL.r(), bgkB.r()], [lg.r()])
                yield
                softplus_(K, (lg.ap(), lg.r()), (lgt.ap(), lgt.r()), None, None)
                yield
                K.ts(lg.ap(), lg.ap(), -1.0 / 16.0, ALU.mult, [lg.r()], [lg.r()])
                yield
                K.mm(PL.ap()[:, 128:256], lg.ap(), maskU.ap(), [lg.r(), maskU.r()], [PL.r()])
                K.mm(PL.ap()[:, 256:384], maskU.ap(), lg.ap(), [lg.r(), maskU.r()], [PL.r()])
                yield
                K.act(Eq.ap(), PL.ap()[:, 128:256], AF.Exp, [PL.r()], [Eq.r()])
                yield
                K.act(Ek.ap(), PL.ap()[:, 128:256], AF.Exp, [PL.r()], [Ek.r()], scale=-1.0)
                yield
                K.act(Ekt.ap(), PL.ap()[:, 256:384], AF.Exp, [PL.r()], [Ekt.r()], scale=-1.0)
                yield
                K.stt(qt.ap(), PF.ap()[:, 0:128], 32.0 ** -0.5, Eq.ap(), ALU.mult, ALU.mult, [PF.r(), Eq.r()], [qt.r()])
                yield
                K.tt(kf.ap(), PF.ap()[:, 128:256], Ek.ap(), ALU.mult, [PF.r(), Ek.r()], [kf.r()])
                yield
                K.tt(km.ap(), bc(kf.ap(), 1, [128, 4, 128]), bc(hm.ap(), 2, [128, 4, 128]), ALU.mult, [kf.r(), hm.r()],
                     [km.r()])
                yield
                K.tt(ktm.ap(), PF.ap()[:, 384:512], Ekt.ap(), ALU.mult, [PF.r(), Ekt.r()], [ktm.r()])
                yield
                K.cp(vtm.ap(), PV.ap()[:, 0:256], [PV.r()], [vtm.r()], eng="act")
                yield
                K.act(sgg.ap(), PV.ap()[:, 256:512], AF.Exp, [PV.r()], [sgg.r()], scale=-1.0)
                yield
                sigmoid_chain(K, sgg.ap(), [sgg.r()])
                yield
                K.tt(sgg.ap(), sgg.ap(), PV.ap()[:, 256:512], ALU.mult, [sgg.r(), PV.r()], [sgg.r()])
                yield
                for hh in range(4):
                    K.mm(PL.ap()[:, hh * 128:(hh + 1) * 128], km.ap()[:, hh, :], qt.ap(), [km.r(), qt.r()], [PL.r()],
                         inc=(hh == 3))
                yield
                K.tt(A.ap(), PL.ap().rearrange("p (h i) -> p h i", h=4), bc(maskU.ap(), 1, [128, 4, 128]), ALU.mult,
                     [PL.r(), maskU.r()], [A.r()])
                yield
                K.mm(PL.ap()[:, 0:256], qt.ap(), Sb.ap(), [qt.r(), Sb.r()], [PL.r()], start=True, stop=False)
                for hh in range(4):
                    K.mm(PL.ap()[:, hh * 64:(hh + 1) * 64], A.ap()[:, hh, :], vtm.ap()[:, hh * 64:(hh + 1) * 64],
                         [A.r(), vtm.r()], [PL.r()], start=False, stop=(hh == 3))
                yield
                K.act(osq.ap(), PL.ap()[:, 0:256], AF.Square, [PL.r()], [osq.r()])
                yield
                K.red(ms.ap(), osq.ap().rearrange("p (h e) -> p h e", h=4), [osq.r()], [ms.r()])
                yield
                rsqrt_(K, ms.ap(), [ms.r()], 1.0 / 64, RMS_EPS)
                yield
                K.tt(on.ap().rearrange("p (h e) -> p h e", h=4), PL.ap()[:, 0:256].rearrange("p (h e) -> p h e", h=4),
                     bc(ms.ap(), 2, [128, 4, 64]), ALU.mult, [PL.r(), ms.r()], [on.r()])
                yield
                K.tt(onb.ap(), on.ap(), sgg.ap(), ALU.mult, [on.r(), sgg.r()], [onb.r()])
                yield
                for q in range(2):
                    K.tr(PT.ap()[:, 768 + q * 128:768 + (q + 1) * 128], onb.ap()[:, q * 128:(q + 1) * 128], identb.ap(),
                         [onb.r(), identb.r()], [PT.r()], inc=(q == 1))
                yield
                K.ts(yT.ap()[:, 6:8, c * 128:(c + 1) * 128], PT.ap()[:, 768:1024].rearrange("p (t n) -> p t n", t=2),
                     gcol.ap(), ALU.mult, [PT.r(), gcol.r()], xr(yT, c // 4, range(6, 8)))
                yield
                K.mm(PL.ap()[:, 256:512], ktm.ap(), vtm.ap(), [ktm.r(), vtm.r()], [PL.r()])
                yield
                K.tt(tmpS.ap(), PL.ap()[:, 256:512], BM.ap(), ALU.mult, [PL.r(), BM.r()], [tmpS.r()])
                yield
                K.tt(S32.ap(), S32.ap(), tmpS.ap(), ALU.add, [S32.r(), tmpS.r()], [S32.r()])
                yield
                K.ts(S32.ap(), S32.ap(), Eq.ap()[:, 127:128], ALU.mult, [S32.r(), Eq.r()], [S32.r()])
                yield
                K.cp(Sb.ap(), S32.ap(), [S32.r()], [Sb.r()], eng="act")
                yield

            for c in range(NCH):
                make_hc(K, C, l, hc[c % 2], c)
                interleave([ssd_body(c), gla_body(c)], ratio=[2, 1])

            xb = XB[(NCH - 1) % 2]
            for i in range(3):
                K.dma(dr["p_conv"][l, i].rearrange("(t p) -> p t", p=128), xlast.ap()[:, :, i], r=[xlast.r()])
            hsT = t1
            for q in range(2):
                K.tr(PZ.ap()[:, q * 128:(q + 1) * 128], HS32.ap().rearrange("p h q -> p (h q)")[:, q * 128:(q + 1) * 128],
                     identf.ap(), [HS32.r(), identf.r()], [PZ.r()], inc=(q == 1))
            K.cp(hsT.ap()[:, 0:256], PZ.ap()[:, 0:256], [PZ.r()], [hsT.r()])
            for g in range(2):
                for q in range(2):
                    K.dma(dr["p_ssd"][l, 4 * g + 2 * q:4 * g + 2 * q + 2].rearrange("h p n -> (h p) n"),
                          hsT.ap()[:, q * 128 + 64 * g:q * 128 + 64 * g + 64], r=[hsT.r()])
            for hh in range(4):
                K.dma(dr["p_gla"][l, hh], S32.ap()[32 * hh:32 * hh + 32, 64 * hh:64 * hh + 64], r=[S32.r()])
            P.barrier()
        ssd_sample(K, dr, C, l, yT, win, aB, dB, dtbB, dbg_out)
        gla_sample(K, dr, C, l, yT, wing, wgk2, bgkB)


def dram_scratch(K, name, shape):
    K.uid += 1
    h = K.nc.dram_tensor(f"scr_{name}_{K.uid}", list(shape), F32)
    return Tn(h, name)


def ssd_sample(K, dr, C, l, yT, win, aB, dB, dtbB, dbg_out):
    P = K.P
    hs, identb = C["hs"], C["identb"]
    with contextlib.ExitStack() as ph:
        cs = K.sb(ph, "cs", [NS, 768], F32)
        wB = K.sb(ph, "wB", [NS, 768], F32)
        gB = K.sb(ph, "gB", [NS, 512], F32)
        xbcs = K.sb(ph, "xbcs", [NS, 768], F32)
        acc = K.sb(ph, "acc", [NS, 768], F32)
        tmpc = K.sb(ph, "tmpc", [NS, 768], F32)
        szs = K.sb(ph, "szs", [NS, 512], F32)
        dts = K.sb(ph, "dts", [NS, 8], F32)
        dtm = K.sb(ph, "dtm", [NS, 8], F32)
        rep = K.sb(ph, "rep", [NS, 2, 8, 64], F32)
        pk = K.sb(ph, "pk", [NS, 8, 3], F32)
        Hs = K.sb(ph, "Hs", [128, 64, 64], F32)
        tmpH = K.sb(ph, "tmpH", [128, 32, 64], F32)
        xh = K.sb(ph, "xh", [128, 64], F32)
        BCh = K.sb(ph, "BCh", [128, 2, 64], F32)
        pkh = K.sb(ph, "pkh", [128, 3], F32)
        dA = K.sb(ph, "dA", [128, 1], F32)
        xdt = K.sb(ph, "xdt", [128, 64], F32)
        yh = K.sb(ph, "yh", [128, 64], F32)
        ysm = K.sb(ph, "ysm", [NS, 512], F32)
        yns = K.sb(ph, "yns", [NS, 512], BF16)
        sss = K.sb(ph, "sss", [NS, 1], F32)
        ps_a = K.ps(ph, "pss_a", [128, 512], F32)
        ps_b = K.ps(ph, "pss_b", [128, 512], F32)
        ps_d = K.ps(ph, "pss_d", [128, 512], F32)
        ps_t = K.ps(ph, "pss_t", [128, 1024], BF16)
        sx = dram_scratch(K, "sx", [NS, 512])
        sbc = dram_scratch(K, "sbc", [2, NS, 512])
        spk = dram_scratch(K, "spk", [NS, 24])
        sy = dram_scratch(K, "sy", [NS, 512])

        K.dma(gB.ap(), dr["ssd_norm_g"][l:l + 1, :].to_broadcast([NS, 512]), w=[gB.r()])
        K.dma(Hs.ap().rearrange("p a b -> p (a b)"), dr["st_ssd"][l].rearrange("b h p n -> (b h) (p n)"), w=[Hs.r()])
        for k in range(KT):
            K.mm(ps_a.ap()[0:NS, :], hs.ap()[:, k, :], win.ap()[:, k, 0:512], [hs.r(), win.r()], [ps_a.r()],
                 start=(k == 0), stop=(k == KT - 1))
        for k in range(KT):
            K.mm(ps_b.ap()[0:NS, :], hs.ap()[:, k, :], win.ap()[:, k, 512:1024], [hs.r(), win.r()], [ps_b.r()],
                 start=(k == 0), stop=(k == KT - 1))
        for k in range(KT):
            K.mm(ps_d.ap()[0:NS, 0:264], hs.ap()[:, k, :], win.ap()[:, k, 1024:1288], [hs.r(), win.r()], [ps_d.r()],
                 start=(k == 0), stop=(k == KT - 1))
        K.act(szs.ap(), ps_a.ap()[0:NS, :], AF.Silu, [ps_a.r()], [szs.r()])
        K.cp(xbcs.ap()[:, 0:512], ps_b.ap()[0:NS, :], [ps_b.r()], [xbcs.r()], eng="act")
        K.cp(xbcs.ap()[:, 512:768], ps_d.ap()[0:NS, 0:256], [ps_d.r()], [xbcs.r()], eng="act")
        K.tt(dts.ap(), ps_d.ap()[0:NS, 256:264], dtbB.ap()[0:NS, :], ALU.add, [ps_d.r(), dtbB.r()], [dts.r()])
        softplus_(K, (dts.ap(), dts.r()), (dtm.ap(), dtm.r()), None, None)
        K.dma(wB.ap(), dr["ssd_conv_w"][l, 3:4, :].to_broadcast([NS, 768]), w=[wB.r()])
        K.tt(acc.ap(), xbcs.ap(), wB.ap(), ALU.mult, [xbcs.r(), wB.r()], [acc.r()])
        for i in range(3):
            K.dma(wB.ap(), dr["ssd_conv_w"][l, i:i + 1, :].to_broadcast([NS, 768]), w=[wB.r()])
            K.dma(cs.ap(), dr["st_conv"][l][:, i, :], w=[cs.r()])
            K.tt(tmpc.ap(), cs.ap(), wB.ap(), ALU.mult, [cs.r(), wB.r()], [tmpc.r()])
            K.tt(acc.ap(), acc.ap(), tmpc.ap(), ALU.add, [acc.r(), tmpc.r()], [acc.r()])
        K.dma(wB.ap(), dr["ssd_conv_b"][l:l + 1, :].to_broadcast([NS, 768]), w=[wB.r()])
        K.tt(acc.ap(), acc.ap(), wB.ap(), ALU.add, [acc.r(), wB.r()], [acc.r()])
        K.act(acc.ap(), acc.ap(), AF.Silu, [acc.r()], [acc.r()])
        K.dma(dr["s_conv"][l][:, 0:2, :], dr["st_conv"][l][:, 1:3, :])
        K.dma(dr["s_conv"][l][:, 2, :], xbcs.ap(), r=[xbcs.r()])
        K.dma(sx.ap(), acc.ap()[:, 0:512], r=[acc.r()], w=[sx.r()])
        K.cp(rep.ap().rearrange("p t (g r) n -> p t g r n", g=2),
             bc(acc.ap()[:, 512:768].rearrange("p (t g n) -> p t g n", t=2, g=2), 3, [NS, 2, 2, 4, 64]),
             [acc.r()], [rep.r()])
        K.cp(pk.ap()[:, :, 0], dts.ap(), [dts.r()], [pk.r()])
        K.cp(pk.ap()[:, :, 1], aB.ap()[0:NS, :], [aB.r()], [pk.r()])
        K.cp(pk.ap()[:, :, 2], dB.ap()[0:NS, :], [dB.r()], [pk.r()])
        K.dma(sbc.ap().rearrange("t b x -> b t x"), rep.ap().rearrange("p t h n -> p t (h n)"), r=[rep.r()],
              w=[sbc.r()])
        K.dma(spk.ap(), pk.ap().rearrange("p h q -> p (h q)"), r=[pk.r()], w=[spk.r()])
        K.dma(xh.ap(), sx.ap().rearrange("b (h p) -> (b h) p", h=8), r=[sx.r()], w=[xh.r()])
        for t in range(2):
            K.dma(BCh.ap()[:, t, :], sbc.ap()[t].rearrange("b (h n) -> (b h) n", h=8), r=[sbc.r()],
                  w=[BCh.r()])
        K.dma(pkh.ap(), spk.ap().rearrange("b (h q) -> (b h) q", h=8), r=[spk.r()], w=[pkh.r()])
        K.act(dA.ap(), pkh.ap()[:, 0:1], AF.Exp, [pkh.r()], [dA.r()], scale=pkh.ap()[:, 1:2])
        K.ts(xdt.ap(), xh.ap(), pkh.ap()[:, 0:1], ALU.mult, [xh.r(), pkh.r()], [xdt.r()])
        K.ts(Hs.ap(), Hs.ap(), dA.ap(), ALU.mult, [Hs.r(), dA.r()], [Hs.r()])
        for hf in range(2):
            sl = slice(32 * hf, 32 * hf + 32)
            K.tt(tmpH.ap(), bc(xdt.ap()[:, sl], 2, [128, 32, 64]), bc(BCh.ap()[:, 0, :], 1, [128, 32, 64]), ALU.mult,
                 [xdt.r(), BCh.r()], [tmpH.r()])
            K.tt(Hs.ap()[:, sl, :], Hs.ap()[:, sl, :], tmpH.ap(), ALU.add, [Hs.r(), tmpH.r()], [Hs.r()])
        K.dma(dr["s_ssd"][l].rearrange("b h p n -> (b h) (p n)"), Hs.ap().rearrange("p a b -> p (a b)"), r=[Hs.r()])
        for hf in range(2):
            sl = slice(32 * hf, 32 * hf + 32)
            K.tt(tmpH.ap(), Hs.ap()[:, sl, :], bc(BCh.ap()[:, 1, :], 1, [128, 32, 64]), ALU.mult, [Hs.r(), BCh.r()],
                 [tmpH.r()])
            K.red(yh.ap()[:, sl], tmpH.ap(), [tmpH.r()], [yh.r()])
        K.stt(yh.ap(), xh.ap(), pkh.ap()[:, 2:3], yh.ap(), ALU.mult, ALU.add, [xh.r(), pkh.r(), yh.r()], [yh.r()])
        K.dma(sy.ap().rearrange("b (h p) -> (b h) p", h=8), yh.ap(), r=[yh.r()], w=[sy.r()])
        K.dma(ysm.ap(), sy.ap(), r=[sy.r()], w=[ysm.r()])
        ssd_epilogue(K, C, ysm, szs, sss, yns, NS)
        K.tt(ysm.ap(), ysm.ap(), gB.ap(), ALU.mult, [ysm.r(), gB.r()], [ysm.r()])
        K.ts(yns.ap(), ysm.ap(), sss.ap(), ALU.mult, [ysm.r(), sss.r()], [yns.r()])
        for q in range(4):
            K.tr(ps_t.ap()[:, q * NS:(q + 1) * NS], yns.ap()[:, q * 128:(q + 1) * 128], identb.ap()[0:NS, 0:NS],
                 [yns.r(), identb.r()], [ps_t.r()], inc=(q == 3))
        K.cp(yT.ap()[:, 0:4, T:T + NS], ps_t.ap()[:, 0:4 * NS].rearrange("p (t n) -> p t n", t=4), [ps_t.r()],
             xr(yT, 4, range(4)))
        P.barrier()


def gla_phase(K, dr, C, l, yT, dbg_out):
    P = K.P
    identb, maskU = C["identb"], C["maskU"]
    hc, hs = C["hc"], C["hs"]
    G0 = OFF["gq"]
    with contextlib.ExitStack() as ph:
        win = K.sb(ph, "win_gla", [128, KT, 784], BF16)
        K.dma(win.ap(), dr["w_in"][l, :, G0:G0 + 784].rearrange("(k p) n -> p k n", p=128), w=[win.r()], q="pool")
        wgk2 = K.sb(ph, "wgk2", [16, 128], BF16)
        K.dma(wgk2.ap(), dr["gla_w_gk2"][l], w=[wgk2.r()], q="pool")
        bgkB = K.sb(ph, "bgkB", [128, 128], F32)
        K.dma(bgkB.ap(), dr["gla_b_gk"][l:l + 1, :].to_broadcast([128, 128]), w=[bgkB.r()])
        gcol = K.sb(ph, "gcol", [128, 1], F32)
        for t in range(2):
            K.dma(gcol.ap()[64 * t:64 * t + 64, :], dr["gla_norm_g"][l].rearrange("(e o) -> e o", o=1), w=[gcol.r()])
        BM = K.sb(ph, "BM", [128, 256], F32)
        hm = K.sb(ph, "hm", [128, 4], F32)
        K.memset(BM.ap(), 1.0, [BM.r()], eng="pool")
        K.memset(hm.ap(), 1.0, [hm.r()], eng="pool")
        for hh in range(4):
            for (t, sl, n) in ((BM, slice(64 * hh, 64 * hh + 64), 64), (hm, slice(hh, hh + 1), 1)):
                ap = t.ap()[:, sl]
                K.P.op("pool", lambda e, ap=ap, n=n, hh=hh: e.affine_select(
                    out=ap, in_=ap, pattern=[[0, n]], compare_op=ALU.is_ge, fill=0.0, base=-32 * hh,
                    channel_multiplier=1), reads=[t.r()], writes=[t.r()])
                K.P.op("pool", lambda e, ap=ap, n=n, hh=hh: e.affine_select(
                    out=ap, in_=ap, pattern=[[0, n]], compare_op=ALU.is_gt, fill=0.0, base=32 * hh + 32,
                    channel_multiplier=-1), reads=[t.r()], writes=[t.r()])
        gla_prompt(K, dr, C, l, yT, win, wgk2, bgkB, gcol, BM, hm)
        P.barrier()
        gla_sample(K, dr, C, l, yT, win, wgk2, bgkB)


def gla_prompt(K, dr, C, l, yT, win, wgk2, bgkB, gcol, BM, hm):
    P = K.P
    identb, maskU = C["identb"], C["maskU"]
    hc = C["hc"]
    with contextlib.ExitStack() as ph:
        glo = K.sb(ph, "glo", [16, 128], BF16)
        lg = K.sb(ph, "lg", [128, 128], F32)
        lgt = K.sb(ph, "lgt", [128, 128], F32)
        Eq = K.sb(ph, "Eq", [128, 128], F32)
        Ek = K.sb(ph, "Ek", [128, 128], F32)
        Ekt = K.sb(ph, "Ekt", [128, 128], F32)
        qt = K.sb(ph, "qt", [128, 128], BF16)
        kf = K.sb(ph, "kf", [128, 128], F32)
        km = K.sb(ph, "km", [128, 4, 128], BF16)
        ktm = K.sb(ph, "ktm", [128, 128], BF16)
        vtm = K.sb(ph, "vtm", [128, 256], BF16)
        sgg = K.sb(ph, "sgg", [128, 256], F32)
        A = K.sb(ph, "A", [128, 4, 128], BF16)
        osq = K.sb(ph, "osq", [128, 256], F32)
        ms = K.sb(ph, "ms", [128, 4], F32)
        on = K.sb(ph, "on", [128, 256], F32)
        onb = K.sb(ph, "onb", [128, 256], BF16)
        tmpS = K.sb(ph, "tmpS", [128, 256], F32)
        S32 = K.sb(ph, "S32", [128, 256], F32)
        Sb = K.sb(ph, "Sb", [128, 256], BF16)
        ps_f = K.ps(ph, "psg_f", [128, 512], F32)
        ps_m = K.ps(ph, "psg_m", [128, 512], F32)
        ps_g = K.ps(ph, "psg_g", [128, 512], F32)
        ps_l = K.ps(ph, "psg_l", [128, 512], F32)
        ps_a = K.ps(ph, "psg_a", [128, 512], F32)
        ps_o = K.ps(ph, "psg_o", [128, 512], F32)
        ps_t = K.ps(ph, "psg_t", [128, 1024], BF16)
        K.memset(S32.ap(), 0.0, [S32.r()])
        K.memset(Sb.ap(), 0.0, [Sb.r()])
        for c in range(NCH):
            h = hc[c % 2]
            make_hc(K, C, l, h, c)
            for (dst, cols, M) in ((ps_f.ap()[:, 0:128], slice(0, 128), 128), (ps_f.ap()[:, 128:256], slice(128, 256), 128),
                                   (ps_f.ap()[0:16, 256:384], slice(512, 528), 16)):
                for k in range(KT):
                    K.mm(dst, win.ap()[:, k, cols], h.ap()[:, k, :], [win.r(), h.r()], [ps_f.r()],
                         start=(k == 0), stop=(k == KT - 1))
            for (dst, cols, pst) in ((ps_m.ap()[:, 0:256], slice(256, 512), ps_m), (ps_m.ap()[:, 256:384], slice(128, 256), ps_m),
                                     (ps_g.ap()[:, 0:256], slice(528, 784), ps_g)):
                for k in range(KT):
                    K.mm(dst, h.ap()[:, k, :], win.ap()[:, k, cols], [win.r(), h.r()], [pst.r()],
                         start=(k == 0), stop=(k == KT - 1))
            K.cp(glo.ap(), ps_f.ap()[0:16, 256:384], [ps_f.r()], [glo.r()], eng="act")
            K.mm(ps_l.ap()[:, 0:128], glo.ap(), wgk2.ap(), [glo.r(), wgk2.r()], [ps_l.r()])
            K.stt(lg.ap(), ps_l.ap()[:, 0:128], -1.0, bgkB.ap(), ALU.mult, ALU.subtract, [ps_l.r(), bgkB.r()], [lg.r()])
            softplus_(K, (lg.ap(), lg.r()), (lgt.ap(), lgt.r()), None, None)
            K.ts(lg.ap(), lg.ap(), -1.0 / 16.0, ALU.mult, [lg.r()], [lg.r()])
            K.mm(ps_l.ap()[:, 128:256], lg.ap(), maskU.ap(), [lg.r(), maskU.r()], [ps_l.r()])
            K.mm(ps_l.ap()[:, 256:384], maskU.ap(), lg.ap(), [lg.r(), maskU.r()], [ps_l.r()])
            K.act(Eq.ap(), ps_l.ap()[:, 128:256], AF.Exp, [ps_l.r()], [Eq.r()])
            K.act(Ek.ap(), ps_l.ap()[:, 128:256], AF.Exp, [ps_l.r()], [Ek.r()], scale=-1.0)
            K.act(Ekt.ap(), ps_l.ap()[:, 256:384], AF.Exp, [ps_l.r()], [Ekt.r()], scale=-1.0)
            K.stt(qt.ap(), ps_f.ap()[:, 0:128], 32.0 ** -0.5, Eq.ap(), ALU.mult, ALU.mult, [ps_f.r(), Eq.r()], [qt.r()])
            K.tt(kf.ap(), ps_f.ap()[:, 128:256], Ek.ap(), ALU.mult, [ps_f.r(), Ek.r()], [kf.r()])
            K.tt(km.ap(), bc(kf.ap(), 1, [128, 4, 128]), bc(hm.ap(), 2, [128, 4, 128]), ALU.mult, [kf.r(), hm.r()],
                 [km.r()])
            K.tt(ktm.ap(), ps_m.ap()[:, 256:384], Ekt.ap(), ALU.mult, [ps_m.r(), Ekt.r()], [ktm.r()])
            K.cp(vtm.ap(), ps_m.ap()[:, 0:256], [ps_m.r()], [vtm.r()], eng="act")
            K.act(sgg.ap(), ps_g.ap()[:, 0:256], AF.Silu, [ps_g.r()], [sgg.r()])
            for hh in range(4):
                K.mm(ps_a.ap()[:, hh * 128:(hh + 1) * 128], km.ap()[:, hh, :], qt.ap(), [km.r(), qt.r()], [ps_a.r()],
                     inc=(hh == 3))
            K.tt(A.ap(), ps_a.ap().rearrange("p (h i) -> p h i", h=4), bc(maskU.ap(), 1, [128, 4, 128]), ALU.mult,
                 [ps_a.r(), maskU.r()], [A.r()])
            K.mm(ps_o.ap()[:, 0:256], qt.ap(), Sb.ap(), [qt.r(), Sb.r()], [ps_o.r()], start=True, stop=False)
            for hh in range(4):
                K.mm(ps_o.ap()[:, hh * 64:(hh + 1) * 64], A.ap()[:, hh, :], vtm.ap()[:, hh * 64:(hh + 1) * 64],
                     [A.r(), vtm.r()], [ps_o.r()], start=False, stop=(hh == 3))
            K.act(osq.ap(), ps_o.ap()[:, 0:256], AF.Square, [ps_o.r()], [osq.r()])
            K.red(ms.ap(), osq.ap().rearrange("p (h e) -> p h e", h=4), [osq.r()], [ms.r()])
            K.act(ms.ap(), ms.ap(), AF.Sqrt, [ms.r()], [ms.r()], scale=1.0 / 64, bias=RMS_EPS)
            K.recip(ms.ap(), ms.ap(), [ms.r()], [ms.r()])
            K.tt(on.ap().rearrange("p (h e) -> p h e", h=4), ps_o.ap()[:, 0:256].rearrange("p (h e) -> p h e", h=4),
                 bc(ms.ap(), 2, [128, 4, 64]), ALU.mult, [ps_o.r(), ms.r()], [on.r()])
            K.tt(onb.ap(), on.ap(), sgg.ap(), ALU.mult, [on.r(), sgg.r()], [onb.r()])
            for q in range(2):
                K.tr(ps_t.ap()[:, q * 128:(q + 1) * 128], onb.ap()[:, q * 128:(q + 1) * 128], identb.ap(),
                     [onb.r(), identb.r()], [ps_t.r()], inc=(q == 1))
            K.ts(yT.ap()[:, 6:8, c * 128:(c + 1) * 128], ps_t.ap()[:, 0:256].rearrange("p (t n) -> p t n", t=2),
                 gcol.ap(), ALU.mult, [ps_t.r(), gcol.r()], xr(yT, c // 4, range(6, 8)))
            K.mm(ps_o.ap()[:, 256:512], ktm.ap(), vtm.ap(), [ktm.r(), vtm.r()], [ps_o.r()])
            K.tt(tmpS.ap(), ps_o.ap()[:, 256:512], BM.ap(), ALU.mult, [ps_o.r(), BM.r()], [tmpS.r()])
            K.tt(S32.ap(), S32.ap(), tmpS.ap(), ALU.add, [S32.r(), tmpS.r()], [S32.r()])
            K.ts(S32.ap(), S32.ap(), Eq.ap()[:, 127:128], ALU.mult, [S32.r(), Eq.r()], [S32.r()])
            K.cp(Sb.ap(), S32.ap(), [S32.r()], [Sb.r()], eng="act")
        for hh in range(4):
            K.dma(dr["p_gla"][l, hh], S32.ap()[32 * hh:32 * hh + 32, 64 * hh:64 * hh + 64], r=[S32.r()])
        P.barrier()


def gla_sample(K, dr, C, l, yT, win, wgk2, bgkB):
    P = K.P
    hs, identb = C["hs"], C["identb"]
    with contextlib.ExitStack() as ph:
        glo = K.sb(ph, "glos", [16, NS], BF16)
        lg = K.sb(ph, "lgs", [NS, 128], F32)
        lgt = K.sb(ph, "lgts", [NS, 128], F32)
        pk = K.sb(ph, "pkg", [NS, 4, 160], F32)
        sgg = K.sb(ph, "sggs", [NS, 256], F32)
        gB = K.sb(ph, "gBg", [64, 64], F32)
        S = K.sb(ph, "Sg", [64, 32, 64], F32)
        tmp = K.sb(ph, "tmpg", [64, 32, 64], F32)
        pkh = K.sb(ph, "pkhg", [64, 160], F32)
        o = K.sb(ph, "og", [64, 64], F32)
        junk = K.sb(ph, "junkg", [64, 64], F32)
        ss = K.sb(ph, "ssg", [64, 1], F32)
        otm = K.sb(ph, "otm", [NS, 256], F32)
        otb = K.sb(ph, "otb", [NS, 256], BF16)
        ps_a = K.ps(ph, "psgs_a", [128, 512], F32)
        ps_b = K.ps(ph, "psgs_b", [128, 512], F32)
        ps_c = K.ps(ph, "psgs_c", [128, 512], F32)
        ps_t = K.ps(ph, "psgs_t", [128, 1024], BF16)
        spk = dram_scratch(K, "gpk", [NS, 640])
        so = dram_scratch(K, "go", [NS, 256])
        K.dma(gB.ap(), dr["gla_norm_g"][l:l + 1, :].to_broadcast([64, 64]), w=[gB.r()])
        K.dma(S.ap().rearrange("p d e -> p (d e)"), dr["st_gla"][l].rearrange("b h d e -> (b h) (d e)"), w=[S.r()])
        for k in range(KT):
            K.mm(ps_a.ap()[0:NS, :], hs.ap()[:, k, :], win.ap()[:, k, 0:512], [hs.r(), win.r()], [ps_a.r()],
                 start=(k == 0), stop=(k == KT - 1))
        for k in range(KT):
            K.mm(ps_b.ap()[0:NS, 0:256], hs.ap()[:, k, :], win.ap()[:, k, 528:784], [hs.r(), win.r()], [ps_b.r()],
                 start=(k == 0), stop=(k == KT - 1))
        for k in range(KT):
            K.mm(ps_c.ap()[0:16, 0:NS], win.ap()[:, k, 512:528], hs.ap()[:, k, :], [hs.r(), win.r()], [ps_c.r()],
                 start=(k == 0), stop=(k == KT - 1))
        K.cp(glo.ap(), ps_c.ap()[0:16, 0:NS], [ps_c.r()], [glo.r()], eng="act")
        K.mm(ps_c.ap()[0:NS, 128:256], glo.ap(), wgk2.ap(), [glo.r(), wgk2.r()], [ps_c.r()])
        K.stt(lg.ap(), ps_c.ap()[0:NS, 128:256], -1.0, bgkB.ap()[0:NS, :], ALU.mult, ALU.subtract,
              [ps_c.r(), bgkB.r()], [lg.r()])
        softplus_(K, (lg.ap(), lg.r()), (lgt.ap(), lgt.r()), None, None)
        K.act(lg.ap(), lg.ap(), AF.Exp, [lg.r()], [lg.r()], scale=-1.0 / 16.0)
        K.act(sgg.ap(), ps_b.ap()[0:NS, 0:256], AF.Silu, [ps_b.r()], [sgg.r()])
        K.ts(pk.ap()[:, :, 0:32], ps_a.ap()[0:NS, 0:128].rearrange("p (h d) -> p h d", h=4), 32.0 ** -0.5, ALU.mult,
             [ps_a.r()], [pk.r()])
        K.cp(pk.ap()[:, :, 32:64], ps_a.ap()[0:NS, 128:256].rearrange("p (h d) -> p h d", h=4), [ps_a.r()], [pk.r()])
        K.cp(pk.ap()[:, :, 64:96], lg.ap().rearrange("p (h d) -> p h d", h=4), [lg.r()], [pk.r()])
        K.cp(pk.ap()[:, :, 96:160], ps_a.ap()[0:NS, 256:512].rearrange("p (h e) -> p h e", h=4), [ps_a.r()], [pk.r()])
        K.dma(spk.ap(), pk.ap().rearrange("p h x -> p (h x)"), r=[pk.r()], w=[spk.r()])
        K.dma(pkh.ap(), spk.ap().rearrange("b (h x) -> (b h) x", h=4), r=[spk.r()], w=[pkh.r()])
        qh, kh, eh, vh = pkh.ap()[:, 0:32], pkh.ap()[:, 32:64], pkh.ap()[:, 64:96], pkh.ap()[:, 96:160]
        K.tt(S.ap(), S.ap(), bc(eh, 2, [64, 32, 64]), ALU.mult, [S.r(), pkh.r()], [S.r()])
        K.tt(tmp.ap(), bc(kh, 2, [64, 32, 64]), bc(vh, 1, [64, 32, 64]), ALU.mult, [pkh.r()], [tmp.r()])
        K.tt(S.ap(), S.ap(), tmp.ap(), ALU.add, [S.r(), tmp.r()], [S.r()])
        K.dma(dr["s_gla"][l].rearrange("b h d e -> (b h) (d e)"), S.ap().rearrange("p d e -> p (d e)"), r=[S.r()])
        K.tt(tmp.ap(), S.ap(), bc(qh, 2, [64, 32, 64]), ALU.mult, [S.r(), pkh.r()], [tmp.r()])
        K.red(o.ap(), tmp.ap().rearrange("p d e -> p e d"), [tmp.r()], [o.r()])
        K.act(junk.ap(), o.ap(), AF.Square, [o.r()], [junk.r(), ss.r()], accum_out=ss.ap())
        K.act(ss.ap(), ss.ap(), AF.Sqrt, [ss.r()], [ss.r()], scale=1.0 / 64, bias=RMS_EPS)
        K.recip(ss.ap(), ss.ap(), [ss.r()], [ss.r()])
        K.stt(o.ap(), o.ap(), ss.ap(), gB.ap(), ALU.mult, ALU.mult, [o.r(), ss.r(), gB.r()], [o.r()])
        K.dma(so.ap().rearrange("b (h e) -> (b h) e", h=4), o.ap(), r=[o.r()], w=[so.r()])
        K.dma(otm.ap(), so.ap(), r=[so.r()], w=[otm.r()])
        K.tt(otb.ap(), otm.ap(), sgg.ap(), ALU.mult, [otm.r(), sgg.r()], [otb.r()])
        for q in range(2):
            K.tr(ps_t.ap()[:, q * NS:(q + 1) * NS], otb.ap()[:, q * 128:(q + 1) * 128], identb.ap()[0:NS, 0:NS],
                 [otb.r(), identb.r()], [ps_t.r()], inc=(q == 1))
        K.cp(yT.ap()[:, 6:8, T:T + NS], ps_t.ap()[:, 0:2 * NS].rearrange("p (t n) -> p t n", t=2), [ps_t.r()],
             xr(yT, 4, range(6, 8)))
        P.barrier()


C0 = float(np.exp(-0.5))


def rwkv_prep(K, C, pc, LW, N, rw, prev, B, pl, pg, pn, ee="dve"):
    blk64 = C["blk64"]
    MX, LI = B["MX"], B["LI"]
    mxa = MX.ap()[:, :, 0:N]
    K.tt(mxa, prev, rw, ALU.subtract, B["_rw_res"], [MX.r()], eng=ee)
    yield
    K.tt(mxa, mxa, bc(pc["mu"].ap(), 2, [128, 7, N]), ALU.mult, [MX.r(), pc["mu"].r()], [MX.r()], eng=ee)
    yield
    K.tt(mxa, mxa, rw, ALU.add, [MX.r()] + B["_rw_res"], [MX.r()], eng=ee)
    yield
    r, k, v = (MX.ap()[:, 0:2, 0:N], MX.ap()[:, 2:4, 0:N], MX.ap()[:, 4:6, 0:N])
    lia = LI.ap()[:, 0:N]
    lif = B["t1"].ap()[:, 0, 0:N]
    K.act(lif, MX.ap()[:, 6, 0:N], AF.Exp, [MX.r(), pc["lisc"].r()], [B["t1"].r()], scale=pc["lisc"].ap())
    yield
    sigmoid_chain(K, lif, [B["t1"].r()])
    yield
    K.ts(lia[0:32], lif[0:32], 2.0, ALU.mult, [B["t1"].r()], [LI.r()], s2=-1.0, op1=ALU.add)
    K.cp(lia[32:64], MX.ap()[32:64, 6, 0:N], [MX.r()], [LI.r()], eng="act")
    K.cp(lia[64:128], lif[64:128], [B["t1"].r()], [LI.r()])
    yield
    for t in range(2):
        cs = slice(t * 128, (t + 1) * 128)
        K.mm(pl.ap()[:, t * N:(t + 1) * N], LW.ap()[0:32, cs], lia[0:32], [LW.r(), LI.r()], [pl.r()], self_wait=True)
        K.mm(pl.ap()[:, (2 + t) * N:(3 + t) * N], LW.ap()[32:64, cs], lia[32:64], [LW.r(), LI.r()], [pl.r()],
             self_wait=True)
        K.mm(pg.ap()[:, t * N:(t + 1) * N], LW.ap()[64:128, cs], lia[64:128], [LW.r(), LI.r()], [pg.r()],
             self_wait=True)
    g = lambda n: B[n].ap()[:, :, 0:N]
    for t in range(2):
        K.act(B["sig"].ap()[:, t, 0:N], pl.ap()[:, t * N:(t + 1) * N], AF.Exp, [pl.r(), pc["nw0"].r()],
              [B["sig"].r()], scale=-1.0, bias=pc["nw0"].ap()[:, t:t + 1])
        K.act(B["aic"].ap()[:, t, 0:N], pl.ap()[:, (2 + t) * N:(3 + t) * N], AF.Exp, [pl.r(), pc["na0"].r()],
              [B["aic"].r()], scale=-1.0, bias=pc["na0"].ap()[:, t:t + 1])
    sigmoid_chain(K, g("sig"), [B["sig"].r()])
    sigmoid_chain(K, g("aic"), [B["aic"].r()])
    for t in range(0):
        pass
    K.cp(g("gate"), pg.ap()[:, 0:2 * N].rearrange("p (t n) -> p t n", t=2), [pg.r()], [B["gate"].r()], eng="act")
    yield
    K.tt(g("kk"), k, bc(pc["k_k"].ap(), 2, [128, 2, N]), ALU.mult, [MX.r(), pc["k_k"].r()], [B["kk"].r()], eng=ee)
    yield
    K.tt(g("t1"), g("kk"), g("kk"), ALU.mult, [B["kk"].r()], [B["t1"].r()], eng=ee)
    yield
    for t in range(2):
        K.mm(pn.ap()[:, t * N:(t + 1) * N], blk64.ap(), B["t1"].ap()[:, t, 0:N], [blk64.r(), B["t1"].r()], [pn.r()])
    K.act(g("t1"), pn.ap()[:, 0:2 * N].rearrange("p (t n) -> p t n", t=2), AF.Ln, [pn.r()], [B["t1"].r()],
          bias=1e-12)
    K.act(g("t1"), g("t1"), AF.Exp, [B["t1"].r()], [B["t1"].r()], scale=-0.5)
    yield
    K.tt(g("kk"), g("kk"), g("t1"), ALU.mult, [B["kk"].r(), B["t1"].r()], [B["kk"].r()], eng=ee)
    yield
    yield
    K.tt(g("t1"), g("aic"), bc(pc["k_a"].ap(), 2, [128, 2, N]), ALU.mult, [B["aic"].r(), pc["k_a"].r()], [B["t1"].r()], eng=ee)
    yield
    K.tt(g("t1"), g("t1"), bc(pc["omka"].ap(), 2, [128, 2, N]), ALU.add, [B["t1"].r(), pc["omka"].r()], [B["t1"].r()], eng=ee)
    yield
    K.tt(g("kp"), k, g("t1"), ALU.mult, [MX.r(), B["t1"].r()], [B["kp"].r()], eng=ee)
    yield
    K.tt(g("t1"), r, g("kp"), ALU.mult, [MX.r(), B["kp"].r()], [B["t1"].r()], eng=ee)
    yield
    K.tt(g("t1"), g("t1"), bc(pc["r_k"].ap(), 2, [128, 2, N]), ALU.mult, [B["t1"].r(), pc["r_k"].r()], [B["t1"].r()], eng=ee)
    yield
    for t in range(2):
        K.mm(pn.ap()[:, t * N:(t + 1) * N], blk64.ap(), B["t1"].ap()[:, t, 0:N], [blk64.r(), B["t1"].r()], [pn.r()])
    K.tt(g("bonus"), pn.ap()[:, 0:2 * N].rearrange("p (t n) -> p t n", t=2), v, ALU.mult, [pn.r(), MX.r()],
         [B["bonus"].r()])
    B['_rkv'] = (r, k, v)
    yield


def rwkv_params(K, dr, l, ph):
    pc = {}
    mu = K.sb(ph, "mu", [128, 7], F32)
    K.dma(mu.ap(), dr["rwkv_mu"][l].rearrange("(t p) -> p t", p=128), w=[mu.r()])
    pc["mu"] = mu
    for n, src in (("w0", dr["rwkv_w0"][l]), ("a0", dr["rwkv_a0"][l]), ("k_k", dr["rwkv_k_k"][l]),
                   ("k_a", dr["rwkv_k_a"][l]), ("r_k", dr["rwkv_r_k"][l].rearrange("h n -> (h n)")),
                   ("ln_g", dr["rwkv_ln_g"][l]), ("ln_b", dr["rwkv_ln_b"][l])):
        t = K.sb(ph, "pc_" + n, [128, 2], F32)
        K.dma(t.ap(), src.rearrange("(t p) -> p t", p=128), w=[t.r()])
        pc[n] = t
    for n in ("w0", "a0"):
        t = K.sb(ph, "pc_n" + n, [128, 2], F32)
        K.ts(t.ap(), pc[n].ap(), -1.0, ALU.mult, [pc[n].r()], [t.r()])
        pc["n" + n] = t
    lisc = K.sb(ph, "lisc", [128, 1], F32)
    K.memset(lisc.ap()[0:32], -2.0, [lisc.r()])
    K.memset(lisc.ap()[32:64], 0.0, [lisc.r()])
    K.memset(lisc.ap()[64:128], -1.0, [lisc.r()])
    pc["lisc"] = lisc
    omka = K.sb(ph, "omka", [128, 2], F32)
    K.ts(omka.ap(), pc["k_a"].ap(), -1.0, ALU.mult, [pc["k_a"].r()], [omka.r()], s2=1.0, op1=ALU.add)
    pc["omka"] = omka
    LW = K.sb(ph, "LW", [128, 256], BF16)
    K.dma(LW.ap()[0:32, :], dr["rwkv_w2"][l], w=[LW.r()], q="pool")
    K.dma(LW.ap()[32:64, :], dr["rwkv_a2"][l], w=[LW.r()], q="pool")
    K.dma(LW.ap()[64:128, :], dr["rwkv_g2"][l], w=[LW.r()], q="pool")
    return pc, LW


def rwkv_epilogue(K, C, pc, B, N, pT, ydst, yres):
    for t in range(2):
        K.act(B["t1"].ap()[:, t, 0:N], pT.ap()[:, t * N:(t + 1) * N], AF.Identity, [pT.r(), pc["ln_g"].r(), pc["ln_b"].r()],
              [B["t1"].r()], scale=pc["ln_g"].ap()[:, t:t + 1], bias=pc["ln_b"].ap()[:, t:t + 1])
    g = lambda n: B[n].ap()[:, :, 0:N]
    K.tt(g("t1"), g("t1"), g("bonus"), ALU.add, [B["t1"].r(), B["bonus"].r()], [B["t1"].r()])
    K.tt(ydst, g("t1"), g("gate"), ALU.mult, [B["t1"].r(), B["gate"].r()], yres)


def groupnorm64(K, o_ap, n, G, scr, res_in, out_ap, out_res):
    mean, xc, sq, var = scr
    K.red(mean.ap()[0:n, 0:G], o_ap, res_in, [mean.r()])
    K.ts(mean.ap()[0:n, 0:G], mean.ap()[0:n, 0:G], 1.0 / 64, ALU.mult, [mean.r()], [mean.r()])
    xca = xc.ap()[0:n, 0:G * 64].rearrange("p (g e) -> p g e", g=G)
    K.tt(xca, o_ap, bc(mean.ap()[0:n, 0:G], 2, [n, G, 64]), ALU.subtract, res_in + [mean.r()], [xc.r()])
    sqa = sq.ap()[0:n, 0:G * 64].rearrange("p (g e) -> p g e", g=G)
    K.tt(sqa, xca, xca, ALU.mult, [xc.r()], [sq.r()])
    K.red(var.ap()[0:n, 0:G], sqa, [sq.r()], [var.r()])
    rsqrt_(K, var.ap()[0:n, 0:G], [var.r()], 1.0 / 64, RWKV_GN_EPS)
    K.tt(out_ap, xca, bc(var.ap()[0:n, 0:G], 2, [n, G, 64]), ALU.mult, [xc.r(), var.r()], out_res)


def rwkv_phase(K, dr, C, l, yT, dbg_out):
    P = K.P
    R0 = OFF["rw"]
    with contextlib.ExitStack() as ph:
        win = K.sb(ph, "win_rwkv", [128, KT, 896], BF16)
        K.dma(win.ap(), dr["w_in"][l, :, R0:R0 + 896].rearrange("(k p) n -> p k n", p=128), w=[win.r()], q="pool")
        pc, LW = rwkv_params(K, dr, l, ph)
        import os
        if os.environ.get("SKIP_RWKV_PROMPT") != "1":
            rwkv_prompt(K, dr, C, l, yT, win, pc, LW, dbg_out)
        P.barrier()
        if os.environ.get("SKIP_RWKV_SAMPLE") != "1":
            rwkv_sample(K, dr, C, l, yT, win, pc, LW, dbg_out)


def interleave(gens, ratio=None):
    gens = [g for g in gens if g is not None]
    ratio = ratio or [1] * len(gens)
    live = list(zip(gens, ratio))
    while live:
        for item in list(live):
            g, n = item
            for _ in range(n):
                try:
                    next(g)
                except StopIteration:
                    live.remove(item)
                    break


def interleave_gen(gens, ratio):
    live = list(zip(gens, ratio))
    while live:
        for item in list(live):
            g, n = item
            for _ in range(n):
                try:
                    next(g)
                except StopIteration:
                    live.remove(item)
                    break
                yield


def rwkv_prompt(K, dr, C, l, yT, win, pc, LW, dbg_out):
    P = K.P
    identb, identf, maskU, maskSU, maskSL, blk64 = (C[k] for k in ["identb", "identf", "maskU", "maskSU", "maskSL", "blk64"])
    hc = C["hc"]
    N = 128
    with contextlib.ExitStack() as ph:
        f3 = lambda n: K.sb(ph, n, [128, 2, N], F32)
        Bs = []
        for i in range(2):
            B = {n: f3(f"rb{i}_" + n) for n in ["sig", "aic", "gate", "kk", "t1", "kp", "bonus", "cs", "e1", "e2", "bb"]}
            Bs.append(B)
        MX = K.sb(ph, "MX", [128, 7, N], F32)
        LI = K.sb(ph, "LI", [128, N], BF16)
        for B in Bs:
            B["MX"], B["LI"] = MX, LI
        RW = [K.sb(ph, f"RW{i}", [128, 7, N + 1], F32) for i in range(2)]
        ones_r = K.sb(ph, "ones_r", [128, N], F32)
        bcol = K.sb(ph, "bcol", [128, 2], F32)
        MK2 = K.sb(ph, "MK2", [128, 2, N], F32)
        ARs = [K.sb(ph, f"AR{i}", [128, 2, 2, N], BF16) for i in range(2)]
        BKs = [K.sb(ph, f"BK{i}", [128, 2, 2, N], BF16) for i in range(2)]
        FH = K.sb(ph, "FH", [128, 3, 2, N], BF16)
        TMs = [K.sb(ph, f"TM{i}", [128, 4, 2, N], BF16) for i in range(2)]
        t2 = f3("rb_t2")
        A1 = K.sb(ph, "A1", [128, 4, 2, N], BF16)
        A2 = K.sb(ph, "A2", [128, 4, 2, N], BF16)
        Lb = [K.sb(ph, f"Lb{i}", [128, 4, N], BF16) for i in range(2)]
        Nb = [K.sb(ph, f"Nb{i}", [128, 4, N], BF16) for i in range(2)]
        X32 = K.sb(ph, "X32", [128, 4, 2, 64], F32)
        Xb = K.sb(ph, "Xb", [128, 4, 2, 64], BF16)
        Apf = K.sb(ph, "Apf", [128, 2, N], BF16)
        XAc = K.sb(ph, "XAc", [128, 256], BF16)
        Utm = K.sb(ph, "Utm", [128, 4, 64], BF16)
        ST32 = K.sb(ph, "ST32", [128, 2, N], F32)
        STb = K.sb(ph, "STb", [128, 2, N], BF16)
        tmpS = K.sb(ph, "tmpSr", [128, 2, N], F32)
        gn = (K.sb(ph, "gn_mean", [128, 4], F32), K.sb(ph, "gn_xc", [128, 256], F32),
              K.sb(ph, "gn_sq", [128, 256], F32), K.sb(ph, "gn_var", [128, 4], F32))
        onb = K.sb(ph, "onbr", [128, 256], BF16)
        stT = K.sb(ph, "stT", [128, 2, N], F32)
        pI = K.ps(ph, "pr_I", [128, 512], F32)
        pM = K.ps(ph, "pr_M", [128, 512], F32)
        pT = K.ps(ph, "pr_T", [128, 1024], BF16)
        pT2 = pT
        pAT = K.ps(ph, "pr_AT", [128, 1024], F32)
        pL = K.ps(ph, "pr_L", [128, 512], F32)
        pL2 = K.ps(ph, "pr_L2", [128, 512], F32)
        pX = K.ps(ph, "pr_X", [128, 512], F32)
        pO = pX

        K.memset(ones_r.ap(), 1.0, [ones_r.r()])
        K.cp(MK2.ap()[:, 0, :], maskSU.ap(), [maskSU.r()], [MK2.r()])
        K.cp(MK2.ap()[:, 1, :], maskU.ap(), [maskU.r()], [MK2.r()])
        K.memset(ST32.ap(), 0.0, [ST32.r()])
        K.memset(STb.ap(), 0.0, [STb.r()])
        K.memset(RW[0].ap()[:, :, 0:1], 0.0, [RW[0].r()])

        import os
        PE1 = os.environ.get("RWKV_S1_ENG", "dve")

        def s1(c):
            B, AR, BK, TM = Bs[c % 2], ARs[c % 2], BKs[c % 2], TMs[c % 2]
            g = lambda n: B[n].ap()
            h = hc[c % 2]
            make_hc(K, C, l, h, c)
            yield
            rw = RW[c % 2]
            for (t0, t1) in ((0, 4), (4, 7)):
                for t in range(t0, t1):
                    for k in range(KT):
                        K.mm(pI.ap()[:, (t - t0) * N:(t - t0 + 1) * N], win.ap()[:, k, t * N:(t + 1) * N], h.ap()[:, k, :],
                             [win.r(), h.r()], [pI.r()], start=(k == 0), stop=(k == KT - 1))
                    yield
                K.cp(rw.ap()[:, t0:t1, 1:N + 1], pI.ap()[:, 0:(t1 - t0) * N].rearrange("p (t n) -> p t n", t=t1 - t0),
                     [pI.r()], [rw.r()], eng="act")
                yield
            if c + 1 < NCH:
                K.cp(RW[(c + 1) % 2].ap()[:, :, 0:1], rw.ap()[:, :, N:N + 1], [rw.r()], [RW[(c + 1) % 2].r()])
            B["_rw_res"] = [rw.r()]
            yield from rwkv_prep(K, C, pc, LW, N, rw.ap()[:, :, 1:N + 1], rw.ap()[:, :, 0:N], B, pM, pI, pI, ee=PE1)
            r, k_, v = B["_rkv"]
            MXr = MX.r()
            for t in range(2):
                K.P.op("dve", lambda e, t=t, B=B: e.tensor_tensor_scan(out=B["cs"].ap()[:, t, :], data0=ones_r.ap(),
                                                                        data1=B["sig"].ap()[:, t, :], initial=0.0,
                                                                        op0=ALU.mult, op1=ALU.add),
                       reads=[ones_r.r(), B["sig"].r()], writes=[B["cs"].r()])
            yield
            K.act(g("e1"), g("cs"), AF.Exp, [B["cs"].r()], [B["e1"].r()], scale=-C0)
            yield
            K.act(g("e2"), g("cs"), AF.Exp, [B["cs"].r()], [B["e2"].r()], scale=C0)
            yield
            K.tt(AR.ap()[:, :, 1, :], r, g("e1"), ALU.mult, [MXr, B["e1"].r()], [AR.r()], eng=PE1)
            yield
            K.tt(g("bb"), g("kk"), g("aic"), ALU.mult, [B["kk"].r(), B["aic"].r()], [B["bb"].r()], eng=PE1)
            yield
            K.tt(BK.ap()[:, :, 0, :], g("bb"), g("e2"), ALU.mult, [B["bb"].r(), B["e2"].r()], [BK.r()], eng=PE1)
            yield
            K.tt(BK.ap()[:, :, 1, :], g("kp"), g("e2"), ALU.mult, [B["kp"].r(), B["e2"].r()], [BK.r()], eng=PE1)
            yield
            K.tt(g("t1"), g("cs"), g("sig"), ALU.subtract, [B["cs"].r(), B["sig"].r()], [B["t1"].r()], eng=PE1)
            yield
            K.act(g("e2"), g("t1"), AF.Exp, [B["t1"].r()], [B["e2"].r()], scale=-C0)
            yield
            K.stt(AR.ap()[:, :, 0, :], g("kk"), -1.0, g("e2"), ALU.mult, ALU.mult, [B["kk"].r(), B["e2"].r()], [AR.r()])
            yield
            K.ts(bcol.ap(), B["cs"].ap()[:, :, N - 1], -C0, ALU.mult, [B["cs"].r()], [bcol.r()])
            yield
            for t in range(2):
                K.act(B["e2"].ap()[:, t, :], B["cs"].ap()[:, t, :], AF.Exp, [B["cs"].r(), bcol.r()], [B["e2"].r()],
                      scale=C0, bias=bcol.ap()[:, t:t + 1])
            yield
            K.tt(FH.ap()[:, 0], g("bb"), g("e2"), ALU.mult, [B["bb"].r(), B["e2"].r()], [FH.r()], eng=PE1)
            yield
            K.tt(FH.ap()[:, 1], g("kp"), g("e2"), ALU.mult, [B["kp"].r(), B["e2"].r()], [FH.r()], eng=PE1)
            yield
            K.cp(FH.ap()[:, 2], v, [MXr], [FH.r()], eng="act")
            yield
            for q in range(4):
                for t in range(2):
                    src = FH.ap()[:, q, t, :] if q < 3 else AR.ap()[:, t, 0, :]
                    K.tr(pT.ap()[:, (q * 2 + t) * N:(q * 2 + t + 1) * N], src, identb.ap(),
                         [FH.r(), AR.r(), identb.r()], [pT.r()], inc=(t == 1))
                yield
            K.cp(TM.ap().rearrange("p q t n -> p (q t n)"), pT.ap(), [pT.r()], [TM.r()], eng="act")
            yield

        def s2(c):
            B, AR, BK, TM = Bs[c % 2], ARs[c % 2], BKs[c % 2], TMs[c % 2]
            mk = bc(MK2.ap(), 1, [128, 4, 2, N])
            for which, Adst in ((0, A1), (1, A2)):
                for hd in range(4):
                    t, o = hd // 2, 64 * (hd % 2)
                    sl = slice(o, o + 64)
                    arf = AR.ap()[sl, t].rearrange("p a n -> p (a n)")
                    K.mm(pAT.ap()[:, hd * 256:(hd + 1) * 256], BK.ap()[sl, t, which, :], arf, [BK.r(), AR.r()], [pAT.r()],
                         self_wait=True)
                yield
                K.tt(Adst.ap(), pAT.ap().rearrange("p (h a n) -> p h a n", h=4, a=2), mk, ALU.mult, [pAT.r(), MK2.r()],
                     [Adst.r()])
                yield
            for hd in range(4):
                t, o = hd // 2, 64 * (hd % 2)
                sl = slice(o, o + 64)
                K.mm(pL.ap()[:, hd * N:(hd + 1) * N], AR.ap()[sl, t, 0, :], BK.ap()[sl, t, 0, :], [BK.r(), AR.r()],
                     [pL.r()], self_wait=True)
            yield
            K.tt(Lb[0].ap(), pL.ap().rearrange("p (h n) -> p h n", h=4), bc(maskSL.ap(), 1, [128, 4, N]), ALU.mult,
                 [pL.r(), maskSL.r()], [Lb[0].r()])
            yield
            vtm = TM.ap()[:, 2].rearrange("p t n -> p (t n)")
            for hd in range(4):
                K.mm(pX.ap()[:, hd * 64:(hd + 1) * 64], A2.ap()[:, hd, 0, :], vtm[:, hd * 64:(hd + 1) * 64],
                     [A2.r(), TM.r()], [pX.r()], inc=(hd == 3))
            yield
            K.cp(X32.ap()[:, :, 0, :], TM.ap()[:, 3].rearrange("p t (hh k) -> p (t hh) k", hh=2), [TM.r()], [X32.r()])
            yield
            K.cp(X32.ap()[:, :, 1, :], pX.ap()[:, 0:256].rearrange("p (h v) -> p h v", h=4), [pX.r()], [X32.r()],
                 eng="act")
            yield
            K.cp(Xb.ap(), X32.ap(), [X32.r()], [Xb.r()], eng="act")
            yield
            for i in range(7):
                if i == 0:
                    nref = lambda hd: A1.ap()[:, hd, 0, :]
                    nres = A1.r()
                    lcur = Lb[0]
                else:
                    nprev_ref, nprev_res, lprev = nref, nres, lcur
                    nnew, lnew = Nb[i % 2], Lb[i % 2]
                    for hd in range(4):
                        K.mm(pL.ap()[:, hd * N:(hd + 1) * N], lprev.ap()[:, hd, :], nprev_ref(hd), [lprev.r(), nprev_res],
                             [pL.r()], inc=(hd == 3))
                    yield
                    if i < 6:
                        for hd in range(4):
                            K.mm(pL2.ap()[:, hd * N:(hd + 1) * N], nprev_ref(hd), lprev.ap()[:, hd, :],
                                 [lprev.r(), nprev_res], [pL2.r()], inc=(hd == 3))
                        yield
                    K.cp(nnew.ap(), pL.ap().rearrange("p (h n) -> p h n", h=4), [pL.r()], [nnew.r()], eng="act")
                    yield
                    if i < 6:
                        K.cp(lnew.ap(), pL2.ap().rearrange("p (h n) -> p h n", h=4), [pL2.r()], [lnew.r()])
                        yield
                    nref = lambda hd, nnew=nnew: nnew.ap()[:, hd, :]
                    nres = nnew.r()
                    lcur = lnew
                for hd in range(4):
                    K.mm(pX.ap()[:, hd * N:(hd + 1) * N], nref(hd), Xb.ap()[:, hd].rearrange("p a k -> p (a k)"),
                         [nres, Xb.r()], [pX.r()], inc=(hd == 3))
                yield
                K.tt(X32.ap(), X32.ap(), pX.ap().rearrange("p (h a k) -> p h a k", h=4, a=2), ALU.add,
                     [X32.r(), pX.r()], [X32.r()])
                yield
                K.cp(Xb.ap(), X32.ap(), [X32.r()], [Xb.r()], eng="act")
                yield
            K.cp(XAc.ap().rearrange("p (h k) -> p h k", h=4), X32.ap()[:, :, 0, :], [X32.r()], [XAc.r()])
            yield
            for t in range(2):
                K.tr(pT2.ap()[:, t * N:(t + 1) * N], XAc.ap()[:, t * N:(t + 1) * N], identb.ap(), [XAc.r(), identb.r()],
                     [pT2.r()], inc=(t == 1))
            yield
            K.cp(Apf.ap(), pT2.ap()[:, 0:2 * N].rearrange("p (t n) -> p t n", t=2), [pT2.r()], [Apf.r()], eng="act")
            yield
            for t in range(2):
                K.mm(pX.ap()[:, t * N:(t + 1) * N], Apf.ap()[:, t, :], STb.ap()[:, t, :], [Apf.r(), STb.r()], [pX.r()],
                     inc=(t == 1))
            yield
            K.tt(Utm.ap(), pX.ap()[:, 0:256].rearrange("p (h v) -> p h v", h=4), X32.ap()[:, :, 1, :], ALU.add,
                 [pX.r(), X32.r()], [Utm.r()])
            yield
            for t in range(2):
                K.mm(pO.ap()[:, t * N:(t + 1) * N], AR.ap()[:, t, 1, :], STb.ap()[:, t, :], [AR.r(), STb.r()], [pO.r()],
                     start=(t == 0), stop=False, sgc=True)
            for hd in range(4):
                K.mm(pO.ap()[:, hd * 64:(hd + 1) * 64], A1.ap()[:, hd, 1, :], Utm.ap()[:, hd, :], [A1.r(), Utm.r()],
                     [pO.r()], start=False, stop=False, sgc=True)
                K.mm(pO.ap()[:, hd * 64:(hd + 1) * 64], A2.ap()[:, hd, 1, :], vtm[:, hd * 64:(hd + 1) * 64],
                     [A2.r(), TM.r()], [pO.r()], start=False, stop=False, sgc=True)
            for t in range(2):
                K.mm(pO.ap()[:, 256 + t * N:256 + (t + 1) * N], TM.ap()[:, 0, t, :],
                     Utm.ap()[:, 2 * t:2 * t + 2, :].rearrange("p h v -> p (h v)"), [TM.r(), Utm.r()], [pO.r()],
                     start=False, stop=False, sgc=True)
                K.mm(pO.ap()[:, 256 + t * N:256 + (t + 1) * N], TM.ap()[:, 1, t, :], vtm[:, t * N:(t + 1) * N],
                     [TM.r()], [pO.r()], start=False, stop=(t == 1), inc=(t == 1), sgc=True)
            yield
            K.tt(tmpS.ap(), pO.ap()[:, 256:512].rearrange("p (t n) -> p t n", t=2), bc(blk64.ap(), 1, [128, 2, N]),
                 ALU.mult, [pO.r(), blk64.r()], [tmpS.r()])
            yield
            K.tt(ST32.ap(), ST32.ap(), bc(B["e1"].ap()[:, :, N - 1], 2, [128, 2, N]), ALU.mult, [ST32.r(), B["e1"].r()],
                 [ST32.r()])
            yield
            K.tt(ST32.ap(), ST32.ap(), tmpS.ap(), ALU.add, [ST32.r(), tmpS.r()], [ST32.r()])
            yield
            K.cp(STb.ap(), ST32.ap(), [ST32.r()], [STb.r()], eng="act")
            yield
            groupnorm64(K, pO.ap()[:, 0:256].rearrange("p (h v) -> p h v", h=4), 128, 4, gn, [pO.r()],
                        onb.ap().rearrange("p (h v) -> p h v", h=4), [onb.r()])
            yield
            for t in range(2):
                K.tr(pT2.ap()[:, t * N:(t + 1) * N], onb.ap()[:, t * N:(t + 1) * N], identb.ap(), [onb.r(), identb.r()],
                     [pT2.r()], inc=(t == 1))
            yield
            B2 = dict(B)
            B2["t1"] = t2
            rwkv_epilogue(K, C, pc, B2, N, pT2, yT.ap()[:, 4:6, c * N:(c + 1) * N], xr(yT, c // 4, range(4, 6)))
            yield

        for _ in s1(0):
            pass
        for c in range(NCH):
            interleave([s2(c), s1(c + 1) if c + 1 < NCH else None], ratio=[3, 2])
        rw = RW[(NCH - 1) % 2]
        K.dma(dr["p_shift"][l].rearrange("(t p) -> p t", p=128), rw.ap()[:, :, N], r=[rw.r()])
        for t in range(2):
            K.tr(pL.ap()[:, t * N:(t + 1) * N], ST32.ap()[:, t, :], identf.ap(), [ST32.r(), identf.r()], [pL.r()],
                 inc=(t == 1))
        K.cp(stT.ap(), pL.ap()[:, 0:2 * N].rearrange("p (t n) -> p t n", t=2), [pL.r()], [stT.r()])
        for hd in range(4):
            t, o = hd // 2, 64 * (hd % 2)
            K.dma(dr["p_rwkv"][l, hd], stT.ap()[o:o + 64, t, o:o + 64], r=[stT.r()])
        P.barrier()


def rwkv_sample(K, dr, C, l, yT, win, pc, LW, dbg_out):
    P = K.P
    hs, identb, identf = C["hs"], C["identb"], C["identf"]
    N = NS
    with contextlib.ExitStack() as ph:
        f3 = lambda n: K.sb(ph, n, [128, 2, N], F32)
        B = {n: f3("rs_" + n) for n in ["sig", "aic", "gate", "kk", "t1", "kp", "bonus", "e1", "e2", "bb"]}
        B["MX"] = K.sb(ph, "MXs", [128, 7, N], F32)
        B["LI"] = K.sb(ph, "LIs", [128, N], BF16)
        rws = K.sb(ph, "rws", [128, 7, N], F32)
        prevs = K.sb(ph, "prevs", [128, 7, N], F32)
        shs = K.sb(ph, "shs", [NS, 896], F32)
        rwtm = K.sb(ph, "rwtm", [NS, 896], F32)
        pkT = K.sb(ph, "pkT", [NS, 6, 256], F32)
        pkh = K.sb(ph, "pkhr", [64, 6, 64], F32)
        S = K.sb(ph, "Sr", [64, 64, 64], F32)
        tmp = K.sb(ph, "tmpr", [64, 64, 64], F32)
        sa = K.sb(ph, "sa", [64, 64], F32)
        o = K.sb(ph, "orr", [64, 64], F32)
        on = K.sb(ph, "onr", [64, 64], F32)
        gn = (K.sb(ph, "gns_mean", [64, 1], F32), K.sb(ph, "gns_xc", [64, 64], F32),
              K.sb(ph, "gns_sq", [64, 64], F32), K.sb(ph, "gns_var", [64, 1], F32))
        otm = K.sb(ph, "otmr", [NS, 256], F32)
        otb = K.sb(ph, "otbr", [NS, 256], BF16)
        pA = K.ps(ph, "prs_A", [128, 1024], F32)
        pB = K.ps(ph, "prs_B", [128, 1024], F32)
        pL = K.ps(ph, "prs_L", [128, 512], F32)
        pM = K.ps(ph, "prs_M", [128, 512], F32)
        pT = K.ps(ph, "prs_T", [128, 1024], BF16)
        scr = dram_scratch(K, "rpk", [NS, 4, 6, 64])
        so = dram_scratch(K, "ro", [NS, 256])

        K.dma(shs.ap(), dr["st_shift"][l], w=[shs.r()])
        K.dma(S.ap().rearrange("p v k -> p (v k)"), dr["st_rwkv"][l].rearrange("b h v k -> (b h) (v k)"), w=[S.r()])
        for t in range(7):
            for k in range(KT):
                K.mm(pM.ap()[:, t * N:(t + 1) * N], win.ap()[:, k, t * 128:(t + 1) * 128], hs.ap()[:, k, :],
                     [win.r(), hs.r()], [pM.r()], start=(k == 0), stop=(k == KT - 1), inc=(k == KT - 1 and t == 6))
        K.cp(rws.ap(), pM.ap()[:, 0:7 * N].rearrange("p (t n) -> p t n", t=7), [pM.r()], [rws.r()], eng="act")
        for (c0, c1) in ((0, 512), (512, 896)):
            for k in range(KT):
                K.mm(pA.ap()[0:NS, c0:c1], hs.ap()[:, k, :], win.ap()[:, k, c0:c1], [win.r(), hs.r()], [pA.r()],
                     start=(k == 0), stop=(k == KT - 1))
        K.cp(rwtm.ap(), pA.ap()[0:NS, 0:896], [pA.r()], [rwtm.r()], eng="act")
        K.dma(dr["s_shift"][l], rwtm.ap(), r=[rwtm.r()])
        for t in range(7):
            K.tr(pL.ap()[:, t * N:(t + 1) * N], shs.ap()[:, t * 128:(t + 1) * 128], identf.ap()[0:NS, 0:NS],
                 [shs.r(), identf.r()], [pL.r()], inc=(t == 6))
        K.cp(prevs.ap(), pL.ap()[:, 0:7 * N].rearrange("p (t n) -> p t n", t=7), [pL.r()], [prevs.r()])
        B["_rw_res"] = [rws.r(), prevs.r()]
        for _ in rwkv_prep(K, C, pc, LW, N, rws.ap(), prevs.ap(), B, pB, pL, pM):
            pass
        r, k_, v = B["_rkv"]
        g = lambda n: B[n].ap()
        K.act(g("e1"), g("sig"), AF.Exp, [B["sig"].r()], [B["e1"].r()], scale=-C0)
        K.ts(g("e2"), g("kk"), -1.0, ALU.mult, [B["kk"].r()], [B["e2"].r()])
        K.tt(g("bb"), g("kk"), g("aic"), ALU.mult, [B["kk"].r(), B["aic"].r()], [B["bb"].r()])
        srcs = [(r, B["MX"].r()), (g("e1"), B["e1"].r()), (g("kp"), B["kp"].r()), (v, B["MX"].r()),
                (g("e2"), B["e2"].r()), (g("bb"), B["bb"].r())]
        for q, (ap, res) in enumerate(srcs):
            pp = pA if q < 4 else pB
            for t in range(2):
                col = ((q % 4) * 2 + t) * 128
                K.tr(pp.ap()[0:NS, col:col + 128], ap[:, t, :], identf.ap(), [res, identf.r()], [pp.r()])
        K.cp(pkT.ap()[:, 0:4, :], pA.ap()[0:NS, :].rearrange("p (q n) -> p q n", q=4), [pA.r()], [pkT.r()], eng="act")
        K.cp(pkT.ap()[:, 4:6, :], pB.ap()[0:NS, 0:512].rearrange("p (q n) -> p q n", q=2), [pB.r()], [pkT.r()])
        for q in range(6):
            K.dma(scr.ap()[:, :, q, :], pkT.ap()[:, q, :].rearrange("p (h k) -> p h k", h=4), r=[pkT.r()], w=[scr.r()])
        K.dma(pkh.ap(), scr.ap().rearrange("b h q k -> (b h) q k"), r=[scr.r()], w=[pkh.r()])
        rq, wq, kq, vq, aq, bq = (pkh.ap()[:, i, :] for i in range(6))
        K.tt(tmp.ap(), S.ap(), bc(aq, 1, [64, 64, 64]), ALU.mult, [S.r(), pkh.r()], [tmp.r()])
        K.red(sa.ap(), tmp.ap(), [tmp.r()], [sa.r()])
        K.tt(S.ap(), S.ap(), bc(wq, 1, [64, 64, 64]), ALU.mult, [S.r(), pkh.r()], [S.r()])
        K.tt(tmp.ap(), bc(sa.ap(), 2, [64, 64, 64]), bc(bq, 1, [64, 64, 64]), ALU.mult, [sa.r(), pkh.r()], [tmp.r()])
        K.tt(S.ap(), S.ap(), tmp.ap(), ALU.add, [S.r(), tmp.r()], [S.r()])
        K.tt(tmp.ap(), bc(vq, 2, [64, 64, 64]), bc(kq, 1, [64, 64, 64]), ALU.mult, [pkh.r()], [tmp.r()])
        K.tt(S.ap(), S.ap(), tmp.ap(), ALU.add, [S.r(), tmp.r()], [S.r()])
        K.dma(dr["s_rwkv"][l].rearrange("b h v k -> (b h) (v k)"), S.ap().rearrange("p v k -> p (v k)"), r=[S.r()])
        K.tt(tmp.ap(), S.ap(), bc(rq, 1, [64, 64, 64]), ALU.mult, [S.r(), pkh.r()], [tmp.r()])
        K.red(o.ap(), tmp.ap(), [tmp.r()], [o.r()])
        groupnorm64(K, o.ap().rearrange("p (g e) -> p g e", g=1), 64, 1, gn, [o.r()],
                    on.ap().rearrange("p (g e) -> p g e", g=1), [on.r()])
        K.dma(so.ap().rearrange("b (h v) -> (b h) v", h=4), on.ap(), r=[on.r()], w=[so.r()])
        K.dma(otm.ap(), so.ap(), r=[so.r()], w=[otm.r()])
        K.cp(otb.ap(), otm.ap(), [otm.r()], [otb.r()])
        for t in range(2):
            K.tr(pT.ap()[:, t * N:(t + 1) * N], otb.ap()[:, t * 128:(t + 1) * 128], identb.ap()[0:NS, 0:NS],
                 [otb.r(), identb.r()], [pT.r()], inc=(t == 1))
        rwkv_epilogue(K, C, pc, B, N, pT, yT.ap()[:, 4:6, T:T + NS], xr(yT, 4, range(4, 6)))
        P.barrier()
```

```python
import contextlib
import numpy as np
import concourse.bass as bass
import concourse.mybir as mybir
from concourse.bass_utils import run_bass_kernel_spmd

F32 = mybir.dt.float32
BF16 = mybir.dt.bfloat16
AF = mybir.ActivationFunctionType
ALU = mybir.AluOpType
AX = mybir.AxisListType

NCORES = 8
D = 1024
KT = 8
T = 2048
NS = 16
NT = T + NS
NCH = T // 128
DEPTH = 2
IN_DIM = 2968
OFF = dict(z=0, xbc=512, dt=1280, rw=1288, gq=2184, gk=2312, gv=2440, glo=2696, gg=2712)
F_DENSE = 2816
ALPHA = (2.0 * DEPTH) ** 0.25
LN_EPS = 1e-5
RMS_EPS = 1e-6
RWKV_GN_EPS = 64 * 1e-5
BLOCKS = [(0, 512), (512, 512), (1024, 512), (1536, 512), (2048, 16)]

ENGS = ["pe", "dve", "act", "pool", "sp"]


class Res:
    __slots__ = ("name", "w", "r", "excl")

    def __init__(self, name="", excl=False):
        self.name = name
        self.w = None
        self.r = []
        self.excl = excl


class Prog:
    NDMA = 8

    def __init__(self, nc):
        self.nc = nc
        self.q = {e: [] for e in ENGS}
        self.cnt = {e: 0 for e in ENGS}
        self.seen = {e: {} for e in ENGS}
        self.dma_i = {e: 0 for e in ENGS}
        self.dma_last = {}
        self.sems = {}

    def sem(self, key):
        if key not in self.sems:
            self.sems[key] = self.nc.alloc_semaphore(name="s_" + "_".join(str(k) for k in key))
        return self.sems[key]

    def _collect(self, eng, reads, writes):
        waits = {}

        def add(tok):
            if tok is None:
                return
            key, val = tok
            if self.seen[eng].get(key, 0) >= val:
                return
            if waits.get(key, 0) < val:
                waits[key] = val

        for r in reads:
            add(r.w)
        for w in writes:
            add(w.w)
            for t in w.r:
                add(t)
        if eng == "pe":
            waits.pop(("e", "pe"), None)
        for k, v in waits.items():
            self.seen[eng][k] = v
        return list(waits.items())

    def _commit(self, tok, reads, writes):
        for r in reads:
            r.r.append(tok)
            if len(r.r) > 64:
                r.r = _prune(r.r)
        for w in writes:
            w.w = tok
            w.r = []

    def op(self, eng, fn, reads=(), writes=(), inc=True, self_wait=False):
        assert inc or eng == "pe"
        if any(r.excl for r in reads):
            writes = list(writes) + [r for r in reads if r.excl]
            reads = [r for r in reads if not r.excl]
        waits = self._collect(eng, reads, writes)
        if self_wait and self.cnt[eng] > 0:
            waits.append((("e", eng), self.cnt[eng]))
        tok = (("e", eng), self.cnt[eng] + 1)
        if inc:
            self.cnt[eng] += 1
        self._commit(tok, reads, writes)

        def emit(e, fn=fn, waits=waits, inc=inc, eng=eng):
            for k, v in waits:
                e.wait_ge(self.sem(k), v)
            ins = fn(e)
            if inc:
                ins.then_inc(self.sem(("e", eng)), 1)
        self.q[eng].append(emit)
        return tok

    def dma(self, queue, out, in_, reads=(), writes=(), **kw):
        i = self.dma_i[queue]
        self.dma_i[queue] += 1
        slot = i % self.NDMA
        key = ("d", queue, slot)
        val = 16 * (i // self.NDMA + 1)
        waits = self._collect(queue, reads, writes)
        prev = val - 16
        if prev > 0 and self.seen[queue].get(key, 0) < prev:
            self.seen[queue][key] = prev
            waits.append((key, prev))
        tok = (key, val)
        self.dma_last[key] = val
        self._commit(tok, reads, writes)

        def emit(e, waits=waits, key=key):
            for k, v in waits:
                e.wait_ge(self.sem(k), v)
            e.dma_start(out=out, in_=in_, **kw).then_inc(self.sem(key), 16)
        self.q[queue].append(emit)
        return tok

    def barrier(self, engines=ENGS):
        toks = [(("e", e), self.cnt[e]) for e in ENGS if self.cnt[e] > 0]
        toks += list(self.dma_last.items())
        for eng in engines:
            waits = []
            for k, v in toks:
                if k == ("e", eng) and eng == "pe":
                    continue
                if self.seen[eng].get(k, 0) < v:
                    self.seen[eng][k] = v
                    waits.append((k, v))

            def emit(e, waits=waits):
                for k, v in waits:
                    e.wait_ge(self.sem(k), v)
            self.q[eng].append(emit)

    def emit(self):
        with self.nc.Block() as block:
            @block.tensor
            def _(e):
                for f in self.q["pe"]:
                    f(e)

            @block.vector
            def _(e):
                for f in self.q["dve"]:
                    f(e)

            @block.scalar
            def _(e):
                for f in self.q["act"]:
                    f(e)

            @block.gpsimd
            def _(e):
                for f in self.q["pool"]:
                    f(e)

            @block.sync
            def _(e):
                for f in self.q["sp"]:
                    f(e)


def _prune(toks):
    best = {}
    for k, v in toks:
        if best.get(k, 0) < v:
            best[k] = v
    return list(best.items())


class Tn:
    def __init__(self, h, name, excl=False):
        self.h = h
        self.name = name
        self._res = {}
        self.excl = excl

    def ap(self):
        return self.h.ap()

    def r(self, key=0):
        if key not in self._res:
            self._res[key] = Res(f"{self.name}:{key}", self.excl)
        return self._res[key]


class KB:
    def __init__(self, nc):
        self.nc = nc
        self.P = Prog(nc)
        self.uid = 0

    def sb(self, stack, name, shape, dt=F32):
        self.uid += 1
        h = stack.enter_context(self.nc.sbuf_tensor(f"{name}_{self.uid}", list(shape), dt))
        return Tn(h, name)

    def ps(self, stack, name, shape, dt=F32):
        self.uid += 1
        h = stack.enter_context(self.nc.psum_tensor(f"{name}_{self.uid}", list(shape), dt))
        return Tn(h, name, excl=True)

    def mm(self, out, lhsT, rhs, r, w, start=True, stop=True, inc=None, self_wait=False, sgc=False):
        inc = stop if inc is None else inc
        kw = {"skip_group_check": True} if sgc else {}
        self.P.op("pe", lambda e: e.matmul(out, lhsT=lhsT, rhs=rhs, start=start, stop=stop, **kw),
                  reads=r, writes=w, inc=inc, self_wait=self_wait)

    def tr(self, out, in_, ident, r, w, inc=True):
        self.P.op("pe", lambda e: e.transpose(out, in_, ident), reads=r, writes=w, inc=inc)

    def act(self, out, in_, func, r, w, scale=None, bias=None, accum_out=None):
        kw = {}
        if scale is not None:
            kw["scale"] = scale
        if bias is not None:
            kw["bias"] = bias
        if accum_out is not None:
            kw["accum_out"] = accum_out
        self.P.op("act", lambda e: e.activation(out=out, in_=in_, func=func, **kw), reads=r, writes=w)

    def tt(self, out, in0, in1, op, r, w, eng="dve"):
        self.P.op(eng, lambda e: e.tensor_tensor(out=out, in0=in0, in1=in1, op=op), reads=r, writes=w)

    def ts(self, out, in0, s1, op0, r, w, s2=None, op1=None, eng="dve", accum_out=None):
        kw = {}
        if op1 is not None:
            kw["op1"] = op1
        if accum_out is not None:
            kw["accum_out"] = accum_out
        self.P.op(eng, lambda e: e.tensor_scalar(out=out, in0=in0, scalar1=s1, scalar2=s2, op0=op0, **kw),
                  reads=r, writes=w)

    def stt(self, out, in0, scalar, in1, op0, op1, r, w, eng="dve"):
        self.P.op(eng, lambda e: e.scalar_tensor_tensor(out=out, in0=in0, scalar=scalar, in1=in1, op0=op0, op1=op1),
                  reads=r, writes=w)

    def cp(self, out, in_, r, w, eng="dve"):
        if eng == "act":
            self.P.op("act", lambda e: e.activation(out=out, in_=in_, func=AF.Copy), reads=r, writes=w)
        else:
            self.P.op(eng, lambda e: e.tensor_copy(out=out, in_=in_), reads=r, writes=w)

    def recip(self, out, in_, r, w):
        self.P.op("dve", lambda e: e.reciprocal(out=out, in_=in_), reads=r, writes=w)

    def red(self, out, in_, r, w, op=ALU.add, axis=AX.X, eng="dve"):
        self.P.op(eng, lambda e: e.tensor_reduce(out=out, in_=in_, axis=axis, op=op), reads=r, writes=w)

    def memset(self, ap, val, w, eng="dve"):
        self.P.op(eng, lambda e: e.memset(ap, val), writes=w)

    def dma(self, out, in_, r=(), w=(), q="sp", **kw):
        return self.P.dma(q, out, in_, reads=r, writes=w, **kw)


W_SHAPES = dict(
    w_ada=[DEPTH, D, 6 * D], b_ada=[DEPTH, 6 * D], w_in=[DEPTH, D, IN_DIM], w_out=[DEPTH, D, D],
    ssd_conv_w=[DEPTH, 4, 768], ssd_conv_b=[DEPTH, 768], ssd_dt_bias=[DEPTH, 8], ssd_a_log=[DEPTH, 8],
    ssd_d=[DEPTH, 8], ssd_norm_g=[DEPTH, 512], rwkv_mu=[DEPTH, 896], rwkv_w0=[DEPTH, 256],
    rwkv_w2=[DEPTH, 32, 256], rwkv_a0=[DEPTH, 256], rwkv_a2=[DEPTH, 32, 256], rwkv_g2=[DEPTH, 64, 256],
    rwkv_k_k=[DEPTH, 256], rwkv_k_a=[DEPTH, 256], rwkv_r_k=[DEPTH, 4, 64], rwkv_ln_g=[DEPTH, 256],
    rwkv_ln_b=[DEPTH, 256], gla_w_gk2=[DEPTH, 16, 128], gla_b_gk=[DEPTH, 128], gla_norm_g=[DEPTH, 64],
    ln_mix_g=[DEPTH, D], ln_mix_b=[DEPTH, D], ln_ffn_g=[DEPTH, D], ln_ffn_b=[DEPTH, D],
    ffn_w_gate=[1, D, F_DENSE], ffn_w_up=[1, D, F_DENSE], ffn_w_down=[1, F_DENSE, D],
    moe_router=[1, D, 8], moe_w_gate=[1, 8, D, D], moe_w_up=[1, 8, D, D], moe_w_down=[1, 8, D, D],
)
IN_SHAPES = dict(
    xp=[T, D], xs=[NS, D], cc=[1 + NS, D],
    st_ssd=[DEPTH, NS, 8, 64, 64], st_conv=[DEPTH, NS, 3, 768], st_rwkv=[DEPTH, NS, 4, 64, 64],
    st_shift=[DEPTH, NS, 896], st_gla=[DEPTH, NS, 4, 32, 64],
)
OUT_SHAPES = dict(
    y_p=[T, D], y_s=[NS, D],
    p_ssd=[DEPTH, 8, 64, 64], p_conv=[DEPTH, 3, 768], p_rwkv=[DEPTH, 4, 64, 64], p_shift=[DEPTH, 896],
    p_gla=[DEPTH, 4, 32, 64],
    s_ssd=[DEPTH, NS, 8, 64, 64], s_conv=[DEPTH, NS, 3, 768], s_rwkv=[DEPTH, NS, 4, 64, 64],
    s_shift=[DEPTH, NS, 896], s_gla=[DEPTH, NS, 4, 32, 64],
)

def xr(t, b, tiles=range(KT)):
    return [t.r((d, b)) for d in tiles]


SH1, SC1, GT1, SH2, SC2, GT2 = 0, 8, 16, 24, 32, 40


def build(stub_mixer=False, dbg=None, n_layers=DEPTH):
    nc = bass.Bass("TRN2", target_bir_lowering=False)
    K = KB(nc)
    P = K.P
    dr = {}
    for n, s in IN_SHAPES.items():
        dr[n] = nc.dram_tensor(n, s, F32, kind="ExternalInput").ap()
    for n, s in W_SHAPES.items():
        dr[n] = nc.dram_tensor(n, s, F32, kind="ExternalInput").ap()
    for n, s in OUT_SHAPES.items():
        dr[n] = nc.dram_tensor(n, s, F32, kind="ExternalOutput").ap()
    dbg_out = {}
    if dbg:
        for n, s in dbg.items():
            dbg_out[n] = nc.dram_tensor("dbg_" + n, s, F32, kind="ExternalOutput").ap()
    out_res = Res("outputs")

    with contextlib.ExitStack() as perm, nc.allow_non_contiguous_dma(reason="small param loads"):
        xT = K.sb(perm, "xT", [128, KT, NT], F32)
        modT = [K.sb(perm, f"modT{l}", [128, 48, 1 + NS], F32) for l in range(DEPTH)]
        identf = K.sb(perm, "identf", [128, 128], F32)
        identb = K.sb(perm, "identb", [128, 128], BF16)
        onesM = K.sb(perm, "onesM", [128, 128], F32)
        ones1 = K.sb(perm, "ones1", [128, 128], F32)
        maskU = K.sb(perm, "maskU", [128, 128], F32)
        maskSU = K.sb(perm, "maskSU", [128, 128], F32)
        maskSL = K.sb(perm, "maskSL", [128, 128], F32)
        blk64 = K.sb(perm, "blk64", [128, 128], F32)
        C = dict(ones1f=ones1, xT=xT, modT=modT, identf=identf, identb=identb, onesM=onesM, ones1=ones1,
                 maskU=maskU, maskSU=maskSU, maskSL=maskSL, blk64=blk64)

        def sel(t, val_keep, cmp, fill, base=0, cm=1, pat=None, ap=None):
            ap = t.ap() if ap is None else ap
            pat = [[-1, ap.shape[-1]]] if pat is None else pat
            P.op("pool", lambda e: e.affine_select(out=ap, in_=ap, pattern=pat, compare_op=cmp, fill=fill,
                                                   base=base, channel_multiplier=cm),
                 reads=[t.r()], writes=[t.r()])

        K.memset(identf.ap(), 0.0, [identf.r()], eng="pool")
        sel(identf, 0, ALU.not_equal, 1.0)
        K.cp(identb.ap(), identf.ap(), [identf.r()], [identb.r()], eng="pool")
        K.memset(onesM.ap(), 1.0 / D, [onesM.r()], eng="pool")
        K.memset(ones1.ap(), 1.0, [ones1.r()], eng="pool")
        K.memset(maskU.ap(), 1.0, [maskU.r()], eng="pool")
        sel(maskU, 1, ALU.is_ge, 0.0, cm=-1, pat=[[1, 128]])
        K.memset(maskSU.ap(), 1.0, [maskSU.r()], eng="pool")
        sel(maskSU, 1, ALU.is_gt, 0.0, cm=-1, pat=[[1, 128]])
        K.memset(maskSL.ap(), 1.0, [maskSL.r()], eng="pool")
        sel(maskSL, 1, ALU.is_gt, 0.0)
        K.memset(blk64.ap(), 0.0, [blk64.r()], eng="pool")
        K.memset(blk64.ap()[0:64, 0:64], 1.0, [blk64.r()], eng="pool")
        K.memset(blk64.ap()[64:128, 64:128], 1.0, [blk64.r()], eng="pool")

        with contextlib.ExitStack() as ph:
            NB0 = 4
            stage = [K.sb(ph, f"stg{i}", [128, D], F32) for i in range(NB0)]
            ctm = K.sb(ph, "ctm", [1 + NS, D], F32)
            scT = K.sb(ph, "scT", [128, KT, 1 + NS], BF16)
            wada = [K.sb(ph, f"wada{i}", [128, KT, 512], BF16) for i in range(NB0)]
            bB = [K.sb(ph, f"bB{i}", [1 + NS, 512], F32) for i in range(NB0)]
            modsb = [K.sb(ph, f"modsb{i}", [1 + NS, 512], F32) for i in range(NB0)]
            pst = [K.ps(ph, f"pst{i}", [128, 1024], F32) for i in range(2)]
            psm = [K.ps(ph, f"psm{i}", [128, 512], F32) for i in range(2)]
            pss = K.ps(ph, "pss", [128, 512], F32)
            for tt in range(NCH + 1):
                st = stage[tt % NB0]
                pt = pst[tt % 2]
                n = 128 if tt < NCH else NS
                src = dr["xp"][tt * 128:(tt + 1) * 128, :] if tt < NCH else dr["xs"]
                K.dma(st.ap()[0:n, :], src, w=[st.r()])
                for d in range(KT):
                    K.tr(pt.ap()[:, d * n:(d + 1) * n], st.ap()[0:n, d * 128:(d + 1) * 128],
                         identf.ap()[0:n, 0:n], [st.r(), identf.r()], [pt.r()], inc=(d == KT - 1))
                dst = xT.ap()[:, :, tt * 128:tt * 128 + n]
                srcp = pt.ap()[:, 0:KT * n].rearrange("p (d n) -> p d n", d=KT)
                K.cp(dst, srcp, [pt.r()], xr(xT, min(tt // 4, 4)), eng=("act" if tt % 2 == 0 else "dve"))
            K.dma(ctm.ap(), dr["cc"], w=[ctm.r()])
            for d in range(KT):
                K.tr(pss.ap()[:, d * 17:(d + 1) * 17], ctm.ap()[:, d * 128:(d + 1) * 128],
                     identf.ap()[0:17, 0:17], [ctm.r(), identf.r()], [pss.r()], inc=(d == KT - 1))
            K.act(scT.ap(), pss.ap()[:, 0:KT * 17].rearrange("p (d n) -> p d n", d=KT), AF.Silu,
                  [pss.r()], [scT.r()])
            it = 0
            for l in range(DEPTH):
                for j in range(12):
                    wb = wada[it % NB0]
                    bb = bB[it % NB0]
                    ms = modsb[it % NB0]
                    pm = psm[it % 2]
                    K.dma(wb.ap(), dr["w_ada"][l, :, j * 512:(j + 1) * 512].rearrange("(k p) n -> p k n", p=128),
                          w=[wb.r()], q="pool")
                    K.dma(bb.ap(), dr["b_ada"][l:l + 1, j * 512:(j + 1) * 512].to_broadcast([1 + NS, 512]),
                          w=[bb.r()])
                    for k in range(KT):
                        K.mm(pm.ap()[0:17, :], scT.ap()[:, k, :], wb.ap()[:, k, :], [scT.r(), wb.r()], [pm.r()],
                             start=(k == 0), stop=(k == KT - 1))
                    K.tt(ms.ap(), pm.ap()[0:17, :], bb.ap(), ALU.add, [pm.r(), bb.r()], [ms.r()])
                    for q4 in range(4):
                        K.tr(pss.ap()[:, q4 * 17:(q4 + 1) * 17], ms.ap()[:, q4 * 128:(q4 + 1) * 128],
                             identf.ap()[0:17, 0:17], [ms.r(), identf.r()], [pss.r()], inc=(q4 == 3))
                    K.cp(modT[l].ap()[:, j * 4:(j + 1) * 4, :],
                         pss.ap()[:, 0:4 * 17].rearrange("p (d n) -> p d n", d=4), [pss.r()], [modT[l].r()],
                         eng="act")
                    it += 1
                for seg in (SC1, GT1, SC2, GT2):
                    K.ts(modT[l].ap()[:, seg:seg + 8, :], modT[l].ap()[:, seg:seg + 8, :], 1.0, ALU.add,
                         [modT[l].r()], [modT[l].r()])
            P.barrier()

        for l in range(n_layers):
            layer(K, dr, C, l, stub_mixer, dbg_out)

        with contextlib.ExitStack() as ph:
            stage = [K.sb(ph, f"ostg{i}", [128, D], F32) for i in range(2)]
            pst = [K.ps(ph, f"opst{i}", [128, 1024], F32) for i in range(2)]
            for tt in range(NCH + 1):
                st = stage[tt % 2]
                pt = pst[tt % 2]
                n = 128 if tt < NCH else NS
                for d in range(KT):
                    K.tr(pt.ap()[0:n, d * 128:(d + 1) * 128], xT.ap()[:, d, tt * 128:tt * 128 + n],
                         identf.ap(), [xT.r((d, min(tt // 4, 4))), identf.r()], [pt.r()], inc=(d == KT - 1))
                K.cp(st.ap()[0:n, :], pt.ap()[0:n, :], [pt.r()], [st.r()], eng=("act" if tt % 2 == 0 else "dve"))
                dst = dr["y_p"][tt * 128:(tt + 1) * 128, :] if tt < NCH else dr["y_s"]
                K.dma(dst, st.ap()[0:n, :], r=[st.r()])
            P.barrier()
    with nc.allow_non_contiguous_dma(reason="small param loads"):
        P.emit()
    return nc


def modulate(K, C, l, src, dst, b, sh, sc, dres):
    mod = C["modT"][l]
    c0, n = BLOCKS[b]
    if b < 4:
        for d in range(KT):
            K.act(dst.ap()[:, d, c0:c0 + n], src.ap()[:, d, c0:c0 + n], AF.Identity,
                  [src.r((d, b)), mod.r()], [dres(d)],
                  scale=mod.ap()[:, sc + d, 0:1], bias=mod.ap()[:, sh + d, 0:1])
    else:
        tmp = C["tmp_s"]
        K.tt(tmp.ap(), src.ap()[:, :, c0:c0 + n], mod.ap()[:, sc:sc + 8, 1:1 + NS], ALU.mult,
             xr(src, b) + [mod.r()], [tmp.r()])
        K.tt(dst.ap()[:, :, c0:c0 + n], tmp.ap(), mod.ap()[:, sh:sh + 8, 1:1 + NS], ALU.add,
             [tmp.r(), mod.r()], [dres(d) for d in range(KT)])


def layernorm(K, C, l, gi, psA, psB, scr):
    xT, onesM, lncol = C["xT"], C["onesM"], C["lncol"]
    sq, mean_sb, var, tt_ = scr["sq"], scr["mean"], scr["var"], scr["t"]
    for b, (c0, n) in enumerate(BLOCKS):
        for d in range(KT):
            s = sq[d % 2]
            xs = xT.ap()[:, d, c0:c0 + n]
            K.act(s.ap()[:, :n], xs, AF.Square, [xT.r((d, b))], [s.r()])
            K.mm(psA.ap()[:, :n], onesM.ap(), xs, [onesM.r(), xT.r((d, b))], [psA.r()],
                 start=(d == 0), stop=(d == KT - 1), inc=True)
            K.mm(psB.ap()[:, :n], onesM.ap(), s.ap()[:, :n], [onesM.r(), s.r()], [psB.r()],
                 start=(d == 0), stop=(d == KT - 1), inc=True)
        K.cp(mean_sb.ap()[:, :n], psA.ap()[:, :n], [psA.r()], [mean_sb.r()], eng="act")
        K.tt(var.ap()[:, :n], mean_sb.ap()[:, :n], mean_sb.ap()[:, :n], ALU.mult, [mean_sb.r()], [var.r()])
        K.tt(var.ap()[:, :n], psB.ap()[:, :n], var.ap()[:, :n], ALU.subtract, [psB.r(), var.r()], [var.r()])
        K.act(var.ap()[:, :n], var.ap()[:, :n], AF.Sqrt, [var.r()], [var.r()], bias=LN_EPS)
        K.recip(var.ap()[:, :n], var.ap()[:, :n], [var.r()], [var.r()])
        for d in range(KT):
            t = tt_[d % 2]
            xs = xT.ap()[:, d, c0:c0 + n]
            K.tt(t.ap()[:, :n], xs, mean_sb.ap()[:, :n], ALU.subtract, [xT.r((d, b)), mean_sb.r()], [t.r()])
            K.tt(t.ap()[:, :n], t.ap()[:, :n], var.ap()[:, :n], ALU.mult, [t.r(), var.r()], [t.r()])
            K.act(xs, t.ap()[:, :n], AF.Identity, [t.r(), lncol.r()], [xT.r((d, b))],
                  scale=lncol.ap()[:, gi, d:d + 1], bias=lncol.ap()[:, gi + 1, d:d + 1])


def residual_add(K, C, l, ps, n, dout, b, gt, comb=None):
    xT, mod = C["xT"], C["modT"][l]
    c0, _ = BLOCKS[b]
    xs = xT.ap()[:, dout, c0:c0 + n]
    src = ps.ap()[:, :n]
    rd = [ps.r(), mod.r(), xT.r((dout, b))]
    if comb is not None:
        tmp = C["tmp_c"][dout % 2]
        K.tt(tmp.ap()[:, :n], src, comb.ap()[:, c0:c0 + n], ALU.mult, [ps.r(), comb.r(b)], [tmp.r()])
        src = tmp.ap()[:, :n]
        rd = [tmp.r(), mod.r(), xT.r((dout, b))]
    if b < 4:
        K.stt(xs, src, mod.ap()[:, gt + dout, 0:1], xs, ALU.mult, ALU.add, rd, [xT.r((dout, b))])
    else:
        tmp2 = C["tmp_s2"]
        K.tt(tmp2.ap(), src, mod.ap()[:, gt + dout, 1:1 + NS], ALU.mult, rd[:2], [tmp2.r()])
        K.tt(xs, tmp2.ap(), xs, ALU.add, [tmp2.r(), xT.r((dout, b))], [xT.r((dout, b))])


def scale_x(K, C):
    xT = C["xT"]
    for b, (c0, n) in enumerate(BLOCKS):
        for d in range(KT):
            xs = xT.ap()[:, d, c0:c0 + n]
            K.P.op("act", lambda e, xs=xs: e.mul(out=xs, in_=xs, mul=ALPHA), reads=[xT.r((d, b))],
                   writes=[xT.r((d, b))])


def ffn_gateup(K, C, l, hT, actb, wpool, srcs, nf, ps):
    wg_src, wu_src, wd_src = srcs
    sg = C["sg"]

    def unit():
        u = wpool["bufs"][wpool["i"] % len(wpool["bufs"])]
        wpool["i"] += 1
        return u

    i = 0
    for f0 in range(0, nf, 4):
        nfu = min(4, nf - f0)
        WG, WU = unit(), unit()
        K.dma(WG.ap()[:, :, 0:nfu * 128], wg_src[:, f0 * 128:(f0 + nfu) * 128].rearrange("(k p) n -> p k n", p=128),
              w=[WG.r()], q="pool")
        K.dma(WU.ap()[:, :, 0:nfu * 128], wu_src[:, f0 * 128:(f0 + nfu) * 128].rearrange("(k p) n -> p k n", p=128),
              w=[WU.r()], q="pool")
        for fu in range(nfu):
            f = f0 + fu
            for b, (c0, n) in enumerate(BLOCKS):
                pg, pu = ps["g"][i % 2], ps["u"][i % 2]
                for k in range(KT):
                    K.mm(pg.ap()[:, :n], WG.ap()[:, k, fu * 128:(fu + 1) * 128], hT.ap()[:, k, c0:c0 + n],
                         [WG.r(), hT.r((k, b))], [pg.r()], start=(k == 0), stop=(k == KT - 1))
                for k in range(KT):
                    K.mm(pu.ap()[:, :n], WU.ap()[:, k, fu * 128:(fu + 1) * 128], hT.ap()[:, k, c0:c0 + n],
                         [WU.r(), hT.r((k, b))], [pu.r()], start=(k == 0), stop=(k == KT - 1))
                s_ = sg[i % 2]
                K.act(s_.ap()[:, :n], pg.ap()[:, :n], AF.Silu, [pg.r()], [s_.r()])
                K.tt(actb.ap()[:, f, c0:c0 + n], s_.ap()[:, :n], pu.ap()[:, :n], ALU.mult, [s_.r(), pu.r()],
                     [actb.r((f, b))])
                i += 1
                yield


def ffn_down(K, C, l, actb, wpool, srcs, nf, ps, comb=None):
    wg_src, wu_src, wd_src = srcs

    def unit():
        u = wpool["bufs"][wpool["i"] % len(wpool["bufs"])]
        wpool["i"] += 1
        return u

    i = 0
    for dh in range(2):
        WD = unit()
        K.dma(WD.ap()[:, 0:nf, :], wd_src[:, dh * 512:(dh + 1) * 512].rearrange("(f p) n -> p f n", p=128),
              w=[WD.r()], q="pool")
        for dd in range(4):
            dout = dh * 4 + dd
            for b, (c0, n) in enumerate(BLOCKS):
                pd = ps["d"][i % 2]
                for f in range(nf):
                    K.mm(pd.ap()[:, :n], WD.ap()[:, f, dd * 128:(dd + 1) * 128], actb.ap()[:, f, c0:c0 + n],
                         [WD.r(), actb.r((f, b))], [pd.r()], start=(f == 0), stop=(f == nf - 1))
                residual_add(K, C, l, pd, n, dout, b, GT2, comb=comb)
                i += 1


def ffn_group(K, C, l, hT, actb, wpool, srcs, nf, ps, comb=None):
    for _ in ffn_gateup(K, C, l, hT, actb, wpool, srcs, nf, ps):
        pass
    ffn_down(K, C, l, actb, wpool, srcs, nf, ps, comb=comb)


def moe_routing(K, C, dr, l, ph, ps):
    xT, mod, identf = C["xT"], C["modT"][l], C["identf"]
    router = K.sb(ph, "router", [128, KT, 8], F32)
    K.dma(router.ap(), dr["moe_router"][0].rearrange("(k p) e -> p k e", p=128), w=[router.r()])
    combT = K.sb(ph, "combT", [8, NT], F32)
    tp = C["tpair"]
    sm = {n: K.sb(ph, "rt_" + n, [128, 8], F32) for n in ["lg", "eq1", "l2", "eq2", "cb"]}
    sc1 = {n: K.sb(ph, "rs_" + n, [128, 1], F32) for n in ["m1", "m2", "e", "w1", "w2"]}
    pl, pt = ps["rA"], ps["rB"]
    rmod = K.sb(ph, "rmod", [128, KT, 8], F32)
    crow = K.sb(ph, "crow", [1, 8], F32)
    K.tt(rmod.ap(), router.ap(), bc(mod.ap()[:, SC2:SC2 + 8, 0], 2, [128, KT, 8]), ALU.mult, [router.r(), mod.r()],
         [rmod.r()])
    for d in range(KT):
        K.mm(pt.ap()[0:1, 0:8], mod.ap()[:, SH2 + d, 0:1], router.ap()[:, d, :], [mod.r(), router.r()], [pt.r()],
             start=(d == 0), stop=(d == KT - 1), inc=True)
    K.cp(crow.ap(), pt.ap()[0:1, 0:8], [pt.r()], [crow.r()])
    yield
    for tt in range(NCH + 1):
        n = 128 if tt < NCH else NS
        c0 = tt * 128
        b = min(tt // 4, 4)
        hap = tp.ap().rearrange("p a (b c) -> p (a b) c", c=128)
        hres = [tp.r(0), tp.r(1)]
        if tt < NCH:
            for d in range(KT):
                K.mm(pl.ap()[0:n, 0:8], xT.ap()[:, d, c0:c0 + n], rmod.ap()[:, d, :], [xT.r((d, b)), rmod.r()], [pl.r()],
                     start=(d == 0), stop=False, inc=True)
            K.mm(pl.ap()[0:n, 0:8], C["ones1f"].ap()[0:1, 0:n], crow.ap(), [C["ones1f"].r(), crow.r()], [pl.r()],
                 start=False, stop=True, inc=True)
        else:
            K.tt(hap[:, :, 0:n], xT.ap()[:, :, c0:c0 + n], mod.ap()[:, SC2:SC2 + 8, 1:1 + NS], ALU.mult,
                 xr(xT, b) + [mod.r()], hres)
            K.tt(hap[:, :, 0:n], hap[:, :, 0:n], mod.ap()[:, SH2:SH2 + 8, 1:1 + NS], ALU.add,
                 hres + [mod.r()], hres)
        if tt == NCH:
            for d in range(KT):
                K.mm(pl.ap()[0:n, 0:8], hap[:, d, 0:n], router.ap()[:, d, :], hres + [router.r()], [pl.r()],
                     start=(d == 0), stop=(d == KT - 1), inc=True)
        lg, eq1, l2, eq2, cb = (sm[k].ap()[0:n, :] for k in ["lg", "eq1", "l2", "eq2", "cb"])
        m1, m2, ee, w1, w2 = (sc1[k].ap()[0:n, :] for k in ["m1", "m2", "e", "w1", "w2"])
        R = lambda *ks: [(sm[k] if k in sm else sc1[k]).r() for k in ks]
        K.cp(lg, pl.ap()[0:n, 0:8], [pl.r()], R("lg"))
        yield
        K.red(m1, lg, R("lg"), R("m1"), op=ALU.max)
        yield
        K.ts(eq1, lg, m1, ALU.is_equal, R("lg", "m1"), R("eq1"))
        yield
        K.stt(l2, eq1, -1e30, lg, ALU.mult, ALU.add, R("eq1", "lg"), R("l2"))
        yield
        K.red(m2, l2, R("l2"), R("m2"), op=ALU.max)
        yield
        K.ts(eq2, l2, m2, ALU.is_equal, R("l2", "m2"), R("eq2"))
        yield
        K.tt(ee, m2, m1, ALU.subtract, R("m1", "m2"), R("e"))
        yield
        K.act(ee, ee, AF.Exp, R("e"), R("e"))
        yield
        K.ts(w1, ee, 1.0, ALU.add, R("e"), R("w1"))
        yield
        K.recip(w1, w1, R("w1"), R("w1"))
        yield
        K.tt(w2, ee, w1, ALU.mult, R("e", "w1"), R("w2"))
        yield
        K.ts(cb, eq1, w1, ALU.mult, R("eq1", "w1"), R("cb"))
        yield
        K.stt(cb, eq2, w2, cb, ALU.mult, ALU.add, R("eq2", "w2", "cb"), R("cb"))
        yield
        K.tr(pt.ap()[0:8, 0:n], cb, identf.ap()[0:n, 0:n], R("cb") + [identf.r()], [pt.r()])
        yield
        K.cp(combT.ap()[:, c0:c0 + n], pt.ap()[0:8, 0:n], [pt.r()], [combT.r()], eng="act")
        yield
    C["_combT"] = combT
    yield


def layer(K, dr, C, l, stub_mixer, dbg_out):
    nc, P = K.nc, K.P
    xT, mod = C["xT"], C["modT"][l]
    with contextlib.ExitStack() as lay:
        bufA = K.sb(lay, "bufA", [128, KT, NT], BF16)
        lncol = K.sb(lay, "lncol", [128, 4, KT], F32)
        C["lncol"] = lncol
        C["tmp_s"] = K.sb(lay, "tmp_s", [128, KT, NS], F32)
        C["tmp_s2"] = K.sb(lay, "tmp_s2", [128, NS], F32)
        for i, nme in enumerate(["ln_mix_g", "ln_mix_b", "ln_ffn_g", "ln_ffn_b"]):
            K.dma(lncol.ap()[:, i, :], dr[nme][l].rearrange("(d p) -> p d", p=128), w=[lncol.r()])

        with contextlib.ExitStack() as ph:
            if stub_mixer:
                for b in range(5):
                    modulate(K, C, l, xT, bufA, b, SH1, SC1, lambda d, b=b: bufA.r((d, b)))
            else:
                mixers(K, dr, C, l, bufA, dbg_out)
            P.barrier()
            wout = K.sb(ph, "wout", [128, KT, D], BF16)
            K.dma(wout.ap(), dr["w_out"][l].rearrange("(k p) n -> p k n", p=128), w=[wout.r()], q="pool")
            scale_x(K, C)
            with contextlib.ExitStack() as ph2:
                pso = [K.ps(ph2, f"pso{i}", [128, 512], F32) for i in range(4)]
                i = 0
                for dout in range(KT):
                    for b, (c0, n) in enumerate(BLOCKS):
                        pd = pso[i % 4]
                        for k in range(KT):
                            K.mm(pd.ap()[:, :n], wout.ap()[:, k, dout * 128:(dout + 1) * 128],
                                 bufA.ap()[:, k, c0:c0 + n], [wout.r(), bufA.r((k, b))], [pd.r()],
                                 start=(k == 0), stop=(k == KT - 1))
                        residual_add(K, C, l, pd, n, dout, b, GT1)
                        i += 1
                P.barrier()

        with contextlib.ExitStack() as ph:
            hT = K.sb(ph, "hT", [128, KT, NT], BF16)
            tpair = K.sb(ph, "tpair", [128, 2, 512], F32)
            C["tpair"] = tpair

            class _V:
                def __init__(self, i):
                    self.i = i

                def ap(self):
                    return tpair.ap()[:, self.i, :]

                def r(self, key=0):
                    return tpair.r(self.i)
            scr = dict(sq=[K.sb(ph, f"sq{i}", [128, 512], F32) for i in range(2)],
                       mean=K.sb(ph, "mean", [128, 512], F32), var=K.sb(ph, "var", [128, 512], F32),
                       t=[_V(0), _V(1)])
            C["sg"] = scr["sq"]
            C["tmp_c"] = scr["t"]
            W = dict(bufs=[K.sb(ph, f"WP{i}", [128, KT, 512], BF16) for i in range(4)], i=0)
            ps = dict(g=[K.ps(ph, f"pg{i}", [128, 512], F32) for i in range(2)],
                      u=[K.ps(ph, f"pu{i}", [128, 512], F32) for i in range(2)],
                      d=[K.ps(ph, f"pd{i}", [128, 512], F32) for i in range(2)])
            psA = K.ps(ph, "psA", [128, 512], F32)
            psB = K.ps(ph, "psB", [128, 512], F32)
            layernorm(K, C, l, 0, psA, psB, scr)
            for b in range(5):
                modulate(K, C, l, xT, hT, b, SH2, SC2, lambda d, b=b: hT.r((d, b)))
            if l % 2 == 0:
                scale_x(K, C)
                i = l // 2
                for f0 in range(0, F_DENSE // 128, 8):
                    nf = min(8, F_DENSE // 128 - f0)
                    srcs = (dr["ffn_w_gate"][i, :, f0 * 128:(f0 + nf) * 128],
                            dr["ffn_w_up"][i, :, f0 * 128:(f0 + nf) * 128],
                            dr["ffn_w_down"][i, f0 * 128:(f0 + nf) * 128, :])
                    ffn_group(K, C, l, hT, bufA, W, srcs, nf, ps)
            else:
                ps["rA"], ps["rB"] = psA, psB
                i = l // 2
                srcs0 = (dr["moe_w_gate"][i, 0], dr["moe_w_up"][i, 0], dr["moe_w_down"][i, 0])
                interleave([moe_routing(K, C, dr, l, ph, ps), ffn_gateup(K, C, l, hT, bufA, W, srcs0, 8, ps)],
                           ratio=[6, 1])
                combT = C["_combT"]
                scale_x(K, C)
                combB = K.sb(ph, "combB", [128, NT], F32)
                sele = K.sb(ph, "sele", [8, 128], F32)
                i = l // 2
                for e_ in range(8):
                    K.memset(sele.ap(), 0.0, [sele.r()])
                    K.P.op("dve", lambda e, e_=e_: e.memset(sele.ap()[e_:e_ + 1, :], 1.0), reads=[sele.r()],
                           writes=[sele.r()]) if False else K.ts(
                        sele.ap(), C["identf"].ap()[0:8, e_:e_ + 1].to_broadcast([8, 128]), 1.0, ALU.mult,
                        [C["identf"].r()], [sele.r()])
                    for b, (c0, n) in enumerate(BLOCKS):
                        pb = ps["d"][b % 2]
                        K.mm(pb.ap()[:, :n], sele.ap(), combT.ap()[:, c0:c0 + n], [sele.r(), combT.r()],
                             [pb.r()])
                        K.cp(combB.ap()[:, c0:c0 + n], pb.ap()[:, :n], [pb.r()], [combB.r(b)], eng="act")
                    srcs = (dr["moe_w_gate"][i, e_], dr["moe_w_up"][i, e_], dr["moe_w_down"][i, e_])
                    if e_ > 0:
                        for _ in ffn_gateup(K, C, l, hT, bufA, W, srcs, 8, ps):
                            pass
                    ffn_down(K, C, l, bufA, W, srcs, 8, ps, comb=combB)
            layernorm(K, C, l, 2, psA, psB, scr)
            P.barrier()


def make_in_maps(inp):
    g = lambda k: np.ascontiguousarray(np.asarray(inp[k], dtype=np.float32))
    xp, xs, cp, cs = g("x_prompt"), g("x_sample"), g("c_prompt"), g("c_sample")
    st = {k: g(k) for k in ["state_ssd", "state_ssd_conv", "state_rwkv", "state_rwkv_shift", "state_gla"]}
    wts = {k: g(k) for k in W_SHAPES}
    maps = []
    for c in range(NCORES):
        sl = slice(c * NS, (c + 1) * NS)
        m = dict(wts)
        m["xp"] = xp[c]
        m["xs"] = np.ascontiguousarray(xs[sl, 0, :])
        m["cc"] = np.ascontiguousarray(np.concatenate([cp[c:c + 1], cs[sl]], axis=0))
        m["st_ssd"] = np.ascontiguousarray(st["state_ssd"][:, sl])
        m["st_conv"] = np.ascontiguousarray(st["state_ssd_conv"][:, sl])
        m["st_rwkv"] = np.ascontiguousarray(st["state_rwkv"][:, sl])
        m["st_shift"] = np.ascontiguousarray(st["state_rwkv_shift"][:, sl])
        m["st_gla"] = np.ascontiguousarray(st["state_gla"][:, sl])
        maps.append(m)
    return maps


_NC_CACHE = {}


def gather(results):
    R = lambda k: [np.asarray(r[k], dtype=np.float32) for r in results]
    y_p = np.stack(R("y_p"), axis=0)
    y_s = np.concatenate(R("y_s"), axis=0)[:, None, :]
    outs = [y_p, y_s]
    for k in ["p_ssd", "p_conv", "p_rwkv", "p_shift", "p_gla"]:
        outs.append(np.stack(R(k), axis=1))
    for k in ["s_ssd", "s_conv", "s_rwkv", "s_shift", "s_gla"]:
        outs.append(np.concatenate(R(k), axis=1))
    return tuple(np.ascontiguousarray(o) for o in outs)


def kernel(**inputs):
    if "nc" not in _NC_CACHE:
        _NC_CACHE["nc"] = build()
    res = run_bass_kernel_spmd(_NC_CACHE["nc"], make_in_maps(inputs), core_ids=list(range(NCORES)))
    return gather(res.results)


def bc(ap, axis, shape):
    return ap.unsqueeze(axis).to_broadcast(list(shape))


def sigmoid_chain(K, ap, res):
    K.act(ap, ap, AF.Ln, res, res, bias=1.0)
    K.act(ap, ap, AF.Exp, res, res, scale=-1.0)


def rsqrt_(K, ap, res, scale, eps):
    K.act(ap, ap, AF.Ln, res, res, scale=scale, bias=eps)
    K.act(ap, ap, AF.Exp, res, res, scale=-0.5)


def softplus_(K, x, tmp, r, n):
    xa, ta = x[0], tmp[0]
    K.act(ta, xa, AF.Abs, [x[1]], [tmp[1]])
    K.act(ta, ta, AF.Exp, [tmp[1]], [tmp[1]], scale=-1.0)
    K.act(ta, ta, AF.Ln, [tmp[1]], [tmp[1]], bias=1.0)
    K.ts(xa, xa, 0.0, ALU.max, [x[1]], [x[1]])
    K.tt(xa, xa, ta, ALU.add, [x[1], tmp[1]], [x[1]])


def make_hc(K, C, l, hc, c):
    xT, mod = C["xT"], C["modT"][l]
    import os
    if os.environ.get("HC_POOL", "1") == "1":
        tmp = C["hc_tmp"]
        xs = xT.ap()[:, :, c * 128:(c + 1) * 128]
        K.tt(tmp.ap(), xs, bc(mod.ap()[:, SC1:SC1 + 8, 0], 2, [128, KT, 128]), ALU.mult,
             xr(xT, c // 4) + [mod.r()], [tmp.r()], eng="pool")
        K.tt(hc.ap(), tmp.ap(), bc(mod.ap()[:, SH1:SH1 + 8, 0], 2, [128, KT, 128]), ALU.add,
             [tmp.r(), mod.r()], [hc.r()], eng="pool")
        return
    for d in range(KT):
        K.act(hc.ap()[:, d, :], xT.ap()[:, d, c * 128:(c + 1) * 128], AF.Identity,
              [xT.r((d, c // 4)), mod.r()], [hc.r()],
              scale=mod.ap()[:, SC1 + d, 0:1], bias=mod.ap()[:, SH1 + d, 0:1])


def mixers(K, dr, C, l, yT, dbg_out):
    P = K.P
    xT, mod = C["xT"], C["modT"][l]
    en = C.get("enable", ("ssd", "rwkv", "gla"))
    with contextlib.ExitStack() as mx:
        hc = [K.sb(mx, f"hc{i}", [128, KT, 128], BF16) for i in range(2)]
        hs = K.sb(mx, "hs", [128, KT, NS], BF16)
        C["hc"], C["hs"] = hc, hs
        C["hc_tmp"] = K.sb(mx, "hc_tmp", [128, KT, 128], F32)
        modulate(K, C, l, xT, _Shift(hs, 2048), 4, SH1, SC1, lambda d: hs.r())
        for name, tiles in (("ssd", range(0, 4)), ("rwkv", range(4, 6)), ("gla", range(6, 8))):
            if name not in en:
                for d in tiles:
                    for b, (c0, n) in enumerate(BLOCKS):
                        K.memset(yT.ap()[:, d, c0:c0 + n], 0.0, [yT.r((d, b))])
        if "ssd" in en and "gla" in en:
            ssd_gla_phase(K, dr, C, l, yT, dbg_out)
            P.barrier()
        else:
            if "ssd" in en:
                ssd_phase(K, dr, C, l, yT, dbg_out)
                P.barrier()
            if "gla" in en:
                gla_phase(K, dr, C, l, yT, dbg_out)
                P.barrier()
        if "rwkv" in en:
            rwkv_phase(K, dr, C, l, yT, dbg_out)
            P.barrier()


class _Shift:
    def __init__(self, t, off):
        self.t, self.off = t, off

    def ap(self):
        return _ShiftAP(self.t.ap(), self.off)

    def r(self, key=0):
        return self.t.r()


class _ShiftAP:
    def __init__(self, ap, off):
        self._ap, self.off = ap, off

    def __getitem__(self, key):
        p, d, s = key
        return self._ap[p, d, slice(s.start - self.off, s.stop - self.off)]


def ssd_phase(K, dr, C, l, yT, dbg_out):
    P = K.P
    nc = K.nc
    identb, identf, maskU, maskSL, ones1 = C["identb"], C["identf"], C["maskU"], C["maskSL"], C["ones1"]
    hc, hs = C["hc"], C["hs"]
    with contextlib.ExitStack() as ph:
        win = K.sb(ph, "win_ssd", [128, KT, 1288], BF16)
        K.dma(win.ap(), dr["w_in"][l, :, 0:1288].rearrange("(k p) n -> p k n", p=128), w=[win.r()], q="pool")
        convw = K.sb(ph, "convw", [128, 6, 4], F32)
        convb = K.sb(ph, "convb", [128, 6], F32)
        for i in range(4):
            K.dma(convw.ap()[:, :, i], dr["ssd_conv_w"][l, i].rearrange("(t p) -> p t", p=128), w=[convw.r()])
        K.dma(convb.ap(), dr["ssd_conv_b"][l].rearrange("(t p) -> p t", p=128), w=[convb.r()])
        normg = K.sb(ph, "normg", [128, 4], F32)
        K.dma(normg.ap(), dr["ssd_norm_g"][l].rearrange("(t p) -> p t", p=128), w=[normg.r()])
        dtbB = K.sb(ph, "dtbB", [128, 8], F32)
        aB = K.sb(ph, "aB", [128, 8], F32)
        dB = K.sb(ph, "dB", [128, 8], F32)
        K.dma(dtbB.ap(), dr["ssd_dt_bias"][l:l + 1, :].to_broadcast([128, 8]), w=[dtbB.r()])
        K.dma(aB.ap(), dr["ssd_a_log"][l:l + 1, :].to_broadcast([128, 8]), w=[aB.r()])
        K.dma(dB.ap(), dr["ssd_d"][l:l + 1, :].to_broadcast([128, 8]), w=[dB.r()])
        K.act(aB.ap(), aB.ap(), AF.Exp, [aB.r()], [aB.r()])
        K.ts(aB.ap(), aB.ap(), -1.0, ALU.mult, [aB.r()], [aB.r()])
        import os
        if os.environ.get("SKIP_SSD_PROMPT") != "1":
            ssd_prompt(K, dr, C, l, yT, win, convw, convb, normg, dtbB, aB, dB, dbg_out)
        P.barrier()
        if os.environ.get("SKIP_SSD_SAMPLE") != "1":
            ssd_sample(K, dr, C, l, yT, win, aB, dB, dtbB, dbg_out)


def ssd_prompt(K, dr, C, l, yT, win, convw, convb, normg, dtbB, aB, dB, dbg_out):
    P = K.P
    identb, identf, maskU, maskSL, ones1 = C["identb"], C["identf"], C["maskU"], C["maskSL"], C["ones1"]
    hc, hs = C["hc"], C["hs"]
    with contextlib.ExitStack() as ph:
        XB = [K.sb(ph, f"XB{i}", [128, 6, 131], F32) for i in range(2)]
        XC = [K.sb(ph, f"XC{i}", [128, 6, 128], BF16) for i in range(2)]
        cacc = [K.sb(ph, f"cacc{i}", [128, 128], F32) for i in range(2)]
        sz = K.sb(ph, "sz", [128, 512], F32)
        dtt = K.sb(ph, "dtt", [128, 8], F32)
        dtmp = K.sb(ph, "dtmp", [128, 8], F32)
        dtA = K.sb(ph, "dtA", [128, 8], F32)
        csb = K.sb(ph, "csb", [128, 16], F32)
        e1 = K.sb(ph, "e1", [128, 8], F32)
        el = K.sb(ph, "el", [128, 8], F32)
        tail = K.sb(ph, "tail", [128, 8], F32)
        Rt = K.sb(ph, "Rt", [128, 8, 128], F32)
        dec = K.sb(ph, "dec", [128, 8, 128], F32)
        Gs = K.sb(ph, "Gs", [128, 2, 128], F32)
        Mb = K.sb(ph, "Mb", [128, 8, 128], BF16)
        XT = K.sb(ph, "XT", [128, 640], BF16)
        xD = K.sb(ph, "xD", [128, 512], BF16)
        xw = K.sb(ph, "xw", [128, 512], BF16)
        t1 = K.sb(ph, "t1", [128, 512], F32)
        yn = K.sb(ph, "yn", [128, 512], BF16)
        ss = K.sb(ph, "ss", [128, 1], F32)
        HS32 = K.sb(ph, "HS32", [128, 4, 64], F32)
        HSb = K.sb(ph, "HSb", [128, 4, 64], BF16)
        hsT = K.sb(ph, "hsT", [128, 2, 128], F32)

        ps_x = K.ps(ph, "ps_x", [128, 1024], F32)
        ps_z = K.ps(ph, "ps_z", [128, 512], F32)
        ps_c = K.ps(ph, "ps_c", [128, 512], F32)
        ps_t = K.ps(ph, "ps_t", [128, 1024], BF16)
        ps_y = K.ps(ph, "ps_y", [128, 512], F32)
        ps_i = K.ps(ph, "ps_i", [128, 512], F32)
        ps_h = K.ps(ph, "ps_h", [128, 512], F32)

        K.memset(HS32.ap(), 0.0, [HS32.r()])
        K.memset(HSb.ap(), 0.0, [HSb.r()])
        K.memset(XB[0].ap()[:, :, 0:3], 0.0, [XB[0].r()])

        for c in range(NCH):
            h = hc[c % 2]
            make_hc(K, C, l, h, c)
            xb, xc = XB[c % 2], XC[c % 2]
            for ct in range(6):
                for k in range(KT):
                    K.mm(ps_x.ap()[:, ct * 128:(ct + 1) * 128], win.ap()[:, k, 512 + ct * 128:512 + (ct + 1) * 128],
                         h.ap()[:, k, :], [win.r(), h.r()], [ps_x.r()], start=(k == 0), stop=(k == KT - 1),
                         inc=(k == KT - 1 and ct == 5))
            K.cp(xb.ap()[:, :, 3:131], ps_x.ap()[:, 0:768].rearrange("p (t n) -> p t n", t=6), [ps_x.r()], [xb.r()],
                 eng="act")
            if c + 1 < NCH:
                K.cp(XB[(c + 1) % 2].ap()[:, :, 0:3], xb.ap()[:, :, 128:131], [xb.r()], [XB[(c + 1) % 2].r()])
            for ct in range(6):
                ca = cacc[ct % 2]
                K.ts(ca.ap(), xb.ap()[:, ct, 0:128], convw.ap()[:, ct, 0:1], ALU.mult, [xb.r(), convw.r(), convb.r()],
                     [ca.r()], s2=convb.ap()[:, ct:ct + 1], op1=ALU.add)
                for i in range(1, 4):
                    K.stt(ca.ap(), xb.ap()[:, ct, i:i + 128], convw.ap()[:, ct, i:i + 1], ca.ap(), ALU.mult, ALU.add,
                          [xb.r(), convw.r(), ca.r()], [ca.r()])
                K.act(xc.ap()[:, ct, :], ca.ap(), AF.Silu, [ca.r()], [xc.r()])
            for k in range(KT):
                K.mm(ps_z.ap(), h.ap()[:, k, :], win.ap()[:, k, 0:512], [h.r(), win.r()], [ps_z.r()],
                     start=(k == 0), stop=(k == KT - 1))
            for k in range(KT):
                K.mm(ps_c.ap()[:, 0:8], h.ap()[:, k, :], win.ap()[:, k, 1280:1288], [h.r(), win.r()], [ps_c.r()],
                     start=(k == 0), stop=(k == KT - 1))
            K.act(sz.ap(), ps_z.ap(), AF.Silu, [ps_z.r()], [sz.r()])
            K.tt(dtt.ap(), ps_c.ap()[:, 0:8], dtbB.ap(), ALU.add, [ps_c.r(), dtbB.r()], [dtt.r()])
            softplus_(K, (dtt.ap(), dtt.r()), (dtmp.ap(), dtmp.r()), None, None)
            K.tt(dtA.ap(), dtt.ap(), aB.ap(), ALU.mult, [dtt.r(), aB.r()], [dtA.r()])
            K.mm(ps_c.ap()[:, 8:16], maskU.ap(), dtA.ap(), [maskU.r(), dtA.r()], [ps_c.r()])
            K.mm(ps_c.ap()[:, 16:24], ones1.ap(), dtA.ap(), [ones1.r(), dtA.r()], [ps_c.r()])
            K.tt(Rt.ap(), bc(maskU.ap(), 1, [128, 8, 128]), bc(dtA.ap(), 2, [128, 8, 128]), ALU.mult,
                 [maskU.r(), dtA.r()], [Rt.r()])
            for hf in range(2):
                K.mm(ps_x.ap()[:, hf * 512:(hf + 1) * 512], maskSL.ap(),
                     Rt.ap()[:, hf * 4:(hf + 1) * 4, :].rearrange("p h i -> p (h i)"), [maskSL.r(), Rt.r()],
                     [ps_x.r()])
            K.act(dec.ap().rearrange("p h i -> p (h i)"), ps_x.ap(), AF.Exp, [ps_x.r()], [dec.r()])
            K.cp(csb.ap(), ps_c.ap()[:, 8:24], [ps_c.r()], [csb.r()], eng="act")
            for g in range(2):
                K.mm(ps_c.ap()[:, 256 + g * 128:256 + (g + 1) * 128], xc.ap()[64 * g:64 * g + 64, 4, :],
                     xc.ap()[64 * g:64 * g + 64, 5, :], [xc.r()], [ps_c.r()], self_wait=(g == 1))
            K.tt(Gs.ap(), ps_c.ap()[:, 256:512].rearrange("p (g i) -> p g i", g=2), bc(maskU.ap(), 1, [128, 2, 128]),
                 ALU.mult, [ps_c.r(), maskU.r()], [Gs.r()])
            K.tt(dec.ap().rearrange("p (g r) i -> p g r i", g=2), dec.ap().rearrange("p (g r) i -> p g r i", g=2),
                 bc(Gs.ap(), 2, [128, 2, 4, 128]), ALU.mult, [dec.r(), Gs.r()], [dec.r()])
            K.tt(Mb.ap(), dec.ap(), bc(dtt.ap(), 2, [128, 8, 128]), ALU.mult, [dec.r(), dtt.r()], [Mb.r()])
            for ct in range(5):
                K.tr(ps_t.ap()[:, ct * 128:(ct + 1) * 128], xc.ap()[:, ct, :], identb.ap(), [xc.r(), identb.r()],
                     [ps_t.r()], inc=(ct == 4))
            K.cp(XT.ap(), ps_t.ap()[:, 0:640], [ps_t.r()], [XT.r()], eng="act")
            K.tt(xD.ap().rearrange("p (h q) -> p h q", h=8), XT.ap()[:, 0:512].rearrange("p (h q) -> p h q", h=8),
                 bc(dB.ap(), 2, [128, 8, 64]), ALU.mult, [XT.r(), dB.r()], [xD.r()])
            K.mm(ps_y.ap(), identb.ap(), xD.ap(), [identb.r(), xD.r()], [ps_y.r()], start=True, stop=False)
            for hh in range(8):
                K.mm(ps_y.ap()[:, hh * 64:(hh + 1) * 64], Mb.ap()[:, hh, :], XT.ap()[:, hh * 64:(hh + 1) * 64],
                     [Mb.r(), XT.r()], [ps_y.r()], start=False, stop=(hh == 7))
            for g in range(2):
                K.mm(ps_i.ap()[:, g * 256:(g + 1) * 256], xc.ap()[64 * g:64 * g + 64, 5, :],
                     HSb.ap()[64 * g:64 * g + 64, :, :].rearrange("p h q -> p (h q)"), [xc.r(), HSb.r()], [ps_i.r()],
                     self_wait=(g == 1))
            K.act(e1.ap(), csb.ap()[:, 0:8], AF.Exp, [csb.r()], [e1.r()])
            K.tt(t1.ap().rearrange("p (h q) -> p h q", h=8), ps_i.ap().rearrange("p (h q) -> p h q", h=8),
                 bc(e1.ap(), 2, [128, 8, 64]), ALU.mult, [ps_i.r(), e1.r()], [t1.r()])
            K.tt(t1.ap(), t1.ap(), ps_y.ap(), ALU.add, [t1.r(), ps_y.r()], [t1.r()])
            ssd_epilogue(K, C, t1, sz, ss, yn, 128)
            for q in range(4):
                K.tr(ps_t.ap()[:, q * 128:(q + 1) * 128], yn.ap()[:, q * 128:(q + 1) * 128], identb.ap(),
                     [yn.r(), identb.r()], [ps_t.r()], inc=(q == 3))
            K.tt(yT.ap()[:, 0:4, c * 128:(c + 1) * 128], ps_t.ap()[:, 0:512].rearrange("p (t n) -> p t n", t=4),
                 bc(normg.ap(), 2, [128, 4, 128]), ALU.mult, [ps_t.r(), normg.r()], xr(yT, c // 4, range(4)))
            K.act(el.ap(), csb.ap()[:, 8:16], AF.Exp, [csb.r()], [el.r()])
            K.tt(tail.ap(), csb.ap()[:, 8:16], csb.ap()[:, 0:8], ALU.subtract, [csb.r()], [tail.r()])
            K.act(tail.ap(), tail.ap(), AF.Exp, [tail.r()], [tail.r()])
            K.tt(tail.ap(), tail.ap(), dtt.ap(), ALU.mult, [tail.r(), dtt.r()], [tail.r()])
            K.tt(xw.ap().rearrange("p (h q) -> p h q", h=8), XT.ap()[:, 0:512].rearrange("p (h q) -> p h q", h=8),
                 bc(tail.ap(), 2, [128, 8, 64]), ALU.mult, [XT.r(), tail.r()], [xw.r()])
            K.mm(ps_h.ap(), XT.ap()[:, 512:640], xw.ap(), [XT.r(), xw.r()], [ps_h.r()])
            for g in range(2):
                sl = slice(64 * g, 64 * g + 64)
                K.tt(HS32.ap()[sl], HS32.ap()[sl], bc(el.ap()[sl, 4 * g:4 * g + 4], 2, [64, 4, 64]), ALU.mult,
                     [HS32.r(), el.r()], [HS32.r()])
                K.tt(HS32.ap()[sl], HS32.ap()[sl],
                     ps_h.ap()[sl, 256 * g:256 * g + 256].rearrange("p (h q) -> p h q", h=4), ALU.add,
                     [HS32.r(), ps_h.r()], [HS32.r()])
            K.cp(HSb.ap(), HS32.ap(), [HS32.r()], [HSb.r()], eng="act")

        xb = XB[(NCH - 1) % 2]
        for i in range(3):
            K.dma(dr["p_conv"][l, i].rearrange("(t p) -> p t", p=128), xb.ap()[:, :, 128 + i], r=[xb.r()])
        for q in range(2):
            K.tr(ps_y.ap()[:, q * 128:(q + 1) * 128], HS32.ap().rearrange("p h q -> p (h q)")[:, q * 128:(q + 1) * 128],
                 identf.ap(), [HS32.r(), identf.r()], [ps_y.r()], inc=(q == 1))
        K.cp(hsT.ap(), ps_y.ap()[:, 0:256].rearrange("p (q n) -> p q n", q=2), [ps_y.r()], [hsT.r()])
        for g in range(2):
            for q in range(2):
                K.dma(dr["p_ssd"][l, 4 * g + 2 * q:4 * g + 2 * q + 2].rearrange("h p n -> (h p) n"),
                      hsT.ap()[:, q, 64 * g:64 * g + 64], r=[hsT.r()])
        P.barrier()


def ssd_epilogue(K, C, y, sz, ss, yn, n):
    K.tt(y.ap()[0:n], y.ap()[0:n], sz.ap()[0:n], ALU.mult, [y.r(), sz.r()], [y.r()])
    K.act(yn.ap()[0:n], y.ap()[0:n], AF.Square, [y.r()], [yn.r(), ss.r()], accum_out=ss.ap()[0:n])
    rsqrt_(K, ss.ap()[0:n], [ss.r()], 1.0 / 512, RMS_EPS)
    K.ts(yn.ap()[0:n], y.ap()[0:n], ss.ap()[0:n], ALU.mult, [y.r(), ss.r()], [yn.r()])


def ssd_gla_phase(K, dr, C, l, yT, dbg_out):
    P = K.P
    identb, identf, maskU, maskSL, ones1 = C["identb"], C["identf"], C["maskU"], C["maskSL"], C["ones1"]
    hc, hs = C["hc"], C["hs"]
    G0 = OFF["gq"]
    with contextlib.ExitStack() as ph0:
        win = K.sb(ph0, "win_ssd", [128, KT, 1288], BF16)
        K.dma(win.ap(), dr["w_in"][l, :, 0:1288].rearrange("(k p) n -> p k n", p=128), w=[win.r()], q="pool")
        convw = K.sb(ph0, "convw", [128, 6, 4], F32)
        convb = K.sb(ph0, "convb", [128, 6], F32)
        for i in range(4):
            K.dma(convw.ap()[:, :, i], dr["ssd_conv_w"][l, i].rearrange("(t p) -> p t", p=128), w=[convw.r()])
        K.dma(convb.ap(), dr["ssd_conv_b"][l].rearrange("(t p) -> p t", p=128), w=[convb.r()])
        normg = K.sb(ph0, "normg", [128, 4], F32)
        K.dma(normg.ap(), dr["ssd_norm_g"][l].rearrange("(t p) -> p t", p=128), w=[normg.r()])
        dtbB = K.sb(ph0, "dtbB", [128, 8], F32)
        aB = K.sb(ph0, "aB", [128, 8], F32)
        dB = K.sb(ph0, "dB", [128, 8], F32)
        K.dma(dtbB.ap(), dr["ssd_dt_bias"][l:l + 1, :].to_broadcast([128, 8]), w=[dtbB.r()])
        K.dma(aB.ap(), dr["ssd_a_log"][l:l + 1, :].to_broadcast([128, 8]), w=[aB.r()])
        K.dma(dB.ap(), dr["ssd_d"][l:l + 1, :].to_broadcast([128, 8]), w=[dB.r()])
        K.act(aB.ap(), aB.ap(), AF.Exp, [aB.r()], [aB.r()])
        K.ts(aB.ap(), aB.ap(), -1.0, ALU.mult, [aB.r()], [aB.r()])
        wing = K.sb(ph0, "win_gla", [128, KT, 784], BF16)
        K.dma(wing.ap(), dr["w_in"][l, :, G0:G0 + 784].rearrange("(k p) n -> p k n", p=128), w=[wing.r()], q="pool")
        wgk2 = K.sb(ph0, "wgk2", [16, 128], BF16)
        K.dma(wgk2.ap(), dr["gla_w_gk2"][l], w=[wgk2.r()], q="pool")
        bgkB = K.sb(ph0, "bgkB", [128, 128], F32)
        K.dma(bgkB.ap(), dr["gla_b_gk"][l:l + 1, :].to_broadcast([128, 128]), w=[bgkB.r()])
        gcol = K.sb(ph0, "gcol", [128, 1], F32)
        for t in range(2):
            K.dma(gcol.ap()[64 * t:64 * t + 64, :], dr["gla_norm_g"][l].rearrange("(e o) -> e o", o=1), w=[gcol.r()])
        with contextlib.ExitStack() as ph:
            BM = K.sb(ph, "BM", [128, 256], F32)
            hm = K.sb(ph, "hm", [128, 4], F32)
            K.memset(BM.ap(), 1.0, [BM.r()], eng="pool")
            K.memset(hm.ap(), 1.0, [hm.r()], eng="pool")
            for hh in range(4):
                for (t, sl, n) in ((BM, slice(64 * hh, 64 * hh + 64), 64), (hm, slice(hh, hh + 1), 1)):
                    ap = t.ap()[:, sl]
                    K.P.op("pool", lambda e, ap=ap, n=n, hh=hh: e.affine_select(
                        out=ap, in_=ap, pattern=[[0, n]], compare_op=ALU.is_ge, fill=0.0, base=-32 * hh,
                        channel_multiplier=1), reads=[t.r()], writes=[t.r()])
                    K.P.op("pool", lambda e, ap=ap, n=n, hh=hh: e.affine_select(
                        out=ap, in_=ap, pattern=[[0, n]], compare_op=ALU.is_gt, fill=0.0, base=32 * hh + 32,
                        channel_multiplier=-1), reads=[t.r()], writes=[t.r()])
            PX = [K.ps(ph, f"PX{i}", [128, 512], F32) for i in range(2)]
            PZ = K.ps(ph, "PZ", [128, 512], F32)
            PC = K.ps(ph, "PC", [128, 512], F32)
            PF = K.ps(ph, "PF", [128, 512], F32)
            PV = K.ps(ph, "PV", [128, 512], F32)
            PL = K.ps(ph, "PL", [128, 512], F32)
            PT = K.ps(ph, "PT", [128, 1024], BF16)
            XB = [K.sb(ph, f"XB{i}", [128, 6, 131], BF16) for i in range(2)]
            DW = K.sb(ph, "DW", [128, 6, 4, 128], BF16)
            negb = K.sb(ph, "negb", [128, 6], F32)
            e6 = K.sb(ph, "e6", [128, 6, 128], F32)
            xlast = K.sb(ph, "xlast", [128, 6, 3], F32)
            K.ts(negb.ap(), convb.ap(), -1.0, ALU.mult, [convb.r()], [negb.r()])
            for ct in range(6):
                for i in range(4):
                    K.ts(DW.ap()[:, ct, i, :], identf.ap(), convw.ap()[:, ct, i:i + 1], ALU.mult,
                         [identf.r(), convw.r()], [DW.r()])
            XC = [K.sb(ph, f"XC{i}", [128, 6, 128], BF16) for i in range(2)]
            sz = K.sb(ph, "sz", [128, 512], F32)
            dtt = K.sb(ph, "dtt", [128, 8], F32)
            dtmp = K.sb(ph, "dtmp", [128, 8], F32)
            dtA = K.sb(ph, "dtA", [128, 8], F32)
            csb = K.sb(ph, "csb", [128, 16], F32)
            e1 = K.sb(ph, "e1", [128, 8], F32)
            el = K.sb(ph, "el", [128, 8], F32)
            tail = K.sb(ph, "tail", [128, 8], F32)
            Rt = K.sb(ph, "Rt", [128, 8, 128], F32)
            dec = K.sb(ph, "dec", [128, 8, 128], F32)
            Gs = K.sb(ph, "Gs", [128, 2, 128], F32)
            Mb = K.sb(ph, "Mb", [128, 8, 128], BF16)
            XT = K.sb(ph, "XT", [128, 640], BF16)
            xD = K.sb(ph, "xD", [128, 512], BF16)
            xw = K.sb(ph, "xw", [128, 512], BF16)
            t1 = K.sb(ph, "t1", [128, 512], F32)
            yn = K.sb(ph, "yn", [128, 512], BF16)
            ss = K.sb(ph, "ss", [128, 1], F32)
            HS32 = K.sb(ph, "HS32", [128, 4, 64], F32)
            HSb = K.sb(ph, "HSb", [128, 4, 64], BF16)
            glo = K.sb(ph, "glo", [16, 128], BF16)
            lg = K.sb(ph, "lg", [128, 128], F32)
            lgt = K.sb(ph, "lgt", [128, 128], F32)
            Eq = K.sb(ph, "Eq", [128, 128], F32)
            Ek = K.sb(ph, "Ek", [128, 128], F32)
            Ekt = K.sb(ph, "Ekt", [128, 128], F32)
            qt = K.sb(ph, "qt", [128, 128], BF16)
            kf = K.sb(ph, "kf", [128, 128], F32)
            km = K.sb(ph, "km", [128, 4, 128], BF16)
            ktm = K.sb(ph, "ktm", [128, 128], BF16)
            vtm = K.sb(ph, "vtm", [128, 256], BF16)
            sgg = K.sb(ph, "sgg", [128, 256], F32)
            A = K.sb(ph, "A", [128, 4, 128], BF16)
            osq = K.sb(ph, "osq", [128, 256], F32)
            ms = K.sb(ph, "ms", [128, 4], F32)
            on = K.sb(ph, "on", [128, 256], F32)
            onb = K.sb(ph, "onb", [128, 256], BF16)
            tmpS = K.sb(ph, "tmpS", [128, 256], F32)
            S32 = K.sb(ph, "S32", [128, 256], F32)
            Sb = K.sb(ph, "Sb", [128, 256], BF16)

            K.memset(HS32.ap(), 0.0, [HS32.r()])
            K.memset(HSb.ap(), 0.0, [HSb.r()])
            K.memset(XB[0].ap()[:, :, 0:3], 0.0, [XB[0].r()])
            K.memset(S32.ap(), 0.0, [S32.r()])
            K.memset(Sb.ap(), 0.0, [Sb.r()])

            def ssd_body(c):
                h = hc[c % 2]
                xb, xc = XB[c % 2], XC[c % 2]
                def chain_a():
                    for ct in range(6):
                        px = PX[0] if ct < 4 else PX[1]
                        cc = ct if ct < 4 else ct - 4
                        for k in range(KT):
                            K.mm(px.ap()[:, cc * 128:(cc + 1) * 128], win.ap()[:, k, 512 + ct * 128:512 + (ct + 1) * 128],
                                 h.ap()[:, k, :], [win.r(), h.r()], [px.r()], start=(k == 0), stop=(k == KT - 1))
                        yield
                    K.cp(xb.ap()[:, 0:4, 3:131], PX[0].ap().rearrange("p (t n) -> p t n", t=4), [PX[0].r()], [xb.r()],
                         eng="act")
                    yield
                    K.cp(xb.ap()[:, 4:6, 3:131], PX[1].ap()[:, 0:256].rearrange("p (t n) -> p t n", t=2), [PX[1].r()],
                         [xb.r()], eng="act")
                    yield
                    if c + 1 < NCH:
                        K.cp(XB[(c + 1) % 2].ap()[:, :, 0:3], xb.ap()[:, :, 128:131], [xb.r()], [XB[(c + 1) % 2].r()])
                    if c == NCH - 1:
                        K.cp(xlast.ap()[:, 0:4, :], PX[0].ap().rearrange("p (t n) -> p t n", t=4)[:, :, 125:128],
                             [PX[0].r()], [xlast.r()])
                        K.cp(xlast.ap()[:, 4:6, :], PX[1].ap()[:, 0:256].rearrange("p (t n) -> p t n", t=2)[:, :, 125:128],
                             [PX[1].r()], [xlast.r()])
                    for ct in range(6):
                        px = PX[0] if ct < 4 else PX[1]
                        cc = ct if ct < 4 else ct - 4
                        for i in range(4):
                            K.mm(px.ap()[:, cc * 128:(cc + 1) * 128], DW.ap()[:, ct, i, :], xb.ap()[:, ct, i:i + 128],
                                 [DW.r(), xb.r()], [px.r()], start=(i == 0), stop=(i == 3))
                        yield
                    for ct in range(6):
                        px = PX[0] if ct < 4 else PX[1]
                        cc = ct if ct < 4 else ct - 4
                        K.act(e6.ap()[:, ct, :], px.ap()[:, cc * 128:(cc + 1) * 128], AF.Exp, [px.r(), negb.r()], [e6.r()],
                              scale=-1.0, bias=negb.ap()[:, ct:ct + 1])
                        yield
                    sigmoid_chain(K, e6.ap(), [e6.r()])
                    yield
                    for ct in range(6):
                        px = PX[0] if ct < 4 else PX[1]
                        cc = ct if ct < 4 else ct - 4
                        K.stt(xc.ap()[:, ct, :], px.ap()[:, cc * 128:(cc + 1) * 128], convb.ap()[:, ct:ct + 1], e6.ap()[:, ct, :],
                              ALU.add, ALU.mult, [px.r(), convb.r(), e6.r()], [xc.r()])
                        yield
                    for g in range(2):
                        K.mm(PC.ap()[:, 256 + g * 128:256 + (g + 1) * 128], xc.ap()[64 * g:64 * g + 64, 4, :],
                             xc.ap()[64 * g:64 * g + 64, 5, :], [xc.r()], [PC.r()], self_wait=(g == 1))
                    yield
                    K.tt(Gs.ap(), PC.ap()[:, 256:512].rearrange("p (g i) -> p g i", g=2), bc(maskU.ap(), 1, [128, 2, 128]),
                         ALU.mult, [PC.r(), maskU.r()], [Gs.r()])
                    yield
                    for ct in range(5):
                        K.tr(PT.ap()[:, ct * 128:(ct + 1) * 128], xc.ap()[:, ct, :], identb.ap(), [xc.r(), identb.r()],
                             [PT.r()], inc=(ct == 4))
                    yield
                    K.cp(XT.ap(), PT.ap()[:, 0:640], [PT.r()], [XT.r()], eng="act")
                    yield

                def chain_b():
                    for k in range(KT):
                        K.mm(PZ.ap(), h.ap()[:, k, :], win.ap()[:, k, 0:512], [h.r(), win.r()], [PZ.r()],
                             start=(k == 0), stop=(k == KT - 1))
                    for k in range(KT):
                        K.mm(PC.ap()[:, 0:8], h.ap()[:, k, :], win.ap()[:, k, 1280:1288], [h.r(), win.r()], [PC.r()],
                             start=(k == 0), stop=(k == KT - 1))
                    yield
                    K.act(sz.ap(), PZ.ap(), AF.Exp, [PZ.r()], [sz.r()], scale=-1.0)
                    yield
                    sigmoid_chain(K, sz.ap(), [sz.r()])
                    yield
                    K.tt(sz.ap(), sz.ap(), PZ.ap(), ALU.mult, [sz.r(), PZ.r()], [sz.r()])
                    yield
                    K.tt(dtt.ap(), PC.ap()[:, 0:8], dtbB.ap(), ALU.add, [PC.r(), dtbB.r()], [dtt.r()])
                    yield
                    softplus_(K, (dtt.ap(), dtt.r()), (dtmp.ap(), dtmp.r()), None, None)
                    yield
                    K.tt(dtA.ap(), dtt.ap(), aB.ap(), ALU.mult, [dtt.r(), aB.r()], [dtA.r()])
                    yield
                    K.mm(PC.ap()[:, 8:16], maskU.ap(), dtA.ap(), [maskU.r(), dtA.r()], [PC.r()])
                    K.mm(PC.ap()[:, 16:24], ones1.ap(), dtA.ap(), [ones1.r(), dtA.r()], [PC.r()])
                    yield
                    K.tt(Rt.ap(), bc(maskU.ap(), 1, [128, 8, 128]), bc(dtA.ap(), 2, [128, 8, 128]), ALU.mult,
                         [maskU.r(), dtA.r()], [Rt.r()])
                    yield
                    for hf in range(2):
                        K.mm(PZ.ap(), maskSL.ap(), Rt.ap()[:, hf * 4:(hf + 1) * 4, :].rearrange("p h i -> p (h i)"),
                             [maskSL.r(), Rt.r()], [PZ.r()])
                        yield
                        K.act(dec.ap()[:, hf * 4:(hf + 1) * 4, :].rearrange("p h i -> p (h i)"), PZ.ap(), AF.Exp,
                              [PZ.r()], [dec.r()])
                        yield
                    K.cp(csb.ap(), PC.ap()[:, 8:24], [PC.r()], [csb.r()], eng="act")
                    yield

                yield from interleave_gen([chain_a(), chain_b()], [2, 1])
                K.tt(dec.ap().rearrange("p (g r) i -> p g r i", g=2), dec.ap().rearrange("p (g r) i -> p g r i", g=2),
                     bc(Gs.ap(), 2, [128, 2, 4, 128]), ALU.mult, [dec.r(), Gs.r()], [dec.r()])
                yield
                K.tt(Mb.ap(), dec.ap(), bc(dtt.ap(), 2, [128, 8, 128]), ALU.mult, [dec.r(), dtt.r()], [Mb.r()])
                yield
                K.tt(xD.ap().rearrange("p (h q) -> p h q", h=8), XT.ap()[:, 0:512].rearrange("p (h q) -> p h q", h=8),
                     bc(dB.ap(), 2, [128, 8, 64]), ALU.mult, [XT.r(), dB.r()], [xD.r()])
                yield
                K.mm(PZ.ap(), identb.ap(), xD.ap(), [identb.r(), xD.r()], [PZ.r()], start=True, stop=False)
                for hh in range(8):
                    K.mm(PZ.ap()[:, hh * 64:(hh + 1) * 64], Mb.ap()[:, hh, :], XT.ap()[:, hh * 64:(hh + 1) * 64],
                         [Mb.r(), XT.r()], [PZ.r()], start=False, stop=(hh == 7))
                for g in range(2):
                    K.mm(PC.ap()[:, g * 256:(g + 1) * 256], xc.ap()[64 * g:64 * g + 64, 5, :],
                         HSb.ap()[64 * g:64 * g + 64, :, :].rearrange("p h q -> p (h q)"), [xc.r(), HSb.r()], [PC.r()],
                         self_wait=(g == 1))
                yield
                K.act(e1.ap(), csb.ap()[:, 0:8], AF.Exp, [csb.r()], [e1.r()])
                yield
                K.tt(t1.ap().rearrange("p (h q) -> p h q", h=8), PC.ap().rearrange("p (h q) -> p h q", h=8),
                     bc(e1.ap(), 2, [128, 8, 64]), ALU.mult, [PC.r(), e1.r()], [t1.r()])
                yield
                K.tt(t1.ap(), t1.ap(), PZ.ap(), ALU.add, [t1.r(), PZ.r()], [t1.r()])
                yield
                ssd_epilogue(K, C, t1, sz, ss, yn, 128)
                yield
                for q in range(4):
                    K.tr(PT.ap()[:, q * 128:(q + 1) * 128], yn.ap()[:, q * 128:(q + 1) * 128], identb.ap(),
                         [yn.r(), identb.r()], [PT.r()], inc=(q == 3))
                yield
                K.tt(yT.ap()[:, 0:4, c * 128:(c + 1) * 128], PT.ap()[:, 0:512].rearrange("p (t n) -> p t n", t=4),
                     bc(normg.ap(), 2, [128, 4, 128]), ALU.mult, [PT.r(), normg.r()], xr(yT, c // 4, range(4)))
                yield
                K.act(el.ap(), csb.ap()[:, 8:16], AF.Exp, [csb.r()], [el.r()])
                yield
                K.tt(tail.ap(), csb.ap()[:, 8:16], csb.ap()[:, 0:8], ALU.subtract, [csb.r()], [tail.r()])
                yield
                K.act(tail.ap(), tail.ap(), AF.Exp, [tail.r()], [tail.r()])
                yield
                K.tt(tail.ap(), tail.ap(), dtt.ap(), ALU.mult, [tail.r(), dtt.r()], [tail.r()])
                yield
                K.tt(xw.ap().rearrange("p (h q) -> p h q", h=8), XT.ap()[:, 0:512].rearrange("p (h q) -> p h q", h=8),
                     bc(tail.ap(), 2, [128, 8, 64]), ALU.mult, [XT.r(), tail.r()], [xw.r()])
                yield
                K.mm(PC.ap(), XT.ap()[:, 512:640], xw.ap(), [XT.r(), xw.r()], [PC.r()])
                yield
                for g in range(2):
                    sl = slice(64 * g, 64 * g + 64)
                    K.tt(HS32.ap()[sl], HS32.ap()[sl], bc(el.ap()[sl, 4 * g:4 * g + 4], 2, [64, 4, 64]), ALU.mult,
                         [HS32.r(), el.r()], [HS32.r()])
                    K.tt(HS32.ap()[sl], HS32.ap()[sl],
                         PC.ap()[sl, 256 * g:256 * g + 256].rearrange("p (h q) -> p h q", h=4), ALU.add,
                         [HS32.r(), PC.r()], [HS32.r()])
                    yield
                K.cp(HSb.ap(), HS32.ap(), [HS32.r()], [HSb.r()], eng="act")
                yield

            def gla_body(c):
                h = hc[c % 2]
                for (dst, cols) in ((PF.ap()[:, 0:128], slice(0, 128)), (PF.ap()[:, 128:256], slice(128, 256)),
                                    (PF.ap()[0:16, 256:384], slice(512, 528))):
                    for k in range(KT):
                        K.mm(dst, wing.ap()[:, k, cols], h.ap()[:, k, :], [wing.r(), h.r()], [PF.r()],
                             start=(k == 0), stop=(k == KT - 1))
                    yield
                for (dst, cols, pst) in ((PV.ap()[:, 0:256], slice(256, 512), PV), (PF.ap()[:, 384:512], slice(128, 256), PF),
                                         (PV.ap()[:, 256:512], slice(528, 784), PV)):
                    for k in range(KT):
                        K.mm(dst, h.ap()[:, k, :], wing.ap()[:, k, cols], [wing.r(), h.r()], [pst.r()],
                             start=(k == 0), stop=(k == KT - 1))
                    yield
                K.cp(glo.ap(), PF.ap()[0:16, 256:384], [PF.r()], [glo.r()], eng="act")
                yield
                K.mm(PL.ap()[:, 0:128], glo.ap(), wgk2.ap(), [glo.r(), wgk2.r()], [PL.r()])
                yield
                K.stt(lg.ap(), PL.ap()[:, 0:128], -1.0, bgkB.ap(), ALU.mult, ALU.subtract, [PL.r(), bgkB.r()], [lg.r()])
                yield
                softplus_(K, (lg.ap(), lg.r()), (lgt.ap(), lgt.r()), None, None)
                yield
                K.ts(lg.ap(), lg.ap(), -1.0 / 16.0, ALU.mult, [lg.r()], [lg.r()])
                yield
                K.mm(PL.ap()[:, 128:256], lg.ap(), maskU.ap(), [lg.r(), maskU.r()], [PL.r()])
                K.mm(PL.ap()[:, 256:384], maskU.ap(), lg.ap(), [lg.r(), maskU.r()], [PL.r()])
                yield
                K.act(Eq.ap(), PL.ap()[:, 128:256], AF.Exp, [PL.r()], [Eq.r()])
                yield
                K.act(Ek.ap(), PL.ap()[:, 128:256], AF.Exp, [PL.r()], [Ek.r()], scale=-1.0)
                yield
                K.act(Ekt.ap(), PL.ap()[:, 256:384], AF.Exp, [PL.r()], [Ekt.r()], scale=-1.0)
                yield
                K.stt(qt.ap(), PF.ap()[:, 0:128], 32.0 ** -0.5, Eq.ap(), ALU.mult, ALU.mult, [PF.r(), Eq.r()], [qt.r()])
                yield
                K.tt(kf.ap(), PF.ap()[:, 128:256], Ek.ap(), ALU.mult, [PF.r(), Ek.r()], [kf.r()])
                yield
                K.tt(km.ap(), bc(kf.ap(), 1, [128, 4, 128]), bc(hm.ap(), 2, [128, 4, 128]), ALU.mult, [kf.r(), hm.r()],
                     [km.r()])
                yield
                K.tt(ktm.ap(), PF.ap()[:, 384:512], Ekt.ap(), ALU.mult, [PF.r(), Ekt.r()], [ktm.r()])
                yield
                K.cp(vtm.ap(), PV.ap()[:, 0:256], [PV.r()], [vtm.r()], eng="act")
                yield
                K.act(sgg.ap(), PV.ap()[:, 256:512], AF.Exp, [PV.r()], [sgg.r()], scale=-1.0)
                yield
                sigmoid_chain(K, sgg.ap(), [sgg.r()])
                yield
                K.tt(sgg.ap(), sgg.ap(), PV.ap()[:, 256:512], ALU.mult, [sgg.r(), PV.r()], [sgg.r()])
                yield
                for hh in range(4):
                    K.mm(PL.ap()[:, hh * 128:(hh + 1) * 128], km.ap()[:, hh, :], qt.ap(), [km.r(), qt.r()], [PL.r()],
                         inc=(hh == 3))
                yield
                K.tt(A.ap(), PL.ap().rearrange("p (h i) -> p h i", h=4), bc(maskU.ap(), 1, [128, 4, 128]), ALU.mult,
                     [PL.r(), maskU.r()], [A.r()])
                yield
                K.mm(PL.ap()[:, 0:256], qt.ap(), Sb.ap(), [qt.r(), Sb.r()], [PL.r()], start=True, stop=False)
                for hh in range(4):
                    K.mm(PL.ap()[:, hh * 64:(hh + 1) * 64], A.ap()[:, hh, :], vtm.ap()[:, hh * 64:(hh + 1) * 64],
                         [A.r(), vtm.r()], [PL.r()], start=False, stop=(hh == 3))
                yield
                K.act(osq.ap(), PL.ap()[:, 0:256], AF.Square, [PL.r()], [osq.r()])
                yield
                K.red(ms.ap(), osq.ap().rearrange("p (h e) -> p h e", h=4), [osq.r()], [ms.r()])
                yield
                rsqrt_(K, ms.ap(), [ms.r()], 1.0 / 64, RMS_EPS)
                yield
                K.tt(on.ap().rearrange("p (h e) -> p h e", h=4), PL.ap()[:, 0:256].rearrange("p (h e) -> p h e", h=4),
                     bc(ms.ap(), 2, [128, 4, 64]), ALU.mult, [PL.r(), ms.r()], [on.r()])
                yield
                K.tt(onb.ap(), on.ap(), sgg.ap(), ALU.mult, [on.r(), sgg.r()], [onb.r()])
                yield
                for q in range(2):
                    K.tr(PT.ap()[:, 768 + q * 128:768 + (q + 1) * 128], onb.ap()[:, q * 128:(q + 1) * 128], identb.ap(),
                         [onb.r(), identb.r()], [PT.r()], inc=(q == 1))
                yield
                K.ts(yT.ap()[:, 6:8, c * 128:(c + 1) * 128], PT.ap()[:, 768:1024].rearrange("p (t n) -> p t n", t=2),
                     gcol.ap(), ALU.mult, [PT.r(), gcol.r()], xr(yT, c // 4, range(6, 8)))
                yield
                K.mm(PL.ap()[:, 256:512], ktm.ap(), vtm.ap(), [ktm.r(), vtm.r()], [PL.r()])
                yield
                K.tt(tmpS.ap(), PL.ap()[:, 256:512], BM.ap(), ALU.mult, [PL.r(), BM.r()], [tmpS.r()])
                yield
                K.tt(S32.ap(), S32.ap(), tmpS.ap(), ALU.add, [S32.r(), tmpS.r()], [S32.r()])
                yield
                K.ts(S32.ap(), S32.ap(), Eq.ap()[:, 127:128], ALU.mult, [S32.r(), Eq.r()], [S32.r()])
                yield
                K.cp(Sb.ap(), S32.ap(), [S32.r()], [Sb.r()], eng="act")
                yield

            for c in range(NCH):
                make_hc(K, C, l, hc[c % 2], c)
                interleave([ssd_body(c), gla_body(c)], ratio=[2, 1])

            xb = XB[(NCH - 1) % 2]
            for i in range(3):
                K.dma(dr["p_conv"][l, i].rearrange("(t p) -> p t", p=128), xlast.ap()[:, :, i], r=[xlast.r()])
            hsT = t1
            for q in range(2):
                K.tr(PZ.ap()[:, q * 128:(q + 1) * 128], HS32.ap().rearrange("p h q -> p (h q)")[:, q * 128:(q + 1) * 128],
                     identf.ap(), [HS32.r(), identf.r()], [PZ.r()], inc=(q == 1))
            K.cp(hsT.ap()[:, 0:256], PZ.ap()[:, 0:256], [PZ.r()], [hsT.r()])
            for g in range(2):
                for q in range(2):
                    K.dma(dr["p_ssd"][l, 4 * g + 2 * q:4 * g + 2 * q + 2].rearrange("h p n -> (h p) n"),
                          hsT.ap()[:, q * 128 + 64 * g:q * 128 + 64 * g + 64], r=[hsT.r()])
            for hh in range(4):
                K.dma(dr["p_gla"][l, hh], S32.ap()[32 * hh:32 * hh + 32, 64 * hh:64 * hh + 64], r=[S32.r()])
            P.barrier()
        ssd_sample(K, dr, C, l, yT, win, aB, dB, dtbB, dbg_out)
        gla_sample(K, dr, C, l, yT, wing, wgk2, bgkB)


def dram_scratch(K, name, shape):
    K.uid += 1
    h = K.nc.dram_tensor(f"scr_{name}_{K.uid}", list(shape), F32)
    return Tn(h, name)


def ssd_sample(K, dr, C, l, yT, win, aB, dB, dtbB, dbg_out):
    P = K.P
    hs, identb = C["hs"], C["identb"]
    with contextlib.ExitStack() as ph:
        cs = K.sb(ph, "cs", [NS, 768], F32)
        wB = K.sb(ph, "wB", [NS, 768], F32)
        gB = K.sb(ph, "gB", [NS, 512], F32)
        xbcs = K.sb(ph, "xbcs", [NS, 768], F32)
        acc = K.sb(ph, "acc", [NS, 768], F32)
        tmpc = K.sb(ph, "tmpc", [NS, 768], F32)
        szs = K.sb(ph, "szs", [NS, 512], F32)
        dts = K.sb(ph, "dts", [NS, 8], F32)
        dtm = K.sb(ph, "dtm", [NS, 8], F32)
        rep = K.sb(ph, "rep", [NS, 2, 8, 64], F32)
        pk = K.sb(ph, "pk", [NS, 8, 3], F32)
        Hs = K.sb(ph, "Hs", [128, 64, 64], F32)
        tmpH = K.sb(ph, "tmpH", [128, 32, 64], F32)
        xh = K.sb(ph, "xh", [128, 64], F32)
        BCh = K.sb(ph, "BCh", [128, 2, 64], F32)
        pkh = K.sb(ph, "pkh", [128, 3], F32)
        dA = K.sb(ph, "dA", [128, 1], F32)
        xdt = K.sb(ph, "xdt", [128, 64], F32)
        yh = K.sb(ph, "yh", [128, 64], F32)
        ysm = K.sb(ph, "ysm", [NS, 512], F32)
        yns = K.sb(ph, "yns", [NS, 512], BF16)
        sss = K.sb(ph, "sss", [NS, 1], F32)
        ps_a = K.ps(ph, "pss_a", [128, 512], F32)
        ps_b = K.ps(ph, "pss_b", [128, 512], F32)
        ps_d = K.ps(ph, "pss_d", [128, 512], F32)
        ps_t = K.ps(ph, "pss_t", [128, 1024], BF16)
        sx = dram_scratch(K, "sx", [NS, 512])
        sbc = dram_scratch(K, "sbc", [2, NS, 512])
        spk = dram_scratch(K, "spk", [NS, 24])
        sy = dram_scratch(K, "sy", [NS, 512])

        K.dma(gB.ap(), dr["ssd_norm_g"][l:l + 1, :].to_broadcast([NS, 512]), w=[gB.r()])
        K.dma(Hs.ap().rearrange("p a b -> p (a b)"), dr["st_ssd"][l].rearrange("b h p n -> (b h) (p n)"), w=[Hs.r()])
        for k in range(KT):
            K.mm(ps_a.ap()[0:NS, :], hs.ap()[:, k, :], win.ap()[:, k, 0:512], [hs.r(), win.r()], [ps_a.r()],
                 start=(k == 0), stop=(k == KT - 1))
        for k in range(KT):
            K.mm(ps_b.ap()[0:NS, :], hs.ap()[:, k, :], win.ap()[:, k, 512:1024], [hs.r(), win.r()], [ps_b.r()],
                 start=(k == 0), stop=(k == KT - 1))
        for k in range(KT):
            K.mm(ps_d.ap()[0:NS, 0:264], hs.ap()[:, k, :], win.ap()[:, k, 1024:1288], [hs.r(), win.r()], [ps_d.r()],
                 start=(k == 0), stop=(k == KT - 1))
        K.act(szs.ap(), ps_a.ap()[0:NS, :], AF.Silu, [ps_a.r()], [szs.r()])
        K.cp(xbcs.ap()[:, 0:512], ps_b.ap()[0:NS, :], [ps_b.r()], [xbcs.r()], eng="act")
        K.cp(xbcs.ap()[:, 512:768], ps_d.ap()[0:NS, 0:256], [ps_d.r()], [xbcs.r()], eng="act")
        K.tt(dts.ap(), ps_d.ap()[0:NS, 256:264], dtbB.ap()[0:NS, :], ALU.add, [ps_d.r(), dtbB.r()], [dts.r()])
        softplus_(K, (dts.ap(), dts.r()), (dtm.ap(), dtm.r()), None, None)
        K.dma(wB.ap(), dr["ssd_conv_w"][l, 3:4, :].to_broadcast([NS, 768]), w=[wB.r()])
        K.tt(acc.ap(), xbcs.ap(), wB.ap(), ALU.mult, [xbcs.r(), wB.r()], [acc.r()])
        for i in range(3):
            K.dma(wB.ap(), dr["ssd_conv_w"][l, i:i + 1, :].to_broadcast([NS, 768]), w=[wB.r()])
            K.dma(cs.ap(), dr["st_conv"][l][:, i, :], w=[cs.r()])
            K.tt(tmpc.ap(), cs.ap(), wB.ap(), ALU.mult, [cs.r(), wB.r()], [tmpc.r()])
            K.tt(acc.ap(), acc.ap(), tmpc.ap(), ALU.add, [acc.r(), tmpc.r()], [acc.r()])
        K.dma(wB.ap(), dr["ssd_conv_b"][l:l + 1, :].to_broadcast([NS, 768]), w=[wB.r()])
        K.tt(acc.ap(), acc.ap(), wB.ap(), ALU.add, [acc.r(), wB.r()], [acc.r()])
        K.act(acc.ap(), acc.ap(), AF.Silu, [acc.r()], [acc.r()])
        K.dma(dr["s_conv"][l][:, 0:2, :], dr["st_conv"][l][:, 1:3, :])
        K.dma(dr["s_conv"][l][:, 2, :], xbcs.ap(), r=[xbcs.r()])
        K.dma(sx.ap(), acc.ap()[:, 0:512], r=[acc.r()], w=[sx.r()])
        K.cp(rep.ap().rearrange("p t (g r) n -> p t g r n", g=2),
             bc(acc.ap()[:, 512:768].rearrange("p (t g n) -> p t g n", t=2, g=2), 3, [NS, 2, 2, 4, 64]),
             [acc.r()], [rep.r()])
        K.cp(pk.ap()[:, :, 0], dts.ap(), [dts.r()], [pk.r()])
        K.cp(pk.ap()[:, :, 1], aB.ap()[0:NS, :], [aB.r()], [pk.r()])
        K.cp(pk.ap()[:, :, 2], dB.ap()[0:NS, :], [dB.r()], [pk.r()])
        K.dma(sbc.ap().rearrange("t b x -> b t x"), rep.ap().rearrange("p t h n -> p t (h n)"), r=[rep.r()],
              w=[sbc.r()])
        K.dma(spk.ap(), pk.ap().rearrange("p h q -> p (h q)"), r=[pk.r()], w=[spk.r()])
        K.dma(xh.ap(), sx.ap().rearrange("b (h p) -> (b h) p", h=8), r=[sx.r()], w=[xh.r()])
        for t in range(2):
            K.dma(BCh.ap()[:, t, :], sbc.ap()[t].rearrange("b (h n) -> (b h) n", h=8), r=[sbc.r()],
                  w=[BCh.r()])
        K.dma(pkh.ap(), spk.ap().rearrange("b (h q) -> (b h) q", h=8), r=[spk.r()], w=[pkh.r()])
        K.act(dA.ap(), pkh.ap()[:, 0:1], AF.Exp, [pkh.r()], [dA.r()], scale=pkh.ap()[:, 1:2])
        K.ts(xdt.ap(), xh.ap(), pkh.ap()[:, 0:1], ALU.mult, [xh.r(), pkh.r()], [xdt.r()])
        K.ts(Hs.ap(), Hs.ap(), dA.ap(), ALU.mult, [Hs.r(), dA.r()], [Hs.r()])
        for hf in range(2):
            sl = slice(32 * hf, 32 * hf + 32)
            K.tt(tmpH.ap(), bc(xdt.ap()[:, sl], 2, [128, 32, 64]), bc(BCh.ap()[:, 0, :], 1, [128, 32, 64]), ALU.mult,
                 [xdt.r(), BCh.r()], [tmpH.r()])
            K.tt(Hs.ap()[:, sl, :], Hs.ap()[:, sl, :], tmpH.ap(), ALU.add, [Hs.r(), tmpH.r()], [Hs.r()])
        K.dma(dr["s_ssd"][l].rearrange("b h p n -> (b h) (p n)"), Hs.ap().rearrange("p a b -> p (a b)"), r=[Hs.r()])
        for hf in range(2):
            sl = slice(32 * hf, 32 * hf + 32)
            K.tt(tmpH.ap(), Hs.ap()[:, sl, :], bc(BCh.ap()[:, 1, :], 1, [128, 32, 64]), ALU.mult, [Hs.r(), BCh.r()],
                 [tmpH.r()])
            K.red(yh.ap()[:, sl], tmpH.ap(), [tmpH.r()], [yh.r()])
        K.stt(yh.ap(), xh.ap(), pkh.ap()[:, 2:3], yh.ap(), ALU.mult, ALU.add, [xh.r(), pkh.r(), yh.r()], [yh.r()])
        K.dma(sy.ap().rearrange("b (h p) -> (b h) p", h=8), yh.ap(), r=[yh.r()], w=[sy.r()])
        K.dma(ysm.ap(), sy.ap(), r=[sy.r()], w=[ysm.r()])
        ssd_epilogue(K, C, ysm, szs, sss, yns, NS)
        K.tt(ysm.ap(), ysm.ap(), gB.ap(), ALU.mult, [ysm.r(), gB.r()], [ysm.r()])
        K.ts(yns.ap(), ysm.ap(), sss.ap(), ALU.mult, [ysm.r(), sss.r()], [yns.r()])
        for q in range(4):
            K.tr(ps_t.ap()[:, q * NS:(q + 1) * NS], yns.ap()[:, q * 128:(q + 1) * 128], identb.ap()[0:NS, 0:NS],
                 [yns.r(), identb.r()], [ps_t.r()], inc=(q == 3))
        K.cp(yT.ap()[:, 0:4, T:T + NS], ps_t.ap()[:, 0:4 * NS].rearrange("p (t n) -> p t n", t=4), [ps_t.r()],
             xr(yT, 4, range(4)))
        P.barrier()


def gla_phase(K, dr, C, l, yT, dbg_out):
    P = K.P
    identb, maskU = C["identb"], C["maskU"]
    hc, hs = C["hc"], C["hs"]
    G0 = OFF["gq"]
    with contextlib.ExitStack() as ph:
        win = K.sb(ph, "win_gla", [128, KT, 784], BF16)
        K.dma(win.ap(), dr["w_in"][l, :, G0:G0 + 784].rearrange("(k p) n -> p k n", p=128), w=[win.r()], q="pool")
        wgk2 = K.sb(ph, "wgk2", [16, 128], BF16)
        K.dma(wgk2.ap(), dr["gla_w_gk2"][l], w=[wgk2.r()], q="pool")
        bgkB = K.sb(ph, "bgkB", [128, 128], F32)
        K.dma(bgkB.ap(), dr["gla_b_gk"][l:l + 1, :].to_broadcast([128, 128]), w=[bgkB.r()])
        gcol = K.sb(ph, "gcol", [128, 1], F32)
        for t in range(2):
            K.dma(gcol.ap()[64 * t:64 * t + 64, :], dr["gla_norm_g"][l].rearrange("(e o) -> e o", o=1), w=[gcol.r()])
        BM = K.sb(ph, "BM", [128, 256], F32)
        hm = K.sb(ph, "hm", [128, 4], F32)
        K.memset(BM.ap(), 1.0, [BM.r()], eng="pool")
        K.memset(hm.ap(), 1.0, [hm.r()], eng="pool")
        for hh in range(4):
            for (t, sl, n) in ((BM, slice(64 * hh, 64 * hh + 64), 64), (hm, slice(hh, hh + 1), 1)):
                ap = t.ap()[:, sl]
                K.P.op("pool", lambda e, ap=ap, n=n, hh=hh: e.affine_select(
                    out=ap, in_=ap, pattern=[[0, n]], compare_op=ALU.is_ge, fill=0.0, base=-32 * hh,
                    channel_multiplier=1), reads=[t.r()], writes=[t.r()])
                K.P.op("pool", lambda e, ap=ap, n=n, hh=hh: e.affine_select(
                    out=ap, in_=ap, pattern=[[0, n]], compare_op=ALU.is_gt, fill=0.0, base=32 * hh + 32,
                    channel_multiplier=-1), reads=[t.r()], writes=[t.r()])
        gla_prompt(K, dr, C, l, yT, win, wgk2, bgkB, gcol, BM, hm)
        P.barrier()
        gla_sample(K, dr, C, l, yT, win, wgk2, bgkB)


def gla_prompt(K, dr, C, l, yT, win, wgk2, bgkB, gcol, BM, hm):
    P = K.P
    identb, maskU = C["identb"], C["maskU"]
    hc = C["hc"]
    with contextlib.ExitStack() as ph:
        glo = K.sb(ph, "glo", [16, 128], BF16)
        lg = K.sb(ph, "lg", [128, 128], F32)
        lgt = K.sb(ph, "lgt", [128, 128], F32)
        Eq = K.sb(ph, "Eq", [128, 128], F32)
        Ek = K.sb(ph, "Ek", [128, 128], F32)
        Ekt = K.sb(ph, "Ekt", [128, 128], F32)
        qt = K.sb(ph, "qt", [128, 128], BF16)
        kf = K.sb(ph, "kf", [128, 128], F32)
        km = K.sb(ph, "km", [128, 4, 128], BF16)
        ktm = K.sb(ph, "ktm", [128, 128], BF16)
        vtm = K.sb(ph, "vtm", [128, 256], BF16)
        sgg = K.sb(ph, "sgg", [128, 256], F32)
        A = K.sb(ph, "A", [128, 4, 128], BF16)
        osq = K.sb(ph, "osq", [128, 256], F32)
        ms = K.sb(ph, "ms", [128, 4], F32)
        on = K.sb(ph, "on", [128, 256], F32)
        onb = K.sb(ph, "onb", [128, 256], BF16)
        tmpS = K.sb(ph, "tmpS", [128, 256], F32)
        S32 = K.sb(ph, "S32", [128, 256], F32)
        Sb = K.sb(ph, "Sb", [128, 256], BF16)
        ps_f = K.ps(ph, "psg_f", [128, 512], F32)
        ps_m = K.ps(ph, "psg_m", [128, 512], F32)
        ps_g = K.ps(ph, "psg_g", [128, 512], F32)
        ps_l = K.ps(ph, "psg_l", [128, 512], F32)
        ps_a = K.ps(ph, "psg_a", [128, 512], F32)
        ps_o = K.ps(ph, "psg_o", [128, 512], F32)
        ps_t = K.ps(ph, "psg_t", [128, 1024], BF16)
        K.memset(S32.ap(), 0.0, [S32.r()])
        K.memset(Sb.ap(), 0.0, [Sb.r()])
        for c in range(NCH):
            h = hc[c % 2]
            make_hc(K, C, l, h, c)
            for (dst, cols, M) in ((ps_f.ap()[:, 0:128], slice(0, 128), 128), (ps_f.ap()[:, 128:256], slice(128, 256), 128),
                                   (ps_f.ap()[0:16, 256:384], slice(512, 528), 16)):
                for k in range(KT):
                    K.mm(dst, win.ap()[:, k, cols], h.ap()[:, k, :], [win.r(), h.r()], [ps_f.r()],
                         start=(k == 0), stop=(k == KT - 1))
            for (dst, cols, pst) in ((ps_m.ap()[:, 0:256], slice(256, 512), ps_m), (ps_m.ap()[:, 256:384], slice(128, 256), ps_m),
                                     (ps_g.ap()[:, 0:256], slice(528, 784), ps_g)):
                for k in range(KT):
                    K.mm(dst, h.ap()[:, k, :], win.ap()[:, k, cols], [win.r(), h.r()], [pst.r()],
                         start=(k == 0), stop=(k == KT - 1))
            K.cp(glo.ap(), ps_f.ap()[0:16, 256:384], [ps_f.r()], [glo.r()], eng="act")
            K.mm(ps_l.ap()[:, 0:128], glo.ap(), wgk2.ap(), [glo.r(), wgk2.r()], [ps_l.r()])
            K.stt(lg.ap(), ps_l.ap()[:, 0:128], -1.0, bgkB.ap(), ALU.mult, ALU.subtract, [ps_l.r(), bgkB.r()], [lg.r()])
            softplus_(K, (lg.ap(), lg.r()), (lgt.ap(), lgt.r()), None, None)
            K.ts(lg.ap(), lg.ap(), -1.0 / 16.0, ALU.mult, [lg.r()], [lg.r()])
            K.mm(ps_l.ap()[:, 128:256], lg.ap(), maskU.ap(), [lg.r(), maskU.r()], [ps_l.r()])
            K.mm(ps_l.ap()[:, 256:384], maskU.ap(), lg.ap(), [lg.r(), maskU.r()], [ps_l.r()])
            K.act(Eq.ap(), ps_l.ap()[:, 128:256], AF.Exp, [ps_l.r()], [Eq.r()])
            K.act(Ek.ap(), ps_l.ap()[:, 128:256], AF.Exp, [ps_l.r()], [Ek.r()], scale=-1.0)
            K.act(Ekt.ap(), ps_l.ap()[:, 256:384], AF.Exp, [ps_l.r()], [Ekt.r()], scale=-1.0)
            K.stt(qt.ap(), ps_f.ap()[:, 0:128], 32.0 ** -0.5, Eq.ap(), ALU.mult, ALU.mult, [ps_f.r(), Eq.r()], [qt.r()])
            K.tt(kf.ap(), ps_f.ap()[:, 128:256], Ek.ap(), ALU.mult, [ps_f.r(), Ek.r()], [kf.r()])
            K.tt(km.ap(), bc(kf.ap(), 1, [128, 4, 128]), bc(hm.ap(), 2, [128, 4, 128]), ALU.mult, [kf.r(), hm.r()],
                 [km.r()])
            K.tt(ktm.ap(), ps_m.ap()[:, 256:384], Ekt.ap(), ALU.mult, [ps_m.r(), Ekt.r()], [ktm.r()])
            K.cp(vtm.ap(), ps_m.ap()[:, 0:256], [ps_m.r()], [vtm.r()], eng="act")
            K.act(sgg.ap(), ps_g.ap()[:, 0:256], AF.Silu, [ps_g.r()], [sgg.r()])
            for hh in range(4):
                K.mm(ps_a.ap()[:, hh * 128:(hh + 1) * 128], km.ap()[:, hh, :], qt.ap(), [km.r(), qt.r()], [ps_a.r()],
                     inc=(hh == 3))
            K.tt(A.ap(), ps_a.ap().rearrange("p (h i) -> p h i", h=4), bc(maskU.ap(), 1, [128, 4, 128]), ALU.mult,
                 [ps_a.r(), maskU.r()], [A.r()])
            K.mm(ps_o.ap()[:, 0:256], qt.ap(), Sb.ap(), [qt.r(), Sb.r()], [ps_o.r()], start=True, stop=False)
            for hh in range(4):
                K.mm(ps_o.ap()[:, hh * 64:(hh + 1) * 64], A.ap()[:, hh, :], vtm.ap()[:, hh * 64:(hh + 1) * 64],
                     [A.r(), vtm.r()], [ps_o.r()], start=False, stop=(hh == 3))
            K.act(osq.ap(), ps_o.ap()[:, 0:256], AF.Square, [ps_o.r()], [osq.r()])
            K.red(ms.ap(), osq.ap().rearrange("p (h e) -> p h e", h=4), [osq.r()], [ms.r()])
            K.act(ms.ap(), ms.ap(), AF.Sqrt, [ms.r()], [ms.r()], scale=1.0 / 64, bias=RMS_EPS)
            K.recip(ms.ap(), ms.ap(), [ms.r()], [ms.r()])
            K.tt(on.ap().rearrange("p (h e) -> p h e", h=4), ps_o.ap()[:, 0:256].rearrange("p (h e) -> p h e", h=4),
                 bc(ms.ap(), 2, [128, 4, 64]), ALU.mult, [ps_o.r(), ms.r()], [on.r()])
            K.tt(onb.ap(), on.ap(), sgg.ap(), ALU.mult, [on.r(), sgg.r()], [onb.r()])
            for q in range(2):
                K.tr(ps_t.ap()[:, q * 128:(q + 1) * 128], onb.ap()[:, q * 128:(q + 1) * 128], identb.ap(),
                     [onb.r(), identb.r()], [ps_t.r()], inc=(q == 1))
            K.ts(yT.ap()[:, 6:8, c * 128:(c + 1) * 128], ps_t.ap()[:, 0:256].rearrange("p (t n) -> p t n", t=2),
                 gcol.ap(), ALU.mult, [ps_t.r(), gcol.r()], xr(yT, c // 4, range(6, 8)))
            K.mm(ps_o.ap()[:, 256:512], ktm.ap(), vtm.ap(), [ktm.r(), vtm.r()], [ps_o.r()])
            K.tt(tmpS.ap(), ps_o.ap()[:, 256:512], BM.ap(), ALU.mult, [ps_o.r(), BM.r()], [tmpS.r()])
            K.tt(S32.ap(), S32.ap(), tmpS.ap(), ALU.add, [S32.r(), tmpS.r()], [S32.r()])
            K.ts(S32.ap(), S32.ap(), Eq.ap()[:, 127:128], ALU.mult, [S32.r(), Eq.r()], [S32.r()])
            K.cp(Sb.ap(), S32.ap(), [S32.r()], [Sb.r()], eng="act")
        for hh in range(4):
            K.dma(dr["p_gla"][l, hh], S32.ap()[32 * hh:32 * hh + 32, 64 * hh:64 * hh + 64], r=[S32.r()])
        P.barrier()


def gla_sample(K, dr, C, l, yT, win, wgk2, bgkB):
    P = K.P
    hs, identb = C["hs"], C["identb"]
    with contextlib.ExitStack() as ph:
        glo = K.sb(ph, "glos", [16, NS], BF16)
        lg = K.sb(ph, "lgs", [NS, 128], F32)
        lgt = K.sb(ph, "lgts", [NS, 128], F32)
        pk = K.sb(ph, "pkg", [NS, 4, 160], F32)
        sgg = K.sb(ph, "sggs", [NS, 256], F32)
        gB = K.sb(ph, "gBg", [64, 64], F32)
        S = K.sb(ph, "Sg", [64, 32, 64], F32)
        tmp = K.sb(ph, "tmpg", [64, 32, 64], F32)
        pkh = K.sb(ph, "pkhg", [64, 160], F32)
        o = K.sb(ph, "og", [64, 64], F32)
        junk = K.sb(ph, "junkg", [64, 64], F32)
        ss = K.sb(ph, "ssg", [64, 1], F32)
        otm = K.sb(ph, "otm", [NS, 256], F32)
        otb = K.sb(ph, "otb", [NS, 256], BF16)
        ps_a = K.ps(ph, "psgs_a", [128, 512], F32)
        ps_b = K.ps(ph, "psgs_b", [128, 512], F32)
        ps_c = K.ps(ph, "psgs_c", [128, 512], F32)
        ps_t = K.ps(ph, "psgs_t", [128, 1024], BF16)
        spk = dram_scratch(K, "gpk", [NS, 640])
        so = dram_scratch(K, "go", [NS, 256])
        K.dma(gB.ap(), dr["gla_norm_g"][l:l + 1, :].to_broadcast([64, 64]), w=[gB.r()])
        K.dma(S.ap().rearrange("p d e -> p (d e)"), dr["st_gla"][l].rearrange("b h d e -> (b h) (d e)"), w=[S.r()])
        for k in range(KT):
            K.mm(ps_a.ap()[0:NS, :], hs.ap()[:, k, :], win.ap()[:, k, 0:512], [hs.r(), win.r()], [ps_a.r()],
                 start=(k == 0), stop=(k == KT - 1))
        for k in range(KT):
            K.mm(ps_b.ap()[0:NS, 0:256], hs.ap()[:, k, :], win.ap()[:, k, 528:784], [hs.r(), win.r()], [ps_b.r()],
                 start=(k == 0), stop=(k == KT - 1))
        for k in range(KT):
            K.mm(ps_c.ap()[0:16, 0:NS], win.ap()[:, k, 512:528], hs.ap()[:, k, :], [hs.r(), win.r()], [ps_c.r()],
                 start=(k == 0), stop=(k == KT - 1))
        K.cp(glo.ap(), ps_c.ap()[0:16, 0:NS], [ps_c.r()], [glo.r()], eng="act")
        K.mm(ps_c.ap()[0:NS, 128:256], glo.ap(), wgk2.ap(), [glo.r(), wgk2.r()], [ps_c.r()])
        K.stt(lg.ap(), ps_c.ap()[0:NS, 128:256], -1.0, bgkB.ap()[0:NS, :], ALU.mult, ALU.subtract,
              [ps_c.r(), bgkB.r()], [lg.r()])
        softplus_(K, (lg.ap(), lg.r()), (lgt.ap(), lgt.r()), None, None)
        K.act(lg.ap(), lg.ap(), AF.Exp, [lg.r()], [lg.r()], scale=-1.0 / 16.0)
        K.act(sgg.ap(), ps_b.ap()[0:NS, 0:256], AF.Silu, [ps_b.r()], [sgg.r()])
        K.ts(pk.ap()[:, :, 0:32], ps_a.ap()[0:NS, 0:128].rearrange("p (h d) -> p h d", h=4), 32.0 ** -0.5, ALU.mult,
             [ps_a.r()], [pk.r()])
        K.cp(pk.ap()[:, :, 32:64], ps_a.ap()[0:NS, 128:256].rearrange("p (h d) -> p h d", h=4), [ps_a.r()], [pk.r()])
        K.cp(pk.ap()[:, :, 64:96], lg.ap().rearrange("p (h d) -> p h d", h=4), [lg.r()], [pk.r()])
        K.cp(pk.ap()[:, :, 96:160], ps_a.ap()[0:NS, 256:512].rearrange("p (h e) -> p h e", h=4), [ps_a.r()], [pk.r()])
        K.dma(spk.ap(), pk.ap().rearrange("p h x -> p (h x)"), r=[pk.r()], w=[spk.r()])
        K.dma(pkh.ap(), spk.ap().rearrange("b (h x) -> (b h) x", h=4), r=[spk.r()], w=[pkh.r()])
        qh, kh, eh, vh = pkh.ap()[:, 0:32], pkh.ap()[:, 32:64], pkh.ap()[:, 64:96], pkh.ap()[:, 96:160]
        K.tt(S.ap(), S.ap(), bc(eh, 2, [64, 32, 64]), ALU.mult, [S.r(), pkh.r()], [S.r()])
        K.tt(tmp.ap(), bc(kh, 2, [64, 32, 64]), bc(vh, 1, [64, 32, 64]), ALU.mult, [pkh.r()], [tmp.r()])
        K.tt(S.ap(), S.ap(), tmp.ap(), ALU.add, [S.r(), tmp.r()], [S.r()])
        K.dma(dr["s_gla"][l].rearrange("b h d e -> (b h) (d e)"), S.ap().rearrange("p d e -> p (d e)"), r=[S.r()])
        K.tt(tmp.ap(), S.ap(), bc(qh, 2, [64, 32, 64]), ALU.mult, [S.r(), pkh.r()], [tmp.r()])
        K.red(o.ap(), tmp.ap().rearrange("p d e -> p e d"), [tmp.r()], [o.r()])
        K.act(junk.ap(), o.ap(), AF.Square, [o.r()], [junk.r(), ss.r()], accum_out=ss.ap())
        K.act(ss.ap(), ss.ap(), AF.Sqrt, [ss.r()], [ss.r()], scale=1.0 / 64, bias=RMS_EPS)
        K.recip(ss.ap(), ss.ap(), [ss.r()], [ss.r()])
        K.stt(o.ap(), o.ap(), ss.ap(), gB.ap(), ALU.mult, ALU.mult, [o.r(), ss.r(), gB.r()], [o.r()])
        K.dma(so.ap().rearrange("b (h e) -> (b h) e", h=4), o.ap(), r=[o.r()], w=[so.r()])
        K.dma(otm.ap(), so.ap(), r=[so.r()], w=[otm.r()])
        K.tt(otb.ap(), otm.ap(), sgg.ap(), ALU.mult, [otm.r(), sgg.r()], [otb.r()])
        for q in range(2):
            K.tr(ps_t.ap()[:, q * NS:(q + 1) * NS], otb.ap()[:, q * 128:(q + 1) * 128], identb.ap()[0:NS, 0:NS],
                 [otb.r(), identb.r()], [ps_t.r()], inc=(q == 1))
        K.cp(yT.ap()[:, 6:8, T:T + NS], ps_t.ap()[:, 0:2 * NS].rearrange("p (t n) -> p t n", t=2), [ps_t.r()],
             xr(yT, 4, range(6, 8)))
        P.barrier()


C0 = float(np.exp(-0.5))


def rwkv_prep(K, C, pc, LW, N, rw, prev, B, pl, pg, pn, ee="dve"):
    blk64 = C["blk64"]
    MX, LI = B["MX"], B["LI"]
    mxa = MX.ap()[:, :, 0:N]
    K.tt(mxa, prev, rw, ALU.subtract, B["_rw_res"], [MX.r()], eng=ee)
    yield
    K.tt(mxa, mxa, bc(pc["mu"].ap(), 2, [128, 7, N]), ALU.mult, [MX.r(), pc["mu"].r()], [MX.r()], eng=ee)
    yield
    K.tt(mxa, mxa, rw, ALU.add, [MX.r()] + B["_rw_res"], [MX.r()], eng=ee)
    yield
    r, k, v = (MX.ap()[:, 0:2, 0:N], MX.ap()[:, 2:4, 0:N], MX.ap()[:, 4:6, 0:N])
    lia = LI.ap()[:, 0:N]
    lif = B["t1"].ap()[:, 0, 0:N]
    K.act(lif, MX.ap()[:, 6, 0:N], AF.Exp, [MX.r(), pc["lisc"].r()], [B["t1"].r()], scale=pc["lisc"].ap())
    yield
    sigmoid_chain(K, lif, [B["t1"].r()])
    yield
    K.ts(lia[0:32], lif[0:32], 2.0, ALU.mult, [B["t1"].r()], [LI.r()], s2=-1.0, op1=ALU.add)
    K.cp(lia[32:64], MX.ap()[32:64, 6, 0:N], [MX.r()], [LI.r()], eng="act")
    K.cp(lia[64:128], lif[64:128], [B["t1"].r()], [LI.r()])
    yield
    for t in range(2):
        cs = slice(t * 128, (t + 1) * 128)
        K.mm(pl.ap()[:, t * N:(t + 1) * N], LW.ap()[0:32, cs], lia[0:32], [LW.r(), LI.r()], [pl.r()], self_wait=True)
        K.mm(pl.ap()[:, (2 + t) * N:(3 + t) * N], LW.ap()[32:64, cs], lia[32:64], [LW.r(), LI.r()], [pl.r()],
             self_wait=True)
        K.mm(pg.ap()[:, t * N:(t + 1) * N], LW.ap()[64:128, cs], lia[64:128], [LW.r(), LI.r()], [pg.r()],
             self_wait=True)
    g = lambda n: B[n].ap()[:, :, 0:N]
    for t in range(2):
        K.act(B["sig"].ap()[:, t, 0:N], pl.ap()[:, t * N:(t + 1) * N], AF.Exp, [pl.r(), pc["nw0"].r()],
              [B["sig"].r()], scale=-1.0, bias=pc["nw0"].ap()[:, t:t + 1])
        K.act(B["aic"].ap()[:, t, 0:N], pl.ap()[:, (2 + t) * N:(3 + t) * N], AF.Exp, [pl.r(), pc["na0"].r()],
              [B["aic"].r()], scale=-1.0, bias=pc["na0"].ap()[:, t:t + 1])
    sigmoid_chain(K, g("sig"), [B["sig"].r()])
    sigmoid_chain(K, g("aic"), [B["aic"].r()])
    for t in range(0):
        pass
    K.cp(g("gate"), pg.ap()[:, 0:2 * N].rearrange("p (t n) -> p t n", t=2), [pg.r()], [B["gate"].r()], eng="act")
    yield
    K.tt(g("kk"), k, bc(pc["k_k"].ap(), 2, [128, 2, N]), ALU.mult, [MX.r(), pc["k_k"].r()], [B["kk"].r()], eng=ee)
    yield
    K.tt(g("t1"), g("kk"), g("kk"), ALU.mult, [B["kk"].r()], [B["t1"].r()], eng=ee)
    yield
    for t in range(2):
        K.mm(pn.ap()[:, t * N:(t + 1) * N], blk64.ap(), B["t1"].ap()[:, t, 0:N], [blk64.r(), B["t1"].r()], [pn.r()])
    K.act(g("t1"), pn.ap()[:, 0:2 * N].rearrange("p (t n) -> p t n", t=2), AF.Ln, [pn.r()], [B["t1"].r()],
          bias=1e-12)
    K.act(g("t1"), g("t1"), AF.Exp, [B["t1"].r()], [B["t1"].r()], scale=-0.5)
    yield
    K.tt(g("kk"), g("kk"), g("t1"), ALU.mult, [B["kk"].r(), B["t1"].r()], [B["kk"].r()], eng=ee)
    yield
    yield
    K.tt(g("t1"), g("aic"), bc(pc["k_a"].ap(), 2, [128, 2, N]), ALU.mult, [B["aic"].r(), pc["k_a"].r()], [B["t1"].r()], eng=ee)
    yield
    K.tt(g("t1"), g("t1"), bc(pc["omka"].ap(), 2, [128, 2, N]), ALU.add, [B["t1"].r(), pc["omka"].r()], [B["t1"].r()], eng=ee)
    yield
    K.tt(g("kp"), k, g("t1"), ALU.mult, [MX.r(), B["t1"].r()], [B["kp"].r()], eng=ee)
    yield
    K.tt(g("t1"), r, g("kp"), ALU.mult, [MX.r(), B["kp"].r()], [B["t1"].r()], eng=ee)
    yield
    K.tt(g("t1"), g("t1"), bc(pc["r_k"].ap(), 2, [128, 2, N]), ALU.mult, [B["t1"].r(), pc["r_k"].r()], [B["t1"].r()], eng=ee)
    yield
    for t in range(2):
        K.mm(pn.ap()[:, t * N:(t + 1) * N], blk64.ap(), B["t1"].ap()[:, t, 0:N], [blk64.r(), B["t1"].r()], [pn.r()])
    K.tt(g("bonus"), pn.ap()[:, 0:2 * N].rearrange("p (t n) -> p t n", t=2), v, ALU.mult, [pn.r(), MX.r()],
         [B["bonus"].r()])
    B['_rkv'] = (r, k, v)
    yield


def rwkv_params(K, dr, l, ph):
    pc = {}
    mu = K.sb(ph, "mu", [128, 7], F32)
    K.dma(mu.ap(), dr["rwkv_mu"][l].rearrange("(t p) -> p t", p=128), w=[mu.r()])
    pc["mu"] = mu
    for n, src in (("w0", dr["rwkv_w0"][l]), ("a0", dr["rwkv_a0"][l]), ("k_k", dr["rwkv_k_k"][l]),
                   ("k_a", dr["rwkv_k_a"][l]), ("r_k", dr["rwkv_r_k"][l].rearrange("h n -> (h n)")),
                   ("ln_g", dr["rwkv_ln_g"][l]), ("ln_b", dr["rwkv_ln_b"][l])):
        t = K.sb(ph, "pc_" + n, [128, 2], F32)
        K.dma(t.ap(), src.rearrange("(t p) -> p t", p=128), w=[t.r()])
        pc[n] = t
    for n in ("w0", "a0"):
        t = K.sb(ph, "pc_n" + n, [128, 2], F32)
        K.ts(t.ap(), pc[n].ap(), -1.0, ALU.mult, [pc[n].r()], [t.r()])
        pc["n" + n] = t
    lisc = K.sb(ph, "lisc", [128, 1], F32)
    K.memset(lisc.ap()[0:32], -2.0, [lisc.r()])
    K.memset(lisc.ap()[32:64], 0.0, [lisc.r()])
    K.memset(lisc.ap()[64:128], -1.0, [lisc.r()])
    pc["lisc"] = lisc
    omka = K.sb(ph, "omka", [128, 2], F32)
    K.ts(omka.ap(), pc["k_a"].ap(), -1.0, ALU.mult, [pc["k_a"].r()], [omka.r()], s2=1.0, op1=ALU.add)
    pc["omka"] = omka
    LW = K.sb(ph, "LW", [128, 256], BF16)
    K.dma(LW.ap()[0:32, :], dr["rwkv_w2"][l], w=[LW.r()], q="pool")
    K.dma(LW.ap()[32:64, :], dr["rwkv_a2"][l], w=[LW.r()], q="pool")
    K.dma(LW.ap()[64:128, :], dr["rwkv_g2"][l], w=[LW.r()], q="pool")
    return pc, LW


def rwkv_epilogue(K, C, pc, B, N, pT, ydst, yres):
    for t in range(2):
        K.act(B["t1"].ap()[:, t, 0:N], pT.ap()[:, t * N:(t + 1) * N], AF.Identity, [pT.r(), pc["ln_g"].r(), pc["ln_b"].r()],
              [B["t1"].r()], scale=pc["ln_g"].ap()[:, t:t + 1], bias=pc["ln_b"].ap()[:, t:t + 1])
    g = lambda n: B[n].ap()[:, :, 0:N]
    K.tt(g("t1"), g("t1"), g("bonus"), ALU.add, [B["t1"].r(), B["bonus"].r()], [B["t1"].r()])
    K.tt(ydst, g("t1"), g("gate"), ALU.mult, [B["t1"].r(), B["gate"].r()], yres)


def groupnorm64(K, o_ap, n, G, scr, res_in, out_ap, out_res):
    mean, xc, sq, var = scr
    K.red(mean.ap()[0:n, 0:G], o_ap, res_in, [mean.r()])
    K.ts(mean.ap()[0:n, 0:G], mean.ap()[0:n, 0:G], 1.0 / 64, ALU.mult, [mean.r()], [mean.r()])
    xca = xc.ap()[0:n, 0:G * 64].rearrange("p (g e) -> p g e", g=G)
    K.tt(xca, o_ap, bc(mean.ap()[0:n, 0:G], 2, [n, G, 64]), ALU.subtract, res_in + [mean.r()], [xc.r()])
    sqa = sq.ap()[0:n, 0:G * 64].rearrange("p (g e) -> p g e", g=G)
    K.tt(sqa, xca, xca, ALU.mult, [xc.r()], [sq.r()])
    K.red(var.ap()[0:n, 0:G], sqa, [sq.r()], [var.r()])
    rsqrt_(K, var.ap()[0:n, 0:G], [var.r()], 1.0 / 64, RWKV_GN_EPS)
    K.tt(out_ap, xca, bc(var.ap()[0:n, 0:G], 2, [n, G, 64]), ALU.mult, [xc.r(), var.r()], out_res)


def rwkv_phase(K, dr, C, l, yT, dbg_out):
    P = K.P
    R0 = OFF["rw"]
    with contextlib.ExitStack() as ph:
        win = K.sb(ph, "win_rwkv", [128, KT, 896], BF16)
        K.dma(win.ap(), dr["w_in"][l, :, R0:R0 + 896].rearrange("(k p) n -> p k n", p=128), w=[win.r()], q="pool")
        pc, LW = rwkv_params(K, dr, l, ph)
        import os
        if os.environ.get("SKIP_RWKV_PROMPT") != "1":
            rwkv_prompt(K, dr, C, l, yT, win, pc, LW, dbg_out)
        P.barrier()
        if os.environ.get("SKIP_RWKV_SAMPLE") != "1":
            rwkv_sample(K, dr, C, l, yT, win, pc, LW, dbg_out)


def interleave(gens, ratio=None):
    gens = [g for g in gens if g is not None]
    ratio = ratio or [1] * len(gens)
    live = list(zip(gens, ratio))
    while live:
        for item in list(live):
            g, n = item
            for _ in range(n):
                try:
                    next(g)
                except StopIteration:
                    live.remove(item)
                    break


def interleave_gen(gens, ratio):
    live = list(zip(gens, ratio))
    while live:
        for item in list(live):
            g, n = item
            for _ in range(n):
                try:
                    next(g)
                except StopIteration:
                    live.remove(item)
                    break
                yield


def rwkv_prompt(K, dr, C, l, yT, win, pc, LW, dbg_out):
    P = K.P
    identb, identf, maskU, maskSU, maskSL, blk64 = (C[k] for k in ["identb", "identf", "maskU", "maskSU", "maskSL", "blk64"])
    hc = C["hc"]
    N = 128
    with contextlib.ExitStack() as ph:
        f3 = lambda n: K.sb(ph, n, [128, 2, N], F32)
        Bs = []
        for i in range(2):
            B = {n: f3(f"rb{i}_" + n) for n in ["sig", "aic", "gate", "kk", "t1", "kp", "bonus", "cs", "e1", "e2", "bb"]}
            Bs.append(B)
        MX = K.sb(ph, "MX", [128, 7, N], F32)
        LI = K.sb(ph, "LI", [128, N], BF16)
        for B in Bs:
            B["MX"], B["LI"] = MX, LI
        RW = [K.sb(ph, f"RW{i}", [128, 7, N + 1], F32) for i in range(2)]
        ones_r = K.sb(ph, "ones_r", [128, N], F32)
        bcol = K.sb(ph, "bcol", [128, 2], F32)
        MK2 = K.sb(ph, "MK2", [128, 2, N], F32)
        ARs = [K.sb(ph, f"AR{i}", [128, 2, 2, N], BF16) for i in range(2)]
        BKs = [K.sb(ph, f"BK{i}", [128, 2, 2, N], BF16) for i in range(2)]
        FH = K.sb(ph, "FH", [128, 3, 2, N], BF16)
        TMs = [K.sb(ph, f"TM{i}", [128, 4, 2, N], BF16) for i in range(2)]
        t2 = f3("rb_t2")
        A1 = K.sb(ph, "A1", [128, 4, 2, N], BF16)
        A2 = K.sb(ph, "A2", [128, 4, 2, N], BF16)
        Lb = [K.sb(ph, f"Lb{i}", [128, 4, N], BF16) for i in range(2)]
        Nb = [K.sb(ph, f"Nb{i}", [128, 4, N], BF16) for i in range(2)]
        X32 = K.sb(ph, "X32", [128, 4, 2, 64], F32)
        Xb = K.sb(ph, "Xb", [128, 4, 2, 64], BF16)
        Apf = K.sb(ph, "Apf", [128, 2, N], BF16)
        XAc = K.sb(ph, "XAc", [128, 256], BF16)
        Utm = K.sb(ph, "Utm", [128, 4, 64], BF16)
        ST32 = K.sb(ph, "ST32", [128, 2, N], F32)
        STb = K.sb(ph, "STb", [128, 2, N], BF16)
        tmpS = K.sb(ph, "tmpSr", [128, 2, N], F32)
        gn = (K.sb(ph, "gn_mean", [128, 4], F32), K.sb(ph, "gn_xc", [128, 256], F32),
              K.sb(ph, "gn_sq", [128, 256], F32), K.sb(ph, "gn_var", [128, 4], F32))
        onb = K.sb(ph, "onbr", [128, 256], BF16)
        stT = K.sb(ph, "stT", [128, 2, N], F32)
        pI = K.ps(ph, "pr_I", [128, 512], F32)
        pM = K.ps(ph, "pr_M", [128, 512], F32)
        pT = K.ps(ph, "pr_T", [128, 1024], BF16)
        pT2 = pT
        pAT = K.ps(ph, "pr_AT", [128, 1024], F32)
        pL = K.ps(ph, "pr_L", [128, 512], F32)
        pL2 = K.ps(ph, "pr_L2", [128, 512], F32)
        pX = K.ps(ph, "pr_X", [128, 512], F32)
        pO = pX

        K.memset(ones_r.ap(), 1.0, [ones_r.r()])
        K.cp(MK2.ap()[:, 0, :], maskSU.ap(), [maskSU.r()], [MK2.r()])
        K.cp(MK2.ap()[:, 1, :], maskU.ap(), [maskU.r()], [MK2.r()])
        K.memset(ST32.ap(), 0.0, [ST32.r()])
        K.memset(STb.ap(), 0.0, [STb.r()])
        K.memset(RW[0].ap()[:, :, 0:1], 0.0, [RW[0].r()])

        import os
        PE1 = os.environ.get("RWKV_S1_ENG", "dve")

        def s1(c):
            B, AR, BK, TM = Bs[c % 2], ARs[c % 2], BKs[c % 2], TMs[c % 2]
            g = lambda n: B[n].ap()
            h = hc[c % 2]
            make_hc(K, C, l, h, c)
            yield
            rw = RW[c % 2]
            for (t0, t1) in ((0, 4), (4, 7)):
                for t in range(t0, t1):
                    for k in range(KT):
                        K.mm(pI.ap()[:, (t - t0) * N:(t - t0 + 1) * N], win.ap()[:, k, t * N:(t + 1) * N], h.ap()[:, k, :],
                             [win.r(), h.r()], [pI.r()], start=(k == 0), stop=(k == KT - 1))
                    yield
                K.cp(rw.ap()[:, t0:t1, 1:N + 1], pI.ap()[:, 0:(t1 - t0) * N].rearrange("p (t n) -> p t n", t=t1 - t0),
                     [pI.r()], [rw.r()], eng="act")
                yield
            if c + 1 < NCH:
                K.cp(RW[(c + 1) % 2].ap()[:, :, 0:1], rw.ap()[:, :, N:N + 1], [rw.r()], [RW[(c + 1) % 2].r()])
            B["_rw_res"] = [rw.r()]
            yield from rwkv_prep(K, C, pc, LW, N, rw.ap()[:, :, 1:N + 1], rw.ap()[:, :, 0:N], B, pM, pI, pI, ee=PE1)
            r, k_, v = B["_rkv"]
            MXr = MX.r()
            for t in range(2):
                K.P.op("dve", lambda e, t=t, B=B: e.tensor_tensor_scan(out=B["cs"].ap()[:, t, :], data0=ones_r.ap(),
                                                                        data1=B["sig"].ap()[:, t, :], initial=0.0,
                                                                        op0=ALU.mult, op1=ALU.add),
                       reads=[ones_r.r(), B["sig"].r()], writes=[B["cs"].r()])
            yield
            K.act(g("e1"), g("cs"), AF.Exp, [B["cs"].r()], [B["e1"].r()], scale=-C0)
            yield
            K.act(g("e2"), g("cs"), AF.Exp, [B["cs"].r()], [B["e2"].r()], scale=C0)
            yield
            K.tt(AR.ap()[:, :, 1, :], r, g("e1"), ALU.mult, [MXr, B["e1"].r()], [AR.r()], eng=PE1)
            yield
            K.tt(g("bb"), g("kk"), g("aic"), ALU.mult, [B["kk"].r(), B["aic"].r()], [B["bb"].r()], eng=PE1)
            yield
            K.tt(BK.ap()[:, :, 0, :], g("bb"), g("e2"), ALU.mult, [B["bb"].r(), B["e2"].r()], [BK.r()], eng=PE1)
            yield
            K.tt(BK.ap()[:, :, 1, :], g("kp"), g("e2"), ALU.mult, [B["kp"].r(), B["e2"].r()], [BK.r()], eng=PE1)
            yield
            K.tt(g("t1"), g("cs"), g("sig"), ALU.subtract, [B["cs"].r(), B["sig"].r()], [B["t1"].r()], eng=PE1)
            yield
            K.act(g("e2"), g("t1"), AF.Exp, [B["t1"].r()], [B["e2"].r()], scale=-C0)
            yield
            K.stt(AR.ap()[:, :, 0, :], g("kk"), -1.0, g("e2"), ALU.mult, ALU.mult, [B["kk"].r(), B["e2"].r()], [AR.r()])
            yield
            K.ts(bcol.ap(), B["cs"].ap()[:, :, N - 1], -C0, ALU.mult, [B["cs"].r()], [bcol.r()])
            yield
            for t in range(2):
                K.act(B["e2"].ap()[:, t, :], B["cs"].ap()[:, t, :], AF.Exp, [B["cs"].r(), bcol.r()], [B["e2"].r()],
                      scale=C0, bias=bcol.ap()[:, t:t + 1])
            yield
            K.tt(FH.ap()[:, 0], g("bb"), g("e2"), ALU.mult, [B["bb"].r(), B["e2"].r()], [FH.r()], eng=PE1)
            yield
            K.tt(FH.ap()[:, 1], g("kp"), g("e2"), ALU.mult, [B["kp"].r(), B["e2"].r()], [FH.r()], eng=PE1)
            yield
            K.cp(FH.ap()[:, 2], v, [MXr], [FH.r()], eng="act")
            yield
            for q in range(4):
                for t in range(2):
                    src = FH.ap()[:, q, t, :] if q < 3 else AR.ap()[:, t, 0, :]
                    K.tr(pT.ap()[:, (q * 2 + t) * N:(q * 2 + t + 1) * N], src, identb.ap(),
                         [FH.r(), AR.r(), identb.r()], [pT.r()], inc=(t == 1))
                yield
            K.cp(TM.ap().rearrange("p q t n -> p (q t n)"), pT.ap(), [pT.r()], [TM.r()], eng="act")
            yield

        def s2(c):
            B, AR, BK, TM = Bs[c % 2], ARs[c % 2], BKs[c % 2], TMs[c % 2]
            mk = bc(MK2.ap(), 1, [128, 4, 2, N])
            for which, Adst in ((0, A1), (1, A2)):
                for hd in range(4):
                    t, o = hd // 2, 64 * (hd % 2)
                    sl = slice(o, o + 64)
                    arf = AR.ap()[sl, t].rearrange("p a n -> p (a n)")
                    K.mm(pAT.ap()[:, hd * 256:(hd + 1) * 256], BK.ap()[sl, t, which, :], arf, [BK.r(), AR.r()], [pAT.r()],
                         self_wait=True)
                yield
                K.tt(Adst.ap(), pAT.ap().rearrange("p (h a n) -> p h a n", h=4, a=2), mk, ALU.mult, [pAT.r(), MK2.r()],
                     [Adst.r()])
                yield
            for hd in range(4):
                t, o = hd // 2, 64 * (hd % 2)
                sl = slice(o, o + 64)
                K.mm(pL.ap()[:, hd * N:(hd + 1) * N], AR.ap()[sl, t, 0, :], BK.ap()[sl, t, 0, :], [BK.r(), AR.r()],
                     [pL.r()], self_wait=True)
            yield
            K.tt(Lb[0].ap(), pL.ap().rearrange("p (h n) -> p h n", h=4), bc(maskSL.ap(), 1, [128, 4, N]), ALU.mult,
                 [pL.r(), maskSL.r()], [Lb[0].r()])
            yield
            vtm = TM.ap()[:, 2].rearrange("p t n -> p (t n)")
            for hd in range(4):
                K.mm(pX.ap()[:, hd * 64:(hd + 1) * 64], A2.ap()[:, hd, 0, :], vtm[:, hd * 64:(hd + 1) * 64],
                     [A2.r(), TM.r()], [pX.r()], inc=(hd == 3))
            yield
            K.cp(X32.ap()[:, :, 0, :], TM.ap()[:, 3].rearrange("p t (hh k) -> p (t hh) k", hh=2), [TM.r()], [X32.r()])
            yield
            K.cp(X32.ap()[:, :, 1, :], pX.ap()[:, 0:256].rearrange("p (h v) -> p h v", h=4), [pX.r()], [X32.r()],
                 eng="act")
            yield
            K.cp(Xb.ap(), X32.ap(), [X32.r()], [Xb.r()], eng="act")
            yield
            for i in range(7):
                if i == 0:
                    nref = lambda hd: A1.ap()[:, hd, 0, :]
                    nres = A1.r()
                    lcur = Lb[0]
                else:
                    nprev_ref, nprev_res, lprev = nref, nres, lcur
                    nnew, lnew = Nb[i % 2], Lb[i % 2]
                    for hd in range(4):
                        K.mm(pL.ap()[:, hd * N:(hd + 1) * N], lprev.ap()[:, hd, :], nprev_ref(hd), [lprev.r(), nprev_res],
                             [pL.r()], inc=(hd == 3))
                    yield
                    if i < 6:
                        for hd in range(4):
                            K.mm(pL2.ap()[:, hd * N:(hd + 1) * N], nprev_ref(hd), lprev.ap()[:, hd, :],
                                 [lprev.r(), nprev_res], [pL2.r()], inc=(hd == 3))
                        yield
                    K.cp(nnew.ap(), pL.ap().rearrange("p (h n) -> p h n", h=4), [pL.r()], [nnew.r()], eng="act")
                    yield
                    if i < 6:
                        K.cp(lnew.ap(), pL2.ap().rearrange("p (h n) -> p h n", h=4), [pL2.r()], [lnew.r()])
                        yield
                    nref = lambda hd, nnew=nnew: nnew.ap()[:, hd, :]
                    nres = nnew.r()
                    lcur = lnew
                for hd in range(4):
                    K.mm(pX.ap()[:, hd * N:(hd + 1) * N], nref(hd), Xb.ap()[:, hd].rearrange("p a k -> p (a k)"),
                         [nres, Xb.r()], [pX.r()], inc=(hd == 3))
                yield
                K.tt(X32.ap(), X32.ap(), pX.ap().rearrange("p (h a k) -> p h a k", h=4, a=2), ALU.add,
                     [X32.r(), pX.r()], [X32.r()])
                yield
                K.cp(Xb.ap(), X32.ap(), [X32.r()], [Xb.r()], eng="act")
                yield
            K.cp(XAc.ap().rearrange("p (h k) -> p h k", h=4), X32.ap()[:, :, 0, :], [X32.r()], [XAc.r()])
            yield
            for t in range(2):
                K.tr(pT2.ap()[:, t * N:(t + 1) * N], XAc.ap()[:, t * N:(t + 1) * N], identb.ap(), [XAc.r(), identb.r()],
                     [pT2.r()], inc=(t == 1))
            yield
            K.cp(Apf.ap(), pT2.ap()[:, 0:2 * N].rearrange("p (t n) -> p t n", t=2), [pT2.r()], [Apf.r()], eng="act")
            yield
            for t in range(2):
                K.mm(pX.ap()[:, t * N:(t + 1) * N], Apf.ap()[:, t, :], STb.ap()[:, t, :], [Apf.r(), STb.r()], [pX.r()],
                     inc=(t == 1))
            yield
            K.tt(Utm.ap(), pX.ap()[:, 0:256].rearrange("p (h v) -> p h v", h=4), X32.ap()[:, :, 1, :], ALU.add,
                 [pX.r(), X32.r()], [Utm.r()])
            yield
            for t in range(2):
                K.mm(pO.ap()[:, t * N:(t + 1) * N], AR.ap()[:, t, 1, :], STb.ap()[:, t, :], [AR.r(), STb.r()], [pO.r()],
                     start=(t == 0), stop=False, sgc=True)
            for hd in range(4):
                K.mm(pO.ap()[:, hd * 64:(hd + 1) * 64], A1.ap()[:, hd, 1, :], Utm.ap()[:, hd, :], [A1.r(), Utm.r()],
                     [pO.r()], start=False, stop=False, sgc=True)
                K.mm(pO.ap()[:, hd * 64:(hd + 1) * 64], A2.ap()[:, hd, 1, :], vtm[:, hd * 64:(hd + 1) * 64],
                     [A2.r(), TM.r()], [pO.r()], start=False, stop=False, sgc=True)
            for t in range(2):
                K.mm(pO.ap()[:, 256 + t * N:256 + (t + 1) * N], TM.ap()[:, 0, t, :],
                     Utm.ap()[:, 2 * t:2 * t + 2, :].rearrange("p h v -> p (h v)"), [TM.r(), Utm.r()], [pO.r()],
                     start=False, stop=False, sgc=True)
                K.mm(pO.ap()[:, 256 + t * N:256 + (t + 1) * N], TM.ap()[:, 1, t, :], vtm[:, t * N:(t + 1) * N],
                     [TM.r()], [pO.r()], start=False, stop=(t == 1), inc=(t == 1), sgc=True)
            yield
            K.tt(tmpS.ap(), pO.ap()[:, 256:512].rearrange("p (t n) -> p t n", t=2), bc(blk64.ap(), 1, [128, 2, N]),
                 ALU.mult, [pO.r(), blk64.r()], [tmpS.r()])
            yield
            K.tt(ST32.ap(), ST32.ap(), bc(B["e1"].ap()[:, :, N - 1], 2, [128, 2, N]), ALU.mult, [ST32.r(), B["e1"].r()],
                 [ST32.r()])
            yield
            K.tt(ST32.ap(), ST32.ap(), tmpS.ap(), ALU.add, [ST32.r(), tmpS.r()], [ST32.r()])
            yield
            K.cp(STb.ap(), ST32.ap(), [ST32.r()], [STb.r()], eng="act")
            yield
            groupnorm64(K, pO.ap()[:, 0:256].rearrange("p (h v) -> p h v", h=4), 128, 4, gn, [pO.r()],
                        onb.ap().rearrange("p (h v) -> p h v", h=4), [onb.r()])
            yield
            for t in range(2):
                K.tr(pT2.ap()[:, t * N:(t + 1) * N], onb.ap()[:, t * N:(t + 1) * N], identb.ap(), [onb.r(), identb.r()],
                     [pT2.r()], inc=(t == 1))
            yield
            B2 = dict(B)
            B2["t1"] = t2
            rwkv_epilogue(K, C, pc, B2, N, pT2, yT.ap()[:, 4:6, c * N:(c + 1) * N], xr(yT, c // 4, range(4, 6)))
            yield

        for _ in s1(0):
            pass
        for c in range(NCH):
            interleave([s2(c), s1(c + 1) if c + 1 < NCH else None], ratio=[3, 2])
        rw = RW[(NCH - 1) % 2]
        K.dma(dr["p_shift"][l].rearrange("(t p) -> p t", p=128), rw.ap()[:, :, N], r=[rw.r()])
        for t in range(2):
            K.tr(pL.ap()[:, t * N:(t + 1) * N], ST32.ap()[:, t, :], identf.ap(), [ST32.r(), identf.r()], [pL.r()],
                 inc=(t == 1))
        K.cp(stT.ap(), pL.ap()[:, 0:2 * N].rearrange("p (t n) -> p t n", t=2), [pL.r()], [stT.r()])
        for hd in range(4):
            t, o = hd // 2, 64 * (hd % 2)
            K.dma(dr["p_rwkv"][l, hd], stT.ap()[o:o + 64, t, o:o + 64], r=[stT.r()])
        P.barrier()


def rwkv_sample(K, dr, C, l, yT, win, pc, LW, dbg_out):
    P = K.P
    hs, identb, identf = C["hs"], C["identb"], C["identf"]
    N = NS
    with contextlib.ExitStack() as ph:
        f3 = lambda n: K.sb(ph, n, [128, 2, N], F32)
        B = {n: f3("rs_" + n) for n in ["sig", "aic", "gate", "kk", "t1", "kp", "bonus", "e1", "e2", "bb"]}
        B["MX"] = K.sb(ph, "MXs", [128, 7, N], F32)
        B["LI"] = K.sb(ph, "LIs", [128, N], BF16)
        rws = K.sb(ph, "rws", [128, 7, N], F32)
        prevs = K.sb(ph, "prevs", [128, 7, N], F32)
        shs = K.sb(ph, "shs", [NS, 896], F32)
        rwtm = K.sb(ph, "rwtm", [NS, 896], F32)
        pkT = K.sb(ph, "pkT", [NS, 6, 256], F32)
        pkh = K.sb(ph, "pkhr", [64, 6, 64], F32)
        S = K.sb(ph, "Sr", [64, 64, 64], F32)
        tmp = K.sb(ph, "tmpr", [64, 64, 64], F32)
        sa = K.sb(ph, "sa", [64, 64], F32)
        o = K.sb(ph, "orr", [64, 64], F32)
        on = K.sb(ph, "onr", [64, 64], F32)
        gn = (K.sb(ph, "gns_mean", [64, 1], F32), K.sb(ph, "gns_xc", [64, 64], F32),
              K.sb(ph, "gns_sq", [64, 64], F32), K.sb(ph, "gns_var", [64, 1], F32))
        otm = K.sb(ph, "otmr", [NS, 256], F32)
        otb = K.sb(ph, "otbr", [NS, 256], BF16)
        pA = K.ps(ph, "prs_A", [128, 1024], F32)
        pB = K.ps(ph, "prs_B", [128, 1024], F32)
        pL = K.ps(ph, "prs_L", [128, 512], F32)
        pM = K.ps(ph, "prs_M", [128, 512], F32)
        pT = K.ps(ph, "prs_T", [128, 1024], BF16)
        scr = dram_scratch(K, "rpk", [NS, 4, 6, 64])
        so = dram_scratch(K, "ro", [NS, 256])

        K.dma(shs.ap(), dr["st_shift"][l], w=[shs.r()])
        K.dma(S.ap().rearrange("p v k -> p (v k)"), dr["st_rwkv"][l].rearrange("b h v k -> (b h) (v k)"), w=[S.r()])
        for t in range(7):
            for k in range(KT):
                K.mm(pM.ap()[:, t * N:(t + 1) * N], win.ap()[:, k, t * 128:(t + 1) * 128], hs.ap()[:, k, :],
                     [win.r(), hs.r()], [pM.r()], start=(k == 0), stop=(k == KT - 1), inc=(k == KT - 1 and t == 6))
        K.cp(rws.ap(), pM.ap()[:, 0:7 * N].rearrange("p (t n) -> p t n", t=7), [pM.r()], [rws.r()], eng="act")
        for (c0, c1) in ((0, 512), (512, 896)):
            for k in range(KT):
                K.mm(pA.ap()[0:NS, c0:c1], hs.ap()[:, k, :], win.ap()[:, k, c0:c1], [win.r(), hs.r()], [pA.r()],
                     start=(k == 0), stop=(k == KT - 1))
        K.cp(rwtm.ap(), pA.ap()[0:NS, 0:896], [pA.r()], [rwtm.r()], eng="act")
        K.dma(dr["s_shift"][l], rwtm.ap(), r=[rwtm.r()])
        for t in range(7):
            K.tr(pL.ap()[:, t * N:(t + 1) * N], shs.ap()[:, t * 128:(t + 1) * 128], identf.ap()[0:NS, 0:NS],
                 [shs.r(), identf.r()], [pL.r()], inc=(t == 6))
        K.cp(prevs.ap(), pL.ap()[:, 0:7 * N].rearrange("p (t n) -> p t n", t=7), [pL.r()], [prevs.r()])
        B["_rw_res"] = [rws.r(), prevs.r()]
        for _ in rwkv_prep(K, C, pc, LW, N, rws.ap(), prevs.ap(), B, pB, pL, pM):
            pass
        r, k_, v = B["_rkv"]
        g = lambda n: B[n].ap()
        K.act(g("e1"), g("sig"), AF.Exp, [B["sig"].r()], [B["e1"].r()], scale=-C0)
        K.ts(g("e2"), g("kk"), -1.0, ALU.mult, [B["kk"].r()], [B["e2"].r()])
        K.tt(g("bb"), g("kk"), g("aic"), ALU.mult, [B["kk"].r(), B["aic"].r()], [B["bb"].r()])
        srcs = [(r, B["MX"].r()), (g("e1"), B["e1"].r()), (g("kp"), B["kp"].r()), (v, B["MX"].r()),
                (g("e2"), B["e2"].r()), (g("bb"), B["bb"].r())]
        for q, (ap, res) in enumerate(srcs):
            pp = pA if q < 4 else pB
            for t in range(2):
                col = ((q % 4) * 2 + t) * 128
                K.tr(pp.ap()[0:NS, col:col + 128], ap[:, t, :], identf.ap(), [res, identf.r()], [pp.r()])
        K.cp(pkT.ap()[:, 0:4, :], pA.ap()[0:NS, :].rearrange("p (q n) -> p q n", q=4), [pA.r()], [pkT.r()], eng="act")
        K.cp(pkT.ap()[:, 4:6, :], pB.ap()[0:NS, 0:512].rearrange("p (q n) -> p q n", q=2), [pB.r()], [pkT.r()])
        for q in range(6):
            K.dma(scr.ap()[:, :, q, :], pkT.ap()[:, q, :].rearrange("p (h k) -> p h k", h=4), r=[pkT.r()], w=[scr.r()])
        K.dma(pkh.ap(), scr.ap().rearrange("b h q k -> (b h) q k"), r=[scr.r()], w=[pkh.r()])
        rq, wq, kq, vq, aq, bq = (pkh.ap()[:, i, :] for i in range(6))
        K.tt(tmp.ap(), S.ap(), bc(aq, 1, [64, 64, 64]), ALU.mult, [S.r(), pkh.r()], [tmp.r()])
        K.red(sa.ap(), tmp.ap(), [tmp.r()], [sa.r()])
        K.tt(S.ap(), S.ap(), bc(wq, 1, [64, 64, 64]), ALU.mult, [S.r(), pkh.r()], [S.r()])
        K.tt(tmp.ap(), bc(sa.ap(), 2, [64, 64, 64]), bc(bq, 1, [64, 64, 64]), ALU.mult, [sa.r(), pkh.r()], [tmp.r()])
        K.tt(S.ap(), S.ap(), tmp.ap(), ALU.add, [S.r(), tmp.r()], [S.r()])
        K.tt(tmp.ap(), bc(vq, 2, [64, 64, 64]), bc(kq, 1, [64, 64, 64]), ALU.mult, [pkh.r()], [tmp.r()])
        K.tt(S.ap(), S.ap(), tmp.ap(), ALU.add, [S.r(), tmp.r()], [S.r()])
        K.dma(dr["s_rwkv"][l].rearrange("b h v k -> (b h) (v k)"), S.ap().rearrange("p v k -> p (v k)"), r=[S.r()])
        K.tt(tmp.ap(), S.ap(), bc(rq, 1, [64, 64, 64]), ALU.mult, [S.r(), pkh.r()], [tmp.r()])
        K.red(o.ap(), tmp.ap(), [tmp.r()], [o.r()])
        groupnorm64(K, o.ap().rearrange("p (g e) -> p g e", g=1), 64, 1, gn, [o.r()],
                    on.ap().rearrange("p (g e) -> p g e", g=1), [on.r()])
        K.dma(so.ap().rearrange("b (h v) -> (b h) v", h=4), on.ap(), r=[on.r()], w=[so.r()])
        K.dma(otm.ap(), so.ap(), r=[so.r()], w=[otm.r()])
        K.cp(otb.ap(), otm.ap(), [otm.r()], [otb.r()])
        for t in range(2):
            K.tr(pT.ap()[:, t * N:(t + 1) * N], otb.ap()[:, t * 128:(t + 1) * 128], identb.ap()[0:NS, 0:NS],
                 [otb.r(), identb.r()], [pT.r()], inc=(t == 1))
        rwkv_epilogue(K, C, pc, B, N, pT, yT.ap()[:, 4:6, T:T + NS], xr(yT, 4, range(4, 6)))
        P.barrier()
```
